# Optimizing a Trainium2 kernel written in Bass

```python
import math
import jax
import jax.numpy as jnp
from jax import lax
import numpy as np

D_MODEL = 1024
BATCH = 4
SEQ = 4096
DEPTH = 2

CTX_LEN = 256
GRID_W = 64
N_MIXERS = 4
MIX_W = D_MODEL
GROUP_W = MIX_W // N_MIXERS
HEAD_DIM = 64
ROPE_FREQS = HEAD_DIM // 4
ROPE_BASE = 10000.0
Q_BLOCK = 128
EPS = 1e-6
S5_CH = 16
S5_GROUPS = GROUP_W // S5_CH
S5_STATE = 64
GA_HEADS = GROUP_W // HEAD_DIM
GA_KV = GA_HEADS // 2
SSD_HEADDIM = 64
SSD_HEADS = GROUP_W // SSD_HEADDIM
SSD_NGROUPS = 2
SSD_STATE = 128
SSD_CHUNK = 128
SSD_CONV = 3
SSD_XBC = GROUP_W + 2 * SSD_NGROUPS * SSD_STATE
WA_HEADS = GROUP_W // HEAD_DIM
WA_KV = WA_HEADS // 2
WINDOW = 128
MOE_GROUPS = 4
MOE_PER_GROUP = 8
N_EXPERTS = MOE_GROUPS * MOE_PER_GROUP
MOE_TOPK = 2
D_EXPERT = D_MODEL // 2
MOE_BLOCK = 128
SPLITS = (GROUP_W, GA_HEADS * HEAD_DIM, GA_KV * HEAD_DIM, GA_KV * HEAD_DIM, GROUP_W, SSD_XBC, 2 * SSD_HEADS, WA_HEADS * HEAD_DIM, WA_KV * HEAD_DIM, WA_KV * HEAD_DIM)
D_IN = sum(SPLITS)

kernel_name = 'hybrid_parallel_heads_dit_block'


def rmsnorm(x, g):
    xf = x.astype(jnp.float32)
    y = xf * lax.rsqrt(jnp.mean(xf * xf, axis=-1, keepdims=True) + EPS)
    return (y * g.astype(jnp.float32)).astype(x.dtype)


def _modulate(h, shift, scale):
    return h * (1.0 + scale) + shift


def _heads(t):
    return t.reshape(t.shape[0], t.shape[1], -1, HEAD_DIM)


def _flip(t, rev):
    return jnp.flip(t, axis=1) if rev else t


def rope_tables(n_tok, dtype):
    n_rows = n_tok // GRID_W
    rows = jnp.repeat(jnp.arange(n_rows), GRID_W)
    cols = jnp.tile(jnp.arange(GRID_W), n_rows)
    inv = jnp.power(ROPE_BASE, -jnp.arange(ROPE_FREQS, dtype=jnp.float32) / ROPE_FREQS)
    ang = jnp.stack([rows, cols], axis=-1).astype(jnp.float32)[..., None] * inv
    return jnp.cos(ang).astype(dtype), jnp.sin(ang).astype(dtype)


def apply_rope(x, cos, sin):
    shp = x.shape
    xv = x.reshape(*shp[:-1], 2, 2, ROPE_FREQS)
    rot = jnp.stack([-xv[..., 1, :], xv[..., 0, :]], axis=-2)
    c = cos[:, None, :, None, :]
    s = sin[:, None, :, None, :]
    return (xv * c + rot * s).reshape(shp)


def s5_discretize(lam_re, lam_im, log_dt, b_re, b_im):
    lr = lam_re.astype(jnp.float32)
    li = lam_im.astype(jnp.float32)
    dt = jnp.exp(log_dt.astype(jnp.float32))[:, None]
    mag = jnp.exp(lr * dt)
    a_re = mag * jnp.cos(li * dt)
    a_im = mag * jnp.sin(li * dt)
    den = lr * lr + li * li
    f_re = ((a_re - 1.0) * lr + a_im * li) / den
    f_im = (a_im * lr - (a_re - 1.0) * li) / den
    br = b_re.astype(jnp.float32)
    bi = b_im.astype(jnp.float32)
    bb_re = f_re[..., None] * br - f_im[..., None] * bi
    bb_im = f_re[..., None] * bi + f_im[..., None] * br
    return a_re, a_im, bb_re, bb_im


def _complex_affine_op(e1, e2):
    ar1, ai1, br1, bi1 = e1
    ar2, ai2, br2, bi2 = e2
    return (ar2 * ar1 - ai2 * ai1, ar2 * ai1 + ai2 * ar1,
            ar2 * br1 - ai2 * bi1 + br2, ar2 * bi1 + ai2 * br1 + bi2)


def s5_scan(bu_re, bu_im, a_re, a_im, h0, rev):
    if h0 is not None:
        first = -1 if rev else 0
        bu_re = bu_re.at[:, first].add(a_re * h0[0] - a_im * h0[1])
        bu_im = bu_im.at[:, first].add(a_re * h0[1] + a_im * h0[0])
    ar = jnp.broadcast_to(a_re, bu_re.shape)
    ai = jnp.broadcast_to(a_im, bu_re.shape)
    _, _, hr, hi = lax.associative_scan(_complex_affine_op, (ar, ai, bu_re, bu_im), reverse=rev, axis=1)
    return hr, hi


def _s5_readout(h, c_re, c_im):
    return jnp.einsum('blgp,gcp->blgc', h[0], c_re) - jnp.einsum('blgp,gcp->blgc', h[1], c_im)


def s5_mixer(u_l, u_c, lam_re, lam_im, log_dt, b_re, b_im, c_re, c_im, d_skip, glu_w, glu_b, ctx_out):
    def grp(u):
        return u.astype(jnp.float32).reshape(u.shape[0], u.shape[1], S5_GROUPS, S5_CH)
    ul, uc = grp(u_l), grp(u_c)
    dsk = d_skip.astype(jnp.float32).reshape(S5_GROUPS, S5_CH)
    y_l = ul * dsk
    y_c = uc * dsk
    for d in range(2):
        rev = d == 1
        a_re, a_im, bb_re, bb_im = s5_discretize(lam_re[d], lam_im[d], log_dt[d], b_re[d], b_im[d])
        cr = c_re[d].astype(jnp.float32)
        ci = c_im[d].astype(jnp.float32)
        hc = s5_scan(jnp.einsum('blgc,gpc->blgp', uc, bb_re), jnp.einsum('blgc,gpc->blgp', uc, bb_im), a_re, a_im, None, rev)
        last = 0 if rev else -1
        h_fin = (hc[0][:, last], hc[1][:, last])
        hl = s5_scan(jnp.einsum('blgc,gpc->blgp', ul, bb_re), jnp.einsum('blgc,gpc->blgp', ul, bb_im), a_re, a_im, h_fin, rev)
        y_l = y_l + _s5_readout(hl, cr, ci)
        if ctx_out:
            y_c = y_c + _s5_readout(hc, cr, ci)

    def glu(y, like):
        g = jax.nn.gelu(y.reshape(y.shape[0], y.shape[1], GROUP_W)).astype(like.dtype)
        return g * jax.nn.sigmoid(g @ glu_w + glu_b)
    return glu(y_l, u_l), (glu(y_c, u_c) if ctx_out else None)


def _attend(qb, k, v):
    s = jnp.einsum('bqkgd,bskd->bkgqs', qb, k).astype(jnp.float32) * (HEAD_DIM ** -0.5)
    p = jax.nn.softmax(s, axis=-1).astype(v.dtype)
    return jnp.einsum('bkgqs,bskd->bqkgd', p, v)


def gqa_global(q_l, k_l, v_l, q_c, k_c, v_c, qn_g, kn_g, cos, sin, ctx_out):
    bsz, L = q_l.shape[:2]
    grp = GA_HEADS // GA_KV
    q_l = apply_rope(rmsnorm(q_l, qn_g), cos, sin)
    k_l = apply_rope(rmsnorm(k_l, kn_g), cos, sin)
    q_c = rmsnorm(q_c, qn_g)
    k_c = rmsnorm(k_c, kn_g)
    k_all = jnp.concatenate([k_c, k_l], axis=1)
    v_all = jnp.concatenate([v_c, v_l], axis=1)
    nb = L // Q_BLOCK
    qb = q_l.reshape(bsz, nb, Q_BLOCK, GA_KV, grp, HEAD_DIM).swapaxes(0, 1)
    o = lax.map(lambda qi: _attend(qi, k_all, v_all), qb)
    y_l = o.swapaxes(0, 1).reshape(bsz, L, GROUP_W)
    y_c = None
    if ctx_out:
        lc = q_c.shape[1]
        y_c = _attend(q_c.reshape(bsz, lc, GA_KV, grp, HEAD_DIM), k_c, v_c).reshape(bsz, lc, GROUP_W)
    return y_l, y_c


def depthwise_conv(x, w, b):
    pad = (SSD_CONV - 1) // 2
    y = lax.conv_general_dilated(x, w[:, None, :].astype(x.dtype), window_strides=(1,),
                                 padding=[(pad, SSD_CONV - 1 - pad)],
                                 dimension_numbers=('NWC', 'WIO', 'NWC'),
                                 feature_group_count=x.shape[-1])
    return y + b


def _segsum(a):
    cs = jnp.cumsum(a, axis=-1)
    diff = cs[..., :, None] - cs[..., None, :]
    n = a.shape[-1]
    return jnp.where(jnp.tril(jnp.ones((n, n), dtype=bool)), diff, -jnp.inf)


def ssd_chunked(X, A, Bh, Ch, h0):
    b, l, h, p = X.shape
    n = Bh.shape[-1]
    nc = l // SSD_CHUNK
    X = X.astype(jnp.float32).reshape(b, nc, SSD_CHUNK, h, p)
    Bc = Bh.astype(jnp.float32).reshape(b, nc, SSD_CHUNK, h, n)
    Cc = Ch.astype(jnp.float32).reshape(b, nc, SSD_CHUNK, h, n)
    A = A.astype(jnp.float32).reshape(b, nc, SSD_CHUNK, h).transpose(0, 3, 1, 2)
    A_cum = jnp.cumsum(A, axis=-1)
    scores = jnp.einsum('bclhn,bcshn->bhcls', Cc, Bc) * jnp.exp(_segsum(A))
    y_diag = jnp.einsum('bhcls,bcshp->bclhp', scores, X)
    decay_states = jnp.exp(A_cum[..., -1:] - A_cum)
    states = jnp.einsum('bclhn,bhcl,bclhp->bchpn', Bc, decay_states, X)
    states = jnp.concatenate([h0[:, None], states], axis=1)
    decay_chunk = jnp.exp(_segsum(jnp.pad(A_cum[..., -1], ((0, 0), (0, 0), (1, 0)))))
    states = jnp.einsum('bhzc,bchpn->bzhpn', decay_chunk, states)
    y_off = jnp.einsum('bclhn,bchpn,bhcl->bclhp', Cc, states[:, :-1], jnp.exp(A_cum))
    return (y_diag + y_off).reshape(b, l, h, p), states[:, -1]


def _ssd_dir(x, bm, cm, dt, a, h0, rev):
    y, h_fin = ssd_chunked(_flip(x * dt[..., None], rev), _flip(dt * a, rev), _flip(bm, rev), _flip(cm, rev), h0)
    return _flip(y, rev), h_fin


def ssd_mixer(z_l, xbc_l, dt_l, z_c, xbc_c, dt_c, conv_w, conv_b, dt_bias, a_log, d_skip, norm_g, ctx_out):
    rep = SSD_HEADS // SSD_NGROUPS

    def prep(xbc, dtr):
        xbc = jax.nn.silu(depthwise_conv(xbc, conv_w, conv_b))
        bsz, L = xbc.shape[:2]
        x, bm, cm = jnp.split(xbc, [GROUP_W, GROUP_W + SSD_NGROUPS * SSD_STATE], axis=-1)
        x = x.reshape(bsz, L, SSD_HEADS, SSD_HEADDIM).astype(jnp.float32)
        bm = jnp.repeat(bm.reshape(bsz, L, SSD_NGROUPS, SSD_STATE), rep, axis=2)
        cm = jnp.repeat(cm.reshape(bsz, L, SSD_NGROUPS, SSD_STATE), rep, axis=2)
        dt = jax.nn.softplus(dtr.astype(jnp.float32).reshape(bsz, L, 2, SSD_HEADS) + dt_bias.astype(jnp.float32))
        return x, bm, cm, dt

    xl, bl, cl, dtl = prep(xbc_l, dt_l)
    xc, bc, cc, dtc = prep(xbc_c, dt_c)
    a = -jnp.exp(a_log.astype(jnp.float32))
    dsk = d_skip.astype(jnp.float32)[:, None]
    y_l = xl * dsk
    y_c = xc * dsk
    bsz = xl.shape[0]
    for d in range(2):
        rev = d == 1
        h0 = jnp.zeros((bsz, SSD_HEADS, SSD_HEADDIM, SSD_STATE), jnp.float32)
        yc_d, h_ctx = _ssd_dir(xc, bc, cc, dtc[:, :, d], a[d], h0, rev)
        yl_d, _ = _ssd_dir(xl, bl, cl, dtl[:, :, d], a[d], h_ctx, rev)
        y_l = y_l + yl_d
        if ctx_out:
            y_c = y_c + yc_d

    def gate(y, z):
        y = y.reshape(z.shape).astype(z.dtype)
        return rmsnorm(y * jax.nn.silu(z), norm_g)
    return gate(y_l, z_l), (gate(y_c, z_c) if ctx_out else None)


def _band(t, nb):
    bsz = t.shape[0]
    tp = jnp.pad(t.reshape(bsz, nb, Q_BLOCK, WA_KV, HEAD_DIM), ((0, 0), (1, 1), (0, 0), (0, 0), (0, 0)))
    return jnp.concatenate([tp[:, :-2], tp[:, 1:-1], tp[:, 2:]], axis=2)


def gqa_window(q_l, k_l, v_l, q_c, k_c, v_c, sink, cos, sin, ctx_out):
    bsz, L = q_l.shape[:2]
    lc = k_c.shape[1]
    grp = WA_HEADS // WA_KV
    scale = HEAD_DIM ** -0.5
    nb = L // Q_BLOCK
    q_l = apply_rope(q_l, cos, sin)
    k_l = apply_rope(k_l, cos, sin)
    qb = q_l.reshape(bsz, nb, Q_BLOCK, WA_KV, grp, HEAD_DIM)
    kb, vb = _band(k_l, nb), _band(v_l, nb)
    qpos = jnp.arange(L).reshape(nb, Q_BLOCK)
    kpos = (jnp.arange(nb)[:, None] - 1) * Q_BLOCK + jnp.arange(3 * Q_BLOCK)[None, :]
    valid = ((jnp.abs(qpos[:, :, None] - kpos[:, None, :]) <= WINDOW)
             & (kpos >= 0)[:, None, :] & (kpos < L)[:, None, :])
    s_band = jnp.einsum('bnqkgd,bnskd->bnkgqs', qb, kb).astype(jnp.float32) * scale
    s_band = jnp.where(valid[None, :, None, None], s_band, -jnp.inf)
    s_ctx = jnp.einsum('bnqkgd,bskd->bnkgqs', qb, k_c).astype(jnp.float32) * scale
    sink_f = sink.astype(jnp.float32).reshape(WA_KV, grp, 1, 1)
    sink_l = jnp.broadcast_to(sink_f, s_ctx.shape[:-1] + (1,))
    p = jax.nn.softmax(jnp.concatenate([sink_l, s_ctx, s_band], axis=-1), axis=-1).astype(v_l.dtype)
    o = (jnp.einsum('bnkgqs,bskd->bnqkgd', p[..., 1:1 + lc], v_c)
         + jnp.einsum('bnkgqs,bnskd->bnqkgd', p[..., 1 + lc:], vb))
    y_l = o.reshape(bsz, L, GROUP_W)
    y_c = None
    if ctx_out:
        qc = q_c.reshape(bsz, lc, WA_KV, grp, HEAD_DIM)
        s_cc = jnp.einsum('bqkgd,bskd->bkgqs', qc, k_c).astype(jnp.float32) * scale
        sink_c = jnp.broadcast_to(sink_f, s_cc.shape[:-1] + (1,))
        pc = jax.nn.softmax(jnp.concatenate([sink_c, s_cc], axis=-1), axis=-1).astype(v_c.dtype)
        y_c = jnp.einsum('bkgqs,bskd->bqkgd', pc[..., 1:], v_c).reshape(bsz, lc, GROUP_W)
    return y_l, y_c


def _swiglu(xb, wg, wu, wd):
    return (jax.nn.silu(xb @ wg) * (xb @ wu)) @ wd


def hier_moe(xf, coarse_w, coarse_b, fine_w, fine_b, w_gate, w_up, w_down):
    T, dm = xf.shape
    pc = jax.nn.softmax((xf @ coarse_w + coarse_b).astype(jnp.float32), axis=-1)
    pg, g = lax.top_k(pc, 1)
    fl = (xf @ fine_w + fine_b).astype(jnp.float32).reshape(T, MOE_GROUPS, MOE_PER_GROUP)
    fl = jnp.take_along_axis(fl, g[:, :, None], axis=1)[:, 0]
    tv, ti = lax.top_k(fl, MOE_TOPK)
    w = pg * jax.nn.softmax(tv, axis=-1)
    flat_e = (g * MOE_PER_GROUP + ti).reshape(-1)
    flat_w = w.reshape(-1)
    flat_tok = jnp.repeat(jnp.arange(T), MOE_TOPK)
    n_slots = T * MOE_TOPK
    order = jnp.argsort(flat_e)
    se, stok, sw = flat_e[order], flat_tok[order], flat_w[order]
    counts = jnp.zeros((N_EXPERTS,), jnp.int32).at[flat_e].add(1)
    starts = jnp.cumsum(counts) - counts
    pcounts = (counts + MOE_BLOCK - 1) // MOE_BLOCK * MOE_BLOCK
    pends = jnp.cumsum(pcounts)
    pstarts = pends - pcounts
    dest = pstarts[se] + jnp.arange(n_slots) - starts[se]
    n_blocks = -(-n_slots // MOE_BLOCK) + N_EXPERTS
    buf = jnp.zeros((n_blocks * MOE_BLOCK, dm), xf.dtype).at[dest].set(xf[stok])
    blk_e = jnp.minimum(jnp.searchsorted(pends, jnp.arange(n_blocks) * MOE_BLOCK, side='right'), N_EXPERTS - 1)
    out = lax.map(lambda a: _swiglu(a[0], w_gate[a[1]], w_up[a[1]], w_down[a[1]]),
                  (buf.reshape(n_blocks, MOE_BLOCK, dm), blk_e))
    out = out.reshape(-1, dm)[dest] * sw[:, None].astype(xf.dtype)
    return jax.ops.segment_sum(out, stok, num_segments=T)


def hybrid_layer(xl, xc, c, c_ctx, cos, sin, ada_w, ada_b, norm1_g, norm2_g, w_in, w_out,
                 s5_lam_re, s5_lam_im, s5_log_dt, s5_b_re, s5_b_im, s5_c_re, s5_c_im, s5_d, s5_glu_w, s5_glu_b,
                 ga_qn_g, ga_kn_g, ssd_conv_w, ssd_conv_b, ssd_dt_bias, ssd_a_log, ssd_d, ssd_norm_g, wa_sink,
                 moe_coarse_w, moe_coarse_b, moe_fine_w, moe_fine_b, moe_w_gate, moe_w_up, moe_w_down, ctx_out):
    dm = xl.shape[-1]
    mod_l = (jax.nn.silu(c) @ ada_w + ada_b)[:, None, :]
    mod_c = jax.nn.silu(c_ctx) @ ada_w + ada_b
    sh1, sc1, g1, sh2, sc2, g2 = jnp.split(mod_l, 6, axis=-1)
    csh1, csc1, cg1, csh2, csc2, cg2 = jnp.split(mod_c, 6, axis=-1)
    cuts = [int(v) for v in np.cumsum(SPLITS)[:-1]]
    pl = jnp.split(_modulate(rmsnorm(xl, norm1_g), sh1, sc1) @ w_in, cuts, axis=-1)
    pc = jnp.split(_modulate(rmsnorm(xc, norm1_g), csh1, csc1) @ w_in, cuts, axis=-1)

    a_l, a_c = s5_mixer(pl[0], pc[0], s5_lam_re, s5_lam_im, s5_log_dt, s5_b_re, s5_b_im, s5_c_re, s5_c_im,
                        s5_d, s5_glu_w, s5_glu_b, ctx_out)
    b_l, b_c = gqa_global(_heads(pl[1]), _heads(pl[2]), _heads(pl[3]), _heads(pc[1]), _heads(pc[2]), _heads(pc[3]),
                          ga_qn_g, ga_kn_g, cos, sin, ctx_out)
    m_l, m_c = ssd_mixer(pl[4], pl[5], pl[6], pc[4], pc[5], pc[6], ssd_conv_w, ssd_conv_b, ssd_dt_bias,
                         ssd_a_log, ssd_d, ssd_norm_g, ctx_out)
    w_l, w_c = gqa_window(_heads(pl[7]), _heads(pl[8]), _heads(pl[9]), _heads(pc[7]), _heads(pc[8]), _heads(pc[9]),
                          wa_sink, cos, sin, ctx_out)

    xl = xl + g1 * (jnp.concatenate([a_l, b_l, m_l, w_l], axis=-1) @ w_out)
    hl = _modulate(rmsnorm(xl, norm2_g), sh2, sc2)
    n_lat = hl.shape[0] * hl.shape[1]
    if ctx_out:
        xc = xc + cg1 * (jnp.concatenate([a_c, b_c, m_c, w_c], axis=-1) @ w_out)
        hc = _modulate(rmsnorm(xc, norm2_g), csh2, csc2)
        tokens = jnp.concatenate([hl.reshape(-1, dm), hc.reshape(-1, dm)], axis=0)
    else:
        tokens = hl.reshape(-1, dm)
    f = hier_moe(tokens, moe_coarse_w, moe_coarse_b, moe_fine_w, moe_fine_b, moe_w_gate, moe_w_up, moe_w_down)
    xl = xl + g2 * f[:n_lat].reshape(xl.shape)
    if ctx_out:
        xc = xc + cg2 * f[n_lat:].reshape(xc.shape)
        return xl, xc
    return xl, None


def setup_inputs(seed: int = 0) -> dict:
    key = jax.random.key(seed)
    ks = iter(jax.random.split(key, 64))
    f32 = jnp.float32
    Dm = D_MODEL

    def nrm(shape, s=1.0):
        return jax.random.normal(next(ks), shape, f32) * s

    def unif(shape, lo, hi):
        return jax.random.uniform(next(ks), shape, f32, lo, hi)

    inp = {}
    inp['x'] = nrm((BATCH, SEQ, Dm))
    inp['c'] = nrm((BATCH, Dm))
    inp['ctx'] = nrm((BATCH, CTX_LEN, Dm))
    inp['c_ctx'] = nrm((Dm,))
    inp['ada_w'] = nrm((DEPTH, Dm, 6 * Dm), 0.5 * Dm ** -0.5)
    inp['ada_b'] = nrm((DEPTH, 6 * Dm), 0.02)
    inp['norm1_g'] = 1.0 + nrm((DEPTH, Dm), 0.05)
    inp['norm2_g'] = 1.0 + nrm((DEPTH, Dm), 0.05)
    inp['w_in'] = nrm((DEPTH, Dm, D_IN), Dm ** -0.5)
    inp['w_out'] = nrm((DEPTH, MIX_W, Dm), MIX_W ** -0.5)
    sshape = (DEPTH, 2, S5_GROUPS, S5_STATE)
    inp['s5_lam_re'] = -0.5 + nrm(sshape, 0.01)
    inp['s5_lam_im'] = jnp.pi * jnp.arange(S5_STATE, dtype=f32) + nrm(sshape, 0.01)
    inp['s5_log_dt'] = unif((DEPTH, 2, S5_GROUPS), math.log(1e-3), math.log(1e-1))
    inp['s5_b_re'] = nrm((DEPTH, 2, S5_GROUPS, S5_STATE, S5_CH), (2 * S5_CH) ** -0.5)
    inp['s5_b_im'] = nrm((DEPTH, 2, S5_GROUPS, S5_STATE, S5_CH), (2 * S5_CH) ** -0.5)
    inp['s5_c_re'] = nrm((DEPTH, 2, S5_GROUPS, S5_CH, S5_STATE), S5_STATE ** -0.5)
    inp['s5_c_im'] = nrm((DEPTH, 2, S5_GROUPS, S5_CH, S5_STATE), S5_STATE ** -0.5)
    inp['s5_d'] = nrm((DEPTH, GROUP_W))
    inp['s5_glu_w'] = nrm((DEPTH, GROUP_W, GROUP_W), GROUP_W ** -0.5)
    inp['s5_glu_b'] = nrm((DEPTH, GROUP_W), 0.02)
    inp['ga_qn_g'] = 1.0 + nrm((DEPTH, HEAD_DIM), 0.05)
    inp['ga_kn_g'] = 1.0 + nrm((DEPTH, HEAD_DIM), 0.05)
    inp['ssd_conv_w'] = nrm((DEPTH, SSD_CONV, SSD_XBC), SSD_CONV ** -0.5)
    inp['ssd_conv_b'] = nrm((DEPTH, SSD_XBC), 0.02)
    dt0 = jnp.exp(unif((DEPTH, 2, SSD_HEADS), math.log(1e-3), math.log(1e-1)))
    inp['ssd_dt_bias'] = dt0 + jnp.log(-jnp.expm1(-dt0))
    inp['ssd_a_log'] = jnp.log(unif((DEPTH, 2, SSD_HEADS), 1.0, 16.0))
    inp['ssd_d'] = 1.0 + nrm((DEPTH, SSD_HEADS), 0.1)
    inp['ssd_norm_g'] = 1.0 + nrm((DEPTH, GROUP_W), 0.05)
    inp['wa_sink'] = nrm((DEPTH, WA_HEADS))
    inp['moe_coarse_w'] = nrm((DEPTH, Dm, MOE_GROUPS), Dm ** -0.5)
    inp['moe_coarse_b'] = nrm((DEPTH, MOE_GROUPS), 0.01)
    inp['moe_fine_w'] = nrm((DEPTH, Dm, N_EXPERTS), Dm ** -0.5)
    inp['moe_fine_b'] = nrm((DEPTH, N_EXPERTS), 0.01)
    inp['moe_w_gate'] = nrm((DEPTH, N_EXPERTS, Dm, D_EXPERT), Dm ** -0.5)
    inp['moe_w_up'] = nrm((DEPTH, N_EXPERTS, Dm, D_EXPERT), Dm ** -0.5)
    inp['moe_w_down'] = nrm((DEPTH, N_EXPERTS, D_EXPERT, Dm), D_EXPERT ** -0.5)
    inp['final_g'] = 1.0 + nrm((Dm,), 0.05)
    return inp


def reference(x, c, ctx, c_ctx, ada_w, ada_b, norm1_g, norm2_g, w_in, w_out,
              s5_lam_re, s5_lam_im, s5_log_dt, s5_b_re, s5_b_im, s5_c_re, s5_c_im, s5_d, s5_glu_w, s5_glu_b,
              ga_qn_g, ga_kn_g, ssd_conv_w, ssd_conv_b, ssd_dt_bias, ssd_a_log, ssd_d, ssd_norm_g, wa_sink,
              moe_coarse_w, moe_coarse_b, moe_fine_w, moe_fine_b, moe_w_gate, moe_w_up, moe_w_down, final_g):
    cos, sin = rope_tables(x.shape[1], x.dtype)
    xl, xc = x, ctx
    for i in range(DEPTH):
        xl, xc = hybrid_layer(
            xl, xc, c, c_ctx, cos, sin, ada_w[i], ada_b[i], norm1_g[i], norm2_g[i], w_in[i], w_out[i],
            s5_lam_re[i], s5_lam_im[i], s5_log_dt[i], s5_b_re[i], s5_b_im[i], s5_c_re[i], s5_c_im[i],
            s5_d[i], s5_glu_w[i], s5_glu_b[i], ga_qn_g[i], ga_kn_g[i], ssd_conv_w[i], ssd_conv_b[i],
            ssd_dt_bias[i], ssd_a_log[i], ssd_d[i], ssd_norm_g[i], wa_sink[i],
            moe_coarse_w[i], moe_coarse_b[i], moe_fine_w[i], moe_fine_b[i], moe_w_gate[i], moe_w_up[i],
            moe_w_down[i], i < DEPTH - 1)
    return rmsnorm(xl, final_g)
```

```python
import numpy as np
from contextlib import ExitStack
import concourse.bass as bass
import concourse.mybir as mybir
from concourse.bass_utils import run_bass_kernel_spmd

F32 = mybir.dt.float32
F32R = mybir.dt.float32r
I32 = mybir.dt.int32
AF = mybir.ActivationFunctionType
ALU = mybir.AluOpType
AX = mybir.AxisListType

LC, L, T, D = 256, 4096, 4352, 1024
NT = T // 128
EPS = 1e-6
CHUNKS = [(0, 256)] + [(256 + 512 * i, 512) for i in range(8)]
PERM = np.concatenate([np.arange(256, 768), np.arange(1800, 2312), np.arange(768, 1024), np.arange(1792, 1800),
                       np.arange(0, 256), np.arange(1024, 1792)])
TM_COLS = 1288
FM_OFF = 1288

ENGS = ["pe", "act", "dve", "pool", "sp"]
N_DMA_SEMS = 8


class Prog:
    def __init__(self, nc, same_engine_sync=True):
        self.nc = nc
        self.st = ExitStack()
        self.same = same_engine_sync
        self.ops = {e: [] for e in ENGS}
        self.sems = {}
        self.cnt = {}
        for e in ["pe", "act", "dve", "pool"]:
            self.sems[e] = self.st.enter_context(nc.semaphore("s_" + e))
            self.cnt[e] = 0
        for q in ["sp", "pool"]:
            for i in range(N_DMA_SEMS):
                nm = "d_%s%d" % (q, i)
                self.sems[nm] = self.st.enter_context(nc.semaphore(nm))
                self.cnt[nm] = 0
        self.drr = {"sp": 0, "pool": 0}
        self.waited = {e: {} for e in ENGS}
        self.last_w = {}
        self.readers = {}
        self.n_ops = 0
        self.final = []
        self.uid = 0
        self.pst = None
        self.barrier = {e: {} for e in ENGS}
        self.children = {}
        self.known = set()
        self.psum_keys = set()

    def begin_phase(self):
        self.pst = ExitStack()

    def end_phase(self):
        self.pst.close()
        self.pst = None
        snap = {s: v for s, v in self.cnt.items() if v > 0}
        for e in ENGS:
            self.barrier[e] = dict(snap)

    def sb(self, name, shape, dtype=F32):
        return (self.pst or self.st).enter_context(self.nc.sbuf_tensor(name, list(shape), dtype))

    def ps(self, name, shape, dtype=F32):
        return (self.pst or self.st).enter_context(self.nc.psum_tensor(name, list(shape), dtype))

    def _related(self, k):
        rel = [k]
        parts = k.split("/")
        for n in range(1, len(parts)):
            rel.append("/".join(parts[:n]))
        rel.extend(self.children.get(k, ()))
        return rel

    def _register(self, k):
        if k in self.known:
            return
        self.known.add(k)
        parts = k.split("/")
        for n in range(1, len(parts)):
            self.children.setdefault("/".join(parts[:n]), set()).add(k)

    def _deps(self, eng, reads, writes):
        deps = {}

        def add(s, v):
            if v > deps.get(s, 0):
                deps[s] = v

        for k in list(reads) + list(writes):
            self._register(k)
        for k in reads:
            for kk in self._related(k):
                t = self.last_w.get(kk)
                if t is not None:
                    add(*t)
        for k in writes:
            for kk in self._related(k):
                t = self.last_w.get(kk)
                if t is not None:
                    add(*t)
                for s_, v_ in self.readers.get(kk, {}).items():
                    add(s_, v_)
        if self.barrier[eng]:
            for s_, v_ in self.barrier[eng].items():
                add(s_, v_)
            self.barrier[eng] = {}
        out = []
        for s, v in deps.items():
            if s == eng and (eng == "pe" or not self.same):
                continue
            if v > self.waited[eng].get(s, 0):
                self.waited[eng][s] = v
                out.append((s, v))
        return out

    def _commit(self, tok, reads, writes):
        for k in writes:
            self.last_w[k] = tok
            self.readers[k] = {}
        for k in reads:
            if k in writes:
                continue
            rd = self.readers.setdefault(k, {})
            if tok[1] > rd.get(tok[0], 0):
                rd[tok[0]] = tok[1]

    def op(self, eng, fn, reads=(), writes=()):
        pr = [k for k in reads if k in self.psum_keys]
        if pr:
            reads = [k for k in reads if k not in self.psum_keys]
            writes = list(writes) + pr
        waits = self._deps(eng, reads, writes)
        self.cnt[eng] += 1
        tok = (eng, self.cnt[eng])
        self.ops[eng].append((waits, fn, tok, 1))
        self._commit(tok, reads, writes)
        self.n_ops += 1
        return tok

    def u(self, base):
        self.uid += 1
        return "%s/%d" % (base, self.uid)

    def i(self, eng, name, reads=(), writes=(), **kw):
        return self.op(eng, lambda e: getattr(e, name)(**kw), reads, writes)

    def d(self, out, in_, reads=(), writes=(), q="sp", final=False):
        return self.dma(lambda e: e.dma_start(out=out, in_=in_), reads, writes, q=q, final=final)

    def dma(self, fn, reads=(), writes=(), q="sp", final=False):
        waits = self._deps(q, reads, writes)
        i = self.drr[q]
        self.drr[q] = (i + 1) % N_DMA_SEMS
        nm = "d_%s%d" % (q, i)
        prev = self.cnt[nm]
        if prev > self.waited[q].get(nm, 0):
            self.waited[q][nm] = prev
            waits.append((nm, prev))
        self.cnt[nm] += 16
        tok = (nm, self.cnt[nm])
        self.ops[q].append((waits, fn, tok, 16))
        self._commit(tok, reads, writes)
        self.n_ops += 1
        if final:
            self.final.append(tok)
        return tok

    def emit(self):
        nc = self.nc
        fin = list(self.final)
        engmap = {"pe": "tensor", "act": "scalar", "dve": "vector", "pool": "gpsimd", "sp": "sync"}
        with nc.Block() as block:
            for e in ENGS:
                lst = self.ops[e]
                extra = fin if e == "sp" else []
                if not lst and not extra:
                    continue

                def body(engine, lst=lst, extra=extra):
                    for (waits, fn, tok, amt) in lst:
                        for (s, v) in waits:
                            engine.wait_ge(self.sems[s], v)
                        fn(engine).then_inc(self.sems[tok[0]], amt)
                    for (s, v) in extra:
                        engine.wait_ge(self.sems[s], v)

                getattr(block, engmap[e])(body)
        self.st.close()


class Rot:
    def __init__(self, P, name, shape, dtype=F32, n=2, psum=False):
        self.bufs = [(P.ps if psum else P.sb)("%s%d" % (name, i), shape, dtype) for i in range(n)]
        self.keys = ["%s%d" % (name, i) for i in range(n)]
        self.i = 0
        if psum:
            P.psum_keys.update(self.keys)

    def next(self):
        j = self.i % len(self.bufs)
        self.i += 1
        return self.bufs[j], self.keys[j]


class Dram:
    def __init__(self, nc, ext_in=(), ext_out=()):
        self.nc, self.t = nc, {}
        self.ext_in, self.ext_out = set(ext_in), set(ext_out)

    def get(self, name, shape=None, dtype=F32, kind=None):
        if name not in self.t:
            if kind is None:
                kind = "ExternalInput" if name in self.ext_in else ("ExternalOutput" if name in self.ext_out else "Internal")
            self.t[name] = self.nc.dram_tensor(name, list(shape), dtype, kind=kind).ap()
        return self.t[name]


def phase_mod(P, Dm, l):
    nc = P.nc
    cvec = Dm.get("cvec", [128, 8, 2], kind="ExternalInput")
    ada_w = Dm.get("ada_w%d" % l, [128, 8, 6144], kind="ExternalInput")
    ada_b = Dm.get("ada_b%d" % l, [1, 6144], kind="ExternalInput")
    modrow = Dm.get("modrow%d" % l, [2, 6144])
    pf = "m%d_" % l
    cv = P.sb(pf + "cv", [128, 8, 2])
    sg = P.sb(pf + "sg", [128, 8, 2])
    sc = P.sb(pf + "sc", [128, 8, 128], F32R)
    ab = P.sb(pf + "ab", [2, 6144])
    mr = P.sb(pf + "mr", [2, 6144])
    wch = Rot(P, pf + "w", [128, 8, 512], F32R, n=2)
    pm = Rot(P, pf + "pm", [128, 512], F32, n=2, psum=True)
    P.dma(lambda e: e.dma_start(out=cv[:], in_=cvec[:, :, :]), writes=[pf + "cv"])
    P.dma(lambda e: e.dma_start(out=ab[:], in_=ada_b[0:1, :].to_broadcast([2, 6144])), writes=[pf + "ab"])
    P.op("act", lambda e: e.activation(out=sg[:], in_=cv[:], func=AF.Sigmoid), reads=[pf + "cv"], writes=[pf + "sg"])
    P.op("dve", lambda e: e.memset(sc[:].bitcast(F32), 0.0), writes=[pf + "sc"])
    P.op("dve", lambda e: e.tensor_tensor(out=sc[:, :, 0:2], in0=cv[:], in1=sg[:], op=ALU.mult), reads=[pf + "cv", pf + "sg"], writes=[pf + "sc"])
    for j in range(12):
        w, wk = wch.next()
        P.dma(lambda e, w=w, j=j: e.dma_start(out=w[:], in_=ada_w[:, :, j * 512:(j + 1) * 512]), writes=[wk], q="pool")
        pt, pk = pm.next()
        for k in range(8):
            P.op("pe", lambda e, w=w, pt=pt, k=k: e.matmul(pt[:], sc[:, k, :], w[:, k, :], start=(k == 0), stop=(k == 7)),
                 reads=[wk, pf + "sc"], writes=[pk])
        P.op("dve", lambda e, pt=pt, j=j: e.tensor_tensor(out=mr[:, j * 512:(j + 1) * 512], in0=pt[0:2, :], in1=ab[:, j * 512:(j + 1) * 512], op=ALU.add),
             reads=[pk, pf + "ab"], writes=[pf + "mr"])
    P.dma(lambda e: e.dma_start(out=modrow[:, :], in_=mr[:]), reads=[pf + "mr"], writes=["modrow%d" % l])


def load_bcast(P, dst, dkey, src_row, reads=()):
    n = src_row.shape[-1]
    P.dma(lambda e: e.dma_start(out=dst, in_=src_row.to_broadcast([128, n])), reads=list(reads), writes=[dkey])


def phase_a(P, Dm, l, src_name, chunks=None, stop=99):
    nc = P.nc
    pf = "a%d_" % l
    xsrc = Dm.get(src_name, [T, D])
    w_in = Dm.get("w_in%d" % l, [128, 8, 2312], kind="ExternalInput")
    modrow = Dm.get("modrow%d" % l, [2, 6144])
    n1g = Dm.get("norm1_g%d" % l, [1, D], kind="ExternalInput")
    qkg = Dm.get("qkg%d" % l, [1, 384], kind="ExternalInput")
    dtb = Dm.get("dtb%d" % l, [1, 8], kind="ExternalInput")
    ropec = Dm.get("rope_cos", [L, 384], kind="ExternalInput")
    ropes = Dm.get("rope_sin", [L, 384], kind="ExternalInput")
    ident_d = Dm.get("ident", [128, 128], kind="ExternalInput")
    FMT = Dm.get("FMT", [1024, T])
    QKT = Dm.get("QKT", [768, T])
    TMS = Dm.get("TMS", [T, 520])
    mk = "modrow%d" % l

    ident = P.sb(pf + "ident", [128, 128])
    P.dma(lambda e: e.dma_start(out=ident[:], in_=ident_d[:, :]), writes=[pf + "ident"])
    win = P.sb(pf + "win", [128, 8, 2312], F32R)
    for k in range(8):
        P.dma(lambda e, k=k: e.dma_start(out=win[:, k, :], in_=w_in[:, k, :]), writes=[pf + "win/%d" % k], q="pool")
    wkeys = [pf + "win/%d" % k for k in range(8)]
    G = [P.sb(pf + "G%d" % r, [128, D]) for r in range(2)]
    SH = [P.sb(pf + "SH%d" % r, [128, D]) for r in range(2)]
    gn = P.sb(pf + "gn", [128, D])
    load_bcast(P, gn[:], pf + "gn", n1g[0:1, :])
    for r in range(2):
        load_bcast(P, SH[r][:], pf + "SH%d" % r, modrow[r:r + 1, 0:1024], reads=[mk])
        load_bcast(P, G[r][:], pf + "G%d" % r, modrow[r:r + 1, 1024:2048], reads=[mk])
        P.op("dve", lambda e, r=r: e.scalar_tensor_tensor(out=G[r][:], in0=G[r][:], scalar=1.0, in1=gn[:], op0=ALU.add, op1=ALU.mult),
             reads=[pf + "G%d" % r, pf + "gn"], writes=[pf + "G%d" % r])
    qkgb = P.sb(pf + "qkgb", [128, 384])
    load_bcast(P, qkgb[:], pf + "qkgb", qkg[0:1, :])
    dtbb = P.sb(pf + "dtbb", [128, 8])
    load_bcast(P, dtbb[:], pf + "dtbb", dtb[0:1, :])
    epsc = P.sb(pf + "eps", [128, 1])
    P.op("dve", lambda e: e.memset(epsc[:], EPS), writes=[pf + "eps"])
    onec = P.sb(pf + "one", [128, 1])
    P.op("dve", lambda e: e.memset(onec[:], 1.0), writes=[pf + "one"])

    xt_r = Rot(P, pf + "xt", [128, D], n=3)
    junk = P.sb(pf + "junk", [128, D])
    st_r = Rot(P, pf + "st", [128, 32], n=3)
    h_r = Rot(P, pf + "h", [128, D], n=2)
    tp_r = Rot(P, pf + "tp", [128, 1024], n=1, psum=True)
    hT_r = Rot(P, pf + "hT", [128, 8, 512], F32R, n=2)
    pj_r = Rot(P, pf + "pj", [128, 512], n=3, psum=True)
    qk_r = Rot(P, pf + "qk", [128, 12, 64], n=2)
    sq_r = Rot(P, pf + "sq", [128, 6, 64], n=2)
    rp_r = Rot(P, pf + "rp", [128, 24, 2, 16], n=2)
    tmp_r = Rot(P, pf + "tmp", [128, 24, 16], n=2)
    cs_r = Rot(P, pf + "cs", [128, 2, 384], n=2)
    tq_r = Rot(P, pf + "tq", [128, 1024], n=1, psum=True)
    qT_r = Rot(P, pf + "qT", [128, 6, 128], n=2)
    tm_r = Rot(P, pf + "tm", [128, 520], n=2)
    fm_r = Rot(P, pf + "fm", [128, 512], n=1, psum=True)
    fo_r = Rot(P, pf + "fo", [128, 512], n=3)

    for (c0, cn) in (chunks or CHUNKS):
        hT, hTk = hT_r.next()
        ntile = cn // 128
        for i in range(ntile):
            t0 = c0 + i * 128
            is_ctx = t0 < LC
            r = 1 if is_ctx else 0
            xt, xk = xt_r.next()
            P.dma(lambda e, xt=xt, t0=t0: e.dma_start(out=xt[:], in_=xsrc[t0:t0 + 128, :]), reads=[src_name], writes=[xk])
            st, sk = st_r.next()
            P.op("act", lambda e, xt=xt, st=st: e.activation(out=junk[:], in_=xt[:], func=AF.Square, accum_out=st[:, 0:1]),
                 reads=[xk], writes=[pf + "junk", sk])
            P.op("act", lambda e, st=st: e.activation(out=st[:, 1:2], in_=st[:, 0:1], func=AF.Sqrt, bias=epsc[:], scale=1.0 / D),
                 reads=[sk, pf + "eps"], writes=[sk])
            P.op("dve", lambda e, st=st: e.reciprocal(out=st[:, 2:3], in_=st[:, 1:2]), reads=[sk], writes=[sk])
            h, hk = h_r.next()
            P.op("dve", lambda e, xt=xt, st=st, h=h, r=r: e.scalar_tensor_tensor(out=h[:], in0=xt[:], scalar=st[:, 2:3], in1=G[r][:], op0=ALU.mult, op1=ALU.mult),
                 reads=[xk, sk, pf + "G%d" % r], writes=[hk])
            P.op("pool", lambda e, h=h, r=r: e.tensor_tensor(out=h[:], in0=h[:], in1=SH[r][:], op=ALU.add),
                 reads=[hk, pf + "SH%d" % r], writes=[hk])
            if stop == 1:
                continue
            tp, tpk = tp_r.next()
            for k in range(8):
                P.op("pe", lambda e, h=h, tp=tp, k=k: e.transpose(tp[:, k * 128:(k + 1) * 128], h[:, k * 128:(k + 1) * 128], ident[:]),
                     reads=[hk, pf + "ident"], writes=[tpk])
            P.op("act", lambda e, hT=hT, tp=tp, i=i: e.activation(out=hT[:, :, i * 128:(i + 1) * 128], in_=tp[:].rearrange("p (k t) -> p k t", k=8), func=AF.Copy),
                 reads=[tpk], writes=[hTk + "/%d" % i])
            if stop == 2:
                continue
            pjs = []
            for (o, n) in [(0, 512), (512, 512), (1024, 264)]:
                pj, pjk = pj_r.next()
                for k in range(8):
                    P.op("pe", lambda e, pj=pj, hT=hT, k=k, o=o, n=n, i=i: e.matmul(pj[:, 0:n], hT[:, k, i * 128:(i + 1) * 128], win[:, k, o:o + n], start=(k == 0), stop=(k == 7)),
                         reads=[hTk + "/%d" % i, wkeys[k]], writes=[pjk])
                pjs.append((pj, pjk))
            (pA, pAk), (pB, pBk), (pC, pCk) = pjs
            if stop == 3:
                continue
            qk, qkk = qk_r.next()
            sq, sqk = sq_r.next()
            pA6 = pA[:, 0:384].rearrange("p (h d) -> p h d", h=6)
            P.op("act", lambda e, sq=sq, pA6=pA6: e.activation(out=sq[:], in_=pA6, func=AF.Square), reads=[pAk], writes=[sqk])
            P.op("dve", lambda e, sq=sq, st=st: e.tensor_reduce(out=st[:, 4:10], in_=sq[:], axis=AX.X, op=ALU.add), reads=[sqk], writes=[sk])
            P.op("act", lambda e, st=st: e.activation(out=st[:, 4:10], in_=st[:, 4:10], func=AF.Sqrt, bias=epsc[:], scale=1.0 / 64), reads=[sk, pf + "eps"], writes=[sk])
            P.op("dve", lambda e, st=st: e.reciprocal(out=st[:, 10:16], in_=st[:, 4:10]), reads=[sk], writes=[sk])
            P.op("dve", lambda e, sq=sq, pA6=pA6, st=st: e.tensor_tensor(out=sq[:], in0=pA6, in1=st[:, 10:16].unsqueeze(2).to_broadcast([128, 6, 64]), op=ALU.mult),
                 reads=[pAk, sk], writes=[sqk])
            P.op("pool", lambda e, sq=sq, qk=qk: e.tensor_tensor(out=qk[:, 0:6, :], in0=sq[:], in1=qkgb[:].rearrange("p (h d) -> p h d", h=6), op=ALU.mult),
                 reads=[sqk, pf + "qkgb"], writes=[qkk + "/a"])
            P.op("act", lambda e, qk=qk, pB=pB: e.activation(out=qk[:, 6:12, :], in_=pB[:, 0:384].rearrange("p (h d) -> p h d", h=6), func=AF.Copy),
                 reads=[pBk], writes=[qkk + "/b"])
            if stop == 4:
                continue
            tm, tmk = tm_r.next()
            P.op("act", lambda e, tm=tm, pA=pA: e.activation(out=tm[:, 0:128], in_=pA[:, 384:512], func=AF.Copy), reads=[pAk], writes=[tmk + "/a"])
            P.op("act", lambda e, tm=tm, pB=pB: e.activation(out=tm[:, 128:256], in_=pB[:, 384:512], func=AF.Copy), reads=[pBk], writes=[tmk + "/b"])
            P.op("act", lambda e, tm=tm, pC=pC: e.activation(out=tm[:, 256:512], in_=pC[:, 0:256], func=AF.Copy), reads=[pCk], writes=[tmk + "/c"])
            P.op("dve", lambda e, tm=tm, pC=pC: e.tensor_tensor(out=tm[:, 512:520], in0=pC[:, 256:264], in1=dtbb[:], op=ALU.add), reads=[pCk, pf + "dtbb"], writes=[tmk + "/d"])
            P.op("act", lambda e, st=st, tm=tm: e.activation(out=st[:, 16:24], in_=tm[:, 512:520], func=AF.Abs), reads=[tmk + "/d", sk], writes=[sk])
            P.op("act", lambda e, st=st: e.activation(out=st[:, 16:24], in_=st[:, 16:24], func=AF.Exp, scale=-1.0), reads=[sk], writes=[sk])
            P.op("act", lambda e, st=st: e.activation(out=st[:, 16:24], in_=st[:, 16:24], func=AF.Ln, bias=onec[:]), reads=[sk], writes=[sk])
            P.op("dve", lambda e, st=st, tm=tm: e.scalar_tensor_tensor(out=tm[:, 512:520], in0=tm[:, 512:520], scalar=0.0, in1=st[:, 16:24], op0=ALU.max, op1=ALU.add),
                 reads=[tmk + "/d", sk], writes=[tmk + "/d"])
            P.dma(lambda e, tm=tm, t0=t0: e.dma_start(out=TMS[t0:t0 + 128, :], in_=tm[:]), reads=[tmk], writes=[P.u("TMS")])
            if stop == 5:
                continue
            rp, rpk = rp_r.next()
            if is_ctx:
                P.op("pool", lambda e, rp=rp, qk=qk: e.tensor_copy(out=rp[:].rearrange("p (h a) b f -> p h (a b f)", a=2), in_=qk[:]),
                     reads=[qkk + "/a", qkk + "/b"], writes=[rpk])
            else:
                cs, csk = cs_r.next()
                tl = t0 - LC
                P.dma(lambda e, cs=cs, tl=tl: e.dma_start(out=cs[:, 0, :], in_=ropec[tl:tl + 128, :]), writes=[csk + "/c"])
                P.dma(lambda e, cs=cs, tl=tl: e.dma_start(out=cs[:, 1, :], in_=ropes[tl:tl + 128, :]), writes=[csk + "/s"])
                qv = qk[:].rearrange("p h (a b f) -> p (h a) b f", a=2, b=2)
                x1, x2 = qv[:, :, 0, :], qv[:, :, 1, :]
                cb = cs[:, 0, :].rearrange("p (g f) -> p g f", f=16)
                sb_ = cs[:, 1, :].rearrange("p (g f) -> p g f", f=16)
                tmp, tmpk = tmp_r.next()
                qkr = [qkk + "/a", qkk + "/b"]
                P.op("dve", lambda e, rp=rp, x1=x1, cb=cb: e.tensor_tensor(out=rp[:, :, 0, :], in0=x1, in1=cb, op=ALU.mult), reads=qkr + [csk + "/c"], writes=[rpk + "/0"])
                P.op("pool", lambda e, tmp=tmp, x2=x2, sb_=sb_: e.tensor_tensor(out=tmp[:], in0=x2, in1=sb_, op=ALU.mult), reads=qkr + [csk + "/s"], writes=[tmpk])
                P.op("dve", lambda e, rp=rp, tmp=tmp: e.tensor_tensor(out=rp[:, :, 0, :], in0=rp[:, :, 0, :], in1=tmp[:], op=ALU.subtract), reads=[rpk + "/0", tmpk], writes=[rpk + "/0"])
                P.op("dve", lambda e, rp=rp, x2=x2, cb=cb: e.tensor_tensor(out=rp[:, :, 1, :], in0=x2, in1=cb, op=ALU.mult), reads=qkr + [csk + "/c"], writes=[rpk + "/1"])
                P.op("pool", lambda e, tmp=tmp, x1=x1, sb_=sb_: e.tensor_tensor(out=tmp[:], in0=x1, in1=sb_, op=ALU.mult), reads=qkr + [csk + "/s"], writes=[tmpk])
                P.op("dve", lambda e, rp=rp, tmp=tmp: e.tensor_tensor(out=rp[:, :, 1, :], in0=rp[:, :, 1, :], in1=tmp[:], op=ALU.add), reads=[rpk + "/1", tmpk], writes=[rpk + "/1"])
            rkeys = [rpk] if is_ctx else [rpk + "/0", rpk + "/1"]
            if stop == 6:
                continue
            rpf = rp[:].rearrange("p g b f -> p (g b f)")
            tq, tqk = tq_r.next()
            for j in range(6):
                P.op("pe", lambda e, tq=tq, rpf=rpf, j=j: e.transpose(tq[:, j * 128:(j + 1) * 128], rpf[:, j * 128:(j + 1) * 128], ident[:]),
                     reads=rkeys + [pf + "ident"], writes=[tqk])
            qT, qTk = qT_r.next()
            P.op("act", lambda e, qT=qT, tq=tq: e.activation(out=qT[:], in_=tq[:, 0:768].rearrange("p (j t) -> p j t", j=6), func=AF.Copy), reads=[tqk], writes=[qTk])
            P.dma(lambda e, qT=qT, t0=t0: e.dma_start(out=QKT[:, t0:t0 + 128].rearrange("(j p) t -> p j t", p=128), in_=qT[:]), reads=[qTk], writes=[P.u("QKT")])
        if stop <= 7:
            continue
        hkeys = [hTk + "/%d" % i for i in range(ntile)]
        for j in range(8):
            fm, fmk = fm_r.next()
            for k in range(8):
                P.op("pe", lambda e, fm=fm, hT=hT, k=k, j=j, cn=cn: e.matmul(fm[:, 0:cn], win[:, k, FM_OFF + j * 128:FM_OFF + (j + 1) * 128], hT[:, k, 0:cn], start=(k == 0), stop=(k == 7)),
                     reads=hkeys + [wkeys[k]], writes=[fmk])
            fo, fok = fo_r.next()
            eng = "act" if j % 2 == 0 else "dve"
            if eng == "act":
                P.op("act", lambda e, fo=fo, fm=fm, cn=cn: e.activation(out=fo[:, 0:cn], in_=fm[:, 0:cn], func=AF.Copy), reads=[fmk], writes=[fok])
            else:
                P.op("dve", lambda e, fo=fo, fm=fm, cn=cn: e.tensor_copy(out=fo[:, 0:cn], in_=fm[:, 0:cn]), reads=[fmk], writes=[fok])
            P.dma(lambda e, fo=fo, j=j, c0=c0, cn=cn: e.dma_start(out=FMT[j * 128:(j + 1) * 128, c0:c0 + cn], in_=fo[:, 0:cn]), reads=[fok], writes=[P.u("FMT")])


def phase_g2(P, Dm, l, tok_chunks, final):
    pf = "G%d_" % l
    ntf = sum(cn for _, cn in tok_chunks) // 128
    NB = 2 * ntf + 32
    X1 = Dm.get("X1", [T, D])
    BUF = Dm.get("BUF%d" % l, [NB * 128, D])
    OBUF = Dm.get("OBUF%d" % l, [NB * 128, D])
    IDXW = Dm.get("IDXW%d" % l, [128, NB], I32)
    DEST = Dm.get("DEST%d" % l, [128, ntf * 2], I32)
    WWd = Dm.get("WWd%d" % l, [128, ntf * 2])
    WG = Dm.get("moe_g%d" % l, [32, 128, 8, 512], kind="ExternalInput")
    WU = Dm.get("moe_u%d" % l, [32, 128, 8, 512], kind="ExternalInput")
    WD = Dm.get("moe_d%d" % l, [32, 128, 4, 1024], kind="ExternalInput")
    ident_d = Dm.get("ident", [128, 128], kind="ExternalInput")
    modrow = Dm.get("modrow%d" % l, [2, 6144])
    mk = "modrow%d" % l
    ident = P.sb(pf + "ident", [128, 128]); P.d(ident[:], ident_d[:, :], writes=[pf + "ident"])
    idxw = P.sb(pf + "idxw", [128, NB], I32); P.d(idxw[:], IDXW[:, :], reads=["IDXW%d" % l], writes=[pf + "idxw"])
    dest = P.sb(pf + "dest", [128, ntf * 2], I32); P.d(dest[:], DEST[:, :], reads=["DEST%d" % l], writes=[pf + "dest"])
    ww = P.sb(pf + "ww", [128, ntf * 2]); P.d(ww[:], WWd[:, :], reads=["WWd%d" % l], writes=[pf + "ww"])
    wg_r = Rot(P, pf + "wg", [128, 8, 512], F32R, n=2)
    wu_r = Rot(P, pf + "wu", [128, 8, 512], F32R, n=2)
    wd_r = Rot(P, pf + "wd", [128, 4, 1024], F32R, n=2)
    xs_r = Rot(P, pf + "xs", [128, D], n=2)
    tp_r = Rot(P, pf + "tp", [128, 1024], n=1, psum=True)
    xT_r = Rot(P, pf + "xT", [128, 8, 128], F32R, n=2)
    pg_r = Rot(P, pf + "pg", [128, 512], n=1, psum=True)
    pu_r = Rot(P, pf + "pu", [128, 512], n=1, psum=True)
    sg_r = Rot(P, pf + "sg", [128, 512], n=2)
    hu_r = Rot(P, pf + "hu", [128, 512], n=2)
    ph_r = Rot(P, pf + "ph", [128, 512], n=1, psum=True)
    hT_r = Rot(P, pf + "hT", [128, 4, 128], F32R, n=2)
    po_r = Rot(P, pf + "po", [128, 1024], n=1, psum=True)
    ob_r = Rot(P, pf + "ob", [128, D], n=2)
    WGf = WG.rearrange("e p k n -> (e p) (k n)")
    WUf = WU.rearrange("e p k n -> (e p) (k n)")
    WDf = WD.rearrange("e p k n -> (e p) (k n)")
    st1 = {}

    def load_gu(b):
        off = bass.IndirectOffsetOnAxis(ap=idxw[:, b:b + 1], axis=0)
        wg, wgk = wg_r.next(); wu, wuk = wu_r.next()
        P.dma(lambda e, wg=wg, off=off: e.indirect_dma_start(out=wg[:].rearrange("p k n -> p (k n)"), out_offset=None, in_=WGf, in_offset=off), reads=[pf + "idxw"], writes=[wgk], q="pool")
        P.dma(lambda e, wu=wu, off=off: e.indirect_dma_start(out=wu[:].rearrange("p k n -> p (k n)"), out_offset=None, in_=WUf, in_offset=off), reads=[pf + "idxw"], writes=[wuk], q="pool")
        st1[("gu", b)] = (wg, wgk, wu, wuk)

    def load_d(b):
        off = bass.IndirectOffsetOnAxis(ap=idxw[:, b:b + 1], axis=0)
        wd, wdk = wd_r.next()
        P.dma(lambda e, wd=wd, off=off: e.indirect_dma_start(out=wd[:].rearrange("p k n -> p (k n)"), out_offset=None, in_=WDf, in_offset=off), reads=[pf + "idxw"], writes=[wdk], q="pool")
        st1[("d", b)] = (wd, wdk)

    def stage1(b):
        (wg, wgk, wu, wuk) = st1.pop(("gu", b))
        xs, xsk = xs_r.next()
        P.d(xs[:], BUF[b * 128:(b + 1) * 128, :], reads=["BUF%d" % l, "BUFz%d" % l], writes=[xsk])
        tp, tpk = tp_r.next()
        for k in range(8):
            P.i("pe", "transpose", reads=[xsk, pf + "ident"], writes=[tpk], out=tp[:, k * 128:(k + 1) * 128], in_=xs[:, k * 128:(k + 1) * 128], identity=ident[:])
        xT, xTk = xT_r.next()
        P.i("act", "activation", reads=[tpk], writes=[xTk + "/0"], out=xT[:, 0:4, :], in_=tp[:, 0:512].rearrange("p (k t) -> p k t", k=4), func=AF.Copy)
        P.i("dve", "tensor_copy", reads=[tpk], writes=[xTk + "/1"], out=xT[:, 4:8, :], in_=tp[:, 512:1024].rearrange("p (k t) -> p k t", k=4))
        pg, pgk = pg_r.next(); pu, puk = pu_r.next()
        for k in range(8):
            P.i("pe", "matmul", reads=[xTk, wgk], writes=[pgk], out=pg[:], lhsT=xT[:, k, :], rhs=wg[:, k, :], start=(k == 0), stop=(k == 7))
        for k in range(8):
            P.i("pe", "matmul", reads=[xTk, wuk], writes=[puk], out=pu[:], lhsT=xT[:, k, :], rhs=wu[:, k, :], start=(k == 0), stop=(k == 7))
        sg, sgk = sg_r.next()
        P.i("act", "activation", reads=[pgk], writes=[sgk], out=sg[:], in_=pg[:], func=AF.Silu)
        hu, huk = hu_r.next()
        P.i("dve", "tensor_tensor", reads=[sgk, puk], writes=[huk], out=hu[:], in0=sg[:], in1=pu[:], op=ALU.mult)
        st1[("h", b)] = (hu, huk)

    def stage2(b):
        (hu, huk) = st1.pop(("h", b))
        (wd, wdk) = st1.pop(("d", b))
        ph, phk = ph_r.next()
        for hc in range(4):
            P.i("pe", "transpose", reads=[huk, pf + "ident"], writes=[phk], out=ph[:, hc * 128:(hc + 1) * 128], in_=hu[:, hc * 128:(hc + 1) * 128], identity=ident[:])
        hT, hTk = hT_r.next()
        P.i("act", "activation", reads=[phk], writes=[hTk], out=hT[:], in_=ph[:].rearrange("p (k t) -> p k t", k=4), func=AF.Copy)
        po, pok = po_r.next()
        for hf in range(2):
            for hc in range(4):
                P.i("pe", "matmul", reads=[hTk, wdk], writes=[pok], out=po[:, hf * 512:(hf + 1) * 512], lhsT=hT[:, hc, :], rhs=wd[:, hc, hf * 512:(hf + 1) * 512], start=(hc == 0), stop=(hc == 3))
        ob, obk = ob_r.next()
        P.i("act", "activation", reads=[pok], writes=[obk + "/0"], out=ob[:, 0:512], in_=po[:, 0:512], func=AF.Copy)
        P.i("dve", "tensor_copy", reads=[pok], writes=[obk + "/1"], out=ob[:, 512:1024], in_=po[:, 512:1024])
        P.d(OBUF[b * 128:(b + 1) * 128, :], ob[:], reads=[obk], writes=[P.u("OBUF%d" % l)])

    load_gu(0); load_d(0); load_gu(1); load_d(1)
    stage1(0)
    for b in range(NB):
        if b + 2 < NB:
            load_gu(b + 2)
        if b + 1 < NB:
            stage1(b + 1)
        stage2(b)
        if b + 2 < NB:
            load_d(b + 2)
    if final:
        fg_d = Dm.get("final_g", [1, D], kind="ExternalInput")
        OUT = Dm.get("out", [L, D], kind="ExternalOutput")
        fg = P.sb(pf + "fg", [128, D]); load_bcast(P, fg[:], pf + "fg", fg_d[0:1, :])
        epsc = P.sb(pf + "eps", [128, 1]); P.i("dve", "memset", writes=[pf + "eps"], ap=epsc[:], constant=EPS)
        junk = P.sb(pf + "junk", [128, D])
    else:
        XN = Dm.get("XN%d" % l, [T, D])
    G2g = [P.sb(pf + "g2%d" % r, [128, D]) for r in range(2)]
    for r in range(2):
        load_bcast(P, G2g[r][:], pf + "g2%d" % r, modrow[r:r + 1, 5120:6144], reads=[mk])
    o1_r = Rot(P, pf + "o1", [128, D], n=2)
    o2_r = Rot(P, pf + "o2", [128, D], n=2)
    xt_r = Rot(P, pf + "xt", [128, D], n=2)
    st_r = Rot(P, pf + "st", [128, 8], n=2)
    ti = 0
    for (c0, cn) in tok_chunks:
        for i in range(cn // 128):
            t0 = c0 + i * 128
            r = 1 if t0 < LC else 0
            o1, o1k = o1_r.next(); o2, o2k = o2_r.next()
            for (o, ok_, col) in ((o1, o1k, ti * 2), (o2, o2k, ti * 2 + 1)):
                P.dma(lambda e, o=o, col=col: e.indirect_dma_start(out=o[:], out_offset=None, in_=OBUF[:, :], in_offset=bass.IndirectOffsetOnAxis(ap=dest[:, col:col + 1], axis=0)),
                      reads=[pf + "dest", "OBUF%d" % l], writes=[ok_], q="pool")
            xt, xk = xt_r.next()
            P.d(xt[:], X1[t0:t0 + 128, :], reads=["X1"], writes=[xk])
            P.i("dve", "tensor_scalar", reads=[o1k, pf + "ww"], writes=[o1k], out=o1[:], in0=o1[:], scalar1=ww[:, ti * 2:ti * 2 + 1], scalar2=None, op0=ALU.mult)
            P.i("dve", "scalar_tensor_tensor", reads=[o1k, o2k, pf + "ww"], writes=[o1k], out=o1[:], in0=o2[:], scalar=ww[:, ti * 2 + 1:ti * 2 + 2], in1=o1[:], op0=ALU.mult, op1=ALU.add)
            P.i("pool", "tensor_tensor", reads=[o1k, pf + "g2%d" % r], writes=[o1k], out=o1[:], in0=o1[:], in1=G2g[r][:], op=ALU.mult)
            P.i("dve", "tensor_tensor", reads=[o1k, xk], writes=[xk], out=xt[:], in0=xt[:], in1=o1[:], op=ALU.add)
            if not final:
                P.d(XN[t0:t0 + 128, :], xt[:], reads=[xk], writes=[P.u("XN%d" % l)])
            else:
                st, sk = st_r.next()
                P.i("act", "activation", reads=[xk], writes=[pf + "junk", sk], out=junk[:], in_=xt[:], func=AF.Square, accum_out=st[:, 0:1])
                P.i("act", "activation", reads=[sk, pf + "eps"], writes=[sk], out=st[:, 1:2], in_=st[:, 0:1], func=AF.Sqrt, bias=epsc[:], scale=1.0 / D)
                P.i("dve", "reciprocal", reads=[sk], writes=[sk], out=st[:, 2:3], in_=st[:, 1:2])
                P.i("dve", "scalar_tensor_tensor", reads=[xk, sk, pf + "fg"], writes=[xk], out=xt[:], in0=xt[:], scalar=st[:, 2:3], in1=fg[:], op0=ALU.mult, op1=ALU.mult)
                P.d(OUT[t0 - LC:t0 - LC + 128, :], xt[:], reads=[xk], writes=[P.u("out")], final=True)
            ti += 1


def rope_tables_np():
    rows = np.repeat(np.arange(L // 64), 64)
    cols = np.tile(np.arange(64), L // 64)
    inv = np.power(np.float32(10000.0), -np.arange(16, dtype=np.float32) / np.float32(16)).astype(np.float32)
    ang = np.stack([rows, cols], -1).astype(np.float32)[..., None] * inv
    cos = np.cos(ang).astype(np.float32).reshape(L, 1, 32)
    sin = np.sin(ang).astype(np.float32).reshape(L, 1, 32)
    return (np.ascontiguousarray(np.broadcast_to(cos, (L, 12, 32)).reshape(L, 384)),
            np.ascontiguousarray(np.broadcast_to(sin, (L, 12, 32)).reshape(L, 384)))


def kmajor(w):
    K, N = w.shape
    return np.ascontiguousarray(w.reshape(K // 128, 128, N).transpose(1, 0, 2))


def core_inputs(inp, b):
    f = np.float32
    m = {}
    m["xin"] = np.ascontiguousarray(np.concatenate([inp["ctx"][b], inp["x"][b]], 0).astype(f))
    cv = np.stack([inp["c"][b], inp["c_ctx"]], -1)
    m["cvec"] = np.ascontiguousarray(cv.reshape(8, 128, 2).transpose(1, 0, 2).astype(f))
    m["ident"] = np.eye(128, dtype=f)
    sh = np.zeros((128, 64), f); sh[64 + np.arange(64), np.arange(64)] = 1.0
    m["shiftm"] = sh
    jj, ii = np.meshgrid(np.arange(128), np.arange(128), indexing="ij")
    wm = np.zeros((128, 2, 2, 128), f)
    wm[:, 0] = (ii <= jj).astype(f)[:, None, :]
    wm[:, 1] = (jj <= ii).astype(f)[:, None, :]
    m["wmask"] = np.ascontiguousarray(wm.reshape(128, 2, 256))
    m["rope_cos"], m["rope_sin"] = rope_tables_np()
    for l in range(2):
        m["ada_w%d" % l] = kmajor(inp["ada_w"][l])
        m["ada_b%d" % l] = np.ascontiguousarray(inp["ada_b"][l].reshape(1, 6144))
        m["w_in%d" % l] = kmajor(inp["w_in"][l][:, PERM])
        m["norm1_g%d" % l] = np.ascontiguousarray(inp["norm1_g"][l].reshape(1, D))
        m["qkg%d" % l] = np.ascontiguousarray(np.concatenate([np.tile(inp["ga_qn_g"][l], 4), np.tile(inp["ga_kn_g"][l], 2)]).reshape(1, 384))
        m["dtb%d" % l] = np.ascontiguousarray(inp["ssd_dt_bias"][l].reshape(1, 8))
        m["sink%d" % l] = np.ascontiguousarray(inp["wa_sink"][l].reshape(1, 4))
        m["w_out%d" % l] = kmajor(inp["w_out"][l])
        m["norm2_g%d" % l] = np.ascontiguousarray(inp["norm2_g"][l].reshape(1, D))
        m["wr%d" % l] = kmajor(np.concatenate([inp["moe_coarse_w"][l], inp["moe_fine_w"][l]], 1))
        m["rb%d" % l] = np.ascontiguousarray(np.concatenate([inp["moe_coarse_b"][l], inp["moe_fine_b"][l]]).reshape(1, 36))
        m["moe_g%d" % l] = np.ascontiguousarray(inp["moe_w_gate"][l].reshape(32, 8, 128, 512).transpose(0, 2, 1, 3))
        m["moe_u%d" % l] = np.ascontiguousarray(inp["moe_w_up"][l].reshape(32, 8, 128, 512).transpose(0, 2, 1, 3))
        m["moe_d%d" % l] = np.ascontiguousarray(inp["moe_w_down"][l].reshape(32, 4, 128, 1024).transpose(0, 2, 1, 3))
    m["final_g"] = np.ascontiguousarray(inp["final_g"].reshape(1, D))
    m["ramp"] = (128.0 * np.arange(72) + 1.0).astype(f).reshape(1, 72)
    m["bst"] = (128.0 * np.arange(104)).astype(f).reshape(1, 104)
    m["pidx"] = np.arange(128).astype(f).reshape(128, 1)
    sp, s_ = np.meshgrid(np.arange(128), np.arange(128), indexing="ij")
    m["stri"] = np.ascontiguousarray(np.stack([(sp < s_), np.ones_like(sp, dtype=bool)], 1).astype(f))
    m["tri"] = np.ascontiguousarray(np.stack([(sp <= s_), (sp >= s_)], 1).astype(f))
    for l in range(2):
        Bm = np.zeros((2, 2, 8, 128, 128), f)
        Cm = np.zeros((2, 2, 8, 128, 128), f)
        for d in range(2):
            for j in range(8):
                for gg in range(2):
                    g = 2 * j + gg
                    r0 = (2 * (j % 4) + gg) * 16
                    Bm[d, 0, j, r0:r0 + 16, gg * 64:(gg + 1) * 64] = inp["s5_b_re"][l, d, g].T
                    Bm[d, 1, j, r0:r0 + 16, gg * 64:(gg + 1) * 64] = inp["s5_b_im"][l, d, g].T
                    Cm[d, 0, j, gg * 64:(gg + 1) * 64, r0:r0 + 16] = inp["s5_c_re"][l, d, g].T
                    Cm[d, 1, j, gg * 64:(gg + 1) * 64, r0:r0 + 16] = inp["s5_c_im"][l, d, g].T
        m["s5B%d" % l] = Bm
        m["s5C%d" % l] = Cm
        lamt = np.zeros((128, 3, 16), f)
        for d in range(2):
            for j in range(8):
                for gg in range(2):
                    g = 2 * j + gg
                    lamt[gg * 64:(gg + 1) * 64, 0, d * 8 + j] = inp["s5_lam_re"][l, d, g]
                    lamt[gg * 64:(gg + 1) * 64, 1, d * 8 + j] = inp["s5_lam_im"][l, d, g]
                    lamt[gg * 64:(gg + 1) * 64, 2, d * 8 + j] = inp["s5_log_dt"][l, d, g]
        m["s5lam%d" % l] = lamt
        cw = np.concatenate([inp["ssd_conv_w"][l], inp["ssd_conv_b"][l][None]], 0)
        m["convw%d" % l] = np.ascontiguousarray(cw.reshape(4, 6, 128).transpose(2, 1, 0).astype(f))
        m["alog%d" % l] = np.ascontiguousarray(inp["ssd_a_log"][l].reshape(1, 8))
        m["ssdd%d" % l] = np.ascontiguousarray(inp["ssd_d"][l].reshape(1, 4))
        m["ssdng%d" % l] = np.ascontiguousarray(inp["ssd_norm_g"][l].reshape(1, 256))
        m["s5d%d" % l] = np.ascontiguousarray(inp["s5_d"][l].reshape(2, 128).T.astype(f))
        m["gluw%d" % l] = np.ascontiguousarray(inp["s5_glu_w"][l].reshape(2, 128, 256).transpose(1, 0, 2).astype(f))
        m["glub%d" % l] = np.ascontiguousarray(inp["s5_glu_b"][l].reshape(2, 128).T.astype(f))
    for l in range(0):
        pass
    return m


def attn_consts(P, Dm, pf):
    c = {}
    shift_d = Dm.get("shiftm", [128, 64], kind="ExternalInput")
    c["shift"] = P.sb(pf + "shift", [128, 64], F32R)
    P.d(c["shift"][:], shift_d[:, :], writes=[pf + "shift"], q="pool")
    return c


def phase_c(P, Dm, l, ctx_out):
    pf = "c%d_" % l
    QKT = Dm.get("QKT", [768, T])
    TMS = Dm.get("TMS", [T, 520])
    CAT = Dm.get("CAT", [1024, T])
    cst = attn_consts(P, Dm, pf)
    KT = P.sb(pf + "KT", [64, 2, T], F32R)
    V = P.sb(pf + "V", [128, NT, 2, 128], F32R)
    for kv in range(2):
        P.d(KT[:, kv, :], QKT[256 + kv * 64:256 + (kv + 1) * 64, :], reads=["QKT"], writes=[pf + "KT"], q="pool")
    P.i("dve", "memset", writes=[pf + "V/1"], ap=V[:].bitcast(F32)[:, :, :, 64:128], constant=1.0)
    for kv in range(2):
        P.d(V[:, :, kv, 0:64], TMS[:, kv * 64:(kv + 1) * 64].rearrange("(n p) d -> p n d", p=128), reads=["TMS"], writes=[pf + "V/0%d" % kv], q="pool")
    vkeys = [pf + "V"]
    q_r = Rot(P, pf + "q", [64, 4, 256], F32R, n=2)
    s_r = Rot(P, pf + "s", [128, 512], n=3, psum=True)
    p_r = Rot(P, pf + "p", [128, 512], F32R, n=3)
    o_r = Rot(P, pf + "o", [128, 512], n=2, psum=True)
    os_r = Rot(P, pf + "os", [128, 512], F32R, n=2)
    dn_r = Rot(P, pf + "dn", [64, 512], n=1, psum=True)
    rd_r = Rot(P, pf + "rd", [64, 512], n=2)
    ot_r = Rot(P, pf + "ot", [64, 512], n=2)
    qtiles = [(LC + 256 * i, NT) for i in range(L // 256)]
    if ctx_out:
        qtiles = [(0, 2)] + qtiles
    for (q0, nkb) in qtiles:
        qt, qtk = q_r.next()
        P.d(qt[:], QKT[0:256, q0:q0 + 256].rearrange("(h d) q -> d h q", d=64), reads=["QKT"], writes=[qtk], q="pool")
        for kv in range(2):
            o, ok = o_r.next()
            its = list(range(nkb))
            pend = []

            def issue_s(s):
                sp_, spk = s_r.next()
                P.i("pe", "matmul", reads=[pf + "KT", qtk], writes=[spk], out=sp_[:], lhsT=KT[:, kv, s * 128:(s + 1) * 128],
                    rhs=qt[:, 2 * kv:2 * kv + 2, :], start=True, stop=True)
                pt, ptk = p_r.next()
                P.i("act", "activation", reads=[spk], writes=[ptk], out=pt[:], in_=sp_[:], func=AF.Exp, scale=0.125)
                return (s, pt, ptk)

            LOOK = 2
            for s in its[:LOOK]:
                pend.append(issue_s(s))
            for idx, s in enumerate(its):
                (s_, pt, ptk) = pend.pop(0)
                if idx + LOOK < len(its):
                    pend.append(issue_s(its[idx + LOOK]))
                P.i("pe", "matmul", reads=[ptk] + vkeys, writes=[ok], out=o[:], lhsT=V[:, s_, kv, :], rhs=pt[:],
                    start=(idx == 0), stop=(idx == len(its) - 1))
            osb, osk = os_r.next()
            P.i("act", "activation", reads=[ok], writes=[osk], out=osb[:], in_=o[:], func=AF.Copy)
            dn, dnk = dn_r.next()
            P.i("pe", "matmul", reads=[osk, pf + "shift"], writes=[dnk], out=dn[:], lhsT=cst["shift"][:], rhs=osb[:], start=True, stop=True)
            rd, rdk = rd_r.next()
            P.i("dve", "reciprocal", reads=[dnk], writes=[rdk], out=rd[:], in_=dn[:])
            ot, otk = ot_r.next()
            P.i("dve", "tensor_tensor", reads=[osk, rdk], writes=[otk], out=ot[:], in0=osb[0:64, :].bitcast(F32), in1=rd[:], op=ALU.mult)
            P.d(CAT[256 + kv * 128:256 + (kv + 1) * 128, q0:q0 + 256].rearrange("(hh d) q -> d hh q", d=64),
                ot[:].rearrange("d (hh q) -> d hh q", hh=2), reads=[otk], writes=[P.u("CAT")])


def phase_e(P, Dm, l, ctx_out):
    pf = "e%d_" % l
    QKT = Dm.get("QKT", [768, T])
    TMS = Dm.get("TMS", [T, 520])
    CAT = Dm.get("CAT", [1024, T])
    sink_d = Dm.get("sink%d" % l, [1, 4], kind="ExternalInput")
    mask_d = Dm.get("wmask", [128, 2, 256], kind="ExternalInput")
    cst = attn_consts(P, Dm, pf)
    KT = P.sb(pf + "KT", [64, 2, T], F32R)
    V = P.sb(pf + "V", [128, NT, 2, 128], F32R)
    for kv in range(2):
        P.d(KT[:, kv, :], QKT[640 + kv * 64:640 + (kv + 1) * 64, :], reads=["QKT"], writes=[pf + "KT"], q="pool")
    P.i("dve", "memset", writes=[pf + "V/1"], ap=V[:].bitcast(F32)[:, :, :, 64:128], constant=1.0)
    for kv in range(2):
        P.d(V[:, :, kv, 0:64], TMS[:, 128 + kv * 64:128 + (kv + 1) * 64].rearrange("(n p) d -> p n d", p=128), reads=["TMS"], writes=[pf + "V/0%d" % kv], q="pool")
    vkeys = [pf + "V"]
    mask = P.sb(pf + "mask", [128, 2, 256])
    P.d(mask[:], mask_d[:, :, :], writes=[pf + "mask"])
    esk = P.sb(pf + "esk", [64, 4])
    P.d(esk[:], sink_d[0:1, :].to_broadcast([64, 4]), writes=[pf + "esk"])
    P.i("act", "activation", reads=[pf + "esk"], writes=[pf + "esk"], out=esk[:], in_=esk[:], func=AF.Exp)
    q_r = Rot(P, pf + "q", [64, 4, 128], F32R, n=2)
    s_r = Rot(P, pf + "s", [128, 256], n=3, psum=True)
    p_r = Rot(P, pf + "p", [128, 256], F32R, n=3)
    o_r = Rot(P, pf + "o", [128, 256], n=2, psum=True)
    os_r = Rot(P, pf + "os", [128, 256], F32R, n=2)
    dn_r = Rot(P, pf + "dn", [64, 256], n=1, psum=True)
    rd_r = Rot(P, pf + "rd", [64, 256], n=2)
    ot_r = Rot(P, pf + "ot", [64, 256], n=2)
    qtiles = []
    if ctx_out:
        qtiles += [(i, [(0, None), (1, None)]) for i in range(2)]
    for n in range(L // 128):
        ti = 2 + n
        kb = [(0, None), (1, None)]
        if n > 0:
            kb.append((ti - 1, 0))
        kb.append((ti, None))
        if n < L // 128 - 1:
            kb.append((ti + 1, 1))
        qtiles.append((ti, kb))
    for (ti, kbs) in qtiles:
        q0 = ti * 128
        qt, qtk = q_r.next()
        P.d(qt[:], QKT[384:640, q0:q0 + 128].rearrange("(h d) q -> d h q", d=64), reads=["QKT"], writes=[qtk], q="pool")
        for kv in range(2):
            o, ok = o_r.next()
            for idx, (s, mi) in enumerate(kbs):
                sp_, spk = s_r.next()
                P.i("pe", "matmul", reads=[pf + "KT", qtk], writes=[spk], out=sp_[:], lhsT=KT[:, kv, s * 128:(s + 1) * 128],
                    rhs=qt[:, 2 * kv:2 * kv + 2, :], start=True, stop=True)
                pt, ptk = p_r.next()
                P.i("act", "activation", reads=[spk], writes=[ptk], out=pt[:], in_=sp_[:], func=AF.Exp, scale=0.125)
                if mi is not None:
                    P.i("dve", "tensor_tensor", reads=[ptk, pf + "mask"], writes=[ptk], out=pt[:], in0=pt[:].bitcast(F32), in1=mask[:, mi, :], op=ALU.mult)
                P.i("pe", "matmul", reads=[ptk] + vkeys, writes=[ok], out=o[:], lhsT=V[:, s, kv, :], rhs=pt[:],
                    start=(idx == 0), stop=(idx == len(kbs) - 1))
            osb, osk = os_r.next()
            P.i("act", "activation", reads=[ok], writes=[osk], out=osb[:], in_=o[:], func=AF.Copy)
            dn, dnk = dn_r.next()
            P.i("pe", "matmul", reads=[osk, pf + "shift"], writes=[dnk], out=dn[:], lhsT=cst["shift"][:], rhs=osb[:], start=True, stop=True)
            rd, rdk = rd_r.next()
            for hh in range(2):
                h = 2 * kv + hh
                P.i("dve", "tensor_scalar", reads=[dnk, pf + "esk"], writes=[rdk + "/%d" % hh], out=rd[:, hh * 128:(hh + 1) * 128], in0=dn[:, hh * 128:(hh + 1) * 128],
                    scalar1=esk[:, h:h + 1], scalar2=None, op0=ALU.add)
            P.i("dve", "reciprocal", reads=[rdk], writes=[rdk], out=rd[:], in_=rd[:])
            ot, otk = ot_r.next()
            P.i("dve", "tensor_tensor", reads=[osk, rdk], writes=[otk], out=ot[:], in0=osb[0:64, :].bitcast(F32), in1=rd[:], op=ALU.mult)
            P.d(CAT[768 + kv * 128:768 + (kv + 1) * 128, q0:q0 + 128].rearrange("(hh d) q -> d hh q", d=64),
                ot[:].rearrange("d (hh q) -> d hh q", hh=2), reads=[otk], writes=[P.u("CAT")])


BIG = 1.0e30


def phase_f(P, Dm, l, src_name, tok_chunks=None):
    pf = "f%d_" % l
    xsrc = Dm.get(src_name, [T, D])
    CAT = Dm.get("CAT", [1024, T])
    w_out = Dm.get("w_out%d" % l, [128, 8, 1024], kind="ExternalInput")
    modrow = Dm.get("modrow%d" % l, [2, 6144])
    n2g = Dm.get("norm2_g%d" % l, [1, D], kind="ExternalInput")
    wr_d = Dm.get("wr%d" % l, [128, 8, 36], kind="ExternalInput")
    rb_d = Dm.get("rb%d" % l, [1, 36], kind="ExternalInput")
    ident_d = Dm.get("ident", [128, 128], kind="ExternalInput")
    X1 = Dm.get("X1", [T, D])
    H2T = Dm.get("H2T", [D, T])
    WTd = Dm.get("WTd", [32, T])
    mk = "modrow%d" % l
    ident = P.sb(pf + "ident", [128, 128])
    P.d(ident[:], ident_d[:, :], writes=[pf + "ident"])
    wo = P.sb(pf + "wo", [128, 8, 1024], F32R)
    for k in range(8):
        P.d(wo[:, k, :], w_out[:, k, :], writes=[pf + "wo/%d" % k], q="pool")
    wr = P.sb(pf + "wr", [128, 8, 36])
    P.d(wr[:], wr_d[:, :, :], writes=[pf + "wr"])
    rb = P.sb(pf + "rb", [128, 36])
    load_bcast(P, rb[:], pf + "rb", rb_d[0:1, :])
    G1 = [P.sb(pf + "G1%d" % r, [128, D]) for r in range(2)]
    G2 = [P.sb(pf + "G2%d" % r, [128, D]) for r in range(2)]
    SH2 = [P.sb(pf + "SH2%d" % r, [128, D]) for r in range(2)]
    gn = P.sb(pf + "gn", [128, D])
    load_bcast(P, gn[:], pf + "gn", n2g[0:1, :])
    for r in range(2):
        load_bcast(P, G1[r][:], pf + "G1%d" % r, modrow[r:r + 1, 2048:3072], reads=[mk])
        load_bcast(P, SH2[r][:], pf + "SH2%d" % r, modrow[r:r + 1, 3072:4096], reads=[mk])
        load_bcast(P, G2[r][:], pf + "G2%d" % r, modrow[r:r + 1, 4096:5120], reads=[mk])
        P.i("dve", "scalar_tensor_tensor", reads=[pf + "G2%d" % r, pf + "gn"], writes=[pf + "G2%d" % r], out=G2[r][:], in0=G2[r][:], scalar=1.0, in1=gn[:], op0=ALU.add, op1=ALU.mult)
    epsc = P.sb(pf + "eps", [128, 1])
    P.i("dve", "memset", writes=[pf + "eps"], ap=epsc[:], constant=EPS)
    ct_r = Rot(P, pf + "ct", [128, 8, 512], F32R, n=2)
    po_r = Rot(P, pf + "po", [128, 1024], n=1, psum=True)
    xt_r = Rot(P, pf + "xt", [128, D], n=2)
    x1_r = Rot(P, pf + "x1", [128, D], n=2)
    h_r = Rot(P, pf + "h", [128, D], n=2)
    junk = P.sb(pf + "junk", [128, D])
    st_r = Rot(P, pf + "st", [128, 64], n=3)
    tp_r = Rot(P, pf + "tp", [128, 1024], n=1, psum=True)
    hT_r = Rot(P, pf + "hT", [128, 8, 128], F32R, n=2)
    hTf_r = Rot(P, pf + "hTf", [128, 8, 128], n=2)
    pr_r = Rot(P, pf + "pr", [128, 512], n=1, psum=True)
    lg_r = Rot(P, pf + "lg", [128, 36], n=2)
    mk_r = Rot(P, pf + "mk", [128, 4, 8], n=2)
    oh_r = Rot(P, pf + "oh", [128, 3, 32], n=2)
    wt_r = Rot(P, pf + "wt", [128, 32], n=2)
    pw_r = Rot(P, pf + "pw", [128, 512], n=1, psum=True)
    wT_r = Rot(P, pf + "wT", [32, 128], n=2)
    for (c0, cn) in (tok_chunks or CHUNKS):
        ct, ctk = ct_r.next()
        P.d(ct[:, :, 0:cn], CAT[:, c0:c0 + cn].rearrange("(k p) t -> p k t", p=128), reads=["CAT"], writes=[ctk], q="pool")
        for i in range(cn // 128):
            t0 = c0 + i * 128
            r = 1 if t0 < LC else 0
            po, pok = po_r.next()
            for hf in range(2):
                for k in range(8):
                    P.i("pe", "matmul", reads=[ctk, pf + "wo/%d" % k], writes=[pok], out=po[:, hf * 512:(hf + 1) * 512], lhsT=ct[:, k, i * 128:(i + 1) * 128],
                        rhs=wo[:, k, hf * 512:(hf + 1) * 512], start=(k == 0), stop=(k == 7))
            xt, xk = xt_r.next()
            P.d(xt[:], xsrc[t0:t0 + 128, :], reads=[src_name], writes=[xk])
            x1, x1k = x1_r.next()
            for hf in range(2):
                sl = slice(hf * 512, (hf + 1) * 512)
                P.i("dve", "tensor_tensor", reads=[pok, pf + "G1%d" % r], writes=[x1k + "/%d" % hf], out=x1[:, sl], in0=po[:, sl], in1=G1[r][:, sl], op=ALU.mult)
            P.i("pool", "tensor_tensor", reads=[x1k, xk], writes=[x1k], out=x1[:], in0=x1[:], in1=xt[:], op=ALU.add)
            P.d(X1[t0:t0 + 128, :], x1[:], reads=[x1k], writes=[P.u("X1")])
            st, sk = st_r.next()
            P.i("act", "activation", reads=[x1k], writes=[pf + "junk", sk], out=junk[:], in_=x1[:], func=AF.Square, accum_out=st[:, 0:1])
            P.i("act", "activation", reads=[sk, pf + "eps"], writes=[sk], out=st[:, 1:2], in_=st[:, 0:1], func=AF.Sqrt, bias=epsc[:], scale=1.0 / D)
            P.i("dve", "reciprocal", reads=[sk], writes=[sk], out=st[:, 2:3], in_=st[:, 1:2])
            h, hk = h_r.next()
            P.i("dve", "scalar_tensor_tensor", reads=[x1k, sk, pf + "G2%d" % r], writes=[hk], out=h[:], in0=x1[:], scalar=st[:, 2:3], in1=G2[r][:], op0=ALU.mult, op1=ALU.mult)
            P.i("pool", "tensor_tensor", reads=[hk, pf + "SH2%d" % r], writes=[hk], out=h[:], in0=h[:], in1=SH2[r][:], op=ALU.add)
            tp, tpk = tp_r.next()
            for k in range(8):
                P.i("pe", "transpose", reads=[hk, pf + "ident"], writes=[tpk], out=tp[:, k * 128:(k + 1) * 128], in_=h[:, k * 128:(k + 1) * 128], identity=ident[:])
            hT, hTk = hT_r.next()
            hTf, hTfk = hTf_r.next()
            P.i("act", "activation", reads=[tpk], writes=[hTk], out=hT[:], in_=tp[:].rearrange("p (k t) -> p k t", k=8), func=AF.Copy)
            P.i("dve", "tensor_copy", reads=[tpk], writes=[hTfk], out=hTf[:], in_=tp[:].rearrange("p (k t) -> p k t", k=8))
            P.d(H2T[:, t0:t0 + 128].rearrange("(k p) t -> p k t", p=128), hT[:].bitcast(F32), reads=[hTk], writes=[P.u("H2T")])
            pr, prk = pr_r.next()
            for k in range(8):
                P.i("pe", "matmul", reads=[hTfk, pf + "wr"], writes=[prk], out=pr[:, 0:36], lhsT=hTf[:, k, :], rhs=wr[:, k, :], start=(k == 0), stop=(k == 7))
            lg, lgk = lg_r.next()
            P.i("dve", "tensor_tensor", reads=[prk, pf + "rb"], writes=[lgk], out=lg[:], in0=pr[:, 0:36], in1=rb[:], op=ALU.add)
            P.i("dve", "tensor_reduce", reads=[lgk], writes=[sk], out=st[:, 4:5], in_=lg[:, 0:4], axis=AX.X, op=ALU.max)
            P.i("dve", "tensor_scalar", reads=[sk], writes=[sk], out=st[:, 5:6], in0=st[:, 4:5], scalar1=-1.0, scalar2=None, op0=ALU.mult)
            P.i("act", "activation", reads=[lgk, sk], writes=[sk], out=st[:, 32:36], in_=lg[:, 0:4], func=AF.Exp, bias=st[:, 5:6], accum_out=st[:, 6:7])
            P.i("dve", "reciprocal", reads=[sk], writes=[sk], out=st[:, 7:8], in_=st[:, 6:7])
            P.i("dve", "tensor_scalar", reads=[lgk, sk], writes=[sk], out=st[:, 8:12], in0=lg[:, 0:4], scalar1=st[:, 4:5], scalar2=None, op0=ALU.is_equal)
            P.i("dve", "tensor_scalar", reads=[sk], writes=[sk], out=st[:, 12:16], in0=st[:, 8:12], scalar1=BIG, scalar2=-BIG, op0=ALU.mult, op1=ALU.add)
            mkd, mkk = mk_r.next()
            P.i("dve", "tensor_tensor", reads=[lgk, sk], writes=[mkk], out=mkd[:], in0=lg[:, 4:36].rearrange("p (g e) -> p g e", g=4),
                in1=st[:, 12:16].unsqueeze(2).to_broadcast([128, 4, 8]), op=ALU.add)
            mflat = mkd[:].rearrange("p g e -> p (g e)")
            P.i("dve", "max", reads=[mkk], writes=[sk], out=st[:, 16:24], in_=mflat)
            oh, ohk = oh_r.next()
            P.i("dve", "tensor_scalar", reads=[mkk, sk], writes=[ohk + "/1"], out=oh[:, 0, :], in0=mflat, scalar1=st[:, 16:17], scalar2=None, op0=ALU.is_equal)
            P.i("dve", "tensor_scalar", reads=[mkk, sk], writes=[ohk + "/2"], out=oh[:, 1, :], in0=mflat, scalar1=st[:, 17:18], scalar2=None, op0=ALU.is_equal)
            P.i("dve", "tensor_tensor", reads=[sk], writes=[sk], out=st[:, 24:25], in0=st[:, 17:18], in1=st[:, 16:17], op=ALU.subtract)
            P.i("act", "activation", reads=[sk], writes=[sk], out=st[:, 25:26], in_=st[:, 24:25], func=AF.Exp)
            P.i("dve", "tensor_scalar", reads=[sk], writes=[sk], out=st[:, 26:27], in0=st[:, 25:26], scalar1=1.0, scalar2=None, op0=ALU.add)
            P.i("dve", "reciprocal", reads=[sk], writes=[sk], out=st[:, 27:28], in_=st[:, 26:27])
            P.i("dve", "tensor_tensor", reads=[sk], writes=[sk], out=st[:, 28:29], in0=st[:, 27:28], in1=st[:, 7:8], op=ALU.mult)
            P.i("dve", "tensor_tensor", reads=[sk], writes=[sk], out=st[:, 29:30], in0=st[:, 7:8], in1=st[:, 28:29], op=ALU.subtract)
            P.i("dve", "tensor_scalar", reads=[ohk + "/1", sk], writes=[ohk + "/3"], out=oh[:, 2, :], in0=oh[:, 0, :], scalar1=st[:, 28:29], scalar2=None, op0=ALU.mult)
            wt, wtk = wt_r.next()
            P.i("dve", "scalar_tensor_tensor", reads=[ohk + "/2", ohk + "/3", sk], writes=[wtk], out=wt[:], in0=oh[:, 1, :], scalar=st[:, 29:30], in1=oh[:, 2, :], op0=ALU.mult, op1=ALU.add)
            pw, pwk = pw_r.next()
            P.i("pe", "transpose", reads=[wtk, pf + "ident"], writes=[pwk], out=pw[0:32, 0:128], in_=wt[:], identity=ident[:])
            wT, wTk = wT_r.next()
            P.i("act", "activation", reads=[pwk], writes=[wTk], out=wT[:], in_=pw[0:32, 0:128], func=AF.Copy)
            P.d(WTd[:, t0:t0 + 128], wT[:], reads=[wTk], writes=[P.u("WTd")])


def phase_f2(P, Dm, l, src_name, tok_chunks=None):
    pf = "F%d_" % l
    xsrc = Dm.get(src_name, [T, D])
    CAT = Dm.get("CAT", [1024, T])
    w_out = Dm.get("w_out%d" % l, [128, 8, 1024], kind="ExternalInput")
    modrow = Dm.get("modrow%d" % l, [2, 6144])
    n2g = Dm.get("norm2_g%d" % l, [1, D], kind="ExternalInput")
    wr_d = Dm.get("wr%d" % l, [128, 8, 36], kind="ExternalInput")
    rb_d = Dm.get("rb%d" % l, [1, 36], kind="ExternalInput")
    ident_d = Dm.get("ident", [128, 128], kind="ExternalInput")
    X1 = Dm.get("X1", [T, D])
    chunks_ = (tok_chunks or CHUNKS)
    ntf = sum(cn for _, cn in chunks_) // 128
    NB = (2 * ntf * 128) // 128 + 32
    BUF = Dm.get("BUF%d" % l, [NB * 128, D])
    IDXW = Dm.get("IDXW%d" % l, [128, NB], I32)
    DEST = Dm.get("DEST%d" % l, [128, ntf * 2], I32)
    WWd = Dm.get("WWd%d" % l, [128, ntf * 2])
    ramp_d = Dm.get("ramp", [1, 72], kind="ExternalInput")
    bst_d = Dm.get("bst", [1, 104], kind="ExternalInput")
    pidx_d = Dm.get("pidx", [128, 1], kind="ExternalInput")
    stri_d = Dm.get("stri", [128, 2, 128], kind="ExternalInput")
    OH = P.sb(pf + "OH", [128, ntf, 2, 32])
    WW = P.sb(pf + "WW", [128, ntf, 2])
    RK = P.sb(pf + "RK", [128, ntf, 32])
    Msum = P.sb(pf + "Msum", [128, 32])
    P.i("dve", "memset", writes=[pf + "Msum"], ap=Msum[:], constant=0.0)
    stri = P.sb(pf + "stri", [128, 2, 128]); P.d(stri[:], stri_d[:, :, :], writes=[pf + "stri"])
    Mt_r = Rot(P, pf + "Mt", [128, 32], n=2)
    zz = P.sb(pf + "zz", [128, 4096])
    P.i("pool", "memset", writes=[pf + "zz"], ap=zz[:], constant=0.0)
    rows_per = 128 * 4
    for r0 in range(0, NB * 128, rows_per):
        P.d(BUF[r0:r0 + rows_per, :].rearrange("(p a) n -> p (a n)", p=128), zz[:], reads=[pf + "zz"], writes=[P.u("BUFz%d" % l)])
    mk = "modrow%d" % l
    ident = P.sb(pf + "ident", [128, 128])
    P.d(ident[:], ident_d[:, :], writes=[pf + "ident"])
    wo = P.sb(pf + "wo", [128, 8, 1024], F32R)
    for k in range(8):
        P.d(wo[:, k, :], w_out[:, k, :], writes=[pf + "wo/%d" % k], q="pool")
    wr = P.sb(pf + "wr", [128, 8, 36])
    P.d(wr[:], wr_d[:, :, :], writes=[pf + "wr"])
    rb = P.sb(pf + "rb", [128, 36])
    load_bcast(P, rb[:], pf + "rb", rb_d[0:1, :])
    G1 = [P.sb(pf + "G1%d" % r, [128, D]) for r in range(2)]
    G2 = [P.sb(pf + "G2%d" % r, [128, D]) for r in range(2)]
    SH2 = [P.sb(pf + "SH2%d" % r, [128, D]) for r in range(2)]
    gn = P.sb(pf + "gn", [128, D])
    load_bcast(P, gn[:], pf + "gn", n2g[0:1, :])
    for r in range(2):
        load_bcast(P, G1[r][:], pf + "G1%d" % r, modrow[r:r + 1, 2048:3072], reads=[mk])
        load_bcast(P, SH2[r][:], pf + "SH2%d" % r, modrow[r:r + 1, 3072:4096], reads=[mk])
        load_bcast(P, G2[r][:], pf + "G2%d" % r, modrow[r:r + 1, 4096:5120], reads=[mk])
        P.i("dve", "scalar_tensor_tensor", reads=[pf + "G2%d" % r, pf + "gn"], writes=[pf + "G2%d" % r], out=G2[r][:], in0=G2[r][:], scalar=1.0, in1=gn[:], op0=ALU.add, op1=ALU.mult)
    epsc = P.sb(pf + "eps", [128, 1])
    P.i("dve", "memset", writes=[pf + "eps"], ap=epsc[:], constant=EPS)
    ct_r = Rot(P, pf + "ct", [128, 8, 512], F32R, n=2)
    po_r = Rot(P, pf + "po", [128, 1024], n=1, psum=True)
    xt_r = Rot(P, pf + "xt", [128, D], n=2)
    x1_r = Rot(P, pf + "x1", [128, D], n=2)
    h_r = Rot(P, pf + "h", [128, D], n=2)
    junk = P.sb(pf + "junk", [128, D])
    st_r = Rot(P, pf + "st", [128, 64], n=3)
    tp_r = Rot(P, pf + "tp", [128, 1024], n=1, psum=True)
    H2 = Dm.get("H2_%d" % l, [T, D])
    hTf_r = Rot(P, pf + "hTf", [128, 8, 128], n=2)
    pr_r = Rot(P, pf + "pr", [128, 512], n=1, psum=True)
    lg_r = Rot(P, pf + "lg", [128, 36], n=2)
    mk_r = Rot(P, pf + "mk", [128, 4, 8], n=2)
    oh_r = Rot(P, pf + "oh", [128, 3, 32], n=2)
    pw_r = Rot(P, pf + "pw", [128, 512], n=1, psum=True)
    tile_i = 0
    for (c0, cn) in chunks_:
        ct, ctk = ct_r.next()
        P.d(ct[:, :, 0:cn], CAT[:, c0:c0 + cn].rearrange("(k p) t -> p k t", p=128), reads=["CAT"], writes=[ctk], q="pool")
        for i in range(cn // 128):
            t0 = c0 + i * 128
            r = 1 if t0 < LC else 0
            po, pok = po_r.next()
            for hf in range(2):
                for k in range(8):
                    P.i("pe", "matmul", reads=[ctk, pf + "wo/%d" % k], writes=[pok], out=po[:, hf * 512:(hf + 1) * 512], lhsT=ct[:, k, i * 128:(i + 1) * 128],
                        rhs=wo[:, k, hf * 512:(hf + 1) * 512], start=(k == 0), stop=(k == 7))
            xt, xk = xt_r.next()
            P.d(xt[:], xsrc[t0:t0 + 128, :], reads=[src_name], writes=[xk])
            x1, x1k = x1_r.next()
            for hf in range(2):
                sl = slice(hf * 512, (hf + 1) * 512)
                P.i("dve", "tensor_tensor", reads=[pok, pf + "G1%d" % r], writes=[x1k + "/%d" % hf], out=x1[:, sl], in0=po[:, sl], in1=G1[r][:, sl], op=ALU.mult)
            P.i("pool", "tensor_tensor", reads=[x1k, xk], writes=[x1k], out=x1[:], in0=x1[:], in1=xt[:], op=ALU.add)
            P.d(X1[t0:t0 + 128, :], x1[:], reads=[x1k], writes=[P.u("X1")])
            st, sk = st_r.next()
            P.i("act", "activation", reads=[x1k], writes=[pf + "junk", sk], out=junk[:], in_=x1[:], func=AF.Square, accum_out=st[:, 0:1])
            P.i("act", "activation", reads=[sk, pf + "eps"], writes=[sk], out=st[:, 1:2], in_=st[:, 0:1], func=AF.Sqrt, bias=epsc[:], scale=1.0 / D)
            P.i("dve", "reciprocal", reads=[sk], writes=[sk], out=st[:, 2:3], in_=st[:, 1:2])
            h, hk = h_r.next()
            P.i("dve", "scalar_tensor_tensor", reads=[x1k, sk, pf + "G2%d" % r], writes=[hk], out=h[:], in0=x1[:], scalar=st[:, 2:3], in1=G2[r][:], op0=ALU.mult, op1=ALU.mult)
            P.i("pool", "tensor_tensor", reads=[hk, pf + "SH2%d" % r], writes=[hk], out=h[:], in0=h[:], in1=SH2[r][:], op=ALU.add)
            P.d(H2[t0:t0 + 128, :], h[:], reads=[hk], writes=[P.u("H2_%d" % l)])
            tp, tpk = tp_r.next()
            for k in range(8):
                P.i("pe", "transpose", reads=[hk, pf + "ident"], writes=[tpk], out=tp[:, k * 128:(k + 1) * 128], in_=h[:, k * 128:(k + 1) * 128], identity=ident[:])
            hTf, hTfk = hTf_r.next()
            P.i("dve", "tensor_copy", reads=[tpk], writes=[hTfk], out=hTf[:], in_=tp[:].rearrange("p (k t) -> p k t", k=8))
            pr, prk = pr_r.next()
            for k in range(8):
                P.i("pe", "matmul", reads=[hTfk, pf + "wr"], writes=[prk], out=pr[:, 0:36], lhsT=hTf[:, k, :], rhs=wr[:, k, :], start=(k == 0), stop=(k == 7))
            lg, lgk = lg_r.next()
            P.i("dve", "tensor_tensor", reads=[prk, pf + "rb"], writes=[lgk], out=lg[:], in0=pr[:, 0:36], in1=rb[:], op=ALU.add)
            P.i("dve", "tensor_reduce", reads=[lgk], writes=[sk], out=st[:, 4:5], in_=lg[:, 0:4], axis=AX.X, op=ALU.max)
            P.i("dve", "tensor_scalar", reads=[sk], writes=[sk], out=st[:, 5:6], in0=st[:, 4:5], scalar1=-1.0, scalar2=None, op0=ALU.mult)
            P.i("act", "activation", reads=[lgk, sk], writes=[sk], out=st[:, 32:36], in_=lg[:, 0:4], func=AF.Exp, bias=st[:, 5:6], accum_out=st[:, 6:7])
            P.i("dve", "reciprocal", reads=[sk], writes=[sk], out=st[:, 7:8], in_=st[:, 6:7])
            P.i("dve", "tensor_scalar", reads=[lgk, sk], writes=[sk], out=st[:, 8:12], in0=lg[:, 0:4], scalar1=st[:, 4:5], scalar2=None, op0=ALU.is_equal)
            P.i("dve", "tensor_scalar", reads=[sk], writes=[sk], out=st[:, 12:16], in0=st[:, 8:12], scalar1=BIG, scalar2=-BIG, op0=ALU.mult, op1=ALU.add)
            mkd, mkk = mk_r.next()
            P.i("dve", "tensor_tensor", reads=[lgk, sk], writes=[mkk], out=mkd[:], in0=lg[:, 4:36].rearrange("p (g e) -> p g e", g=4),
                in1=st[:, 12:16].unsqueeze(2).to_broadcast([128, 4, 8]), op=ALU.add)
            mflat = mkd[:].rearrange("p g e -> p (g e)")
            P.i("dve", "max", reads=[mkk], writes=[sk], out=st[:, 16:24], in_=mflat)
            oh, ohk = oh_r.next()
            P.i("dve", "tensor_scalar", reads=[mkk, sk], writes=[ohk + "/1"], out=oh[:, 0, :], in0=mflat, scalar1=st[:, 16:17], scalar2=None, op0=ALU.is_equal)
            P.i("dve", "tensor_scalar", reads=[mkk, sk], writes=[ohk + "/2"], out=oh[:, 1, :], in0=mflat, scalar1=st[:, 17:18], scalar2=None, op0=ALU.is_equal)
            P.i("dve", "tensor_tensor", reads=[sk], writes=[sk], out=st[:, 24:25], in0=st[:, 17:18], in1=st[:, 16:17], op=ALU.subtract)
            P.i("act", "activation", reads=[sk], writes=[sk], out=st[:, 25:26], in_=st[:, 24:25], func=AF.Exp)
            P.i("dve", "tensor_scalar", reads=[sk], writes=[sk], out=st[:, 26:27], in0=st[:, 25:26], scalar1=1.0, scalar2=None, op0=ALU.add)
            P.i("dve", "reciprocal", reads=[sk], writes=[sk], out=st[:, 27:28], in_=st[:, 26:27])
            P.i("dve", "tensor_tensor", reads=[sk], writes=[sk], out=st[:, 28:29], in0=st[:, 27:28], in1=st[:, 7:8], op=ALU.mult)
            P.i("dve", "tensor_tensor", reads=[sk], writes=[sk], out=st[:, 29:30], in0=st[:, 7:8], in1=st[:, 28:29], op=ALU.subtract)
            ti = tile_i
            tile_i += 1
            P.i("act", "activation", reads=[ohk + "/1"], writes=[pf + "OH/%d_0" % ti], out=OH[:, ti, 0, :], in_=oh[:, 0, :], func=AF.Copy)
            P.i("act", "activation", reads=[ohk + "/2"], writes=[pf + "OH/%d_1" % ti], out=OH[:, ti, 1, :], in_=oh[:, 1, :], func=AF.Copy)
            P.i("act", "activation", reads=[sk], writes=[pf + "WW/%d" % ti], out=WW[:, ti, :], in_=st[:, 28:30], func=AF.Copy)
            Mt, Mtk = Mt_r.next()
            P.i("dve", "tensor_tensor", reads=[ohk + "/1", ohk + "/2"], writes=[Mtk], out=Mt[:], in0=oh[:, 0, :], in1=oh[:, 1, :], op=ALU.add)
            pw, pwk = pw_r.next()
            P.i("pe", "matmul", reads=[Mtk, pf + "stri"], writes=[pwk], out=pw[:, 0:32], lhsT=stri[:, 0, :], rhs=Mt[:], start=True, stop=False)
            P.i("pe", "matmul", reads=[pf + "Msum", pf + "stri"], writes=[pwk], out=pw[:, 0:32], lhsT=stri[:, 1, :], rhs=Msum[:], start=False, stop=True)
            P.i("act", "activation", reads=[pwk], writes=[pf + "RK/%d" % ti], out=RK[:, ti, :], in_=pw[:, 0:32], func=AF.Copy)
            P.i("dve", "tensor_tensor", reads=[Mtk, pf + "Msum"], writes=[pf + "Msum"], out=Msum[:], in0=Msum[:], in1=Mt[:], op=ALU.add)
    ramp = P.sb(pf + "ramp", [128, 72]); load_bcast(P, ramp[:], pf + "ramp", ramp_d[0:1, :])
    bst = P.sb(pf + "bst", [128, 104]); load_bcast(P, bst[:], pf + "bst", bst_d[0:1, :])
    pidx = P.sb(pf + "pidx", [128, 1]); P.d(pidx[:], pidx_d[:, :], writes=[pf + "pidx"])
    q = P.sb(pf + "q", [128, 8, 32])
    QK = pf + "q"
    big = P.sb(pf + "big", [128, 104 * 32])
    ones32 = P.sb(pf + "ones32", [128, 32]); P.i("dve", "memset", writes=[pf + "ones32"], ap=ones32[:], constant=1.0)
    pw, pwk = pw_r.next()
    P.i("pe", "matmul", reads=[pf + "Msum", pf + "stri"], writes=[pwk], out=pw[:, 0:32], lhsT=stri[:, 1, :], rhs=Msum[:], start=True, stop=True)
    P.i("dve", "tensor_copy", reads=[pwk], writes=[QK], out=q[:, 0, :], in_=pw[:, 0:32])
    NM = 2 * ntf
    b3 = big[:, 0:32 * NM].rearrange("p (e m) -> p e m", e=32)
    P.i("dve", "tensor_tensor", reads=[QK, pf + "ramp"], writes=[pf + "big"], out=b3, in0=q[:, 0, :].unsqueeze(2).to_broadcast([128, 32, NM]),
        in1=ramp[:, 0:NM].unsqueeze(1).to_broadcast([128, 32, NM]), op=ALU.is_ge)
    P.i("dve", "tensor_reduce", reads=[pf + "big"], writes=[QK], out=q[:, 1, :], in_=b3, axis=AX.X, op=ALU.add)
    P.i("dve", "tensor_scalar", reads=[QK], writes=[QK], out=q[:, 1, :], in0=q[:, 1, :], scalar1=128.0, scalar2=None, op0=ALU.mult)
    P.i("dve", "tensor_tensor_scan", reads=[QK, pf + "ones32"], writes=[QK], out=q[:, 2, :], data0=ones32[:], data1=q[:, 1, :], initial=0.0, op0=ALU.mult, op1=ALU.add)
    P.i("dve", "tensor_tensor", reads=[QK], writes=[QK], out=q[:, 3, :], in0=q[:, 2, :], in1=q[:, 1, :], op=ALU.subtract)
    be = P.sb(pf + "be", [128, 104])
    b3 = big[:, 0:NB * 32].rearrange("p (b e) -> p b e", e=32)
    P.i("dve", "tensor_tensor", reads=[QK, pf + "bst"], writes=[pf + "big"], out=b3, in0=q[:, 2, :].unsqueeze(1).to_broadcast([128, NB, 32]),
        in1=bst[:, 0:NB].unsqueeze(2).to_broadcast([128, NB, 32]), op=ALU.is_le)
    P.i("dve", "tensor_reduce", reads=[pf + "big"], writes=[pf + "be"], out=be[:, 0:NB], in_=b3, axis=AX.X, op=ALU.add)
    P.i("dve", "tensor_scalar", reads=[pf + "be"], writes=[pf + "be"], out=be[:, 0:NB], in0=be[:, 0:NB], scalar1=31.0, scalar2=128.0, op0=ALU.min, op1=ALU.mult)
    P.i("dve", "tensor_scalar", reads=[pf + "be", pf + "pidx"], writes=[pf + "be"], out=be[:, 0:NB], in0=be[:, 0:NB], scalar1=pidx[:, 0:1], scalar2=None, op0=ALU.add)
    bei = P.sb(pf + "bei", [128, 104], I32)
    P.i("dve", "tensor_copy", reads=[pf + "be"], writes=[pf + "bei"], out=bei[:, 0:NB], in_=be[:, 0:NB])
    P.d(IDXW[:, :], bei[:, 0:NB], reads=[pf + "bei"], writes=[P.u("IDXW%d" % l)])
    dsf = P.sb(pf + "dsf", [128, ntf, 2])
    pos_r = Rot(P, pf + "pos", [128, 2, 32], n=2)
    for ti in range(ntf):
        pos, posk = pos_r.next()
        P.i("dve", "tensor_tensor", reads=[pf + "RK/%d" % ti, QK], writes=[posk + "/p"], out=pos[:, 0, :], in0=RK[:, ti, :], in1=q[:, 3, :], op=ALU.add)
        for k in range(2):
            P.i("dve", "tensor_tensor", reads=[posk + "/p", pf + "OH/%d_%d" % (ti, k)], writes=[posk + "/t"], out=pos[:, 1, :], in0=pos[:, 0, :], in1=OH[:, ti, k, :], op=ALU.mult)
            P.i("dve", "tensor_reduce", reads=[posk + "/t"], writes=[pf + "dsf/%d_%d" % (ti, k)], out=dsf[:, ti, k:k + 1], in_=pos[:, 1, :], axis=AX.X, op=ALU.add)
    dsi = P.sb(pf + "dsi", [128, ntf * 2], I32)
    P.i("dve", "tensor_copy", reads=[pf + "dsf"], writes=[pf + "dsi"], out=dsi[:], in_=dsf[:].rearrange("p t k -> p (t k)"))
    P.d(DEST[:, :], dsi[:], reads=[pf + "dsi"], writes=[P.u("DEST%d" % l)])
    P.d(WWd[:, :], WW[:].rearrange("p t k -> p (t k)"), reads=[pf + "WW"], writes=[P.u("WWd%d" % l)])
    h2_r = Rot(P, pf + "h2s", [128, D], n=3)
    ti = 0
    for (c0, cn) in chunks_:
        for i in range(cn // 128):
            t0 = c0 + i * 128
            h2, h2k = h2_r.next()
            P.d(h2[:], H2[t0:t0 + 128, :], reads=["H2_%d" % l], writes=[h2k])
            for k in range(2):
                P.dma(lambda e, h2=h2, col=ti * 2 + k: e.indirect_dma_start(out=BUF[:, :], out_offset=bass.IndirectOffsetOnAxis(ap=dsi[:, col:col + 1], axis=0), in_=h2[:], in_offset=None),
                      reads=[h2k, pf + "dsi", "BUFz%d" % l], writes=[P.u("BUF%d" % l)], q="pool")
            ti += 1


def phase_g(P, Dm, l, tok_chunks, final):
    pf = "g%d_" % l
    X1 = Dm.get("X1", [T, D])
    H2T = Dm.get("H2T", [D, T])
    WTd = Dm.get("WTd", [32, T])
    WG = Dm.get("moe_g%d" % l, [32, 128, 8, 512], kind="ExternalInput")
    WU = Dm.get("moe_u%d" % l, [32, 128, 8, 512], kind="ExternalInput")
    WD = Dm.get("moe_d%d" % l, [32, 128, 4, 1024], kind="ExternalInput")
    modrow = Dm.get("modrow%d" % l, [2, 6144])
    mk = "modrow%d" % l
    if final:
        fg_d = Dm.get("final_g", [1, D], kind="ExternalInput")
        OUT = Dm.get("out", [L, D], kind="ExternalOutput")
        fg = P.sb(pf + "fg", [128, D])
        load_bcast(P, fg[:], pf + "fg", fg_d[0:1, :])
        epsc = P.sb(pf + "eps", [128, 1])
        P.i("dve", "memset", writes=[pf + "eps"], ap=epsc[:], constant=EPS)
        junk = P.sb(pf + "junk", [128, D])
    else:
        XN = Dm.get("XN%d" % l, [T, D])
    G2g = [P.sb(pf + "g2%d" % r, [128, D]) for r in range(2)]
    for r in range(2):
        load_bcast(P, G2g[r][:], pf + "g2%d" % r, modrow[r:r + 1, 5120:6144], reads=[mk])
    hT_r = Rot(P, pf + "hT", [128, 8, 512], F32R, n=2)
    acc_r = Rot(P, pf + "acc", [128, 4, 1024], n=1)
    wg_r = Rot(P, pf + "wg", [128, 8, 512], F32R, n=2)
    wu_r = Rot(P, pf + "wu", [128, 8, 512], F32R, n=2)
    wd_r = Rot(P, pf + "wd", [128, 4, 1024], F32R, n=2)
    wb_r = Rot(P, pf + "wb", [128, 512], n=2)
    pg_r = Rot(P, pf + "pg", [128, 512], n=2, psum=True)
    pu_r = Rot(P, pf + "pu", [128, 512], n=2, psum=True)
    pd_r = Rot(P, pf + "pd", [128, 512], n=2, psum=True)
    sg_r = Rot(P, pf + "sg", [128, 512], n=2)
    hu_r = Rot(P, pf + "hu", [128, 512], n=2)
    hid_r = Rot(P, pf + "hid", [128, 4, 512], F32R, n=2)
    xt_r = Rot(P, pf + "xt", [128, D], n=2)
    st_r = Rot(P, pf + "st", [128, 8], n=2)
    for (c0, cn) in tok_chunks:
        nt = cn // 128
        hT, hTk = hT_r.next()
        P.d(hT[:, :, 0:cn], H2T[:, c0:c0 + cn].rearrange("(k p) t -> p k t", p=128), reads=["H2T"], writes=[hTk], q="pool")
        acc, acck = acc_r.next()
        for e in range(32):
            wg, wgk = wg_r.next()
            wu, wuk = wu_r.next()
            wd, wdk = wd_r.next()
            P.d(wg[:], WG[e], writes=[wgk], q="pool")
            P.d(wu[:], WU[e], writes=[wuk], q="pool")
            P.d(wd[:], WD[e], writes=[wdk], q="pool")
            wb, wbk = wb_r.next()
            P.d(wb[:, 0:cn], WTd[e:e + 1, c0:c0 + cn].to_broadcast([128, cn]), reads=["WTd"], writes=[wbk])
            hid, hidk = hid_r.next()
            for hc in range(4):
                pg, pgk = pg_r.next()
                pu, puk = pu_r.next()
                for k in range(8):
                    P.i("pe", "matmul", reads=[wgk, hTk], writes=[pgk], out=pg[:, 0:cn], lhsT=wg[:, k, hc * 128:(hc + 1) * 128], rhs=hT[:, k, 0:cn], start=(k == 0), stop=(k == 7))
                for k in range(8):
                    P.i("pe", "matmul", reads=[wuk, hTk], writes=[puk], out=pu[:, 0:cn], lhsT=wu[:, k, hc * 128:(hc + 1) * 128], rhs=hT[:, k, 0:cn], start=(k == 0), stop=(k == 7))
                sg, sgk = sg_r.next()
                P.i("act", "activation", reads=[pgk], writes=[sgk], out=sg[:, 0:cn], in_=pg[:, 0:cn], func=AF.Silu)
                hu, huk = hu_r.next()
                P.i("dve", "tensor_tensor", reads=[sgk, puk], writes=[huk], out=hu[:, 0:cn], in0=sg[:, 0:cn], in1=pu[:, 0:cn], op=ALU.mult)
                P.i("pool", "tensor_tensor", reads=[huk, wbk], writes=[hidk + "/%d" % hc], out=hid[:, hc, 0:cn], in0=hu[:, 0:cn], in1=wb[:, 0:cn], op=ALU.mult)
            hkeys = [hidk + "/%d" % hc for hc in range(4)]
            for tt in range(nt):
                for hf in range(2):
                    pd, pdk = pd_r.next()
                    for hc in range(4):
                        P.i("pe", "matmul", reads=hkeys + [wdk], writes=[pdk], out=pd[:], lhsT=hid[:, hc, tt * 128:(tt + 1) * 128], rhs=wd[:, hc, hf * 512:(hf + 1) * 512],
                            start=(hc == 0), stop=(hc == 3))
                    ak = acck + "/%d_%d" % (tt, hf)
                    if e == 0:
                        P.i("dve", "tensor_copy", reads=[pdk], writes=[ak], out=acc[:, tt, hf * 512:(hf + 1) * 512], in_=pd[:])
                    else:
                        P.i("dve", "tensor_tensor", reads=[pdk, ak], writes=[ak], out=acc[:, tt, hf * 512:(hf + 1) * 512], in0=acc[:, tt, hf * 512:(hf + 1) * 512], in1=pd[:], op=ALU.add)
        for tt in range(nt):
            t0 = c0 + tt * 128
            r = 1 if t0 < LC else 0
            aks = [acck + "/%d_%d" % (tt, hf) for hf in range(2)]
            xt, xk = xt_r.next()
            P.d(xt[:], X1[t0:t0 + 128, :], reads=["X1"], writes=[xk])
            P.i("pool", "tensor_tensor", reads=aks + [pf + "g2%d" % r], writes=aks, out=acc[:, tt, :], in0=acc[:, tt, :], in1=G2g[r][:], op=ALU.mult)
            P.i("dve", "tensor_tensor", reads=aks + [xk], writes=[xk], out=xt[:], in0=xt[:], in1=acc[:, tt, :], op=ALU.add)
            if not final:
                P.d(XN[t0:t0 + 128, :], xt[:], reads=[xk], writes=[P.u("XN%d" % l)])
            else:
                st, sk = st_r.next()
                P.i("act", "activation", reads=[xk], writes=[pf + "junk", sk], out=junk[:], in_=xt[:], func=AF.Square, accum_out=st[:, 0:1])
                P.i("act", "activation", reads=[sk, pf + "eps"], writes=[sk], out=st[:, 1:2], in_=st[:, 0:1], func=AF.Sqrt, bias=epsc[:], scale=1.0 / D)
                P.i("dve", "reciprocal", reads=[sk], writes=[sk], out=st[:, 2:3], in_=st[:, 1:2])
                P.i("dve", "scalar_tensor_tensor", reads=[xk, sk, pf + "fg"], writes=[xk], out=xt[:], in0=xt[:], scalar=st[:, 2:3], in1=fg[:], op0=ALU.mult, op1=ALU.mult)
                P.d(OUT[t0 - LC:t0 - LC + 128, :], xt[:], reads=[xk], writes=[P.u("out")], final=True)


def phase_b(P, Dm, l):
    pf = "b%d_" % l
    FMT = Dm.get("FMT", [1024, T])
    CAT = Dm.get("CAT", [1024, T])
    Bd = Dm.get("s5B%d" % l, [2, 2, 8, 128, 128], kind="ExternalInput")
    Cd = Dm.get("s5C%d" % l, [2, 2, 8, 128, 128], kind="ExternalInput")
    lam_d = Dm.get("s5lam%d" % l, [128, 3, 16], kind="ExternalInput")
    dsk_d = Dm.get("s5d%d" % l, [128, 2], kind="ExternalInput")
    gw_d = Dm.get("gluw%d" % l, [128, 2, 256], kind="ExternalInput")
    gb_d = Dm.get("glub%d" % l, [128, 2], kind="ExternalInput")
    NMAX = 512
    lam = P.sb(pf + "lam", [128, 3, 16])
    P.d(lam[:], lam_d[:, :, :], writes=[pf + "lam"])
    dsk = P.sb(pf + "dsk", [128, 2]); P.d(dsk[:], dsk_d[:, :], writes=[pf + "dsk"])
    gb = P.sb(pf + "gb", [128, 2]); P.d(gb[:], gb_d[:, :], writes=[pf + "gb"])
    gw = P.sb(pf + "gw", [128, 2, 256], F32R); P.d(gw[:], gw_d[:, :, :], writes=[pf + "gw"], q="pool")
    Bb = P.sb(pf + "Bb", [128, 16, 128], F32R)
    Cb = P.sb(pf + "Cb", [128, 16, 128], F32R)
    sc = P.sb(pf + "sc", [128, 24, 16])
    K_ = pf + "sc"
    LR, LI, LDT = lam[:, 0, :], lam[:, 1, :], lam[:, 2, :]
    (DT, RM, TH, C_, S_, T1, T2, T3, ARE, AIM, DEN, AM1, FRE, FIM, HP) = range(15)

    def S(i):
        return sc[:, i, :]

    def tt(o, a, b, op, eng="dve"):
        P.i(eng, "tensor_tensor", reads=[K_, pf + "lam"], writes=[K_], out=o, in0=a, in1=b, op=op)

    hpi = P.sb(pf + "hpi", [128, 1])
    P.i("dve", "memset", writes=[pf + "hpi"], ap=hpi[:], constant=float(np.pi / 2))
    P.i("act", "activation", reads=[pf + "lam"], writes=[K_], out=S(DT), in_=LDT, func=AF.Exp)
    tt(S(RM), LR, S(DT), ALU.mult)
    P.i("act", "activation", reads=[K_], writes=[K_], out=S(RM), in_=S(RM), func=AF.Exp)
    tt(S(TH), LI, S(DT), ALU.mult)
    P.i("act", "activation", reads=[K_], writes=[K_], out=S(S_), in_=S(TH), func=AF.Sin, scale=1.0 / 32)
    P.i("act", "activation", reads=[K_, pf + "hpi"], writes=[K_], out=S(C_), in_=S(TH), func=AF.Sin, scale=1.0 / 32, bias=hpi[:])
    for _ in range(5):
        tt(S(T1), S(C_), S(C_), ALU.mult)
        tt(S(T2), S(S_), S(S_), ALU.mult)
        tt(S(T3), S(C_), S(S_), ALU.mult)
        tt(S(C_), S(T1), S(T2), ALU.subtract)
        tt(S(S_), S(T3), S(T3), ALU.add)
    tt(S(ARE), S(RM), S(C_), ALU.mult)
    tt(S(AIM), S(RM), S(S_), ALU.mult)
    tt(S(T1), LR, LR, ALU.mult)
    tt(S(T2), LI, LI, ALU.mult)
    tt(S(DEN), S(T1), S(T2), ALU.add)
    P.i("dve", "reciprocal", reads=[K_], writes=[K_], out=S(DEN), in_=S(DEN))
    P.i("dve", "tensor_scalar", reads=[K_], writes=[K_], out=S(AM1), in0=S(ARE), scalar1=-1.0, scalar2=None, op0=ALU.add)
    tt(S(T1), S(AM1), LR, ALU.mult)
    tt(S(T2), S(AIM), LI, ALU.mult)
    tt(S(T1), S(T1), S(T2), ALU.add)
    tt(S(FRE), S(T1), S(DEN), ALU.mult)
    tt(S(T1), S(AIM), LR, ALU.mult)
    tt(S(T2), S(AM1), LI, ALU.mult)
    tt(S(T1), S(T1), S(T2), ALU.subtract)
    tt(S(FIM), S(T1), S(DEN), ALU.mult)
    Ep = P.sb(pf + "Ep", [128, 2, 8, NMAX])
    Tm = P.sb(pf + "Tm", [128, 2, 8, NMAX])
    pw = P.sb(pf + "pw", [128, 4, 8])
    tb = P.sb(pf + "tb", [128, 2, 8, NMAX // 2])
    carry = P.sb(pf + "carry", [128, 16, 2])
    P.i("dve", "memset", writes=[pf + "carry"], ap=carry[:], constant=0.0)
    yacc = P.sb(pf + "yacc", [128, 2, T])
    uc_r = Rot(P, pf + "uc", [128, 2, NMAX], F32R, n=2)
    pb_r = Rot(P, pf + "pb", [128, 512], n=4, psum=True)
    py_r = Rot(P, pf + "py", [128, 512], n=2, psum=True)
    br_r = Rot(P, pf + "br", [128, 2, NMAX], n=2)
    t_r = Rot(P, pf + "t", [128, 4, NMAX], n=2)
    v_r = Rot(P, pf + "v", [128, 2, NMAX], n=2)
    g_r = Rot(P, pf + "g", [128, 2, NMAX], n=2)
    h_r = Rot(P, pf + "h", [128, 2, NMAX], F32R, n=2)
    TK, EK = pf + "Tm", pf + "Ep"
    for d in range(2):
        dsl = slice(d * 8, d * 8 + 8)
        P.d(Bb[:], Bd[d].rearrange("c j k m -> k (c j) m"), writes=[pf + "Bb"], q="pool")
        P.d(Cb[:], Cd[d].rearrange("c j k m -> k (c j) m"), writes=[pf + "Cb"], q="pool")
        P.i("act", "activation", reads=[pf + "Cb"], writes=[pf + "Cb"], out=Cb[:, 8:16, :], in_=Cb[:, 8:16, :].bitcast(F32), func=AF.Copy, scale=-1.0)
        P.i("dve", "tensor_copy", reads=[K_], writes=[EK], out=Ep[:, 0, :, 0:1], in_=S(C_)[:, dsl].unsqueeze(2))
        P.i("dve", "tensor_copy", reads=[K_], writes=[EK], out=Ep[:, 1, :, 0:1], in_=S(S_)[:, dsl].unsqueeze(2))
        P.i("dve", "tensor_copy", reads=[K_], writes=[pf + "pw"], out=pw[:, 0, :], in_=S(C_)[:, dsl])
        P.i("dve", "tensor_copy", reads=[K_], writes=[pf + "pw"], out=pw[:, 1, :], in_=S(S_)[:, dsl])
        n = 1
        while n < NMAX:
            cn_b = pw[:, 0, :].unsqueeze(2).to_broadcast([128, 8, n])
            sn_b = pw[:, 1, :].unsqueeze(2).to_broadcast([128, 8, n])
            ire, iim = Ep[:, 0, :, 0:n], Ep[:, 1, :, 0:n]
            ore, oim = Ep[:, 0, :, n:2 * n], Ep[:, 1, :, n:2 * n]
            P.i("dve", "tensor_tensor", reads=[EK, pf + "pw"], writes=[pf + "tb"], out=tb[:, 0, :, 0:n], in0=iim, in1=sn_b, op=ALU.mult)
            P.i("pool", "tensor_tensor", reads=[EK, pf + "pw"], writes=[pf + "tb2"], out=tb[:, 1, :, 0:n], in0=iim, in1=cn_b, op=ALU.mult)
            P.i("dve", "tensor_tensor", reads=[EK, pf + "pw"], writes=[EK + "/a"], out=ore, in0=ire, in1=cn_b, op=ALU.mult)
            P.i("pool", "tensor_tensor", reads=[EK, pf + "pw"], writes=[EK + "/b"], out=oim, in0=ire, in1=sn_b, op=ALU.mult)
            P.i("dve", "tensor_tensor", reads=[EK + "/a", pf + "tb"], writes=[EK + "/a"], out=ore, in0=ore, in1=tb[:, 0, :, 0:n], op=ALU.subtract)
            P.i("pool", "tensor_tensor", reads=[EK + "/b", pf + "tb2"], writes=[EK + "/b"], out=oim, in0=oim, in1=tb[:, 1, :, 0:n], op=ALU.add)
            P.i("dve", "tensor_tensor", reads=[pf + "pw"], writes=[pf + "pw"], out=pw[:, 2, :], in0=pw[:, 0, :], in1=pw[:, 1, :], op=ALU.mult)
            P.i("dve", "tensor_tensor", reads=[pf + "pw"], writes=[pf + "pw"], out=pw[:, 0, :], in0=pw[:, 0, :], in1=pw[:, 0, :], op=ALU.mult)
            P.i("dve", "tensor_tensor", reads=[pf + "pw"], writes=[pf + "pw"], out=pw[:, 3, :], in0=pw[:, 1, :], in1=pw[:, 1, :], op=ALU.mult)
            P.i("dve", "tensor_tensor", reads=[pf + "pw"], writes=[pf + "pw"], out=pw[:, 0, :], in0=pw[:, 0, :], in1=pw[:, 3, :], op=ALU.subtract)
            P.i("dve", "tensor_tensor", reads=[pf + "pw"], writes=[pf + "pw"], out=pw[:, 1, :], in0=pw[:, 2, :], in1=pw[:, 2, :], op=ALU.add)
            n *= 2
        for j in range(8):
            fr = S(FRE)[:, d * 8 + j:d * 8 + j + 1]
            fi = S(FIM)[:, d * 8 + j:d * 8 + j + 1]
            t, tk = t_r.next()
            P.i("dve", "tensor_scalar", reads=[EK, K_], writes=[tk + "/0"], out=t[:, 0, :], in0=Ep[:, 1, j, :], scalar1=fi, scalar2=None, op0=ALU.mult)
            P.i("dve", "scalar_tensor_tensor", reads=[EK, K_, tk + "/0"], writes=[TK + "/a%d" % j], out=Tm[:, 0, j, :], in0=Ep[:, 0, j, :], scalar=fr, in1=t[:, 0, :], op0=ALU.mult, op1=ALU.add)
            P.i("dve", "tensor_scalar", reads=[EK, K_], writes=[tk + "/1"], out=t[:, 1, :], in0=Ep[:, 1, j, :], scalar1=fr, scalar2=None, op0=ALU.mult)
            P.i("dve", "scalar_tensor_tensor", reads=[EK, K_, tk + "/1"], writes=[TK + "/b%d" % j], out=Tm[:, 1, j, :], in0=Ep[:, 0, j, :], scalar=fi, in1=t[:, 1, :], op0=ALU.mult, op1=ALU.subtract)
        order = CHUNKS if d == 0 else [CHUNKS[0]] + CHUNKS[:0:-1]
        rev = (d == 1)

        def R(ap):
            return ap[:, ::-1] if rev else ap

        for (c0, N) in order:
            uc, uck = uc_r.next()
            P.d(uc[:, :, 0:N], FMT[0:256, c0:c0 + N].rearrange("(oc p) t -> p oc t", p=128), reads=["FMT"], writes=[uck], q="pool")
            pys = [py_r.next() for _ in range(2)]
            for j in range(8):
                oc = j // 4
                dj = d * 8 + j
                pbr, pbrk = pb_r.next()
                pbi, pbik = pb_r.next()
                P.i("pe", "matmul", reads=[pf + "Bb", uck], writes=[pbrk], out=pbr[:, 0:N], lhsT=Bb[:, j, :], rhs=uc[:, oc, 0:N], start=True, stop=True)
                P.i("pe", "matmul", reads=[pf + "Bb", uck], writes=[pbik], out=pbi[:, 0:N], lhsT=Bb[:, 8 + j, :], rhs=uc[:, oc, 0:N], start=True, stop=True)
                br, brk = br_r.next()
                P.i("act", "activation", reads=[pbrk], writes=[brk + "/0"], out=br[:, 0, 0:N], in_=R(pbr[:, 0:N]), func=AF.Copy)
                P.i("act", "activation", reads=[pbik], writes=[brk + "/1"], out=br[:, 1, 0:N], in_=R(pbi[:, 0:N]), func=AF.Copy)
                t, tk = t_r.next()
                v, vk = v_r.next()
                b0, b1 = br[:, 0, 0:N], br[:, 1, 0:N]
                tmr, tmi = Tm[:, 0, j, 0:N], Tm[:, 1, j, 0:N]
                P.i("dve", "tensor_tensor", reads=[brk + "/0", TK], writes=[tk + "/0"], out=t[:, 0, 0:N], in0=tmr, in1=b0, op=ALU.mult)
                P.i("pool", "tensor_tensor", reads=[brk + "/1", TK], writes=[tk + "/1"], out=t[:, 1, 0:N], in0=tmi, in1=b1, op=ALU.mult)
                P.i("pool", "tensor_tensor", reads=[brk + "/1", TK], writes=[tk + "/2"], out=t[:, 2, 0:N], in0=tmr, in1=b1, op=ALU.mult)
                P.i("pool", "tensor_tensor", reads=[brk + "/0", TK], writes=[tk + "/3"], out=t[:, 3, 0:N], in0=tmi, in1=b0, op=ALU.mult)
                P.i("dve", "tensor_tensor", reads=[tk + "/0", tk + "/1"], writes=[vk + "/0"], out=v[:, 0, 0:N], in0=t[:, 0, 0:N], in1=t[:, 1, 0:N], op=ALU.subtract)
                P.i("dve", "tensor_tensor", reads=[tk + "/2", tk + "/3"], writes=[vk + "/1"], out=v[:, 1, 0:N], in0=t[:, 2, 0:N], in1=t[:, 3, 0:N], op=ALU.add)
                g, gk = g_r.next()
                for c in range(2):
                    P.i("dve", "tensor_tensor_scan", reads=[vk + "/%d" % c, K_, pf + "carry"], writes=[gk + "/%d" % c], out=g[:, c, 0:N], data0=S(RM)[:, dj:dj + 1].to_broadcast([128, N]), data1=v[:, c, 0:N],
                        initial=carry[:, dj, c:c + 1], op0=ALU.mult, op1=ALU.add)
                h, hk = h_r.next()
                t, tk = t_r.next()
                epr, epi = Ep[:, 0, j, 0:N], Ep[:, 1, j, 0:N]
                g0, g1 = g[:, 0, 0:N], g[:, 1, 0:N]
                P.i("dve", "tensor_tensor", reads=[gk + "/0", EK], writes=[tk + "/0"], out=t[:, 0, 0:N], in0=epr, in1=g0, op=ALU.mult)
                P.i("pool", "tensor_tensor", reads=[gk + "/1", EK], writes=[tk + "/1"], out=t[:, 1, 0:N], in0=epi, in1=g1, op=ALU.mult)
                P.i("pool", "tensor_tensor", reads=[gk + "/1", EK], writes=[tk + "/2"], out=t[:, 2, 0:N], in0=epr, in1=g1, op=ALU.mult)
                P.i("dve", "tensor_tensor", reads=[gk + "/0", EK], writes=[tk + "/3"], out=t[:, 3, 0:N], in0=epi, in1=g0, op=ALU.mult)
                P.i("dve", "tensor_tensor", reads=[tk + "/0", tk + "/1"], writes=[hk + "/0"], out=h[:, 0, 0:N], in0=t[:, 0, 0:N], in1=t[:, 1, 0:N], op=ALU.subtract)
                P.i("dve", "tensor_tensor", reads=[tk + "/2", tk + "/3"], writes=[hk + "/1"], out=h[:, 1, 0:N], in0=t[:, 2, 0:N], in1=t[:, 3, 0:N], op=ALU.add)
                P.i("act", "activation", reads=[hk], writes=[pf + "carry"], out=carry[:, dj, :], in_=h[:].bitcast(F32)[:, :, N - 1], func=AF.Copy)
                py, pyk = pys[oc]
                P.i("pe", "matmul", reads=[pf + "Cb", hk + "/0"], writes=[pyk], out=py[:, 0:N], lhsT=Cb[:, j, :], rhs=h[:, 0, 0:N], start=(j % 4 == 0), stop=False)
                P.i("pe", "matmul", reads=[pf + "Cb", hk + "/1"], writes=[pyk], out=py[:, 0:N], lhsT=Cb[:, 8 + j, :], rhs=h[:, 1, 0:N], start=False, stop=(j % 4 == 3))
            for oc in range(2):
                py, pyk = pys[oc]
                ya = yacc[:, oc, c0:c0 + N]
                yk = pf + "yacc/%d_%d" % (oc, c0)
                if d == 0:
                    P.i("dve", "scalar_tensor_tensor", reads=[uck, pf + "dsk", pyk], writes=[yk], out=ya, in0=uc[:, oc, 0:N].bitcast(F32), scalar=dsk[:, oc:oc + 1], in1=py[:, 0:N], op0=ALU.mult, op1=ALU.add)
                else:
                    P.i("dve", "tensor_tensor", reads=[yk, pyk], writes=[yk], out=ya, in0=ya, in1=R(py[:, 0:N]), op=ALU.add)
    e_r = Rot(P, pf + "e", [128, 3, NMAX], n=1)
    gT_r = Rot(P, pf + "gT", [128, 2, NMAX], F32R, n=1)
    pq_r = Rot(P, pf + "pq", [128, 512], n=2, psum=True)
    a_r = Rot(P, pf + "a", [128, NMAX], n=2)
    for (c0, N) in CHUNKS:
        gT, gTk = gT_r.next()
        for oc in range(2):
            y = yacc[:, oc, c0:c0 + N]
            yk = pf + "yacc/%d_%d" % (oc, c0)
            ee, ek = e_r.next()
            P.i("pool", "tensor_tensor", reads=[yk], writes=[ek + "/0"], out=ee[:, 0, 0:N], in0=y, in1=y, op=ALU.mult)
            P.i("dve", "tensor_scalar", reads=[ek + "/0"], writes=[ek + "/0"], out=ee[:, 0, 0:N], in0=ee[:, 0, 0:N], scalar1=0.044715, scalar2=1.0, op0=ALU.mult, op1=ALU.add)
            P.i("pool", "tensor_tensor", reads=[ek + "/0", yk], writes=[ek + "/1"], out=ee[:, 1, 0:N], in0=ee[:, 0, 0:N], in1=y, op=ALU.mult)
            P.i("act", "activation", reads=[ek + "/1"], writes=[ek + "/2"], out=ee[:, 2, 0:N], in_=ee[:, 1, 0:N], func=AF.Sigmoid, scale=1.5957691216057308)
            P.i("dve", "tensor_tensor", reads=[ek + "/2", yk], writes=[gTk + "/%d" % oc], out=gT[:, oc, 0:N], in0=ee[:, 2, 0:N], in1=y, op=ALU.mult)
        for oc2 in range(2):
            pq, pqk = pq_r.next()
            for oc in range(2):
                P.i("pe", "matmul", reads=[gTk, pf + "gw"], writes=[pqk], out=pq[:, 0:N], lhsT=gw[:, oc, oc2 * 128:(oc2 + 1) * 128], rhs=gT[:, oc, 0:N], start=(oc == 0), stop=(oc == 1))
            a, ak = a_r.next()
            P.i("act", "activation", reads=[pqk, pf + "gb"], writes=[ak], out=a[:, 0:N], in_=pq[:, 0:N], func=AF.Sigmoid, bias=gb[:, oc2:oc2 + 1])
            P.i("dve", "tensor_tensor", reads=[ak, gTk], writes=[ak], out=a[:, 0:N], in0=a[:, 0:N], in1=gT[:, oc2, 0:N].bitcast(F32), op=ALU.mult)
            P.d(CAT[oc2 * 128:(oc2 + 1) * 128, c0:c0 + N], a[:, 0:N], reads=[ak], writes=[P.u("CAT")])


def phase_d1(P, Dm, l):
    pf = "d%d_" % l
    FMT = Dm.get("FMT", [1024, T])
    XBCT = Dm.get("XBCT", [768, T])
    XBtok = Dm.get("XBtok", [T, 512])
    cw_d = Dm.get("convw%d" % l, [128, 6, 4], kind="ExternalInput")
    ident_d = Dm.get("ident", [128, 128], kind="ExternalInput")
    ident = P.sb(pf + "ident", [128, 128]); P.d(ident[:], ident_d[:, :], writes=[pf + "ident"])
    cw = P.sb(pf + "cw", [128, 6, 4]); P.d(cw[:], cw_d[:, :, :], writes=[pf + "cw"])
    xi_r = Rot(P, pf + "xi", [128, 6, 514], n=2)
    ac_r = Rot(P, pf + "ac", [128, 512], n=2)
    co_r = Rot(P, pf + "co", [128, 6, 512], n=2)
    tp_r = Rot(P, pf + "tp", [128, 512], n=2, psum=True)
    tk_r = Rot(P, pf + "tk", [128, 512], n=2)
    for (c0, N) in CHUNKS:
        xi, xik = xi_r.next()
        lz = c0 in (0, LC)
        rz = (c0 + N) in (LC, T)
        lo = c0 - (0 if lz else 1)
        hi = c0 + N + (0 if rz else 1)
        if lz:
            P.i("dve", "memset", writes=[xik + "/l"], ap=xi[:, :, 0:1], constant=0.0)
        if rz:
            P.i("dve", "memset", writes=[xik + "/r"], ap=xi[:, :, N + 1:N + 2], constant=0.0)
        P.d(xi[:, :, (1 if lz else 0):(1 if lz else 0) + hi - lo], FMT[256:1024, lo:hi].rearrange("(r p) t -> p r t", p=128), reads=["FMT"], writes=[xik + "/m"])
        co, cok = co_r.next()
        for rc in range(6):
            ac, ack = ac_r.next()
            P.i("dve", "tensor_scalar", reads=[xik, pf + "cw"], writes=[ack], out=ac[:, 0:N], in0=xi[:, rc, 1:N + 1], scalar1=cw[:, rc, 1:2], scalar2=None, op0=ALU.mult)
            P.i("dve", "scalar_tensor_tensor", reads=[xik, pf + "cw", ack], writes=[ack], out=ac[:, 0:N], in0=xi[:, rc, 0:N], scalar=cw[:, rc, 0:1], in1=ac[:, 0:N], op0=ALU.mult, op1=ALU.add)
            P.i("dve", "scalar_tensor_tensor", reads=[xik, pf + "cw", ack], writes=[ack], out=ac[:, 0:N], in0=xi[:, rc, 2:N + 2], scalar=cw[:, rc, 2:3], in1=ac[:, 0:N], op0=ALU.mult, op1=ALU.add)
            P.i("act", "activation", reads=[ack, pf + "cw"], writes=[cok + "/%d" % rc], out=co[:, rc, 0:N], in_=ac[:, 0:N], func=AF.Silu, bias=cw[:, rc, 3:4])
        P.d(XBCT[:, c0:c0 + N].rearrange("(r p) t -> p r t", p=128), co[:, :, 0:N], reads=[cok], writes=[P.u("XBCT")])
        for i in range(N // 128):
            tp, tpk = tp_r.next()
            for rc in range(4):
                P.i("pe", "transpose", reads=[cok + "/%d" % rc, pf + "ident"], writes=[tpk], out=tp[:, rc * 128:(rc + 1) * 128], in_=co[:, rc, i * 128:(i + 1) * 128], identity=ident[:])
            tk, tkk = tk_r.next()
            P.i("act", "activation", reads=[tpk], writes=[tkk], out=tk[:], in_=tp[:], func=AF.Copy)
            P.d(XBtok[c0 + i * 128:c0 + (i + 1) * 128, :], tk[:], reads=[tkk], writes=[P.u("XBtok")])


def phase_d2(P, Dm, l):
    pf = "D%d_" % l
    XBCT = Dm.get("XBCT", [768, T])
    XBtok = Dm.get("XBtok", [T, 512])
    TMS = Dm.get("TMS", [T, 520])
    CAT = Dm.get("CAT", [1024, T])
    ident_d = Dm.get("ident", [128, 128], kind="ExternalInput")
    tri_d = Dm.get("tri", [128, 2, 128], kind="ExternalInput")
    alog_d = Dm.get("alog%d" % l, [1, 8], kind="ExternalInput")
    dsk_d = Dm.get("ssdd%d" % l, [1, 4], kind="ExternalInput")
    ng_d = Dm.get("ssdng%d" % l, [1, 256], kind="ExternalInput")
    ident = P.sb(pf + "ident", [128, 128]); P.d(ident[:], ident_d[:, :], writes=[pf + "ident"])
    tri = P.sb(pf + "tri", [128, 2, 128]); P.d(tri[:], tri_d[:, :, :], writes=[pf + "tri"])
    A = P.sb(pf + "A", [128, 8]); load_bcast(P, A[:], pf + "A", alog_d[0:1, :])
    P.i("act", "activation", reads=[pf + "A"], writes=[pf + "A"], out=A[:], in_=A[:], func=AF.Exp)
    P.i("dve", "tensor_scalar", reads=[pf + "A"], writes=[pf + "A"], out=A[:], in0=A[:], scalar1=-1.0, scalar2=None, op0=ALU.mult)
    dsk = P.sb(pf + "dsk", [128, 4]); load_bcast(P, dsk[:], pf + "dsk", dsk_d[0:1, :])
    ng = P.sb(pf + "ng", [128, 256]); load_bcast(P, ng[:], pf + "ng", ng_d[0:1, :])
    epsc = P.sb(pf + "eps", [128, 1]); P.i("dve", "memset", writes=[pf + "eps"], ap=epsc[:], constant=EPS)
    Yacc = P.sb(pf + "Yacc", [128, NT, 256])
    Sst = P.sb(pf + "S", [128, 4, 64], F32R)
    bc_r = Rot(P, pf + "bc", [128, 4, 128], F32R, n=2)
    xb_r = Rot(P, pf + "xb", [128, 512], n=2)
    xbr_r = Rot(P, pf + "xbr", [128, 256], F32R, n=2)
    dt_r = Rot(P, pf + "dt", [128, 8], n=2)
    sm_r = Rot(P, pf + "sm", [128, 64], n=2)
    abc_r = Rot(P, pf + "abc", [128, 4, 128], n=2)
    X_r = Rot(P, pf + "X", [128, 4, 64], F32R, n=2)
    Xd_r = Rot(P, pf + "Xd", [128, 4, 64], F32R, n=2)
    pG_r = Rot(P, pf + "pG", [128, 512], n=1, psum=True)
    pR_r = Rot(P, pf + "pR", [128, 512], n=1, psum=True)
    pC_r = Rot(P, pf + "pC", [128, 512], n=1, psum=True)
    pY_r = Rot(P, pf + "pY", [128, 512], n=2, psum=True)
    pS_r = Rot(P, pf + "pS", [128, 512], n=1, psum=True)
    Gm_r = Rot(P, pf + "Gm", [128, 2, 128], n=2)
    df_r = Rot(P, pf + "df", [128, 128], n=2)
    sc_r = Rot(P, pf + "scT", [128, 128], F32R, n=2)
    yo_r = Rot(P, pf + "yo", [128, 64], n=2)
    for d in range(2):
        order = list(range(NT)) if d == 0 else [1, 0] + list(range(NT - 1, 1, -1))
        P.i("dve", "memset", writes=[pf + "S"], ap=Sst[:].bitcast(F32), constant=0.0)
        for ci, c in enumerate(order):
            t0 = c * 128
            bc, bck = bc_r.next()
            P.d(bc[:], XBCT[256:768, t0:t0 + 128].rearrange("(r p) t -> p r t", p=128), reads=["XBCT"], writes=[bck], q="pool")
            xb, xbk = xb_r.next()
            P.d(xb[:], XBtok[t0:t0 + 128, :], reads=["XBtok"], writes=[xbk])
            xbr, xbrk = xbr_r.next()
            P.i("act", "activation", reads=[xbk], writes=[xbrk], out=xbr[:], in_=xb[:, 256:512], func=AF.Copy)
            dt, dtk = dt_r.next()
            P.d(dt[:], TMS[t0:t0 + 128, 512:520], reads=["TMS"], writes=[dtk])
            sm, smk = sm_r.next()
            P.i("dve", "tensor_tensor", reads=[dtk, pf + "A"], writes=[smk], out=sm[:, 0:4], in0=dt[:, d * 4:d * 4 + 4], in1=A[:, d * 4:d * 4 + 4], op=ALU.mult)
            abc, abck = abc_r.next()
            P.i("dve", "tensor_copy", reads=[smk], writes=[abck], out=abc[:], in_=sm[:, 0:4].unsqueeze(2).to_broadcast([128, 4, 128]))
            X, Xk = X_r.next()
            P.i("pool", "tensor_tensor", reads=[xbk, dtk], writes=[Xk], out=X[:], in0=xb[:, 0:256].rearrange("p (h e) -> p h e", h=4),
                in1=dt[:, d * 4:d * 4 + 4].unsqueeze(2).to_broadcast([128, 4, 64]), op=ALU.mult)
            pG, pGk = pG_r.next()
            for g in range(2):
                P.i("pe", "matmul", reads=[bck], writes=[pGk], out=pG[:, g * 128:(g + 1) * 128], lhsT=bc[:, g, :], rhs=bc[:, 2 + g, :], start=True, stop=True)
            Gm, Gmk = Gm_r.next()
            P.i("dve", "tensor_tensor", reads=[pGk, pf + "tri"], writes=[Gmk], out=Gm[:], in0=pG[:, 0:256].rearrange("p (g l) -> p g l", g=2),
                in1=tri[:, d, :].unsqueeze(1).to_broadcast([128, 2, 128]), op=ALU.mult)
            pC, pCk = pC_r.next()
            P.i("pe", "matmul", reads=[smk, pf + "tri"], writes=[pCk], out=pC[:, 0:4], lhsT=tri[:, d, :], rhs=sm[:, 0:4], start=True, stop=True)
            pR, pRk = pR_r.next()
            for h in range(4):
                P.i("pe", "matmul", reads=[abck, pf + "tri"], writes=[pRk], out=pR[:, h * 128:(h + 1) * 128], lhsT=abc[:, h, :], rhs=tri[:, d, :], start=True, stop=True)
            P.i("dve", "tensor_copy", reads=[pCk], writes=[smk], out=sm[:, 4:8], in_=pC[:, 0:4])
            P.i("act", "activation", reads=[smk], writes=[smk], out=sm[:, 8:12], in_=sm[:, 4:8], func=AF.Exp)
            last = 127 if d == 0 else 0
            P.i("dve", "tensor_copy", reads=[pRk], writes=[smk], out=sm[:, 20:24], in_=pR[:].rearrange("p (h l) -> p h l", h=4)[:, :, last])
            P.i("dve", "tensor_tensor", reads=[smk], writes=[smk], out=sm[:, 12:16], in0=sm[:, 20:24], in1=sm[:, 4:8], op=ALU.subtract)
            P.i("act", "activation", reads=[smk], writes=[smk], out=sm[:, 12:16], in_=sm[:, 12:16], func=AF.Exp)
            P.i("act", "activation", reads=[smk], writes=[smk], out=sm[:, 16:20], in_=sm[:, 20:24], func=AF.Exp)
            Xd, Xdk = Xd_r.next()
            P.i("pool", "tensor_tensor", reads=[Xk, smk], writes=[Xdk], out=Xd[:], in0=X[:].bitcast(F32), in1=sm[:, 12:16].unsqueeze(2).to_broadcast([128, 4, 64]), op=ALU.mult)
            pY, pYk = pY_r.next()
            pS, pSk = pS_r.next()
            for h in range(4):
                g = h // 2
                df, dfk = df_r.next()
                P.i("dve", "tensor_scalar", reads=[pRk, smk], writes=[dfk], out=df[:], in0=pR[:, h * 128:(h + 1) * 128], scalar1=sm[:, 4 + h:5 + h], scalar2=0.0, op0=ALU.subtract, op1=ALU.min)
                P.i("act", "activation", reads=[dfk], writes=[dfk], out=df[:], in_=df[:], func=AF.Exp)
                scT, scTk = sc_r.next()
                P.i("pool", "tensor_tensor", reads=[dfk, Gmk], writes=[scTk], out=scT[:], in0=df[:], in1=Gm[:, g, :], op=ALU.mult)
                P.i("pe", "matmul", reads=[scTk, Xk], writes=[pYk], out=pY[:, h * 128:h * 128 + 64], lhsT=scT[:], rhs=X[:, h, :], start=True, stop=True)
                P.i("pe", "matmul", reads=[bck, pf + "S"], writes=[pYk], out=pY[:, h * 128 + 64:h * 128 + 128], lhsT=bc[:, 2 + g, :], rhs=Sst[:, h, :], start=True, stop=True)
                P.i("pe", "matmul", reads=[xbrk, Xdk], writes=[pSk], out=pS[:, h * 64:(h + 1) * 64], lhsT=xbr[:, g * 128:(g + 1) * 128], rhs=Xd[:, h, :], start=True, stop=True)
            for h in range(4):
                yo, yok = yo_r.next()
                P.i("act", "activation", reads=[pYk, smk], writes=[yok], out=yo[:], in_=pY[:, h * 128 + 64:h * 128 + 128], func=AF.Copy, scale=sm[:, 8 + h:9 + h])
                ya = Yacc[:, c, h * 64:(h + 1) * 64]
                yk = pf + "Yacc/%d_%d" % (c, h)
                if d == 0:
                    P.i("dve", "tensor_tensor", reads=[pYk, yok], writes=[yk], out=ya, in0=pY[:, h * 128:h * 128 + 64], in1=yo[:], op=ALU.add)
                else:
                    P.i("dve", "tensor_tensor", reads=[pYk, yok], writes=[yok], out=yo[:], in0=pY[:, h * 128:h * 128 + 64], in1=yo[:], op=ALU.add)
                    P.i("pool", "tensor_tensor", reads=[yk, yok], writes=[yk], out=ya, in0=ya, in1=yo[:], op=ALU.add)
                P.i("dve", "scalar_tensor_tensor", reads=[pf + "S", smk, pSk], writes=[pf + "S"], out=Sst[:, h, :], in0=Sst[:, h, :].bitcast(F32), scalar=sm[:, 16 + h:17 + h],
                    in1=pS[:, h * 64:(h + 1) * 64], op0=ALU.mult, op1=ALU.add)
    z_r = Rot(P, pf + "z", [128, 256], n=2)
    yt_r = Rot(P, pf + "yt", [128, 256], n=2)
    jk = P.sb(pf + "jk", [128, 256])
    pT_r = Rot(P, pf + "pT", [128, 512], n=2, psum=True)
    oT_r = Rot(P, pf + "oT", [128, 2, 128], n=2)
    for c in range(NT):
        t0 = c * 128
        xb, xbk = xb_r.next()
        P.d(xb[:], XBtok[t0:t0 + 128, :], reads=["XBtok"], writes=[xbk])
        z, zk = z_r.next()
        P.d(z[:], TMS[t0:t0 + 128, 256:512], reads=["TMS"], writes=[zk])
        yt, ytk = yt_r.next()
        P.i("pool", "tensor_tensor", reads=[xbk, pf + "dsk"], writes=[ytk], out=yt[:].rearrange("p (h e) -> p h e", h=4), in0=xb[:, 0:256].rearrange("p (h e) -> p h e", h=4),
            in1=dsk[:].unsqueeze(2).to_broadcast([128, 4, 64]), op=ALU.mult)
        P.i("dve", "tensor_tensor", reads=[ytk, pf + "Yacc/%d" % c], writes=[ytk], out=yt[:], in0=yt[:], in1=Yacc[:, c, :], op=ALU.add)
        P.i("act", "activation", reads=[zk], writes=[zk], out=z[:], in_=z[:], func=AF.Silu)
        P.i("dve", "tensor_tensor", reads=[ytk, zk], writes=[ytk], out=yt[:], in0=yt[:], in1=z[:], op=ALU.mult)
        sm, smk = sm_r.next()
        P.i("act", "activation", reads=[ytk], writes=[pf + "jk", smk], out=jk[:], in_=yt[:], func=AF.Square, accum_out=sm[:, 0:1])
        P.i("act", "activation", reads=[smk, pf + "eps"], writes=[smk], out=sm[:, 1:2], in_=sm[:, 0:1], func=AF.Sqrt, bias=epsc[:], scale=1.0 / 256)
        P.i("dve", "reciprocal", reads=[smk], writes=[smk], out=sm[:, 2:3], in_=sm[:, 1:2])
        P.i("dve", "scalar_tensor_tensor", reads=[ytk, smk, pf + "ng"], writes=[ytk], out=yt[:], in0=yt[:], scalar=sm[:, 2:3], in1=ng[:], op0=ALU.mult, op1=ALU.mult)
        pT, pTk = pT_r.next()
        for q in range(2):
            P.i("pe", "transpose", reads=[ytk, pf + "ident"], writes=[pTk], out=pT[:, q * 128:(q + 1) * 128], in_=yt[:, q * 128:(q + 1) * 128], identity=ident[:])
        oT, oTk = oT_r.next()
        P.i("act", "activation", reads=[pTk], writes=[oTk], out=oT[:], in_=pT[:, 0:256].rearrange("p (q t) -> p q t", q=2), func=AF.Copy)
        P.d(CAT[512:768, t0:t0 + 128].rearrange("(q p) t -> p q t", p=128), oT[:], reads=[oTk], writes=[P.u("CAT")])


LAT_CHUNKS = [(256 + 512 * i, 512) for i in range(8)]
ALL_CHUNKS512 = [(512 * i, 512) for i in range(8)] + [(4096, 256)]


def build_program():
    nc = bass.Bass("TRN2", target_bir_lowering=False)
    P = Prog(nc)
    Dm = Dram(nc, ext_in=["xin"])
    src = "xin"
    for l in range(2):
        last = (l == 1)
        ctx_out = not last
        for fn in (lambda: phase_mod(P, Dm, l),
                   lambda: phase_a(P, Dm, l, src),
                   lambda: phase_b(P, Dm, l),
                   lambda: phase_c(P, Dm, l, ctx_out),
                   lambda: phase_d1(P, Dm, l),
                   lambda: phase_d2(P, Dm, l),
                   lambda: phase_e(P, Dm, l, ctx_out),
                   lambda: phase_f2(P, Dm, l, src, tok_chunks=(LAT_CHUNKS if last else None)),
                   lambda: phase_g2(P, Dm, l, (LAT_CHUNKS if last else CHUNKS), last)):
            P.begin_phase()
            fn()
            P.end_phase()
        src = "XN%d" % l
    P.emit()
    return nc, P, Dm


_CACHE = {}


def kernel(**inputs):
    inp = {k: np.asarray(v) for k, v in inputs.items()}
    if "nc" not in _CACHE:
        _CACHE["nc"] = build_program()
    nc, P, Dm = _CACHE["nc"]
    n_cores = 8
    maps = []
    per_b = {}
    for cidx in range(n_cores):
        b = cidx % 4
        if b not in per_b:
            m = core_inputs(inp, b)
            per_b[b] = {k: v for k, v in m.items() if k in Dm.t}
        maps.append(per_b[b])
    res = run_bass_kernel_spmd(nc, maps, core_ids=list(range(n_cores)))
    out = np.stack([np.asarray(res.results[b]["out"]) for b in range(4)], 0)
    return out.astype(np.float32)
```

```python
import numpy as np
from contextlib import ExitStack
import concourse.bass as bass
import concourse.mybir as mybir
from concourse.bass_utils import run_bass_kernel_spmd

F32 = mybir.dt.float32
F32R = mybir.dt.float32r
I32 = mybir.dt.int32
AF = mybir.ActivationFunctionType
ALU = mybir.AluOpType
AX = mybir.AxisListType

LC, L, T, D = 256, 4096, 4352, 1024
NT = T // 128
EPS = 1e-6
CHUNKS = [(0, 256)] + [(256 + 512 * i, 512) for i in range(8)]
PERM = np.concatenate([np.arange(256, 768), np.arange(1800, 2312), np.arange(768, 1024), np.arange(1792, 1800),
                       np.arange(0, 256), np.arange(1024, 1792)])
TM_COLS = 1288
FM_OFF = 1288

ENGS = ["pe", "act", "dve", "pool", "sp"]
N_DMA_SEMS = 8


class Prog:
    def __init__(self, nc, same_engine_sync=True):
        self.nc = nc
        self.st = ExitStack()
        self.same = same_engine_sync
        self.ops = {e: [] for e in ENGS}
        self.sems = {}
        self.cnt = {}
        for e in ["pe", "act", "dve", "pool"]:
            self.sems[e] = self.st.enter_context(nc.semaphore("s_" + e))
            self.cnt[e] = 0
        for q in ["sp", "pool"]:
            for i in range(N_DMA_SEMS):
                nm = "d_%s%d" % (q, i)
                self.sems[nm] = self.st.enter_context(nc.semaphore(nm))
                self.cnt[nm] = 0
        self.drr = {"sp": 0, "pool": 0}
        self.waited = {e: {} for e in ENGS}
        self.last_w = {}
        self.readers = {}
        self.n_ops = 0
        self.final = []
        self.uid = 0
        self.pst = None
        self.barrier = {e: {} for e in ENGS}
        self.children = {}
        self.known = set()
        self.scopes = False
        self.bound_reg = None
        self.psum_keys = set()

    def begin_phase(self, name=None):
        self.pst = ExitStack()
        self.phase_name = name

    def end_phase(self):
        self.pst.close()
        self.pst = None
        snap = {s: v for s, v in self.cnt.items() if v > 0}
        for e in ENGS:
            self.barrier[e] = dict(snap)

    def bound(self):
        if self.bound_reg is None:
            self.bound_reg = self.nc.gpsimd.alloc_register("bc4095")
        return self.bound_reg

    def sb(self, name, shape, dtype=F32):
        return (self.pst or self.st).enter_context(self.nc.sbuf_tensor(name, list(shape), dtype))

    def ps(self, name, shape, dtype=F32):
        return (self.pst or self.st).enter_context(self.nc.psum_tensor(name, list(shape), dtype))

    def _related(self, k):
        rel = [k]
        parts = k.split("/")
        for n in range(1, len(parts)):
            rel.append("/".join(parts[:n]))
        rel.extend(self.children.get(k, ()))
        return rel

    def _register(self, k):
        if k in self.known:
            return
        self.known.add(k)
        parts = k.split("/")
        for n in range(1, len(parts)):
            self.children.setdefault("/".join(parts[:n]), set()).add(k)

    def _deps(self, eng, reads, writes):
        deps = {}

        def add(s, v):
            if v > deps.get(s, 0):
                deps[s] = v

        for k in list(reads) + list(writes):
            self._register(k)
        for k in reads:
            for kk in self._related(k):
                t = self.last_w.get(kk)
                if t is not None:
                    add(*t)
        for k in writes:
            for kk in self._related(k):
                t = self.last_w.get(kk)
                if t is not None:
                    add(*t)
                for s_, v_ in self.readers.get(kk, {}).items():
                    add(s_, v_)
        if self.barrier[eng]:
            for s_, v_ in self.barrier[eng].items():
                add(s_, v_)
            self.barrier[eng] = {}
        out = []
        for s, v in deps.items():
            if s == eng and (eng == "pe" or not self.same):
                continue
            if v > self.waited[eng].get(s, 0):
                self.waited[eng][s] = v
                out.append((s, v))
        return out

    def _commit(self, tok, reads, writes):
        for k in writes:
            self.last_w[k] = tok
            self.readers[k] = {}
        for k in reads:
            if k in writes:
                continue
            rd = self.readers.setdefault(k, {})
            if tok[1] > rd.get(tok[0], 0):
                rd[tok[0]] = tok[1]

    def op(self, eng, fn, reads=(), writes=()):
        pr = [k for k in reads if k in self.psum_keys]
        if pr:
            reads = [k for k in reads if k not in self.psum_keys]
            writes = list(writes) + pr
        waits = self._deps(eng, reads, writes)
        self.cnt[eng] += 1
        tok = (eng, self.cnt[eng])
        self.ops[eng].append((waits, fn, tok, 1, getattr(self, "phase_name", None)))
        self._commit(tok, reads, writes)
        self.n_ops += 1
        return tok

    def u(self, base):
        self.uid += 1
        return "%s/%d" % (base, self.uid)

    def i(self, eng, name, reads=(), writes=(), **kw):
        return self.op(eng, lambda e: getattr(e, name)(**kw), reads, writes)

    def d(self, out, in_, reads=(), writes=(), q="sp", final=False):
        return self.dma(lambda e: e.dma_start(out=out, in_=in_), reads, writes, q=q, final=final)

    def dma(self, fn, reads=(), writes=(), q="sp", final=False):
        waits = self._deps(q, reads, writes)
        i = self.drr[q]
        self.drr[q] = (i + 1) % N_DMA_SEMS
        nm = "d_%s%d" % (q, i)
        prev = self.cnt[nm]
        if prev > self.waited[q].get(nm, 0):
            self.waited[q][nm] = prev
            waits.append((nm, prev))
        self.cnt[nm] += 16
        tok = (nm, self.cnt[nm])
        self.ops[q].append((waits, fn, tok, 16, getattr(self, "phase_name", None)))
        self._commit(tok, reads, writes)
        self.n_ops += 1
        if final:
            self.final.append(tok)
        return tok

    def emit(self):
        nc = self.nc
        fin = list(self.final)
        engmap = {"pe": "tensor", "act": "scalar", "dve": "vector", "pool": "gpsimd", "sp": "sync"}
        with nc.Block() as block:
            for e in ENGS:
                lst = self.ops[e]
                extra = fin if e == "sp" else []
                if not lst and not extra:
                    continue

                def body(engine, lst=lst, extra=extra, e=e):
                    cur = None
                    scope = None
                    if e == "pool" and self.bound_reg is not None:
                        engine.reg_mov(self.bound_reg, 4095)
                    for (waits, fn, tok, amt, ph) in lst:
                        if self.scopes and ph != cur:
                            if scope is not None:
                                scope.__exit__(None, None, None)
                            scope = nc.named_scope(ph or "none")
                            scope.__enter__()
                            cur = ph
                        for (s, v) in waits:
                            engine.wait_ge(self.sems[s], v)
                        fn(engine).then_inc(self.sems[tok[0]], amt)
                    if scope is not None:
                        scope.__exit__(None, None, None)
                    for (s, v) in extra:
                        engine.wait_ge(self.sems[s], v)

                getattr(block, engmap[e])(body)
        self.st.close()


class Rot:
    def __init__(self, P, name, shape, dtype=F32, n=2, psum=False):
        self.bufs = [(P.ps if psum else P.sb)("%s%d" % (name, i), shape, dtype) for i in range(n)]
        self.keys = ["%s%d" % (name, i) for i in range(n)]
        self.i = 0
        if psum:
            P.psum_keys.update(self.keys)

    def next(self):
        j = self.i % len(self.bufs)
        self.i += 1
        return self.bufs[j], self.keys[j]


class Dram:
    def __init__(self, nc, ext_in=(), ext_out=()):
        self.nc, self.t = nc, {}
        self.ext_in, self.ext_out = set(ext_in), set(ext_out)

    def get(self, name, shape=None, dtype=F32, kind=None):
        if name not in self.t:
            if kind is None:
                kind = "ExternalInput" if name in self.ext_in else ("ExternalOutput" if name in self.ext_out else "Internal")
            self.t[name] = self.nc.dram_tensor(name, list(shape), dtype, kind=kind).ap()
        return self.t[name]


def phase_mod(P, Dm, l):
    nc = P.nc
    cvec = Dm.get("cvec", [128, 8, 2], kind="ExternalInput")
    ada_w = Dm.get("ada_w%d" % l, [128, 8, 6144], kind="ExternalInput")
    ada_b = Dm.get("ada_b%d" % l, [1, 6144], kind="ExternalInput")
    modrow = Dm.get("modrow%d" % l, [2, 6144])
    pf = "m%d_" % l
    cv = P.sb(pf + "cv", [128, 8, 2])
    sg = P.sb(pf + "sg", [128, 8, 2])
    sc = P.sb(pf + "sc", [128, 8, 128], F32R)
    ab = P.sb(pf + "ab", [2, 6144])
    mr = P.sb(pf + "mr", [2, 6144])
    wch = Rot(P, pf + "w", [128, 8, 512], F32R, n=2)
    pm = Rot(P, pf + "pm", [128, 512], F32, n=2, psum=True)
    P.dma(lambda e: e.dma_start(out=cv[:], in_=cvec[:, :, :]), writes=[pf + "cv"])
    P.dma(lambda e: e.dma_start(out=ab[:], in_=ada_b[0:1, :].to_broadcast([2, 6144])), writes=[pf + "ab"])
    P.op("act", lambda e: e.activation(out=sg[:], in_=cv[:], func=AF.Sigmoid), reads=[pf + "cv"], writes=[pf + "sg"])
    P.op("dve", lambda e: e.memset(sc[:].bitcast(F32), 0.0), writes=[pf + "sc"])
    P.op("dve", lambda e: e.tensor_tensor(out=sc[:, :, 0:2], in0=cv[:], in1=sg[:], op=ALU.mult), reads=[pf + "cv", pf + "sg"], writes=[pf + "sc"])
    for j in range(12):
        w, wk = wch.next()
        P.dma(lambda e, w=w, j=j: e.dma_start(out=w[:], in_=ada_w[:, :, j * 512:(j + 1) * 512]), writes=[wk], q="pool")
        pt, pk = pm.next()
        for k in range(8):
            P.op("pe", lambda e, w=w, pt=pt, k=k: e.matmul(pt[:], sc[:, k, :], w[:, k, :], start=(k == 0), stop=(k == 7)),
                 reads=[wk, pf + "sc"], writes=[pk])
        P.op("dve", lambda e, pt=pt, j=j: e.tensor_tensor(out=mr[:, j * 512:(j + 1) * 512], in0=pt[0:2, :], in1=ab[:, j * 512:(j + 1) * 512], op=ALU.add),
             reads=[pk, pf + "ab"], writes=[pf + "mr"])
    P.dma(lambda e: e.dma_start(out=modrow[:, :], in_=mr[:]), reads=[pf + "mr"], writes=["modrow%d" % l])


def load_bcast(P, dst, dkey, src_row, reads=()):
    n = src_row.shape[-1]
    P.dma(lambda e: e.dma_start(out=dst, in_=src_row.to_broadcast([128, n])), reads=list(reads), writes=[dkey])


def phase_a(P, Dm, l, src_name, chunks=None, stop=99):
    nc = P.nc
    pf = "a%d_" % l
    xsrc = Dm.get(src_name, [T, D])
    w_in = Dm.get("w_in%d" % l, [128, 8, 2312], kind="ExternalInput")
    modrow = Dm.get("modrow%d" % l, [2, 6144])
    n1g = Dm.get("norm1_g%d" % l, [1, D], kind="ExternalInput")
    qkg = Dm.get("qkg%d" % l, [1, 384], kind="ExternalInput")
    dtb = Dm.get("dtb%d" % l, [1, 8], kind="ExternalInput")
    ropec = Dm.get("rope_cos", [L, 384], kind="ExternalInput")
    ropes = Dm.get("rope_sin", [L, 384], kind="ExternalInput")
    ident_d = Dm.get("ident", [128, 128], kind="ExternalInput")
    FMT = Dm.get("FMT", [1024, T])
    QKT = Dm.get("QKT", [768, T])
    TMS = Dm.get("TMS", [T, 520])
    mk = "modrow%d" % l

    ident = P.sb(pf + "ident", [128, 128])
    P.dma(lambda e: e.dma_start(out=ident[:], in_=ident_d[:, :]), writes=[pf + "ident"])
    win = P.sb(pf + "win", [128, 8, 2312], F32R)
    for k in range(8):
        P.dma(lambda e, k=k: e.dma_start(out=win[:, k, :], in_=w_in[:, k, :]), writes=[pf + "win/%d" % k], q="pool")
    wkeys = [pf + "win/%d" % k for k in range(8)]
    G = [P.sb(pf + "G%d" % r, [128, D]) for r in range(2)]
    SH = [P.sb(pf + "SH%d" % r, [128, D]) for r in range(2)]
    gn = P.sb(pf + "gn", [128, D])
    load_bcast(P, gn[:], pf + "gn", n1g[0:1, :])
    for r in range(2):
        load_bcast(P, SH[r][:], pf + "SH%d" % r, modrow[r:r + 1, 0:1024], reads=[mk])
        load_bcast(P, G[r][:], pf + "G%d" % r, modrow[r:r + 1, 1024:2048], reads=[mk])
        P.op("dve", lambda e, r=r: e.scalar_tensor_tensor(out=G[r][:], in0=G[r][:], scalar=1.0, in1=gn[:], op0=ALU.add, op1=ALU.mult),
             reads=[pf + "G%d" % r, pf + "gn"], writes=[pf + "G%d" % r])
    qkgb = P.sb(pf + "qkgb", [128, 384])
    load_bcast(P, qkgb[:], pf + "qkgb", qkg[0:1, :])
    dtbb = P.sb(pf + "dtbb", [128, 8])
    load_bcast(P, dtbb[:], pf + "dtbb", dtb[0:1, :])
    epsc = P.sb(pf + "eps", [128, 1])
    P.op("dve", lambda e: e.memset(epsc[:], EPS), writes=[pf + "eps"])
    onec = P.sb(pf + "one", [128, 1])
    P.op("dve", lambda e: e.memset(onec[:], 1.0), writes=[pf + "one"])

    xt_r = Rot(P, pf + "xt", [128, D], n=3)
    junk = P.sb(pf + "junk", [128, D])
    st_r = Rot(P, pf + "st", [128, 32], n=3)
    h_r = Rot(P, pf + "h", [128, D], n=2)
    tp_r = Rot(P, pf + "tp", [128, 1024], n=1, psum=True)
    hT_r = Rot(P, pf + "hT", [128, 8, 512], F32R, n=2)
    pj_r = Rot(P, pf + "pj", [128, 512], n=3, psum=True)
    qk_r = Rot(P, pf + "qk", [128, 12, 64], n=2)
    sq_r = Rot(P, pf + "sq", [128, 6, 64], n=2)
    rp_r = Rot(P, pf + "rp", [128, 24, 2, 16], n=2)
    tmp_r = Rot(P, pf + "tmp", [128, 24, 16], n=2)
    cs_r = Rot(P, pf + "cs", [128, 2, 384], n=2)
    tq_r = Rot(P, pf + "tq", [128, 1024], n=1, psum=True)
    qT_r = Rot(P, pf + "qT", [128, 6, 128], n=2)
    tm_r = Rot(P, pf + "tm", [128, 520], n=2)
    fm_r = Rot(P, pf + "fm", [128, 512], n=1, psum=True)
    fo_r = Rot(P, pf + "fo", [128, 512], n=3)

    for (c0, cn) in (chunks or CHUNKS):
        hT, hTk = hT_r.next()
        ntile = cn // 128
        for i in range(ntile):
            t0 = c0 + i * 128
            is_ctx = t0 < LC
            r = 1 if is_ctx else 0
            xt, xk = xt_r.next()
            P.dma(lambda e, xt=xt, t0=t0: e.dma_start(out=xt[:], in_=xsrc[t0:t0 + 128, :]), reads=[src_name], writes=[xk])
            st, sk = st_r.next()
            P.op("act", lambda e, xt=xt, st=st: e.activation(out=junk[:], in_=xt[:], func=AF.Square, accum_out=st[:, 0:1]),
                 reads=[xk], writes=[pf + "junk", sk])
            P.op("act", lambda e, st=st: e.activation(out=st[:, 1:2], in_=st[:, 0:1], func=AF.Sqrt, bias=epsc[:], scale=1.0 / D),
                 reads=[sk, pf + "eps"], writes=[sk])
            P.op("dve", lambda e, st=st: e.reciprocal(out=st[:, 2:3], in_=st[:, 1:2]), reads=[sk], writes=[sk])
            h, hk = h_r.next()
            P.op("dve", lambda e, xt=xt, st=st, h=h, r=r: e.scalar_tensor_tensor(out=h[:], in0=xt[:], scalar=st[:, 2:3], in1=G[r][:], op0=ALU.mult, op1=ALU.mult),
                 reads=[xk, sk, pf + "G%d" % r], writes=[hk])
            P.op("pool", lambda e, h=h, r=r: e.tensor_tensor(out=h[:], in0=h[:], in1=SH[r][:], op=ALU.add),
                 reads=[hk, pf + "SH%d" % r], writes=[hk])
            if stop == 1:
                continue
            tp, tpk = tp_r.next()
            for k in range(8):
                P.op("pe", lambda e, h=h, tp=tp, k=k: e.transpose(tp[:, k * 128:(k + 1) * 128], h[:, k * 128:(k + 1) * 128], ident[:]),
                     reads=[hk, pf + "ident"], writes=[tpk])
            P.op("act", lambda e, hT=hT, tp=tp, i=i: e.activation(out=hT[:, :, i * 128:(i + 1) * 128], in_=tp[:].rearrange("p (k t) -> p k t", k=8), func=AF.Copy),
                 reads=[tpk], writes=[hTk + "/%d" % i])
            if stop == 2:
                continue
            pjs = []
            for (o, n) in [(0, 512), (512, 512), (1024, 264)]:
                pj, pjk = pj_r.next()
                for k in range(8):
                    P.op("pe", lambda e, pj=pj, hT=hT, k=k, o=o, n=n, i=i: e.matmul(pj[:, 0:n], hT[:, k, i * 128:(i + 1) * 128], win[:, k, o:o + n], start=(k == 0), stop=(k == 7)),
                         reads=[hTk + "/%d" % i, wkeys[k]], writes=[pjk])
                pjs.append((pj, pjk))
            (pA, pAk), (pB, pBk), (pC, pCk) = pjs
            if stop == 3:
                continue
            qk, qkk = qk_r.next()
            sq, sqk = sq_r.next()
            pA6 = pA[:, 0:384].rearrange("p (h d) -> p h d", h=6)
            P.op("act", lambda e, sq=sq, pA6=pA6: e.activation(out=sq[:], in_=pA6, func=AF.Square), reads=[pAk], writes=[sqk])
            P.op("dve", lambda e, sq=sq, st=st: e.tensor_reduce(out=st[:, 4:10], in_=sq[:], axis=AX.X, op=ALU.add), reads=[sqk], writes=[sk])
            P.op("act", lambda e, st=st: e.activation(out=st[:, 4:10], in_=st[:, 4:10], func=AF.Sqrt, bias=epsc[:], scale=1.0 / 64), reads=[sk, pf + "eps"], writes=[sk])
            P.op("dve", lambda e, st=st: e.reciprocal(out=st[:, 10:16], in_=st[:, 4:10]), reads=[sk], writes=[sk])
            P.op("dve", lambda e, sq=sq, pA6=pA6, st=st: e.tensor_tensor(out=sq[:], in0=pA6, in1=st[:, 10:16].unsqueeze(2).to_broadcast([128, 6, 64]), op=ALU.mult),
                 reads=[pAk, sk], writes=[sqk])
            P.op("pool", lambda e, sq=sq, qk=qk: e.tensor_tensor(out=qk[:, 0:6, :], in0=sq[:], in1=qkgb[:].rearrange("p (h d) -> p h d", h=6), op=ALU.mult),
                 reads=[sqk, pf + "qkgb"], writes=[qkk + "/a"])
            P.op("act", lambda e, qk=qk, pB=pB: e.activation(out=qk[:, 6:12, :], in_=pB[:, 0:384].rearrange("p (h d) -> p h d", h=6), func=AF.Copy),
                 reads=[pBk], writes=[qkk + "/b"])
            if stop == 4:
                continue
            tm, tmk = tm_r.next()
            P.op("act", lambda e, tm=tm, pA=pA: e.activation(out=tm[:, 0:128], in_=pA[:, 384:512], func=AF.Copy), reads=[pAk], writes=[tmk + "/a"])
            P.op("act", lambda e, tm=tm, pB=pB: e.activation(out=tm[:, 128:256], in_=pB[:, 384:512], func=AF.Copy), reads=[pBk], writes=[tmk + "/b"])
            P.op("act", lambda e, tm=tm, pC=pC: e.activation(out=tm[:, 256:512], in_=pC[:, 0:256], func=AF.Copy), reads=[pCk], writes=[tmk + "/c"])
            P.op("dve", lambda e, tm=tm, pC=pC: e.tensor_tensor(out=tm[:, 512:520], in0=pC[:, 256:264], in1=dtbb[:], op=ALU.add), reads=[pCk, pf + "dtbb"], writes=[tmk + "/d"])
            P.op("act", lambda e, st=st, tm=tm: e.activation(out=st[:, 16:24], in_=tm[:, 512:520], func=AF.Abs), reads=[tmk + "/d", sk], writes=[sk])
            P.op("act", lambda e, st=st: e.activation(out=st[:, 16:24], in_=st[:, 16:24], func=AF.Exp, scale=-1.0), reads=[sk], writes=[sk])
            P.op("act", lambda e, st=st: e.activation(out=st[:, 16:24], in_=st[:, 16:24], func=AF.Ln, bias=onec[:]), reads=[sk], writes=[sk])
            P.op("dve", lambda e, st=st, tm=tm: e.scalar_tensor_tensor(out=tm[:, 512:520], in0=tm[:, 512:520], scalar=0.0, in1=st[:, 16:24], op0=ALU.max, op1=ALU.add),
                 reads=[tmk + "/d", sk], writes=[tmk + "/d"])
            P.dma(lambda e, tm=tm, t0=t0: e.dma_start(out=TMS[t0:t0 + 128, :], in_=tm[:]), reads=[tmk], writes=[P.u("TMS")])
            if stop == 5:
                continue
            rp, rpk = rp_r.next()
            if is_ctx:
                P.op("pool", lambda e, rp=rp, qk=qk: e.tensor_copy(out=rp[:].rearrange("p (h a) b f -> p h (a b f)", a=2), in_=qk[:]),
                     reads=[qkk + "/a", qkk + "/b"], writes=[rpk])
            else:
                cs, csk = cs_r.next()
                tl = t0 - LC
                P.dma(lambda e, cs=cs, tl=tl: e.dma_start(out=cs[:, 0, :], in_=ropec[tl:tl + 128, :]), writes=[csk + "/c"])
                P.dma(lambda e, cs=cs, tl=tl: e.dma_start(out=cs[:, 1, :], in_=ropes[tl:tl + 128, :]), writes=[csk + "/s"])
                qv = qk[:].rearrange("p h (a b f) -> p (h a) b f", a=2, b=2)
                x1, x2 = qv[:, :, 0, :], qv[:, :, 1, :]
                cb = cs[:, 0, :].rearrange("p (g f) -> p g f", f=16)
                sb_ = cs[:, 1, :].rearrange("p (g f) -> p g f", f=16)
                tmp, tmpk = tmp_r.next()
                qkr = [qkk + "/a", qkk + "/b"]
                P.op("dve", lambda e, rp=rp, x1=x1, cb=cb: e.tensor_tensor(out=rp[:, :, 0, :], in0=x1, in1=cb, op=ALU.mult), reads=qkr + [csk + "/c"], writes=[rpk + "/0"])
                P.op("pool", lambda e, tmp=tmp, x2=x2, sb_=sb_: e.tensor_tensor(out=tmp[:], in0=x2, in1=sb_, op=ALU.mult), reads=qkr + [csk + "/s"], writes=[tmpk])
                P.op("dve", lambda e, rp=rp, tmp=tmp: e.tensor_tensor(out=rp[:, :, 0, :], in0=rp[:, :, 0, :], in1=tmp[:], op=ALU.subtract), reads=[rpk + "/0", tmpk], writes=[rpk + "/0"])
                P.op("dve", lambda e, rp=rp, x2=x2, cb=cb: e.tensor_tensor(out=rp[:, :, 1, :], in0=x2, in1=cb, op=ALU.mult), reads=qkr + [csk + "/c"], writes=[rpk + "/1"])
                P.op("pool", lambda e, tmp=tmp, x1=x1, sb_=sb_: e.tensor_tensor(out=tmp[:], in0=x1, in1=sb_, op=ALU.mult), reads=qkr + [csk + "/s"], writes=[tmpk])
                P.op("dve", lambda e, rp=rp, tmp=tmp: e.tensor_tensor(out=rp[:, :, 1, :], in0=rp[:, :, 1, :], in1=tmp[:], op=ALU.add), reads=[rpk + "/1", tmpk], writes=[rpk + "/1"])
            rkeys = [rpk] if is_ctx else [rpk + "/0", rpk + "/1"]
            if stop == 6:
                continue
            rpf = rp[:].rearrange("p g b f -> p (g b f)")
            tq, tqk = tq_r.next()
            for j in range(6):
                P.op("pe", lambda e, tq=tq, rpf=rpf, j=j: e.transpose(tq[:, j * 128:(j + 1) * 128], rpf[:, j * 128:(j + 1) * 128], ident[:]),
                     reads=rkeys + [pf + "ident"], writes=[tqk])
            qT, qTk = qT_r.next()
            P.op("act", lambda e, qT=qT, tq=tq: e.activation(out=qT[:], in_=tq[:, 0:768].rearrange("p (j t) -> p j t", j=6), func=AF.Copy), reads=[tqk], writes=[qTk])
            P.dma(lambda e, qT=qT, t0=t0: e.dma_start(out=QKT[:, t0:t0 + 128].rearrange("(j p) t -> p j t", p=128), in_=qT[:]), reads=[qTk], writes=[P.u("QKT")])
        if stop <= 7:
            continue
        hkeys = [hTk + "/%d" % i for i in range(ntile)]
        for j in range(8):
            fm, fmk = fm_r.next()
            for k in range(8):
                P.op("pe", lambda e, fm=fm, hT=hT, k=k, j=j, cn=cn: e.matmul(fm[:, 0:cn], win[:, k, FM_OFF + j * 128:FM_OFF + (j + 1) * 128], hT[:, k, 0:cn], start=(k == 0), stop=(k == 7)),
                     reads=hkeys + [wkeys[k]], writes=[fmk])
            fo, fok = fo_r.next()
            eng = "act" if j % 2 == 0 else "dve"
            if eng == "act":
                P.op("act", lambda e, fo=fo, fm=fm, cn=cn: e.activation(out=fo[:, 0:cn], in_=fm[:, 0:cn], func=AF.Copy), reads=[fmk], writes=[fok])
            else:
                P.op("dve", lambda e, fo=fo, fm=fm, cn=cn: e.tensor_copy(out=fo[:, 0:cn], in_=fm[:, 0:cn]), reads=[fmk], writes=[fok])
            P.dma(lambda e, fo=fo, j=j, c0=c0, cn=cn: e.dma_start(out=FMT[j * 128:(j + 1) * 128, c0:c0 + cn], in_=fo[:, 0:cn]), reads=[fok], writes=[P.u("FMT")])


def phase_g2(P, Dm, l, tok_chunks, final):
    pf = "G%d_" % l
    ntf = sum(cn for _, cn in tok_chunks) // 128
    NB = 2 * ntf + 32
    X1 = Dm.get("X1", [T, D])
    BUF = Dm.get("BUF%d" % l, [NB * 128, D])
    OBUF = Dm.get("OBUF%d" % l, [NB * 128, D])
    IDXW = Dm.get("IDXW%d" % l, [128, NB], I32)
    DEST = Dm.get("DEST%d" % l, [128, ntf * 2], I32)
    WWd = Dm.get("WWd%d" % l, [128, ntf * 2])
    WGU = Dm.get("moe_gu%d" % l, [32, 128, 8, 1024], kind="ExternalInput")
    WD = Dm.get("moe_d%d" % l, [32, 128, 4, 1024], kind="ExternalInput")
    ident_d = Dm.get("ident", [128, 128], kind="ExternalInput")
    modrow = Dm.get("modrow%d" % l, [2, 6144])
    mk = "modrow%d" % l
    ident = P.sb(pf + "ident", [128, 128]); P.d(ident[:], ident_d[:, :], writes=[pf + "ident"])
    idxw = P.sb(pf + "idxw", [128, NB], I32); P.d(idxw[:], IDXW[:, :], reads=["IDXW%d" % l], writes=[pf + "idxw"])
    dest = P.sb(pf + "dest", [128, ntf * 2], I32); P.d(dest[:], DEST[:, :], reads=["DEST%d" % l], writes=[pf + "dest"])
    ww = P.sb(pf + "ww", [128, ntf * 2]); P.d(ww[:], WWd[:, :], reads=["WWd%d" % l], writes=[pf + "ww"])
    wgu_r = Rot(P, pf + "wgu", [128, 8, 1024], F32R, n=1)
    wd_r = Rot(P, pf + "wd", [128, 4, 1024], F32R, n=1)
    xs_r = Rot(P, pf + "xs", [128, D], n=3)
    tp_r = Rot(P, pf + "tp", [128, 1024], n=1, psum=True)
    xT_r = Rot(P, pf + "xT", [128, 8, 128], F32R, n=2)
    pg_r = Rot(P, pf + "pg", [128, 512], n=1, psum=True)
    pu_r = Rot(P, pf + "pu", [128, 512], n=1, psum=True)
    sg_r = Rot(P, pf + "sg", [128, 512], n=2)
    hu_r = Rot(P, pf + "hu", [128, 512], n=2)
    ph_r = Rot(P, pf + "ph", [128, 512], n=1, psum=True)
    hT_r = Rot(P, pf + "hT", [128, 4, 128], F32R, n=2)
    po_r = Rot(P, pf + "po", [128, 1024], n=1, psum=True)
    ob_r = Rot(P, pf + "ob", [128, D], n=2)
    WGUf = WGU.rearrange("e p k n -> (e p) (k n)")
    WDf = WD.rearrange("e p k n -> (e p) (k n)")
    st1 = {}
    breg = P.bound()

    def load_gu(b):
        off = bass.IndirectOffsetOnAxis(ap=idxw[:, b:b + 1], axis=0)
        wgu, wguk = wgu_r.next()
        P.dma(lambda e, wgu=wgu, off=off: e.indirect_dma_start(out=wgu[:].rearrange("p k n -> p (k n)"), out_offset=None, in_=WGUf, in_offset=off, bounds_check=breg, oob_is_err=False), reads=[pf + "idxw"], writes=[wguk], q="pool")
        st1[("gu", b)] = (wgu, wguk)

    def load_d(b):
        off = bass.IndirectOffsetOnAxis(ap=idxw[:, b:b + 1], axis=0)
        wd, wdk = wd_r.next()
        P.dma(lambda e, wd=wd, off=off: e.indirect_dma_start(out=wd[:].rearrange("p k n -> p (k n)"), out_offset=None, in_=WDf, in_offset=off, bounds_check=breg, oob_is_err=False), reads=[pf + "idxw"], writes=[wdk], q="pool")
        st1[("d", b)] = (wd, wdk)

    def stage1(b):
        (wgu, wguk) = st1.pop(("gu", b))
        xs, xsk = xs_r.next()
        P.d(xs[:], BUF[b * 128:(b + 1) * 128, :], reads=["BUF%d" % l, "BUFz%d" % l], writes=[xsk])
        tp, tpk = tp_r.next()
        for k in range(8):
            P.i("pe", "transpose", reads=[xsk, pf + "ident"], writes=[tpk], out=tp[:, k * 128:(k + 1) * 128], in_=xs[:, k * 128:(k + 1) * 128], identity=ident[:])
        xT, xTk = xT_r.next()
        P.i("act", "activation", reads=[tpk], writes=[xTk + "/0"], out=xT[:, 0:4, :], in_=tp[:, 0:512].rearrange("p (k t) -> p k t", k=4), func=AF.Copy)
        P.i("dve", "tensor_copy", reads=[tpk], writes=[xTk + "/1"], out=xT[:, 4:8, :], in_=tp[:, 512:1024].rearrange("p (k t) -> p k t", k=4))
        pg, pgk = pg_r.next(); pu, puk = pu_r.next()
        for k in range(8):
            P.i("pe", "matmul", reads=[xTk, wguk], writes=[pgk], out=pg[:], lhsT=xT[:, k, :], rhs=wgu[:, k, 0:512], start=(k == 0), stop=(k == 7))
        for k in range(8):
            P.i("pe", "matmul", reads=[xTk, wguk], writes=[puk], out=pu[:], lhsT=xT[:, k, :], rhs=wgu[:, k, 512:1024], start=(k == 0), stop=(k == 7))
        sg, sgk = sg_r.next()
        P.i("act", "activation", reads=[pgk], writes=[sgk], out=sg[:], in_=pg[:], func=AF.Silu)
        hu, huk = hu_r.next()
        P.i("dve", "tensor_tensor", reads=[sgk, puk], writes=[huk], out=hu[:], in0=sg[:], in1=pu[:], op=ALU.mult)
        st1[("h", b)] = (hu, huk)

    def stage2(b):
        (hu, huk) = st1.pop(("h", b))
        (wd, wdk) = st1.pop(("d", b))
        ph, phk = ph_r.next()
        for hc in range(4):
            P.i("pe", "transpose", reads=[huk, pf + "ident"], writes=[phk], out=ph[:, hc * 128:(hc + 1) * 128], in_=hu[:, hc * 128:(hc + 1) * 128], identity=ident[:])
        hT, hTk = hT_r.next()
        P.i("act", "activation", reads=[phk], writes=[hTk], out=hT[:], in_=ph[:].rearrange("p (k t) -> p k t", k=4), func=AF.Copy)
        po, pok = po_r.next()
        for hf in range(2):
            for hc in range(4):
                P.i("pe", "matmul", reads=[hTk, wdk], writes=[pok], out=po[:, hf * 512:(hf + 1) * 512], lhsT=hT[:, hc, :], rhs=wd[:, hc, hf * 512:(hf + 1) * 512], start=(hc == 0), stop=(hc == 3))
        ob, obk = ob_r.next()
        P.i("act", "activation", reads=[pok], writes=[obk + "/0"], out=ob[:, 0:512], in_=po[:, 0:512], func=AF.Copy)
        P.i("dve", "tensor_copy", reads=[pok], writes=[obk + "/1"], out=ob[:, 512:1024], in_=po[:, 512:1024])
        P.d(OBUF[b * 128:(b + 1) * 128, :], ob[:], reads=[obk], writes=[P.u("OBUF%d" % l)])

    load_gu(0); load_d(0)
    for b in range(NB):
        stage1(b)
        if b + 1 < NB:
            load_gu(b + 1)
        stage2(b)
        if b + 1 < NB:
            load_d(b + 1)
    if final:
        fg_d = Dm.get("final_g", [1, D], kind="ExternalInput")
        OUT = Dm.get("out", [L, D], kind="ExternalOutput")
        fg = P.sb(pf + "fg", [128, D]); load_bcast(P, fg[:], pf + "fg", fg_d[0:1, :])
        epsc = P.sb(pf + "eps", [128, 1]); P.i("dve", "memset", writes=[pf + "eps"], ap=epsc[:], constant=EPS)
        junk = P.sb(pf + "junk", [128, D])
    else:
        XN = Dm.get("XN%d" % l, [T, D])
    G2g = [P.sb(pf + "g2%d" % r, [128, D]) for r in range(2)]
    for r in range(2):
        load_bcast(P, G2g[r][:], pf + "g2%d" % r, modrow[r:r + 1, 5120:6144], reads=[mk])
    o1_r = Rot(P, pf + "o1", [128, D], n=2)
    o2_r = Rot(P, pf + "o2", [128, D], n=2)
    xt_r = Rot(P, pf + "xt", [128, D], n=2)
    st_r = Rot(P, pf + "st", [128, 8], n=2)
    ti = 0
    for (c0, cn) in tok_chunks:
        for i in range(cn // 128):
            t0 = c0 + i * 128
            r = 1 if t0 < LC else 0
            o1, o1k = o1_r.next(); o2, o2k = o2_r.next()
            for (o, ok_, col) in ((o1, o1k, ti * 2), (o2, o2k, ti * 2 + 1)):
                P.dma(lambda e, o=o, col=col: e.indirect_dma_start(out=o[:], out_offset=None, in_=OBUF[:, :], in_offset=bass.IndirectOffsetOnAxis(ap=dest[:, col:col + 1], axis=0)),
                      reads=[pf + "dest", "OBUF%d" % l], writes=[ok_], q="pool")
            xt, xk = xt_r.next()
            P.d(xt[:], X1[t0:t0 + 128, :], reads=["X1"], writes=[xk])
            P.i("dve", "tensor_scalar", reads=[o1k, pf + "ww"], writes=[o1k], out=o1[:], in0=o1[:], scalar1=ww[:, ti * 2:ti * 2 + 1], scalar2=None, op0=ALU.mult)
            P.i("dve", "scalar_tensor_tensor", reads=[o1k, o2k, pf + "ww"], writes=[o1k], out=o1[:], in0=o2[:], scalar=ww[:, ti * 2 + 1:ti * 2 + 2], in1=o1[:], op0=ALU.mult, op1=ALU.add)
            P.i("pool", "tensor_tensor", reads=[o1k, pf + "g2%d" % r], writes=[o1k], out=o1[:], in0=o1[:], in1=G2g[r][:], op=ALU.mult)
            P.i("dve", "tensor_tensor", reads=[o1k, xk], writes=[xk], out=xt[:], in0=xt[:], in1=o1[:], op=ALU.add)
            if not final:
                P.d(XN[t0:t0 + 128, :], xt[:], reads=[xk], writes=[P.u("XN%d" % l)])
            else:
                st, sk = st_r.next()
                P.i("act", "activation", reads=[xk], writes=[pf + "junk", sk], out=junk[:], in_=xt[:], func=AF.Square, accum_out=st[:, 0:1])
                P.i("act", "activation", reads=[sk, pf + "eps"], writes=[sk], out=st[:, 1:2], in_=st[:, 0:1], func=AF.Sqrt, bias=epsc[:], scale=1.0 / D)
                P.i("dve", "reciprocal", reads=[sk], writes=[sk], out=st[:, 2:3], in_=st[:, 1:2])
                P.i("dve", "scalar_tensor_tensor", reads=[xk, sk, pf + "fg"], writes=[xk], out=xt[:], in0=xt[:], scalar=st[:, 2:3], in1=fg[:], op0=ALU.mult, op1=ALU.mult)
                P.d(OUT[t0 - LC:t0 - LC + 128, :], xt[:], reads=[xk], writes=[P.u("out")], final=True)
            ti += 1


def rope_tables_np():
    rows = np.repeat(np.arange(L // 64), 64)
    cols = np.tile(np.arange(64), L // 64)
    inv = np.power(np.float32(10000.0), -np.arange(16, dtype=np.float32) / np.float32(16)).astype(np.float32)
    ang = np.stack([rows, cols], -1).astype(np.float32)[..., None] * inv
    cos = np.cos(ang).astype(np.float32).reshape(L, 1, 32)
    sin = np.sin(ang).astype(np.float32).reshape(L, 1, 32)
    return (np.ascontiguousarray(np.broadcast_to(cos, (L, 12, 32)).reshape(L, 384)),
            np.ascontiguousarray(np.broadcast_to(sin, (L, 12, 32)).reshape(L, 384)))


def kmajor(w):
    K, N = w.shape
    return np.ascontiguousarray(w.reshape(K // 128, 128, N).transpose(1, 0, 2))


def core_inputs(inp, b):
    f = np.float32
    m = {}
    m["xin"] = np.ascontiguousarray(np.concatenate([inp["ctx"][b], inp["x"][b]], 0).astype(f))
    cv = np.stack([inp["c"][b], inp["c_ctx"]], -1)
    m["cvec"] = np.ascontiguousarray(cv.reshape(8, 128, 2).transpose(1, 0, 2).astype(f))
    m["ident"] = np.eye(128, dtype=f)
    sh = np.zeros((128, 64), f); sh[64 + np.arange(64), np.arange(64)] = 1.0
    m["shiftm"] = sh
    jj, ii = np.meshgrid(np.arange(128), np.arange(128), indexing="ij")
    wm = np.zeros((128, 2, 2, 128), f)
    wm[:, 0] = (ii <= jj).astype(f)[:, None, :]
    wm[:, 1] = (jj <= ii).astype(f)[:, None, :]
    m["wmask"] = np.ascontiguousarray(wm.reshape(128, 2, 256))
    m["rope_cos"], m["rope_sin"] = rope_tables_np()
    for l in range(2):
        m["ada_w%d" % l] = kmajor(inp["ada_w"][l])
        m["ada_b%d" % l] = np.ascontiguousarray(inp["ada_b"][l].reshape(1, 6144))
        m["w_in%d" % l] = kmajor(inp["w_in"][l][:, PERM])
        m["norm1_g%d" % l] = np.ascontiguousarray(inp["norm1_g"][l].reshape(1, D))
        m["qkg%d" % l] = np.ascontiguousarray(np.concatenate([np.tile(inp["ga_qn_g"][l], 4), np.tile(inp["ga_kn_g"][l], 2)]).reshape(1, 384))
        m["dtb%d" % l] = np.ascontiguousarray(inp["ssd_dt_bias"][l].reshape(1, 8))
        m["sink%d" % l] = np.ascontiguousarray(inp["wa_sink"][l].reshape(1, 4))
        m["w_out%d" % l] = kmajor(inp["w_out"][l])
        m["norm2_g%d" % l] = np.ascontiguousarray(inp["norm2_g"][l].reshape(1, D))
        m["wr%d" % l] = kmajor(np.concatenate([inp["moe_coarse_w"][l], inp["moe_fine_w"][l]], 1))
        m["rb%d" % l] = np.ascontiguousarray(np.concatenate([inp["moe_coarse_b"][l], inp["moe_fine_b"][l]]).reshape(1, 36))
        m["moe_g%d" % l] = np.ascontiguousarray(inp["moe_w_gate"][l].reshape(32, 8, 128, 512).transpose(0, 2, 1, 3))
        m["moe_u%d" % l] = np.ascontiguousarray(inp["moe_w_up"][l].reshape(32, 8, 128, 512).transpose(0, 2, 1, 3))
        m["moe_gu%d" % l] = np.ascontiguousarray(np.concatenate([m["moe_g%d" % l], m["moe_u%d" % l]], -1))
        m["moe_d%d" % l] = np.ascontiguousarray(inp["moe_w_down"][l].reshape(32, 4, 128, 1024).transpose(0, 2, 1, 3))
    m["final_g"] = np.ascontiguousarray(inp["final_g"].reshape(1, D))
    m["ramp"] = (128.0 * np.arange(72) + 1.0).astype(f).reshape(1, 72)
    m["bst"] = (128.0 * np.arange(104)).astype(f).reshape(1, 104)
    m["pidx"] = np.arange(128).astype(f).reshape(128, 1)
    sp, s_ = np.meshgrid(np.arange(128), np.arange(128), indexing="ij")
    m["stri"] = np.ascontiguousarray(np.stack([(sp < s_), np.ones_like(sp, dtype=bool)], 1).astype(f))
    m["tri"] = np.ascontiguousarray(np.stack([(sp <= s_), (sp >= s_)], 1).astype(f))
    for l in range(2):
        Bm = np.zeros((2, 2, 8, 128, 128), f)
        Cm = np.zeros((2, 2, 8, 128, 128), f)
        for d in range(2):
            for j in range(8):
                for gg in range(2):
                    g = 2 * j + gg
                    r0 = (2 * (j % 4) + gg) * 16
                    Bm[d, 0, j, r0:r0 + 16, gg * 64:(gg + 1) * 64] = inp["s5_b_re"][l, d, g].T
                    Bm[d, 1, j, r0:r0 + 16, gg * 64:(gg + 1) * 64] = inp["s5_b_im"][l, d, g].T
                    Cm[d, 0, j, gg * 64:(gg + 1) * 64, r0:r0 + 16] = inp["s5_c_re"][l, d, g].T
                    Cm[d, 1, j, gg * 64:(gg + 1) * 64, r0:r0 + 16] = inp["s5_c_im"][l, d, g].T
        m["s5B%d" % l] = Bm
        m["s5C%d" % l] = Cm
        lamt = np.zeros((128, 3, 16), f)
        for d in range(2):
            for j in range(8):
                for gg in range(2):
                    g = 2 * j + gg
                    lamt[gg * 64:(gg + 1) * 64, 0, d * 8 + j] = inp["s5_lam_re"][l, d, g]
                    lamt[gg * 64:(gg + 1) * 64, 1, d * 8 + j] = inp["s5_lam_im"][l, d, g]
                    lamt[gg * 64:(gg + 1) * 64, 2, d * 8 + j] = inp["s5_log_dt"][l, d, g]
        m["s5lam%d" % l] = lamt
        cw = np.concatenate([inp["ssd_conv_w"][l], inp["ssd_conv_b"][l][None]], 0)
        m["convw%d" % l] = np.ascontiguousarray(cw.reshape(4, 6, 128).transpose(2, 1, 0).astype(f))
        m["alog%d" % l] = np.ascontiguousarray(inp["ssd_a_log"][l].reshape(1, 8))
        m["ssdd%d" % l] = np.ascontiguousarray(inp["ssd_d"][l].reshape(1, 4))
        m["ssdng%d" % l] = np.ascontiguousarray(inp["ssd_norm_g"][l].reshape(1, 256))
        m["s5d%d" % l] = np.ascontiguousarray(inp["s5_d"][l].reshape(2, 128).T.astype(f))
        m["gluw%d" % l] = np.ascontiguousarray(inp["s5_glu_w"][l].reshape(2, 128, 256).transpose(1, 0, 2).astype(f))
        m["glub%d" % l] = np.ascontiguousarray(inp["s5_glu_b"][l].reshape(2, 128).T.astype(f))
    for l in range(0):
        pass
    return m


def attn_consts(P, Dm, pf):
    c = {}
    shift_d = Dm.get("shiftm", [128, 64], kind="ExternalInput")
    c["shift"] = P.sb(pf + "shift", [128, 64], F32R)
    P.d(c["shift"][:], shift_d[:, :], writes=[pf + "shift"], q="pool")
    return c


def phase_c(P, Dm, l, ctx_out):
    pf = "c%d_" % l
    QKT = Dm.get("QKT", [768, T])
    TMS = Dm.get("TMS", [T, 520])
    CAT = Dm.get("CAT", [1024, T])
    cst = attn_consts(P, Dm, pf)
    KT = P.sb(pf + "KT", [64, 2, T], F32R)
    V = P.sb(pf + "V", [128, NT, 2, 128], F32R)
    for kv in range(2):
        P.d(KT[:, kv, :], QKT[256 + kv * 64:256 + (kv + 1) * 64, :], reads=["QKT"], writes=[pf + "KT"], q="pool")
    P.i("dve", "memset", writes=[pf + "V/1"], ap=V[:].bitcast(F32)[:, :, :, 64:128], constant=1.0)
    for kv in range(2):
        P.d(V[:, :, kv, 0:64], TMS[:, kv * 64:(kv + 1) * 64].rearrange("(n p) d -> p n d", p=128), reads=["TMS"], writes=[pf + "V/0%d" % kv], q="pool")
    vkeys = [pf + "V"]
    q_r = Rot(P, pf + "q", [64, 4, 256], F32R, n=2)
    s_r = Rot(P, pf + "s", [128, 512], n=3, psum=True)
    p_r = Rot(P, pf + "p", [128, 512], F32R, n=3)
    o_r = Rot(P, pf + "o", [128, 512], n=2, psum=True)
    os_r = Rot(P, pf + "os", [128, 512], F32R, n=2)
    dn_r = Rot(P, pf + "dn", [64, 512], n=1, psum=True)
    rd_r = Rot(P, pf + "rd", [64, 512], n=2)
    ot_r = Rot(P, pf + "ot", [64, 512], n=2)
    qtiles = [(LC + 256 * i, NT) for i in range(L // 256)]
    if ctx_out:
        qtiles = [(0, 2)] + qtiles
    for (q0, nkb) in qtiles:
        qt, qtk = q_r.next()
        P.d(qt[:], QKT[0:256, q0:q0 + 256].rearrange("(h d) q -> d h q", d=64), reads=["QKT"], writes=[qtk], q="pool")
        for kv in range(2):
            o, ok = o_r.next()
            its = list(range(nkb))
            pend = []

            def issue_s(s):
                sp_, spk = s_r.next()
                P.i("pe", "matmul", reads=[pf + "KT", qtk], writes=[spk], out=sp_[:], lhsT=KT[:, kv, s * 128:(s + 1) * 128],
                    rhs=qt[:, 2 * kv:2 * kv + 2, :], start=True, stop=True)
                pt, ptk = p_r.next()
                P.i("act", "activation", reads=[spk], writes=[ptk], out=pt[:], in_=sp_[:], func=AF.Exp, scale=0.125)
                return (s, pt, ptk)

            LOOK = 2
            for s in its[:LOOK]:
                pend.append(issue_s(s))
            for idx, s in enumerate(its):
                (s_, pt, ptk) = pend.pop(0)
                if idx + LOOK < len(its):
                    pend.append(issue_s(its[idx + LOOK]))
                P.i("pe", "matmul", reads=[ptk] + vkeys, writes=[ok], out=o[:], lhsT=V[:, s_, kv, :], rhs=pt[:],
                    start=(idx == 0), stop=(idx == len(its) - 1))
            osb, osk = os_r.next()
            P.i("act", "activation", reads=[ok], writes=[osk], out=osb[:], in_=o[:], func=AF.Copy)
            dn, dnk = dn_r.next()
            P.i("pe", "matmul", reads=[osk, pf + "shift"], writes=[dnk], out=dn[:], lhsT=cst["shift"][:], rhs=osb[:], start=True, stop=True)
            rd, rdk = rd_r.next()
            P.i("dve", "reciprocal", reads=[dnk], writes=[rdk], out=rd[:], in_=dn[:])
            ot, otk = ot_r.next()
            P.i("dve", "tensor_tensor", reads=[osk, rdk], writes=[otk], out=ot[:], in0=osb[0:64, :].bitcast(F32), in1=rd[:], op=ALU.mult)
            P.d(CAT[256 + kv * 128:256 + (kv + 1) * 128, q0:q0 + 256].rearrange("(hh d) q -> d hh q", d=64),
                ot[:].rearrange("d (hh q) -> d hh q", hh=2), reads=[otk], writes=[P.u("CAT")])


def phase_e(P, Dm, l, ctx_out):
    pf = "e%d_" % l
    QKT = Dm.get("QKT", [768, T])
    TMS = Dm.get("TMS", [T, 520])
    CAT = Dm.get("CAT", [1024, T])
    sink_d = Dm.get("sink%d" % l, [1, 4], kind="ExternalInput")
    mask_d = Dm.get("wmask", [128, 2, 256], kind="ExternalInput")
    cst = attn_consts(P, Dm, pf)
    KT = P.sb(pf + "KT", [64, 2, T], F32R)
    V = P.sb(pf + "V", [128, NT, 2, 128], F32R)
    for kv in range(2):
        P.d(KT[:, kv, :], QKT[640 + kv * 64:640 + (kv + 1) * 64, :], reads=["QKT"], writes=[pf + "KT"], q="pool")
    P.i("dve", "memset", writes=[pf + "V/1"], ap=V[:].bitcast(F32)[:, :, :, 64:128], constant=1.0)
    for kv in range(2):
        P.d(V[:, :, kv, 0:64], TMS[:, 128 + kv * 64:128 + (kv + 1) * 64].rearrange("(n p) d -> p n d", p=128), reads=["TMS"], writes=[pf + "V/0%d" % kv], q="pool")
    vkeys = [pf + "V"]
    mask = P.sb(pf + "mask", [128, 2, 256])
    P.d(mask[:], mask_d[:, :, :], writes=[pf + "mask"])
    esk = P.sb(pf + "esk", [64, 4])
    P.d(esk[:], sink_d[0:1, :].to_broadcast([64, 4]), writes=[pf + "esk"])
    P.i("act", "activation", reads=[pf + "esk"], writes=[pf + "esk"], out=esk[:], in_=esk[:], func=AF.Exp)
    q_r = Rot(P, pf + "q", [64, 4, 128], F32R, n=2)
    s_r = Rot(P, pf + "s", [128, 256], n=3, psum=True)
    p_r = Rot(P, pf + "p", [128, 256], F32R, n=3)
    o_r = Rot(P, pf + "o", [128, 256], n=2, psum=True)
    os_r = Rot(P, pf + "os", [128, 256], F32R, n=2)
    dn_r = Rot(P, pf + "dn", [64, 256], n=1, psum=True)
    rd_r = Rot(P, pf + "rd", [64, 256], n=2)
    ot_r = Rot(P, pf + "ot", [64, 256], n=2)
    qtiles = []
    if ctx_out:
        qtiles += [(i, [(0, None), (1, None)]) for i in range(2)]
    for n in range(L // 128):
        ti = 2 + n
        kb = [(0, None), (1, None)]
        if n > 0:
            kb.append((ti - 1, 0))
        kb.append((ti, None))
        if n < L // 128 - 1:
            kb.append((ti + 1, 1))
        qtiles.append((ti, kb))
    for (ti, kbs) in qtiles:
        q0 = ti * 128
        qt, qtk = q_r.next()
        P.d(qt[:], QKT[384:640, q0:q0 + 128].rearrange("(h d) q -> d h q", d=64), reads=["QKT"], writes=[qtk], q="pool")
        for kv in range(2):
            o, ok = o_r.next()
            for idx, (s, mi) in enumerate(kbs):
                sp_, spk = s_r.next()
                P.i("pe", "matmul", reads=[pf + "KT", qtk], writes=[spk], out=sp_[:], lhsT=KT[:, kv, s * 128:(s + 1) * 128],
                    rhs=qt[:, 2 * kv:2 * kv + 2, :], start=True, stop=True)
                pt, ptk = p_r.next()
                P.i("act", "activation", reads=[spk], writes=[ptk], out=pt[:], in_=sp_[:], func=AF.Exp, scale=0.125)
                if mi is not None:
                    P.i("dve", "tensor_tensor", reads=[ptk, pf + "mask"], writes=[ptk], out=pt[:], in0=pt[:].bitcast(F32), in1=mask[:, mi, :], op=ALU.mult)
                P.i("pe", "matmul", reads=[ptk] + vkeys, writes=[ok], out=o[:], lhsT=V[:, s, kv, :], rhs=pt[:],
                    start=(idx == 0), stop=(idx == len(kbs) - 1))
            osb, osk = os_r.next()
            P.i("act", "activation", reads=[ok], writes=[osk], out=osb[:], in_=o[:], func=AF.Copy)
            dn, dnk = dn_r.next()
            P.i("pe", "matmul", reads=[osk, pf + "shift"], writes=[dnk], out=dn[:], lhsT=cst["shift"][:], rhs=osb[:], start=True, stop=True)
            rd, rdk = rd_r.next()
            for hh in range(2):
                h = 2 * kv + hh
                P.i("dve", "tensor_scalar", reads=[dnk, pf + "esk"], writes=[rdk + "/%d" % hh], out=rd[:, hh * 128:(hh + 1) * 128], in0=dn[:, hh * 128:(hh + 1) * 128],
                    scalar1=esk[:, h:h + 1], scalar2=None, op0=ALU.add)
            P.i("dve", "reciprocal", reads=[rdk], writes=[rdk], out=rd[:], in_=rd[:])
            ot, otk = ot_r.next()
            P.i("dve", "tensor_tensor", reads=[osk, rdk], writes=[otk], out=ot[:], in0=osb[0:64, :].bitcast(F32), in1=rd[:], op=ALU.mult)
            P.d(CAT[768 + kv * 128:768 + (kv + 1) * 128, q0:q0 + 128].rearrange("(hh d) q -> d hh q", d=64),
                ot[:].rearrange("d (hh q) -> d hh q", hh=2), reads=[otk], writes=[P.u("CAT")])


BIG = 1.0e30
S5_ENG2 = "dve"


def phase_f(P, Dm, l, src_name, tok_chunks=None):
    pf = "f%d_" % l
    xsrc = Dm.get(src_name, [T, D])
    CAT = Dm.get("CAT", [1024, T])
    w_out = Dm.get("w_out%d" % l, [128, 8, 1024], kind="ExternalInput")
    modrow = Dm.get("modrow%d" % l, [2, 6144])
    n2g = Dm.get("norm2_g%d" % l, [1, D], kind="ExternalInput")
    wr_d = Dm.get("wr%d" % l, [128, 8, 36], kind="ExternalInput")
    rb_d = Dm.get("rb%d" % l, [1, 36], kind="ExternalInput")
    ident_d = Dm.get("ident", [128, 128], kind="ExternalInput")
    X1 = Dm.get("X1", [T, D])
    H2T = Dm.get("H2T", [D, T])
    WTd = Dm.get("WTd", [32, T])
    mk = "modrow%d" % l
    ident = P.sb(pf + "ident", [128, 128])
    P.d(ident[:], ident_d[:, :], writes=[pf + "ident"])
    wo = P.sb(pf + "wo", [128, 8, 1024], F32R)
    for k in range(8):
        P.d(wo[:, k, :], w_out[:, k, :], writes=[pf + "wo/%d" % k], q="pool")
    wr = P.sb(pf + "wr", [128, 8, 36])
    P.d(wr[:], wr_d[:, :, :], writes=[pf + "wr"])
    rb = P.sb(pf + "rb", [128, 36])
    load_bcast(P, rb[:], pf + "rb", rb_d[0:1, :])
    G1 = [P.sb(pf + "G1%d" % r, [128, D]) for r in range(2)]
    G2 = [P.sb(pf + "G2%d" % r, [128, D]) for r in range(2)]
    SH2 = [P.sb(pf + "SH2%d" % r, [128, D]) for r in range(2)]
    gn = P.sb(pf + "gn", [128, D])
    load_bcast(P, gn[:], pf + "gn", n2g[0:1, :])
    for r in range(2):
        load_bcast(P, G1[r][:], pf + "G1%d" % r, modrow[r:r + 1, 2048:3072], reads=[mk])
        load_bcast(P, SH2[r][:], pf + "SH2%d" % r, modrow[r:r + 1, 3072:4096], reads=[mk])
        load_bcast(P, G2[r][:], pf + "G2%d" % r, modrow[r:r + 1, 4096:5120], reads=[mk])
        P.i("dve", "scalar_tensor_tensor", reads=[pf + "G2%d" % r, pf + "gn"], writes=[pf + "G2%d" % r], out=G2[r][:], in0=G2[r][:], scalar=1.0, in1=gn[:], op0=ALU.add, op1=ALU.mult)
    epsc = P.sb(pf + "eps", [128, 1])
    P.i("dve", "memset", writes=[pf + "eps"], ap=epsc[:], constant=EPS)
    ct_r = Rot(P, pf + "ct", [128, 8, 512], F32R, n=2)
    po_r = Rot(P, pf + "po", [128, 1024], n=1, psum=True)
    xt_r = Rot(P, pf + "xt", [128, D], n=2)
    x1_r = Rot(P, pf + "x1", [128, D], n=2)
    h_r = Rot(P, pf + "h", [128, D], n=2)
    junk = P.sb(pf + "junk", [128, D])
    st_r = Rot(P, pf + "st", [128, 64], n=3)
    tp_r = Rot(P, pf + "tp", [128, 1024], n=1, psum=True)
    hT_r = Rot(P, pf + "hT", [128, 8, 128], F32R, n=2)
    hTf_r = Rot(P, pf + "hTf", [128, 8, 128], n=2)
    pr_r = Rot(P, pf + "pr", [128, 512], n=1, psum=True)
    lg_r = Rot(P, pf + "lg", [128, 36], n=2)
    mk_r = Rot(P, pf + "mk", [128, 4, 8], n=2)
    oh_r = Rot(P, pf + "oh", [128, 3, 32], n=2)
    wt_r = Rot(P, pf + "wt", [128, 32], n=2)
    pw_r = Rot(P, pf + "pw", [128, 512], n=1, psum=True)
    wT_r = Rot(P, pf + "wT", [32, 128], n=2)
    for (c0, cn) in (tok_chunks or CHUNKS):
        ct, ctk = ct_r.next()
        P.d(ct[:, :, 0:cn], CAT[:, c0:c0 + cn].rearrange("(k p) t -> p k t", p=128), reads=["CAT"], writes=[ctk], q="pool")
        for i in range(cn // 128):
            t0 = c0 + i * 128
            r = 1 if t0 < LC else 0
            po, pok = po_r.next()
            for hf in range(2):
                for k in range(8):
                    P.i("pe", "matmul", reads=[ctk, pf + "wo/%d" % k], writes=[pok], out=po[:, hf * 512:(hf + 1) * 512], lhsT=ct[:, k, i * 128:(i + 1) * 128],
                        rhs=wo[:, k, hf * 512:(hf + 1) * 512], start=(k == 0), stop=(k == 7))
            xt, xk = xt_r.next()
            P.d(xt[:], xsrc[t0:t0 + 128, :], reads=[src_name], writes=[xk])
            x1, x1k = x1_r.next()
            for hf in range(2):
                sl = slice(hf * 512, (hf + 1) * 512)
                P.i("dve", "tensor_tensor", reads=[pok, pf + "G1%d" % r], writes=[x1k + "/%d" % hf], out=x1[:, sl], in0=po[:, sl], in1=G1[r][:, sl], op=ALU.mult)
            P.i("pool", "tensor_tensor", reads=[x1k, xk], writes=[x1k], out=x1[:], in0=x1[:], in1=xt[:], op=ALU.add)
            P.d(X1[t0:t0 + 128, :], x1[:], reads=[x1k], writes=[P.u("X1")])
            st, sk = st_r.next()
            P.i("act", "activation", reads=[x1k], writes=[pf + "junk", sk], out=junk[:], in_=x1[:], func=AF.Square, accum_out=st[:, 0:1])
            P.i("act", "activation", reads=[sk, pf + "eps"], writes=[sk], out=st[:, 1:2], in_=st[:, 0:1], func=AF.Sqrt, bias=epsc[:], scale=1.0 / D)
            P.i("dve", "reciprocal", reads=[sk], writes=[sk], out=st[:, 2:3], in_=st[:, 1:2])
            h, hk = h_r.next()
            P.i("dve", "scalar_tensor_tensor", reads=[x1k, sk, pf + "G2%d" % r], writes=[hk], out=h[:], in0=x1[:], scalar=st[:, 2:3], in1=G2[r][:], op0=ALU.mult, op1=ALU.mult)
            P.i("pool", "tensor_tensor", reads=[hk, pf + "SH2%d" % r], writes=[hk], out=h[:], in0=h[:], in1=SH2[r][:], op=ALU.add)
            tp, tpk = tp_r.next()
            for k in range(8):
                P.i("pe", "transpose", reads=[hk, pf + "ident"], writes=[tpk], out=tp[:, k * 128:(k + 1) * 128], in_=h[:, k * 128:(k + 1) * 128], identity=ident[:])
            hT, hTk = hT_r.next()
            hTf, hTfk = hTf_r.next()
            P.i("act", "activation", reads=[tpk], writes=[hTk], out=hT[:], in_=tp[:].rearrange("p (k t) -> p k t", k=8), func=AF.Copy)
            P.i("dve", "tensor_copy", reads=[tpk], writes=[hTfk], out=hTf[:], in_=tp[:].rearrange("p (k t) -> p k t", k=8))
            P.d(H2T[:, t0:t0 + 128].rearrange("(k p) t -> p k t", p=128), hT[:].bitcast(F32), reads=[hTk], writes=[P.u("H2T")])
            pr, prk = pr_r.next()
            for k in range(8):
                P.i("pe", "matmul", reads=[hTfk, pf + "wr"], writes=[prk], out=pr[:, 0:36], lhsT=hTf[:, k, :], rhs=wr[:, k, :], start=(k == 0), stop=(k == 7))
            lg, lgk = lg_r.next()
            P.i("dve", "tensor_tensor", reads=[prk, pf + "rb"], writes=[lgk], out=lg[:], in0=pr[:, 0:36], in1=rb[:], op=ALU.add)
            P.i("dve", "tensor_reduce", reads=[lgk], writes=[sk], out=st[:, 4:5], in_=lg[:, 0:4], axis=AX.X, op=ALU.max)
            P.i("dve", "tensor_scalar", reads=[sk], writes=[sk], out=st[:, 5:6], in0=st[:, 4:5], scalar1=-1.0, scalar2=None, op0=ALU.mult)
            P.i("act", "activation", reads=[lgk, sk], writes=[sk], out=st[:, 32:36], in_=lg[:, 0:4], func=AF.Exp, bias=st[:, 5:6], accum_out=st[:, 6:7])
            P.i("dve", "reciprocal", reads=[sk], writes=[sk], out=st[:, 7:8], in_=st[:, 6:7])
            P.i("dve", "tensor_scalar", reads=[lgk, sk], writes=[sk], out=st[:, 8:12], in0=lg[:, 0:4], scalar1=st[:, 4:5], scalar2=None, op0=ALU.is_equal)
            P.i("dve", "tensor_scalar", reads=[sk], writes=[sk], out=st[:, 12:16], in0=st[:, 8:12], scalar1=BIG, scalar2=-BIG, op0=ALU.mult, op1=ALU.add)
            mkd, mkk = mk_r.next()
            P.i("dve", "tensor_tensor", reads=[lgk, sk], writes=[mkk], out=mkd[:], in0=lg[:, 4:36].rearrange("p (g e) -> p g e", g=4),
                in1=st[:, 12:16].unsqueeze(2).to_broadcast([128, 4, 8]), op=ALU.add)
            mflat = mkd[:].rearrange("p g e -> p (g e)")
            P.i("dve", "max", reads=[mkk], writes=[sk], out=st[:, 16:24], in_=mflat)
            oh, ohk = oh_r.next()
            P.i("dve", "tensor_scalar", reads=[mkk, sk], writes=[ohk + "/1"], out=oh[:, 0, :], in0=mflat, scalar1=st[:, 16:17], scalar2=None, op0=ALU.is_equal)
            P.i("dve", "tensor_scalar", reads=[mkk, sk], writes=[ohk + "/2"], out=oh[:, 1, :], in0=mflat, scalar1=st[:, 17:18], scalar2=None, op0=ALU.is_equal)
            P.i("dve", "tensor_tensor", reads=[sk], writes=[sk], out=st[:, 24:25], in0=st[:, 17:18], in1=st[:, 16:17], op=ALU.subtract)
            P.i("act", "activation", reads=[sk], writes=[sk], out=st[:, 25:26], in_=st[:, 24:25], func=AF.Exp)
            P.i("dve", "tensor_scalar", reads=[sk], writes=[sk], out=st[:, 26:27], in0=st[:, 25:26], scalar1=1.0, scalar2=None, op0=ALU.add)
            P.i("dve", "reciprocal", reads=[sk], writes=[sk], out=st[:, 27:28], in_=st[:, 26:27])
            P.i("dve", "tensor_tensor", reads=[sk], writes=[sk], out=st[:, 28:29], in0=st[:, 27:28], in1=st[:, 7:8], op=ALU.mult)
            P.i("dve", "tensor_tensor", reads=[sk], writes=[sk], out=st[:, 29:30], in0=st[:, 7:8], in1=st[:, 28:29], op=ALU.subtract)
            P.i("dve", "tensor_scalar", reads=[ohk + "/1", sk], writes=[ohk + "/3"], out=oh[:, 2, :], in0=oh[:, 0, :], scalar1=st[:, 28:29], scalar2=None, op0=ALU.mult)
            wt, wtk = wt_r.next()
            P.i("dve", "scalar_tensor_tensor", reads=[ohk + "/2", ohk + "/3", sk], writes=[wtk], out=wt[:], in0=oh[:, 1, :], scalar=st[:, 29:30], in1=oh[:, 2, :], op0=ALU.mult, op1=ALU.add)
            pw, pwk = pw_r.next()
            P.i("pe", "transpose", reads=[wtk, pf + "ident"], writes=[pwk], out=pw[0:32, 0:128], in_=wt[:], identity=ident[:])
            wT, wTk = wT_r.next()
            P.i("act", "activation", reads=[pwk], writes=[wTk], out=wT[:], in_=pw[0:32, 0:128], func=AF.Copy)
            P.d(WTd[:, t0:t0 + 128], wT[:], reads=[wTk], writes=[P.u("WTd")])


def phase_f2(P, Dm, l, src_name, tok_chunks=None):
    pf = "F%d_" % l
    xsrc = Dm.get(src_name, [T, D])
    CAT = Dm.get("CAT", [1024, T])
    w_out = Dm.get("w_out%d" % l, [128, 8, 1024], kind="ExternalInput")
    modrow = Dm.get("modrow%d" % l, [2, 6144])
    n2g = Dm.get("norm2_g%d" % l, [1, D], kind="ExternalInput")
    wr_d = Dm.get("wr%d" % l, [128, 8, 36], kind="ExternalInput")
    rb_d = Dm.get("rb%d" % l, [1, 36], kind="ExternalInput")
    ident_d = Dm.get("ident", [128, 128], kind="ExternalInput")
    X1 = Dm.get("X1", [T, D])
    chunks_ = (tok_chunks or CHUNKS)
    ntf = sum(cn for _, cn in chunks_) // 128
    NB = (2 * ntf * 128) // 128 + 32
    BUF = Dm.get("BUF%d" % l, [NB * 128, D])
    IDXW = Dm.get("IDXW%d" % l, [128, NB], I32)
    DEST = Dm.get("DEST%d" % l, [128, ntf * 2], I32)
    WWd = Dm.get("WWd%d" % l, [128, ntf * 2])
    ramp_d = Dm.get("ramp", [1, 72], kind="ExternalInput")
    bst_d = Dm.get("bst", [1, 104], kind="ExternalInput")
    pidx_d = Dm.get("pidx", [128, 1], kind="ExternalInput")
    stri_d = Dm.get("stri", [128, 2, 128], kind="ExternalInput")
    OH = P.sb(pf + "OH", [128, ntf, 2, 32])
    WW = P.sb(pf + "WW", [128, ntf, 2])
    RK = P.sb(pf + "RK", [128, ntf, 32])
    Msum = P.sb(pf + "Msum", [128, 32])
    P.i("dve", "memset", writes=[pf + "Msum"], ap=Msum[:], constant=0.0)
    stri = P.sb(pf + "stri", [128, 2, 128]); P.d(stri[:], stri_d[:, :, :], writes=[pf + "stri"])
    Mt_r = Rot(P, pf + "Mt", [128, 32], n=2)
    zz = P.sb(pf + "zz", [128, 4096])
    P.i("pool", "memset", writes=[pf + "zz"], ap=zz[:], constant=0.0)
    rows_per = 128 * 4
    for r0 in range(0, NB * 128, rows_per):
        P.d(BUF[r0:r0 + rows_per, :].rearrange("(p a) n -> p (a n)", p=128), zz[:], reads=[pf + "zz"], writes=[P.u("BUFz%d" % l)])
    mk = "modrow%d" % l
    ident = P.sb(pf + "ident", [128, 128])
    P.d(ident[:], ident_d[:, :], writes=[pf + "ident"])
    wo = P.sb(pf + "wo", [128, 8, 1024], F32R)
    for k in range(8):
        P.d(wo[:, k, :], w_out[:, k, :], writes=[pf + "wo/%d" % k], q="pool")
    wr = P.sb(pf + "wr", [128, 8, 36])
    P.d(wr[:], wr_d[:, :, :], writes=[pf + "wr"])
    rb = P.sb(pf + "rb", [128, 36])
    load_bcast(P, rb[:], pf + "rb", rb_d[0:1, :])
    G1 = [P.sb(pf + "G1%d" % r, [128, D]) for r in range(2)]
    G2 = [P.sb(pf + "G2%d" % r, [128, D]) for r in range(2)]
    SH2 = [P.sb(pf + "SH2%d" % r, [128, D]) for r in range(2)]
    gn = P.sb(pf + "gn", [128, D])
    load_bcast(P, gn[:], pf + "gn", n2g[0:1, :])
    for r in range(2):
        load_bcast(P, G1[r][:], pf + "G1%d" % r, modrow[r:r + 1, 2048:3072], reads=[mk])
        load_bcast(P, SH2[r][:], pf + "SH2%d" % r, modrow[r:r + 1, 3072:4096], reads=[mk])
        load_bcast(P, G2[r][:], pf + "G2%d" % r, modrow[r:r + 1, 4096:5120], reads=[mk])
        P.i("dve", "scalar_tensor_tensor", reads=[pf + "G2%d" % r, pf + "gn"], writes=[pf + "G2%d" % r], out=G2[r][:], in0=G2[r][:], scalar=1.0, in1=gn[:], op0=ALU.add, op1=ALU.mult)
    epsc = P.sb(pf + "eps", [128, 1])
    P.i("dve", "memset", writes=[pf + "eps"], ap=epsc[:], constant=EPS)
    ct_r = Rot(P, pf + "ct", [128, 8, 512], F32R, n=2)
    po_r = Rot(P, pf + "po", [128, 1024], n=1, psum=True)
    xt_r = Rot(P, pf + "xt", [128, D], n=2)
    x1_r = Rot(P, pf + "x1", [128, D], n=2)
    h_r = Rot(P, pf + "h", [128, D], n=2)
    junk = P.sb(pf + "junk", [128, D])
    st_r = Rot(P, pf + "st", [128, 64], n=3)
    tp_r = Rot(P, pf + "tp", [128, 1024], n=1, psum=True)
    H2 = Dm.get("H2_%d" % l, [T, D])
    hTf_r = Rot(P, pf + "hTf", [128, 8, 128], n=2)
    pr_r = Rot(P, pf + "pr", [128, 512], n=1, psum=True)
    lg_r = Rot(P, pf + "lg", [128, 36], n=2)
    mk_r = Rot(P, pf + "mk", [128, 4, 8], n=2)
    oh_r = Rot(P, pf + "oh", [128, 3, 32], n=2)
    pw_r = Rot(P, pf + "pw", [128, 512], n=1, psum=True)
    tile_i = 0
    for (c0, cn) in chunks_:
        ct, ctk = ct_r.next()
        P.d(ct[:, :, 0:cn], CAT[:, c0:c0 + cn].rearrange("(k p) t -> p k t", p=128), reads=["CAT"], writes=[ctk], q="pool")
        for i in range(cn // 128):
            t0 = c0 + i * 128
            r = 1 if t0 < LC else 0
            po, pok = po_r.next()
            for hf in range(2):
                for k in range(8):
                    P.i("pe", "matmul", reads=[ctk, pf + "wo/%d" % k], writes=[pok], out=po[:, hf * 512:(hf + 1) * 512], lhsT=ct[:, k, i * 128:(i + 1) * 128],
                        rhs=wo[:, k, hf * 512:(hf + 1) * 512], start=(k == 0), stop=(k == 7))
            xt, xk = xt_r.next()
            P.d(xt[:], xsrc[t0:t0 + 128, :], reads=[src_name], writes=[xk])
            x1, x1k = x1_r.next()
            for hf in range(2):
                sl = slice(hf * 512, (hf + 1) * 512)
                P.i("dve", "tensor_tensor", reads=[pok, pf + "G1%d" % r], writes=[x1k + "/%d" % hf], out=x1[:, sl], in0=po[:, sl], in1=G1[r][:, sl], op=ALU.mult)
            P.i("pool", "tensor_tensor", reads=[x1k, xk], writes=[x1k], out=x1[:], in0=x1[:], in1=xt[:], op=ALU.add)
            P.d(X1[t0:t0 + 128, :], x1[:], reads=[x1k], writes=[P.u("X1")])
            st, sk = st_r.next()
            P.i("act", "activation", reads=[x1k], writes=[pf + "junk", sk], out=junk[:], in_=x1[:], func=AF.Square, accum_out=st[:, 0:1])
            P.i("act", "activation", reads=[sk, pf + "eps"], writes=[sk], out=st[:, 1:2], in_=st[:, 0:1], func=AF.Sqrt, bias=epsc[:], scale=1.0 / D)
            P.i("dve", "reciprocal", reads=[sk], writes=[sk], out=st[:, 2:3], in_=st[:, 1:2])
            h, hk = h_r.next()
            P.i("dve", "scalar_tensor_tensor", reads=[x1k, sk, pf + "G2%d" % r], writes=[hk], out=h[:], in0=x1[:], scalar=st[:, 2:3], in1=G2[r][:], op0=ALU.mult, op1=ALU.mult)
            P.i("pool", "tensor_tensor", reads=[hk, pf + "SH2%d" % r], writes=[hk], out=h[:], in0=h[:], in1=SH2[r][:], op=ALU.add)
            P.d(H2[t0:t0 + 128, :], h[:], reads=[hk], writes=[P.u("H2_%d" % l)])
            tp, tpk = tp_r.next()
            for k in range(8):
                P.i("pe", "transpose", reads=[hk, pf + "ident"], writes=[tpk], out=tp[:, k * 128:(k + 1) * 128], in_=h[:, k * 128:(k + 1) * 128], identity=ident[:])
            hTf, hTfk = hTf_r.next()
            P.i("dve", "tensor_copy", reads=[tpk], writes=[hTfk], out=hTf[:], in_=tp[:].rearrange("p (k t) -> p k t", k=8))
            pr, prk = pr_r.next()
            for k in range(8):
                P.i("pe", "matmul", reads=[hTfk, pf + "wr"], writes=[prk], out=pr[:, 0:36], lhsT=hTf[:, k, :], rhs=wr[:, k, :], start=(k == 0), stop=(k == 7))
            lg, lgk = lg_r.next()
            P.i("dve", "tensor_tensor", reads=[prk, pf + "rb"], writes=[lgk], out=lg[:], in0=pr[:, 0:36], in1=rb[:], op=ALU.add)
            P.i("dve", "tensor_reduce", reads=[lgk], writes=[sk], out=st[:, 4:5], in_=lg[:, 0:4], axis=AX.X, op=ALU.max)
            P.i("dve", "tensor_scalar", reads=[sk], writes=[sk], out=st[:, 5:6], in0=st[:, 4:5], scalar1=-1.0, scalar2=None, op0=ALU.mult)
            P.i("act", "activation", reads=[lgk, sk], writes=[sk], out=st[:, 32:36], in_=lg[:, 0:4], func=AF.Exp, bias=st[:, 5:6], accum_out=st[:, 6:7])
            P.i("dve", "reciprocal", reads=[sk], writes=[sk], out=st[:, 7:8], in_=st[:, 6:7])
            P.i("dve", "tensor_scalar", reads=[lgk, sk], writes=[sk], out=st[:, 8:12], in0=lg[:, 0:4], scalar1=st[:, 4:5], scalar2=None, op0=ALU.is_equal)
            P.i("dve", "tensor_scalar", reads=[sk], writes=[sk], out=st[:, 12:16], in0=st[:, 8:12], scalar1=BIG, scalar2=-BIG, op0=ALU.mult, op1=ALU.add)
            mkd, mkk = mk_r.next()
            P.i("dve", "tensor_tensor", reads=[lgk, sk], writes=[mkk], out=mkd[:], in0=lg[:, 4:36].rearrange("p (g e) -> p g e", g=4),
                in1=st[:, 12:16].unsqueeze(2).to_broadcast([128, 4, 8]), op=ALU.add)
            mflat = mkd[:].rearrange("p g e -> p (g e)")
            P.i("dve", "max", reads=[mkk], writes=[sk], out=st[:, 16:24], in_=mflat)
            oh, ohk = oh_r.next()
            P.i("dve", "tensor_scalar", reads=[mkk, sk], writes=[ohk + "/1"], out=oh[:, 0, :], in0=mflat, scalar1=st[:, 16:17], scalar2=None, op0=ALU.is_equal)
            P.i("dve", "tensor_scalar", reads=[mkk, sk], writes=[ohk + "/2"], out=oh[:, 1, :], in0=mflat, scalar1=st[:, 17:18], scalar2=None, op0=ALU.is_equal)
            P.i("dve", "tensor_tensor", reads=[sk], writes=[sk], out=st[:, 24:25], in0=st[:, 17:18], in1=st[:, 16:17], op=ALU.subtract)
            P.i("act", "activation", reads=[sk], writes=[sk], out=st[:, 25:26], in_=st[:, 24:25], func=AF.Exp)
            P.i("dve", "tensor_scalar", reads=[sk], writes=[sk], out=st[:, 26:27], in0=st[:, 25:26], scalar1=1.0, scalar2=None, op0=ALU.add)
            P.i("dve", "reciprocal", reads=[sk], writes=[sk], out=st[:, 27:28], in_=st[:, 26:27])
            P.i("dve", "tensor_tensor", reads=[sk], writes=[sk], out=st[:, 28:29], in0=st[:, 27:28], in1=st[:, 7:8], op=ALU.mult)
            P.i("dve", "tensor_tensor", reads=[sk], writes=[sk], out=st[:, 29:30], in0=st[:, 7:8], in1=st[:, 28:29], op=ALU.subtract)
            ti = tile_i
            tile_i += 1
            P.i("act", "activation", reads=[ohk + "/1"], writes=[pf + "OH/%d_0" % ti], out=OH[:, ti, 0, :], in_=oh[:, 0, :], func=AF.Copy)
            P.i("act", "activation", reads=[ohk + "/2"], writes=[pf + "OH/%d_1" % ti], out=OH[:, ti, 1, :], in_=oh[:, 1, :], func=AF.Copy)
            P.i("act", "activation", reads=[sk], writes=[pf + "WW/%d" % ti], out=WW[:, ti, :], in_=st[:, 28:30], func=AF.Copy)
            Mt, Mtk = Mt_r.next()
            P.i("dve", "tensor_tensor", reads=[ohk + "/1", ohk + "/2"], writes=[Mtk], out=Mt[:], in0=oh[:, 0, :], in1=oh[:, 1, :], op=ALU.add)
            pw, pwk = pw_r.next()
            P.i("pe", "matmul", reads=[Mtk, pf + "stri"], writes=[pwk], out=pw[:, 0:32], lhsT=stri[:, 0, :], rhs=Mt[:], start=True, stop=False)
            P.i("pe", "matmul", reads=[pf + "Msum", pf + "stri"], writes=[pwk], out=pw[:, 0:32], lhsT=stri[:, 1, :], rhs=Msum[:], start=False, stop=True)
            P.i("act", "activation", reads=[pwk], writes=[pf + "RK/%d" % ti], out=RK[:, ti, :], in_=pw[:, 0:32], func=AF.Copy)
            P.i("dve", "tensor_tensor", reads=[Mtk, pf + "Msum"], writes=[pf + "Msum"], out=Msum[:], in0=Msum[:], in1=Mt[:], op=ALU.add)
    ramp = P.sb(pf + "ramp", [128, 72]); load_bcast(P, ramp[:], pf + "ramp", ramp_d[0:1, :])
    bst = P.sb(pf + "bst", [128, 104]); load_bcast(P, bst[:], pf + "bst", bst_d[0:1, :])
    pidx = P.sb(pf + "pidx", [128, 1]); P.d(pidx[:], pidx_d[:, :], writes=[pf + "pidx"])
    q = P.sb(pf + "q", [128, 8, 32])
    QK = pf + "q"
    big = P.sb(pf + "big", [128, 104 * 32])
    ones32 = P.sb(pf + "ones32", [128, 32]); P.i("dve", "memset", writes=[pf + "ones32"], ap=ones32[:], constant=1.0)
    pw, pwk = pw_r.next()
    P.i("pe", "matmul", reads=[pf + "Msum", pf + "stri"], writes=[pwk], out=pw[:, 0:32], lhsT=stri[:, 1, :], rhs=Msum[:], start=True, stop=True)
    P.i("dve", "tensor_copy", reads=[pwk], writes=[QK], out=q[:, 0, :], in_=pw[:, 0:32])
    NM = 2 * ntf
    b3 = big[:, 0:32 * NM].rearrange("p (e m) -> p e m", e=32)
    P.i("dve", "tensor_tensor", reads=[QK, pf + "ramp"], writes=[pf + "big"], out=b3, in0=q[:, 0, :].unsqueeze(2).to_broadcast([128, 32, NM]),
        in1=ramp[:, 0:NM].unsqueeze(1).to_broadcast([128, 32, NM]), op=ALU.is_ge)
    P.i("dve", "tensor_reduce", reads=[pf + "big"], writes=[QK], out=q[:, 1, :], in_=b3, axis=AX.X, op=ALU.add)
    P.i("dve", "tensor_scalar", reads=[QK], writes=[QK], out=q[:, 1, :], in0=q[:, 1, :], scalar1=128.0, scalar2=None, op0=ALU.mult)
    P.i("dve", "tensor_tensor_scan", reads=[QK, pf + "ones32"], writes=[QK], out=q[:, 2, :], data0=ones32[:], data1=q[:, 1, :], initial=0.0, op0=ALU.mult, op1=ALU.add)
    P.i("dve", "tensor_tensor", reads=[QK], writes=[QK], out=q[:, 3, :], in0=q[:, 2, :], in1=q[:, 1, :], op=ALU.subtract)
    be = P.sb(pf + "be", [128, 104])
    b3 = big[:, 0:NB * 32].rearrange("p (b e) -> p b e", e=32)
    P.i("dve", "tensor_tensor", reads=[QK, pf + "bst"], writes=[pf + "big"], out=b3, in0=q[:, 2, :].unsqueeze(1).to_broadcast([128, NB, 32]),
        in1=bst[:, 0:NB].unsqueeze(2).to_broadcast([128, NB, 32]), op=ALU.is_le)
    P.i("dve", "tensor_reduce", reads=[pf + "big"], writes=[pf + "be"], out=be[:, 0:NB], in_=b3, axis=AX.X, op=ALU.add)
    P.i("dve", "tensor_scalar", reads=[pf + "be"], writes=[pf + "be"], out=be[:, 0:NB], in0=be[:, 0:NB], scalar1=31.0, scalar2=None, op0=ALU.min)
    same = P.sb(pf + "same", [128, 104])
    P.i("dve", "memset", writes=[pf + "same"], ap=same[:, 0:1], constant=0.0)
    P.i("dve", "tensor_tensor", reads=[pf + "be"], writes=[pf + "same"], out=same[:, 1:NB], in0=be[:, 1:NB], in1=be[:, 0:NB - 1], op=ALU.is_equal)
    P.i("dve", "tensor_scalar", reads=[pf + "be", pf + "pidx"], writes=[pf + "be"], out=be[:, 0:NB], in0=be[:, 0:NB], scalar1=128.0, scalar2=pidx[:, 0:1], op0=ALU.mult, op1=ALU.add)
    P.i("dve", "scalar_tensor_tensor", reads=[pf + "be", pf + "same"], writes=[pf + "be"], out=be[:, 0:NB], in0=same[:, 0:NB], scalar=1048576.0, in1=be[:, 0:NB], op0=ALU.mult, op1=ALU.add)
    bei = P.sb(pf + "bei", [128, 104], I32)
    P.i("dve", "tensor_copy", reads=[pf + "be"], writes=[pf + "bei"], out=bei[:, 0:NB], in_=be[:, 0:NB])
    P.d(IDXW[:, :], bei[:, 0:NB], reads=[pf + "bei"], writes=[P.u("IDXW%d" % l)])
    dsf = P.sb(pf + "dsf", [128, ntf, 2])
    pos_r = Rot(P, pf + "pos", [128, 2, 32], n=2)
    for ti in range(ntf):
        pos, posk = pos_r.next()
        P.i("dve", "tensor_tensor", reads=[pf + "RK/%d" % ti, QK], writes=[posk + "/p"], out=pos[:, 0, :], in0=RK[:, ti, :], in1=q[:, 3, :], op=ALU.add)
        for k in range(2):
            P.i("dve", "tensor_tensor", reads=[posk + "/p", pf + "OH/%d_%d" % (ti, k)], writes=[posk + "/t"], out=pos[:, 1, :], in0=pos[:, 0, :], in1=OH[:, ti, k, :], op=ALU.mult)
            P.i("dve", "tensor_reduce", reads=[posk + "/t"], writes=[pf + "dsf/%d_%d" % (ti, k)], out=dsf[:, ti, k:k + 1], in_=pos[:, 1, :], axis=AX.X, op=ALU.add)
    dsi = P.sb(pf + "dsi", [128, ntf * 2], I32)
    P.i("dve", "tensor_copy", reads=[pf + "dsf"], writes=[pf + "dsi"], out=dsi[:], in_=dsf[:].rearrange("p t k -> p (t k)"))
    P.d(DEST[:, :], dsi[:], reads=[pf + "dsi"], writes=[P.u("DEST%d" % l)])
    P.d(WWd[:, :], WW[:].rearrange("p t k -> p (t k)"), reads=[pf + "WW"], writes=[P.u("WWd%d" % l)])
    h2_r = Rot(P, pf + "h2s", [128, D], n=3)
    ti = 0
    for (c0, cn) in chunks_:
        for i in range(cn // 128):
            t0 = c0 + i * 128
            h2, h2k = h2_r.next()
            P.d(h2[:], H2[t0:t0 + 128, :], reads=["H2_%d" % l], writes=[h2k])
            for k in range(2):
                P.dma(lambda e, h2=h2, col=ti * 2 + k: e.indirect_dma_start(out=BUF[:, :], out_offset=bass.IndirectOffsetOnAxis(ap=dsi[:, col:col + 1], axis=0), in_=h2[:], in_offset=None),
                      reads=[h2k, pf + "dsi", "BUFz%d" % l], writes=[P.u("BUF%d" % l)], q="pool")
            ti += 1


def phase_g(P, Dm, l, tok_chunks, final):
    pf = "g%d_" % l
    X1 = Dm.get("X1", [T, D])
    H2T = Dm.get("H2T", [D, T])
    WTd = Dm.get("WTd", [32, T])
    WG = Dm.get("moe_g%d" % l, [32, 128, 8, 512], kind="ExternalInput")
    WU = Dm.get("moe_u%d" % l, [32, 128, 8, 512], kind="ExternalInput")
    WD = Dm.get("moe_d%d" % l, [32, 128, 4, 1024], kind="ExternalInput")
    modrow = Dm.get("modrow%d" % l, [2, 6144])
    mk = "modrow%d" % l
    if final:
        fg_d = Dm.get("final_g", [1, D], kind="ExternalInput")
        OUT = Dm.get("out", [L, D], kind="ExternalOutput")
        fg = P.sb(pf + "fg", [128, D])
        load_bcast(P, fg[:], pf + "fg", fg_d[0:1, :])
        epsc = P.sb(pf + "eps", [128, 1])
        P.i("dve", "memset", writes=[pf + "eps"], ap=epsc[:], constant=EPS)
        junk = P.sb(pf + "junk", [128, D])
    else:
        XN = Dm.get("XN%d" % l, [T, D])
    G2g = [P.sb(pf + "g2%d" % r, [128, D]) for r in range(2)]
    for r in range(2):
        load_bcast(P, G2g[r][:], pf + "g2%d" % r, modrow[r:r + 1, 5120:6144], reads=[mk])
    hT_r = Rot(P, pf + "hT", [128, 8, 512], F32R, n=2)
    acc_r = Rot(P, pf + "acc", [128, 4, 1024], n=1)
    wg_r = Rot(P, pf + "wg", [128, 8, 512], F32R, n=2)
    wu_r = Rot(P, pf + "wu", [128, 8, 512], F32R, n=2)
    wd_r = Rot(P, pf + "wd", [128, 4, 1024], F32R, n=2)
    wb_r = Rot(P, pf + "wb", [128, 512], n=2)
    pg_r = Rot(P, pf + "pg", [128, 512], n=2, psum=True)
    pu_r = Rot(P, pf + "pu", [128, 512], n=2, psum=True)
    pd_r = Rot(P, pf + "pd", [128, 512], n=2, psum=True)
    sg_r = Rot(P, pf + "sg", [128, 512], n=2)
    hu_r = Rot(P, pf + "hu", [128, 512], n=2)
    hid_r = Rot(P, pf + "hid", [128, 4, 512], F32R, n=2)
    xt_r = Rot(P, pf + "xt", [128, D], n=2)
    st_r = Rot(P, pf + "st", [128, 8], n=2)
    for (c0, cn) in tok_chunks:
        nt = cn // 128
        hT, hTk = hT_r.next()
        P.d(hT[:, :, 0:cn], H2T[:, c0:c0 + cn].rearrange("(k p) t -> p k t", p=128), reads=["H2T"], writes=[hTk], q="pool")
        acc, acck = acc_r.next()
        for e in range(32):
            wg, wgk = wg_r.next()
            wu, wuk = wu_r.next()
            wd, wdk = wd_r.next()
            P.d(wg[:], WG[e], writes=[wgk], q="pool")
            P.d(wu[:], WU[e], writes=[wuk], q="pool")
            P.d(wd[:], WD[e], writes=[wdk], q="pool")
            wb, wbk = wb_r.next()
            P.d(wb[:, 0:cn], WTd[e:e + 1, c0:c0 + cn].to_broadcast([128, cn]), reads=["WTd"], writes=[wbk])
            hid, hidk = hid_r.next()
            for hc in range(4):
                pg, pgk = pg_r.next()
                pu, puk = pu_r.next()
                for k in range(8):
                    P.i("pe", "matmul", reads=[wgk, hTk], writes=[pgk], out=pg[:, 0:cn], lhsT=wg[:, k, hc * 128:(hc + 1) * 128], rhs=hT[:, k, 0:cn], start=(k == 0), stop=(k == 7))
                for k in range(8):
                    P.i("pe", "matmul", reads=[wuk, hTk], writes=[puk], out=pu[:, 0:cn], lhsT=wu[:, k, hc * 128:(hc + 1) * 128], rhs=hT[:, k, 0:cn], start=(k == 0), stop=(k == 7))
                sg, sgk = sg_r.next()
                P.i("act", "activation", reads=[pgk], writes=[sgk], out=sg[:, 0:cn], in_=pg[:, 0:cn], func=AF.Silu)
                hu, huk = hu_r.next()
                P.i("dve", "tensor_tensor", reads=[sgk, puk], writes=[huk], out=hu[:, 0:cn], in0=sg[:, 0:cn], in1=pu[:, 0:cn], op=ALU.mult)
                P.i("pool", "tensor_tensor", reads=[huk, wbk], writes=[hidk + "/%d" % hc], out=hid[:, hc, 0:cn], in0=hu[:, 0:cn], in1=wb[:, 0:cn], op=ALU.mult)
            hkeys = [hidk + "/%d" % hc for hc in range(4)]
            for tt in range(nt):
                for hf in range(2):
                    pd, pdk = pd_r.next()
                    for hc in range(4):
                        P.i("pe", "matmul", reads=hkeys + [wdk], writes=[pdk], out=pd[:], lhsT=hid[:, hc, tt * 128:(tt + 1) * 128], rhs=wd[:, hc, hf * 512:(hf + 1) * 512],
                            start=(hc == 0), stop=(hc == 3))
                    ak = acck + "/%d_%d" % (tt, hf)
                    if e == 0:
                        P.i("dve", "tensor_copy", reads=[pdk], writes=[ak], out=acc[:, tt, hf * 512:(hf + 1) * 512], in_=pd[:])
                    else:
                        P.i("dve", "tensor_tensor", reads=[pdk, ak], writes=[ak], out=acc[:, tt, hf * 512:(hf + 1) * 512], in0=acc[:, tt, hf * 512:(hf + 1) * 512], in1=pd[:], op=ALU.add)
        for tt in range(nt):
            t0 = c0 + tt * 128
            r = 1 if t0 < LC else 0
            aks = [acck + "/%d_%d" % (tt, hf) for hf in range(2)]
            xt, xk = xt_r.next()
            P.d(xt[:], X1[t0:t0 + 128, :], reads=["X1"], writes=[xk])
            P.i("pool", "tensor_tensor", reads=aks + [pf + "g2%d" % r], writes=aks, out=acc[:, tt, :], in0=acc[:, tt, :], in1=G2g[r][:], op=ALU.mult)
            P.i("dve", "tensor_tensor", reads=aks + [xk], writes=[xk], out=xt[:], in0=xt[:], in1=acc[:, tt, :], op=ALU.add)
            if not final:
                P.d(XN[t0:t0 + 128, :], xt[:], reads=[xk], writes=[P.u("XN%d" % l)])
            else:
                st, sk = st_r.next()
                P.i("act", "activation", reads=[xk], writes=[pf + "junk", sk], out=junk[:], in_=xt[:], func=AF.Square, accum_out=st[:, 0:1])
                P.i("act", "activation", reads=[sk, pf + "eps"], writes=[sk], out=st[:, 1:2], in_=st[:, 0:1], func=AF.Sqrt, bias=epsc[:], scale=1.0 / D)
                P.i("dve", "reciprocal", reads=[sk], writes=[sk], out=st[:, 2:3], in_=st[:, 1:2])
                P.i("dve", "scalar_tensor_tensor", reads=[xk, sk, pf + "fg"], writes=[xk], out=xt[:], in0=xt[:], scalar=st[:, 2:3], in1=fg[:], op0=ALU.mult, op1=ALU.mult)
                P.d(OUT[t0 - LC:t0 - LC + 128, :], xt[:], reads=[xk], writes=[P.u("out")], final=True)


def phase_b(P, Dm, l):
    pf = "b%d_" % l
    FMT = Dm.get("FMT", [1024, T])
    CAT = Dm.get("CAT", [1024, T])
    Bd = Dm.get("s5B%d" % l, [2, 2, 8, 128, 128], kind="ExternalInput")
    Cd = Dm.get("s5C%d" % l, [2, 2, 8, 128, 128], kind="ExternalInput")
    lam_d = Dm.get("s5lam%d" % l, [128, 3, 16], kind="ExternalInput")
    dsk_d = Dm.get("s5d%d" % l, [128, 2], kind="ExternalInput")
    gw_d = Dm.get("gluw%d" % l, [128, 2, 256], kind="ExternalInput")
    gb_d = Dm.get("glub%d" % l, [128, 2], kind="ExternalInput")
    NMAX = 512
    lam = P.sb(pf + "lam", [128, 3, 16])
    P.d(lam[:], lam_d[:, :, :], writes=[pf + "lam"])
    dsk = P.sb(pf + "dsk", [128, 2]); P.d(dsk[:], dsk_d[:, :], writes=[pf + "dsk"])
    gb = P.sb(pf + "gb", [128, 2]); P.d(gb[:], gb_d[:, :], writes=[pf + "gb"])
    gw = P.sb(pf + "gw", [128, 2, 256], F32R); P.d(gw[:], gw_d[:, :, :], writes=[pf + "gw"], q="pool")
    Bb = P.sb(pf + "Bb", [128, 16, 128], F32R)
    Cb = P.sb(pf + "Cb", [128, 16, 128], F32R)
    sc = P.sb(pf + "sc", [128, 24, 16])
    K_ = pf + "sc"
    LR, LI, LDT = lam[:, 0, :], lam[:, 1, :], lam[:, 2, :]
    (DT, RM, TH, C_, S_, T1, T2, T3, ARE, AIM, DEN, AM1, FRE, FIM, HP) = range(15)

    def S(i):
        return sc[:, i, :]

    def tt(o, a, b, op, eng="dve"):
        P.i(eng, "tensor_tensor", reads=[K_, pf + "lam"], writes=[K_], out=o, in0=a, in1=b, op=op)

    hpi = P.sb(pf + "hpi", [128, 1])
    P.i("dve", "memset", writes=[pf + "hpi"], ap=hpi[:], constant=float(np.pi / 2))
    P.i("act", "activation", reads=[pf + "lam"], writes=[K_], out=S(DT), in_=LDT, func=AF.Exp)
    tt(S(RM), LR, S(DT), ALU.mult)
    P.i("act", "activation", reads=[K_], writes=[K_], out=S(RM), in_=S(RM), func=AF.Exp)
    tt(S(TH), LI, S(DT), ALU.mult)
    P.i("act", "activation", reads=[K_], writes=[K_], out=S(S_), in_=S(TH), func=AF.Sin, scale=1.0 / 32)
    P.i("act", "activation", reads=[K_, pf + "hpi"], writes=[K_], out=S(C_), in_=S(TH), func=AF.Sin, scale=1.0 / 32, bias=hpi[:])
    for _ in range(5):
        tt(S(T1), S(C_), S(C_), ALU.mult)
        tt(S(T2), S(S_), S(S_), ALU.mult)
        tt(S(T3), S(C_), S(S_), ALU.mult)
        tt(S(C_), S(T1), S(T2), ALU.subtract)
        tt(S(S_), S(T3), S(T3), ALU.add)
    tt(S(ARE), S(RM), S(C_), ALU.mult)
    tt(S(AIM), S(RM), S(S_), ALU.mult)
    tt(S(T1), LR, LR, ALU.mult)
    tt(S(T2), LI, LI, ALU.mult)
    tt(S(DEN), S(T1), S(T2), ALU.add)
    P.i("dve", "reciprocal", reads=[K_], writes=[K_], out=S(DEN), in_=S(DEN))
    P.i("dve", "tensor_scalar", reads=[K_], writes=[K_], out=S(AM1), in0=S(ARE), scalar1=-1.0, scalar2=None, op0=ALU.add)
    tt(S(T1), S(AM1), LR, ALU.mult)
    tt(S(T2), S(AIM), LI, ALU.mult)
    tt(S(T1), S(T1), S(T2), ALU.add)
    tt(S(FRE), S(T1), S(DEN), ALU.mult)
    tt(S(T1), S(AIM), LR, ALU.mult)
    tt(S(T2), S(AM1), LI, ALU.mult)
    tt(S(T1), S(T1), S(T2), ALU.subtract)
    tt(S(FIM), S(T1), S(DEN), ALU.mult)
    Ep = P.sb(pf + "Ep", [128, 2, 8, NMAX])
    Tm = P.sb(pf + "Tm", [128, 2, 8, NMAX])
    pw = P.sb(pf + "pw", [128, 4, 8])
    tb = P.sb(pf + "tb", [128, 2, 8, NMAX // 2])
    carry = P.sb(pf + "carry", [128, 16, 2])
    P.i("dve", "memset", writes=[pf + "carry"], ap=carry[:], constant=0.0)
    yacc = P.sb(pf + "yacc", [128, 2, T])
    uc_r = Rot(P, pf + "uc", [128, 2, NMAX], F32R, n=2)
    pb_r = Rot(P, pf + "pb", [128, 512], n=4, psum=True)
    py_r = Rot(P, pf + "py", [128, 512], n=2, psum=True)
    br_r = Rot(P, pf + "br", [128, 2, NMAX], n=2)
    t_r = Rot(P, pf + "t", [128, 4, NMAX], n=2)
    v_r = Rot(P, pf + "v", [128, 2, NMAX], n=2)
    g_r = Rot(P, pf + "g", [128, 2, NMAX], n=2)
    h_r = Rot(P, pf + "h", [128, 2, NMAX], F32R, n=2)
    TK, EK = pf + "Tm", pf + "Ep"
    for d in range(2):
        dsl = slice(d * 8, d * 8 + 8)
        P.d(Bb[:], Bd[d].rearrange("c j k m -> k (c j) m"), writes=[pf + "Bb"], q="pool")
        P.d(Cb[:], Cd[d].rearrange("c j k m -> k (c j) m"), writes=[pf + "Cb"], q="pool")
        P.i("act", "activation", reads=[pf + "Cb"], writes=[pf + "Cb"], out=Cb[:, 8:16, :], in_=Cb[:, 8:16, :].bitcast(F32), func=AF.Copy, scale=-1.0)
        P.i("dve", "tensor_copy", reads=[K_], writes=[EK], out=Ep[:, 0, :, 0:1], in_=S(C_)[:, dsl].unsqueeze(2))
        P.i("dve", "tensor_copy", reads=[K_], writes=[EK], out=Ep[:, 1, :, 0:1], in_=S(S_)[:, dsl].unsqueeze(2))
        P.i("dve", "tensor_copy", reads=[K_], writes=[pf + "pw"], out=pw[:, 0, :], in_=S(C_)[:, dsl])
        P.i("dve", "tensor_copy", reads=[K_], writes=[pf + "pw"], out=pw[:, 1, :], in_=S(S_)[:, dsl])
        n = 1
        while n < NMAX:
            cn_b = pw[:, 0, :].unsqueeze(2).to_broadcast([128, 8, n])
            sn_b = pw[:, 1, :].unsqueeze(2).to_broadcast([128, 8, n])
            ire, iim = Ep[:, 0, :, 0:n], Ep[:, 1, :, 0:n]
            ore, oim = Ep[:, 0, :, n:2 * n], Ep[:, 1, :, n:2 * n]
            P.i("dve", "tensor_tensor", reads=[EK, pf + "pw"], writes=[pf + "tb"], out=tb[:, 0, :, 0:n], in0=iim, in1=sn_b, op=ALU.mult)
            P.i("pool", "tensor_tensor", reads=[EK, pf + "pw"], writes=[pf + "tb2"], out=tb[:, 1, :, 0:n], in0=iim, in1=cn_b, op=ALU.mult)
            P.i("dve", "tensor_tensor", reads=[EK, pf + "pw"], writes=[EK + "/a"], out=ore, in0=ire, in1=cn_b, op=ALU.mult)
            P.i("pool", "tensor_tensor", reads=[EK, pf + "pw"], writes=[EK + "/b"], out=oim, in0=ire, in1=sn_b, op=ALU.mult)
            P.i("dve", "tensor_tensor", reads=[EK + "/a", pf + "tb"], writes=[EK + "/a"], out=ore, in0=ore, in1=tb[:, 0, :, 0:n], op=ALU.subtract)
            P.i("pool", "tensor_tensor", reads=[EK + "/b", pf + "tb2"], writes=[EK + "/b"], out=oim, in0=oim, in1=tb[:, 1, :, 0:n], op=ALU.add)
            P.i("dve", "tensor_tensor", reads=[pf + "pw"], writes=[pf + "pw"], out=pw[:, 2, :], in0=pw[:, 0, :], in1=pw[:, 1, :], op=ALU.mult)
            P.i("dve", "tensor_tensor", reads=[pf + "pw"], writes=[pf + "pw"], out=pw[:, 0, :], in0=pw[:, 0, :], in1=pw[:, 0, :], op=ALU.mult)
            P.i("dve", "tensor_tensor", reads=[pf + "pw"], writes=[pf + "pw"], out=pw[:, 3, :], in0=pw[:, 1, :], in1=pw[:, 1, :], op=ALU.mult)
            P.i("dve", "tensor_tensor", reads=[pf + "pw"], writes=[pf + "pw"], out=pw[:, 0, :], in0=pw[:, 0, :], in1=pw[:, 3, :], op=ALU.subtract)
            P.i("dve", "tensor_tensor", reads=[pf + "pw"], writes=[pf + "pw"], out=pw[:, 1, :], in0=pw[:, 2, :], in1=pw[:, 2, :], op=ALU.add)
            n *= 2
        for j in range(8):
            fr = S(FRE)[:, d * 8 + j:d * 8 + j + 1]
            fi = S(FIM)[:, d * 8 + j:d * 8 + j + 1]
            t, tk = t_r.next()
            P.i("dve", "tensor_scalar", reads=[EK, K_], writes=[tk + "/0"], out=t[:, 0, :], in0=Ep[:, 1, j, :], scalar1=fi, scalar2=None, op0=ALU.mult)
            P.i("dve", "scalar_tensor_tensor", reads=[EK, K_, tk + "/0"], writes=[TK + "/a%d" % j], out=Tm[:, 0, j, :], in0=Ep[:, 0, j, :], scalar=fr, in1=t[:, 0, :], op0=ALU.mult, op1=ALU.add)
            P.i("dve", "tensor_scalar", reads=[EK, K_], writes=[tk + "/1"], out=t[:, 1, :], in0=Ep[:, 1, j, :], scalar1=fr, scalar2=None, op0=ALU.mult)
            P.i("dve", "scalar_tensor_tensor", reads=[EK, K_, tk + "/1"], writes=[TK + "/b%d" % j], out=Tm[:, 1, j, :], in0=Ep[:, 0, j, :], scalar=fi, in1=t[:, 1, :], op0=ALU.mult, op1=ALU.subtract)
        order = CHUNKS if d == 0 else [CHUNKS[0]] + CHUNKS[:0:-1]
        rev = (d == 1)

        def R(ap):
            return ap[:, ::-1] if rev else ap

        for (c0, N) in order:
            uc, uck = uc_r.next()
            P.d(uc[:, :, 0:N], FMT[0:256, c0:c0 + N].rearrange("(oc p) t -> p oc t", p=128), reads=["FMT"], writes=[uck], q="pool")
            pys = [py_r.next() for _ in range(2)]
            for j in range(8):
                oc = j // 4
                dj = d * 8 + j
                pbr, pbrk = pb_r.next()
                pbi, pbik = pb_r.next()
                P.i("pe", "matmul", reads=[pf + "Bb", uck], writes=[pbrk], out=pbr[:, 0:N], lhsT=Bb[:, j, :], rhs=uc[:, oc, 0:N], start=True, stop=True)
                P.i("pe", "matmul", reads=[pf + "Bb", uck], writes=[pbik], out=pbi[:, 0:N], lhsT=Bb[:, 8 + j, :], rhs=uc[:, oc, 0:N], start=True, stop=True)
                br, brk = br_r.next()
                P.i("act", "activation", reads=[pbrk], writes=[brk + "/0"], out=br[:, 0, 0:N], in_=R(pbr[:, 0:N]), func=AF.Copy)
                P.i("act", "activation", reads=[pbik], writes=[brk + "/1"], out=br[:, 1, 0:N], in_=R(pbi[:, 0:N]), func=AF.Copy)
                t, tk = t_r.next()
                v, vk = v_r.next()
                b0, b1 = br[:, 0, 0:N], br[:, 1, 0:N]
                tmr, tmi = Tm[:, 0, j, 0:N], Tm[:, 1, j, 0:N]
                P.i("dve", "tensor_tensor", reads=[brk + "/0", TK], writes=[tk + "/0"], out=t[:, 0, 0:N], in0=tmr, in1=b0, op=ALU.mult)
                P.i(S5_ENG2, "tensor_tensor", reads=[brk + "/1", TK], writes=[tk + "/1"], out=t[:, 1, 0:N], in0=tmi, in1=b1, op=ALU.mult)
                P.i(S5_ENG2, "tensor_tensor", reads=[brk + "/1", TK], writes=[tk + "/2"], out=t[:, 2, 0:N], in0=tmr, in1=b1, op=ALU.mult)
                P.i(S5_ENG2, "tensor_tensor", reads=[brk + "/0", TK], writes=[tk + "/3"], out=t[:, 3, 0:N], in0=tmi, in1=b0, op=ALU.mult)
                P.i("dve", "tensor_tensor", reads=[tk + "/0", tk + "/1"], writes=[vk + "/0"], out=v[:, 0, 0:N], in0=t[:, 0, 0:N], in1=t[:, 1, 0:N], op=ALU.subtract)
                P.i("dve", "tensor_tensor", reads=[tk + "/2", tk + "/3"], writes=[vk + "/1"], out=v[:, 1, 0:N], in0=t[:, 2, 0:N], in1=t[:, 3, 0:N], op=ALU.add)
                g, gk = g_r.next()
                for c in range(2):
                    P.i("dve", "tensor_tensor_scan", reads=[vk + "/%d" % c, K_, pf + "carry"], writes=[gk + "/%d" % c], out=g[:, c, 0:N], data0=S(RM)[:, dj:dj + 1].to_broadcast([128, N]), data1=v[:, c, 0:N],
                        initial=carry[:, dj, c:c + 1], op0=ALU.mult, op1=ALU.add)
                h, hk = h_r.next()
                t, tk = t_r.next()
                epr, epi = Ep[:, 0, j, 0:N], Ep[:, 1, j, 0:N]
                g0, g1 = g[:, 0, 0:N], g[:, 1, 0:N]
                P.i("dve", "tensor_tensor", reads=[gk + "/0", EK], writes=[tk + "/0"], out=t[:, 0, 0:N], in0=epr, in1=g0, op=ALU.mult)
                P.i(S5_ENG2, "tensor_tensor", reads=[gk + "/1", EK], writes=[tk + "/1"], out=t[:, 1, 0:N], in0=epi, in1=g1, op=ALU.mult)
                P.i(S5_ENG2, "tensor_tensor", reads=[gk + "/1", EK], writes=[tk + "/2"], out=t[:, 2, 0:N], in0=epr, in1=g1, op=ALU.mult)
                P.i("dve", "tensor_tensor", reads=[gk + "/0", EK], writes=[tk + "/3"], out=t[:, 3, 0:N], in0=epi, in1=g0, op=ALU.mult)
                P.i("dve", "tensor_tensor", reads=[tk + "/0", tk + "/1"], writes=[hk + "/0"], out=h[:, 0, 0:N], in0=t[:, 0, 0:N], in1=t[:, 1, 0:N], op=ALU.subtract)
                P.i("dve", "tensor_tensor", reads=[tk + "/2", tk + "/3"], writes=[hk + "/1"], out=h[:, 1, 0:N], in0=t[:, 2, 0:N], in1=t[:, 3, 0:N], op=ALU.add)
                P.i("act", "activation", reads=[hk], writes=[pf + "carry"], out=carry[:, dj, :], in_=h[:].bitcast(F32)[:, :, N - 1], func=AF.Copy)
                py, pyk = pys[oc]
                P.i("pe", "matmul", reads=[pf + "Cb", hk + "/0"], writes=[pyk], out=py[:, 0:N], lhsT=Cb[:, j, :], rhs=h[:, 0, 0:N], start=(j % 4 == 0), stop=False)
                P.i("pe", "matmul", reads=[pf + "Cb", hk + "/1"], writes=[pyk], out=py[:, 0:N], lhsT=Cb[:, 8 + j, :], rhs=h[:, 1, 0:N], start=False, stop=(j % 4 == 3))
            for oc in range(2):
                py, pyk = pys[oc]
                ya = yacc[:, oc, c0:c0 + N]
                yk = pf + "yacc/%d_%d" % (oc, c0)
                if d == 0:
                    P.i("dve", "scalar_tensor_tensor", reads=[uck, pf + "dsk", pyk], writes=[yk], out=ya, in0=uc[:, oc, 0:N].bitcast(F32), scalar=dsk[:, oc:oc + 1], in1=py[:, 0:N], op0=ALU.mult, op1=ALU.add)
                else:
                    P.i("dve", "tensor_tensor", reads=[yk, pyk], writes=[yk], out=ya, in0=ya, in1=R(py[:, 0:N]), op=ALU.add)
    e_r = Rot(P, pf + "e", [128, 3, NMAX], n=1)
    gT_r = Rot(P, pf + "gT", [128, 2, NMAX], F32R, n=1)
    pq_r = Rot(P, pf + "pq", [128, 512], n=2, psum=True)
    a_r = Rot(P, pf + "a", [128, NMAX], n=2)
    for (c0, N) in CHUNKS:
        gT, gTk = gT_r.next()
        for oc in range(2):
            y = yacc[:, oc, c0:c0 + N]
            yk = pf + "yacc/%d_%d" % (oc, c0)
            ee, ek = e_r.next()
            P.i("pool", "tensor_tensor", reads=[yk], writes=[ek + "/0"], out=ee[:, 0, 0:N], in0=y, in1=y, op=ALU.mult)
            P.i("dve", "tensor_scalar", reads=[ek + "/0"], writes=[ek + "/0"], out=ee[:, 0, 0:N], in0=ee[:, 0, 0:N], scalar1=0.044715, scalar2=1.0, op0=ALU.mult, op1=ALU.add)
            P.i("pool", "tensor_tensor", reads=[ek + "/0", yk], writes=[ek + "/1"], out=ee[:, 1, 0:N], in0=ee[:, 0, 0:N], in1=y, op=ALU.mult)
            P.i("act", "activation", reads=[ek + "/1"], writes=[ek + "/2"], out=ee[:, 2, 0:N], in_=ee[:, 1, 0:N], func=AF.Sigmoid, scale=1.5957691216057308)
            P.i("dve", "tensor_tensor", reads=[ek + "/2", yk], writes=[gTk + "/%d" % oc], out=gT[:, oc, 0:N], in0=ee[:, 2, 0:N], in1=y, op=ALU.mult)
        for oc2 in range(2):
            pq, pqk = pq_r.next()
            for oc in range(2):
                P.i("pe", "matmul", reads=[gTk, pf + "gw"], writes=[pqk], out=pq[:, 0:N], lhsT=gw[:, oc, oc2 * 128:(oc2 + 1) * 128], rhs=gT[:, oc, 0:N], start=(oc == 0), stop=(oc == 1))
            a, ak = a_r.next()
            P.i("act", "activation", reads=[pqk, pf + "gb"], writes=[ak], out=a[:, 0:N], in_=pq[:, 0:N], func=AF.Sigmoid, bias=gb[:, oc2:oc2 + 1])
            P.i("dve", "tensor_tensor", reads=[ak, gTk], writes=[ak], out=a[:, 0:N], in0=a[:, 0:N], in1=gT[:, oc2, 0:N].bitcast(F32), op=ALU.mult)
            P.d(CAT[oc2 * 128:(oc2 + 1) * 128, c0:c0 + N], a[:, 0:N], reads=[ak], writes=[P.u("CAT")])


def phase_d1(P, Dm, l):
    pf = "d%d_" % l
    FMT = Dm.get("FMT", [1024, T])
    XBCT = Dm.get("XBCT", [768, T])
    XBtok = Dm.get("XBtok", [T, 512])
    cw_d = Dm.get("convw%d" % l, [128, 6, 4], kind="ExternalInput")
    ident_d = Dm.get("ident", [128, 128], kind="ExternalInput")
    ident = P.sb(pf + "ident", [128, 128]); P.d(ident[:], ident_d[:, :], writes=[pf + "ident"])
    cw = P.sb(pf + "cw", [128, 6, 4]); P.d(cw[:], cw_d[:, :, :], writes=[pf + "cw"])
    xi_r = Rot(P, pf + "xi", [128, 6, 514], n=2)
    ac_r = Rot(P, pf + "ac", [128, 512], n=2)
    co_r = Rot(P, pf + "co", [128, 6, 512], n=2)
    tp_r = Rot(P, pf + "tp", [128, 512], n=2, psum=True)
    tk_r = Rot(P, pf + "tk", [128, 512], n=2)
    for (c0, N) in CHUNKS:
        xi, xik = xi_r.next()
        lz = c0 in (0, LC)
        rz = (c0 + N) in (LC, T)
        lo = c0 - (0 if lz else 1)
        hi = c0 + N + (0 if rz else 1)
        if lz:
            P.i("dve", "memset", writes=[xik + "/l"], ap=xi[:, :, 0:1], constant=0.0)
        if rz:
            P.i("dve", "memset", writes=[xik + "/r"], ap=xi[:, :, N + 1:N + 2], constant=0.0)
        P.d(xi[:, :, (1 if lz else 0):(1 if lz else 0) + hi - lo], FMT[256:1024, lo:hi].rearrange("(r p) t -> p r t", p=128), reads=["FMT"], writes=[xik + "/m"])
        co, cok = co_r.next()
        for rc in range(6):
            ac, ack = ac_r.next()
            P.i("dve", "tensor_scalar", reads=[xik, pf + "cw"], writes=[ack], out=ac[:, 0:N], in0=xi[:, rc, 1:N + 1], scalar1=cw[:, rc, 1:2], scalar2=None, op0=ALU.mult)
            P.i("dve", "scalar_tensor_tensor", reads=[xik, pf + "cw", ack], writes=[ack], out=ac[:, 0:N], in0=xi[:, rc, 0:N], scalar=cw[:, rc, 0:1], in1=ac[:, 0:N], op0=ALU.mult, op1=ALU.add)
            P.i("dve", "scalar_tensor_tensor", reads=[xik, pf + "cw", ack], writes=[ack], out=ac[:, 0:N], in0=xi[:, rc, 2:N + 2], scalar=cw[:, rc, 2:3], in1=ac[:, 0:N], op0=ALU.mult, op1=ALU.add)
            P.i("act", "activation", reads=[ack, pf + "cw"], writes=[cok + "/%d" % rc], out=co[:, rc, 0:N], in_=ac[:, 0:N], func=AF.Silu, bias=cw[:, rc, 3:4])
        P.d(XBCT[:, c0:c0 + N].rearrange("(r p) t -> p r t", p=128), co[:, :, 0:N], reads=[cok], writes=[P.u("XBCT")])
        for i in range(N // 128):
            tp, tpk = tp_r.next()
            for rc in range(4):
                P.i("pe", "transpose", reads=[cok + "/%d" % rc, pf + "ident"], writes=[tpk], out=tp[:, rc * 128:(rc + 1) * 128], in_=co[:, rc, i * 128:(i + 1) * 128], identity=ident[:])
            tk, tkk = tk_r.next()
            P.i("act", "activation", reads=[tpk], writes=[tkk], out=tk[:], in_=tp[:], func=AF.Copy)
            P.d(XBtok[c0 + i * 128:c0 + (i + 1) * 128, :], tk[:], reads=[tkk], writes=[P.u("XBtok")])


def phase_d2(P, Dm, l):
    pf = "D%d_" % l
    XBCT = Dm.get("XBCT", [768, T])
    XBtok = Dm.get("XBtok", [T, 512])
    TMS = Dm.get("TMS", [T, 520])
    CAT = Dm.get("CAT", [1024, T])
    ident_d = Dm.get("ident", [128, 128], kind="ExternalInput")
    tri_d = Dm.get("tri", [128, 2, 128], kind="ExternalInput")
    alog_d = Dm.get("alog%d" % l, [1, 8], kind="ExternalInput")
    dsk_d = Dm.get("ssdd%d" % l, [1, 4], kind="ExternalInput")
    ng_d = Dm.get("ssdng%d" % l, [1, 256], kind="ExternalInput")
    ident = P.sb(pf + "ident", [128, 128]); P.d(ident[:], ident_d[:, :], writes=[pf + "ident"])
    tri = P.sb(pf + "tri", [128, 2, 128]); P.d(tri[:], tri_d[:, :, :], writes=[pf + "tri"])
    A = P.sb(pf + "A", [128, 8]); load_bcast(P, A[:], pf + "A", alog_d[0:1, :])
    P.i("act", "activation", reads=[pf + "A"], writes=[pf + "A"], out=A[:], in_=A[:], func=AF.Exp)
    P.i("dve", "tensor_scalar", reads=[pf + "A"], writes=[pf + "A"], out=A[:], in0=A[:], scalar1=-1.0, scalar2=None, op0=ALU.mult)
    dsk = P.sb(pf + "dsk", [128, 4]); load_bcast(P, dsk[:], pf + "dsk", dsk_d[0:1, :])
    ng = P.sb(pf + "ng", [128, 256]); load_bcast(P, ng[:], pf + "ng", ng_d[0:1, :])
    epsc = P.sb(pf + "eps", [128, 1]); P.i("dve", "memset", writes=[pf + "eps"], ap=epsc[:], constant=EPS)
    Yacc = P.sb(pf + "Yacc", [128, NT, 256])
    Sst = P.sb(pf + "S", [128, 4, 64], F32R)
    bc_r = Rot(P, pf + "bc", [128, 4, 128], F32R, n=2)
    xb_r = Rot(P, pf + "xb", [128, 512], n=2)
    xbr_r = Rot(P, pf + "xbr", [128, 256], F32R, n=2)
    dt_r = Rot(P, pf + "dt", [128, 8], n=2)
    sm_r = Rot(P, pf + "sm", [128, 64], n=2)
    abc_r = Rot(P, pf + "abc", [128, 4, 128], n=2)
    X_r = Rot(P, pf + "X", [128, 4, 64], F32R, n=2)
    Xd_r = Rot(P, pf + "Xd", [128, 4, 64], F32R, n=2)
    pG_r = Rot(P, pf + "pG", [128, 512], n=1, psum=True)
    pR_r = Rot(P, pf + "pR", [128, 512], n=1, psum=True)
    pC_r = Rot(P, pf + "pC", [128, 512], n=1, psum=True)
    pY_r = Rot(P, pf + "pY", [128, 512], n=2, psum=True)
    pS_r = Rot(P, pf + "pS", [128, 512], n=1, psum=True)
    Gm_r = Rot(P, pf + "Gm", [128, 2, 128], n=2)
    df_r = Rot(P, pf + "df", [128, 128], n=2)
    sc_r = Rot(P, pf + "scT", [128, 128], F32R, n=2)
    yo_r = Rot(P, pf + "yo", [128, 64], n=2)
    for d in range(2):
        order = list(range(NT)) if d == 0 else [1, 0] + list(range(NT - 1, 1, -1))
        P.i("dve", "memset", writes=[pf + "S"], ap=Sst[:].bitcast(F32), constant=0.0)
        for ci, c in enumerate(order):
            t0 = c * 128
            bc, bck = bc_r.next()
            P.d(bc[:], XBCT[256:768, t0:t0 + 128].rearrange("(r p) t -> p r t", p=128), reads=["XBCT"], writes=[bck], q="pool")
            xb, xbk = xb_r.next()
            P.d(xb[:], XBtok[t0:t0 + 128, :], reads=["XBtok"], writes=[xbk])
            xbr, xbrk = xbr_r.next()
            P.i("act", "activation", reads=[xbk], writes=[xbrk], out=xbr[:], in_=xb[:, 256:512], func=AF.Copy)
            dt, dtk = dt_r.next()
            P.d(dt[:], TMS[t0:t0 + 128, 512:520], reads=["TMS"], writes=[dtk])
            sm, smk = sm_r.next()
            P.i("dve", "tensor_tensor", reads=[dtk, pf + "A"], writes=[smk], out=sm[:, 0:4], in0=dt[:, d * 4:d * 4 + 4], in1=A[:, d * 4:d * 4 + 4], op=ALU.mult)
            abc, abck = abc_r.next()
            P.i("dve", "tensor_copy", reads=[smk], writes=[abck], out=abc[:], in_=sm[:, 0:4].unsqueeze(2).to_broadcast([128, 4, 128]))
            X, Xk = X_r.next()
            P.i("pool", "tensor_tensor", reads=[xbk, dtk], writes=[Xk], out=X[:], in0=xb[:, 0:256].rearrange("p (h e) -> p h e", h=4),
                in1=dt[:, d * 4:d * 4 + 4].unsqueeze(2).to_broadcast([128, 4, 64]), op=ALU.mult)
            pG, pGk = pG_r.next()
            for g in range(2):
                P.i("pe", "matmul", reads=[bck], writes=[pGk], out=pG[:, g * 128:(g + 1) * 128], lhsT=bc[:, g, :], rhs=bc[:, 2 + g, :], start=True, stop=True)
            Gm, Gmk = Gm_r.next()
            P.i("dve", "tensor_tensor", reads=[pGk, pf + "tri"], writes=[Gmk], out=Gm[:], in0=pG[:, 0:256].rearrange("p (g l) -> p g l", g=2),
                in1=tri[:, d, :].unsqueeze(1).to_broadcast([128, 2, 128]), op=ALU.mult)
            pC, pCk = pC_r.next()
            P.i("pe", "matmul", reads=[smk, pf + "tri"], writes=[pCk], out=pC[:, 0:4], lhsT=tri[:, d, :], rhs=sm[:, 0:4], start=True, stop=True)
            pR, pRk = pR_r.next()
            for h in range(4):
                P.i("pe", "matmul", reads=[abck, pf + "tri"], writes=[pRk], out=pR[:, h * 128:(h + 1) * 128], lhsT=abc[:, h, :], rhs=tri[:, d, :], start=True, stop=True)
            P.i("dve", "tensor_copy", reads=[pCk], writes=[smk], out=sm[:, 4:8], in_=pC[:, 0:4])
            P.i("act", "activation", reads=[smk], writes=[smk], out=sm[:, 8:12], in_=sm[:, 4:8], func=AF.Exp)
            last = 127 if d == 0 else 0
            P.i("dve", "tensor_copy", reads=[pRk], writes=[smk], out=sm[:, 20:24], in_=pR[:].rearrange("p (h l) -> p h l", h=4)[:, :, last])
            P.i("dve", "tensor_tensor", reads=[smk], writes=[smk], out=sm[:, 12:16], in0=sm[:, 20:24], in1=sm[:, 4:8], op=ALU.subtract)
            P.i("act", "activation", reads=[smk], writes=[smk], out=sm[:, 12:16], in_=sm[:, 12:16], func=AF.Exp)
            P.i("act", "activation", reads=[smk], writes=[smk], out=sm[:, 16:20], in_=sm[:, 20:24], func=AF.Exp)
            Xd, Xdk = Xd_r.next()
            P.i("pool", "tensor_tensor", reads=[Xk, smk], writes=[Xdk], out=Xd[:], in0=X[:].bitcast(F32), in1=sm[:, 12:16].unsqueeze(2).to_broadcast([128, 4, 64]), op=ALU.mult)
            pY, pYk = pY_r.next()
            pS, pSk = pS_r.next()
            for h in range(4):
                g = h // 2
                df, dfk = df_r.next()
                P.i("dve", "tensor_scalar", reads=[pRk, smk], writes=[dfk], out=df[:], in0=pR[:, h * 128:(h + 1) * 128], scalar1=sm[:, 4 + h:5 + h], scalar2=0.0, op0=ALU.subtract, op1=ALU.min)
                P.i("act", "activation", reads=[dfk], writes=[dfk], out=df[:], in_=df[:], func=AF.Exp)
                scT, scTk = sc_r.next()
                P.i("pool", "tensor_tensor", reads=[dfk, Gmk], writes=[scTk], out=scT[:], in0=df[:], in1=Gm[:, g, :], op=ALU.mult)
                P.i("pe", "matmul", reads=[scTk, Xk], writes=[pYk], out=pY[:, h * 128:h * 128 + 64], lhsT=scT[:], rhs=X[:, h, :], start=True, stop=True)
                P.i("pe", "matmul", reads=[bck, pf + "S"], writes=[pYk], out=pY[:, h * 128 + 64:h * 128 + 128], lhsT=bc[:, 2 + g, :], rhs=Sst[:, h, :], start=True, stop=True)
                P.i("pe", "matmul", reads=[xbrk, Xdk], writes=[pSk], out=pS[:, h * 64:(h + 1) * 64], lhsT=xbr[:, g * 128:(g + 1) * 128], rhs=Xd[:, h, :], start=True, stop=True)
            for h in range(4):
                yo, yok = yo_r.next()
                P.i("act", "activation", reads=[pYk, smk], writes=[yok], out=yo[:], in_=pY[:, h * 128 + 64:h * 128 + 128], func=AF.Copy, scale=sm[:, 8 + h:9 + h])
                ya = Yacc[:, c, h * 64:(h + 1) * 64]
                yk = pf + "Yacc/%d_%d" % (c, h)
                if d == 0:
                    P.i("dve", "tensor_tensor", reads=[pYk, yok], writes=[yk], out=ya, in0=pY[:, h * 128:h * 128 + 64], in1=yo[:], op=ALU.add)
                else:
                    P.i("dve", "tensor_tensor", reads=[pYk, yok], writes=[yok], out=yo[:], in0=pY[:, h * 128:h * 128 + 64], in1=yo[:], op=ALU.add)
                    P.i("pool", "tensor_tensor", reads=[yk, yok], writes=[yk], out=ya, in0=ya, in1=yo[:], op=ALU.add)
                P.i("dve", "scalar_tensor_tensor", reads=[pf + "S", smk, pSk], writes=[pf + "S"], out=Sst[:, h, :], in0=Sst[:, h, :].bitcast(F32), scalar=sm[:, 16 + h:17 + h],
                    in1=pS[:, h * 64:(h + 1) * 64], op0=ALU.mult, op1=ALU.add)
    z_r = Rot(P, pf + "z", [128, 256], n=2)
    yt_r = Rot(P, pf + "yt", [128, 256], n=2)
    jk = P.sb(pf + "jk", [128, 256])
    pT_r = Rot(P, pf + "pT", [128, 512], n=2, psum=True)
    oT_r = Rot(P, pf + "oT", [128, 2, 128], n=2)
    for c in range(NT):
        t0 = c * 128
        xb, xbk = xb_r.next()
        P.d(xb[:], XBtok[t0:t0 + 128, :], reads=["XBtok"], writes=[xbk])
        z, zk = z_r.next()
        P.d(z[:], TMS[t0:t0 + 128, 256:512], reads=["TMS"], writes=[zk])
        yt, ytk = yt_r.next()
        P.i("pool", "tensor_tensor", reads=[xbk, pf + "dsk"], writes=[ytk], out=yt[:].rearrange("p (h e) -> p h e", h=4), in0=xb[:, 0:256].rearrange("p (h e) -> p h e", h=4),
            in1=dsk[:].unsqueeze(2).to_broadcast([128, 4, 64]), op=ALU.mult)
        P.i("dve", "tensor_tensor", reads=[ytk, pf + "Yacc/%d" % c], writes=[ytk], out=yt[:], in0=yt[:], in1=Yacc[:, c, :], op=ALU.add)
        P.i("act", "activation", reads=[zk], writes=[zk], out=z[:], in_=z[:], func=AF.Silu)
        P.i("dve", "tensor_tensor", reads=[ytk, zk], writes=[ytk], out=yt[:], in0=yt[:], in1=z[:], op=ALU.mult)
        sm, smk = sm_r.next()
        P.i("act", "activation", reads=[ytk], writes=[pf + "jk", smk], out=jk[:], in_=yt[:], func=AF.Square, accum_out=sm[:, 0:1])
        P.i("act", "activation", reads=[smk, pf + "eps"], writes=[smk], out=sm[:, 1:2], in_=sm[:, 0:1], func=AF.Sqrt, bias=epsc[:], scale=1.0 / 256)
        P.i("dve", "reciprocal", reads=[smk], writes=[smk], out=sm[:, 2:3], in_=sm[:, 1:2])
        P.i("dve", "scalar_tensor_tensor", reads=[ytk, smk, pf + "ng"], writes=[ytk], out=yt[:], in0=yt[:], scalar=sm[:, 2:3], in1=ng[:], op0=ALU.mult, op1=ALU.mult)
        pT, pTk = pT_r.next()
        for q in range(2):
            P.i("pe", "transpose", reads=[ytk, pf + "ident"], writes=[pTk], out=pT[:, q * 128:(q + 1) * 128], in_=yt[:, q * 128:(q + 1) * 128], identity=ident[:])
        oT, oTk = oT_r.next()
        P.i("act", "activation", reads=[pTk], writes=[oTk], out=oT[:], in_=pT[:, 0:256].rearrange("p (q t) -> p q t", q=2), func=AF.Copy)
        P.d(CAT[512:768, t0:t0 + 128].rearrange("(q p) t -> p q t", p=128), oT[:], reads=[oTk], writes=[P.u("CAT")])


LAT_CHUNKS = [(256 + 512 * i, 512) for i in range(8)]
ALL_CHUNKS512 = [(512 * i, 512) for i in range(8)] + [(4096, 256)]


def build_program(scopes=False):
    nc = bass.Bass("TRN2", target_bir_lowering=False)
    P = Prog(nc)
    P.scopes = scopes
    Dm = Dram(nc, ext_in=["xin"])
    src = "xin"
    for l in range(2):
        last = (l == 1)
        ctx_out = not last
        for fi, fn in enumerate((lambda: phase_mod(P, Dm, l),
                   lambda: phase_a(P, Dm, l, src),
                   lambda: phase_b(P, Dm, l),
                   lambda: phase_c(P, Dm, l, ctx_out),
                   lambda: phase_d1(P, Dm, l),
                   lambda: phase_d2(P, Dm, l),
                   lambda: phase_e(P, Dm, l, ctx_out),
                   lambda: phase_f2(P, Dm, l, src, tok_chunks=(LAT_CHUNKS if last else None)),
                   lambda: phase_g2(P, Dm, l, (LAT_CHUNKS if last else CHUNKS), last))):
            P.begin_phase("L%d_%s" % (l, "mabcdDefg"[fi]))
            fn()
            P.end_phase()
        src = "XN%d" % l
    P.emit()
    return nc, P, Dm


_CACHE = {}


def kernel(**inputs):
    inp = {k: np.asarray(v) for k, v in inputs.items()}
    if "nc" not in _CACHE:
        _CACHE["nc"] = build_program()
    nc, P, Dm = _CACHE["nc"]
    n_cores = 8
    maps = []
    per_b = {}
    for cidx in range(n_cores):
        b = cidx % 4
        if b not in per_b:
            m = core_inputs(inp, b)
            per_b[b] = {k: v for k, v in m.items() if k in Dm.t}
        maps.append(per_b[b])
    res = run_bass_kernel_spmd(nc, maps, core_ids=list(range(n_cores)))
    out = np.stack([np.asarray(res.results[b]["out"]) for b in range(4)], 0)
    return out.astype(np.float32)
```

```python
import numpy as np
from contextlib import ExitStack
import concourse.bass as bass
import concourse.mybir as mybir
from concourse.bass_utils import run_bass_kernel_spmd

F32 = mybir.dt.float32
F32R = mybir.dt.float32r
I32 = mybir.dt.int32
AF = mybir.ActivationFunctionType
ALU = mybir.AluOpType
AX = mybir.AxisListType

LC, L, T, D = 256, 4096, 4352, 1024
NT = T // 128
EPS = 1e-6
CHUNKS = [(0, 256)] + [(256 + 512 * i, 512) for i in range(8)]
PERM = np.concatenate([np.arange(256, 768), np.arange(1800, 2312), np.arange(768, 1024), np.arange(1792, 1800),
                       np.arange(0, 256), np.arange(1024, 1792)])
TM_COLS = 1288
FM_OFF = 1288

ENGS = ["pe", "act", "dve", "pool", "sp"]
N_DMA_SEMS = 8


class Prog:
    def __init__(self, nc, same_engine_sync=True):
        self.nc = nc
        self.st = ExitStack()
        self.same = same_engine_sync
        self.ops = {e: [] for e in ENGS}
        self.sems = {}
        self.cnt = {}
        for e in ["pe", "act", "dve", "pool"]:
            self.sems[e] = self.st.enter_context(nc.semaphore("s_" + e))
            self.cnt[e] = 0
        for q in ["sp", "pool"]:
            for i in range(N_DMA_SEMS):
                nm = "d_%s%d" % (q, i)
                self.sems[nm] = self.st.enter_context(nc.semaphore(nm))
                self.cnt[nm] = 0
        self.drr = {"sp": 0, "pool": 0}
        self.waited = {e: {} for e in ENGS}
        self.last_w = {}
        self.readers = {}
        self.n_ops = 0
        self.final = []
        self.uid = 0
        self.pst = None
        self.barrier = {e: {} for e in ENGS}
        self.children = {}
        self.known = set()
        self.scopes = False
        self.bound_reg = None
        self.psum_keys = set()

    def begin_phase(self, name=None):
        self.pst = ExitStack()
        self.phase_name = name

    def end_phase(self):
        self.pst.close()
        self.pst = None
        snap = {s: v for s, v in self.cnt.items() if v > 0}
        for e in ENGS:
            self.barrier[e] = dict(snap)

    def bound(self):
        if self.bound_reg is None:
            self.bound_reg = self.nc.gpsimd.alloc_register("bc4095")
        return self.bound_reg

    def sb(self, name, shape, dtype=F32):
        return (self.pst or self.st).enter_context(self.nc.sbuf_tensor(name, list(shape), dtype))

    def ps(self, name, shape, dtype=F32):
        return (self.pst or self.st).enter_context(self.nc.psum_tensor(name, list(shape), dtype))

    def _related(self, k):
        rel = [k]
        parts = k.split("/")
        for n in range(1, len(parts)):
            rel.append("/".join(parts[:n]))
        rel.extend(self.children.get(k, ()))
        return rel

    def _register(self, k):
        if k in self.known:
            return
        self.known.add(k)
        parts = k.split("/")
        for n in range(1, len(parts)):
            self.children.setdefault("/".join(parts[:n]), set()).add(k)

    def _deps(self, eng, reads, writes):
        deps = {}

        def add(s, v):
            if v > deps.get(s, 0):
                deps[s] = v

        for k in list(reads) + list(writes):
            self._register(k)
        for k in reads:
            for kk in self._related(k):
                t = self.last_w.get(kk)
                if t is not None:
                    add(*t)
        for k in writes:
            for kk in self._related(k):
                t = self.last_w.get(kk)
                if t is not None:
                    add(*t)
                for s_, v_ in self.readers.get(kk, {}).items():
                    add(s_, v_)
        if self.barrier[eng]:
            for s_, v_ in self.barrier[eng].items():
                add(s_, v_)
            self.barrier[eng] = {}
        out = []
        for s, v in deps.items():
            if s == eng and (eng == "pe" or not self.same):
                continue
            if v > self.waited[eng].get(s, 0):
                self.waited[eng][s] = v
                out.append((s, v))
        return out

    def _commit(self, tok, reads, writes):
        for k in writes:
            self.last_w[k] = tok
            self.readers[k] = {}
        for k in reads:
            if k in writes:
                continue
            rd = self.readers.setdefault(k, {})
            if tok[1] > rd.get(tok[0], 0):
                rd[tok[0]] = tok[1]

    def op(self, eng, fn, reads=(), writes=()):
        pr = [k for k in reads if k in self.psum_keys]
        if pr:
            reads = [k for k in reads if k not in self.psum_keys]
            writes = list(writes) + pr
        waits = self._deps(eng, reads, writes)
        self.cnt[eng] += 1
        tok = (eng, self.cnt[eng])
        self.ops[eng].append((waits, fn, tok, 1, getattr(self, "phase_name", None)))
        self._commit(tok, reads, writes)
        self.n_ops += 1
        return tok

    def u(self, base):
        self.uid += 1
        return "%s/%d" % (base, self.uid)

    def i(self, eng, name, reads=(), writes=(), **kw):
        return self.op(eng, lambda e: getattr(e, name)(**kw), reads, writes)

    def d(self, out, in_, reads=(), writes=(), q="sp", final=False):
        return self.dma(lambda e: e.dma_start(out=out, in_=in_), reads, writes, q=q, final=final)

    def dma(self, fn, reads=(), writes=(), q="sp", final=False):
        waits = self._deps(q, reads, writes)
        i = self.drr[q]
        self.drr[q] = (i + 1) % N_DMA_SEMS
        nm = "d_%s%d" % (q, i)
        prev = self.cnt[nm]
        if prev > self.waited[q].get(nm, 0):
            self.waited[q][nm] = prev
            waits.append((nm, prev))
        self.cnt[nm] += 16
        tok = (nm, self.cnt[nm])
        self.ops[q].append((waits, fn, tok, 16, getattr(self, "phase_name", None)))
        self._commit(tok, reads, writes)
        self.n_ops += 1
        if final:
            self.final.append(tok)
        return tok

    def emit(self):
        nc = self.nc
        fin = list(self.final)
        engmap = {"pe": "tensor", "act": "scalar", "dve": "vector", "pool": "gpsimd", "sp": "sync"}
        with nc.Block() as block:
            for e in ENGS:
                lst = self.ops[e]
                extra = fin if e == "sp" else []
                if not lst and not extra:
                    continue

                def body(engine, lst=lst, extra=extra, e=e):
                    cur = None
                    scope = None
                    if e == "pool" and self.bound_reg is not None:
                        engine.reg_mov(self.bound_reg, 4095)
                    for (waits, fn, tok, amt, ph) in lst:
                        if self.scopes and ph != cur:
                            if scope is not None:
                                scope.__exit__(None, None, None)
                            scope = nc.named_scope(ph or "none")
                            scope.__enter__()
                            cur = ph
                        for (s, v) in waits:
                            engine.wait_ge(self.sems[s], v)
                        fn(engine).then_inc(self.sems[tok[0]], amt)
                    if scope is not None:
                        scope.__exit__(None, None, None)
                    for (s, v) in extra:
                        engine.wait_ge(self.sems[s], v)

                getattr(block, engmap[e])(body)
        self.st.close()


class Rot:
    def __init__(self, P, name, shape, dtype=F32, n=2, psum=False):
        self.bufs = [(P.ps if psum else P.sb)("%s%d" % (name, i), shape, dtype) for i in range(n)]
        self.keys = ["%s%d" % (name, i) for i in range(n)]
        self.i = 0
        if psum:
            P.psum_keys.update(self.keys)

    def next(self):
        j = self.i % len(self.bufs)
        self.i += 1
        return self.bufs[j], self.keys[j]


class Dram:
    def __init__(self, nc, ext_in=(), ext_out=()):
        self.nc, self.t = nc, {}
        self.ext_in, self.ext_out = set(ext_in), set(ext_out)

    def get(self, name, shape=None, dtype=F32, kind=None):
        if name not in self.t:
            if kind is None:
                kind = "ExternalInput" if name in self.ext_in else ("ExternalOutput" if name in self.ext_out else "Internal")
            self.t[name] = self.nc.dram_tensor(name, list(shape), dtype, kind=kind).ap()
        return self.t[name]


def phase_mod(P, Dm, l):
    nc = P.nc
    cvec = Dm.get("cvec", [128, 8, 2], kind="ExternalInput")
    ada_w = Dm.get("ada_w%d" % l, [128, 8, 6144], kind="ExternalInput")
    ada_b = Dm.get("ada_b%d" % l, [1, 6144], kind="ExternalInput")
    modrow = Dm.get("modrow%d" % l, [2, 6144])
    pf = "m%d_" % l
    cv = P.sb(pf + "cv", [128, 8, 2])
    sg = P.sb(pf + "sg", [128, 8, 2])
    sc = P.sb(pf + "sc", [128, 8, 128], F32R)
    ab = P.sb(pf + "ab", [2, 6144])
    mr = P.sb(pf + "mr", [2, 6144])
    wch = Rot(P, pf + "w", [128, 8, 512], F32R, n=2)
    pm = Rot(P, pf + "pm", [128, 512], F32, n=2, psum=True)
    P.dma(lambda e: e.dma_start(out=cv[:], in_=cvec[:, :, :]), writes=[pf + "cv"])
    P.dma(lambda e: e.dma_start(out=ab[:], in_=ada_b[0:1, :].to_broadcast([2, 6144])), writes=[pf + "ab"])
    P.op("act", lambda e: e.activation(out=sg[:], in_=cv[:], func=AF.Sigmoid), reads=[pf + "cv"], writes=[pf + "sg"])
    P.op("dve", lambda e: e.memset(sc[:].bitcast(F32), 0.0), writes=[pf + "sc"])
    P.op("dve", lambda e: e.tensor_tensor(out=sc[:, :, 0:2], in0=cv[:], in1=sg[:], op=ALU.mult), reads=[pf + "cv", pf + "sg"], writes=[pf + "sc"])
    for j in range(12):
        w, wk = wch.next()
        P.dma(lambda e, w=w, j=j: e.dma_start(out=w[:], in_=ada_w[:, :, j * 512:(j + 1) * 512]), writes=[wk], q="pool")
        pt, pk = pm.next()
        for k in range(8):
            P.op("pe", lambda e, w=w, pt=pt, k=k: e.matmul(pt[:], sc[:, k, :], w[:, k, :], start=(k == 0), stop=(k == 7)),
                 reads=[wk, pf + "sc"], writes=[pk])
        P.op("dve", lambda e, pt=pt, j=j: e.tensor_tensor(out=mr[:, j * 512:(j + 1) * 512], in0=pt[0:2, :], in1=ab[:, j * 512:(j + 1) * 512], op=ALU.add),
             reads=[pk, pf + "ab"], writes=[pf + "mr"])
    P.dma(lambda e: e.dma_start(out=modrow[:, :], in_=mr[:]), reads=[pf + "mr"], writes=["modrow%d" % l])


def load_bcast(P, dst, dkey, src_row, reads=()):
    n = src_row.shape[-1]
    P.dma(lambda e: e.dma_start(out=dst, in_=src_row.to_broadcast([128, n])), reads=list(reads), writes=[dkey])


def phase_a(P, Dm, l, src_name, chunks=None, stop=99):
    nc = P.nc
    pf = "a%d_" % l
    xsrc = Dm.get(src_name, [T, D])
    w_in = Dm.get("w_in%d" % l, [128, 8, 2312], kind="ExternalInput")
    modrow = Dm.get("modrow%d" % l, [2, 6144])
    n1g = Dm.get("norm1_g%d" % l, [1, D], kind="ExternalInput")
    qkg = Dm.get("qkg%d" % l, [1, 384], kind="ExternalInput")
    dtb = Dm.get("dtb%d" % l, [1, 8], kind="ExternalInput")
    ropec = Dm.get("rope_cos", [L, 384], kind="ExternalInput")
    ropes = Dm.get("rope_sin", [L, 384], kind="ExternalInput")
    ident_d = Dm.get("ident", [128, 128], kind="ExternalInput")
    FMT = Dm.get("FMT", [1024, T])
    QKT = Dm.get("QKT", [768, T])
    TMS = Dm.get("TMS", [T, 520])
    mk = "modrow%d" % l

    ident = P.sb(pf + "ident", [128, 128])
    P.dma(lambda e: e.dma_start(out=ident[:], in_=ident_d[:, :]), writes=[pf + "ident"])
    win = P.sb(pf + "win", [128, 8, 2312], F32R)
    for k in range(8):
        P.dma(lambda e, k=k: e.dma_start(out=win[:, k, :], in_=w_in[:, k, :]), writes=[pf + "win/%d" % k], q="pool")
    wkeys = [pf + "win/%d" % k for k in range(8)]
    G = [P.sb(pf + "G%d" % r, [128, D]) for r in range(2)]
    SH = [P.sb(pf + "SH%d" % r, [128, D]) for r in range(2)]
    gn = P.sb(pf + "gn", [128, D])
    load_bcast(P, gn[:], pf + "gn", n1g[0:1, :])
    for r in range(2):
        load_bcast(P, SH[r][:], pf + "SH%d" % r, modrow[r:r + 1, 0:1024], reads=[mk])
        load_bcast(P, G[r][:], pf + "G%d" % r, modrow[r:r + 1, 1024:2048], reads=[mk])
        P.op("dve", lambda e, r=r: e.scalar_tensor_tensor(out=G[r][:], in0=G[r][:], scalar=1.0, in1=gn[:], op0=ALU.add, op1=ALU.mult),
             reads=[pf + "G%d" % r, pf + "gn"], writes=[pf + "G%d" % r])
    qkgb = P.sb(pf + "qkgb", [128, 384])
    load_bcast(P, qkgb[:], pf + "qkgb", qkg[0:1, :])
    dtbb = P.sb(pf + "dtbb", [128, 8])
    load_bcast(P, dtbb[:], pf + "dtbb", dtb[0:1, :])
    epsc = P.sb(pf + "eps", [128, 1])
    P.op("dve", lambda e: e.memset(epsc[:], EPS), writes=[pf + "eps"])
    onec = P.sb(pf + "one", [128, 1])
    P.op("dve", lambda e: e.memset(onec[:], 1.0), writes=[pf + "one"])

    xt_r = Rot(P, pf + "xt", [128, D], n=3)
    junk = P.sb(pf + "junk", [128, D])
    st_r = Rot(P, pf + "st", [128, 32], n=3)
    h_r = Rot(P, pf + "h", [128, D], n=2)
    tp_r = Rot(P, pf + "tp", [128, 1024], n=1, psum=True)
    hT_r = Rot(P, pf + "hT", [128, 8, 512], F32R, n=2)
    pj_r = Rot(P, pf + "pj", [128, 512], n=3, psum=True)
    qk_r = Rot(P, pf + "qk", [128, 12, 64], n=2)
    sq_r = Rot(P, pf + "sq", [128, 6, 64], n=2)
    rp_r = Rot(P, pf + "rp", [128, 24, 2, 16], n=2)
    tmp_r = Rot(P, pf + "tmp", [128, 24, 16], n=2)
    cs_r = Rot(P, pf + "cs", [128, 2, 384], n=2)
    tq_r = Rot(P, pf + "tq", [128, 1024], n=1, psum=True)
    qT_r = Rot(P, pf + "qT", [128, 6, 128], n=2)
    tm_r = Rot(P, pf + "tm", [128, 520], n=2)
    fm_r = Rot(P, pf + "fm", [128, 512], n=1, psum=True)
    fo_r = Rot(P, pf + "fo", [128, 512], n=3)

    for (c0, cn) in (chunks or CHUNKS):
        hT, hTk = hT_r.next()
        ntile = cn // 128
        for i in range(ntile):
            t0 = c0 + i * 128
            is_ctx = t0 < LC
            r = 1 if is_ctx else 0
            xt, xk = xt_r.next()
            P.dma(lambda e, xt=xt, t0=t0: e.dma_start(out=xt[:], in_=xsrc[t0:t0 + 128, :]), reads=[src_name], writes=[xk])
            st, sk = st_r.next()
            P.op("act", lambda e, xt=xt, st=st: e.activation(out=junk[:], in_=xt[:], func=AF.Square, accum_out=st[:, 0:1]),
                 reads=[xk], writes=[pf + "junk", sk])
            P.op("act", lambda e, st=st: e.activation(out=st[:, 1:2], in_=st[:, 0:1], func=AF.Sqrt, bias=epsc[:], scale=1.0 / D),
                 reads=[sk, pf + "eps"], writes=[sk])
            P.op("dve", lambda e, st=st: e.reciprocal(out=st[:, 2:3], in_=st[:, 1:2]), reads=[sk], writes=[sk])
            h, hk = h_r.next()
            P.op("dve", lambda e, xt=xt, st=st, h=h, r=r: e.scalar_tensor_tensor(out=h[:], in0=xt[:], scalar=st[:, 2:3], in1=G[r][:], op0=ALU.mult, op1=ALU.mult),
                 reads=[xk, sk, pf + "G%d" % r], writes=[hk])
            P.op("pool", lambda e, h=h, r=r: e.tensor_tensor(out=h[:], in0=h[:], in1=SH[r][:], op=ALU.add),
                 reads=[hk, pf + "SH%d" % r], writes=[hk])
            if stop == 1:
                continue
            tp, tpk = tp_r.next()
            for k in range(8):
                P.op("pe", lambda e, h=h, tp=tp, k=k: e.transpose(tp[:, k * 128:(k + 1) * 128], h[:, k * 128:(k + 1) * 128], ident[:]),
                     reads=[hk, pf + "ident"], writes=[tpk])
            P.op("act", lambda e, hT=hT, tp=tp, i=i: e.activation(out=hT[:, :, i * 128:(i + 1) * 128], in_=tp[:].rearrange("p (k t) -> p k t", k=8), func=AF.Copy),
                 reads=[tpk], writes=[hTk + "/%d" % i])
            if stop == 2:
                continue
            pjs = []
            for (o, n) in [(0, 512), (512, 512), (1024, 264)]:
                pj, pjk = pj_r.next()
                for k in range(8):
                    P.op("pe", lambda e, pj=pj, hT=hT, k=k, o=o, n=n, i=i: e.matmul(pj[:, 0:n], hT[:, k, i * 128:(i + 1) * 128], win[:, k, o:o + n], start=(k == 0), stop=(k == 7)),
                         reads=[hTk + "/%d" % i, wkeys[k]], writes=[pjk])
                pjs.append((pj, pjk))
            (pA, pAk), (pB, pBk), (pC, pCk) = pjs
            if stop == 3:
                continue
            qk, qkk = qk_r.next()
            sq, sqk = sq_r.next()
            pA6 = pA[:, 0:384].rearrange("p (h d) -> p h d", h=6)
            P.op("act", lambda e, sq=sq, pA6=pA6: e.activation(out=sq[:], in_=pA6, func=AF.Square), reads=[pAk], writes=[sqk])
            P.op("dve", lambda e, sq=sq, st=st: e.tensor_reduce(out=st[:, 4:10], in_=sq[:], axis=AX.X, op=ALU.add), reads=[sqk], writes=[sk])
            P.op("act", lambda e, st=st: e.activation(out=st[:, 4:10], in_=st[:, 4:10], func=AF.Sqrt, bias=epsc[:], scale=1.0 / 64), reads=[sk, pf + "eps"], writes=[sk])
            P.op("dve", lambda e, st=st: e.reciprocal(out=st[:, 10:16], in_=st[:, 4:10]), reads=[sk], writes=[sk])
            P.op("dve", lambda e, sq=sq, pA6=pA6, st=st: e.tensor_tensor(out=sq[:], in0=pA6, in1=st[:, 10:16].unsqueeze(2).to_broadcast([128, 6, 64]), op=ALU.mult),
                 reads=[pAk, sk], writes=[sqk])
            P.op("pool", lambda e, sq=sq, qk=qk: e.tensor_tensor(out=qk[:, 0:6, :], in0=sq[:], in1=qkgb[:].rearrange("p (h d) -> p h d", h=6), op=ALU.mult),
                 reads=[sqk, pf + "qkgb"], writes=[qkk + "/a"])
            P.op("act", lambda e, qk=qk, pB=pB: e.activation(out=qk[:, 6:12, :], in_=pB[:, 0:384].rearrange("p (h d) -> p h d", h=6), func=AF.Copy),
                 reads=[pBk], writes=[qkk + "/b"])
            if stop == 4:
                continue
            tm, tmk = tm_r.next()
            P.op("act", lambda e, tm=tm, pA=pA: e.activation(out=tm[:, 0:128], in_=pA[:, 384:512], func=AF.Copy), reads=[pAk], writes=[tmk + "/a"])
            P.op("act", lambda e, tm=tm, pB=pB: e.activation(out=tm[:, 128:256], in_=pB[:, 384:512], func=AF.Copy), reads=[pBk], writes=[tmk + "/b"])
            P.op("act", lambda e, tm=tm, pC=pC: e.activation(out=tm[:, 256:512], in_=pC[:, 0:256], func=AF.Copy), reads=[pCk], writes=[tmk + "/c"])
            P.op("dve", lambda e, tm=tm, pC=pC: e.tensor_tensor(out=tm[:, 512:520], in0=pC[:, 256:264], in1=dtbb[:], op=ALU.add), reads=[pCk, pf + "dtbb"], writes=[tmk + "/d"])
            P.op("act", lambda e, st=st, tm=tm: e.activation(out=st[:, 16:24], in_=tm[:, 512:520], func=AF.Abs), reads=[tmk + "/d", sk], writes=[sk])
            P.op("act", lambda e, st=st: e.activation(out=st[:, 16:24], in_=st[:, 16:24], func=AF.Exp, scale=-1.0), reads=[sk], writes=[sk])
            P.op("act", lambda e, st=st: e.activation(out=st[:, 16:24], in_=st[:, 16:24], func=AF.Ln, bias=onec[:]), reads=[sk], writes=[sk])
            P.op("dve", lambda e, st=st, tm=tm: e.scalar_tensor_tensor(out=tm[:, 512:520], in0=tm[:, 512:520], scalar=0.0, in1=st[:, 16:24], op0=ALU.max, op1=ALU.add),
                 reads=[tmk + "/d", sk], writes=[tmk + "/d"])
            P.dma(lambda e, tm=tm, t0=t0: e.dma_start(out=TMS[t0:t0 + 128, :], in_=tm[:]), reads=[tmk], writes=[P.u("TMS")])
            if stop == 5:
                continue
            rp, rpk = rp_r.next()
            if is_ctx:
                P.op("pool", lambda e, rp=rp, qk=qk: e.tensor_copy(out=rp[:].rearrange("p (h a) b f -> p h (a b f)", a=2), in_=qk[:]),
                     reads=[qkk + "/a", qkk + "/b"], writes=[rpk])
            else:
                cs, csk = cs_r.next()
                tl = t0 - LC
                P.dma(lambda e, cs=cs, tl=tl: e.dma_start(out=cs[:, 0, :], in_=ropec[tl:tl + 128, :]), writes=[csk + "/c"])
                P.dma(lambda e, cs=cs, tl=tl: e.dma_start(out=cs[:, 1, :], in_=ropes[tl:tl + 128, :]), writes=[csk + "/s"])
                qv = qk[:].rearrange("p h (a b f) -> p (h a) b f", a=2, b=2)
                x1, x2 = qv[:, :, 0, :], qv[:, :, 1, :]
                cb = cs[:, 0, :].rearrange("p (g f) -> p g f", f=16)
                sb_ = cs[:, 1, :].rearrange("p (g f) -> p g f", f=16)
                tmp, tmpk = tmp_r.next()
                qkr = [qkk + "/a", qkk + "/b"]
                P.op("dve", lambda e, rp=rp, x1=x1, cb=cb: e.tensor_tensor(out=rp[:, :, 0, :], in0=x1, in1=cb, op=ALU.mult), reads=qkr + [csk + "/c"], writes=[rpk + "/0"])
                P.op("pool", lambda e, tmp=tmp, x2=x2, sb_=sb_: e.tensor_tensor(out=tmp[:], in0=x2, in1=sb_, op=ALU.mult), reads=qkr + [csk + "/s"], writes=[tmpk])
                P.op("dve", lambda e, rp=rp, tmp=tmp: e.tensor_tensor(out=rp[:, :, 0, :], in0=rp[:, :, 0, :], in1=tmp[:], op=ALU.subtract), reads=[rpk + "/0", tmpk], writes=[rpk + "/0"])
                P.op("dve", lambda e, rp=rp, x2=x2, cb=cb: e.tensor_tensor(out=rp[:, :, 1, :], in0=x2, in1=cb, op=ALU.mult), reads=qkr + [csk + "/c"], writes=[rpk + "/1"])
                P.op("pool", lambda e, tmp=tmp, x1=x1, sb_=sb_: e.tensor_tensor(out=tmp[:], in0=x1, in1=sb_, op=ALU.mult), reads=qkr + [csk + "/s"], writes=[tmpk])
                P.op("dve", lambda e, rp=rp, tmp=tmp: e.tensor_tensor(out=rp[:, :, 1, :], in0=rp[:, :, 1, :], in1=tmp[:], op=ALU.add), reads=[rpk + "/1", tmpk], writes=[rpk + "/1"])
            rkeys = [rpk] if is_ctx else [rpk + "/0", rpk + "/1"]
            if stop == 6:
                continue
            rpf = rp[:].rearrange("p g b f -> p (g b f)")
            tq, tqk = tq_r.next()
            for j in range(6):
                P.op("pe", lambda e, tq=tq, rpf=rpf, j=j: e.transpose(tq[:, j * 128:(j + 1) * 128], rpf[:, j * 128:(j + 1) * 128], ident[:]),
                     reads=rkeys + [pf + "ident"], writes=[tqk])
            qT, qTk = qT_r.next()
            P.op("act", lambda e, qT=qT, tq=tq: e.activation(out=qT[:], in_=tq[:, 0:768].rearrange("p (j t) -> p j t", j=6), func=AF.Copy), reads=[tqk], writes=[qTk])
            P.dma(lambda e, qT=qT, t0=t0: e.dma_start(out=QKT[:, t0:t0 + 128].rearrange("(j p) t -> p j t", p=128), in_=qT[:]), reads=[qTk], writes=[P.u("QKT")])
        if stop <= 7:
            continue
        hkeys = [hTk + "/%d" % i for i in range(ntile)]
        for j in range(8):
            fm, fmk = fm_r.next()
            for k in range(8):
                P.op("pe", lambda e, fm=fm, hT=hT, k=k, j=j, cn=cn: e.matmul(fm[:, 0:cn], win[:, k, FM_OFF + j * 128:FM_OFF + (j + 1) * 128], hT[:, k, 0:cn], start=(k == 0), stop=(k == 7)),
                     reads=hkeys + [wkeys[k]], writes=[fmk])
            fo, fok = fo_r.next()
            eng = "act" if j % 2 == 0 else "dve"
            if eng == "act":
                P.op("act", lambda e, fo=fo, fm=fm, cn=cn: e.activation(out=fo[:, 0:cn], in_=fm[:, 0:cn], func=AF.Copy), reads=[fmk], writes=[fok])
            else:
                P.op("dve", lambda e, fo=fo, fm=fm, cn=cn: e.tensor_copy(out=fo[:, 0:cn], in_=fm[:, 0:cn]), reads=[fmk], writes=[fok])
            P.dma(lambda e, fo=fo, j=j, c0=c0, cn=cn: e.dma_start(out=FMT[j * 128:(j + 1) * 128, c0:c0 + cn], in_=fo[:, 0:cn]), reads=[fok], writes=[P.u("FMT")])


def phase_g2(P, Dm, l, tok_chunks, final):
    pf = "G%d_" % l
    ntf = sum(cn for _, cn in tok_chunks) // 128
    NB = 2 * ntf + 32
    X1 = Dm.get("X1", [T, D])
    BUF = Dm.get("BUF%d" % l, [NB * 128, D])
    OBUF = Dm.get("OBUF%d" % l, [NB * 128, D])
    IDXW = Dm.get("IDXW%d" % l, [128, NB], I32)
    DEST = Dm.get("DEST%d" % l, [128, ntf * 2], I32)
    WWd = Dm.get("WWd%d" % l, [128, ntf * 2])
    WGU = Dm.get("moe_gu%d" % l, [32, 128, 8, 1024], kind="ExternalInput")
    WD = Dm.get("moe_d%d" % l, [32, 128, 4, 1024], kind="ExternalInput")
    ident_d = Dm.get("ident", [128, 128], kind="ExternalInput")
    modrow = Dm.get("modrow%d" % l, [2, 6144])
    mk = "modrow%d" % l
    ident = P.sb(pf + "ident", [128, 128]); P.d(ident[:], ident_d[:, :], writes=[pf + "ident"])
    idxw = P.sb(pf + "idxw", [128, NB], I32); P.d(idxw[:], IDXW[:, :], reads=["IDXW%d" % l], writes=[pf + "idxw"])
    dest = P.sb(pf + "dest", [128, ntf * 2], I32); P.d(dest[:], DEST[:, :], reads=["DEST%d" % l], writes=[pf + "dest"])
    ww = P.sb(pf + "ww", [128, ntf * 2]); P.d(ww[:], WWd[:, :], reads=["WWd%d" % l], writes=[pf + "ww"])
    wgu_r = Rot(P, pf + "wgu", [128, 8, 1024], F32R, n=1)
    wd_r = Rot(P, pf + "wd", [128, 4, 1024], F32R, n=1)
    xs_r = Rot(P, pf + "xs", [128, D], n=3)
    tp_r = Rot(P, pf + "tp", [128, 1024], n=1, psum=True)
    xT_r = Rot(P, pf + "xT", [128, 8, 128], F32R, n=2)
    pg_r = Rot(P, pf + "pg", [128, 512], n=1, psum=True)
    pu_r = Rot(P, pf + "pu", [128, 512], n=1, psum=True)
    sg_r = Rot(P, pf + "sg", [128, 512], n=2)
    hu_r = Rot(P, pf + "hu", [128, 512], n=2)
    ph_r = Rot(P, pf + "ph", [128, 512], n=1, psum=True)
    hT_r = Rot(P, pf + "hT", [128, 4, 128], F32R, n=2)
    po_r = Rot(P, pf + "po", [128, 1024], n=1, psum=True)
    ob_r = Rot(P, pf + "ob", [128, D], n=2)
    WGUf = WGU.rearrange("e p k n -> (e p) (k n)")
    WDf = WD.rearrange("e p k n -> (e p) (k n)")
    st1 = {}
    breg = P.bound()

    def load_gu(b):
        off = bass.IndirectOffsetOnAxis(ap=idxw[:, b:b + 1], axis=0)
        wgu, wguk = wgu_r.next()
        P.dma(lambda e, wgu=wgu, off=off: e.indirect_dma_start(out=wgu[:].rearrange("p k n -> p (k n)"), out_offset=None, in_=WGUf, in_offset=off, bounds_check=breg, oob_is_err=False), reads=[pf + "idxw"], writes=[wguk], q="pool")
        st1[("gu", b)] = (wgu, wguk)

    def load_d(b):
        off = bass.IndirectOffsetOnAxis(ap=idxw[:, b:b + 1], axis=0)
        wd, wdk = wd_r.next()
        P.dma(lambda e, wd=wd, off=off: e.indirect_dma_start(out=wd[:].rearrange("p k n -> p (k n)"), out_offset=None, in_=WDf, in_offset=off, bounds_check=breg, oob_is_err=False), reads=[pf + "idxw"], writes=[wdk], q="pool")
        st1[("d", b)] = (wd, wdk)

    def stage1(b):
        (wgu, wguk) = st1.pop(("gu", b))
        xs, xsk = xs_r.next()
        P.d(xs[:], BUF[b * 128:(b + 1) * 128, :], reads=["BUF%d" % l, "BUFz%d" % l], writes=[xsk])
        tp, tpk = tp_r.next()
        for k in range(8):
            P.i("pe", "transpose", reads=[xsk, pf + "ident"], writes=[tpk], out=tp[:, k * 128:(k + 1) * 128], in_=xs[:, k * 128:(k + 1) * 128], identity=ident[:])
        xT, xTk = xT_r.next()
        P.i("act", "activation", reads=[tpk], writes=[xTk + "/0"], out=xT[:, 0:4, :], in_=tp[:, 0:512].rearrange("p (k t) -> p k t", k=4), func=AF.Copy)
        P.i("dve", "tensor_copy", reads=[tpk], writes=[xTk + "/1"], out=xT[:, 4:8, :], in_=tp[:, 512:1024].rearrange("p (k t) -> p k t", k=4))
        pg, pgk = pg_r.next(); pu, puk = pu_r.next()
        for k in range(8):
            P.i("pe", "matmul", reads=[xTk, wguk], writes=[pgk], out=pg[:], lhsT=xT[:, k, :], rhs=wgu[:, k, 0:512], start=(k == 0), stop=(k == 7))
        for k in range(8):
            P.i("pe", "matmul", reads=[xTk, wguk], writes=[puk], out=pu[:], lhsT=xT[:, k, :], rhs=wgu[:, k, 512:1024], start=(k == 0), stop=(k == 7))
        sg, sgk = sg_r.next()
        P.i("act", "activation", reads=[pgk], writes=[sgk], out=sg[:], in_=pg[:], func=AF.Silu)
        hu, huk = hu_r.next()
        P.i("dve", "tensor_tensor", reads=[sgk, puk], writes=[huk], out=hu[:], in0=sg[:], in1=pu[:], op=ALU.mult)
        st1[("h", b)] = (hu, huk)

    def stage2(b):
        (hu, huk) = st1.pop(("h", b))
        (wd, wdk) = st1.pop(("d", b))
        ph, phk = ph_r.next()
        for hc in range(4):
            P.i("pe", "transpose", reads=[huk, pf + "ident"], writes=[phk], out=ph[:, hc * 128:(hc + 1) * 128], in_=hu[:, hc * 128:(hc + 1) * 128], identity=ident[:])
        hT, hTk = hT_r.next()
        P.i("act", "activation", reads=[phk], writes=[hTk], out=hT[:], in_=ph[:].rearrange("p (k t) -> p k t", k=4), func=AF.Copy)
        po, pok = po_r.next()
        for hf in range(2):
            for hc in range(4):
                P.i("pe", "matmul", reads=[hTk, wdk], writes=[pok], out=po[:, hf * 512:(hf + 1) * 512], lhsT=hT[:, hc, :], rhs=wd[:, hc, hf * 512:(hf + 1) * 512], start=(hc == 0), stop=(hc == 3))
        ob, obk = ob_r.next()
        P.i("act", "activation", reads=[pok], writes=[obk + "/0"], out=ob[:, 0:512], in_=po[:, 0:512], func=AF.Copy)
        P.i("dve", "tensor_copy", reads=[pok], writes=[obk + "/1"], out=ob[:, 512:1024], in_=po[:, 512:1024])
        P.d(OBUF[b * 128:(b + 1) * 128, :], ob[:], reads=[obk], writes=[P.u("OBUF%d" % l)])

    load_gu(0); load_d(0)
    for b in range(NB):
        stage1(b)
        if b + 1 < NB:
            load_gu(b + 1)
        stage2(b)
        if b + 1 < NB:
            load_d(b + 1)
    if final:
        fg_d = Dm.get("final_g", [1, D], kind="ExternalInput")
        OUT = Dm.get("out", [L, D], kind="ExternalOutput")
        fg = P.sb(pf + "fg", [128, D]); load_bcast(P, fg[:], pf + "fg", fg_d[0:1, :])
        epsc = P.sb(pf + "eps", [128, 1]); P.i("dve", "memset", writes=[pf + "eps"], ap=epsc[:], constant=EPS)
        junk = P.sb(pf + "junk", [128, D])
    else:
        XN = Dm.get("XN%d" % l, [T, D])
    G2g = [P.sb(pf + "g2%d" % r, [128, D]) for r in range(2)]
    for r in range(2):
        load_bcast(P, G2g[r][:], pf + "g2%d" % r, modrow[r:r + 1, 5120:6144], reads=[mk])
    o1_r = Rot(P, pf + "o1", [128, D], n=2)
    o2_r = Rot(P, pf + "o2", [128, D], n=2)
    xt_r = Rot(P, pf + "xt", [128, D], n=2)
    st_r = Rot(P, pf + "st", [128, 8], n=2)
    ti = 0
    for (c0, cn) in tok_chunks:
        for i in range(cn // 128):
            t0 = c0 + i * 128
            r = 1 if t0 < LC else 0
            o1, o1k = o1_r.next(); o2, o2k = o2_r.next()
            for (o, ok_, col) in ((o1, o1k, ti * 2), (o2, o2k, ti * 2 + 1)):
                P.dma(lambda e, o=o, col=col: e.indirect_dma_start(out=o[:], out_offset=None, in_=OBUF[:, :], in_offset=bass.IndirectOffsetOnAxis(ap=dest[:, col:col + 1], axis=0)),
                      reads=[pf + "dest", "OBUF%d" % l], writes=[ok_], q="pool")
            xt, xk = xt_r.next()
            P.d(xt[:], X1[t0:t0 + 128, :], reads=["X1"], writes=[xk])
            P.i("dve", "tensor_scalar", reads=[o1k, pf + "ww"], writes=[o1k], out=o1[:], in0=o1[:], scalar1=ww[:, ti * 2:ti * 2 + 1], scalar2=None, op0=ALU.mult)
            P.i("dve", "scalar_tensor_tensor", reads=[o1k, o2k, pf + "ww"], writes=[o1k], out=o1[:], in0=o2[:], scalar=ww[:, ti * 2 + 1:ti * 2 + 2], in1=o1[:], op0=ALU.mult, op1=ALU.add)
            P.i("pool", "tensor_tensor", reads=[o1k, pf + "g2%d" % r], writes=[o1k], out=o1[:], in0=o1[:], in1=G2g[r][:], op=ALU.mult)
            P.i("dve", "tensor_tensor", reads=[o1k, xk], writes=[xk], out=xt[:], in0=xt[:], in1=o1[:], op=ALU.add)
            if not final:
                P.d(XN[t0:t0 + 128, :], xt[:], reads=[xk], writes=[P.u("XN%d" % l)])
            else:
                st, sk = st_r.next()
                P.i("act", "activation", reads=[xk], writes=[pf + "junk", sk], out=junk[:], in_=xt[:], func=AF.Square, accum_out=st[:, 0:1])
                P.i("act", "activation", reads=[sk, pf + "eps"], writes=[sk], out=st[:, 1:2], in_=st[:, 0:1], func=AF.Sqrt, bias=epsc[:], scale=1.0 / D)
                P.i("dve", "reciprocal", reads=[sk], writes=[sk], out=st[:, 2:3], in_=st[:, 1:2])
                P.i("dve", "scalar_tensor_tensor", reads=[xk, sk, pf + "fg"], writes=[xk], out=xt[:], in0=xt[:], scalar=st[:, 2:3], in1=fg[:], op0=ALU.mult, op1=ALU.mult)
                P.d(OUT[t0 - LC:t0 - LC + 128, :], xt[:], reads=[xk], writes=[P.u("out")], final=True)
            ti += 1


def rope_tables_np():
    rows = np.repeat(np.arange(L // 64), 64)
    cols = np.tile(np.arange(64), L // 64)
    inv = np.power(np.float32(10000.0), -np.arange(16, dtype=np.float32) / np.float32(16)).astype(np.float32)
    ang = np.stack([rows, cols], -1).astype(np.float32)[..., None] * inv
    cos = np.cos(ang).astype(np.float32).reshape(L, 1, 32)
    sin = np.sin(ang).astype(np.float32).reshape(L, 1, 32)
    return (np.ascontiguousarray(np.broadcast_to(cos, (L, 12, 32)).reshape(L, 384)),
            np.ascontiguousarray(np.broadcast_to(sin, (L, 12, 32)).reshape(L, 384)))


def kmajor(w):
    K, N = w.shape
    return np.ascontiguousarray(w.reshape(K // 128, 128, N).transpose(1, 0, 2))


def core_inputs(inp, b):
    f = np.float32
    m = {}
    m["xin"] = np.ascontiguousarray(np.concatenate([inp["ctx"][b], inp["x"][b]], 0).astype(f))
    cv = np.stack([inp["c"][b], inp["c_ctx"]], -1)
    m["cvec"] = np.ascontiguousarray(cv.reshape(8, 128, 2).transpose(1, 0, 2).astype(f))
    m["ident"] = np.eye(128, dtype=f)
    sh = np.zeros((128, 64), f); sh[64 + np.arange(64), np.arange(64)] = 1.0
    m["shiftm"] = sh
    jj, ii = np.meshgrid(np.arange(128), np.arange(128), indexing="ij")
    wm = np.zeros((128, 2, 2, 128), f)
    wm[:, 0] = (ii <= jj).astype(f)[:, None, :]
    wm[:, 1] = (jj <= ii).astype(f)[:, None, :]
    m["wmask"] = np.ascontiguousarray(wm.reshape(128, 2, 256))
    m["rope_cos"], m["rope_sin"] = rope_tables_np()
    for l in range(2):
        m["ada_w%d" % l] = kmajor(inp["ada_w"][l])
        m["ada_b%d" % l] = np.ascontiguousarray(inp["ada_b"][l].reshape(1, 6144))
        m["w_in%d" % l] = kmajor(inp["w_in"][l][:, PERM])
        m["norm1_g%d" % l] = np.ascontiguousarray(inp["norm1_g"][l].reshape(1, D))
        m["qkg%d" % l] = np.ascontiguousarray(np.concatenate([np.tile(inp["ga_qn_g"][l], 4), np.tile(inp["ga_kn_g"][l], 2)]).reshape(1, 384))
        m["dtb%d" % l] = np.ascontiguousarray(inp["ssd_dt_bias"][l].reshape(1, 8))
        m["sink%d" % l] = np.ascontiguousarray(inp["wa_sink"][l].reshape(1, 4))
        m["w_out%d" % l] = kmajor(inp["w_out"][l])
        m["norm2_g%d" % l] = np.ascontiguousarray(inp["norm2_g"][l].reshape(1, D))
        m["wr%d" % l] = kmajor(np.concatenate([inp["moe_coarse_w"][l], inp["moe_fine_w"][l]], 1))
        m["rb%d" % l] = np.ascontiguousarray(np.concatenate([inp["moe_coarse_b"][l], inp["moe_fine_b"][l]]).reshape(1, 36))
        m["moe_g%d" % l] = np.ascontiguousarray(inp["moe_w_gate"][l].reshape(32, 8, 128, 512).transpose(0, 2, 1, 3))
        m["moe_u%d" % l] = np.ascontiguousarray(inp["moe_w_up"][l].reshape(32, 8, 128, 512).transpose(0, 2, 1, 3))
        m["moe_gu%d" % l] = np.ascontiguousarray(np.concatenate([m["moe_g%d" % l], m["moe_u%d" % l]], -1))
        m["moe_d%d" % l] = np.ascontiguousarray(inp["moe_w_down"][l].reshape(32, 4, 128, 1024).transpose(0, 2, 1, 3))
    m["final_g"] = np.ascontiguousarray(inp["final_g"].reshape(1, D))
    m["ramp"] = (128.0 * np.arange(72) + 1.0).astype(f).reshape(1, 72)
    m["bst"] = (128.0 * np.arange(104)).astype(f).reshape(1, 104)
    m["pidx"] = np.arange(128).astype(f).reshape(128, 1)
    sp, s_ = np.meshgrid(np.arange(128), np.arange(128), indexing="ij")
    m["stri"] = np.ascontiguousarray(np.stack([(sp < s_), np.ones_like(sp, dtype=bool)], 1).astype(f))
    m["tri"] = np.ascontiguousarray(np.stack([(sp <= s_), (sp >= s_)], 1).astype(f))
    for l in range(2):
        Bm = np.zeros((2, 2, 8, 128, 128), f)
        Cm = np.zeros((2, 2, 8, 128, 128), f)
        for d in range(2):
            for j in range(8):
                for gg in range(2):
                    g = 2 * j + gg
                    r0 = (2 * (j % 4) + gg) * 16
                    Bm[d, 0, j, r0:r0 + 16, gg * 64:(gg + 1) * 64] = inp["s5_b_re"][l, d, g].T
                    Bm[d, 1, j, r0:r0 + 16, gg * 64:(gg + 1) * 64] = inp["s5_b_im"][l, d, g].T
                    Cm[d, 0, j, gg * 64:(gg + 1) * 64, r0:r0 + 16] = inp["s5_c_re"][l, d, g].T
                    Cm[d, 1, j, gg * 64:(gg + 1) * 64, r0:r0 + 16] = inp["s5_c_im"][l, d, g].T
        m["s5B%d" % l] = Bm
        m["s5C%d" % l] = Cm
        lamt = np.zeros((128, 3, 16), f)
        for d in range(2):
            for j in range(8):
                for gg in range(2):
                    g = 2 * j + gg
                    lamt[gg * 64:(gg + 1) * 64, 0, d * 8 + j] = inp["s5_lam_re"][l, d, g]
                    lamt[gg * 64:(gg + 1) * 64, 1, d * 8 + j] = inp["s5_lam_im"][l, d, g]
                    lamt[gg * 64:(gg + 1) * 64, 2, d * 8 + j] = inp["s5_log_dt"][l, d, g]
        m["s5lam%d" % l] = lamt
        cw = np.concatenate([inp["ssd_conv_w"][l], inp["ssd_conv_b"][l][None]], 0)
        m["convw%d" % l] = np.ascontiguousarray(cw.reshape(4, 6, 128).transpose(2, 1, 0).astype(f))
        m["alog%d" % l] = np.ascontiguousarray(inp["ssd_a_log"][l].reshape(1, 8))
        m["ssdd%d" % l] = np.ascontiguousarray(inp["ssd_d"][l].reshape(1, 4))
        m["ssdng%d" % l] = np.ascontiguousarray(inp["ssd_norm_g"][l].reshape(1, 256))
        m["s5d%d" % l] = np.ascontiguousarray(inp["s5_d"][l].reshape(2, 128).T.astype(f))
        m["gluw%d" % l] = np.ascontiguousarray(inp["s5_glu_w"][l].reshape(2, 128, 256).transpose(1, 0, 2).astype(f))
        m["glub%d" % l] = np.ascontiguousarray(inp["s5_glu_b"][l].reshape(2, 128).T.astype(f))
    for l in range(0):
        pass
    return m


def attn_consts(P, Dm, pf):
    c = {}
    shift_d = Dm.get("shiftm", [128, 64], kind="ExternalInput")
    c["shift"] = P.sb(pf + "shift", [128, 64], F32R)
    P.d(c["shift"][:], shift_d[:, :], writes=[pf + "shift"], q="pool")
    return c


def phase_c(P, Dm, l, ctx_out):
    pf = "c%d_" % l
    QKT = Dm.get("QKT", [768, T])
    TMS = Dm.get("TMS", [T, 520])
    CAT = Dm.get("CAT", [1024, T])
    cst = attn_consts(P, Dm, pf)
    KT = P.sb(pf + "KT", [64, 2, T], F32R)
    V = P.sb(pf + "V", [128, NT, 2, 128], F32R)
    for kv in range(2):
        P.d(KT[:, kv, :], QKT[256 + kv * 64:256 + (kv + 1) * 64, :], reads=["QKT"], writes=[pf + "KT"], q="pool")
    P.i("dve", "memset", writes=[pf + "V/1"], ap=V[:].bitcast(F32)[:, :, :, 64:128], constant=1.0)
    for kv in range(2):
        P.d(V[:, :, kv, 0:64], TMS[:, kv * 64:(kv + 1) * 64].rearrange("(n p) d -> p n d", p=128), reads=["TMS"], writes=[pf + "V/0%d" % kv], q="pool")
    vkeys = [pf + "V"]
    q_r = Rot(P, pf + "q", [64, 4, 256], F32R, n=2)
    s_r = Rot(P, pf + "s", [128, 1024], n=2, psum=True)
    p_r = Rot(P, pf + "p", [128, 1024], F32R, n=3)
    o_r = Rot(P, pf + "o", [128, 512], n=2, psum=True)
    os_r = Rot(P, pf + "os", [128, 512], F32R, n=2)
    dn_r = Rot(P, pf + "dn", [64, 512], n=1, psum=True)
    rd_r = Rot(P, pf + "rd", [64, 512], n=2)
    ot_r = Rot(P, pf + "ot", [64, 512], n=2)
    qtiles = [(LC + 256 * i, NT) for i in range(L // 256)]
    if ctx_out:
        qtiles = [(0, 2)] + qtiles
    for (q0, nkb) in qtiles:
        qt, qtk = q_r.next()
        P.d(qt[:], QKT[0:256, q0:q0 + 256].rearrange("(h d) q -> d h q", d=64), reads=["QKT"], writes=[qtk], q="pool")
        for kv in range(2):
            o, ok = o_r.next()
            its = list(range(nkb // 2))
            pend = []

            def issue_s(sp):
                sp_, spk = s_r.next()
                for u in range(2):
                    s = 2 * sp + u
                    P.i("pe", "matmul", reads=[pf + "KT", qtk], writes=[spk], out=sp_[:, u * 512:(u + 1) * 512], lhsT=KT[:, kv, s * 128:(s + 1) * 128],
                        rhs=qt[:, 2 * kv:2 * kv + 2, :], start=True, stop=True)
                pt, ptk = p_r.next()
                P.i("act", "activation", reads=[spk], writes=[ptk], out=pt[:], in_=sp_[:], func=AF.Exp, scale=0.125)
                return (sp, pt, ptk)

            LOOK = 1
            for sp in its[:LOOK]:
                pend.append(issue_s(sp))
            for idx, sp in enumerate(its):
                (sp_i, pt, ptk) = pend.pop(0)
                if idx + LOOK < len(its):
                    pend.append(issue_s(its[idx + LOOK]))
                for u in range(2):
                    s_ = 2 * sp_i + u
                    P.i("pe", "matmul", reads=[ptk] + vkeys, writes=[ok], out=o[:], lhsT=V[:, s_, kv, :], rhs=pt[:, u * 512:(u + 1) * 512],
                        start=(idx == 0 and u == 0), stop=(idx == len(its) - 1 and u == 1))
            osb, osk = os_r.next()
            P.i("act", "activation", reads=[ok], writes=[osk], out=osb[:], in_=o[:], func=AF.Copy)
            dn, dnk = dn_r.next()
            P.i("pe", "matmul", reads=[osk, pf + "shift"], writes=[dnk], out=dn[:], lhsT=cst["shift"][:], rhs=osb[:], start=True, stop=True)
            rd, rdk = rd_r.next()
            P.i("dve", "reciprocal", reads=[dnk], writes=[rdk], out=rd[:], in_=dn[:])
            ot, otk = ot_r.next()
            P.i("dve", "tensor_tensor", reads=[osk, rdk], writes=[otk], out=ot[:], in0=osb[0:64, :].bitcast(F32), in1=rd[:], op=ALU.mult)
            P.d(CAT[256 + kv * 128:256 + (kv + 1) * 128, q0:q0 + 256].rearrange("(hh d) q -> d hh q", d=64),
                ot[:].rearrange("d (hh q) -> d hh q", hh=2), reads=[otk], writes=[P.u("CAT")])


def phase_e(P, Dm, l, ctx_out):
    pf = "e%d_" % l
    QKT = Dm.get("QKT", [768, T])
    TMS = Dm.get("TMS", [T, 520])
    CAT = Dm.get("CAT", [1024, T])
    sink_d = Dm.get("sink%d" % l, [1, 4], kind="ExternalInput")
    mask_d = Dm.get("wmask", [128, 2, 256], kind="ExternalInput")
    cst = attn_consts(P, Dm, pf)
    KT = P.sb(pf + "KT", [64, 2, T], F32R)
    V = P.sb(pf + "V", [128, NT, 2, 128], F32R)
    for kv in range(2):
        P.d(KT[:, kv, :], QKT[640 + kv * 64:640 + (kv + 1) * 64, :], reads=["QKT"], writes=[pf + "KT"], q="pool")
    P.i("dve", "memset", writes=[pf + "V/1"], ap=V[:].bitcast(F32)[:, :, :, 64:128], constant=1.0)
    for kv in range(2):
        P.d(V[:, :, kv, 0:64], TMS[:, 128 + kv * 64:128 + (kv + 1) * 64].rearrange("(n p) d -> p n d", p=128), reads=["TMS"], writes=[pf + "V/0%d" % kv], q="pool")
    vkeys = [pf + "V"]
    mask = P.sb(pf + "mask", [128, 2, 256])
    P.d(mask[:], mask_d[:, :, :], writes=[pf + "mask"])
    esk = P.sb(pf + "esk", [64, 4])
    P.d(esk[:], sink_d[0:1, :].to_broadcast([64, 4]), writes=[pf + "esk"])
    P.i("act", "activation", reads=[pf + "esk"], writes=[pf + "esk"], out=esk[:], in_=esk[:], func=AF.Exp)
    q_r = Rot(P, pf + "q", [64, 4, 128], F32R, n=2)
    s_r = Rot(P, pf + "s", [128, 256], n=3, psum=True)
    p_r = Rot(P, pf + "p", [128, 256], F32R, n=3)
    o_r = Rot(P, pf + "o", [128, 256], n=2, psum=True)
    os_r = Rot(P, pf + "os", [128, 256], F32R, n=2)
    dn_r = Rot(P, pf + "dn", [64, 256], n=1, psum=True)
    rd_r = Rot(P, pf + "rd", [64, 256], n=2)
    ot_r = Rot(P, pf + "ot", [64, 256], n=2)
    qtiles = []
    if ctx_out:
        qtiles += [(i, [(0, None), (1, None)]) for i in range(2)]
    for n in range(L // 128):
        ti = 2 + n
        kb = [(0, None), (1, None)]
        if n > 0:
            kb.append((ti - 1, 0))
        kb.append((ti, None))
        if n < L // 128 - 1:
            kb.append((ti + 1, 1))
        qtiles.append((ti, kb))
    for (ti, kbs) in qtiles:
        q0 = ti * 128
        qt, qtk = q_r.next()
        P.d(qt[:], QKT[384:640, q0:q0 + 128].rearrange("(h d) q -> d h q", d=64), reads=["QKT"], writes=[qtk], q="pool")
        for kv in range(2):
            o, ok = o_r.next()
            for idx, (s, mi) in enumerate(kbs):
                sp_, spk = s_r.next()
                P.i("pe", "matmul", reads=[pf + "KT", qtk], writes=[spk], out=sp_[:], lhsT=KT[:, kv, s * 128:(s + 1) * 128],
                    rhs=qt[:, 2 * kv:2 * kv + 2, :], start=True, stop=True)
                pt, ptk = p_r.next()
                P.i("act", "activation", reads=[spk], writes=[ptk], out=pt[:], in_=sp_[:], func=AF.Exp, scale=0.125)
                if mi is not None:
                    P.i("dve", "tensor_tensor", reads=[ptk, pf + "mask"], writes=[ptk], out=pt[:], in0=pt[:].bitcast(F32), in1=mask[:, mi, :], op=ALU.mult)
                P.i("pe", "matmul", reads=[ptk] + vkeys, writes=[ok], out=o[:], lhsT=V[:, s, kv, :], rhs=pt[:],
                    start=(idx == 0), stop=(idx == len(kbs) - 1))
            osb, osk = os_r.next()
            P.i("act", "activation", reads=[ok], writes=[osk], out=osb[:], in_=o[:], func=AF.Copy)
            dn, dnk = dn_r.next()
            P.i("pe", "matmul", reads=[osk, pf + "shift"], writes=[dnk], out=dn[:], lhsT=cst["shift"][:], rhs=osb[:], start=True, stop=True)
            rd, rdk = rd_r.next()
            for hh in range(2):
                h = 2 * kv + hh
                P.i("dve", "tensor_scalar", reads=[dnk, pf + "esk"], writes=[rdk + "/%d" % hh], out=rd[:, hh * 128:(hh + 1) * 128], in0=dn[:, hh * 128:(hh + 1) * 128],
                    scalar1=esk[:, h:h + 1], scalar2=None, op0=ALU.add)
            P.i("dve", "reciprocal", reads=[rdk], writes=[rdk], out=rd[:], in_=rd[:])
            ot, otk = ot_r.next()
            P.i("dve", "tensor_tensor", reads=[osk, rdk], writes=[otk], out=ot[:], in0=osb[0:64, :].bitcast(F32), in1=rd[:], op=ALU.mult)
            P.d(CAT[768 + kv * 128:768 + (kv + 1) * 128, q0:q0 + 128].rearrange("(hh d) q -> d hh q", d=64),
                ot[:].rearrange("d (hh q) -> d hh q", hh=2), reads=[otk], writes=[P.u("CAT")])


BIG = 1.0e30
S5_ENG2 = "dve"


def phase_f(P, Dm, l, src_name, tok_chunks=None):
    pf = "f%d_" % l
    xsrc = Dm.get(src_name, [T, D])
    CAT = Dm.get("CAT", [1024, T])
    w_out = Dm.get("w_out%d" % l, [128, 8, 1024], kind="ExternalInput")
    modrow = Dm.get("modrow%d" % l, [2, 6144])
    n2g = Dm.get("norm2_g%d" % l, [1, D], kind="ExternalInput")
    wr_d = Dm.get("wr%d" % l, [128, 8, 36], kind="ExternalInput")
    rb_d = Dm.get("rb%d" % l, [1, 36], kind="ExternalInput")
    ident_d = Dm.get("ident", [128, 128], kind="ExternalInput")
    X1 = Dm.get("X1", [T, D])
    H2T = Dm.get("H2T", [D, T])
    WTd = Dm.get("WTd", [32, T])
    mk = "modrow%d" % l
    ident = P.sb(pf + "ident", [128, 128])
    P.d(ident[:], ident_d[:, :], writes=[pf + "ident"])
    wo = P.sb(pf + "wo", [128, 8, 1024], F32R)
    for k in range(8):
        P.d(wo[:, k, :], w_out[:, k, :], writes=[pf + "wo/%d" % k], q="pool")
    wr = P.sb(pf + "wr", [128, 8, 36])
    P.d(wr[:], wr_d[:, :, :], writes=[pf + "wr"])
    rb = P.sb(pf + "rb", [128, 36])
    load_bcast(P, rb[:], pf + "rb", rb_d[0:1, :])
    G1 = [P.sb(pf + "G1%d" % r, [128, D]) for r in range(2)]
    G2 = [P.sb(pf + "G2%d" % r, [128, D]) for r in range(2)]
    SH2 = [P.sb(pf + "SH2%d" % r, [128, D]) for r in range(2)]
    gn = P.sb(pf + "gn", [128, D])
    load_bcast(P, gn[:], pf + "gn", n2g[0:1, :])
    for r in range(2):
        load_bcast(P, G1[r][:], pf + "G1%d" % r, modrow[r:r + 1, 2048:3072], reads=[mk])
        load_bcast(P, SH2[r][:], pf + "SH2%d" % r, modrow[r:r + 1, 3072:4096], reads=[mk])
        load_bcast(P, G2[r][:], pf + "G2%d" % r, modrow[r:r + 1, 4096:5120], reads=[mk])
        P.i("dve", "scalar_tensor_tensor", reads=[pf + "G2%d" % r, pf + "gn"], writes=[pf + "G2%d" % r], out=G2[r][:], in0=G2[r][:], scalar=1.0, in1=gn[:], op0=ALU.add, op1=ALU.mult)
    epsc = P.sb(pf + "eps", [128, 1])
    P.i("dve", "memset", writes=[pf + "eps"], ap=epsc[:], constant=EPS)
    ct_r = Rot(P, pf + "ct", [128, 8, 512], F32R, n=2)
    po_r = Rot(P, pf + "po", [128, 1024], n=1, psum=True)
    xt_r = Rot(P, pf + "xt", [128, D], n=2)
    x1_r = Rot(P, pf + "x1", [128, D], n=2)
    h_r = Rot(P, pf + "h", [128, D], n=2)
    junk = P.sb(pf + "junk", [128, D])
    st_r = Rot(P, pf + "st", [128, 64], n=3)
    tp_r = Rot(P, pf + "tp", [128, 1024], n=1, psum=True)
    hT_r = Rot(P, pf + "hT", [128, 8, 128], F32R, n=2)
    hTf_r = Rot(P, pf + "hTf", [128, 8, 128], n=2)
    pr_r = Rot(P, pf + "pr", [128, 512], n=1, psum=True)
    lg_r = Rot(P, pf + "lg", [128, 36], n=2)
    mk_r = Rot(P, pf + "mk", [128, 4, 8], n=2)
    oh_r = Rot(P, pf + "oh", [128, 3, 32], n=2)
    wt_r = Rot(P, pf + "wt", [128, 32], n=2)
    pw_r = Rot(P, pf + "pw", [128, 512], n=1, psum=True)
    wT_r = Rot(P, pf + "wT", [32, 128], n=2)
    for (c0, cn) in (tok_chunks or CHUNKS):
        ct, ctk = ct_r.next()
        P.d(ct[:, :, 0:cn], CAT[:, c0:c0 + cn].rearrange("(k p) t -> p k t", p=128), reads=["CAT"], writes=[ctk], q="pool")
        for i in range(cn // 128):
            t0 = c0 + i * 128
            r = 1 if t0 < LC else 0
            po, pok = po_r.next()
            for hf in range(2):
                for k in range(8):
                    P.i("pe", "matmul", reads=[ctk, pf + "wo/%d" % k], writes=[pok], out=po[:, hf * 512:(hf + 1) * 512], lhsT=ct[:, k, i * 128:(i + 1) * 128],
                        rhs=wo[:, k, hf * 512:(hf + 1) * 512], start=(k == 0), stop=(k == 7))
            xt, xk = xt_r.next()
            P.d(xt[:], xsrc[t0:t0 + 128, :], reads=[src_name], writes=[xk])
            x1, x1k = x1_r.next()
            for hf in range(2):
                sl = slice(hf * 512, (hf + 1) * 512)
                P.i("dve", "tensor_tensor", reads=[pok, pf + "G1%d" % r], writes=[x1k + "/%d" % hf], out=x1[:, sl], in0=po[:, sl], in1=G1[r][:, sl], op=ALU.mult)
            P.i("pool", "tensor_tensor", reads=[x1k, xk], writes=[x1k], out=x1[:], in0=x1[:], in1=xt[:], op=ALU.add)
            P.d(X1[t0:t0 + 128, :], x1[:], reads=[x1k], writes=[P.u("X1")])
            st, sk = st_r.next()
            P.i("act", "activation", reads=[x1k], writes=[pf + "junk", sk], out=junk[:], in_=x1[:], func=AF.Square, accum_out=st[:, 0:1])
            P.i("act", "activation", reads=[sk, pf + "eps"], writes=[sk], out=st[:, 1:2], in_=st[:, 0:1], func=AF.Sqrt, bias=epsc[:], scale=1.0 / D)
            P.i("dve", "reciprocal", reads=[sk], writes=[sk], out=st[:, 2:3], in_=st[:, 1:2])
            h, hk = h_r.next()
            P.i("dve", "scalar_tensor_tensor", reads=[x1k, sk, pf + "G2%d" % r], writes=[hk], out=h[:], in0=x1[:], scalar=st[:, 2:3], in1=G2[r][:], op0=ALU.mult, op1=ALU.mult)
            P.i("pool", "tensor_tensor", reads=[hk, pf + "SH2%d" % r], writes=[hk], out=h[:], in0=h[:], in1=SH2[r][:], op=ALU.add)
            tp, tpk = tp_r.next()
            for k in range(8):
                P.i("pe", "transpose", reads=[hk, pf + "ident"], writes=[tpk], out=tp[:, k * 128:(k + 1) * 128], in_=h[:, k * 128:(k + 1) * 128], identity=ident[:])
            hT, hTk = hT_r.next()
            hTf, hTfk = hTf_r.next()
            P.i("act", "activation", reads=[tpk], writes=[hTk], out=hT[:], in_=tp[:].rearrange("p (k t) -> p k t", k=8), func=AF.Copy)
            P.i("dve", "tensor_copy", reads=[tpk], writes=[hTfk], out=hTf[:], in_=tp[:].rearrange("p (k t) -> p k t", k=8))
            P.d(H2T[:, t0:t0 + 128].rearrange("(k p) t -> p k t", p=128), hT[:].bitcast(F32), reads=[hTk], writes=[P.u("H2T")])
            pr, prk = pr_r.next()
            for k in range(8):
                P.i("pe", "matmul", reads=[hTfk, pf + "wr"], writes=[prk], out=pr[:, 0:36], lhsT=hTf[:, k, :], rhs=wr[:, k, :], start=(k == 0), stop=(k == 7))
            lg, lgk = lg_r.next()
            P.i("dve", "tensor_tensor", reads=[prk, pf + "rb"], writes=[lgk], out=lg[:], in0=pr[:, 0:36], in1=rb[:], op=ALU.add)
            P.i("dve", "tensor_reduce", reads=[lgk], writes=[sk], out=st[:, 4:5], in_=lg[:, 0:4], axis=AX.X, op=ALU.max)
            P.i("dve", "tensor_scalar", reads=[sk], writes=[sk], out=st[:, 5:6], in0=st[:, 4:5], scalar1=-1.0, scalar2=None, op0=ALU.mult)
            P.i("act", "activation", reads=[lgk, sk], writes=[sk], out=st[:, 32:36], in_=lg[:, 0:4], func=AF.Exp, bias=st[:, 5:6], accum_out=st[:, 6:7])
            P.i("dve", "reciprocal", reads=[sk], writes=[sk], out=st[:, 7:8], in_=st[:, 6:7])
            P.i("dve", "tensor_scalar", reads=[lgk, sk], writes=[sk], out=st[:, 8:12], in0=lg[:, 0:4], scalar1=st[:, 4:5], scalar2=None, op0=ALU.is_equal)
            P.i("dve", "tensor_scalar", reads=[sk], writes=[sk], out=st[:, 12:16], in0=st[:, 8:12], scalar1=BIG, scalar2=-BIG, op0=ALU.mult, op1=ALU.add)
            mkd, mkk = mk_r.next()
            P.i("dve", "tensor_tensor", reads=[lgk, sk], writes=[mkk], out=mkd[:], in0=lg[:, 4:36].rearrange("p (g e) -> p g e", g=4),
                in1=st[:, 12:16].unsqueeze(2).to_broadcast([128, 4, 8]), op=ALU.add)
            mflat = mkd[:].rearrange("p g e -> p (g e)")
            P.i("dve", "max", reads=[mkk], writes=[sk], out=st[:, 16:24], in_=mflat)
            oh, ohk = oh_r.next()
            P.i("dve", "tensor_scalar", reads=[mkk, sk], writes=[ohk + "/1"], out=oh[:, 0, :], in0=mflat, scalar1=st[:, 16:17], scalar2=None, op0=ALU.is_equal)
            P.i("dve", "tensor_scalar", reads=[mkk, sk], writes=[ohk + "/2"], out=oh[:, 1, :], in0=mflat, scalar1=st[:, 17:18], scalar2=None, op0=ALU.is_equal)
            P.i("dve", "tensor_tensor", reads=[sk], writes=[sk], out=st[:, 24:25], in0=st[:, 17:18], in1=st[:, 16:17], op=ALU.subtract)
            P.i("act", "activation", reads=[sk], writes=[sk], out=st[:, 25:26], in_=st[:, 24:25], func=AF.Exp)
            P.i("dve", "tensor_scalar", reads=[sk], writes=[sk], out=st[:, 26:27], in0=st[:, 25:26], scalar1=1.0, scalar2=None, op0=ALU.add)
            P.i("dve", "reciprocal", reads=[sk], writes=[sk], out=st[:, 27:28], in_=st[:, 26:27])
            P.i("dve", "tensor_tensor", reads=[sk], writes=[sk], out=st[:, 28:29], in0=st[:, 27:28], in1=st[:, 7:8], op=ALU.mult)
            P.i("dve", "tensor_tensor", reads=[sk], writes=[sk], out=st[:, 29:30], in0=st[:, 7:8], in1=st[:, 28:29], op=ALU.subtract)
            P.i("dve", "tensor_scalar", reads=[ohk + "/1", sk], writes=[ohk + "/3"], out=oh[:, 2, :], in0=oh[:, 0, :], scalar1=st[:, 28:29], scalar2=None, op0=ALU.mult)
            wt, wtk = wt_r.next()
            P.i("dve", "scalar_tensor_tensor", reads=[ohk + "/2", ohk + "/3", sk], writes=[wtk], out=wt[:], in0=oh[:, 1, :], scalar=st[:, 29:30], in1=oh[:, 2, :], op0=ALU.mult, op1=ALU.add)
            pw, pwk = pw_r.next()
            P.i("pe", "transpose", reads=[wtk, pf + "ident"], writes=[pwk], out=pw[0:32, 0:128], in_=wt[:], identity=ident[:])
            wT, wTk = wT_r.next()
            P.i("act", "activation", reads=[pwk], writes=[wTk], out=wT[:], in_=pw[0:32, 0:128], func=AF.Copy)
            P.d(WTd[:, t0:t0 + 128], wT[:], reads=[wTk], writes=[P.u("WTd")])


def phase_f2(P, Dm, l, src_name, tok_chunks=None):
    pf = "F%d_" % l
    xsrc = Dm.get(src_name, [T, D])
    CAT = Dm.get("CAT", [1024, T])
    w_out = Dm.get("w_out%d" % l, [128, 8, 1024], kind="ExternalInput")
    modrow = Dm.get("modrow%d" % l, [2, 6144])
    n2g = Dm.get("norm2_g%d" % l, [1, D], kind="ExternalInput")
    wr_d = Dm.get("wr%d" % l, [128, 8, 36], kind="ExternalInput")
    rb_d = Dm.get("rb%d" % l, [1, 36], kind="ExternalInput")
    ident_d = Dm.get("ident", [128, 128], kind="ExternalInput")
    X1 = Dm.get("X1", [T, D])
    chunks_ = (tok_chunks or CHUNKS)
    ntf = sum(cn for _, cn in chunks_) // 128
    NB = (2 * ntf * 128) // 128 + 32
    BUF = Dm.get("BUF%d" % l, [NB * 128, D])
    IDXW = Dm.get("IDXW%d" % l, [128, NB], I32)
    DEST = Dm.get("DEST%d" % l, [128, ntf * 2], I32)
    WWd = Dm.get("WWd%d" % l, [128, ntf * 2])
    ramp_d = Dm.get("ramp", [1, 72], kind="ExternalInput")
    bst_d = Dm.get("bst", [1, 104], kind="ExternalInput")
    pidx_d = Dm.get("pidx", [128, 1], kind="ExternalInput")
    stri_d = Dm.get("stri", [128, 2, 128], kind="ExternalInput")
    OH = P.sb(pf + "OH", [128, ntf, 2, 32])
    WW = P.sb(pf + "WW", [128, ntf, 2])
    RK = P.sb(pf + "RK", [128, ntf, 32])
    Msum = P.sb(pf + "Msum", [128, 32])
    P.i("dve", "memset", writes=[pf + "Msum"], ap=Msum[:], constant=0.0)
    stri = P.sb(pf + "stri", [128, 2, 128]); P.d(stri[:], stri_d[:, :, :], writes=[pf + "stri"])
    Mt_r = Rot(P, pf + "Mt", [128, 32], n=2)
    zz = P.sb(pf + "zz", [128, 4096])
    P.i("pool", "memset", writes=[pf + "zz"], ap=zz[:], constant=0.0)
    rows_per = 128 * 4
    for r0 in range(0, NB * 128, rows_per):
        P.d(BUF[r0:r0 + rows_per, :].rearrange("(p a) n -> p (a n)", p=128), zz[:], reads=[pf + "zz"], writes=[P.u("BUFz%d" % l)])
    mk = "modrow%d" % l
    ident = P.sb(pf + "ident", [128, 128])
    P.d(ident[:], ident_d[:, :], writes=[pf + "ident"])
    wo = P.sb(pf + "wo", [128, 8, 1024], F32R)
    for k in range(8):
        P.d(wo[:, k, :], w_out[:, k, :], writes=[pf + "wo/%d" % k], q="pool")
    wr = P.sb(pf + "wr", [128, 8, 36])
    P.d(wr[:], wr_d[:, :, :], writes=[pf + "wr"])
    rb = P.sb(pf + "rb", [128, 36])
    load_bcast(P, rb[:], pf + "rb", rb_d[0:1, :])
    G1 = [P.sb(pf + "G1%d" % r, [128, D]) for r in range(2)]
    G2 = [P.sb(pf + "G2%d" % r, [128, D]) for r in range(2)]
    SH2 = [P.sb(pf + "SH2%d" % r, [128, D]) for r in range(2)]
    gn = P.sb(pf + "gn", [128, D])
    load_bcast(P, gn[:], pf + "gn", n2g[0:1, :])
    for r in range(2):
        load_bcast(P, G1[r][:], pf + "G1%d" % r, modrow[r:r + 1, 2048:3072], reads=[mk])
        load_bcast(P, SH2[r][:], pf + "SH2%d" % r, modrow[r:r + 1, 3072:4096], reads=[mk])
        load_bcast(P, G2[r][:], pf + "G2%d" % r, modrow[r:r + 1, 4096:5120], reads=[mk])
        P.i("dve", "scalar_tensor_tensor", reads=[pf + "G2%d" % r, pf + "gn"], writes=[pf + "G2%d" % r], out=G2[r][:], in0=G2[r][:], scalar=1.0, in1=gn[:], op0=ALU.add, op1=ALU.mult)
    epsc = P.sb(pf + "eps", [128, 1])
    P.i("dve", "memset", writes=[pf + "eps"], ap=epsc[:], constant=EPS)
    ct_r = Rot(P, pf + "ct", [128, 8, 512], F32R, n=2)
    po_r = Rot(P, pf + "po", [128, 1024], n=1, psum=True)
    xt_r = Rot(P, pf + "xt", [128, D], n=2)
    x1_r = Rot(P, pf + "x1", [128, D], n=2)
    h_r = Rot(P, pf + "h", [128, D], n=2)
    junk = P.sb(pf + "junk", [128, D])
    st_r = Rot(P, pf + "st", [128, 64], n=3)
    tp_r = Rot(P, pf + "tp", [128, 1024], n=1, psum=True)
    H2 = Dm.get("H2_%d" % l, [T, D])
    hTf_r = Rot(P, pf + "hTf", [128, 8, 128], n=2)
    pr_r = Rot(P, pf + "pr", [128, 512], n=1, psum=True)
    lg_r = Rot(P, pf + "lg", [128, 36], n=2)
    mk_r = Rot(P, pf + "mk", [128, 4, 8], n=2)
    oh_r = Rot(P, pf + "oh", [128, 3, 32], n=2)
    pw_r = Rot(P, pf + "pw", [128, 512], n=1, psum=True)
    tile_i = 0
    for (c0, cn) in chunks_:
        ct, ctk = ct_r.next()
        P.d(ct[:, :, 0:cn], CAT[:, c0:c0 + cn].rearrange("(k p) t -> p k t", p=128), reads=["CAT"], writes=[ctk], q="pool")
        for i in range(cn // 128):
            t0 = c0 + i * 128
            r = 1 if t0 < LC else 0
            po, pok = po_r.next()
            for hf in range(2):
                for k in range(8):
                    P.i("pe", "matmul", reads=[ctk, pf + "wo/%d" % k], writes=[pok], out=po[:, hf * 512:(hf + 1) * 512], lhsT=ct[:, k, i * 128:(i + 1) * 128],
                        rhs=wo[:, k, hf * 512:(hf + 1) * 512], start=(k == 0), stop=(k == 7))
            xt, xk = xt_r.next()
            P.d(xt[:], xsrc[t0:t0 + 128, :], reads=[src_name], writes=[xk])
            x1, x1k = x1_r.next()
            for hf in range(2):
                sl = slice(hf * 512, (hf + 1) * 512)
                P.i("dve", "tensor_tensor", reads=[pok, pf + "G1%d" % r], writes=[x1k + "/%d" % hf], out=x1[:, sl], in0=po[:, sl], in1=G1[r][:, sl], op=ALU.mult)
            P.i("pool", "tensor_tensor", reads=[x1k, xk], writes=[x1k], out=x1[:], in0=x1[:], in1=xt[:], op=ALU.add)
            P.d(X1[t0:t0 + 128, :], x1[:], reads=[x1k], writes=[P.u("X1")])
            st, sk = st_r.next()
            P.i("act", "activation", reads=[x1k], writes=[pf + "junk", sk], out=junk[:], in_=x1[:], func=AF.Square, accum_out=st[:, 0:1])
            P.i("act", "activation", reads=[sk, pf + "eps"], writes=[sk], out=st[:, 1:2], in_=st[:, 0:1], func=AF.Sqrt, bias=epsc[:], scale=1.0 / D)
            P.i("dve", "reciprocal", reads=[sk], writes=[sk], out=st[:, 2:3], in_=st[:, 1:2])
            h, hk = h_r.next()
            P.i("dve", "scalar_tensor_tensor", reads=[x1k, sk, pf + "G2%d" % r], writes=[hk], out=h[:], in0=x1[:], scalar=st[:, 2:3], in1=G2[r][:], op0=ALU.mult, op1=ALU.mult)
            P.i("pool", "tensor_tensor", reads=[hk, pf + "SH2%d" % r], writes=[hk], out=h[:], in0=h[:], in1=SH2[r][:], op=ALU.add)
            P.d(H2[t0:t0 + 128, :], h[:], reads=[hk], writes=[P.u("H2_%d" % l)])
            tp, tpk = tp_r.next()
            for k in range(8):
                P.i("pe", "transpose", reads=[hk, pf + "ident"], writes=[tpk], out=tp[:, k * 128:(k + 1) * 128], in_=h[:, k * 128:(k + 1) * 128], identity=ident[:])
            hTf, hTfk = hTf_r.next()
            P.i("dve", "tensor_copy", reads=[tpk], writes=[hTfk], out=hTf[:], in_=tp[:].rearrange("p (k t) -> p k t", k=8))
            pr, prk = pr_r.next()
            for k in range(8):
                P.i("pe", "matmul", reads=[hTfk, pf + "wr"], writes=[prk], out=pr[:, 0:36], lhsT=hTf[:, k, :], rhs=wr[:, k, :], start=(k == 0), stop=(k == 7))
            lg, lgk = lg_r.next()
            P.i("dve", "tensor_tensor", reads=[prk, pf + "rb"], writes=[lgk], out=lg[:], in0=pr[:, 0:36], in1=rb[:], op=ALU.add)
            P.i("dve", "tensor_reduce", reads=[lgk], writes=[sk], out=st[:, 4:5], in_=lg[:, 0:4], axis=AX.X, op=ALU.max)
            P.i("dve", "tensor_scalar", reads=[sk], writes=[sk], out=st[:, 5:6], in0=st[:, 4:5], scalar1=-1.0, scalar2=None, op0=ALU.mult)
            P.i("act", "activation", reads=[lgk, sk], writes=[sk], out=st[:, 32:36], in_=lg[:, 0:4], func=AF.Exp, bias=st[:, 5:6], accum_out=st[:, 6:7])
            P.i("dve", "reciprocal", reads=[sk], writes=[sk], out=st[:, 7:8], in_=st[:, 6:7])
            P.i("dve", "tensor_scalar", reads=[lgk, sk], writes=[sk], out=st[:, 8:12], in0=lg[:, 0:4], scalar1=st[:, 4:5], scalar2=None, op0=ALU.is_equal)
            P.i("dve", "tensor_scalar", reads=[sk], writes=[sk], out=st[:, 12:16], in0=st[:, 8:12], scalar1=BIG, scalar2=-BIG, op0=ALU.mult, op1=ALU.add)
            mkd, mkk = mk_r.next()
            P.i("dve", "tensor_tensor", reads=[lgk, sk], writes=[mkk], out=mkd[:], in0=lg[:, 4:36].rearrange("p (g e) -> p g e", g=4),
                in1=st[:, 12:16].unsqueeze(2).to_broadcast([128, 4, 8]), op=ALU.add)
            mflat = mkd[:].rearrange("p g e -> p (g e)")
            P.i("dve", "max", reads=[mkk], writes=[sk], out=st[:, 16:24], in_=mflat)
            oh, ohk = oh_r.next()
            P.i("dve", "tensor_scalar", reads=[mkk, sk], writes=[ohk + "/1"], out=oh[:, 0, :], in0=mflat, scalar1=st[:, 16:17], scalar2=None, op0=ALU.is_equal)
            P.i("dve", "tensor_scalar", reads=[mkk, sk], writes=[ohk + "/2"], out=oh[:, 1, :], in0=mflat, scalar1=st[:, 17:18], scalar2=None, op0=ALU.is_equal)
            P.i("dve", "tensor_tensor", reads=[sk], writes=[sk], out=st[:, 24:25], in0=st[:, 17:18], in1=st[:, 16:17], op=ALU.subtract)
            P.i("act", "activation", reads=[sk], writes=[sk], out=st[:, 25:26], in_=st[:, 24:25], func=AF.Exp)
            P.i("dve", "tensor_scalar", reads=[sk], writes=[sk], out=st[:, 26:27], in0=st[:, 25:26], scalar1=1.0, scalar2=None, op0=ALU.add)
            P.i("dve", "reciprocal", reads=[sk], writes=[sk], out=st[:, 27:28], in_=st[:, 26:27])
            P.i("dve", "tensor_tensor", reads=[sk], writes=[sk], out=st[:, 28:29], in0=st[:, 27:28], in1=st[:, 7:8], op=ALU.mult)
            P.i("dve", "tensor_tensor", reads=[sk], writes=[sk], out=st[:, 29:30], in0=st[:, 7:8], in1=st[:, 28:29], op=ALU.subtract)
            ti = tile_i
            tile_i += 1
            P.i("act", "activation", reads=[ohk + "/1"], writes=[pf + "OH/%d_0" % ti], out=OH[:, ti, 0, :], in_=oh[:, 0, :], func=AF.Copy)
            P.i("act", "activation", reads=[ohk + "/2"], writes=[pf + "OH/%d_1" % ti], out=OH[:, ti, 1, :], in_=oh[:, 1, :], func=AF.Copy)
            P.i("act", "activation", reads=[sk], writes=[pf + "WW/%d" % ti], out=WW[:, ti, :], in_=st[:, 28:30], func=AF.Copy)
            Mt, Mtk = Mt_r.next()
            P.i("dve", "tensor_tensor", reads=[ohk + "/1", ohk + "/2"], writes=[Mtk], out=Mt[:], in0=oh[:, 0, :], in1=oh[:, 1, :], op=ALU.add)
            pw, pwk = pw_r.next()
            P.i("pe", "matmul", reads=[Mtk, pf + "stri"], writes=[pwk], out=pw[:, 0:32], lhsT=stri[:, 0, :], rhs=Mt[:], start=True, stop=False)
            P.i("pe", "matmul", reads=[pf + "Msum", pf + "stri"], writes=[pwk], out=pw[:, 0:32], lhsT=stri[:, 1, :], rhs=Msum[:], start=False, stop=True)
            P.i("act", "activation", reads=[pwk], writes=[pf + "RK/%d" % ti], out=RK[:, ti, :], in_=pw[:, 0:32], func=AF.Copy)
            P.i("dve", "tensor_tensor", reads=[Mtk, pf + "Msum"], writes=[pf + "Msum"], out=Msum[:], in0=Msum[:], in1=Mt[:], op=ALU.add)
    ramp = P.sb(pf + "ramp", [128, 72]); load_bcast(P, ramp[:], pf + "ramp", ramp_d[0:1, :])
    bst = P.sb(pf + "bst", [128, 104]); load_bcast(P, bst[:], pf + "bst", bst_d[0:1, :])
    pidx = P.sb(pf + "pidx", [128, 1]); P.d(pidx[:], pidx_d[:, :], writes=[pf + "pidx"])
    q = P.sb(pf + "q", [128, 8, 32])
    QK = pf + "q"
    big = P.sb(pf + "big", [128, 104 * 32])
    ones32 = P.sb(pf + "ones32", [128, 32]); P.i("dve", "memset", writes=[pf + "ones32"], ap=ones32[:], constant=1.0)
    pw, pwk = pw_r.next()
    P.i("pe", "matmul", reads=[pf + "Msum", pf + "stri"], writes=[pwk], out=pw[:, 0:32], lhsT=stri[:, 1, :], rhs=Msum[:], start=True, stop=True)
    P.i("dve", "tensor_copy", reads=[pwk], writes=[QK], out=q[:, 0, :], in_=pw[:, 0:32])
    NM = 2 * ntf
    b3 = big[:, 0:32 * NM].rearrange("p (e m) -> p e m", e=32)
    P.i("dve", "tensor_tensor", reads=[QK, pf + "ramp"], writes=[pf + "big"], out=b3, in0=q[:, 0, :].unsqueeze(2).to_broadcast([128, 32, NM]),
        in1=ramp[:, 0:NM].unsqueeze(1).to_broadcast([128, 32, NM]), op=ALU.is_ge)
    P.i("dve", "tensor_reduce", reads=[pf + "big"], writes=[QK], out=q[:, 1, :], in_=b3, axis=AX.X, op=ALU.add)
    P.i("dve", "tensor_scalar", reads=[QK], writes=[QK], out=q[:, 1, :], in0=q[:, 1, :], scalar1=128.0, scalar2=None, op0=ALU.mult)
    P.i("dve", "tensor_tensor_scan", reads=[QK, pf + "ones32"], writes=[QK], out=q[:, 2, :], data0=ones32[:], data1=q[:, 1, :], initial=0.0, op0=ALU.mult, op1=ALU.add)
    P.i("dve", "tensor_tensor", reads=[QK], writes=[QK], out=q[:, 3, :], in0=q[:, 2, :], in1=q[:, 1, :], op=ALU.subtract)
    be = P.sb(pf + "be", [128, 104])
    b3 = big[:, 0:NB * 32].rearrange("p (b e) -> p b e", e=32)
    P.i("dve", "tensor_tensor", reads=[QK, pf + "bst"], writes=[pf + "big"], out=b3, in0=q[:, 2, :].unsqueeze(1).to_broadcast([128, NB, 32]),
        in1=bst[:, 0:NB].unsqueeze(2).to_broadcast([128, NB, 32]), op=ALU.is_le)
    P.i("dve", "tensor_reduce", reads=[pf + "big"], writes=[pf + "be"], out=be[:, 0:NB], in_=b3, axis=AX.X, op=ALU.add)
    P.i("dve", "tensor_scalar", reads=[pf + "be"], writes=[pf + "be"], out=be[:, 0:NB], in0=be[:, 0:NB], scalar1=31.0, scalar2=None, op0=ALU.min)
    same = P.sb(pf + "same", [128, 104])
    P.i("dve", "memset", writes=[pf + "same"], ap=same[:, 0:1], constant=0.0)
    P.i("dve", "tensor_tensor", reads=[pf + "be"], writes=[pf + "same"], out=same[:, 1:NB], in0=be[:, 1:NB], in1=be[:, 0:NB - 1], op=ALU.is_equal)
    P.i("dve", "tensor_scalar", reads=[pf + "be", pf + "pidx"], writes=[pf + "be"], out=be[:, 0:NB], in0=be[:, 0:NB], scalar1=128.0, scalar2=pidx[:, 0:1], op0=ALU.mult, op1=ALU.add)
    P.i("dve", "scalar_tensor_tensor", reads=[pf + "be", pf + "same"], writes=[pf + "be"], out=be[:, 0:NB], in0=same[:, 0:NB], scalar=1048576.0, in1=be[:, 0:NB], op0=ALU.mult, op1=ALU.add)
    bei = P.sb(pf + "bei", [128, 104], I32)
    P.i("dve", "tensor_copy", reads=[pf + "be"], writes=[pf + "bei"], out=bei[:, 0:NB], in_=be[:, 0:NB])
    P.d(IDXW[:, :], bei[:, 0:NB], reads=[pf + "bei"], writes=[P.u("IDXW%d" % l)])
    dsf = P.sb(pf + "dsf", [128, ntf, 2])
    pos_r = Rot(P, pf + "pos", [128, 2, 32], n=2)
    for ti in range(ntf):
        pos, posk = pos_r.next()
        P.i("dve", "tensor_tensor", reads=[pf + "RK/%d" % ti, QK], writes=[posk + "/p"], out=pos[:, 0, :], in0=RK[:, ti, :], in1=q[:, 3, :], op=ALU.add)
        for k in range(2):
            P.i("dve", "tensor_tensor", reads=[posk + "/p", pf + "OH/%d_%d" % (ti, k)], writes=[posk + "/t"], out=pos[:, 1, :], in0=pos[:, 0, :], in1=OH[:, ti, k, :], op=ALU.mult)
            P.i("dve", "tensor_reduce", reads=[posk + "/t"], writes=[pf + "dsf/%d_%d" % (ti, k)], out=dsf[:, ti, k:k + 1], in_=pos[:, 1, :], axis=AX.X, op=ALU.add)
    dsi = P.sb(pf + "dsi", [128, ntf * 2], I32)
    P.i("dve", "tensor_copy", reads=[pf + "dsf"], writes=[pf + "dsi"], out=dsi[:], in_=dsf[:].rearrange("p t k -> p (t k)"))
    P.d(DEST[:, :], dsi[:], reads=[pf + "dsi"], writes=[P.u("DEST%d" % l)])
    P.d(WWd[:, :], WW[:].rearrange("p t k -> p (t k)"), reads=[pf + "WW"], writes=[P.u("WWd%d" % l)])
    h2_r = Rot(P, pf + "h2s", [128, D], n=3)
    ti = 0
    for (c0, cn) in chunks_:
        for i in range(cn // 128):
            t0 = c0 + i * 128
            h2, h2k = h2_r.next()
            P.d(h2[:], H2[t0:t0 + 128, :], reads=["H2_%d" % l], writes=[h2k])
            for k in range(2):
                P.dma(lambda e, h2=h2, col=ti * 2 + k: e.indirect_dma_start(out=BUF[:, :], out_offset=bass.IndirectOffsetOnAxis(ap=dsi[:, col:col + 1], axis=0), in_=h2[:], in_offset=None),
                      reads=[h2k, pf + "dsi", "BUFz%d" % l], writes=[P.u("BUF%d" % l)], q="pool")
            ti += 1


def phase_g(P, Dm, l, tok_chunks, final):
    pf = "g%d_" % l
    X1 = Dm.get("X1", [T, D])
    H2T = Dm.get("H2T", [D, T])
    WTd = Dm.get("WTd", [32, T])
    WG = Dm.get("moe_g%d" % l, [32, 128, 8, 512], kind="ExternalInput")
    WU = Dm.get("moe_u%d" % l, [32, 128, 8, 512], kind="ExternalInput")
    WD = Dm.get("moe_d%d" % l, [32, 128, 4, 1024], kind="ExternalInput")
    modrow = Dm.get("modrow%d" % l, [2, 6144])
    mk = "modrow%d" % l
    if final:
        fg_d = Dm.get("final_g", [1, D], kind="ExternalInput")
        OUT = Dm.get("out", [L, D], kind="ExternalOutput")
        fg = P.sb(pf + "fg", [128, D])
        load_bcast(P, fg[:], pf + "fg", fg_d[0:1, :])
        epsc = P.sb(pf + "eps", [128, 1])
        P.i("dve", "memset", writes=[pf + "eps"], ap=epsc[:], constant=EPS)
        junk = P.sb(pf + "junk", [128, D])
    else:
        XN = Dm.get("XN%d" % l, [T, D])
    G2g = [P.sb(pf + "g2%d" % r, [128, D]) for r in range(2)]
    for r in range(2):
        load_bcast(P, G2g[r][:], pf + "g2%d" % r, modrow[r:r + 1, 5120:6144], reads=[mk])
    hT_r = Rot(P, pf + "hT", [128, 8, 512], F32R, n=2)
    acc_r = Rot(P, pf + "acc", [128, 4, 1024], n=1)
    wg_r = Rot(P, pf + "wg", [128, 8, 512], F32R, n=2)
    wu_r = Rot(P, pf + "wu", [128, 8, 512], F32R, n=2)
    wd_r = Rot(P, pf + "wd", [128, 4, 1024], F32R, n=2)
    wb_r = Rot(P, pf + "wb", [128, 512], n=2)
    pg_r = Rot(P, pf + "pg", [128, 512], n=2, psum=True)
    pu_r = Rot(P, pf + "pu", [128, 512], n=2, psum=True)
    pd_r = Rot(P, pf + "pd", [128, 512], n=2, psum=True)
    sg_r = Rot(P, pf + "sg", [128, 512], n=2)
    hu_r = Rot(P, pf + "hu", [128, 512], n=2)
    hid_r = Rot(P, pf + "hid", [128, 4, 512], F32R, n=2)
    xt_r = Rot(P, pf + "xt", [128, D], n=2)
    st_r = Rot(P, pf + "st", [128, 8], n=2)
    for (c0, cn) in tok_chunks:
        nt = cn // 128
        hT, hTk = hT_r.next()
        P.d(hT[:, :, 0:cn], H2T[:, c0:c0 + cn].rearrange("(k p) t -> p k t", p=128), reads=["H2T"], writes=[hTk], q="pool")
        acc, acck = acc_r.next()
        for e in range(32):
            wg, wgk = wg_r.next()
            wu, wuk = wu_r.next()
            wd, wdk = wd_r.next()
            P.d(wg[:], WG[e], writes=[wgk], q="pool")
            P.d(wu[:], WU[e], writes=[wuk], q="pool")
            P.d(wd[:], WD[e], writes=[wdk], q="pool")
            wb, wbk = wb_r.next()
            P.d(wb[:, 0:cn], WTd[e:e + 1, c0:c0 + cn].to_broadcast([128, cn]), reads=["WTd"], writes=[wbk])
            hid, hidk = hid_r.next()
            for hc in range(4):
                pg, pgk = pg_r.next()
                pu, puk = pu_r.next()
                for k in range(8):
                    P.i("pe", "matmul", reads=[wgk, hTk], writes=[pgk], out=pg[:, 0:cn], lhsT=wg[:, k, hc * 128:(hc + 1) * 128], rhs=hT[:, k, 0:cn], start=(k == 0), stop=(k == 7))
                for k in range(8):
                    P.i("pe", "matmul", reads=[wuk, hTk], writes=[puk], out=pu[:, 0:cn], lhsT=wu[:, k, hc * 128:(hc + 1) * 128], rhs=hT[:, k, 0:cn], start=(k == 0), stop=(k == 7))
                sg, sgk = sg_r.next()
                P.i("act", "activation", reads=[pgk], writes=[sgk], out=sg[:, 0:cn], in_=pg[:, 0:cn], func=AF.Silu)
                hu, huk = hu_r.next()
                P.i("dve", "tensor_tensor", reads=[sgk, puk], writes=[huk], out=hu[:, 0:cn], in0=sg[:, 0:cn], in1=pu[:, 0:cn], op=ALU.mult)
                P.i("pool", "tensor_tensor", reads=[huk, wbk], writes=[hidk + "/%d" % hc], out=hid[:, hc, 0:cn], in0=hu[:, 0:cn], in1=wb[:, 0:cn], op=ALU.mult)
            hkeys = [hidk + "/%d" % hc for hc in range(4)]
            for tt in range(nt):
                for hf in range(2):
                    pd, pdk = pd_r.next()
                    for hc in range(4):
                        P.i("pe", "matmul", reads=hkeys + [wdk], writes=[pdk], out=pd[:], lhsT=hid[:, hc, tt * 128:(tt + 1) * 128], rhs=wd[:, hc, hf * 512:(hf + 1) * 512],
                            start=(hc == 0), stop=(hc == 3))
                    ak = acck + "/%d_%d" % (tt, hf)
                    if e == 0:
                        P.i("dve", "tensor_copy", reads=[pdk], writes=[ak], out=acc[:, tt, hf * 512:(hf + 1) * 512], in_=pd[:])
                    else:
                        P.i("dve", "tensor_tensor", reads=[pdk, ak], writes=[ak], out=acc[:, tt, hf * 512:(hf + 1) * 512], in0=acc[:, tt, hf * 512:(hf + 1) * 512], in1=pd[:], op=ALU.add)
        for tt in range(nt):
            t0 = c0 + tt * 128
            r = 1 if t0 < LC else 0
            aks = [acck + "/%d_%d" % (tt, hf) for hf in range(2)]
            xt, xk = xt_r.next()
            P.d(xt[:], X1[t0:t0 + 128, :], reads=["X1"], writes=[xk])
            P.i("pool", "tensor_tensor", reads=aks + [pf + "g2%d" % r], writes=aks, out=acc[:, tt, :], in0=acc[:, tt, :], in1=G2g[r][:], op=ALU.mult)
            P.i("dve", "tensor_tensor", reads=aks + [xk], writes=[xk], out=xt[:], in0=xt[:], in1=acc[:, tt, :], op=ALU.add)
            if not final:
                P.d(XN[t0:t0 + 128, :], xt[:], reads=[xk], writes=[P.u("XN%d" % l)])
            else:
                st, sk = st_r.next()
                P.i("act", "activation", reads=[xk], writes=[pf + "junk", sk], out=junk[:], in_=xt[:], func=AF.Square, accum_out=st[:, 0:1])
                P.i("act", "activation", reads=[sk, pf + "eps"], writes=[sk], out=st[:, 1:2], in_=st[:, 0:1], func=AF.Sqrt, bias=epsc[:], scale=1.0 / D)
                P.i("dve", "reciprocal", reads=[sk], writes=[sk], out=st[:, 2:3], in_=st[:, 1:2])
                P.i("dve", "scalar_tensor_tensor", reads=[xk, sk, pf + "fg"], writes=[xk], out=xt[:], in0=xt[:], scalar=st[:, 2:3], in1=fg[:], op0=ALU.mult, op1=ALU.mult)
                P.d(OUT[t0 - LC:t0 - LC + 128, :], xt[:], reads=[xk], writes=[P.u("out")], final=True)


def phase_b(P, Dm, l):
    pf = "b%d_" % l
    FMT = Dm.get("FMT", [1024, T])
    CAT = Dm.get("CAT", [1024, T])
    Bd = Dm.get("s5B%d" % l, [2, 2, 8, 128, 128], kind="ExternalInput")
    Cd = Dm.get("s5C%d" % l, [2, 2, 8, 128, 128], kind="ExternalInput")
    lam_d = Dm.get("s5lam%d" % l, [128, 3, 16], kind="ExternalInput")
    dsk_d = Dm.get("s5d%d" % l, [128, 2], kind="ExternalInput")
    gw_d = Dm.get("gluw%d" % l, [128, 2, 256], kind="ExternalInput")
    gb_d = Dm.get("glub%d" % l, [128, 2], kind="ExternalInput")
    NMAX = 512
    lam = P.sb(pf + "lam", [128, 3, 16])
    P.d(lam[:], lam_d[:, :, :], writes=[pf + "lam"])
    dsk = P.sb(pf + "dsk", [128, 2]); P.d(dsk[:], dsk_d[:, :], writes=[pf + "dsk"])
    gb = P.sb(pf + "gb", [128, 2]); P.d(gb[:], gb_d[:, :], writes=[pf + "gb"])
    gw = P.sb(pf + "gw", [128, 2, 256], F32R); P.d(gw[:], gw_d[:, :, :], writes=[pf + "gw"], q="pool")
    Bb = P.sb(pf + "Bb", [128, 16, 128], F32R)
    Cb = P.sb(pf + "Cb", [128, 16, 128], F32R)
    sc = P.sb(pf + "sc", [128, 24, 16])
    K_ = pf + "sc"
    LR, LI, LDT = lam[:, 0, :], lam[:, 1, :], lam[:, 2, :]
    (DT, RM, TH, C_, S_, T1, T2, T3, ARE, AIM, DEN, AM1, FRE, FIM, HP) = range(15)

    def S(i):
        return sc[:, i, :]

    def tt(o, a, b, op, eng="dve"):
        P.i(eng, "tensor_tensor", reads=[K_, pf + "lam"], writes=[K_], out=o, in0=a, in1=b, op=op)

    hpi = P.sb(pf + "hpi", [128, 1])
    P.i("dve", "memset", writes=[pf + "hpi"], ap=hpi[:], constant=float(np.pi / 2))
    P.i("act", "activation", reads=[pf + "lam"], writes=[K_], out=S(DT), in_=LDT, func=AF.Exp)
    tt(S(RM), LR, S(DT), ALU.mult)
    P.i("act", "activation", reads=[K_], writes=[K_], out=S(RM), in_=S(RM), func=AF.Exp)
    tt(S(TH), LI, S(DT), ALU.mult)
    P.i("act", "activation", reads=[K_], writes=[K_], out=S(S_), in_=S(TH), func=AF.Sin, scale=1.0 / 32)
    P.i("act", "activation", reads=[K_, pf + "hpi"], writes=[K_], out=S(C_), in_=S(TH), func=AF.Sin, scale=1.0 / 32, bias=hpi[:])
    for _ in range(5):
        tt(S(T1), S(C_), S(C_), ALU.mult)
        tt(S(T2), S(S_), S(S_), ALU.mult)
        tt(S(T3), S(C_), S(S_), ALU.mult)
        tt(S(C_), S(T1), S(T2), ALU.subtract)
        tt(S(S_), S(T3), S(T3), ALU.add)
    tt(S(ARE), S(RM), S(C_), ALU.mult)
    tt(S(AIM), S(RM), S(S_), ALU.mult)
    tt(S(T1), LR, LR, ALU.mult)
    tt(S(T2), LI, LI, ALU.mult)
    tt(S(DEN), S(T1), S(T2), ALU.add)
    P.i("dve", "reciprocal", reads=[K_], writes=[K_], out=S(DEN), in_=S(DEN))
    P.i("dve", "tensor_scalar", reads=[K_], writes=[K_], out=S(AM1), in0=S(ARE), scalar1=-1.0, scalar2=None, op0=ALU.add)
    tt(S(T1), S(AM1), LR, ALU.mult)
    tt(S(T2), S(AIM), LI, ALU.mult)
    tt(S(T1), S(T1), S(T2), ALU.add)
    tt(S(FRE), S(T1), S(DEN), ALU.mult)
    tt(S(T1), S(AIM), LR, ALU.mult)
    tt(S(T2), S(AM1), LI, ALU.mult)
    tt(S(T1), S(T1), S(T2), ALU.subtract)
    tt(S(FIM), S(T1), S(DEN), ALU.mult)
    Ep = P.sb(pf + "Ep", [128, 2, 8, NMAX])
    Tm = P.sb(pf + "Tm", [128, 2, 8, NMAX])
    pw = P.sb(pf + "pw", [128, 4, 8])
    tb = P.sb(pf + "tb", [128, 2, 8, NMAX // 2])
    carry = P.sb(pf + "carry", [128, 16, 2])
    P.i("dve", "memset", writes=[pf + "carry"], ap=carry[:], constant=0.0)
    yacc = P.sb(pf + "yacc", [128, 2, T])
    uc_r = Rot(P, pf + "uc", [128, 2, NMAX], F32R, n=2)
    pb_r = Rot(P, pf + "pb", [128, 512], n=4, psum=True)
    py_r = Rot(P, pf + "py", [128, 512], n=2, psum=True)
    br_r = Rot(P, pf + "br", [128, 2, NMAX], n=2)
    t_r = Rot(P, pf + "t", [128, 4, NMAX], n=2)
    v_r = Rot(P, pf + "v", [128, 2, NMAX], n=2)
    g_r = Rot(P, pf + "g", [128, 2, NMAX], n=2)
    h_r = Rot(P, pf + "h", [128, 2, NMAX], F32R, n=2)
    TK, EK = pf + "Tm", pf + "Ep"
    for d in range(2):
        dsl = slice(d * 8, d * 8 + 8)
        P.d(Bb[:], Bd[d].rearrange("c j k m -> k (c j) m"), writes=[pf + "Bb"], q="pool")
        P.d(Cb[:], Cd[d].rearrange("c j k m -> k (c j) m"), writes=[pf + "Cb"], q="pool")
        P.i("act", "activation", reads=[pf + "Cb"], writes=[pf + "Cb"], out=Cb[:, 8:16, :], in_=Cb[:, 8:16, :].bitcast(F32), func=AF.Copy, scale=-1.0)
        P.i("dve", "tensor_copy", reads=[K_], writes=[EK], out=Ep[:, 0, :, 0:1], in_=S(C_)[:, dsl].unsqueeze(2))
        P.i("dve", "tensor_copy", reads=[K_], writes=[EK], out=Ep[:, 1, :, 0:1], in_=S(S_)[:, dsl].unsqueeze(2))
        P.i("dve", "tensor_copy", reads=[K_], writes=[pf + "pw"], out=pw[:, 0, :], in_=S(C_)[:, dsl])
        P.i("dve", "tensor_copy", reads=[K_], writes=[pf + "pw"], out=pw[:, 1, :], in_=S(S_)[:, dsl])
        n = 1
        while n < NMAX:
            cn_b = pw[:, 0, :].unsqueeze(2).to_broadcast([128, 8, n])
            sn_b = pw[:, 1, :].unsqueeze(2).to_broadcast([128, 8, n])
            ire, iim = Ep[:, 0, :, 0:n], Ep[:, 1, :, 0:n]
            ore, oim = Ep[:, 0, :, n:2 * n], Ep[:, 1, :, n:2 * n]
            P.i("dve", "tensor_tensor", reads=[EK, pf + "pw"], writes=[pf + "tb"], out=tb[:, 0, :, 0:n], in0=iim, in1=sn_b, op=ALU.mult)
            P.i("pool", "tensor_tensor", reads=[EK, pf + "pw"], writes=[pf + "tb2"], out=tb[:, 1, :, 0:n], in0=iim, in1=cn_b, op=ALU.mult)
            P.i("dve", "tensor_tensor", reads=[EK, pf + "pw"], writes=[EK + "/a"], out=ore, in0=ire, in1=cn_b, op=ALU.mult)
            P.i("pool", "tensor_tensor", reads=[EK, pf + "pw"], writes=[EK + "/b"], out=oim, in0=ire, in1=sn_b, op=ALU.mult)
            P.i("dve", "tensor_tensor", reads=[EK + "/a", pf + "tb"], writes=[EK + "/a"], out=ore, in0=ore, in1=tb[:, 0, :, 0:n], op=ALU.subtract)
            P.i("pool", "tensor_tensor", reads=[EK + "/b", pf + "tb2"], writes=[EK + "/b"], out=oim, in0=oim, in1=tb[:, 1, :, 0:n], op=ALU.add)
            P.i("dve", "tensor_tensor", reads=[pf + "pw"], writes=[pf + "pw"], out=pw[:, 2, :], in0=pw[:, 0, :], in1=pw[:, 1, :], op=ALU.mult)
            P.i("dve", "tensor_tensor", reads=[pf + "pw"], writes=[pf + "pw"], out=pw[:, 0, :], in0=pw[:, 0, :], in1=pw[:, 0, :], op=ALU.mult)
            P.i("dve", "tensor_tensor", reads=[pf + "pw"], writes=[pf + "pw"], out=pw[:, 3, :], in0=pw[:, 1, :], in1=pw[:, 1, :], op=ALU.mult)
            P.i("dve", "tensor_tensor", reads=[pf + "pw"], writes=[pf + "pw"], out=pw[:, 0, :], in0=pw[:, 0, :], in1=pw[:, 3, :], op=ALU.subtract)
            P.i("dve", "tensor_tensor", reads=[pf + "pw"], writes=[pf + "pw"], out=pw[:, 1, :], in0=pw[:, 2, :], in1=pw[:, 2, :], op=ALU.add)
            n *= 2
        for j in range(8):
            fr = S(FRE)[:, d * 8 + j:d * 8 + j + 1]
            fi = S(FIM)[:, d * 8 + j:d * 8 + j + 1]
            t, tk = t_r.next()
            P.i("dve", "tensor_scalar", reads=[EK, K_], writes=[tk + "/0"], out=t[:, 0, :], in0=Ep[:, 1, j, :], scalar1=fi, scalar2=None, op0=ALU.mult)
            P.i("dve", "scalar_tensor_tensor", reads=[EK, K_, tk + "/0"], writes=[TK + "/a%d" % j], out=Tm[:, 0, j, :], in0=Ep[:, 0, j, :], scalar=fr, in1=t[:, 0, :], op0=ALU.mult, op1=ALU.add)
            P.i("dve", "tensor_scalar", reads=[EK, K_], writes=[tk + "/1"], out=t[:, 1, :], in0=Ep[:, 1, j, :], scalar1=fr, scalar2=None, op0=ALU.mult)
            P.i("dve", "scalar_tensor_tensor", reads=[EK, K_, tk + "/1"], writes=[TK + "/b%d" % j], out=Tm[:, 1, j, :], in0=Ep[:, 0, j, :], scalar=fi, in1=t[:, 1, :], op0=ALU.mult, op1=ALU.subtract)
        order = CHUNKS if d == 0 else [CHUNKS[0]] + CHUNKS[:0:-1]
        rev = (d == 1)

        def R(ap):
            return ap[:, ::-1] if rev else ap

        for (c0, N) in order:
            uc, uck = uc_r.next()
            P.d(uc[:, :, 0:N], FMT[0:256, c0:c0 + N].rearrange("(oc p) t -> p oc t", p=128), reads=["FMT"], writes=[uck], q="pool")
            pys = [py_r.next() for _ in range(2)]
            def s5_iter(j):
                    oc = j // 4
                    dj = d * 8 + j
                    pbr, pbrk = pb_r.next()
                    pbi, pbik = pb_r.next()
                    P.i("pe", "matmul", reads=[pf + "Bb", uck], writes=[pbrk], out=pbr[:, 0:N], lhsT=Bb[:, j, :], rhs=uc[:, oc, 0:N], start=True, stop=True)
                    P.i("pe", "matmul", reads=[pf + "Bb", uck], writes=[pbik], out=pbi[:, 0:N], lhsT=Bb[:, 8 + j, :], rhs=uc[:, oc, 0:N], start=True, stop=True)
                    yield
                    br, brk = br_r.next()
                    P.i("act", "activation", reads=[pbrk], writes=[brk + "/0"], out=br[:, 0, 0:N], in_=R(pbr[:, 0:N]), func=AF.Copy)
                    P.i("act", "activation", reads=[pbik], writes=[brk + "/1"], out=br[:, 1, 0:N], in_=R(pbi[:, 0:N]), func=AF.Copy)
                    yield
                    t, tk = t_r.next()
                    v, vk = v_r.next()
                    b0, b1 = br[:, 0, 0:N], br[:, 1, 0:N]
                    tmr, tmi = Tm[:, 0, j, 0:N], Tm[:, 1, j, 0:N]
                    P.i("dve", "tensor_tensor", reads=[brk + "/0", TK], writes=[tk + "/0"], out=t[:, 0, 0:N], in0=tmr, in1=b0, op=ALU.mult)
                    P.i(S5_ENG2, "tensor_tensor", reads=[brk + "/1", TK], writes=[tk + "/1"], out=t[:, 1, 0:N], in0=tmi, in1=b1, op=ALU.mult)
                    P.i(S5_ENG2, "tensor_tensor", reads=[brk + "/1", TK], writes=[tk + "/2"], out=t[:, 2, 0:N], in0=tmr, in1=b1, op=ALU.mult)
                    P.i(S5_ENG2, "tensor_tensor", reads=[brk + "/0", TK], writes=[tk + "/3"], out=t[:, 3, 0:N], in0=tmi, in1=b0, op=ALU.mult)
                    P.i("dve", "tensor_tensor", reads=[tk + "/0", tk + "/1"], writes=[vk + "/0"], out=v[:, 0, 0:N], in0=t[:, 0, 0:N], in1=t[:, 1, 0:N], op=ALU.subtract)
                    P.i("dve", "tensor_tensor", reads=[tk + "/2", tk + "/3"], writes=[vk + "/1"], out=v[:, 1, 0:N], in0=t[:, 2, 0:N], in1=t[:, 3, 0:N], op=ALU.add)
                    yield
                    g, gk = g_r.next()
                    for c in range(2):
                        P.i("dve", "tensor_tensor_scan", reads=[vk + "/%d" % c, K_, pf + "carry"], writes=[gk + "/%d" % c], out=g[:, c, 0:N], data0=S(RM)[:, dj:dj + 1].to_broadcast([128, N]), data1=v[:, c, 0:N],
                            initial=carry[:, dj, c:c + 1], op0=ALU.mult, op1=ALU.add)
                    yield
                    h, hk = h_r.next()
                    t, tk = t_r.next()
                    epr, epi = Ep[:, 0, j, 0:N], Ep[:, 1, j, 0:N]
                    g0, g1 = g[:, 0, 0:N], g[:, 1, 0:N]
                    P.i("dve", "tensor_tensor", reads=[gk + "/0", EK], writes=[tk + "/0"], out=t[:, 0, 0:N], in0=epr, in1=g0, op=ALU.mult)
                    P.i(S5_ENG2, "tensor_tensor", reads=[gk + "/1", EK], writes=[tk + "/1"], out=t[:, 1, 0:N], in0=epi, in1=g1, op=ALU.mult)
                    P.i(S5_ENG2, "tensor_tensor", reads=[gk + "/1", EK], writes=[tk + "/2"], out=t[:, 2, 0:N], in0=epr, in1=g1, op=ALU.mult)
                    P.i("dve", "tensor_tensor", reads=[gk + "/0", EK], writes=[tk + "/3"], out=t[:, 3, 0:N], in0=epi, in1=g0, op=ALU.mult)
                    P.i("dve", "tensor_tensor", reads=[tk + "/0", tk + "/1"], writes=[hk + "/0"], out=h[:, 0, 0:N], in0=t[:, 0, 0:N], in1=t[:, 1, 0:N], op=ALU.subtract)
                    P.i("dve", "tensor_tensor", reads=[tk + "/2", tk + "/3"], writes=[hk + "/1"], out=h[:, 1, 0:N], in0=t[:, 2, 0:N], in1=t[:, 3, 0:N], op=ALU.add)
                    yield
                    P.i("act", "activation", reads=[hk], writes=[pf + "carry"], out=carry[:, dj, :], in_=h[:].bitcast(F32)[:, :, N - 1], func=AF.Copy)
                    py, pyk = pys[oc]
                    P.i("pe", "matmul", reads=[pf + "Cb", hk + "/0"], writes=[pyk], out=py[:, 0:N], lhsT=Cb[:, j, :], rhs=h[:, 0, 0:N], start=(j % 4 == 0), stop=False)
                    P.i("pe", "matmul", reads=[pf + "Cb", hk + "/1"], writes=[pyk], out=py[:, 0:N], lhsT=Cb[:, 8 + j, :], rhs=h[:, 1, 0:N], start=False, stop=(j % 4 == 3))
            for jp in range(0, 8, 2):
                gens = [s5_iter(jp), s5_iter(jp + 1)]
                alive = True
                while alive:
                    alive = False
                    for g_ in gens:
                        try:
                            next(g_)
                            alive = True
                        except StopIteration:
                            pass
            for oc in range(2):
                py, pyk = pys[oc]
                ya = yacc[:, oc, c0:c0 + N]
                yk = pf + "yacc/%d_%d" % (oc, c0)
                if d == 0:
                    P.i("dve", "scalar_tensor_tensor", reads=[uck, pf + "dsk", pyk], writes=[yk], out=ya, in0=uc[:, oc, 0:N].bitcast(F32), scalar=dsk[:, oc:oc + 1], in1=py[:, 0:N], op0=ALU.mult, op1=ALU.add)
                else:
                    P.i("dve", "tensor_tensor", reads=[yk, pyk], writes=[yk], out=ya, in0=ya, in1=R(py[:, 0:N]), op=ALU.add)
    e_r = Rot(P, pf + "e", [128, 3, NMAX], n=1)
    gT_r = Rot(P, pf + "gT", [128, 2, NMAX], F32R, n=1)
    pq_r = Rot(P, pf + "pq", [128, 512], n=2, psum=True)
    a_r = Rot(P, pf + "a", [128, NMAX], n=2)
    for (c0, N) in CHUNKS:
        gT, gTk = gT_r.next()
        for oc in range(2):
            y = yacc[:, oc, c0:c0 + N]
            yk = pf + "yacc/%d_%d" % (oc, c0)
            ee, ek = e_r.next()
            P.i("pool", "tensor_tensor", reads=[yk], writes=[ek + "/0"], out=ee[:, 0, 0:N], in0=y, in1=y, op=ALU.mult)
            P.i("dve", "tensor_scalar", reads=[ek + "/0"], writes=[ek + "/0"], out=ee[:, 0, 0:N], in0=ee[:, 0, 0:N], scalar1=0.044715, scalar2=1.0, op0=ALU.mult, op1=ALU.add)
            P.i("pool", "tensor_tensor", reads=[ek + "/0", yk], writes=[ek + "/1"], out=ee[:, 1, 0:N], in0=ee[:, 0, 0:N], in1=y, op=ALU.mult)
            P.i("act", "activation", reads=[ek + "/1"], writes=[ek + "/2"], out=ee[:, 2, 0:N], in_=ee[:, 1, 0:N], func=AF.Sigmoid, scale=1.5957691216057308)
            P.i("dve", "tensor_tensor", reads=[ek + "/2", yk], writes=[gTk + "/%d" % oc], out=gT[:, oc, 0:N], in0=ee[:, 2, 0:N], in1=y, op=ALU.mult)
        for oc2 in range(2):
            pq, pqk = pq_r.next()
            for oc in range(2):
                P.i("pe", "matmul", reads=[gTk, pf + "gw"], writes=[pqk], out=pq[:, 0:N], lhsT=gw[:, oc, oc2 * 128:(oc2 + 1) * 128], rhs=gT[:, oc, 0:N], start=(oc == 0), stop=(oc == 1))
            a, ak = a_r.next()
            P.i("act", "activation", reads=[pqk, pf + "gb"], writes=[ak], out=a[:, 0:N], in_=pq[:, 0:N], func=AF.Sigmoid, bias=gb[:, oc2:oc2 + 1])
            P.i("dve", "tensor_tensor", reads=[ak, gTk], writes=[ak], out=a[:, 0:N], in0=a[:, 0:N], in1=gT[:, oc2, 0:N].bitcast(F32), op=ALU.mult)
            P.d(CAT[oc2 * 128:(oc2 + 1) * 128, c0:c0 + N], a[:, 0:N], reads=[ak], writes=[P.u("CAT")])


def phase_d1(P, Dm, l):
    pf = "d%d_" % l
    FMT = Dm.get("FMT", [1024, T])
    XBCT = Dm.get("XBCT", [768, T])
    XBtok = Dm.get("XBtok", [T, 512])
    cw_d = Dm.get("convw%d" % l, [128, 6, 4], kind="ExternalInput")
    ident_d = Dm.get("ident", [128, 128], kind="ExternalInput")
    ident = P.sb(pf + "ident", [128, 128]); P.d(ident[:], ident_d[:, :], writes=[pf + "ident"])
    cw = P.sb(pf + "cw", [128, 6, 4]); P.d(cw[:], cw_d[:, :, :], writes=[pf + "cw"])
    xi_r = Rot(P, pf + "xi", [128, 6, 514], n=2)
    ac_r = Rot(P, pf + "ac", [128, 512], n=2)
    co_r = Rot(P, pf + "co", [128, 6, 512], n=2)
    tp_r = Rot(P, pf + "tp", [128, 512], n=2, psum=True)
    tk_r = Rot(P, pf + "tk", [128, 512], n=2)
    for (c0, N) in CHUNKS:
        xi, xik = xi_r.next()
        lz = c0 in (0, LC)
        rz = (c0 + N) in (LC, T)
        lo = c0 - (0 if lz else 1)
        hi = c0 + N + (0 if rz else 1)
        if lz:
            P.i("dve", "memset", writes=[xik + "/l"], ap=xi[:, :, 0:1], constant=0.0)
        if rz:
            P.i("dve", "memset", writes=[xik + "/r"], ap=xi[:, :, N + 1:N + 2], constant=0.0)
        P.d(xi[:, :, (1 if lz else 0):(1 if lz else 0) + hi - lo], FMT[256:1024, lo:hi].rearrange("(r p) t -> p r t", p=128), reads=["FMT"], writes=[xik + "/m"])
        co, cok = co_r.next()
        for rc in range(6):
            ac, ack = ac_r.next()
            P.i("dve", "tensor_scalar", reads=[xik, pf + "cw"], writes=[ack], out=ac[:, 0:N], in0=xi[:, rc, 1:N + 1], scalar1=cw[:, rc, 1:2], scalar2=None, op0=ALU.mult)
            P.i("dve", "scalar_tensor_tensor", reads=[xik, pf + "cw", ack], writes=[ack], out=ac[:, 0:N], in0=xi[:, rc, 0:N], scalar=cw[:, rc, 0:1], in1=ac[:, 0:N], op0=ALU.mult, op1=ALU.add)
            P.i("dve", "scalar_tensor_tensor", reads=[xik, pf + "cw", ack], writes=[ack], out=ac[:, 0:N], in0=xi[:, rc, 2:N + 2], scalar=cw[:, rc, 2:3], in1=ac[:, 0:N], op0=ALU.mult, op1=ALU.add)
            P.i("act", "activation", reads=[ack, pf + "cw"], writes=[cok + "/%d" % rc], out=co[:, rc, 0:N], in_=ac[:, 0:N], func=AF.Silu, bias=cw[:, rc, 3:4])
        P.d(XBCT[:, c0:c0 + N].rearrange("(r p) t -> p r t", p=128), co[:, :, 0:N], reads=[cok], writes=[P.u("XBCT")])
        for i in range(N // 128):
            tp, tpk = tp_r.next()
            for rc in range(4):
                P.i("pe", "transpose", reads=[cok + "/%d" % rc, pf + "ident"], writes=[tpk], out=tp[:, rc * 128:(rc + 1) * 128], in_=co[:, rc, i * 128:(i + 1) * 128], identity=ident[:])
            tk, tkk = tk_r.next()
            P.i("act", "activation", reads=[tpk], writes=[tkk], out=tk[:], in_=tp[:], func=AF.Copy)
            P.d(XBtok[c0 + i * 128:c0 + (i + 1) * 128, :], tk[:], reads=[tkk], writes=[P.u("XBtok")])


def phase_d2(P, Dm, l):
    pf = "D%d_" % l
    XBCT = Dm.get("XBCT", [768, T])
    XBtok = Dm.get("XBtok", [T, 512])
    TMS = Dm.get("TMS", [T, 520])
    CAT = Dm.get("CAT", [1024, T])
    ident_d = Dm.get("ident", [128, 128], kind="ExternalInput")
    tri_d = Dm.get("tri", [128, 2, 128], kind="ExternalInput")
    alog_d = Dm.get("alog%d" % l, [1, 8], kind="ExternalInput")
    dsk_d = Dm.get("ssdd%d" % l, [1, 4], kind="ExternalInput")
    ng_d = Dm.get("ssdng%d" % l, [1, 256], kind="ExternalInput")
    ident = P.sb(pf + "ident", [128, 128]); P.d(ident[:], ident_d[:, :], writes=[pf + "ident"])
    tri = P.sb(pf + "tri", [128, 2, 128]); P.d(tri[:], tri_d[:, :, :], writes=[pf + "tri"])
    A = P.sb(pf + "A", [128, 8]); load_bcast(P, A[:], pf + "A", alog_d[0:1, :])
    P.i("act", "activation", reads=[pf + "A"], writes=[pf + "A"], out=A[:], in_=A[:], func=AF.Exp)
    P.i("dve", "tensor_scalar", reads=[pf + "A"], writes=[pf + "A"], out=A[:], in0=A[:], scalar1=-1.0, scalar2=None, op0=ALU.mult)
    dsk = P.sb(pf + "dsk", [128, 4]); load_bcast(P, dsk[:], pf + "dsk", dsk_d[0:1, :])
    ng = P.sb(pf + "ng", [128, 256]); load_bcast(P, ng[:], pf + "ng", ng_d[0:1, :])
    epsc = P.sb(pf + "eps", [128, 1]); P.i("dve", "memset", writes=[pf + "eps"], ap=epsc[:], constant=EPS)
    Yacc = P.sb(pf + "Yacc", [128, NT, 256])
    Sst = P.sb(pf + "S", [128, 4, 64], F32R)
    bc_r = Rot(P, pf + "bc", [128, 4, 128], F32R, n=2)
    xb_r = Rot(P, pf + "xb", [128, 512], n=2)
    xbr_r = Rot(P, pf + "xbr", [128, 256], F32R, n=2)
    dt_r = Rot(P, pf + "dt", [128, 8], n=2)
    sm_r = Rot(P, pf + "sm", [128, 64], n=2)
    abc_r = Rot(P, pf + "abc", [128, 4, 128], n=2)
    X_r = Rot(P, pf + "X", [128, 4, 64], F32R, n=2)
    Xd_r = Rot(P, pf + "Xd", [128, 4, 64], F32R, n=2)
    pG_r = Rot(P, pf + "pG", [128, 512], n=1, psum=True)
    pR_r = Rot(P, pf + "pR", [128, 512], n=1, psum=True)
    pC_r = Rot(P, pf + "pC", [128, 512], n=1, psum=True)
    pY_r = Rot(P, pf + "pY", [128, 512], n=2, psum=True)
    pS_r = Rot(P, pf + "pS", [128, 512], n=1, psum=True)
    Gm_r = Rot(P, pf + "Gm", [128, 2, 128], n=2)
    df_r = Rot(P, pf + "df", [128, 128], n=2)
    sc_r = Rot(P, pf + "scT", [128, 128], F32R, n=2)
    yo_r = Rot(P, pf + "yo", [128, 64], n=2)
    for d in range(2):
        order = list(range(NT)) if d == 0 else [1, 0] + list(range(NT - 1, 1, -1))
        P.i("dve", "memset", writes=[pf + "S"], ap=Sst[:].bitcast(F32), constant=0.0)
        for ci, c in enumerate(order):
            t0 = c * 128
            bc, bck = bc_r.next()
            P.d(bc[:], XBCT[256:768, t0:t0 + 128].rearrange("(r p) t -> p r t", p=128), reads=["XBCT"], writes=[bck], q="pool")
            xb, xbk = xb_r.next()
            P.d(xb[:], XBtok[t0:t0 + 128, :], reads=["XBtok"], writes=[xbk])
            xbr, xbrk = xbr_r.next()
            P.i("act", "activation", reads=[xbk], writes=[xbrk], out=xbr[:], in_=xb[:, 256:512], func=AF.Copy)
            dt, dtk = dt_r.next()
            P.d(dt[:], TMS[t0:t0 + 128, 512:520], reads=["TMS"], writes=[dtk])
            sm, smk = sm_r.next()
            P.i("dve", "tensor_tensor", reads=[dtk, pf + "A"], writes=[smk], out=sm[:, 0:4], in0=dt[:, d * 4:d * 4 + 4], in1=A[:, d * 4:d * 4 + 4], op=ALU.mult)
            abc, abck = abc_r.next()
            P.i("dve", "tensor_copy", reads=[smk], writes=[abck], out=abc[:], in_=sm[:, 0:4].unsqueeze(2).to_broadcast([128, 4, 128]))
            X, Xk = X_r.next()
            P.i("pool", "tensor_tensor", reads=[xbk, dtk], writes=[Xk], out=X[:], in0=xb[:, 0:256].rearrange("p (h e) -> p h e", h=4),
                in1=dt[:, d * 4:d * 4 + 4].unsqueeze(2).to_broadcast([128, 4, 64]), op=ALU.mult)
            pG, pGk = pG_r.next()
            for g in range(2):
                P.i("pe", "matmul", reads=[bck], writes=[pGk], out=pG[:, g * 128:(g + 1) * 128], lhsT=bc[:, g, :], rhs=bc[:, 2 + g, :], start=True, stop=True)
            Gm, Gmk = Gm_r.next()
            P.i("dve", "tensor_tensor", reads=[pGk, pf + "tri"], writes=[Gmk], out=Gm[:], in0=pG[:, 0:256].rearrange("p (g l) -> p g l", g=2),
                in1=tri[:, d, :].unsqueeze(1).to_broadcast([128, 2, 128]), op=ALU.mult)
            pC, pCk = pC_r.next()
            P.i("pe", "matmul", reads=[smk, pf + "tri"], writes=[pCk], out=pC[:, 0:4], lhsT=tri[:, d, :], rhs=sm[:, 0:4], start=True, stop=True)
            pR, pRk = pR_r.next()
            for h in range(4):
                P.i("pe", "matmul", reads=[abck, pf + "tri"], writes=[pRk], out=pR[:, h * 128:(h + 1) * 128], lhsT=abc[:, h, :], rhs=tri[:, d, :], start=True, stop=True)
            P.i("dve", "tensor_copy", reads=[pCk], writes=[smk], out=sm[:, 4:8], in_=pC[:, 0:4])
            P.i("act", "activation", reads=[smk], writes=[smk], out=sm[:, 8:12], in_=sm[:, 4:8], func=AF.Exp)
            last = 127 if d == 0 else 0
            P.i("dve", "tensor_copy", reads=[pRk], writes=[smk], out=sm[:, 20:24], in_=pR[:].rearrange("p (h l) -> p h l", h=4)[:, :, last])
            P.i("dve", "tensor_tensor", reads=[smk], writes=[smk], out=sm[:, 12:16], in0=sm[:, 20:24], in1=sm[:, 4:8], op=ALU.subtract)
            P.i("act", "activation", reads=[smk], writes=[smk], out=sm[:, 12:16], in_=sm[:, 12:16], func=AF.Exp)
            P.i("act", "activation", reads=[smk], writes=[smk], out=sm[:, 16:20], in_=sm[:, 20:24], func=AF.Exp)
            Xd, Xdk = Xd_r.next()
            P.i("pool", "tensor_tensor", reads=[Xk, smk], writes=[Xdk], out=Xd[:], in0=X[:].bitcast(F32), in1=sm[:, 12:16].unsqueeze(2).to_broadcast([128, 4, 64]), op=ALU.mult)
            pY, pYk = pY_r.next()
            pS, pSk = pS_r.next()
            for h in range(4):
                g = h // 2
                df, dfk = df_r.next()
                P.i("dve", "tensor_scalar", reads=[pRk, smk], writes=[dfk], out=df[:], in0=pR[:, h * 128:(h + 1) * 128], scalar1=sm[:, 4 + h:5 + h], scalar2=0.0, op0=ALU.subtract, op1=ALU.min)
                P.i("act", "activation", reads=[dfk], writes=[dfk], out=df[:], in_=df[:], func=AF.Exp)
                scT, scTk = sc_r.next()
                P.i("pool", "tensor_tensor", reads=[dfk, Gmk], writes=[scTk], out=scT[:], in0=df[:], in1=Gm[:, g, :], op=ALU.mult)
                P.i("pe", "matmul", reads=[scTk, Xk], writes=[pYk], out=pY[:, h * 128:h * 128 + 64], lhsT=scT[:], rhs=X[:, h, :], start=True, stop=True)
                P.i("pe", "matmul", reads=[bck, pf + "S"], writes=[pYk], out=pY[:, h * 128 + 64:h * 128 + 128], lhsT=bc[:, 2 + g, :], rhs=Sst[:, h, :], start=True, stop=True)
                P.i("pe", "matmul", reads=[xbrk, Xdk], writes=[pSk], out=pS[:, h * 64:(h + 1) * 64], lhsT=xbr[:, g * 128:(g + 1) * 128], rhs=Xd[:, h, :], start=True, stop=True)
            for h in range(4):
                yo, yok = yo_r.next()
                P.i("act", "activation", reads=[pYk, smk], writes=[yok], out=yo[:], in_=pY[:, h * 128 + 64:h * 128 + 128], func=AF.Copy, scale=sm[:, 8 + h:9 + h])
                ya = Yacc[:, c, h * 64:(h + 1) * 64]
                yk = pf + "Yacc/%d_%d" % (c, h)
                if d == 0:
                    P.i("dve", "tensor_tensor", reads=[pYk, yok], writes=[yk], out=ya, in0=pY[:, h * 128:h * 128 + 64], in1=yo[:], op=ALU.add)
                else:
                    P.i("dve", "tensor_tensor", reads=[pYk, yok], writes=[yok], out=yo[:], in0=pY[:, h * 128:h * 128 + 64], in1=yo[:], op=ALU.add)
                    P.i("pool", "tensor_tensor", reads=[yk, yok], writes=[yk], out=ya, in0=ya, in1=yo[:], op=ALU.add)
                P.i("dve", "scalar_tensor_tensor", reads=[pf + "S", smk, pSk], writes=[pf + "S"], out=Sst[:, h, :], in0=Sst[:, h, :].bitcast(F32), scalar=sm[:, 16 + h:17 + h],
                    in1=pS[:, h * 64:(h + 1) * 64], op0=ALU.mult, op1=ALU.add)
    z_r = Rot(P, pf + "z", [128, 256], n=2)
    yt_r = Rot(P, pf + "yt", [128, 256], n=2)
    jk = P.sb(pf + "jk", [128, 256])
    pT_r = Rot(P, pf + "pT", [128, 512], n=2, psum=True)
    oT_r = Rot(P, pf + "oT", [128, 2, 128], n=2)
    for c in range(NT):
        t0 = c * 128
        xb, xbk = xb_r.next()
        P.d(xb[:], XBtok[t0:t0 + 128, :], reads=["XBtok"], writes=[xbk])
        z, zk = z_r.next()
        P.d(z[:], TMS[t0:t0 + 128, 256:512], reads=["TMS"], writes=[zk])
        yt, ytk = yt_r.next()
        P.i("pool", "tensor_tensor", reads=[xbk, pf + "dsk"], writes=[ytk], out=yt[:].rearrange("p (h e) -> p h e", h=4), in0=xb[:, 0:256].rearrange("p (h e) -> p h e", h=4),
            in1=dsk[:].unsqueeze(2).to_broadcast([128, 4, 64]), op=ALU.mult)
        P.i("dve", "tensor_tensor", reads=[ytk, pf + "Yacc/%d" % c], writes=[ytk], out=yt[:], in0=yt[:], in1=Yacc[:, c, :], op=ALU.add)
        P.i("act", "activation", reads=[zk], writes=[zk], out=z[:], in_=z[:], func=AF.Silu)
        P.i("dve", "tensor_tensor", reads=[ytk, zk], writes=[ytk], out=yt[:], in0=yt[:], in1=z[:], op=ALU.mult)
        sm, smk = sm_r.next()
        P.i("act", "activation", reads=[ytk], writes=[pf + "jk", smk], out=jk[:], in_=yt[:], func=AF.Square, accum_out=sm[:, 0:1])
        P.i("act", "activation", reads=[smk, pf + "eps"], writes=[smk], out=sm[:, 1:2], in_=sm[:, 0:1], func=AF.Sqrt, bias=epsc[:], scale=1.0 / 256)
        P.i("dve", "reciprocal", reads=[smk], writes=[smk], out=sm[:, 2:3], in_=sm[:, 1:2])
        P.i("dve", "scalar_tensor_tensor", reads=[ytk, smk, pf + "ng"], writes=[ytk], out=yt[:], in0=yt[:], scalar=sm[:, 2:3], in1=ng[:], op0=ALU.mult, op1=ALU.mult)
        pT, pTk = pT_r.next()
        for q in range(2):
            P.i("pe", "transpose", reads=[ytk, pf + "ident"], writes=[pTk], out=pT[:, q * 128:(q + 1) * 128], in_=yt[:, q * 128:(q + 1) * 128], identity=ident[:])
        oT, oTk = oT_r.next()
        P.i("act", "activation", reads=[pTk], writes=[oTk], out=oT[:], in_=pT[:, 0:256].rearrange("p (q t) -> p q t", q=2), func=AF.Copy)
        P.d(CAT[512:768, t0:t0 + 128].rearrange("(q p) t -> p q t", p=128), oT[:], reads=[oTk], writes=[P.u("CAT")])


LAT_CHUNKS = [(256 + 512 * i, 512) for i in range(8)]
ALL_CHUNKS512 = [(512 * i, 512) for i in range(8)] + [(4096, 256)]


def build_program(scopes=False):
    nc = bass.Bass("TRN2", target_bir_lowering=False)
    P = Prog(nc)
    P.scopes = scopes
    Dm = Dram(nc, ext_in=["xin"])
    src = "xin"
    for l in range(2):
        last = (l == 1)
        ctx_out = not last
        for fi, fn in enumerate((lambda: phase_mod(P, Dm, l),
                   lambda: phase_a(P, Dm, l, src),
                   lambda: phase_b(P, Dm, l),
                   lambda: phase_c(P, Dm, l, ctx_out),
                   lambda: phase_d1(P, Dm, l),
                   lambda: phase_d2(P, Dm, l),
                   lambda: phase_e(P, Dm, l, ctx_out),
                   lambda: phase_f2(P, Dm, l, src, tok_chunks=(LAT_CHUNKS if last else None)),
                   lambda: phase_g2(P, Dm, l, (LAT_CHUNKS if last else CHUNKS), last))):
            P.begin_phase("L%d_%s" % (l, "mabcdDefg"[fi]))
            fn()
            P.end_phase()
        src = "XN%d" % l
    P.emit()
    return nc, P, Dm


_CACHE = {}


def kernel(**inputs):
    inp = {k: np.asarray(v) for k, v in inputs.items()}
    if "nc" not in _CACHE:
        _CACHE["nc"] = build_program()
    nc, P, Dm = _CACHE["nc"]
    n_cores = 8
    maps = []
    per_b = {}
    for cidx in range(n_cores):
        b = cidx % 4
        if b not in per_b:
            m = core_inputs(inp, b)
            per_b[b] = {k: v for k, v in m.items() if k in Dm.t}
        maps.append(per_b[b])
    res = run_bass_kernel_spmd(nc, maps, core_ids=list(range(n_cores)))
    out = np.stack([np.asarray(res.results[b]["out"]) for b in range(4)], 0)
    return out.astype(np.float32)
```

```python
import numpy as np
from contextlib import ExitStack
import concourse.bass as bass
import concourse.mybir as mybir
from concourse.bass_utils import run_bass_kernel_spmd

F32 = mybir.dt.float32
F32R = mybir.dt.float32r
I32 = mybir.dt.int32
AF = mybir.ActivationFunctionType
ALU = mybir.AluOpType
AX = mybir.AxisListType

LC, L, T, D = 256, 4096, 4352, 1024
NT = T // 128
EPS = 1e-6
CHUNKS = [(0, 256)] + [(256 + 512 * i, 512) for i in range(8)]
PERM = np.concatenate([np.arange(256, 768), np.arange(1800, 2312), np.arange(768, 1024), np.arange(1792, 1800),
                       np.arange(0, 256), np.arange(1024, 1792)])
TM_COLS = 1288
FM_OFF = 1288

ENGS = ["pe", "act", "dve", "pool", "sp"]
N_DMA_SEMS = 8


class Prog:
    def __init__(self, nc, same_engine_sync=True):
        self.nc = nc
        self.st = ExitStack()
        self.same = same_engine_sync
        self.ops = {e: [] for e in ENGS}
        self.sems = {}
        self.cnt = {}
        for e in ["pe", "act", "dve", "pool"]:
            self.sems[e] = self.st.enter_context(nc.semaphore("s_" + e))
            self.cnt[e] = 0
        for q in ["sp", "pool"]:
            for i in range(N_DMA_SEMS):
                nm = "d_%s%d" % (q, i)
                self.sems[nm] = self.st.enter_context(nc.semaphore(nm))
                self.cnt[nm] = 0
        self.drr = {"sp": 0, "pool": 0}
        self.waited = {e: {} for e in ENGS}
        self.last_w = {}
        self.readers = {}
        self.n_ops = 0
        self.final = []
        self.uid = 0
        self.pst = None
        self.barrier = {e: {} for e in ENGS}
        self.children = {}
        self.known = set()
        self.scopes = False
        self.bound_reg = None
        self.pending_noinc = {e: False for e in ENGS}
        self.psum_keys = set()

    def begin_phase(self, name=None):
        self.pst = ExitStack()
        self.phase_name = name

    def end_phase(self):
        self.pst.close()
        self.pst = None
        assert not any(self.pending_noinc.values()), self.pending_noinc
        snap = {s: v for s, v in self.cnt.items() if v > 0}
        for e in ENGS:
            self.barrier[e] = dict(snap)

    def bound(self):
        if self.bound_reg is None:
            self.bound_reg = self.nc.gpsimd.alloc_register("bc4095")
        return self.bound_reg

    def sb(self, name, shape, dtype=F32):
        return (self.pst or self.st).enter_context(self.nc.sbuf_tensor(name, list(shape), dtype))

    def ps(self, name, shape, dtype=F32):
        return (self.pst or self.st).enter_context(self.nc.psum_tensor(name, list(shape), dtype))

    def _related(self, k):
        rel = [k]
        parts = k.split("/")
        for n in range(1, len(parts)):
            rel.append("/".join(parts[:n]))
        rel.extend(self.children.get(k, ()))
        return rel

    def _register(self, k):
        if k in self.known:
            return
        self.known.add(k)
        parts = k.split("/")
        for n in range(1, len(parts)):
            self.children.setdefault("/".join(parts[:n]), set()).add(k)

    def _deps(self, eng, reads, writes):
        deps = {}

        def add(s, v):
            if v > deps.get(s, 0):
                deps[s] = v

        for k in list(reads) + list(writes):
            self._register(k)
        for k in reads:
            for kk in self._related(k):
                t = self.last_w.get(kk)
                if t is not None:
                    add(*t)
        for k in writes:
            for kk in self._related(k):
                t = self.last_w.get(kk)
                if t is not None:
                    add(*t)
                for s_, v_ in self.readers.get(kk, {}).items():
                    add(s_, v_)
        if self.barrier[eng]:
            for s_, v_ in self.barrier[eng].items():
                add(s_, v_)
            self.barrier[eng] = {}
        out = []
        for s, v in deps.items():
            if s == eng and (eng == "pe" or not self.same):
                continue
            if v > self.waited[eng].get(s, 0):
                self.waited[eng][s] = v
                out.append((s, v))
        return out

    def _commit(self, tok, reads, writes):
        for k in writes:
            self.last_w[k] = tok
            self.readers[k] = {}
        for k in reads:
            if k in writes:
                continue
            rd = self.readers.setdefault(k, {})
            if tok[1] > rd.get(tok[0], 0):
                rd[tok[0]] = tok[1]

    def op(self, eng, fn, reads=(), writes=(), noinc=False):
        pr = [k for k in reads if k in self.psum_keys]
        if pr:
            reads = [k for k in reads if k not in self.psum_keys]
            writes = list(writes) + pr
        waits = self._deps(eng, reads, writes)
        if noinc:
            tok = (eng, self.cnt[eng] + 1)
            self.pending_noinc[eng] = True
            self.ops[eng].append((waits, fn, tok, 0, getattr(self, "phase_name", None)))
        else:
            self.cnt[eng] += 1
            tok = (eng, self.cnt[eng])
            self.pending_noinc[eng] = False
            self.ops[eng].append((waits, fn, tok, 1, getattr(self, "phase_name", None)))
        self._commit(tok, reads, writes)
        self.n_ops += 1
        return tok

    def u(self, base):
        self.uid += 1
        return "%s/%d" % (base, self.uid)

    def i(self, eng, name, reads=(), writes=(), noinc=False, **kw):
        if name == "matmul" and kw.get("stop") is False:
            noinc = True
        return self.op(eng, lambda e: getattr(e, name)(**kw), reads, writes, noinc=noinc)

    def d(self, out, in_, reads=(), writes=(), q="sp", final=False):
        return self.dma(lambda e: e.dma_start(out=out, in_=in_), reads, writes, q=q, final=final)

    def dma(self, fn, reads=(), writes=(), q="sp", final=False):
        waits = self._deps(q, reads, writes)
        i = self.drr[q]
        self.drr[q] = (i + 1) % N_DMA_SEMS
        nm = "d_%s%d" % (q, i)
        prev = self.cnt[nm]
        if prev > self.waited[q].get(nm, 0):
            self.waited[q][nm] = prev
            waits.append((nm, prev))
        self.cnt[nm] += 16
        tok = (nm, self.cnt[nm])
        self.ops[q].append((waits, fn, tok, 16, getattr(self, "phase_name", None)))
        self._commit(tok, reads, writes)
        self.n_ops += 1
        if final:
            self.final.append(tok)
        return tok

    def emit(self):
        nc = self.nc
        fin = list(self.final)
        engmap = {"pe": "tensor", "act": "scalar", "dve": "vector", "pool": "gpsimd", "sp": "sync"}
        with nc.Block() as block:
            for e in ENGS:
                lst = self.ops[e]
                extra = fin if e == "sp" else []
                if not lst and not extra:
                    continue

                def body(engine, lst=lst, extra=extra, e=e):
                    cur = None
                    scope = None
                    if e == "pool" and self.bound_reg is not None:
                        engine.reg_mov(self.bound_reg, 4095)
                    for (waits, fn, tok, amt, ph) in lst:
                        if self.scopes and ph != cur:
                            if scope is not None:
                                scope.__exit__(None, None, None)
                            scope = nc.named_scope(ph or "none")
                            scope.__enter__()
                            cur = ph
                        for (s, v) in waits:
                            engine.wait_ge(self.sems[s], v)
                        ins = fn(engine)
                        if amt:
                            ins.then_inc(self.sems[tok[0]], amt)
                    if scope is not None:
                        scope.__exit__(None, None, None)
                    for (s, v) in extra:
                        engine.wait_ge(self.sems[s], v)

                getattr(block, engmap[e])(body)
        self.st.close()


class Rot:
    def __init__(self, P, name, shape, dtype=F32, n=2, psum=False):
        self.bufs = [(P.ps if psum else P.sb)("%s%d" % (name, i), shape, dtype) for i in range(n)]
        self.keys = ["%s%d" % (name, i) for i in range(n)]
        self.i = 0
        if psum:
            P.psum_keys.update(self.keys)

    def next(self):
        j = self.i % len(self.bufs)
        self.i += 1
        return self.bufs[j], self.keys[j]


class Dram:
    def __init__(self, nc, ext_in=(), ext_out=()):
        self.nc, self.t = nc, {}
        self.ext_in, self.ext_out = set(ext_in), set(ext_out)

    def get(self, name, shape=None, dtype=F32, kind=None):
        if name not in self.t:
            if kind is None:
                kind = "ExternalInput" if name in self.ext_in else ("ExternalOutput" if name in self.ext_out else "Internal")
            self.t[name] = self.nc.dram_tensor(name, list(shape), dtype, kind=kind).ap()
        return self.t[name]


def phase_mod(P, Dm, l):
    nc = P.nc
    cvec = Dm.get("cvec", [128, 8, 2], kind="ExternalInput")
    ada_w = Dm.get("ada_w%d" % l, [128, 8, 6144], kind="ExternalInput")
    ada_b = Dm.get("ada_b%d" % l, [1, 6144], kind="ExternalInput")
    modrow = Dm.get("modrow%d" % l, [2, 6144])
    pf = "m%d_" % l
    cv = P.sb(pf + "cv", [128, 8, 2])
    sg = P.sb(pf + "sg", [128, 8, 2])
    sc = P.sb(pf + "sc", [128, 8, 128], F32R)
    ab = P.sb(pf + "ab", [2, 6144])
    mr = P.sb(pf + "mr", [2, 6144])
    wch = Rot(P, pf + "w", [128, 8, 512], F32R, n=2)
    pm = Rot(P, pf + "pm", [128, 512], F32, n=2, psum=True)
    P.dma(lambda e: e.dma_start(out=cv[:], in_=cvec[:, :, :]), writes=[pf + "cv"])
    P.dma(lambda e: e.dma_start(out=ab[:], in_=ada_b[0:1, :].to_broadcast([2, 6144])), writes=[pf + "ab"])
    P.op("act", lambda e: e.activation(out=sg[:], in_=cv[:], func=AF.Sigmoid), reads=[pf + "cv"], writes=[pf + "sg"])
    P.op("dve", lambda e: e.memset(sc[:].bitcast(F32), 0.0), writes=[pf + "sc"])
    P.op("dve", lambda e: e.tensor_tensor(out=sc[:, :, 0:2], in0=cv[:], in1=sg[:], op=ALU.mult), reads=[pf + "cv", pf + "sg"], writes=[pf + "sc"])
    for j in range(12):
        w, wk = wch.next()
        P.dma(lambda e, w=w, j=j: e.dma_start(out=w[:], in_=ada_w[:, :, j * 512:(j + 1) * 512]), writes=[wk], q="pool")
        pt, pk = pm.next()
        for k in range(8):
            P.op("pe", lambda e, w=w, pt=pt, k=k: e.matmul(pt[:], sc[:, k, :], w[:, k, :], start=(k == 0), stop=(k == 7)),
                 reads=[wk, pf + "sc"], writes=[pk], noinc=(k != 7))
        P.op("dve", lambda e, pt=pt, j=j: e.tensor_tensor(out=mr[:, j * 512:(j + 1) * 512], in0=pt[0:2, :], in1=ab[:, j * 512:(j + 1) * 512], op=ALU.add),
             reads=[pk, pf + "ab"], writes=[pf + "mr"])
    P.dma(lambda e: e.dma_start(out=modrow[:, :], in_=mr[:]), reads=[pf + "mr"], writes=["modrow%d" % l])


def load_bcast(P, dst, dkey, src_row, reads=()):
    n = src_row.shape[-1]
    P.dma(lambda e: e.dma_start(out=dst, in_=src_row.to_broadcast([128, n])), reads=list(reads), writes=[dkey])


def phase_a(P, Dm, l, src_name, chunks=None, stop=99):
    nc = P.nc
    pf = "a%d_" % l
    xsrc = Dm.get(src_name, [T, D])
    w_in = Dm.get("w_in%d" % l, [128, 8, 2312], kind="ExternalInput")
    modrow = Dm.get("modrow%d" % l, [2, 6144])
    n1g = Dm.get("norm1_g%d" % l, [1, D], kind="ExternalInput")
    qkg = Dm.get("qkg%d" % l, [1, 384], kind="ExternalInput")
    dtb = Dm.get("dtb%d" % l, [1, 8], kind="ExternalInput")
    ropec = Dm.get("rope_cos", [L, 384], kind="ExternalInput")
    ropes = Dm.get("rope_sin", [L, 384], kind="ExternalInput")
    ident_d = Dm.get("ident", [128, 128], kind="ExternalInput")
    FMT = Dm.get("FMT", [1024, T])
    QKT = Dm.get("QKT", [768, T])
    TMS = Dm.get("TMS", [T, 520])
    mk = "modrow%d" % l

    ident = P.sb(pf + "ident", [128, 128])
    P.dma(lambda e: e.dma_start(out=ident[:], in_=ident_d[:, :]), writes=[pf + "ident"])
    win = P.sb(pf + "win", [128, 8, 2312], F32R)
    for k in range(8):
        P.dma(lambda e, k=k: e.dma_start(out=win[:, k, :], in_=w_in[:, k, :]), writes=[pf + "win/%d" % k], q="pool")
    wkeys = [pf + "win/%d" % k for k in range(8)]
    G = [P.sb(pf + "G%d" % r, [128, D]) for r in range(2)]
    SH = [P.sb(pf + "SH%d" % r, [128, D]) for r in range(2)]
    gn = P.sb(pf + "gn", [128, D])
    load_bcast(P, gn[:], pf + "gn", n1g[0:1, :])
    for r in range(2):
        load_bcast(P, SH[r][:], pf + "SH%d" % r, modrow[r:r + 1, 0:1024], reads=[mk])
        load_bcast(P, G[r][:], pf + "G%d" % r, modrow[r:r + 1, 1024:2048], reads=[mk])
        P.op("dve", lambda e, r=r: e.scalar_tensor_tensor(out=G[r][:], in0=G[r][:], scalar=1.0, in1=gn[:], op0=ALU.add, op1=ALU.mult),
             reads=[pf + "G%d" % r, pf + "gn"], writes=[pf + "G%d" % r])
    qkgb = P.sb(pf + "qkgb", [128, 384])
    load_bcast(P, qkgb[:], pf + "qkgb", qkg[0:1, :])
    dtbb = P.sb(pf + "dtbb", [128, 8])
    load_bcast(P, dtbb[:], pf + "dtbb", dtb[0:1, :])
    epsc = P.sb(pf + "eps", [128, 1])
    P.op("dve", lambda e: e.memset(epsc[:], EPS), writes=[pf + "eps"])
    onec = P.sb(pf + "one", [128, 1])
    P.op("dve", lambda e: e.memset(onec[:], 1.0), writes=[pf + "one"])

    xt_r = Rot(P, pf + "xt", [128, D], n=3)
    junk = P.sb(pf + "junk", [128, D])
    st_r = Rot(P, pf + "st", [128, 32], n=3)
    h_r = Rot(P, pf + "h", [128, D], n=2)
    tp_r = Rot(P, pf + "tp", [128, 1024], n=1, psum=True)
    hT_r = Rot(P, pf + "hT", [128, 8, 512], F32R, n=2)
    pj_r = Rot(P, pf + "pj", [128, 512], n=3, psum=True)
    qk_r = Rot(P, pf + "qk", [128, 12, 64], n=2)
    sq_r = Rot(P, pf + "sq", [128, 6, 64], n=2)
    rp_r = Rot(P, pf + "rp", [128, 24, 2, 16], n=2)
    tmp_r = Rot(P, pf + "tmp", [128, 24, 16], n=2)
    cs_r = Rot(P, pf + "cs", [128, 2, 384], n=2)
    tq_r = Rot(P, pf + "tq", [128, 1024], n=1, psum=True)
    qT_r = Rot(P, pf + "qT", [128, 6, 128], n=2)
    tm_r = Rot(P, pf + "tm", [128, 520], n=2)
    fm_r = Rot(P, pf + "fm", [128, 512], n=1, psum=True)
    fo_r = Rot(P, pf + "fo", [128, 512], n=3)

    for (c0, cn) in (chunks or CHUNKS):
        hT, hTk = hT_r.next()
        ntile = cn // 128
        for i in range(ntile):
            t0 = c0 + i * 128
            is_ctx = t0 < LC
            r = 1 if is_ctx else 0
            xt, xk = xt_r.next()
            P.dma(lambda e, xt=xt, t0=t0: e.dma_start(out=xt[:], in_=xsrc[t0:t0 + 128, :]), reads=[src_name], writes=[xk])
            st, sk = st_r.next()
            P.op("act", lambda e, xt=xt, st=st: e.activation(out=junk[:], in_=xt[:], func=AF.Square, accum_out=st[:, 0:1]),
                 reads=[xk], writes=[pf + "junk", sk])
            P.op("act", lambda e, st=st: e.activation(out=st[:, 1:2], in_=st[:, 0:1], func=AF.Sqrt, bias=epsc[:], scale=1.0 / D),
                 reads=[sk, pf + "eps"], writes=[sk])
            P.op("dve", lambda e, st=st: e.reciprocal(out=st[:, 2:3], in_=st[:, 1:2]), reads=[sk], writes=[sk])
            h, hk = h_r.next()
            P.op("dve", lambda e, xt=xt, st=st, h=h, r=r: e.scalar_tensor_tensor(out=h[:], in0=xt[:], scalar=st[:, 2:3], in1=G[r][:], op0=ALU.mult, op1=ALU.mult),
                 reads=[xk, sk, pf + "G%d" % r], writes=[hk])
            P.op("pool", lambda e, h=h, r=r: e.tensor_tensor(out=h[:], in0=h[:], in1=SH[r][:], op=ALU.add),
                 reads=[hk, pf + "SH%d" % r], writes=[hk])
            if stop == 1:
                continue
            tp, tpk = tp_r.next()
            for k in range(8):
                P.op("pe", lambda e, h=h, tp=tp, k=k: e.transpose(tp[:, k * 128:(k + 1) * 128], h[:, k * 128:(k + 1) * 128], ident[:]),
                     reads=[hk, pf + "ident"], writes=[tpk], noinc=(k != 7))
            P.op("act", lambda e, hT=hT, tp=tp, i=i: e.activation(out=hT[:, :, i * 128:(i + 1) * 128], in_=tp[:].rearrange("p (k t) -> p k t", k=8), func=AF.Copy),
                 reads=[tpk], writes=[hTk + "/%d" % i])
            if stop == 2:
                continue
            pjs = []
            for (o, n) in [(0, 512), (512, 512), (1024, 264)]:
                pj, pjk = pj_r.next()
                for k in range(8):
                    P.op("pe", lambda e, pj=pj, hT=hT, k=k, o=o, n=n, i=i: e.matmul(pj[:, 0:n], hT[:, k, i * 128:(i + 1) * 128], win[:, k, o:o + n], start=(k == 0), stop=(k == 7)),
                         reads=[hTk + "/%d" % i, wkeys[k]], writes=[pjk], noinc=(k != 7))
                pjs.append((pj, pjk))
            (pA, pAk), (pB, pBk), (pC, pCk) = pjs
            if stop == 3:
                continue
            qk, qkk = qk_r.next()
            sq, sqk = sq_r.next()
            pA6 = pA[:, 0:384].rearrange("p (h d) -> p h d", h=6)
            P.op("act", lambda e, sq=sq, pA6=pA6: e.activation(out=sq[:], in_=pA6, func=AF.Square), reads=[pAk], writes=[sqk])
            P.op("dve", lambda e, sq=sq, st=st: e.tensor_reduce(out=st[:, 4:10], in_=sq[:], axis=AX.X, op=ALU.add), reads=[sqk], writes=[sk])
            P.op("act", lambda e, st=st: e.activation(out=st[:, 4:10], in_=st[:, 4:10], func=AF.Sqrt, bias=epsc[:], scale=1.0 / 64), reads=[sk, pf + "eps"], writes=[sk])
            P.op("dve", lambda e, st=st: e.reciprocal(out=st[:, 10:16], in_=st[:, 4:10]), reads=[sk], writes=[sk])
            P.op("dve", lambda e, sq=sq, pA6=pA6, st=st: e.tensor_tensor(out=sq[:], in0=pA6, in1=st[:, 10:16].unsqueeze(2).to_broadcast([128, 6, 64]), op=ALU.mult),
                 reads=[pAk, sk], writes=[sqk])
            P.op("pool", lambda e, sq=sq, qk=qk: e.tensor_tensor(out=qk[:, 0:6, :], in0=sq[:], in1=qkgb[:].rearrange("p (h d) -> p h d", h=6), op=ALU.mult),
                 reads=[sqk, pf + "qkgb"], writes=[qkk + "/a"])
            P.op("act", lambda e, qk=qk, pB=pB: e.activation(out=qk[:, 6:12, :], in_=pB[:, 0:384].rearrange("p (h d) -> p h d", h=6), func=AF.Copy),
                 reads=[pBk], writes=[qkk + "/b"])
            if stop == 4:
                continue
            tm, tmk = tm_r.next()
            P.op("act", lambda e, tm=tm, pA=pA: e.activation(out=tm[:, 0:128], in_=pA[:, 384:512], func=AF.Copy), reads=[pAk], writes=[tmk + "/a"])
            P.op("act", lambda e, tm=tm, pB=pB: e.activation(out=tm[:, 128:256], in_=pB[:, 384:512], func=AF.Copy), reads=[pBk], writes=[tmk + "/b"])
            P.op("act", lambda e, tm=tm, pC=pC: e.activation(out=tm[:, 256:512], in_=pC[:, 0:256], func=AF.Copy), reads=[pCk], writes=[tmk + "/c"])
            P.op("dve", lambda e, tm=tm, pC=pC: e.tensor_tensor(out=tm[:, 512:520], in0=pC[:, 256:264], in1=dtbb[:], op=ALU.add), reads=[pCk, pf + "dtbb"], writes=[tmk + "/d"])
            P.op("act", lambda e, st=st, tm=tm: e.activation(out=st[:, 16:24], in_=tm[:, 512:520], func=AF.Abs), reads=[tmk + "/d", sk], writes=[sk])
            P.op("act", lambda e, st=st: e.activation(out=st[:, 16:24], in_=st[:, 16:24], func=AF.Exp, scale=-1.0), reads=[sk], writes=[sk])
            P.op("act", lambda e, st=st: e.activation(out=st[:, 16:24], in_=st[:, 16:24], func=AF.Ln, bias=onec[:]), reads=[sk], writes=[sk])
            P.op("dve", lambda e, st=st, tm=tm: e.scalar_tensor_tensor(out=tm[:, 512:520], in0=tm[:, 512:520], scalar=0.0, in1=st[:, 16:24], op0=ALU.max, op1=ALU.add),
                 reads=[tmk + "/d", sk], writes=[tmk + "/d"])
            P.dma(lambda e, tm=tm, t0=t0: e.dma_start(out=TMS[t0:t0 + 128, :], in_=tm[:]), reads=[tmk], writes=[P.u("TMS")])
            if stop == 5:
                continue
            rp, rpk = rp_r.next()
            if is_ctx:
                P.op("pool", lambda e, rp=rp, qk=qk: e.tensor_copy(out=rp[:].rearrange("p (h a) b f -> p h (a b f)", a=2), in_=qk[:]),
                     reads=[qkk + "/a", qkk + "/b"], writes=[rpk])
            else:
                cs, csk = cs_r.next()
                tl = t0 - LC
                P.dma(lambda e, cs=cs, tl=tl: e.dma_start(out=cs[:, 0, :], in_=ropec[tl:tl + 128, :]), writes=[csk + "/c"])
                P.dma(lambda e, cs=cs, tl=tl: e.dma_start(out=cs[:, 1, :], in_=ropes[tl:tl + 128, :]), writes=[csk + "/s"])
                qv = qk[:].rearrange("p h (a b f) -> p (h a) b f", a=2, b=2)
                x1, x2 = qv[:, :, 0, :], qv[:, :, 1, :]
                cb = cs[:, 0, :].rearrange("p (g f) -> p g f", f=16)
                sb_ = cs[:, 1, :].rearrange("p (g f) -> p g f", f=16)
                tmp, tmpk = tmp_r.next()
                qkr = [qkk + "/a", qkk + "/b"]
                P.op("dve", lambda e, rp=rp, x1=x1, cb=cb: e.tensor_tensor(out=rp[:, :, 0, :], in0=x1, in1=cb, op=ALU.mult), reads=qkr + [csk + "/c"], writes=[rpk + "/0"])
                P.op("pool", lambda e, tmp=tmp, x2=x2, sb_=sb_: e.tensor_tensor(out=tmp[:], in0=x2, in1=sb_, op=ALU.mult), reads=qkr + [csk + "/s"], writes=[tmpk])
                P.op("dve", lambda e, rp=rp, tmp=tmp: e.tensor_tensor(out=rp[:, :, 0, :], in0=rp[:, :, 0, :], in1=tmp[:], op=ALU.subtract), reads=[rpk + "/0", tmpk], writes=[rpk + "/0"])
                P.op("dve", lambda e, rp=rp, x2=x2, cb=cb: e.tensor_tensor(out=rp[:, :, 1, :], in0=x2, in1=cb, op=ALU.mult), reads=qkr + [csk + "/c"], writes=[rpk + "/1"])
                P.op("pool", lambda e, tmp=tmp, x1=x1, sb_=sb_: e.tensor_tensor(out=tmp[:], in0=x1, in1=sb_, op=ALU.mult), reads=qkr + [csk + "/s"], writes=[tmpk])
                P.op("dve", lambda e, rp=rp, tmp=tmp: e.tensor_tensor(out=rp[:, :, 1, :], in0=rp[:, :, 1, :], in1=tmp[:], op=ALU.add), reads=[rpk + "/1", tmpk], writes=[rpk + "/1"])
            rkeys = [rpk] if is_ctx else [rpk + "/0", rpk + "/1"]
            if stop == 6:
                continue
            rpf = rp[:].rearrange("p g b f -> p (g b f)")
            tq, tqk = tq_r.next()
            for j in range(6):
                P.op("pe", lambda e, tq=tq, rpf=rpf, j=j: e.transpose(tq[:, j * 128:(j + 1) * 128], rpf[:, j * 128:(j + 1) * 128], ident[:]),
                     reads=rkeys + [pf + "ident"], writes=[tqk], noinc=(j != 5))
            qT, qTk = qT_r.next()
            P.op("act", lambda e, qT=qT, tq=tq: e.activation(out=qT[:], in_=tq[:, 0:768].rearrange("p (j t) -> p j t", j=6), func=AF.Copy), reads=[tqk], writes=[qTk])
            P.dma(lambda e, qT=qT, t0=t0: e.dma_start(out=QKT[:, t0:t0 + 128].rearrange("(j p) t -> p j t", p=128), in_=qT[:]), reads=[qTk], writes=[P.u("QKT")])
        if stop <= 7:
            continue
        hkeys = [hTk + "/%d" % i for i in range(ntile)]
        for j in range(8):
            fm, fmk = fm_r.next()
            for k in range(8):
                P.op("pe", lambda e, fm=fm, hT=hT, k=k, j=j, cn=cn: e.matmul(fm[:, 0:cn], win[:, k, FM_OFF + j * 128:FM_OFF + (j + 1) * 128], hT[:, k, 0:cn], start=(k == 0), stop=(k == 7)),
                     reads=hkeys + [wkeys[k]], writes=[fmk], noinc=(k != 7))
            fo, fok = fo_r.next()
            eng = "act" if j % 2 == 0 else "dve"
            if eng == "act":
                P.op("act", lambda e, fo=fo, fm=fm, cn=cn: e.activation(out=fo[:, 0:cn], in_=fm[:, 0:cn], func=AF.Copy), reads=[fmk], writes=[fok])
            else:
                P.op("dve", lambda e, fo=fo, fm=fm, cn=cn: e.tensor_copy(out=fo[:, 0:cn], in_=fm[:, 0:cn]), reads=[fmk], writes=[fok])
            P.dma(lambda e, fo=fo, j=j, c0=c0, cn=cn: e.dma_start(out=FMT[j * 128:(j + 1) * 128, c0:c0 + cn], in_=fo[:, 0:cn]), reads=[fok], writes=[P.u("FMT")])


def phase_g2(P, Dm, l, tok_chunks, final):
    pf = "G%d_" % l
    ntf = sum(cn for _, cn in tok_chunks) // 128
    NB = 2 * ntf + 32
    X1 = Dm.get("X1", [T, D])
    BUF = Dm.get("BUF%d" % l, [NB * 128, D])
    OBUF = Dm.get("OBUF%d" % l, [NB * 128, D])
    IDXW = Dm.get("IDXW%d" % l, [128, NB], I32)
    DEST = Dm.get("DEST%d" % l, [128, ntf * 2], I32)
    WWd = Dm.get("WWd%d" % l, [128, ntf * 2])
    WGU = Dm.get("moe_gu%d" % l, [32, 128, 8, 1024], kind="ExternalInput")
    WD = Dm.get("moe_d%d" % l, [32, 128, 4, 1024], kind="ExternalInput")
    ident_d = Dm.get("ident", [128, 128], kind="ExternalInput")
    modrow = Dm.get("modrow%d" % l, [2, 6144])
    mk = "modrow%d" % l
    ident = P.sb(pf + "ident", [128, 128]); P.d(ident[:], ident_d[:, :], writes=[pf + "ident"])
    idxw = P.sb(pf + "idxw", [128, NB], I32); P.d(idxw[:], IDXW[:, :], reads=["IDXW%d" % l], writes=[pf + "idxw"])
    dest = P.sb(pf + "dest", [128, ntf * 2], I32); P.d(dest[:], DEST[:, :], reads=["DEST%d" % l], writes=[pf + "dest"])
    ww = P.sb(pf + "ww", [128, ntf * 2]); P.d(ww[:], WWd[:, :], reads=["WWd%d" % l], writes=[pf + "ww"])
    wgu_r = Rot(P, pf + "wgu", [128, 8, 1024], F32R, n=2)
    wd_r = Rot(P, pf + "wd", [128, 4, 1024], F32R, n=2)
    xs_r = Rot(P, pf + "xs", [128, D], n=3)
    tp_r = Rot(P, pf + "tp", [128, 1024], n=1, psum=True)
    xT_r = Rot(P, pf + "xT", [128, 8, 128], F32R, n=2)
    pg_r = Rot(P, pf + "pg", [128, 512], n=1, psum=True)
    pu_r = Rot(P, pf + "pu", [128, 512], n=1, psum=True)
    sg_r = Rot(P, pf + "sg", [128, 512], n=2)
    hu_r = Rot(P, pf + "hu", [128, 512], n=2)
    ph_r = Rot(P, pf + "ph", [128, 512], n=1, psum=True)
    hT_r = Rot(P, pf + "hT", [128, 4, 128], F32R, n=2)
    po_r = Rot(P, pf + "po", [128, 1024], n=1, psum=True)
    ob_r = Rot(P, pf + "ob", [128, D], n=2)
    WGUf = WGU.rearrange("e p k n -> (e p) (k n)")
    WDf = WD.rearrange("e p k n -> (e p) (k n)")
    st1 = {}
    breg = P.bound()
    hNB = NB // 2
    seq = []
    for i_ in range(hNB):
        seq += [i_, hNB + i_]

    def load_gu(b):
        off = bass.IndirectOffsetOnAxis(ap=idxw[:, seq[b]:seq[b] + 1], axis=0)
        wgu, wguk = wgu_r.next()
        P.dma(lambda e, wgu=wgu, off=off: e.indirect_dma_start(out=wgu[:].rearrange("p k n -> p (k n)"), out_offset=None, in_=WGUf, in_offset=off, bounds_check=breg, oob_is_err=False), reads=[pf + "idxw"], writes=[wguk], q="pool")
        st1[("gu", b)] = (wgu, wguk)

    def load_d(b):
        off = bass.IndirectOffsetOnAxis(ap=idxw[:, seq[b]:seq[b] + 1], axis=0)
        wd, wdk = wd_r.next()
        P.dma(lambda e, wd=wd, off=off: e.indirect_dma_start(out=wd[:].rearrange("p k n -> p (k n)"), out_offset=None, in_=WDf, in_offset=off, bounds_check=breg, oob_is_err=False), reads=[pf + "idxw"], writes=[wdk], q="pool")
        st1[("d", b)] = (wd, wdk)

    def s1a(b):
        xs, xsk = xs_r.next()
        P.d(xs[:], BUF[seq[b] * 128:(seq[b] + 1) * 128, :], reads=["BUF%d" % l, "BUFz%d" % l], writes=[xsk])
        tp, tpk = tp_r.next()
        for k in range(8):
            P.i("pe", "transpose", reads=[xsk, pf + "ident"], writes=[tpk], noinc=(k != 7), out=tp[:, k * 128:(k + 1) * 128], in_=xs[:, k * 128:(k + 1) * 128], identity=ident[:])
        xT, xTk = xT_r.next()
        P.i("act", "activation", reads=[tpk], writes=[xTk + "/0"], out=xT[:, 0:4, :], in_=tp[:, 0:512].rearrange("p (k t) -> p k t", k=4), func=AF.Copy)
        P.i("dve", "tensor_copy", reads=[tpk], writes=[xTk + "/1"], out=xT[:, 4:8, :], in_=tp[:, 512:1024].rearrange("p (k t) -> p k t", k=4))
        st1[("xT", b)] = (xT, xTk)

    def s1b(b):
        (wgu, wguk) = st1.pop(("gu", b))
        (xT, xTk) = st1.pop(("xT", b))
        pg, pgk = pg_r.next(); pu, puk = pu_r.next()
        for k in range(8):
            P.i("pe", "matmul", reads=[xTk, wguk], writes=[pgk], out=pg[:], lhsT=xT[:, k, :], rhs=wgu[:, k, 0:512], start=(k == 0), stop=(k == 7))
        for k in range(8):
            P.i("pe", "matmul", reads=[xTk, wguk], writes=[puk], out=pu[:], lhsT=xT[:, k, :], rhs=wgu[:, k, 512:1024], start=(k == 0), stop=(k == 7))
        sg, sgk = sg_r.next()
        P.i("act", "activation", reads=[pgk], writes=[sgk], out=sg[:], in_=pg[:], func=AF.Silu)
        hu, huk = hu_r.next()
        P.i("dve", "tensor_tensor", reads=[sgk, puk], writes=[huk], out=hu[:], in0=sg[:], in1=pu[:], op=ALU.mult)
        st1[("h", b)] = (hu, huk)

    def stage2(b):
        (hu, huk) = st1.pop(("h", b))
        (wd, wdk) = st1.pop(("d", b))
        ph, phk = ph_r.next()
        for hc in range(4):
            P.i("pe", "transpose", reads=[huk, pf + "ident"], writes=[phk], noinc=(hc != 3), out=ph[:, hc * 128:(hc + 1) * 128], in_=hu[:, hc * 128:(hc + 1) * 128], identity=ident[:])
        hT, hTk = hT_r.next()
        P.i("act", "activation", reads=[phk], writes=[hTk], out=hT[:], in_=ph[:].rearrange("p (k t) -> p k t", k=4), func=AF.Copy)
        po, pok = po_r.next()
        for hf in range(2):
            for hc in range(4):
                P.i("pe", "matmul", reads=[hTk, wdk], writes=[pok], out=po[:, hf * 512:(hf + 1) * 512], lhsT=hT[:, hc, :], rhs=wd[:, hc, hf * 512:(hf + 1) * 512], start=(hc == 0), stop=(hc == 3))
        ob, obk = ob_r.next()
        P.i("act", "activation", reads=[pok], writes=[obk + "/0"], out=ob[:, 0:512], in_=po[:, 0:512], func=AF.Copy)
        P.i("dve", "tensor_copy", reads=[pok], writes=[obk + "/1"], out=ob[:, 512:1024], in_=po[:, 512:1024])
        P.d(OBUF[seq[b] * 128:(seq[b] + 1) * 128, :], ob[:], reads=[obk], writes=[P.u("OBUF%d" % l)])

    load_gu(0); load_d(0); load_gu(1); load_d(1)
    s1a(0); s1b(0)
    if NB > 2:
        load_gu(2)
    s1a(1)
    for b in range(NB):
        stage2(b)
        if b + 2 < NB:
            load_d(b + 2)
        if b + 1 < NB:
            s1b(b + 1)
            if b + 3 < NB:
                load_gu(b + 3)
        if b + 2 < NB:
            s1a(b + 2)
    if final:
        fg_d = Dm.get("final_g", [1, D], kind="ExternalInput")
        OUT = Dm.get("out", [L, D], kind="ExternalOutput")
        fg = P.sb(pf + "fg", [128, D]); load_bcast(P, fg[:], pf + "fg", fg_d[0:1, :])
        epsc = P.sb(pf + "eps", [128, 1]); P.i("dve", "memset", writes=[pf + "eps"], ap=epsc[:], constant=EPS)
        junk = P.sb(pf + "junk", [128, D])
    else:
        XN = Dm.get("XN%d" % l, [T, D])
    G2g = [P.sb(pf + "g2%d" % r, [128, D]) for r in range(2)]
    for r in range(2):
        load_bcast(P, G2g[r][:], pf + "g2%d" % r, modrow[r:r + 1, 5120:6144], reads=[mk])
    o1_r = Rot(P, pf + "o1", [128, D], n=2)
    o2_r = Rot(P, pf + "o2", [128, D], n=2)
    xt_r = Rot(P, pf + "xt", [128, D], n=2)
    st_r = Rot(P, pf + "st", [128, 8], n=2)
    ti = 0
    for (c0, cn) in tok_chunks:
        for i in range(cn // 128):
            t0 = c0 + i * 128
            r = 1 if t0 < LC else 0
            o1, o1k = o1_r.next(); o2, o2k = o2_r.next()
            for (o, ok_, col) in ((o1, o1k, ti * 2), (o2, o2k, ti * 2 + 1)):
                P.dma(lambda e, o=o, col=col: e.indirect_dma_start(out=o[:], out_offset=None, in_=OBUF[:, :], in_offset=bass.IndirectOffsetOnAxis(ap=dest[:, col:col + 1], axis=0)),
                      reads=[pf + "dest", "OBUF%d" % l], writes=[ok_], q="pool")
            xt, xk = xt_r.next()
            P.d(xt[:], X1[t0:t0 + 128, :], reads=["X1"], writes=[xk])
            P.i("dve", "tensor_scalar", reads=[o1k, pf + "ww"], writes=[o1k], out=o1[:], in0=o1[:], scalar1=ww[:, ti * 2:ti * 2 + 1], scalar2=None, op0=ALU.mult)
            P.i("dve", "scalar_tensor_tensor", reads=[o1k, o2k, pf + "ww"], writes=[o1k], out=o1[:], in0=o2[:], scalar=ww[:, ti * 2 + 1:ti * 2 + 2], in1=o1[:], op0=ALU.mult, op1=ALU.add)
            P.i("pool", "tensor_tensor", reads=[o1k, pf + "g2%d" % r], writes=[o1k], out=o1[:], in0=o1[:], in1=G2g[r][:], op=ALU.mult)
            P.i("dve", "tensor_tensor", reads=[o1k, xk], writes=[xk], out=xt[:], in0=xt[:], in1=o1[:], op=ALU.add)
            if not final:
                P.d(XN[t0:t0 + 128, :], xt[:], reads=[xk], writes=[P.u("XN%d" % l)])
            else:
                st, sk = st_r.next()
                P.i("act", "activation", reads=[xk], writes=[pf + "junk", sk], out=junk[:], in_=xt[:], func=AF.Square, accum_out=st[:, 0:1])
                P.i("act", "activation", reads=[sk, pf + "eps"], writes=[sk], out=st[:, 1:2], in_=st[:, 0:1], func=AF.Sqrt, bias=epsc[:], scale=1.0 / D)
                P.i("dve", "reciprocal", reads=[sk], writes=[sk], out=st[:, 2:3], in_=st[:, 1:2])
                P.i("dve", "scalar_tensor_tensor", reads=[xk, sk, pf + "fg"], writes=[xk], out=xt[:], in0=xt[:], scalar=st[:, 2:3], in1=fg[:], op0=ALU.mult, op1=ALU.mult)
                P.d(OUT[t0 - LC:t0 - LC + 128, :], xt[:], reads=[xk], writes=[P.u("out")], final=True)
            ti += 1


def rope_tables_np():
    rows = np.repeat(np.arange(L // 64), 64)
    cols = np.tile(np.arange(64), L // 64)
    inv = np.power(np.float32(10000.0), -np.arange(16, dtype=np.float32) / np.float32(16)).astype(np.float32)
    ang = np.stack([rows, cols], -1).astype(np.float32)[..., None] * inv
    cos = np.cos(ang).astype(np.float32).reshape(L, 1, 32)
    sin = np.sin(ang).astype(np.float32).reshape(L, 1, 32)
    return (np.ascontiguousarray(np.broadcast_to(cos, (L, 12, 32)).reshape(L, 384)),
            np.ascontiguousarray(np.broadcast_to(sin, (L, 12, 32)).reshape(L, 384)))


def kmajor(w):
    K, N = w.shape
    return np.ascontiguousarray(w.reshape(K // 128, 128, N).transpose(1, 0, 2))


def core_inputs(inp, b):
    f = np.float32
    m = {}
    m["xin"] = np.ascontiguousarray(np.concatenate([inp["ctx"][b], inp["x"][b]], 0).astype(f))
    cv = np.stack([inp["c"][b], inp["c_ctx"]], -1)
    m["cvec"] = np.ascontiguousarray(cv.reshape(8, 128, 2).transpose(1, 0, 2).astype(f))
    m["ident"] = np.eye(128, dtype=f)
    sh = np.zeros((128, 64), f); sh[64 + np.arange(64), np.arange(64)] = 1.0
    m["shiftm"] = sh
    jj, ii = np.meshgrid(np.arange(128), np.arange(128), indexing="ij")
    wm = np.zeros((128, 2, 2, 128), f)
    wm[:, 0] = (ii <= jj).astype(f)[:, None, :]
    wm[:, 1] = (jj <= ii).astype(f)[:, None, :]
    m["wmask"] = np.ascontiguousarray(wm.reshape(128, 2, 256))
    m["rope_cos"], m["rope_sin"] = rope_tables_np()
    for l in range(2):
        m["ada_w%d" % l] = kmajor(inp["ada_w"][l])
        m["ada_b%d" % l] = np.ascontiguousarray(inp["ada_b"][l].reshape(1, 6144))
        m["w_in%d" % l] = kmajor(inp["w_in"][l][:, PERM])
        m["norm1_g%d" % l] = np.ascontiguousarray(inp["norm1_g"][l].reshape(1, D))
        m["qkg%d" % l] = np.ascontiguousarray(np.concatenate([np.tile(inp["ga_qn_g"][l], 4), np.tile(inp["ga_kn_g"][l], 2)]).reshape(1, 384))
        m["dtb%d" % l] = np.ascontiguousarray(inp["ssd_dt_bias"][l].reshape(1, 8))
        m["sink%d" % l] = np.ascontiguousarray(inp["wa_sink"][l].reshape(1, 4))
        m["w_out%d" % l] = kmajor(inp["w_out"][l])
        m["norm2_g%d" % l] = np.ascontiguousarray(inp["norm2_g"][l].reshape(1, D))
        m["wr%d" % l] = kmajor(np.concatenate([inp["moe_coarse_w"][l], inp["moe_fine_w"][l]], 1))
        m["rb%d" % l] = np.ascontiguousarray(np.concatenate([inp["moe_coarse_b"][l], inp["moe_fine_b"][l]]).reshape(1, 36))
        m["moe_g%d" % l] = np.ascontiguousarray(inp["moe_w_gate"][l].reshape(32, 8, 128, 512).transpose(0, 2, 1, 3))
        m["moe_u%d" % l] = np.ascontiguousarray(inp["moe_w_up"][l].reshape(32, 8, 128, 512).transpose(0, 2, 1, 3))
        m["moe_gu%d" % l] = np.ascontiguousarray(np.concatenate([m["moe_g%d" % l], m["moe_u%d" % l]], -1))
        m["moe_d%d" % l] = np.ascontiguousarray(inp["moe_w_down"][l].reshape(32, 4, 128, 1024).transpose(0, 2, 1, 3))
    m["final_g"] = np.ascontiguousarray(inp["final_g"].reshape(1, D))
    m["ramp"] = (128.0 * np.arange(72) + 1.0).astype(f).reshape(1, 72)
    m["bst"] = (128.0 * np.arange(104)).astype(f).reshape(1, 104)
    m["pidx"] = np.arange(128).astype(f).reshape(128, 1)
    sp, s_ = np.meshgrid(np.arange(128), np.arange(128), indexing="ij")
    m["stri"] = np.ascontiguousarray(np.stack([(sp < s_), np.ones_like(sp, dtype=bool)], 1).astype(f))
    m["tri"] = np.ascontiguousarray(np.stack([(sp <= s_), (sp >= s_)], 1).astype(f))
    for l in range(2):
        Bm = np.zeros((2, 2, 8, 128, 128), f)
        Cm = np.zeros((2, 2, 8, 128, 128), f)
        for d in range(2):
            for j in range(8):
                for gg in range(2):
                    g = 2 * j + gg
                    r0 = (2 * (j % 4) + gg) * 16
                    Bm[d, 0, j, r0:r0 + 16, gg * 64:(gg + 1) * 64] = inp["s5_b_re"][l, d, g].T
                    Bm[d, 1, j, r0:r0 + 16, gg * 64:(gg + 1) * 64] = inp["s5_b_im"][l, d, g].T
                    Cm[d, 0, j, gg * 64:(gg + 1) * 64, r0:r0 + 16] = inp["s5_c_re"][l, d, g].T
                    Cm[d, 1, j, gg * 64:(gg + 1) * 64, r0:r0 + 16] = inp["s5_c_im"][l, d, g].T
        m["s5B%d" % l] = Bm
        m["s5C%d" % l] = Cm
        lamt = np.zeros((128, 3, 16), f)
        for d in range(2):
            for j in range(8):
                for gg in range(2):
                    g = 2 * j + gg
                    lamt[gg * 64:(gg + 1) * 64, 0, d * 8 + j] = inp["s5_lam_re"][l, d, g]
                    lamt[gg * 64:(gg + 1) * 64, 1, d * 8 + j] = inp["s5_lam_im"][l, d, g]
                    lamt[gg * 64:(gg + 1) * 64, 2, d * 8 + j] = inp["s5_log_dt"][l, d, g]
        m["s5lam%d" % l] = lamt
        cw = np.concatenate([inp["ssd_conv_w"][l], inp["ssd_conv_b"][l][None]], 0)
        m["convw%d" % l] = np.ascontiguousarray(cw.reshape(4, 6, 128).transpose(2, 1, 0).astype(f))
        m["alog%d" % l] = np.ascontiguousarray(inp["ssd_a_log"][l].reshape(1, 8))
        m["ssdd%d" % l] = np.ascontiguousarray(inp["ssd_d"][l].reshape(1, 4))
        m["ssdng%d" % l] = np.ascontiguousarray(inp["ssd_norm_g"][l].reshape(1, 256))
        m["s5d%d" % l] = np.ascontiguousarray(inp["s5_d"][l].reshape(2, 128).T.astype(f))
        m["gluw%d" % l] = np.ascontiguousarray(inp["s5_glu_w"][l].reshape(2, 128, 256).transpose(1, 0, 2).astype(f))
        m["glub%d" % l] = np.ascontiguousarray(inp["s5_glu_b"][l].reshape(2, 128).T.astype(f))
    for l in range(0):
        pass
    return m


def attn_consts(P, Dm, pf):
    c = {}
    shift_d = Dm.get("shiftm", [128, 64], kind="ExternalInput")
    c["shift"] = P.sb(pf + "shift", [128, 64], F32R)
    P.d(c["shift"][:], shift_d[:, :], writes=[pf + "shift"], q="pool")
    return c


def phase_c(P, Dm, l, ctx_out):
    pf = "c%d_" % l
    QKT = Dm.get("QKT", [768, T])
    TMS = Dm.get("TMS", [T, 520])
    CAT = Dm.get("CAT", [1024, T])
    cst = attn_consts(P, Dm, pf)
    KT = P.sb(pf + "KT", [64, 2, T], F32R)
    V = P.sb(pf + "V", [128, NT, 2, 128], F32R)
    for kv in range(2):
        P.d(KT[:, kv, :], QKT[256 + kv * 64:256 + (kv + 1) * 64, :], reads=["QKT"], writes=[pf + "KT"], q="pool")
    P.i("dve", "memset", writes=[pf + "V/1"], ap=V[:].bitcast(F32)[:, :, :, 64:128], constant=1.0)
    for kv in range(2):
        P.d(V[:, :, kv, 0:64], TMS[:, kv * 64:(kv + 1) * 64].rearrange("(n p) d -> p n d", p=128), reads=["TMS"], writes=[pf + "V/0%d" % kv], q="pool")
    vkeys = [pf + "V"]
    q_r = Rot(P, pf + "q", [64, 4, 256], F32R, n=2)
    s_r = Rot(P, pf + "s", [128, 1024], n=2, psum=True)
    p_r = Rot(P, pf + "p", [128, 1024], F32R, n=3)
    o_r = Rot(P, pf + "o", [128, 512], n=2, psum=True)
    os_r = Rot(P, pf + "os", [128, 512], F32R, n=2)
    dn_r = Rot(P, pf + "dn", [64, 512], n=1, psum=True)
    rd_r = Rot(P, pf + "rd", [64, 512], n=2)
    ot_r = Rot(P, pf + "ot", [64, 512], n=2)
    qtiles = [(LC + 256 * i, NT) for i in range(L // 256)]
    if ctx_out:
        qtiles = [(0, 2)] + qtiles
    for (q0, nkb) in qtiles:
        qt, qtk = q_r.next()
        P.d(qt[:], QKT[0:256, q0:q0 + 256].rearrange("(h d) q -> d h q", d=64), reads=["QKT"], writes=[qtk], q="pool")
        for kv in range(2):
            o, ok = o_r.next()
            its = list(range(nkb // 2))
            pend = []

            def issue_s(sp):
                sp_, spk = s_r.next()
                for u in range(2):
                    s = 2 * sp + u
                    P.i("pe", "matmul", reads=[pf + "KT", qtk], writes=[spk], out=sp_[:, u * 512:(u + 1) * 512], lhsT=KT[:, kv, s * 128:(s + 1) * 128],
                        rhs=qt[:, 2 * kv:2 * kv + 2, :], start=True, stop=True)
                pt, ptk = p_r.next()
                P.i("act", "activation", reads=[spk], writes=[ptk], out=pt[:], in_=sp_[:], func=AF.Exp, scale=0.125)
                return (sp, pt, ptk)

            LOOK = 1
            for sp in its[:LOOK]:
                pend.append(issue_s(sp))
            for idx, sp in enumerate(its):
                (sp_i, pt, ptk) = pend.pop(0)
                if idx + LOOK < len(its):
                    pend.append(issue_s(its[idx + LOOK]))
                for u in range(2):
                    s_ = 2 * sp_i + u
                    P.i("pe", "matmul", reads=[ptk] + vkeys, writes=[ok], out=o[:], lhsT=V[:, s_, kv, :], rhs=pt[:, u * 512:(u + 1) * 512],
                        start=(idx == 0 and u == 0), stop=(idx == len(its) - 1 and u == 1))
            osb, osk = os_r.next()
            P.i("act", "activation", reads=[ok], writes=[osk], out=osb[:], in_=o[:], func=AF.Copy)
            dn, dnk = dn_r.next()
            P.i("pe", "matmul", reads=[osk, pf + "shift"], writes=[dnk], out=dn[:], lhsT=cst["shift"][:], rhs=osb[:], start=True, stop=True)
            rd, rdk = rd_r.next()
            P.i("dve", "reciprocal", reads=[dnk], writes=[rdk], out=rd[:], in_=dn[:])
            ot, otk = ot_r.next()
            P.i("dve", "tensor_tensor", reads=[osk, rdk], writes=[otk], out=ot[:], in0=osb[0:64, :].bitcast(F32), in1=rd[:], op=ALU.mult)
            P.d(CAT[256 + kv * 128:256 + (kv + 1) * 128, q0:q0 + 256].rearrange("(hh d) q -> d hh q", d=64),
                ot[:].rearrange("d (hh q) -> d hh q", hh=2), reads=[otk], writes=[P.u("CAT")])


def phase_e(P, Dm, l, ctx_out):
    pf = "e%d_" % l
    QKT = Dm.get("QKT", [768, T])
    TMS = Dm.get("TMS", [T, 520])
    CAT = Dm.get("CAT", [1024, T])
    sink_d = Dm.get("sink%d" % l, [1, 4], kind="ExternalInput")
    mask_d = Dm.get("wmask", [128, 2, 256], kind="ExternalInput")
    cst = attn_consts(P, Dm, pf)
    KT = P.sb(pf + "KT", [64, 2, T], F32R)
    V = P.sb(pf + "V", [128, NT, 2, 128], F32R)
    for kv in range(2):
        P.d(KT[:, kv, :], QKT[640 + kv * 64:640 + (kv + 1) * 64, :], reads=["QKT"], writes=[pf + "KT"], q="pool")
    P.i("dve", "memset", writes=[pf + "V/1"], ap=V[:].bitcast(F32)[:, :, :, 64:128], constant=1.0)
    for kv in range(2):
        P.d(V[:, :, kv, 0:64], TMS[:, 128 + kv * 64:128 + (kv + 1) * 64].rearrange("(n p) d -> p n d", p=128), reads=["TMS"], writes=[pf + "V/0%d" % kv], q="pool")
    vkeys = [pf + "V"]
    mask = P.sb(pf + "mask", [128, 2, 256])
    P.d(mask[:], mask_d[:, :, :], writes=[pf + "mask"])
    esk = P.sb(pf + "esk", [64, 4])
    P.d(esk[:], sink_d[0:1, :].to_broadcast([64, 4]), writes=[pf + "esk"])
    P.i("act", "activation", reads=[pf + "esk"], writes=[pf + "esk"], out=esk[:], in_=esk[:], func=AF.Exp)
    q_r = Rot(P, pf + "q", [64, 4, 128], F32R, n=2)
    s_r = Rot(P, pf + "s", [128, 256], n=3, psum=True)
    p_r = Rot(P, pf + "p", [128, 256], F32R, n=3)
    o_r = Rot(P, pf + "o", [128, 256], n=2, psum=True)
    os_r = Rot(P, pf + "os", [128, 256], F32R, n=2)
    dn_r = Rot(P, pf + "dn", [64, 256], n=1, psum=True)
    rd_r = Rot(P, pf + "rd", [64, 256], n=2)
    ot_r = Rot(P, pf + "ot", [64, 256], n=2)
    qtiles = []
    if ctx_out:
        qtiles += [(i, [(0, None), (1, None)]) for i in range(2)]
    for n in range(L // 128):
        ti = 2 + n
        kb = [(0, None), (1, None)]
        if n > 0:
            kb.append((ti - 1, 0))
        kb.append((ti, None))
        if n < L // 128 - 1:
            kb.append((ti + 1, 1))
        qtiles.append((ti, kb))
    for (ti, kbs) in qtiles:
        q0 = ti * 128
        qt, qtk = q_r.next()
        P.d(qt[:], QKT[384:640, q0:q0 + 128].rearrange("(h d) q -> d h q", d=64), reads=["QKT"], writes=[qtk], q="pool")
        for kv in range(2):
            o, ok = o_r.next()
            for idx, (s, mi) in enumerate(kbs):
                sp_, spk = s_r.next()
                P.i("pe", "matmul", reads=[pf + "KT", qtk], writes=[spk], out=sp_[:], lhsT=KT[:, kv, s * 128:(s + 1) * 128],
                    rhs=qt[:, 2 * kv:2 * kv + 2, :], start=True, stop=True)
                pt, ptk = p_r.next()
                P.i("act", "activation", reads=[spk], writes=[ptk], out=pt[:], in_=sp_[:], func=AF.Exp, scale=0.125)
                if mi is not None:
                    P.i("dve", "tensor_tensor", reads=[ptk, pf + "mask"], writes=[ptk], out=pt[:], in0=pt[:].bitcast(F32), in1=mask[:, mi, :], op=ALU.mult)
                P.i("pe", "matmul", reads=[ptk] + vkeys, writes=[ok], out=o[:], lhsT=V[:, s, kv, :], rhs=pt[:],
                    start=(idx == 0), stop=(idx == len(kbs) - 1))
            osb, osk = os_r.next()
            P.i("act", "activation", reads=[ok], writes=[osk], out=osb[:], in_=o[:], func=AF.Copy)
            dn, dnk = dn_r.next()
            P.i("pe", "matmul", reads=[osk, pf + "shift"], writes=[dnk], out=dn[:], lhsT=cst["shift"][:], rhs=osb[:], start=True, stop=True)
            rd, rdk = rd_r.next()
            for hh in range(2):
                h = 2 * kv + hh
                P.i("dve", "tensor_scalar", reads=[dnk, pf + "esk"], writes=[rdk + "/%d" % hh], out=rd[:, hh * 128:(hh + 1) * 128], in0=dn[:, hh * 128:(hh + 1) * 128],
                    scalar1=esk[:, h:h + 1], scalar2=None, op0=ALU.add)
            P.i("dve", "reciprocal", reads=[rdk], writes=[rdk], out=rd[:], in_=rd[:])
            ot, otk = ot_r.next()
            P.i("dve", "tensor_tensor", reads=[osk, rdk], writes=[otk], out=ot[:], in0=osb[0:64, :].bitcast(F32), in1=rd[:], op=ALU.mult)
            P.d(CAT[768 + kv * 128:768 + (kv + 1) * 128, q0:q0 + 128].rearrange("(hh d) q -> d hh q", d=64),
                ot[:].rearrange("d (hh q) -> d hh q", hh=2), reads=[otk], writes=[P.u("CAT")])


BIG = 1.0e30
S5_ENG2 = "dve"


def phase_f(P, Dm, l, src_name, tok_chunks=None):
    pf = "f%d_" % l
    xsrc = Dm.get(src_name, [T, D])
    CAT = Dm.get("CAT", [1024, T])
    w_out = Dm.get("w_out%d" % l, [128, 8, 1024], kind="ExternalInput")
    modrow = Dm.get("modrow%d" % l, [2, 6144])
    n2g = Dm.get("norm2_g%d" % l, [1, D], kind="ExternalInput")
    wr_d = Dm.get("wr%d" % l, [128, 8, 36], kind="ExternalInput")
    rb_d = Dm.get("rb%d" % l, [1, 36], kind="ExternalInput")
    ident_d = Dm.get("ident", [128, 128], kind="ExternalInput")
    X1 = Dm.get("X1", [T, D])
    H2T = Dm.get("H2T", [D, T])
    WTd = Dm.get("WTd", [32, T])
    mk = "modrow%d" % l
    ident = P.sb(pf + "ident", [128, 128])
    P.d(ident[:], ident_d[:, :], writes=[pf + "ident"])
    wo = P.sb(pf + "wo", [128, 8, 1024], F32R)
    for k in range(8):
        P.d(wo[:, k, :], w_out[:, k, :], writes=[pf + "wo/%d" % k], q="pool")
    wr = P.sb(pf + "wr", [128, 8, 36])
    P.d(wr[:], wr_d[:, :, :], writes=[pf + "wr"])
    rb = P.sb(pf + "rb", [128, 36])
    load_bcast(P, rb[:], pf + "rb", rb_d[0:1, :])
    G1 = [P.sb(pf + "G1%d" % r, [128, D]) for r in range(2)]
    G2 = [P.sb(pf + "G2%d" % r, [128, D]) for r in range(2)]
    SH2 = [P.sb(pf + "SH2%d" % r, [128, D]) for r in range(2)]
    gn = P.sb(pf + "gn", [128, D])
    load_bcast(P, gn[:], pf + "gn", n2g[0:1, :])
    for r in range(2):
        load_bcast(P, G1[r][:], pf + "G1%d" % r, modrow[r:r + 1, 2048:3072], reads=[mk])
        load_bcast(P, SH2[r][:], pf + "SH2%d" % r, modrow[r:r + 1, 3072:4096], reads=[mk])
        load_bcast(P, G2[r][:], pf + "G2%d" % r, modrow[r:r + 1, 4096:5120], reads=[mk])
        P.i("dve", "scalar_tensor_tensor", reads=[pf + "G2%d" % r, pf + "gn"], writes=[pf + "G2%d" % r], out=G2[r][:], in0=G2[r][:], scalar=1.0, in1=gn[:], op0=ALU.add, op1=ALU.mult)
    epsc = P.sb(pf + "eps", [128, 1])
    P.i("dve", "memset", writes=[pf + "eps"], ap=epsc[:], constant=EPS)
    ct_r = Rot(P, pf + "ct", [128, 8, 512], F32R, n=2)
    po_r = Rot(P, pf + "po", [128, 1024], n=1, psum=True)
    xt_r = Rot(P, pf + "xt", [128, D], n=2)
    x1_r = Rot(P, pf + "x1", [128, D], n=2)
    h_r = Rot(P, pf + "h", [128, D], n=2)
    junk = P.sb(pf + "junk", [128, D])
    st_r = Rot(P, pf + "st", [128, 64], n=3)
    tp_r = Rot(P, pf + "tp", [128, 1024], n=1, psum=True)
    hT_r = Rot(P, pf + "hT", [128, 8, 128], F32R, n=2)
    hTf_r = Rot(P, pf + "hTf", [128, 8, 128], n=2)
    pr_r = Rot(P, pf + "pr", [128, 512], n=1, psum=True)
    lg_r = Rot(P, pf + "lg", [128, 36], n=2)
    mk_r = Rot(P, pf + "mk", [128, 4, 8], n=2)
    oh_r = Rot(P, pf + "oh", [128, 3, 32], n=2)
    wt_r = Rot(P, pf + "wt", [128, 32], n=2)
    pw_r = Rot(P, pf + "pw", [128, 512], n=1, psum=True)
    wT_r = Rot(P, pf + "wT", [32, 128], n=2)
    for (c0, cn) in (tok_chunks or CHUNKS):
        ct, ctk = ct_r.next()
        P.d(ct[:, :, 0:cn], CAT[:, c0:c0 + cn].rearrange("(k p) t -> p k t", p=128), reads=["CAT"], writes=[ctk], q="pool")
        for i in range(cn // 128):
            t0 = c0 + i * 128
            r = 1 if t0 < LC else 0
            po, pok = po_r.next()
            for hf in range(2):
                for k in range(8):
                    P.i("pe", "matmul", reads=[ctk, pf + "wo/%d" % k], writes=[pok], out=po[:, hf * 512:(hf + 1) * 512], lhsT=ct[:, k, i * 128:(i + 1) * 128],
                        rhs=wo[:, k, hf * 512:(hf + 1) * 512], start=(k == 0), stop=(k == 7))
            xt, xk = xt_r.next()
            P.d(xt[:], xsrc[t0:t0 + 128, :], reads=[src_name], writes=[xk])
            x1, x1k = x1_r.next()
            for hf in range(2):
                sl = slice(hf * 512, (hf + 1) * 512)
                P.i("dve", "tensor_tensor", reads=[pok, pf + "G1%d" % r], writes=[x1k + "/%d" % hf], out=x1[:, sl], in0=po[:, sl], in1=G1[r][:, sl], op=ALU.mult)
            P.i("pool", "tensor_tensor", reads=[x1k, xk], writes=[x1k], out=x1[:], in0=x1[:], in1=xt[:], op=ALU.add)
            P.d(X1[t0:t0 + 128, :], x1[:], reads=[x1k], writes=[P.u("X1")])
            st, sk = st_r.next()
            P.i("act", "activation", reads=[x1k], writes=[pf + "junk", sk], out=junk[:], in_=x1[:], func=AF.Square, accum_out=st[:, 0:1])
            P.i("act", "activation", reads=[sk, pf + "eps"], writes=[sk], out=st[:, 1:2], in_=st[:, 0:1], func=AF.Sqrt, bias=epsc[:], scale=1.0 / D)
            P.i("dve", "reciprocal", reads=[sk], writes=[sk], out=st[:, 2:3], in_=st[:, 1:2])
            h, hk = h_r.next()
            P.i("dve", "scalar_tensor_tensor", reads=[x1k, sk, pf + "G2%d" % r], writes=[hk], out=h[:], in0=x1[:], scalar=st[:, 2:3], in1=G2[r][:], op0=ALU.mult, op1=ALU.mult)
            P.i("pool", "tensor_tensor", reads=[hk, pf + "SH2%d" % r], writes=[hk], out=h[:], in0=h[:], in1=SH2[r][:], op=ALU.add)
            tp, tpk = tp_r.next()
            for k in range(8):
                P.i("pe", "transpose", reads=[hk, pf + "ident"], writes=[tpk], noinc=(k != 7), out=tp[:, k * 128:(k + 1) * 128], in_=h[:, k * 128:(k + 1) * 128], identity=ident[:])
            hT, hTk = hT_r.next()
            hTf, hTfk = hTf_r.next()
            P.i("act", "activation", reads=[tpk], writes=[hTk], out=hT[:], in_=tp[:].rearrange("p (k t) -> p k t", k=8), func=AF.Copy)
            P.i("dve", "tensor_copy", reads=[tpk], writes=[hTfk], out=hTf[:], in_=tp[:].rearrange("p (k t) -> p k t", k=8))
            P.d(H2T[:, t0:t0 + 128].rearrange("(k p) t -> p k t", p=128), hT[:].bitcast(F32), reads=[hTk], writes=[P.u("H2T")])
            pr, prk = pr_r.next()
            for k in range(8):
                P.i("pe", "matmul", reads=[hTfk, pf + "wr"], writes=[prk], out=pr[:, 0:36], lhsT=hTf[:, k, :], rhs=wr[:, k, :], start=(k == 0), stop=(k == 7))
            lg, lgk = lg_r.next()
            P.i("dve", "tensor_tensor", reads=[prk, pf + "rb"], writes=[lgk], out=lg[:], in0=pr[:, 0:36], in1=rb[:], op=ALU.add)
            P.i("dve", "tensor_reduce", reads=[lgk], writes=[sk], out=st[:, 4:5], in_=lg[:, 0:4], axis=AX.X, op=ALU.max)
            P.i("dve", "tensor_scalar", reads=[sk], writes=[sk], out=st[:, 5:6], in0=st[:, 4:5], scalar1=-1.0, scalar2=None, op0=ALU.mult)
            P.i("act", "activation", reads=[lgk, sk], writes=[sk], out=st[:, 32:36], in_=lg[:, 0:4], func=AF.Exp, bias=st[:, 5:6], accum_out=st[:, 6:7])
            P.i("dve", "reciprocal", reads=[sk], writes=[sk], out=st[:, 7:8], in_=st[:, 6:7])
            P.i("dve", "tensor_scalar", reads=[lgk, sk], writes=[sk], out=st[:, 8:12], in0=lg[:, 0:4], scalar1=st[:, 4:5], scalar2=None, op0=ALU.is_equal)
            P.i("dve", "tensor_scalar", reads=[sk], writes=[sk], out=st[:, 12:16], in0=st[:, 8:12], scalar1=BIG, scalar2=-BIG, op0=ALU.mult, op1=ALU.add)
            mkd, mkk = mk_r.next()
            P.i("dve", "tensor_tensor", reads=[lgk, sk], writes=[mkk], out=mkd[:], in0=lg[:, 4:36].rearrange("p (g e) -> p g e", g=4),
                in1=st[:, 12:16].unsqueeze(2).to_broadcast([128, 4, 8]), op=ALU.add)
            mflat = mkd[:].rearrange("p g e -> p (g e)")
            P.i("dve", "max", reads=[mkk], writes=[sk], out=st[:, 16:24], in_=mflat)
            oh, ohk = oh_r.next()
            P.i("dve", "tensor_scalar", reads=[mkk, sk], writes=[ohk + "/1"], out=oh[:, 0, :], in0=mflat, scalar1=st[:, 16:17], scalar2=None, op0=ALU.is_equal)
            P.i("dve", "tensor_scalar", reads=[mkk, sk], writes=[ohk + "/2"], out=oh[:, 1, :], in0=mflat, scalar1=st[:, 17:18], scalar2=None, op0=ALU.is_equal)
            P.i("dve", "tensor_tensor", reads=[sk], writes=[sk], out=st[:, 24:25], in0=st[:, 17:18], in1=st[:, 16:17], op=ALU.subtract)
            P.i("act", "activation", reads=[sk], writes=[sk], out=st[:, 25:26], in_=st[:, 24:25], func=AF.Exp)
            P.i("dve", "tensor_scalar", reads=[sk], writes=[sk], out=st[:, 26:27], in0=st[:, 25:26], scalar1=1.0, scalar2=None, op0=ALU.add)
            P.i("dve", "reciprocal", reads=[sk], writes=[sk], out=st[:, 27:28], in_=st[:, 26:27])
            P.i("dve", "tensor_tensor", reads=[sk], writes=[sk], out=st[:, 28:29], in0=st[:, 27:28], in1=st[:, 7:8], op=ALU.mult)
            P.i("dve", "tensor_tensor", reads=[sk], writes=[sk], out=st[:, 29:30], in0=st[:, 7:8], in1=st[:, 28:29], op=ALU.subtract)
            P.i("dve", "tensor_scalar", reads=[ohk + "/1", sk], writes=[ohk + "/3"], out=oh[:, 2, :], in0=oh[:, 0, :], scalar1=st[:, 28:29], scalar2=None, op0=ALU.mult)
            wt, wtk = wt_r.next()
            P.i("dve", "scalar_tensor_tensor", reads=[ohk + "/2", ohk + "/3", sk], writes=[wtk], out=wt[:], in0=oh[:, 1, :], scalar=st[:, 29:30], in1=oh[:, 2, :], op0=ALU.mult, op1=ALU.add)
            pw, pwk = pw_r.next()
            P.i("pe", "transpose", reads=[wtk, pf + "ident"], writes=[pwk], out=pw[0:32, 0:128], in_=wt[:], identity=ident[:])
            wT, wTk = wT_r.next()
            P.i("act", "activation", reads=[pwk], writes=[wTk], out=wT[:], in_=pw[0:32, 0:128], func=AF.Copy)
            P.d(WTd[:, t0:t0 + 128], wT[:], reads=[wTk], writes=[P.u("WTd")])


def phase_f2(P, Dm, l, src_name, tok_chunks=None):
    pf = "F%d_" % l
    xsrc = Dm.get(src_name, [T, D])
    CAT = Dm.get("CAT", [1024, T])
    w_out = Dm.get("w_out%d" % l, [128, 8, 1024], kind="ExternalInput")
    modrow = Dm.get("modrow%d" % l, [2, 6144])
    n2g = Dm.get("norm2_g%d" % l, [1, D], kind="ExternalInput")
    wr_d = Dm.get("wr%d" % l, [128, 8, 36], kind="ExternalInput")
    rb_d = Dm.get("rb%d" % l, [1, 36], kind="ExternalInput")
    ident_d = Dm.get("ident", [128, 128], kind="ExternalInput")
    X1 = Dm.get("X1", [T, D])
    chunks_ = (tok_chunks or CHUNKS)
    ntf = sum(cn for _, cn in chunks_) // 128
    NB = (2 * ntf * 128) // 128 + 32
    BUF = Dm.get("BUF%d" % l, [NB * 128, D])
    IDXW = Dm.get("IDXW%d" % l, [128, NB], I32)
    DEST = Dm.get("DEST%d" % l, [128, ntf * 2], I32)
    WWd = Dm.get("WWd%d" % l, [128, ntf * 2])
    ramp_d = Dm.get("ramp", [1, 72], kind="ExternalInput")
    bst_d = Dm.get("bst", [1, 104], kind="ExternalInput")
    pidx_d = Dm.get("pidx", [128, 1], kind="ExternalInput")
    stri_d = Dm.get("stri", [128, 2, 128], kind="ExternalInput")
    OH = P.sb(pf + "OH", [128, ntf, 2, 32])
    WW = P.sb(pf + "WW", [128, ntf, 2])
    RK = P.sb(pf + "RK", [128, ntf, 32])
    Msum = P.sb(pf + "Msum", [128, 32])
    P.i("dve", "memset", writes=[pf + "Msum"], ap=Msum[:], constant=0.0)
    stri = P.sb(pf + "stri", [128, 2, 128]); P.d(stri[:], stri_d[:, :, :], writes=[pf + "stri"])
    Mt_r = Rot(P, pf + "Mt", [128, 32], n=2)
    zz = P.sb(pf + "zz", [128, 4096])
    P.i("pool", "memset", writes=[pf + "zz"], ap=zz[:], constant=0.0)
    rows_per = 128 * 4
    for r0 in range(0, NB * 128, rows_per):
        P.d(BUF[r0:r0 + rows_per, :].rearrange("(p a) n -> p (a n)", p=128), zz[:], reads=[pf + "zz"], writes=[P.u("BUFz%d" % l)])
    mk = "modrow%d" % l
    ident = P.sb(pf + "ident", [128, 128])
    P.d(ident[:], ident_d[:, :], writes=[pf + "ident"])
    wo = P.sb(pf + "wo", [128, 8, 1024], F32R)
    for k in range(8):
        P.d(wo[:, k, :], w_out[:, k, :], writes=[pf + "wo/%d" % k], q="pool")
    wr = P.sb(pf + "wr", [128, 8, 36])
    P.d(wr[:], wr_d[:, :, :], writes=[pf + "wr"])
    rb = P.sb(pf + "rb", [128, 36])
    load_bcast(P, rb[:], pf + "rb", rb_d[0:1, :])
    G1 = [P.sb(pf + "G1%d" % r, [128, D]) for r in range(2)]
    G2 = [P.sb(pf + "G2%d" % r, [128, D]) for r in range(2)]
    SH2 = [P.sb(pf + "SH2%d" % r, [128, D]) for r in range(2)]
    gn = P.sb(pf + "gn", [128, D])
    load_bcast(P, gn[:], pf + "gn", n2g[0:1, :])
    for r in range(2):
        load_bcast(P, G1[r][:], pf + "G1%d" % r, modrow[r:r + 1, 2048:3072], reads=[mk])
        load_bcast(P, SH2[r][:], pf + "SH2%d" % r, modrow[r:r + 1, 3072:4096], reads=[mk])
        load_bcast(P, G2[r][:], pf + "G2%d" % r, modrow[r:r + 1, 4096:5120], reads=[mk])
        P.i("dve", "scalar_tensor_tensor", reads=[pf + "G2%d" % r, pf + "gn"], writes=[pf + "G2%d" % r], out=G2[r][:], in0=G2[r][:], scalar=1.0, in1=gn[:], op0=ALU.add, op1=ALU.mult)
    epsc = P.sb(pf + "eps", [128, 1])
    P.i("dve", "memset", writes=[pf + "eps"], ap=epsc[:], constant=EPS)
    ct_r = Rot(P, pf + "ct", [128, 8, 512], F32R, n=2)
    po_r = Rot(P, pf + "po", [128, 1024], n=1, psum=True)
    xt_r = Rot(P, pf + "xt", [128, D], n=2)
    x1_r = Rot(P, pf + "x1", [128, D], n=2)
    h_r = Rot(P, pf + "h", [128, D], n=2)
    junk = P.sb(pf + "junk", [128, D])
    st_r = Rot(P, pf + "st", [128, 64], n=3)
    tp_r = Rot(P, pf + "tp", [128, 1024], n=1, psum=True)
    H2 = Dm.get("H2_%d" % l, [T, D])
    hTf_r = Rot(P, pf + "hTf", [128, 8, 128], n=2)
    pr_r = Rot(P, pf + "pr", [128, 512], n=1, psum=True)
    lg_r = Rot(P, pf + "lg", [128, 36], n=2)
    mk_r = Rot(P, pf + "mk", [128, 4, 8], n=2)
    oh_r = Rot(P, pf + "oh", [128, 3, 32], n=2)
    pw_r = Rot(P, pf + "pw", [128, 512], n=1, psum=True)
    tile_i = 0
    for (c0, cn) in chunks_:
        ct, ctk = ct_r.next()
        P.d(ct[:, :, 0:cn], CAT[:, c0:c0 + cn].rearrange("(k p) t -> p k t", p=128), reads=["CAT"], writes=[ctk], q="pool")
        for i in range(cn // 128):
            t0 = c0 + i * 128
            r = 1 if t0 < LC else 0
            po, pok = po_r.next()
            for hf in range(2):
                for k in range(8):
                    P.i("pe", "matmul", reads=[ctk, pf + "wo/%d" % k], writes=[pok], out=po[:, hf * 512:(hf + 1) * 512], lhsT=ct[:, k, i * 128:(i + 1) * 128],
                        rhs=wo[:, k, hf * 512:(hf + 1) * 512], start=(k == 0), stop=(k == 7))
            xt, xk = xt_r.next()
            P.d(xt[:], xsrc[t0:t0 + 128, :], reads=[src_name], writes=[xk])
            x1, x1k = x1_r.next()
            for hf in range(2):
                sl = slice(hf * 512, (hf + 1) * 512)
                P.i("dve", "tensor_tensor", reads=[pok, pf + "G1%d" % r], writes=[x1k + "/%d" % hf], out=x1[:, sl], in0=po[:, sl], in1=G1[r][:, sl], op=ALU.mult)
            P.i("pool", "tensor_tensor", reads=[x1k, xk], writes=[x1k], out=x1[:], in0=x1[:], in1=xt[:], op=ALU.add)
            P.d(X1[t0:t0 + 128, :], x1[:], reads=[x1k], writes=[P.u("X1")])
            st, sk = st_r.next()
            P.i("act", "activation", reads=[x1k], writes=[pf + "junk", sk], out=junk[:], in_=x1[:], func=AF.Square, accum_out=st[:, 0:1])
            P.i("act", "activation", reads=[sk, pf + "eps"], writes=[sk], out=st[:, 1:2], in_=st[:, 0:1], func=AF.Sqrt, bias=epsc[:], scale=1.0 / D)
            P.i("dve", "reciprocal", reads=[sk], writes=[sk], out=st[:, 2:3], in_=st[:, 1:2])
            h, hk = h_r.next()
            P.i("dve", "scalar_tensor_tensor", reads=[x1k, sk, pf + "G2%d" % r], writes=[hk], out=h[:], in0=x1[:], scalar=st[:, 2:3], in1=G2[r][:], op0=ALU.mult, op1=ALU.mult)
            P.i("pool", "tensor_tensor", reads=[hk, pf + "SH2%d" % r], writes=[hk], out=h[:], in0=h[:], in1=SH2[r][:], op=ALU.add)
            P.d(H2[t0:t0 + 128, :], h[:], reads=[hk], writes=[P.u("H2_%d" % l)])
            tp, tpk = tp_r.next()
            for k in range(8):
                P.i("pe", "transpose", reads=[hk, pf + "ident"], writes=[tpk], noinc=(k != 7), out=tp[:, k * 128:(k + 1) * 128], in_=h[:, k * 128:(k + 1) * 128], identity=ident[:])
            hTf, hTfk = hTf_r.next()
            P.i("dve", "tensor_copy", reads=[tpk], writes=[hTfk], out=hTf[:], in_=tp[:].rearrange("p (k t) -> p k t", k=8))
            pr, prk = pr_r.next()
            for k in range(8):
                P.i("pe", "matmul", reads=[hTfk, pf + "wr"], writes=[prk], out=pr[:, 0:36], lhsT=hTf[:, k, :], rhs=wr[:, k, :], start=(k == 0), stop=(k == 7))
            lg, lgk = lg_r.next()
            P.i("dve", "tensor_tensor", reads=[prk, pf + "rb"], writes=[lgk], out=lg[:], in0=pr[:, 0:36], in1=rb[:], op=ALU.add)
            P.i("dve", "tensor_reduce", reads=[lgk], writes=[sk], out=st[:, 4:5], in_=lg[:, 0:4], axis=AX.X, op=ALU.max)
            P.i("dve", "tensor_scalar", reads=[sk], writes=[sk], out=st[:, 5:6], in0=st[:, 4:5], scalar1=-1.0, scalar2=None, op0=ALU.mult)
            P.i("act", "activation", reads=[lgk, sk], writes=[sk], out=st[:, 32:36], in_=lg[:, 0:4], func=AF.Exp, bias=st[:, 5:6], accum_out=st[:, 6:7])
            P.i("dve", "reciprocal", reads=[sk], writes=[sk], out=st[:, 7:8], in_=st[:, 6:7])
            P.i("dve", "tensor_scalar", reads=[lgk, sk], writes=[sk], out=st[:, 8:12], in0=lg[:, 0:4], scalar1=st[:, 4:5], scalar2=None, op0=ALU.is_equal)
            P.i("dve", "tensor_scalar", reads=[sk], writes=[sk], out=st[:, 12:16], in0=st[:, 8:12], scalar1=BIG, scalar2=-BIG, op0=ALU.mult, op1=ALU.add)
            mkd, mkk = mk_r.next()
            P.i("dve", "tensor_tensor", reads=[lgk, sk], writes=[mkk], out=mkd[:], in0=lg[:, 4:36].rearrange("p (g e) -> p g e", g=4),
                in1=st[:, 12:16].unsqueeze(2).to_broadcast([128, 4, 8]), op=ALU.add)
            mflat = mkd[:].rearrange("p g e -> p (g e)")
            P.i("dve", "max", reads=[mkk], writes=[sk], out=st[:, 16:24], in_=mflat)
            oh, ohk = oh_r.next()
            P.i("dve", "tensor_scalar", reads=[mkk, sk], writes=[ohk + "/1"], out=oh[:, 0, :], in0=mflat, scalar1=st[:, 16:17], scalar2=None, op0=ALU.is_equal)
            P.i("dve", "tensor_scalar", reads=[mkk, sk], writes=[ohk + "/2"], out=oh[:, 1, :], in0=mflat, scalar1=st[:, 17:18], scalar2=None, op0=ALU.is_equal)
            P.i("dve", "tensor_tensor", reads=[sk], writes=[sk], out=st[:, 24:25], in0=st[:, 17:18], in1=st[:, 16:17], op=ALU.subtract)
            P.i("act", "activation", reads=[sk], writes=[sk], out=st[:, 25:26], in_=st[:, 24:25], func=AF.Exp)
            P.i("dve", "tensor_scalar", reads=[sk], writes=[sk], out=st[:, 26:27], in0=st[:, 25:26], scalar1=1.0, scalar2=None, op0=ALU.add)
            P.i("dve", "reciprocal", reads=[sk], writes=[sk], out=st[:, 27:28], in_=st[:, 26:27])
            P.i("dve", "tensor_tensor", reads=[sk], writes=[sk], out=st[:, 28:29], in0=st[:, 27:28], in1=st[:, 7:8], op=ALU.mult)
            P.i("dve", "tensor_tensor", reads=[sk], writes=[sk], out=st[:, 29:30], in0=st[:, 7:8], in1=st[:, 28:29], op=ALU.subtract)
            ti = tile_i
            tile_i += 1
            P.i("act", "activation", reads=[ohk + "/1"], writes=[pf + "OH/%d_0" % ti], out=OH[:, ti, 0, :], in_=oh[:, 0, :], func=AF.Copy)
            P.i("act", "activation", reads=[ohk + "/2"], writes=[pf + "OH/%d_1" % ti], out=OH[:, ti, 1, :], in_=oh[:, 1, :], func=AF.Copy)
            P.i("act", "activation", reads=[sk], writes=[pf + "WW/%d" % ti], out=WW[:, ti, :], in_=st[:, 28:30], func=AF.Copy)
            Mt, Mtk = Mt_r.next()
            P.i("dve", "tensor_tensor", reads=[ohk + "/1", ohk + "/2"], writes=[Mtk], out=Mt[:], in0=oh[:, 0, :], in1=oh[:, 1, :], op=ALU.add)
            pw, pwk = pw_r.next()
            P.i("pe", "matmul", reads=[Mtk, pf + "stri"], writes=[pwk], out=pw[:, 0:32], lhsT=stri[:, 0, :], rhs=Mt[:], start=True, stop=False)
            P.i("pe", "matmul", reads=[pf + "Msum", pf + "stri"], writes=[pwk], out=pw[:, 0:32], lhsT=stri[:, 1, :], rhs=Msum[:], start=False, stop=True)
            P.i("act", "activation", reads=[pwk], writes=[pf + "RK/%d" % ti], out=RK[:, ti, :], in_=pw[:, 0:32], func=AF.Copy)
            P.i("dve", "tensor_tensor", reads=[Mtk, pf + "Msum"], writes=[pf + "Msum"], out=Msum[:], in0=Msum[:], in1=Mt[:], op=ALU.add)
    ramp = P.sb(pf + "ramp", [128, 72]); load_bcast(P, ramp[:], pf + "ramp", ramp_d[0:1, :])
    bst = P.sb(pf + "bst", [128, 104]); load_bcast(P, bst[:], pf + "bst", bst_d[0:1, :])
    pidx = P.sb(pf + "pidx", [128, 1]); P.d(pidx[:], pidx_d[:, :], writes=[pf + "pidx"])
    q = P.sb(pf + "q", [128, 8, 32])
    QK = pf + "q"
    big = P.sb(pf + "big", [128, 104 * 32])
    ones32 = P.sb(pf + "ones32", [128, 32]); P.i("dve", "memset", writes=[pf + "ones32"], ap=ones32[:], constant=1.0)
    pw, pwk = pw_r.next()
    P.i("pe", "matmul", reads=[pf + "Msum", pf + "stri"], writes=[pwk], out=pw[:, 0:32], lhsT=stri[:, 1, :], rhs=Msum[:], start=True, stop=True)
    P.i("dve", "tensor_copy", reads=[pwk], writes=[QK], out=q[:, 0, :], in_=pw[:, 0:32])
    NM = 2 * ntf
    b3 = big[:, 0:32 * NM].rearrange("p (e m) -> p e m", e=32)
    P.i("dve", "tensor_tensor", reads=[QK, pf + "ramp"], writes=[pf + "big"], out=b3, in0=q[:, 0, :].unsqueeze(2).to_broadcast([128, 32, NM]),
        in1=ramp[:, 0:NM].unsqueeze(1).to_broadcast([128, 32, NM]), op=ALU.is_ge)
    P.i("dve", "tensor_reduce", reads=[pf + "big"], writes=[QK], out=q[:, 1, :], in_=b3, axis=AX.X, op=ALU.add)
    P.i("dve", "tensor_scalar", reads=[QK], writes=[QK], out=q[:, 1, :], in0=q[:, 1, :], scalar1=128.0, scalar2=None, op0=ALU.mult)
    P.i("dve", "tensor_tensor_scan", reads=[QK, pf + "ones32"], writes=[QK], out=q[:, 2, :], data0=ones32[:], data1=q[:, 1, :], initial=0.0, op0=ALU.mult, op1=ALU.add)
    P.i("dve", "tensor_tensor", reads=[QK], writes=[QK], out=q[:, 3, :], in0=q[:, 2, :], in1=q[:, 1, :], op=ALU.subtract)
    be = P.sb(pf + "be", [128, 104])
    b3 = big[:, 0:NB * 32].rearrange("p (b e) -> p b e", e=32)
    P.i("dve", "tensor_tensor", reads=[QK, pf + "bst"], writes=[pf + "big"], out=b3, in0=q[:, 2, :].unsqueeze(1).to_broadcast([128, NB, 32]),
        in1=bst[:, 0:NB].unsqueeze(2).to_broadcast([128, NB, 32]), op=ALU.is_le)
    P.i("dve", "tensor_reduce", reads=[pf + "big"], writes=[pf + "be"], out=be[:, 0:NB], in_=b3, axis=AX.X, op=ALU.add)
    P.i("dve", "tensor_scalar", reads=[pf + "be"], writes=[pf + "be"], out=be[:, 0:NB], in0=be[:, 0:NB], scalar1=31.0, scalar2=None, op0=ALU.min)
    same = P.sb(pf + "same", [128, 104])
    P.i("dve", "memset", writes=[pf + "same"], ap=same[:, 0:1], constant=0.0)
    P.i("dve", "tensor_tensor", reads=[pf + "be"], writes=[pf + "same"], out=same[:, 1:NB], in0=be[:, 1:NB], in1=be[:, 0:NB - 1], op=ALU.is_equal)
    P.i("dve", "memset", writes=[pf + "same"], ap=same[:, NB // 2:NB // 2 + 1], constant=0.0)
    P.i("dve", "tensor_scalar", reads=[pf + "be", pf + "pidx"], writes=[pf + "be"], out=be[:, 0:NB], in0=be[:, 0:NB], scalar1=128.0, scalar2=pidx[:, 0:1], op0=ALU.mult, op1=ALU.add)
    P.i("dve", "scalar_tensor_tensor", reads=[pf + "be", pf + "same"], writes=[pf + "be"], out=be[:, 0:NB], in0=same[:, 0:NB], scalar=1048576.0, in1=be[:, 0:NB], op0=ALU.mult, op1=ALU.add)
    bei = P.sb(pf + "bei", [128, 104], I32)
    P.i("dve", "tensor_copy", reads=[pf + "be"], writes=[pf + "bei"], out=bei[:, 0:NB], in_=be[:, 0:NB])
    P.d(IDXW[:, :], bei[:, 0:NB], reads=[pf + "bei"], writes=[P.u("IDXW%d" % l)])
    dsf = P.sb(pf + "dsf", [128, ntf, 2])
    pos_r = Rot(P, pf + "pos", [128, 2, 32], n=2)
    for ti in range(ntf):
        pos, posk = pos_r.next()
        P.i("dve", "tensor_tensor", reads=[pf + "RK/%d" % ti, QK], writes=[posk + "/p"], out=pos[:, 0, :], in0=RK[:, ti, :], in1=q[:, 3, :], op=ALU.add)
        for k in range(2):
            P.i("dve", "tensor_tensor", reads=[posk + "/p", pf + "OH/%d_%d" % (ti, k)], writes=[posk + "/t"], out=pos[:, 1, :], in0=pos[:, 0, :], in1=OH[:, ti, k, :], op=ALU.mult)
            P.i("dve", "tensor_reduce", reads=[posk + "/t"], writes=[pf + "dsf/%d_%d" % (ti, k)], out=dsf[:, ti, k:k + 1], in_=pos[:, 1, :], axis=AX.X, op=ALU.add)
    dsi = P.sb(pf + "dsi", [128, ntf * 2], I32)
    P.i("dve", "tensor_copy", reads=[pf + "dsf"], writes=[pf + "dsi"], out=dsi[:], in_=dsf[:].rearrange("p t k -> p (t k)"))
    P.d(DEST[:, :], dsi[:], reads=[pf + "dsi"], writes=[P.u("DEST%d" % l)])
    P.d(WWd[:, :], WW[:].rearrange("p t k -> p (t k)"), reads=[pf + "WW"], writes=[P.u("WWd%d" % l)])
    h2_r = Rot(P, pf + "h2s", [128, D], n=3)
    ti = 0
    for (c0, cn) in chunks_:
        for i in range(cn // 128):
            t0 = c0 + i * 128
            h2, h2k = h2_r.next()
            P.d(h2[:], H2[t0:t0 + 128, :], reads=["H2_%d" % l], writes=[h2k])
            for k in range(2):
                P.dma(lambda e, h2=h2, col=ti * 2 + k: e.indirect_dma_start(out=BUF[:, :], out_offset=bass.IndirectOffsetOnAxis(ap=dsi[:, col:col + 1], axis=0), in_=h2[:], in_offset=None),
                      reads=[h2k, pf + "dsi", "BUFz%d" % l], writes=[P.u("BUF%d" % l)], q="pool")
            ti += 1


def phase_g(P, Dm, l, tok_chunks, final):
    pf = "g%d_" % l
    X1 = Dm.get("X1", [T, D])
    H2T = Dm.get("H2T", [D, T])
    WTd = Dm.get("WTd", [32, T])
    WG = Dm.get("moe_g%d" % l, [32, 128, 8, 512], kind="ExternalInput")
    WU = Dm.get("moe_u%d" % l, [32, 128, 8, 512], kind="ExternalInput")
    WD = Dm.get("moe_d%d" % l, [32, 128, 4, 1024], kind="ExternalInput")
    modrow = Dm.get("modrow%d" % l, [2, 6144])
    mk = "modrow%d" % l
    if final:
        fg_d = Dm.get("final_g", [1, D], kind="ExternalInput")
        OUT = Dm.get("out", [L, D], kind="ExternalOutput")
        fg = P.sb(pf + "fg", [128, D])
        load_bcast(P, fg[:], pf + "fg", fg_d[0:1, :])
        epsc = P.sb(pf + "eps", [128, 1])
        P.i("dve", "memset", writes=[pf + "eps"], ap=epsc[:], constant=EPS)
        junk = P.sb(pf + "junk", [128, D])
    else:
        XN = Dm.get("XN%d" % l, [T, D])
    G2g = [P.sb(pf + "g2%d" % r, [128, D]) for r in range(2)]
    for r in range(2):
        load_bcast(P, G2g[r][:], pf + "g2%d" % r, modrow[r:r + 1, 5120:6144], reads=[mk])
    hT_r = Rot(P, pf + "hT", [128, 8, 512], F32R, n=2)
    acc_r = Rot(P, pf + "acc", [128, 4, 1024], n=1)
    wg_r = Rot(P, pf + "wg", [128, 8, 512], F32R, n=2)
    wu_r = Rot(P, pf + "wu", [128, 8, 512], F32R, n=2)
    wd_r = Rot(P, pf + "wd", [128, 4, 1024], F32R, n=2)
    wb_r = Rot(P, pf + "wb", [128, 512], n=2)
    pg_r = Rot(P, pf + "pg", [128, 512], n=2, psum=True)
    pu_r = Rot(P, pf + "pu", [128, 512], n=2, psum=True)
    pd_r = Rot(P, pf + "pd", [128, 512], n=2, psum=True)
    sg_r = Rot(P, pf + "sg", [128, 512], n=2)
    hu_r = Rot(P, pf + "hu", [128, 512], n=2)
    hid_r = Rot(P, pf + "hid", [128, 4, 512], F32R, n=2)
    xt_r = Rot(P, pf + "xt", [128, D], n=2)
    st_r = Rot(P, pf + "st", [128, 8], n=2)
    for (c0, cn) in tok_chunks:
        nt = cn // 128
        hT, hTk = hT_r.next()
        P.d(hT[:, :, 0:cn], H2T[:, c0:c0 + cn].rearrange("(k p) t -> p k t", p=128), reads=["H2T"], writes=[hTk], q="pool")
        acc, acck = acc_r.next()
        for e in range(32):
            wg, wgk = wg_r.next()
            wu, wuk = wu_r.next()
            wd, wdk = wd_r.next()
            P.d(wg[:], WG[e], writes=[wgk], q="pool")
            P.d(wu[:], WU[e], writes=[wuk], q="pool")
            P.d(wd[:], WD[e], writes=[wdk], q="pool")
            wb, wbk = wb_r.next()
            P.d(wb[:, 0:cn], WTd[e:e + 1, c0:c0 + cn].to_broadcast([128, cn]), reads=["WTd"], writes=[wbk])
            hid, hidk = hid_r.next()
            for hc in range(4):
                pg, pgk = pg_r.next()
                pu, puk = pu_r.next()
                for k in range(8):
                    P.i("pe", "matmul", reads=[wgk, hTk], writes=[pgk], out=pg[:, 0:cn], lhsT=wg[:, k, hc * 128:(hc + 1) * 128], rhs=hT[:, k, 0:cn], start=(k == 0), stop=(k == 7))
                for k in range(8):
                    P.i("pe", "matmul", reads=[wuk, hTk], writes=[puk], out=pu[:, 0:cn], lhsT=wu[:, k, hc * 128:(hc + 1) * 128], rhs=hT[:, k, 0:cn], start=(k == 0), stop=(k == 7))
                sg, sgk = sg_r.next()
                P.i("act", "activation", reads=[pgk], writes=[sgk], out=sg[:, 0:cn], in_=pg[:, 0:cn], func=AF.Silu)
                hu, huk = hu_r.next()
                P.i("dve", "tensor_tensor", reads=[sgk, puk], writes=[huk], out=hu[:, 0:cn], in0=sg[:, 0:cn], in1=pu[:, 0:cn], op=ALU.mult)
                P.i("pool", "tensor_tensor", reads=[huk, wbk], writes=[hidk + "/%d" % hc], out=hid[:, hc, 0:cn], in0=hu[:, 0:cn], in1=wb[:, 0:cn], op=ALU.mult)
            hkeys = [hidk + "/%d" % hc for hc in range(4)]
            for tt in range(nt):
                for hf in range(2):
                    pd, pdk = pd_r.next()
                    for hc in range(4):
                        P.i("pe", "matmul", reads=hkeys + [wdk], writes=[pdk], out=pd[:], lhsT=hid[:, hc, tt * 128:(tt + 1) * 128], rhs=wd[:, hc, hf * 512:(hf + 1) * 512],
                            start=(hc == 0), stop=(hc == 3))
                    ak = acck + "/%d_%d" % (tt, hf)
                    if e == 0:
                        P.i("dve", "tensor_copy", reads=[pdk], writes=[ak], out=acc[:, tt, hf * 512:(hf + 1) * 512], in_=pd[:])
                    else:
                        P.i("dve", "tensor_tensor", reads=[pdk, ak], writes=[ak], out=acc[:, tt, hf * 512:(hf + 1) * 512], in0=acc[:, tt, hf * 512:(hf + 1) * 512], in1=pd[:], op=ALU.add)
        for tt in range(nt):
            t0 = c0 + tt * 128
            r = 1 if t0 < LC else 0
            aks = [acck + "/%d_%d" % (tt, hf) for hf in range(2)]
            xt, xk = xt_r.next()
            P.d(xt[:], X1[t0:t0 + 128, :], reads=["X1"], writes=[xk])
            P.i("pool", "tensor_tensor", reads=aks + [pf + "g2%d" % r], writes=aks, out=acc[:, tt, :], in0=acc[:, tt, :], in1=G2g[r][:], op=ALU.mult)
            P.i("dve", "tensor_tensor", reads=aks + [xk], writes=[xk], out=xt[:], in0=xt[:], in1=acc[:, tt, :], op=ALU.add)
            if not final:
                P.d(XN[t0:t0 + 128, :], xt[:], reads=[xk], writes=[P.u("XN%d" % l)])
            else:
                st, sk = st_r.next()
                P.i("act", "activation", reads=[xk], writes=[pf + "junk", sk], out=junk[:], in_=xt[:], func=AF.Square, accum_out=st[:, 0:1])
                P.i("act", "activation", reads=[sk, pf + "eps"], writes=[sk], out=st[:, 1:2], in_=st[:, 0:1], func=AF.Sqrt, bias=epsc[:], scale=1.0 / D)
                P.i("dve", "reciprocal", reads=[sk], writes=[sk], out=st[:, 2:3], in_=st[:, 1:2])
                P.i("dve", "scalar_tensor_tensor", reads=[xk, sk, pf + "fg"], writes=[xk], out=xt[:], in0=xt[:], scalar=st[:, 2:3], in1=fg[:], op0=ALU.mult, op1=ALU.mult)
                P.d(OUT[t0 - LC:t0 - LC + 128, :], xt[:], reads=[xk], writes=[P.u("out")], final=True)


def phase_b(P, Dm, l):
    pf = "b%d_" % l
    FMT = Dm.get("FMT", [1024, T])
    CAT = Dm.get("CAT", [1024, T])
    Bd = Dm.get("s5B%d" % l, [2, 2, 8, 128, 128], kind="ExternalInput")
    Cd = Dm.get("s5C%d" % l, [2, 2, 8, 128, 128], kind="ExternalInput")
    lam_d = Dm.get("s5lam%d" % l, [128, 3, 16], kind="ExternalInput")
    dsk_d = Dm.get("s5d%d" % l, [128, 2], kind="ExternalInput")
    gw_d = Dm.get("gluw%d" % l, [128, 2, 256], kind="ExternalInput")
    gb_d = Dm.get("glub%d" % l, [128, 2], kind="ExternalInput")
    NMAX = 512
    lam = P.sb(pf + "lam", [128, 3, 16])
    P.d(lam[:], lam_d[:, :, :], writes=[pf + "lam"])
    dsk = P.sb(pf + "dsk", [128, 2]); P.d(dsk[:], dsk_d[:, :], writes=[pf + "dsk"])
    gb = P.sb(pf + "gb", [128, 2]); P.d(gb[:], gb_d[:, :], writes=[pf + "gb"])
    gw = P.sb(pf + "gw", [128, 2, 256], F32R); P.d(gw[:], gw_d[:, :, :], writes=[pf + "gw"], q="pool")
    Bb = P.sb(pf + "Bb", [128, 16, 128], F32R)
    Cb = P.sb(pf + "Cb", [128, 16, 128], F32R)
    sc = P.sb(pf + "sc", [128, 24, 16])
    K_ = pf + "sc"
    LR, LI, LDT = lam[:, 0, :], lam[:, 1, :], lam[:, 2, :]
    (DT, RM, TH, C_, S_, T1, T2, T3, ARE, AIM, DEN, AM1, FRE, FIM, HP) = range(15)

    def S(i):
        return sc[:, i, :]

    def tt(o, a, b, op, eng="dve"):
        P.i(eng, "tensor_tensor", reads=[K_, pf + "lam"], writes=[K_], out=o, in0=a, in1=b, op=op)

    hpi = P.sb(pf + "hpi", [128, 1])
    P.i("dve", "memset", writes=[pf + "hpi"], ap=hpi[:], constant=float(np.pi / 2))
    P.i("act", "activation", reads=[pf + "lam"], writes=[K_], out=S(DT), in_=LDT, func=AF.Exp)
    tt(S(RM), LR, S(DT), ALU.mult)
    P.i("act", "activation", reads=[K_], writes=[K_], out=S(RM), in_=S(RM), func=AF.Exp)
    tt(S(TH), LI, S(DT), ALU.mult)
    P.i("act", "activation", reads=[K_], writes=[K_], out=S(S_), in_=S(TH), func=AF.Sin, scale=1.0 / 32)
    P.i("act", "activation", reads=[K_, pf + "hpi"], writes=[K_], out=S(C_), in_=S(TH), func=AF.Sin, scale=1.0 / 32, bias=hpi[:])
    for _ in range(5):
        tt(S(T1), S(C_), S(C_), ALU.mult)
        tt(S(T2), S(S_), S(S_), ALU.mult)
        tt(S(T3), S(C_), S(S_), ALU.mult)
        tt(S(C_), S(T1), S(T2), ALU.subtract)
        tt(S(S_), S(T3), S(T3), ALU.add)
    tt(S(ARE), S(RM), S(C_), ALU.mult)
    tt(S(AIM), S(RM), S(S_), ALU.mult)
    tt(S(T1), LR, LR, ALU.mult)
    tt(S(T2), LI, LI, ALU.mult)
    tt(S(DEN), S(T1), S(T2), ALU.add)
    P.i("dve", "reciprocal", reads=[K_], writes=[K_], out=S(DEN), in_=S(DEN))
    P.i("dve", "tensor_scalar", reads=[K_], writes=[K_], out=S(AM1), in0=S(ARE), scalar1=-1.0, scalar2=None, op0=ALU.add)
    tt(S(T1), S(AM1), LR, ALU.mult)
    tt(S(T2), S(AIM), LI, ALU.mult)
    tt(S(T1), S(T1), S(T2), ALU.add)
    tt(S(FRE), S(T1), S(DEN), ALU.mult)
    tt(S(T1), S(AIM), LR, ALU.mult)
    tt(S(T2), S(AM1), LI, ALU.mult)
    tt(S(T1), S(T1), S(T2), ALU.subtract)
    tt(S(FIM), S(T1), S(DEN), ALU.mult)
    Ep = P.sb(pf + "Ep", [128, 2, 8, NMAX])
    Tm = P.sb(pf + "Tm", [128, 2, 8, NMAX])
    pw = P.sb(pf + "pw", [128, 4, 8])
    tb = P.sb(pf + "tb", [128, 2, 8, NMAX // 2])
    carry = P.sb(pf + "carry", [128, 16, 2])
    P.i("dve", "memset", writes=[pf + "carry"], ap=carry[:], constant=0.0)
    yacc = P.sb(pf + "yacc", [128, 2, T])
    uc_r = Rot(P, pf + "uc", [128, 2, NMAX], F32R, n=2)
    pb_r = Rot(P, pf + "pb", [128, 512], n=4, psum=True)
    py_r = Rot(P, pf + "py", [128, 512], n=2, psum=True)
    br_r = Rot(P, pf + "br", [128, 2, NMAX], n=2)
    t_r = Rot(P, pf + "t", [128, 4, NMAX], n=2)
    v_r = Rot(P, pf + "v", [128, 2, NMAX], n=2)
    g_r = Rot(P, pf + "g", [128, 2, NMAX], n=2)
    h_r = Rot(P, pf + "h", [128, 2, NMAX], F32R, n=2)
    TK, EK = pf + "Tm", pf + "Ep"
    for d in range(2):
        dsl = slice(d * 8, d * 8 + 8)
        P.d(Bb[:], Bd[d].rearrange("c j k m -> k (c j) m"), writes=[pf + "Bb"], q="pool")
        P.d(Cb[:], Cd[d].rearrange("c j k m -> k (c j) m"), writes=[pf + "Cb"], q="pool")
        P.i("act", "activation", reads=[pf + "Cb"], writes=[pf + "Cb"], out=Cb[:, 8:16, :], in_=Cb[:, 8:16, :].bitcast(F32), func=AF.Copy, scale=-1.0)
        P.i("dve", "tensor_copy", reads=[K_], writes=[EK], out=Ep[:, 0, :, 0:1], in_=S(C_)[:, dsl].unsqueeze(2))
        P.i("dve", "tensor_copy", reads=[K_], writes=[EK], out=Ep[:, 1, :, 0:1], in_=S(S_)[:, dsl].unsqueeze(2))
        P.i("dve", "tensor_copy", reads=[K_], writes=[pf + "pw"], out=pw[:, 0, :], in_=S(C_)[:, dsl])
        P.i("dve", "tensor_copy", reads=[K_], writes=[pf + "pw"], out=pw[:, 1, :], in_=S(S_)[:, dsl])
        n = 1
        while n < NMAX:
            cn_b = pw[:, 0, :].unsqueeze(2).to_broadcast([128, 8, n])
            sn_b = pw[:, 1, :].unsqueeze(2).to_broadcast([128, 8, n])
            ire, iim = Ep[:, 0, :, 0:n], Ep[:, 1, :, 0:n]
            ore, oim = Ep[:, 0, :, n:2 * n], Ep[:, 1, :, n:2 * n]
            P.i("dve", "tensor_tensor", reads=[EK, pf + "pw"], writes=[pf + "tb"], out=tb[:, 0, :, 0:n], in0=iim, in1=sn_b, op=ALU.mult)
            P.i("pool", "tensor_tensor", reads=[EK, pf + "pw"], writes=[pf + "tb2"], out=tb[:, 1, :, 0:n], in0=iim, in1=cn_b, op=ALU.mult)
            P.i("dve", "tensor_tensor", reads=[EK, pf + "pw"], writes=[EK + "/a"], out=ore, in0=ire, in1=cn_b, op=ALU.mult)
            P.i("pool", "tensor_tensor", reads=[EK, pf + "pw"], writes=[EK + "/b"], out=oim, in0=ire, in1=sn_b, op=ALU.mult)
            P.i("dve", "tensor_tensor", reads=[EK + "/a", pf + "tb"], writes=[EK + "/a"], out=ore, in0=ore, in1=tb[:, 0, :, 0:n], op=ALU.subtract)
            P.i("pool", "tensor_tensor", reads=[EK + "/b", pf + "tb2"], writes=[EK + "/b"], out=oim, in0=oim, in1=tb[:, 1, :, 0:n], op=ALU.add)
            P.i("dve", "tensor_tensor", reads=[pf + "pw"], writes=[pf + "pw"], out=pw[:, 2, :], in0=pw[:, 0, :], in1=pw[:, 1, :], op=ALU.mult)
            P.i("dve", "tensor_tensor", reads=[pf + "pw"], writes=[pf + "pw"], out=pw[:, 0, :], in0=pw[:, 0, :], in1=pw[:, 0, :], op=ALU.mult)
            P.i("dve", "tensor_tensor", reads=[pf + "pw"], writes=[pf + "pw"], out=pw[:, 3, :], in0=pw[:, 1, :], in1=pw[:, 1, :], op=ALU.mult)
            P.i("dve", "tensor_tensor", reads=[pf + "pw"], writes=[pf + "pw"], out=pw[:, 0, :], in0=pw[:, 0, :], in1=pw[:, 3, :], op=ALU.subtract)
            P.i("dve", "tensor_tensor", reads=[pf + "pw"], writes=[pf + "pw"], out=pw[:, 1, :], in0=pw[:, 2, :], in1=pw[:, 2, :], op=ALU.add)
            n *= 2
        for j in range(8):
            fr = S(FRE)[:, d * 8 + j:d * 8 + j + 1]
            fi = S(FIM)[:, d * 8 + j:d * 8 + j + 1]
            t, tk = t_r.next()
            P.i("dve", "tensor_scalar", reads=[EK, K_], writes=[tk + "/0"], out=t[:, 0, :], in0=Ep[:, 1, j, :], scalar1=fi, scalar2=None, op0=ALU.mult)
            P.i("dve", "scalar_tensor_tensor", reads=[EK, K_, tk + "/0"], writes=[TK + "/a%d" % j], out=Tm[:, 0, j, :], in0=Ep[:, 0, j, :], scalar=fr, in1=t[:, 0, :], op0=ALU.mult, op1=ALU.add)
            P.i("dve", "tensor_scalar", reads=[EK, K_], writes=[tk + "/1"], out=t[:, 1, :], in0=Ep[:, 1, j, :], scalar1=fr, scalar2=None, op0=ALU.mult)
            P.i("dve", "scalar_tensor_tensor", reads=[EK, K_, tk + "/1"], writes=[TK + "/b%d" % j], out=Tm[:, 1, j, :], in0=Ep[:, 0, j, :], scalar=fi, in1=t[:, 1, :], op0=ALU.mult, op1=ALU.subtract)
        order = CHUNKS if d == 0 else [CHUNKS[0]] + CHUNKS[:0:-1]
        rev = (d == 1)

        def R(ap):
            return ap[:, ::-1] if rev else ap

        for (c0, N) in order:
            uc, uck = uc_r.next()
            P.d(uc[:, :, 0:N], FMT[0:256, c0:c0 + N].rearrange("(oc p) t -> p oc t", p=128), reads=["FMT"], writes=[uck], q="pool")
            pys = [py_r.next() for _ in range(2)]
            def s5_iter(j):
                    oc = j // 4
                    dj = d * 8 + j
                    pbr, pbrk = pb_r.next()
                    pbi, pbik = pb_r.next()
                    P.i("pe", "matmul", reads=[pf + "Bb", uck], writes=[pbrk], out=pbr[:, 0:N], lhsT=Bb[:, j, :], rhs=uc[:, oc, 0:N], start=True, stop=True)
                    P.i("pe", "matmul", reads=[pf + "Bb", uck], writes=[pbik], out=pbi[:, 0:N], lhsT=Bb[:, 8 + j, :], rhs=uc[:, oc, 0:N], start=True, stop=True)
                    yield
                    br, brk = br_r.next()
                    P.i("act", "activation", reads=[pbrk], writes=[brk + "/0"], out=br[:, 0, 0:N], in_=R(pbr[:, 0:N]), func=AF.Copy)
                    P.i("act", "activation", reads=[pbik], writes=[brk + "/1"], out=br[:, 1, 0:N], in_=R(pbi[:, 0:N]), func=AF.Copy)
                    yield
                    t, tk = t_r.next()
                    v, vk = v_r.next()
                    b0, b1 = br[:, 0, 0:N], br[:, 1, 0:N]
                    tmr, tmi = Tm[:, 0, j, 0:N], Tm[:, 1, j, 0:N]
                    P.i("dve", "tensor_tensor", reads=[brk + "/0", TK], writes=[tk + "/0"], out=t[:, 0, 0:N], in0=tmr, in1=b0, op=ALU.mult)
                    P.i(S5_ENG2, "tensor_tensor", reads=[brk + "/1", TK], writes=[tk + "/1"], out=t[:, 1, 0:N], in0=tmi, in1=b1, op=ALU.mult)
                    P.i(S5_ENG2, "tensor_tensor", reads=[brk + "/1", TK], writes=[tk + "/2"], out=t[:, 2, 0:N], in0=tmr, in1=b1, op=ALU.mult)
                    P.i(S5_ENG2, "tensor_tensor", reads=[brk + "/0", TK], writes=[tk + "/3"], out=t[:, 3, 0:N], in0=tmi, in1=b0, op=ALU.mult)
                    P.i("dve", "tensor_tensor", reads=[tk + "/0", tk + "/1"], writes=[vk + "/0"], out=v[:, 0, 0:N], in0=t[:, 0, 0:N], in1=t[:, 1, 0:N], op=ALU.subtract)
                    P.i("dve", "tensor_tensor", reads=[tk + "/2", tk + "/3"], writes=[vk + "/1"], out=v[:, 1, 0:N], in0=t[:, 2, 0:N], in1=t[:, 3, 0:N], op=ALU.add)
                    yield
                    g, gk = g_r.next()
                    for c in range(2):
                        P.i("dve", "tensor_tensor_scan", reads=[vk + "/%d" % c, K_, pf + "carry"], writes=[gk + "/%d" % c], out=g[:, c, 0:N], data0=S(RM)[:, dj:dj + 1].to_broadcast([128, N]), data1=v[:, c, 0:N],
                            initial=carry[:, dj, c:c + 1], op0=ALU.mult, op1=ALU.add)
                    yield
                    h, hk = h_r.next()
                    t, tk = t_r.next()
                    epr, epi = Ep[:, 0, j, 0:N], Ep[:, 1, j, 0:N]
                    g0, g1 = g[:, 0, 0:N], g[:, 1, 0:N]
                    P.i("dve", "tensor_tensor", reads=[gk + "/0", EK], writes=[tk + "/0"], out=t[:, 0, 0:N], in0=epr, in1=g0, op=ALU.mult)
                    P.i(S5_ENG2, "tensor_tensor", reads=[gk + "/1", EK], writes=[tk + "/1"], out=t[:, 1, 0:N], in0=epi, in1=g1, op=ALU.mult)
                    P.i(S5_ENG2, "tensor_tensor", reads=[gk + "/1", EK], writes=[tk + "/2"], out=t[:, 2, 0:N], in0=epr, in1=g1, op=ALU.mult)
                    P.i("dve", "tensor_tensor", reads=[gk + "/0", EK], writes=[tk + "/3"], out=t[:, 3, 0:N], in0=epi, in1=g0, op=ALU.mult)
                    P.i("dve", "tensor_tensor", reads=[tk + "/0", tk + "/1"], writes=[hk + "/0"], out=h[:, 0, 0:N], in0=t[:, 0, 0:N], in1=t[:, 1, 0:N], op=ALU.subtract)
                    P.i("dve", "tensor_tensor", reads=[tk + "/2", tk + "/3"], writes=[hk + "/1"], out=h[:, 1, 0:N], in0=t[:, 2, 0:N], in1=t[:, 3, 0:N], op=ALU.add)
                    yield
                    P.i("act", "activation", reads=[hk], writes=[pf + "carry"], out=carry[:, dj, :], in_=h[:].bitcast(F32)[:, :, N - 1], func=AF.Copy)
                    py, pyk = pys[oc]
                    P.i("pe", "matmul", reads=[pf + "Cb", hk + "/0"], writes=[pyk], out=py[:, 0:N], lhsT=Cb[:, j, :], rhs=h[:, 0, 0:N], start=(j % 4 == 0), stop=False)
                    P.i("pe", "matmul", reads=[pf + "Cb", hk + "/1"], writes=[pyk], out=py[:, 0:N], lhsT=Cb[:, 8 + j, :], rhs=h[:, 1, 0:N], start=False, stop=(j % 4 == 3))
            for jp in range(0, 8, 2):
                gens = [s5_iter(jp), s5_iter(jp + 1)]
                alive = True
                while alive:
                    alive = False
                    for g_ in gens:
                        try:
                            next(g_)
                            alive = True
                        except StopIteration:
                            pass
            for oc in range(2):
                py, pyk = pys[oc]
                ya = yacc[:, oc, c0:c0 + N]
                yk = pf + "yacc/%d_%d" % (oc, c0)
                if d == 0:
                    P.i("dve", "scalar_tensor_tensor", reads=[uck, pf + "dsk", pyk], writes=[yk], out=ya, in0=uc[:, oc, 0:N].bitcast(F32), scalar=dsk[:, oc:oc + 1], in1=py[:, 0:N], op0=ALU.mult, op1=ALU.add)
                else:
                    P.i("dve", "tensor_tensor", reads=[yk, pyk], writes=[yk], out=ya, in0=ya, in1=R(py[:, 0:N]), op=ALU.add)
    e_r = Rot(P, pf + "e", [128, 3, NMAX], n=1)
    gT_r = Rot(P, pf + "gT", [128, 2, NMAX], F32R, n=1)
    pq_r = Rot(P, pf + "pq", [128, 512], n=2, psum=True)
    a_r = Rot(P, pf + "a", [128, NMAX], n=2)
    for (c0, N) in CHUNKS:
        gT, gTk = gT_r.next()
        for oc in range(2):
            y = yacc[:, oc, c0:c0 + N]
            yk = pf + "yacc/%d_%d" % (oc, c0)
            ee, ek = e_r.next()
            P.i("pool", "tensor_tensor", reads=[yk], writes=[ek + "/0"], out=ee[:, 0, 0:N], in0=y, in1=y, op=ALU.mult)
            P.i("dve", "tensor_scalar", reads=[ek + "/0"], writes=[ek + "/0"], out=ee[:, 0, 0:N], in0=ee[:, 0, 0:N], scalar1=0.044715, scalar2=1.0, op0=ALU.mult, op1=ALU.add)
            P.i("pool", "tensor_tensor", reads=[ek + "/0", yk], writes=[ek + "/1"], out=ee[:, 1, 0:N], in0=ee[:, 0, 0:N], in1=y, op=ALU.mult)
            P.i("act", "activation", reads=[ek + "/1"], writes=[ek + "/2"], out=ee[:, 2, 0:N], in_=ee[:, 1, 0:N], func=AF.Sigmoid, scale=1.5957691216057308)
            P.i("dve", "tensor_tensor", reads=[ek + "/2", yk], writes=[gTk + "/%d" % oc], out=gT[:, oc, 0:N], in0=ee[:, 2, 0:N], in1=y, op=ALU.mult)
        for oc2 in range(2):
            pq, pqk = pq_r.next()
            for oc in range(2):
                P.i("pe", "matmul", reads=[gTk, pf + "gw"], writes=[pqk], out=pq[:, 0:N], lhsT=gw[:, oc, oc2 * 128:(oc2 + 1) * 128], rhs=gT[:, oc, 0:N], start=(oc == 0), stop=(oc == 1))
            a, ak = a_r.next()
            P.i("act", "activation", reads=[pqk, pf + "gb"], writes=[ak], out=a[:, 0:N], in_=pq[:, 0:N], func=AF.Sigmoid, bias=gb[:, oc2:oc2 + 1])
            P.i("dve", "tensor_tensor", reads=[ak, gTk], writes=[ak], out=a[:, 0:N], in0=a[:, 0:N], in1=gT[:, oc2, 0:N].bitcast(F32), op=ALU.mult)
            P.d(CAT[oc2 * 128:(oc2 + 1) * 128, c0:c0 + N], a[:, 0:N], reads=[ak], writes=[P.u("CAT")])


def phase_d1(P, Dm, l):
    pf = "d%d_" % l
    FMT = Dm.get("FMT", [1024, T])
    XBCT = Dm.get("XBCT", [768, T])
    XBtok = Dm.get("XBtok", [T, 512])
    cw_d = Dm.get("convw%d" % l, [128, 6, 4], kind="ExternalInput")
    ident_d = Dm.get("ident", [128, 128], kind="ExternalInput")
    ident = P.sb(pf + "ident", [128, 128]); P.d(ident[:], ident_d[:, :], writes=[pf + "ident"])
    cw = P.sb(pf + "cw", [128, 6, 4]); P.d(cw[:], cw_d[:, :, :], writes=[pf + "cw"])
    xi_r = Rot(P, pf + "xi", [128, 6, 514], n=2)
    ac_r = Rot(P, pf + "ac", [128, 512], n=2)
    co_r = Rot(P, pf + "co", [128, 6, 512], n=2)
    tp_r = Rot(P, pf + "tp", [128, 512], n=2, psum=True)
    tk_r = Rot(P, pf + "tk", [128, 512], n=2)
    for (c0, N) in CHUNKS:
        xi, xik = xi_r.next()
        lz = c0 in (0, LC)
        rz = (c0 + N) in (LC, T)
        lo = c0 - (0 if lz else 1)
        hi = c0 + N + (0 if rz else 1)
        if lz:
            P.i("dve", "memset", writes=[xik + "/l"], ap=xi[:, :, 0:1], constant=0.0)
        if rz:
            P.i("dve", "memset", writes=[xik + "/r"], ap=xi[:, :, N + 1:N + 2], constant=0.0)
        P.d(xi[:, :, (1 if lz else 0):(1 if lz else 0) + hi - lo], FMT[256:1024, lo:hi].rearrange("(r p) t -> p r t", p=128), reads=["FMT"], writes=[xik + "/m"])
        co, cok = co_r.next()
        for rc in range(6):
            ac, ack = ac_r.next()
            P.i("dve", "tensor_scalar", reads=[xik, pf + "cw"], writes=[ack], out=ac[:, 0:N], in0=xi[:, rc, 1:N + 1], scalar1=cw[:, rc, 1:2], scalar2=None, op0=ALU.mult)
            P.i("dve", "scalar_tensor_tensor", reads=[xik, pf + "cw", ack], writes=[ack], out=ac[:, 0:N], in0=xi[:, rc, 0:N], scalar=cw[:, rc, 0:1], in1=ac[:, 0:N], op0=ALU.mult, op1=ALU.add)
            P.i("dve", "scalar_tensor_tensor", reads=[xik, pf + "cw", ack], writes=[ack], out=ac[:, 0:N], in0=xi[:, rc, 2:N + 2], scalar=cw[:, rc, 2:3], in1=ac[:, 0:N], op0=ALU.mult, op1=ALU.add)
            P.i("act", "activation", reads=[ack, pf + "cw"], writes=[cok + "/%d" % rc], out=co[:, rc, 0:N], in_=ac[:, 0:N], func=AF.Silu, bias=cw[:, rc, 3:4])
        P.d(XBCT[:, c0:c0 + N].rearrange("(r p) t -> p r t", p=128), co[:, :, 0:N], reads=[cok], writes=[P.u("XBCT")])
        for i in range(N // 128):
            tp, tpk = tp_r.next()
            for rc in range(4):
                P.i("pe", "transpose", reads=[cok + "/%d" % rc, pf + "ident"], writes=[tpk], noinc=(rc != 3), out=tp[:, rc * 128:(rc + 1) * 128], in_=co[:, rc, i * 128:(i + 1) * 128], identity=ident[:])
            tk, tkk = tk_r.next()
            P.i("act", "activation", reads=[tpk], writes=[tkk], out=tk[:], in_=tp[:], func=AF.Copy)
            P.d(XBtok[c0 + i * 128:c0 + (i + 1) * 128, :], tk[:], reads=[tkk], writes=[P.u("XBtok")])


def phase_d2(P, Dm, l):
    pf = "D%d_" % l
    XBCT = Dm.get("XBCT", [768, T])
    XBtok = Dm.get("XBtok", [T, 512])
    TMS = Dm.get("TMS", [T, 520])
    CAT = Dm.get("CAT", [1024, T])
    ident_d = Dm.get("ident", [128, 128], kind="ExternalInput")
    tri_d = Dm.get("tri", [128, 2, 128], kind="ExternalInput")
    alog_d = Dm.get("alog%d" % l, [1, 8], kind="ExternalInput")
    dsk_d = Dm.get("ssdd%d" % l, [1, 4], kind="ExternalInput")
    ng_d = Dm.get("ssdng%d" % l, [1, 256], kind="ExternalInput")
    ident = P.sb(pf + "ident", [128, 128]); P.d(ident[:], ident_d[:, :], writes=[pf + "ident"])
    tri = P.sb(pf + "tri", [128, 2, 128]); P.d(tri[:], tri_d[:, :, :], writes=[pf + "tri"])
    A = P.sb(pf + "A", [128, 8]); load_bcast(P, A[:], pf + "A", alog_d[0:1, :])
    P.i("act", "activation", reads=[pf + "A"], writes=[pf + "A"], out=A[:], in_=A[:], func=AF.Exp)
    P.i("dve", "tensor_scalar", reads=[pf + "A"], writes=[pf + "A"], out=A[:], in0=A[:], scalar1=-1.0, scalar2=None, op0=ALU.mult)
    dsk = P.sb(pf + "dsk", [128, 4]); load_bcast(P, dsk[:], pf + "dsk", dsk_d[0:1, :])
    ng = P.sb(pf + "ng", [128, 256]); load_bcast(P, ng[:], pf + "ng", ng_d[0:1, :])
    epsc = P.sb(pf + "eps", [128, 1]); P.i("dve", "memset", writes=[pf + "eps"], ap=epsc[:], constant=EPS)
    Yacc = P.sb(pf + "Yacc", [128, NT, 256])
    Sst = P.sb(pf + "S", [128, 4, 64], F32R)
    bc_r = Rot(P, pf + "bc", [128, 4, 128], F32R, n=2)
    xb_r = Rot(P, pf + "xb", [128, 512], n=2)
    xbr_r = Rot(P, pf + "xbr", [128, 256], F32R, n=2)
    dt_r = Rot(P, pf + "dt", [128, 8], n=2)
    sm_r = Rot(P, pf + "sm", [128, 64], n=2)
    abc_r = Rot(P, pf + "abc", [128, 4, 128], n=2)
    X_r = Rot(P, pf + "X", [128, 4, 64], F32R, n=2)
    Xd_r = Rot(P, pf + "Xd", [128, 4, 64], F32R, n=2)
    pG_r = Rot(P, pf + "pG", [128, 512], n=1, psum=True)
    pR_r = Rot(P, pf + "pR", [128, 512], n=1, psum=True)
    pC_r = Rot(P, pf + "pC", [128, 512], n=1, psum=True)
    pY_r = Rot(P, pf + "pY", [128, 512], n=2, psum=True)
    pS_r = Rot(P, pf + "pS", [128, 512], n=1, psum=True)
    Gm_r = Rot(P, pf + "Gm", [128, 2, 128], n=2)
    df_r = Rot(P, pf + "df", [128, 128], n=2)
    sc_r = Rot(P, pf + "scT", [128, 128], F32R, n=2)
    yo_r = Rot(P, pf + "yo", [128, 64], n=2)
    for d in range(2):
        order = list(range(NT)) if d == 0 else [1, 0] + list(range(NT - 1, 1, -1))
        P.i("dve", "memset", writes=[pf + "S"], ap=Sst[:].bitcast(F32), constant=0.0)
        for ci, c in enumerate(order):
            t0 = c * 128
            bc, bck = bc_r.next()
            P.d(bc[:], XBCT[256:768, t0:t0 + 128].rearrange("(r p) t -> p r t", p=128), reads=["XBCT"], writes=[bck], q="pool")
            xb, xbk = xb_r.next()
            P.d(xb[:], XBtok[t0:t0 + 128, :], reads=["XBtok"], writes=[xbk])
            xbr, xbrk = xbr_r.next()
            P.i("act", "activation", reads=[xbk], writes=[xbrk], out=xbr[:], in_=xb[:, 256:512], func=AF.Copy)
            dt, dtk = dt_r.next()
            P.d(dt[:], TMS[t0:t0 + 128, 512:520], reads=["TMS"], writes=[dtk])
            sm, smk = sm_r.next()
            P.i("dve", "tensor_tensor", reads=[dtk, pf + "A"], writes=[smk], out=sm[:, 0:4], in0=dt[:, d * 4:d * 4 + 4], in1=A[:, d * 4:d * 4 + 4], op=ALU.mult)
            abc, abck = abc_r.next()
            P.i("dve", "tensor_copy", reads=[smk], writes=[abck], out=abc[:], in_=sm[:, 0:4].unsqueeze(2).to_broadcast([128, 4, 128]))
            X, Xk = X_r.next()
            P.i("pool", "tensor_tensor", reads=[xbk, dtk], writes=[Xk], out=X[:], in0=xb[:, 0:256].rearrange("p (h e) -> p h e", h=4),
                in1=dt[:, d * 4:d * 4 + 4].unsqueeze(2).to_broadcast([128, 4, 64]), op=ALU.mult)
            pG, pGk = pG_r.next()
            for g in range(2):
                P.i("pe", "matmul", reads=[bck], writes=[pGk], out=pG[:, g * 128:(g + 1) * 128], lhsT=bc[:, g, :], rhs=bc[:, 2 + g, :], start=True, stop=True)
            Gm, Gmk = Gm_r.next()
            P.i("dve", "tensor_tensor", reads=[pGk, pf + "tri"], writes=[Gmk], out=Gm[:], in0=pG[:, 0:256].rearrange("p (g l) -> p g l", g=2),
                in1=tri[:, d, :].unsqueeze(1).to_broadcast([128, 2, 128]), op=ALU.mult)
            pC, pCk = pC_r.next()
            P.i("pe", "matmul", reads=[smk, pf + "tri"], writes=[pCk], out=pC[:, 0:4], lhsT=tri[:, d, :], rhs=sm[:, 0:4], start=True, stop=True)
            pR, pRk = pR_r.next()
            for h in range(4):
                P.i("pe", "matmul", reads=[abck, pf + "tri"], writes=[pRk], out=pR[:, h * 128:(h + 1) * 128], lhsT=abc[:, h, :], rhs=tri[:, d, :], start=True, stop=True)
            P.i("dve", "tensor_copy", reads=[pCk], writes=[smk], out=sm[:, 4:8], in_=pC[:, 0:4])
            P.i("act", "activation", reads=[smk], writes=[smk], out=sm[:, 8:12], in_=sm[:, 4:8], func=AF.Exp)
            last = 127 if d == 0 else 0
            P.i("dve", "tensor_copy", reads=[pRk], writes=[smk], out=sm[:, 20:24], in_=pR[:].rearrange("p (h l) -> p h l", h=4)[:, :, last])
            P.i("dve", "tensor_tensor", reads=[smk], writes=[smk], out=sm[:, 12:16], in0=sm[:, 20:24], in1=sm[:, 4:8], op=ALU.subtract)
            P.i("act", "activation", reads=[smk], writes=[smk], out=sm[:, 12:16], in_=sm[:, 12:16], func=AF.Exp)
            P.i("act", "activation", reads=[smk], writes=[smk], out=sm[:, 16:20], in_=sm[:, 20:24], func=AF.Exp)
            Xd, Xdk = Xd_r.next()
            P.i("pool", "tensor_tensor", reads=[Xk, smk], writes=[Xdk], out=Xd[:], in0=X[:].bitcast(F32), in1=sm[:, 12:16].unsqueeze(2).to_broadcast([128, 4, 64]), op=ALU.mult)
            pY, pYk = pY_r.next()
            pS, pSk = pS_r.next()
            for h in range(4):
                g = h // 2
                df, dfk = df_r.next()
                P.i("dve", "tensor_scalar", reads=[pRk, smk], writes=[dfk], out=df[:], in0=pR[:, h * 128:(h + 1) * 128], scalar1=sm[:, 4 + h:5 + h], scalar2=0.0, op0=ALU.subtract, op1=ALU.min)
                P.i("act", "activation", reads=[dfk], writes=[dfk], out=df[:], in_=df[:], func=AF.Exp)
                scT, scTk = sc_r.next()
                P.i("pool", "tensor_tensor", reads=[dfk, Gmk], writes=[scTk], out=scT[:], in0=df[:], in1=Gm[:, g, :], op=ALU.mult)
                P.i("pe", "matmul", reads=[scTk, Xk], writes=[pYk], out=pY[:, h * 128:h * 128 + 64], lhsT=scT[:], rhs=X[:, h, :], start=True, stop=True)
                P.i("pe", "matmul", reads=[bck, pf + "S"], writes=[pYk], out=pY[:, h * 128 + 64:h * 128 + 128], lhsT=bc[:, 2 + g, :], rhs=Sst[:, h, :], start=True, stop=True)
                P.i("pe", "matmul", reads=[xbrk, Xdk], writes=[pSk], out=pS[:, h * 64:(h + 1) * 64], lhsT=xbr[:, g * 128:(g + 1) * 128], rhs=Xd[:, h, :], start=True, stop=True)
            for h in range(4):
                yo, yok = yo_r.next()
                P.i("act", "activation", reads=[pYk, smk], writes=[yok], out=yo[:], in_=pY[:, h * 128 + 64:h * 128 + 128], func=AF.Copy, scale=sm[:, 8 + h:9 + h])
                ya = Yacc[:, c, h * 64:(h + 1) * 64]
                yk = pf + "Yacc/%d_%d" % (c, h)
                if d == 0:
                    P.i("dve", "tensor_tensor", reads=[pYk, yok], writes=[yk], out=ya, in0=pY[:, h * 128:h * 128 + 64], in1=yo[:], op=ALU.add)
                else:
                    P.i("dve", "tensor_tensor", reads=[pYk, yok], writes=[yok], out=yo[:], in0=pY[:, h * 128:h * 128 + 64], in1=yo[:], op=ALU.add)
                    P.i("pool", "tensor_tensor", reads=[yk, yok], writes=[yk], out=ya, in0=ya, in1=yo[:], op=ALU.add)
                P.i("dve", "scalar_tensor_tensor", reads=[pf + "S", smk, pSk], writes=[pf + "S"], out=Sst[:, h, :], in0=Sst[:, h, :].bitcast(F32), scalar=sm[:, 16 + h:17 + h],
                    in1=pS[:, h * 64:(h + 1) * 64], op0=ALU.mult, op1=ALU.add)
    z_r = Rot(P, pf + "z", [128, 256], n=2)
    yt_r = Rot(P, pf + "yt", [128, 256], n=2)
    jk = P.sb(pf + "jk", [128, 256])
    pT_r = Rot(P, pf + "pT", [128, 512], n=2, psum=True)
    oT_r = Rot(P, pf + "oT", [128, 2, 128], n=2)
    for c in range(NT):
        t0 = c * 128
        xb, xbk = xb_r.next()
        P.d(xb[:], XBtok[t0:t0 + 128, :], reads=["XBtok"], writes=[xbk])
        z, zk = z_r.next()
        P.d(z[:], TMS[t0:t0 + 128, 256:512], reads=["TMS"], writes=[zk])
        yt, ytk = yt_r.next()
        P.i("pool", "tensor_tensor", reads=[xbk, pf + "dsk"], writes=[ytk], out=yt[:].rearrange("p (h e) -> p h e", h=4), in0=xb[:, 0:256].rearrange("p (h e) -> p h e", h=4),
            in1=dsk[:].unsqueeze(2).to_broadcast([128, 4, 64]), op=ALU.mult)
        P.i("dve", "tensor_tensor", reads=[ytk, pf + "Yacc/%d" % c], writes=[ytk], out=yt[:], in0=yt[:], in1=Yacc[:, c, :], op=ALU.add)
        P.i("act", "activation", reads=[zk], writes=[zk], out=z[:], in_=z[:], func=AF.Silu)
        P.i("dve", "tensor_tensor", reads=[ytk, zk], writes=[ytk], out=yt[:], in0=yt[:], in1=z[:], op=ALU.mult)
        sm, smk = sm_r.next()
        P.i("act", "activation", reads=[ytk], writes=[pf + "jk", smk], out=jk[:], in_=yt[:], func=AF.Square, accum_out=sm[:, 0:1])
        P.i("act", "activation", reads=[smk, pf + "eps"], writes=[smk], out=sm[:, 1:2], in_=sm[:, 0:1], func=AF.Sqrt, bias=epsc[:], scale=1.0 / 256)
        P.i("dve", "reciprocal", reads=[smk], writes=[smk], out=sm[:, 2:3], in_=sm[:, 1:2])
        P.i("dve", "scalar_tensor_tensor", reads=[ytk, smk, pf + "ng"], writes=[ytk], out=yt[:], in0=yt[:], scalar=sm[:, 2:3], in1=ng[:], op0=ALU.mult, op1=ALU.mult)
        pT, pTk = pT_r.next()
        for q in range(2):
            P.i("pe", "transpose", reads=[ytk, pf + "ident"], writes=[pTk], out=pT[:, q * 128:(q + 1) * 128], in_=yt[:, q * 128:(q + 1) * 128], identity=ident[:])
        oT, oTk = oT_r.next()
        P.i("act", "activation", reads=[pTk], writes=[oTk], out=oT[:], in_=pT[:, 0:256].rearrange("p (q t) -> p q t", q=2), func=AF.Copy)
        P.d(CAT[512:768, t0:t0 + 128].rearrange("(q p) t -> p q t", p=128), oT[:], reads=[oTk], writes=[P.u("CAT")])


LAT_CHUNKS = [(256 + 512 * i, 512) for i in range(8)]
ALL_CHUNKS512 = [(512 * i, 512) for i in range(8)] + [(4096, 256)]


def build_program(scopes=False):
    nc = bass.Bass("TRN2", target_bir_lowering=False)
    P = Prog(nc)
    P.scopes = scopes
    Dm = Dram(nc, ext_in=["xin"])
    src = "xin"
    for l in range(2):
        last = (l == 1)
        ctx_out = not last
        for fi, fn in enumerate((lambda: phase_mod(P, Dm, l),
                   lambda: phase_a(P, Dm, l, src),
                   lambda: phase_b(P, Dm, l),
                   lambda: phase_c(P, Dm, l, ctx_out),
                   lambda: phase_d1(P, Dm, l),
                   lambda: phase_d2(P, Dm, l),
                   lambda: phase_e(P, Dm, l, ctx_out),
                   lambda: phase_f2(P, Dm, l, src, tok_chunks=(LAT_CHUNKS if last else None)),
                   lambda: phase_g2(P, Dm, l, (LAT_CHUNKS if last else CHUNKS), last))):
            P.begin_phase("L%d_%s" % (l, "mabcdDefg"[fi]))
            fn()
            P.end_phase()
        src = "XN%d" % l
    P.emit()
    return nc, P, Dm


_CACHE = {}


def kernel(**inputs):
    inp = {k: np.asarray(v) for k, v in inputs.items()}
    if "nc" not in _CACHE:
        _CACHE["nc"] = build_program()
    nc, P, Dm = _CACHE["nc"]
    n_cores = 8
    maps = []
    per_b = {}
    for cidx in range(n_cores):
        b = cidx % 4
        if b not in per_b:
            m = core_inputs(inp, b)
            per_b[b] = {k: v for k, v in m.items() if k in Dm.t}
        maps.append(per_b[b])
    res = run_bass_kernel_spmd(nc, maps, core_ids=list(range(n_cores)))
    out = np.stack([np.asarray(res.results[b]["out"]) for b in range(4)], 0)
    return out.astype(np.float32)
```

```python
import numpy as np
from contextlib import ExitStack
import concourse.bass as bass
import concourse.mybir as mybir
from concourse.bass_utils import run_bass_kernel_spmd

F32 = mybir.dt.float32
F32R = mybir.dt.float32r
I32 = mybir.dt.int32
AF = mybir.ActivationFunctionType
ALU = mybir.AluOpType
AX = mybir.AxisListType

LC, L, T, D = 256, 4096, 4352, 1024
NT = T // 128
EPS = 1e-6
CHUNKS = [(0, 256)] + [(256 + 512 * i, 512) for i in range(8)]
PERM = np.concatenate([np.arange(256, 768), np.arange(1800, 2312), np.arange(768, 1024), np.arange(1792, 1800),
                       np.arange(0, 256), np.arange(1024, 1792)])
TM_COLS = 1288
FM_OFF = 1288

ENGS = ["pe", "act", "dve", "pool", "sp"]
N_DMA_SEMS = 8


class Prog:
    def __init__(self, nc, same_engine_sync=True):
        self.nc = nc
        self.st = ExitStack()
        self.same = same_engine_sync
        self.ops = {e: [] for e in ENGS}
        self.sems = {}
        self.cnt = {}
        for e in ["pe", "act", "dve", "pool"]:
            self.sems[e] = self.st.enter_context(nc.semaphore("s_" + e))
            self.cnt[e] = 0
        for q in ["sp", "pool"]:
            for i in range(N_DMA_SEMS):
                nm = "d_%s%d" % (q, i)
                self.sems[nm] = self.st.enter_context(nc.semaphore(nm))
                self.cnt[nm] = 0
        self.drr = {"sp": 0, "pool": 0}
        self.waited = {e: {} for e in ENGS}
        self.last_w = {}
        self.readers = {}
        self.n_ops = 0
        self.final = []
        self.uid = 0
        self.pst = None
        self.barrier = {e: {} for e in ENGS}
        self.children = {}
        self.known = set()
        self.scopes = False
        self.bound_reg = None
        self.pending_noinc = {e: False for e in ENGS}
        self.psum_keys = set()

    def begin_phase(self, name=None):
        self.pst = ExitStack()
        self.phase_name = name

    def end_phase(self):
        self.pst.close()
        self.pst = None
        assert not any(self.pending_noinc.values()), self.pending_noinc
        snap = {s: v for s, v in self.cnt.items() if v > 0}
        for e in ENGS:
            self.barrier[e] = dict(snap)

    def bound(self):
        if self.bound_reg is None:
            self.bound_reg = self.nc.gpsimd.alloc_register("bc4095")
        return self.bound_reg

    def sb(self, name, shape, dtype=F32):
        return (self.pst or self.st).enter_context(self.nc.sbuf_tensor(name, list(shape), dtype))

    def ps(self, name, shape, dtype=F32):
        return (self.pst or self.st).enter_context(self.nc.psum_tensor(name, list(shape), dtype))

    def _related(self, k):
        rel = [k]
        parts = k.split("/")
        for n in range(1, len(parts)):
            rel.append("/".join(parts[:n]))
        rel.extend(self.children.get(k, ()))
        return rel

    def _register(self, k):
        if k in self.known:
            return
        self.known.add(k)
        parts = k.split("/")
        for n in range(1, len(parts)):
            self.children.setdefault("/".join(parts[:n]), set()).add(k)

    def _deps(self, eng, reads, writes):
        deps = {}

        def add(s, v):
            if v > deps.get(s, 0):
                deps[s] = v

        for k in list(reads) + list(writes):
            self._register(k)
        for k in reads:
            for kk in self._related(k):
                t = self.last_w.get(kk)
                if t is not None:
                    add(*t)
        for k in writes:
            for kk in self._related(k):
                t = self.last_w.get(kk)
                if t is not None:
                    add(*t)
                for s_, v_ in self.readers.get(kk, {}).items():
                    add(s_, v_)
        if self.barrier[eng]:
            for s_, v_ in self.barrier[eng].items():
                add(s_, v_)
            self.barrier[eng] = {}
        out = []
        for s, v in deps.items():
            if s == eng and (eng == "pe" or not self.same):
                continue
            if v > self.waited[eng].get(s, 0):
                self.waited[eng][s] = v
                out.append((s, v))
        return out

    def _commit(self, tok, reads, writes):
        for k in writes:
            self.last_w[k] = tok
            self.readers[k] = {}
        for k in reads:
            if k in writes:
                continue
            rd = self.readers.setdefault(k, {})
            if tok[1] > rd.get(tok[0], 0):
                rd[tok[0]] = tok[1]

    def op(self, eng, fn, reads=(), writes=(), noinc=False):
        pr = [k for k in reads if k in self.psum_keys]
        if pr:
            reads = [k for k in reads if k not in self.psum_keys]
            writes = list(writes) + pr
        waits = self._deps(eng, reads, writes)
        if noinc:
            tok = (eng, self.cnt[eng] + 1)
            self.pending_noinc[eng] = True
            self.ops[eng].append((waits, fn, tok, 0, getattr(self, "phase_name", None)))
        else:
            self.cnt[eng] += 1
            tok = (eng, self.cnt[eng])
            self.pending_noinc[eng] = False
            self.ops[eng].append((waits, fn, tok, 1, getattr(self, "phase_name", None)))
        self._commit(tok, reads, writes)
        self.n_ops += 1
        return tok

    def u(self, base):
        self.uid += 1
        return "%s/%d" % (base, self.uid)

    def i(self, eng, name, reads=(), writes=(), noinc=False, **kw):
        if name == "matmul" and kw.get("stop") is False:
            noinc = True
        return self.op(eng, lambda e: getattr(e, name)(**kw), reads, writes, noinc=noinc)

    def d(self, out, in_, reads=(), writes=(), q="sp", final=False):
        return self.dma(lambda e: e.dma_start(out=out, in_=in_), reads, writes, q=q, final=final)

    def dma(self, fn, reads=(), writes=(), q="sp", final=False):
        waits = self._deps(q, reads, writes)
        i = self.drr[q]
        self.drr[q] = (i + 1) % N_DMA_SEMS
        nm = "d_%s%d" % (q, i)
        prev = self.cnt[nm]
        if prev > self.waited[q].get(nm, 0):
            self.waited[q][nm] = prev
            waits.append((nm, prev))
        self.cnt[nm] += 16
        tok = (nm, self.cnt[nm])
        self.ops[q].append((waits, fn, tok, 16, getattr(self, "phase_name", None)))
        self._commit(tok, reads, writes)
        self.n_ops += 1
        if final:
            self.final.append(tok)
        return tok

    def emit(self):
        nc = self.nc
        fin = list(self.final)
        engmap = {"pe": "tensor", "act": "scalar", "dve": "vector", "pool": "gpsimd", "sp": "sync"}
        with nc.Block() as block:
            for e in ENGS:
                lst = self.ops[e]
                extra = fin if e == "sp" else []
                if not lst and not extra:
                    continue

                def body(engine, lst=lst, extra=extra, e=e):
                    cur = None
                    scope = None
                    if e == "pool" and self.bound_reg is not None:
                        engine.reg_mov(self.bound_reg, 4095)
                    for (waits, fn, tok, amt, ph) in lst:
                        if self.scopes and ph != cur:
                            if scope is not None:
                                scope.__exit__(None, None, None)
                            scope = nc.named_scope(ph or "none")
                            scope.__enter__()
                            cur = ph
                        for (s, v) in waits:
                            engine.wait_ge(self.sems[s], v)
                        ins = fn(engine)
                        if amt:
                            ins.then_inc(self.sems[tok[0]], amt)
                    if scope is not None:
                        scope.__exit__(None, None, None)
                    for (s, v) in extra:
                        engine.wait_ge(self.sems[s], v)

                getattr(block, engmap[e])(body)
        self.st.close()


class Rot:
    def __init__(self, P, name, shape, dtype=F32, n=2, psum=False):
        self.bufs = [(P.ps if psum else P.sb)("%s%d" % (name, i), shape, dtype) for i in range(n)]
        self.keys = ["%s%d" % (name, i) for i in range(n)]
        self.i = 0
        if psum:
            P.psum_keys.update(self.keys)

    def next(self):
        j = self.i % len(self.bufs)
        self.i += 1
        return self.bufs[j], self.keys[j]


class Dram:
    def __init__(self, nc, ext_in=(), ext_out=()):
        self.nc, self.t = nc, {}
        self.ext_in, self.ext_out = set(ext_in), set(ext_out)

    def get(self, name, shape=None, dtype=F32, kind=None):
        if name not in self.t:
            if kind is None:
                kind = "ExternalInput" if name in self.ext_in else ("ExternalOutput" if name in self.ext_out else "Internal")
            self.t[name] = self.nc.dram_tensor(name, list(shape), dtype, kind=kind).ap()
        return self.t[name]


def phase_mod(P, Dm, l):
    nc = P.nc
    cvec = Dm.get("cvec", [128, 8, 2], kind="ExternalInput")
    ada_w = Dm.get("ada_w%d" % l, [128, 8, 6144], kind="ExternalInput")
    ada_b = Dm.get("ada_b%d" % l, [1, 6144], kind="ExternalInput")
    modrow = Dm.get("modrow%d" % l, [2, 6144])
    pf = "m%d_" % l
    cv = P.sb(pf + "cv", [128, 8, 2])
    sg = P.sb(pf + "sg", [128, 8, 2])
    sc = P.sb(pf + "sc", [128, 8, 128], F32R)
    ab = P.sb(pf + "ab", [2, 6144])
    mr = P.sb(pf + "mr", [2, 6144])
    wch = Rot(P, pf + "w", [128, 8, 512], F32R, n=2)
    pm = Rot(P, pf + "pm", [128, 512], F32, n=2, psum=True)
    P.dma(lambda e: e.dma_start(out=cv[:], in_=cvec[:, :, :]), writes=[pf + "cv"])
    P.dma(lambda e: e.dma_start(out=ab[:], in_=ada_b[0:1, :].to_broadcast([2, 6144])), writes=[pf + "ab"])
    P.op("act", lambda e: e.activation(out=sg[:], in_=cv[:], func=AF.Sigmoid), reads=[pf + "cv"], writes=[pf + "sg"])
    P.op("dve", lambda e: e.memset(sc[:].bitcast(F32), 0.0), writes=[pf + "sc"])
    P.op("dve", lambda e: e.tensor_tensor(out=sc[:, :, 0:2], in0=cv[:], in1=sg[:], op=ALU.mult), reads=[pf + "cv", pf + "sg"], writes=[pf + "sc"])
    for j in range(12):
        w, wk = wch.next()
        P.dma(lambda e, w=w, j=j: e.dma_start(out=w[:], in_=ada_w[:, :, j * 512:(j + 1) * 512]), writes=[wk], q="pool")
        pt, pk = pm.next()
        for k in range(8):
            P.op("pe", lambda e, w=w, pt=pt, k=k: e.matmul(pt[:], sc[:, k, :], w[:, k, :], start=(k == 0), stop=(k == 7)),
                 reads=[wk, pf + "sc"], writes=[pk], noinc=(k != 7))
        P.op("dve", lambda e, pt=pt, j=j: e.tensor_tensor(out=mr[:, j * 512:(j + 1) * 512], in0=pt[0:2, :], in1=ab[:, j * 512:(j + 1) * 512], op=ALU.add),
             reads=[pk, pf + "ab"], writes=[pf + "mr"])
    P.dma(lambda e: e.dma_start(out=modrow[:, :], in_=mr[:]), reads=[pf + "mr"], writes=["modrow%d" % l])


def load_bcast(P, dst, dkey, src_row, reads=()):
    n = src_row.shape[-1]
    P.dma(lambda e: e.dma_start(out=dst, in_=src_row.to_broadcast([128, n])), reads=list(reads), writes=[dkey])


def phase_a(P, Dm, l, src_name, chunks=None, stop=99):
    nc = P.nc
    pf = "a%d_" % l
    xsrc = Dm.get(src_name, [T, D])
    w_in = Dm.get("w_in%d" % l, [128, 8, 2312], kind="ExternalInput")
    modrow = Dm.get("modrow%d" % l, [2, 6144])
    n1g = Dm.get("norm1_g%d" % l, [1, D], kind="ExternalInput")
    qkg = Dm.get("qkg%d" % l, [1, 384], kind="ExternalInput")
    dtb = Dm.get("dtb%d" % l, [1, 8], kind="ExternalInput")
    ropec = Dm.get("rope_cos", [L, 384], kind="ExternalInput")
    ropes = Dm.get("rope_sin", [L, 384], kind="ExternalInput")
    ident_d = Dm.get("ident", [128, 128], kind="ExternalInput")
    FMT = Dm.get("FMT", [1024, T])
    QKT = Dm.get("QKT", [768, T])
    TMS = Dm.get("TMS", [T, 520])
    mk = "modrow%d" % l

    ident = P.sb(pf + "ident", [128, 128])
    P.dma(lambda e: e.dma_start(out=ident[:], in_=ident_d[:, :]), writes=[pf + "ident"])
    win = P.sb(pf + "win", [128, 8, 2312], F32R)
    for k in range(8):
        P.dma(lambda e, k=k: e.dma_start(out=win[:, k, :], in_=w_in[:, k, :]), writes=[pf + "win/%d" % k], q="pool")
    wkeys = [pf + "win/%d" % k for k in range(8)]
    G = [P.sb(pf + "G%d" % r, [128, D]) for r in range(2)]
    SH = [P.sb(pf + "SH%d" % r, [128, D]) for r in range(2)]
    gn = P.sb(pf + "gn", [128, D])
    load_bcast(P, gn[:], pf + "gn", n1g[0:1, :])
    for r in range(2):
        load_bcast(P, SH[r][:], pf + "SH%d" % r, modrow[r:r + 1, 0:1024], reads=[mk])
        load_bcast(P, G[r][:], pf + "G%d" % r, modrow[r:r + 1, 1024:2048], reads=[mk])
        P.op("dve", lambda e, r=r: e.scalar_tensor_tensor(out=G[r][:], in0=G[r][:], scalar=1.0, in1=gn[:], op0=ALU.add, op1=ALU.mult),
             reads=[pf + "G%d" % r, pf + "gn"], writes=[pf + "G%d" % r])
    qkgb = P.sb(pf + "qkgb", [128, 384])
    load_bcast(P, qkgb[:], pf + "qkgb", qkg[0:1, :])
    dtbb = P.sb(pf + "dtbb", [128, 8])
    load_bcast(P, dtbb[:], pf + "dtbb", dtb[0:1, :])
    epsc = P.sb(pf + "eps", [128, 1])
    P.op("dve", lambda e: e.memset(epsc[:], EPS), writes=[pf + "eps"])
    onec = P.sb(pf + "one", [128, 1])
    P.op("dve", lambda e: e.memset(onec[:], 1.0), writes=[pf + "one"])

    xt_r = Rot(P, pf + "xt", [128, D], n=3)
    junk = P.sb(pf + "junk", [128, D])
    st_r = Rot(P, pf + "st", [128, 32], n=3)
    h_r = Rot(P, pf + "h", [128, D], n=2)
    tp_r = Rot(P, pf + "tp", [128, 1024], n=1, psum=True)
    hT_r = Rot(P, pf + "hT", [128, 8, 512], F32R, n=2)
    pj_r = Rot(P, pf + "pj", [128, 512], n=3, psum=True)
    qk_r = Rot(P, pf + "qk", [128, 12, 64], n=2)
    sq_r = Rot(P, pf + "sq", [128, 6, 64], n=2)
    rp_r = Rot(P, pf + "rp", [128, 24, 2, 16], n=2)
    tmp_r = Rot(P, pf + "tmp", [128, 24, 16], n=2)
    cs_r = Rot(P, pf + "cs", [128, 2, 384], n=2)
    tq_r = Rot(P, pf + "tq", [128, 1024], n=1, psum=True)
    qT_r = Rot(P, pf + "qT", [128, 6, 128], n=2)
    tm_r = Rot(P, pf + "tm", [128, 520], n=2)
    fm_r = Rot(P, pf + "fm", [128, 512], n=1, psum=True)
    fo_r = Rot(P, pf + "fo", [128, 512], n=3)

    for (c0, cn) in (chunks or CHUNKS):
        hT, hTk = hT_r.next()
        ntile = cn // 128
        for i in range(ntile):
            t0 = c0 + i * 128
            is_ctx = t0 < LC
            r = 1 if is_ctx else 0
            xt, xk = xt_r.next()
            P.dma(lambda e, xt=xt, t0=t0: e.dma_start(out=xt[:], in_=xsrc[t0:t0 + 128, :]), reads=[src_name], writes=[xk])
            st, sk = st_r.next()
            P.op("act", lambda e, xt=xt, st=st: e.activation(out=junk[:], in_=xt[:], func=AF.Square, accum_out=st[:, 0:1]),
                 reads=[xk], writes=[pf + "junk", sk])
            P.op("act", lambda e, st=st: e.activation(out=st[:, 1:2], in_=st[:, 0:1], func=AF.Ln, bias=epsc[:], scale=1.0 / D),
                 reads=[sk, pf + "eps"], writes=[sk])
            P.op("act", lambda e, st=st: e.activation(out=st[:, 2:3], in_=st[:, 1:2], func=AF.Exp, scale=-0.5), reads=[sk], writes=[sk])
            h, hk = h_r.next()
            P.op("dve", lambda e, xt=xt, st=st, h=h, r=r: e.scalar_tensor_tensor(out=h[:], in0=xt[:], scalar=st[:, 2:3], in1=G[r][:], op0=ALU.mult, op1=ALU.mult),
                 reads=[xk, sk, pf + "G%d" % r], writes=[hk])
            P.op("pool", lambda e, h=h, r=r: e.tensor_tensor(out=h[:], in0=h[:], in1=SH[r][:], op=ALU.add),
                 reads=[hk, pf + "SH%d" % r], writes=[hk])
            if stop == 1:
                continue
            tp, tpk = tp_r.next()
            for k in range(8):
                P.op("pe", lambda e, h=h, tp=tp, k=k: e.transpose(tp[:, k * 128:(k + 1) * 128], h[:, k * 128:(k + 1) * 128], ident[:]),
                     reads=[hk, pf + "ident"], writes=[tpk], noinc=(k != 7))
            P.op("act", lambda e, hT=hT, tp=tp, i=i: e.activation(out=hT[:, :, i * 128:(i + 1) * 128], in_=tp[:].rearrange("p (k t) -> p k t", k=8), func=AF.Copy),
                 reads=[tpk], writes=[hTk + "/%d" % i])
            if stop == 2:
                continue
            pjs = []
            for (o, n) in [(0, 512), (512, 512), (1024, 264)]:
                pj, pjk = pj_r.next()
                for k in range(8):
                    P.op("pe", lambda e, pj=pj, hT=hT, k=k, o=o, n=n, i=i: e.matmul(pj[:, 0:n], hT[:, k, i * 128:(i + 1) * 128], win[:, k, o:o + n], start=(k == 0), stop=(k == 7)),
                         reads=[hTk + "/%d" % i, wkeys[k]], writes=[pjk], noinc=(k != 7))
                pjs.append((pj, pjk))
            (pA, pAk), (pB, pBk), (pC, pCk) = pjs
            if stop == 3:
                continue
            qk, qkk = qk_r.next()
            sq, sqk = sq_r.next()
            pA6 = pA[:, 0:384].rearrange("p (h d) -> p h d", h=6)
            P.op("act", lambda e, sq=sq, pA6=pA6: e.activation(out=sq[:], in_=pA6, func=AF.Square), reads=[pAk], writes=[sqk])
            P.op("dve", lambda e, sq=sq, st=st: e.tensor_reduce(out=st[:, 4:10], in_=sq[:], axis=AX.X, op=ALU.add), reads=[sqk], writes=[sk])
            P.op("act", lambda e, st=st: e.activation(out=st[:, 4:10], in_=st[:, 4:10], func=AF.Ln, bias=epsc[:], scale=1.0 / 64), reads=[sk, pf + "eps"], writes=[sk])
            P.op("act", lambda e, st=st: e.activation(out=st[:, 10:16], in_=st[:, 4:10], func=AF.Exp, scale=-0.5), reads=[sk], writes=[sk])
            P.op("dve", lambda e, sq=sq, pA6=pA6, st=st: e.tensor_tensor(out=sq[:], in0=pA6, in1=st[:, 10:16].unsqueeze(2).to_broadcast([128, 6, 64]), op=ALU.mult),
                 reads=[pAk, sk], writes=[sqk])
            P.op("pool", lambda e, sq=sq, qk=qk: e.tensor_tensor(out=qk[:, 0:6, :], in0=sq[:], in1=qkgb[:].rearrange("p (h d) -> p h d", h=6), op=ALU.mult),
                 reads=[sqk, pf + "qkgb"], writes=[qkk + "/a"])
            P.op("act", lambda e, qk=qk, pB=pB: e.activation(out=qk[:, 6:12, :], in_=pB[:, 0:384].rearrange("p (h d) -> p h d", h=6), func=AF.Copy),
                 reads=[pBk], writes=[qkk + "/b"])
            if stop == 4:
                continue
            tm, tmk = tm_r.next()
            P.op("act", lambda e, tm=tm, pA=pA: e.activation(out=tm[:, 0:128], in_=pA[:, 384:512], func=AF.Copy), reads=[pAk], writes=[tmk + "/a"])
            P.op("act", lambda e, tm=tm, pB=pB: e.activation(out=tm[:, 128:256], in_=pB[:, 384:512], func=AF.Copy), reads=[pBk], writes=[tmk + "/b"])
            P.op("act", lambda e, tm=tm, pC=pC: e.activation(out=tm[:, 256:512], in_=pC[:, 0:256], func=AF.Copy), reads=[pCk], writes=[tmk + "/c"])
            P.op("dve", lambda e, tm=tm, pC=pC: e.tensor_tensor(out=tm[:, 512:520], in0=pC[:, 256:264], in1=dtbb[:], op=ALU.add), reads=[pCk, pf + "dtbb"], writes=[tmk + "/d"])
            P.op("act", lambda e, st=st, tm=tm: e.activation(out=st[:, 16:24], in_=tm[:, 512:520], func=AF.Abs), reads=[tmk + "/d", sk], writes=[sk])
            P.op("act", lambda e, st=st: e.activation(out=st[:, 16:24], in_=st[:, 16:24], func=AF.Exp, scale=-1.0), reads=[sk], writes=[sk])
            P.op("act", lambda e, st=st: e.activation(out=st[:, 16:24], in_=st[:, 16:24], func=AF.Ln, bias=onec[:]), reads=[sk], writes=[sk])
            P.op("dve", lambda e, st=st, tm=tm: e.scalar_tensor_tensor(out=tm[:, 512:520], in0=tm[:, 512:520], scalar=0.0, in1=st[:, 16:24], op0=ALU.max, op1=ALU.add),
                 reads=[tmk + "/d", sk], writes=[tmk + "/d"])
            P.dma(lambda e, tm=tm, t0=t0: e.dma_start(out=TMS[t0:t0 + 128, :], in_=tm[:]), reads=[tmk], writes=[P.u("TMS")])
            if stop == 5:
                continue
            rp, rpk = rp_r.next()
            if is_ctx:
                P.op("pool", lambda e, rp=rp, qk=qk: e.tensor_copy(out=rp[:].rearrange("p (h a) b f -> p h (a b f)", a=2), in_=qk[:]),
                     reads=[qkk + "/a", qkk + "/b"], writes=[rpk])
            else:
                cs, csk = cs_r.next()
                tl = t0 - LC
                P.dma(lambda e, cs=cs, tl=tl: e.dma_start(out=cs[:, 0, :], in_=ropec[tl:tl + 128, :]), writes=[csk + "/c"])
                P.dma(lambda e, cs=cs, tl=tl: e.dma_start(out=cs[:, 1, :], in_=ropes[tl:tl + 128, :]), writes=[csk + "/s"])
                qv = qk[:].rearrange("p h (a b f) -> p (h a) b f", a=2, b=2)
                x1, x2 = qv[:, :, 0, :], qv[:, :, 1, :]
                cb = cs[:, 0, :].rearrange("p (g f) -> p g f", f=16)
                sb_ = cs[:, 1, :].rearrange("p (g f) -> p g f", f=16)
                tmp, tmpk = tmp_r.next()
                qkr = [qkk + "/a", qkk + "/b"]
                P.op("dve", lambda e, rp=rp, x1=x1, cb=cb: e.tensor_tensor(out=rp[:, :, 0, :], in0=x1, in1=cb, op=ALU.mult), reads=qkr + [csk + "/c"], writes=[rpk + "/0"])
                P.op("pool", lambda e, tmp=tmp, x2=x2, sb_=sb_: e.tensor_tensor(out=tmp[:], in0=x2, in1=sb_, op=ALU.mult), reads=qkr + [csk + "/s"], writes=[tmpk])
                P.op("dve", lambda e, rp=rp, tmp=tmp: e.tensor_tensor(out=rp[:, :, 0, :], in0=rp[:, :, 0, :], in1=tmp[:], op=ALU.subtract), reads=[rpk + "/0", tmpk], writes=[rpk + "/0"])
                P.op("dve", lambda e, rp=rp, x2=x2, cb=cb: e.tensor_tensor(out=rp[:, :, 1, :], in0=x2, in1=cb, op=ALU.mult), reads=qkr + [csk + "/c"], writes=[rpk + "/1"])
                P.op("pool", lambda e, tmp=tmp, x1=x1, sb_=sb_: e.tensor_tensor(out=tmp[:], in0=x1, in1=sb_, op=ALU.mult), reads=qkr + [csk + "/s"], writes=[tmpk])
                P.op("dve", lambda e, rp=rp, tmp=tmp: e.tensor_tensor(out=rp[:, :, 1, :], in0=rp[:, :, 1, :], in1=tmp[:], op=ALU.add), reads=[rpk + "/1", tmpk], writes=[rpk + "/1"])
            rkeys = [rpk] if is_ctx else [rpk + "/0", rpk + "/1"]
            if stop == 6:
                continue
            rpf = rp[:].rearrange("p g b f -> p (g b f)")
            tq, tqk = tq_r.next()
            for j in range(6):
                P.op("pe", lambda e, tq=tq, rpf=rpf, j=j: e.transpose(tq[:, j * 128:(j + 1) * 128], rpf[:, j * 128:(j + 1) * 128], ident[:]),
                     reads=rkeys + [pf + "ident"], writes=[tqk], noinc=(j != 5))
            qT, qTk = qT_r.next()
            P.op("act", lambda e, qT=qT, tq=tq: e.activation(out=qT[:], in_=tq[:, 0:768].rearrange("p (j t) -> p j t", j=6), func=AF.Copy), reads=[tqk], writes=[qTk])
            P.dma(lambda e, qT=qT, t0=t0: e.dma_start(out=QKT[:, t0:t0 + 128].rearrange("(j p) t -> p j t", p=128), in_=qT[:]), reads=[qTk], writes=[P.u("QKT")])
        if stop <= 7:
            continue
        hkeys = [hTk + "/%d" % i for i in range(ntile)]
        for j in range(8):
            fm, fmk = fm_r.next()
            for k in range(8):
                P.op("pe", lambda e, fm=fm, hT=hT, k=k, j=j, cn=cn: e.matmul(fm[:, 0:cn], win[:, k, FM_OFF + j * 128:FM_OFF + (j + 1) * 128], hT[:, k, 0:cn], start=(k == 0), stop=(k == 7)),
                     reads=hkeys + [wkeys[k]], writes=[fmk], noinc=(k != 7))
            fo, fok = fo_r.next()
            eng = "act" if j % 2 == 0 else "dve"
            if eng == "act":
                P.op("act", lambda e, fo=fo, fm=fm, cn=cn: e.activation(out=fo[:, 0:cn], in_=fm[:, 0:cn], func=AF.Copy), reads=[fmk], writes=[fok])
            else:
                P.op("dve", lambda e, fo=fo, fm=fm, cn=cn: e.tensor_copy(out=fo[:, 0:cn], in_=fm[:, 0:cn]), reads=[fmk], writes=[fok])
            P.dma(lambda e, fo=fo, j=j, c0=c0, cn=cn: e.dma_start(out=FMT[j * 128:(j + 1) * 128, c0:c0 + cn], in_=fo[:, 0:cn]), reads=[fok], writes=[P.u("FMT")])


def phase_g2(P, Dm, l, tok_chunks, final):
    pf = "G%d_" % l
    ntf = sum(cn for _, cn in tok_chunks) // 128
    NB = 2 * ntf + 32
    X1 = Dm.get("X1", [T, D])
    BUF = Dm.get("BUF%d" % l, [NB * 128, D])
    OBUF = Dm.get("OBUF%d" % l, [NB * 128, D])
    IDXW = Dm.get("IDXW%d" % l, [128, NB], I32)
    DEST = Dm.get("DEST%d" % l, [128, ntf * 2], I32)
    WWd = Dm.get("WWd%d" % l, [128, ntf * 2])
    WGU = Dm.get("moe_gu%d" % l, [32, 128, 8, 1024], kind="ExternalInput")
    WD = Dm.get("moe_d%d" % l, [32, 128, 4, 1024], kind="ExternalInput")
    ident_d = Dm.get("ident", [128, 128], kind="ExternalInput")
    modrow = Dm.get("modrow%d" % l, [2, 6144])
    mk = "modrow%d" % l
    ident = P.sb(pf + "ident", [128, 128]); P.d(ident[:], ident_d[:, :], writes=[pf + "ident"])
    idxw = P.sb(pf + "idxw", [128, NB], I32); P.d(idxw[:], IDXW[:, :], reads=["IDXW%d" % l], writes=[pf + "idxw"])
    dest = P.sb(pf + "dest", [128, ntf * 2], I32); P.d(dest[:], DEST[:, :], reads=["DEST%d" % l], writes=[pf + "dest"])
    ww = P.sb(pf + "ww", [128, ntf * 2]); P.d(ww[:], WWd[:, :], reads=["WWd%d" % l], writes=[pf + "ww"])
    wgu_r = Rot(P, pf + "wgu", [128, 8, 1024], F32R, n=2)
    wd_r = Rot(P, pf + "wd", [128, 4, 1024], F32R, n=2)
    xs_r = Rot(P, pf + "xs", [128, D], n=3)
    tp_r = Rot(P, pf + "tp", [128, 1024], n=1, psum=True)
    xT_r = Rot(P, pf + "xT", [128, 8, 128], F32R, n=2)
    pg_r = Rot(P, pf + "pg", [128, 512], n=1, psum=True)
    pu_r = Rot(P, pf + "pu", [128, 512], n=1, psum=True)
    sg_r = Rot(P, pf + "sg", [128, 512], n=2)
    hu_r = Rot(P, pf + "hu", [128, 512], n=2)
    ph_r = Rot(P, pf + "ph", [128, 512], n=1, psum=True)
    hT_r = Rot(P, pf + "hT", [128, 4, 128], F32R, n=2)
    po_r = Rot(P, pf + "po", [128, 1024], n=1, psum=True)
    ob_r = Rot(P, pf + "ob", [128, D], n=2)
    WGUf = WGU.rearrange("e p k n -> (e p) (k n)")
    WDf = WD.rearrange("e p k n -> (e p) (k n)")
    st1 = {}
    breg = P.bound()
    hNB = NB // 2
    seq = []
    for i_ in range(hNB):
        seq += [i_, hNB + i_]

    def load_gu(b):
        off = bass.IndirectOffsetOnAxis(ap=idxw[:, seq[b]:seq[b] + 1], axis=0)
        wgu, wguk = wgu_r.next()
        P.dma(lambda e, wgu=wgu, off=off: e.indirect_dma_start(out=wgu[:].rearrange("p k n -> p (k n)"), out_offset=None, in_=WGUf, in_offset=off, bounds_check=breg, oob_is_err=False), reads=[pf + "idxw"], writes=[wguk], q="pool")
        st1[("gu", b)] = (wgu, wguk)

    def load_d(b):
        off = bass.IndirectOffsetOnAxis(ap=idxw[:, seq[b]:seq[b] + 1], axis=0)
        wd, wdk = wd_r.next()
        P.dma(lambda e, wd=wd, off=off: e.indirect_dma_start(out=wd[:].rearrange("p k n -> p (k n)"), out_offset=None, in_=WDf, in_offset=off, bounds_check=breg, oob_is_err=False), reads=[pf + "idxw"], writes=[wdk], q="pool")
        st1[("d", b)] = (wd, wdk)

    def s1a(b):
        xs, xsk = xs_r.next()
        P.d(xs[:], BUF[seq[b] * 128:(seq[b] + 1) * 128, :], reads=["BUF%d" % l, "BUFz%d" % l], writes=[xsk])
        tp, tpk = tp_r.next()
        for k in range(8):
            P.i("pe", "transpose", reads=[xsk, pf + "ident"], writes=[tpk], noinc=(k != 7), out=tp[:, k * 128:(k + 1) * 128], in_=xs[:, k * 128:(k + 1) * 128], identity=ident[:])
        xT, xTk = xT_r.next()
        P.i("act", "activation", reads=[tpk], writes=[xTk + "/0"], out=xT[:, 0:4, :], in_=tp[:, 0:512].rearrange("p (k t) -> p k t", k=4), func=AF.Copy)
        P.i("dve", "tensor_copy", reads=[tpk], writes=[xTk + "/1"], out=xT[:, 4:8, :], in_=tp[:, 512:1024].rearrange("p (k t) -> p k t", k=4))
        st1[("xT", b)] = (xT, xTk)

    def s1b(b):
        (wgu, wguk) = st1.pop(("gu", b))
        (xT, xTk) = st1.pop(("xT", b))
        pg, pgk = pg_r.next(); pu, puk = pu_r.next()
        for k in range(8):
            P.i("pe", "matmul", reads=[xTk, wguk], writes=[pgk], out=pg[:], lhsT=xT[:, k, :], rhs=wgu[:, k, 0:512], start=(k == 0), stop=(k == 7))
        for k in range(8):
            P.i("pe", "matmul", reads=[xTk, wguk], writes=[puk], out=pu[:], lhsT=xT[:, k, :], rhs=wgu[:, k, 512:1024], start=(k == 0), stop=(k == 7))
        sg, sgk = sg_r.next()
        P.i("act", "activation", reads=[pgk], writes=[sgk], out=sg[:], in_=pg[:], func=AF.Silu)
        hu, huk = hu_r.next()
        P.i("dve", "tensor_tensor", reads=[sgk, puk], writes=[huk], out=hu[:], in0=sg[:], in1=pu[:], op=ALU.mult)
        st1[("h", b)] = (hu, huk)

    def stage2(b):
        (hu, huk) = st1.pop(("h", b))
        (wd, wdk) = st1.pop(("d", b))
        ph, phk = ph_r.next()
        for hc in range(4):
            P.i("pe", "transpose", reads=[huk, pf + "ident"], writes=[phk], noinc=(hc != 3), out=ph[:, hc * 128:(hc + 1) * 128], in_=hu[:, hc * 128:(hc + 1) * 128], identity=ident[:])
        hT, hTk = hT_r.next()
        P.i("act", "activation", reads=[phk], writes=[hTk], out=hT[:], in_=ph[:].rearrange("p (k t) -> p k t", k=4), func=AF.Copy)
        po, pok = po_r.next()
        for hf in range(2):
            for hc in range(4):
                P.i("pe", "matmul", reads=[hTk, wdk], writes=[pok], out=po[:, hf * 512:(hf + 1) * 512], lhsT=hT[:, hc, :], rhs=wd[:, hc, hf * 512:(hf + 1) * 512], start=(hc == 0), stop=(hc == 3))
        ob, obk = ob_r.next()
        P.i("act", "activation", reads=[pok], writes=[obk + "/0"], out=ob[:, 0:512], in_=po[:, 0:512], func=AF.Copy)
        P.i("dve", "tensor_copy", reads=[pok], writes=[obk + "/1"], out=ob[:, 512:1024], in_=po[:, 512:1024])
        P.d(OBUF[seq[b] * 128:(seq[b] + 1) * 128, :], ob[:], reads=[obk], writes=[P.u("OBUF%d" % l)])

    load_gu(0); load_d(0); load_gu(1); load_d(1)
    s1a(0); s1b(0)
    if NB > 2:
        load_gu(2)
    s1a(1)
    for b in range(NB):
        stage2(b)
        if b + 2 < NB:
            load_d(b + 2)
        if b + 1 < NB:
            s1b(b + 1)
            if b + 3 < NB:
                load_gu(b + 3)
        if b + 2 < NB:
            s1a(b + 2)
    if final:
        fg_d = Dm.get("final_g", [1, D], kind="ExternalInput")
        OUT = Dm.get("out", [L, D], kind="ExternalOutput")
        fg = P.sb(pf + "fg", [128, D]); load_bcast(P, fg[:], pf + "fg", fg_d[0:1, :])
        epsc = P.sb(pf + "eps", [128, 1]); P.i("dve", "memset", writes=[pf + "eps"], ap=epsc[:], constant=EPS)
        junk = P.sb(pf + "junk", [128, D])
    else:
        XN = Dm.get("XN%d" % l, [T, D])
    G2g = [P.sb(pf + "g2%d" % r, [128, D]) for r in range(2)]
    for r in range(2):
        load_bcast(P, G2g[r][:], pf + "g2%d" % r, modrow[r:r + 1, 5120:6144], reads=[mk])
    o1_r = Rot(P, pf + "o1", [128, D], n=2)
    o2_r = Rot(P, pf + "o2", [128, D], n=2)
    xt_r = Rot(P, pf + "xt", [128, D], n=2)
    st_r = Rot(P, pf + "st", [128, 8], n=2)
    ti = 0
    for (c0, cn) in tok_chunks:
        for i in range(cn // 128):
            t0 = c0 + i * 128
            r = 1 if t0 < LC else 0
            o1, o1k = o1_r.next(); o2, o2k = o2_r.next()
            for (o, ok_, col) in ((o1, o1k, ti * 2), (o2, o2k, ti * 2 + 1)):
                P.dma(lambda e, o=o, col=col: e.indirect_dma_start(out=o[:], out_offset=None, in_=OBUF[:, :], in_offset=bass.IndirectOffsetOnAxis(ap=dest[:, col:col + 1], axis=0)),
                      reads=[pf + "dest", "OBUF%d" % l], writes=[ok_], q="pool")
            xt, xk = xt_r.next()
            P.d(xt[:], X1[t0:t0 + 128, :], reads=["X1"], writes=[xk])
            P.i("dve", "tensor_scalar", reads=[o1k, pf + "ww"], writes=[o1k], out=o1[:], in0=o1[:], scalar1=ww[:, ti * 2:ti * 2 + 1], scalar2=None, op0=ALU.mult)
            P.i("dve", "scalar_tensor_tensor", reads=[o1k, o2k, pf + "ww"], writes=[o1k], out=o1[:], in0=o2[:], scalar=ww[:, ti * 2 + 1:ti * 2 + 2], in1=o1[:], op0=ALU.mult, op1=ALU.add)
            P.i("pool", "tensor_tensor", reads=[o1k, pf + "g2%d" % r], writes=[o1k], out=o1[:], in0=o1[:], in1=G2g[r][:], op=ALU.mult)
            P.i("dve", "tensor_tensor", reads=[o1k, xk], writes=[xk], out=xt[:], in0=xt[:], in1=o1[:], op=ALU.add)
            if not final:
                P.d(XN[t0:t0 + 128, :], xt[:], reads=[xk], writes=[P.u("XN%d" % l)])
            else:
                st, sk = st_r.next()
                P.i("act", "activation", reads=[xk], writes=[pf + "junk", sk], out=junk[:], in_=xt[:], func=AF.Square, accum_out=st[:, 0:1])
                P.i("act", "activation", reads=[sk, pf + "eps"], writes=[sk], out=st[:, 1:2], in_=st[:, 0:1], func=AF.Ln, bias=epsc[:], scale=1.0 / D)
                P.i("act", "activation", reads=[sk], writes=[sk], out=st[:, 2:3], in_=st[:, 1:2], func=AF.Exp, scale=-0.5)
                P.i("dve", "scalar_tensor_tensor", reads=[xk, sk, pf + "fg"], writes=[xk], out=xt[:], in0=xt[:], scalar=st[:, 2:3], in1=fg[:], op0=ALU.mult, op1=ALU.mult)
                P.d(OUT[t0 - LC:t0 - LC + 128, :], xt[:], reads=[xk], writes=[P.u("out")], final=True)
            ti += 1


def rope_tables_np():
    rows = np.repeat(np.arange(L // 64), 64)
    cols = np.tile(np.arange(64), L // 64)
    inv = np.power(np.float32(10000.0), -np.arange(16, dtype=np.float32) / np.float32(16)).astype(np.float32)
    ang = np.stack([rows, cols], -1).astype(np.float32)[..., None] * inv
    cos = np.cos(ang).astype(np.float32).reshape(L, 1, 32)
    sin = np.sin(ang).astype(np.float32).reshape(L, 1, 32)
    return (np.ascontiguousarray(np.broadcast_to(cos, (L, 12, 32)).reshape(L, 384)),
            np.ascontiguousarray(np.broadcast_to(sin, (L, 12, 32)).reshape(L, 384)))


def kmajor(w):
    K, N = w.shape
    return np.ascontiguousarray(w.reshape(K // 128, 128, N).transpose(1, 0, 2))


def core_inputs(inp, b):
    f = np.float32
    m = {}
    m["xin"] = np.ascontiguousarray(np.concatenate([inp["ctx"][b], inp["x"][b]], 0).astype(f))
    cv = np.stack([inp["c"][b], inp["c_ctx"]], -1)
    m["cvec"] = np.ascontiguousarray(cv.reshape(8, 128, 2).transpose(1, 0, 2).astype(f))
    m["ident"] = np.eye(128, dtype=f)
    sh = np.zeros((128, 64), f); sh[64 + np.arange(64), np.arange(64)] = 1.0
    m["shiftm"] = sh
    jj, ii = np.meshgrid(np.arange(128), np.arange(128), indexing="ij")
    wm = np.zeros((128, 2, 2, 128), f)
    wm[:, 0] = (ii <= jj).astype(f)[:, None, :]
    wm[:, 1] = (jj <= ii).astype(f)[:, None, :]
    m["wmask"] = np.ascontiguousarray(wm.reshape(128, 2, 256))
    m["rope_cos"], m["rope_sin"] = rope_tables_np()
    for l in range(2):
        m["ada_w%d" % l] = kmajor(inp["ada_w"][l])
        m["ada_b%d" % l] = np.ascontiguousarray(inp["ada_b"][l].reshape(1, 6144))
        m["w_in%d" % l] = kmajor(inp["w_in"][l][:, PERM])
        m["norm1_g%d" % l] = np.ascontiguousarray(inp["norm1_g"][l].reshape(1, D))
        m["qkg%d" % l] = np.ascontiguousarray(np.concatenate([np.tile(inp["ga_qn_g"][l], 4), np.tile(inp["ga_kn_g"][l], 2)]).reshape(1, 384))
        m["dtb%d" % l] = np.ascontiguousarray(inp["ssd_dt_bias"][l].reshape(1, 8))
        m["sink%d" % l] = np.ascontiguousarray(inp["wa_sink"][l].reshape(1, 4))
        m["w_out%d" % l] = kmajor(inp["w_out"][l])
        m["norm2_g%d" % l] = np.ascontiguousarray(inp["norm2_g"][l].reshape(1, D))
        m["wr%d" % l] = kmajor(np.concatenate([inp["moe_coarse_w"][l], inp["moe_fine_w"][l]], 1))
        m["rb%d" % l] = np.ascontiguousarray(np.concatenate([inp["moe_coarse_b"][l], inp["moe_fine_b"][l]]).reshape(1, 36))
        m["moe_g%d" % l] = np.ascontiguousarray(inp["moe_w_gate"][l].reshape(32, 8, 128, 512).transpose(0, 2, 1, 3))
        m["moe_u%d" % l] = np.ascontiguousarray(inp["moe_w_up"][l].reshape(32, 8, 128, 512).transpose(0, 2, 1, 3))
        m["moe_gu%d" % l] = np.ascontiguousarray(np.concatenate([m["moe_g%d" % l], m["moe_u%d" % l]], -1))
        m["moe_d%d" % l] = np.ascontiguousarray(inp["moe_w_down"][l].reshape(32, 4, 128, 1024).transpose(0, 2, 1, 3))
    m["final_g"] = np.ascontiguousarray(inp["final_g"].reshape(1, D))
    m["ramp"] = (128.0 * np.arange(72) + 1.0).astype(f).reshape(1, 72)
    m["bst"] = (128.0 * np.arange(104)).astype(f).reshape(1, 104)
    m["pidx"] = np.arange(128).astype(f).reshape(128, 1)
    sp, s_ = np.meshgrid(np.arange(128), np.arange(128), indexing="ij")
    m["stri"] = np.ascontiguousarray(np.stack([(sp < s_), np.ones_like(sp, dtype=bool)], 1).astype(f))
    m["tri"] = np.ascontiguousarray(np.stack([(sp <= s_), (sp >= s_)], 1).astype(f))
    for l in range(2):
        Bm = np.zeros((2, 2, 8, 128, 128), f)
        Cm = np.zeros((2, 2, 8, 128, 128), f)
        for d in range(2):
            for j in range(8):
                for gg in range(2):
                    g = 2 * j + gg
                    r0 = (2 * (j % 4) + gg) * 16
                    Bm[d, 0, j, r0:r0 + 16, gg * 64:(gg + 1) * 64] = inp["s5_b_re"][l, d, g].T
                    Bm[d, 1, j, r0:r0 + 16, gg * 64:(gg + 1) * 64] = inp["s5_b_im"][l, d, g].T
                    Cm[d, 0, j, gg * 64:(gg + 1) * 64, r0:r0 + 16] = inp["s5_c_re"][l, d, g].T
                    Cm[d, 1, j, gg * 64:(gg + 1) * 64, r0:r0 + 16] = inp["s5_c_im"][l, d, g].T
        m["s5B%d" % l] = Bm
        m["s5C%d" % l] = Cm
        lamt = np.zeros((128, 3, 16), f)
        for d in range(2):
            for j in range(8):
                for gg in range(2):
                    g = 2 * j + gg
                    lamt[gg * 64:(gg + 1) * 64, 0, d * 8 + j] = inp["s5_lam_re"][l, d, g]
                    lamt[gg * 64:(gg + 1) * 64, 1, d * 8 + j] = inp["s5_lam_im"][l, d, g]
                    lamt[gg * 64:(gg + 1) * 64, 2, d * 8 + j] = inp["s5_log_dt"][l, d, g]
        m["s5lam%d" % l] = lamt
        cw = np.concatenate([inp["ssd_conv_w"][l], inp["ssd_conv_b"][l][None]], 0)
        m["convw%d" % l] = np.ascontiguousarray(cw.reshape(4, 6, 128).transpose(2, 1, 0).astype(f))
        m["alog%d" % l] = np.ascontiguousarray(inp["ssd_a_log"][l].reshape(1, 8))
        m["ssdd%d" % l] = np.ascontiguousarray(inp["ssd_d"][l].reshape(1, 4))
        m["ssdng%d" % l] = np.ascontiguousarray(inp["ssd_norm_g"][l].reshape(1, 256))
        m["s5d%d" % l] = np.ascontiguousarray(inp["s5_d"][l].reshape(2, 128).T.astype(f))
        m["gluw%d" % l] = np.ascontiguousarray(inp["s5_glu_w"][l].reshape(2, 128, 256).transpose(1, 0, 2).astype(f))
        m["glub%d" % l] = np.ascontiguousarray(inp["s5_glu_b"][l].reshape(2, 128).T.astype(f))
    for l in range(0):
        pass
    return m


def attn_consts(P, Dm, pf):
    c = {}
    shift_d = Dm.get("shiftm", [128, 64], kind="ExternalInput")
    c["shift"] = P.sb(pf + "shift", [128, 64], F32R)
    P.d(c["shift"][:], shift_d[:, :], writes=[pf + "shift"], q="pool")
    return c


def phase_c(P, Dm, l, ctx_out):
    pf = "c%d_" % l
    QKT = Dm.get("QKT", [768, T])
    TMS = Dm.get("TMS", [T, 520])
    CAT = Dm.get("CAT", [1024, T])
    cst = attn_consts(P, Dm, pf)
    KT = P.sb(pf + "KT", [64, 2, T], F32R)
    V = P.sb(pf + "V", [128, NT, 2, 128], F32R)
    for kv in range(2):
        P.d(KT[:, kv, :], QKT[256 + kv * 64:256 + (kv + 1) * 64, :], reads=["QKT"], writes=[pf + "KT"], q="pool")
    P.i("dve", "memset", writes=[pf + "V/1"], ap=V[:].bitcast(F32)[:, :, :, 64:128], constant=1.0)
    for kv in range(2):
        P.d(V[:, :, kv, 0:64], TMS[:, kv * 64:(kv + 1) * 64].rearrange("(n p) d -> p n d", p=128), reads=["TMS"], writes=[pf + "V/0%d" % kv], q="pool")
    vkeys = [pf + "V"]
    q_r = Rot(P, pf + "q", [64, 4, 256], F32R, n=2)
    s_r = Rot(P, pf + "s", [128, 1024], n=2, psum=True)
    p_r = Rot(P, pf + "p", [128, 1024], F32R, n=3)
    o_r = Rot(P, pf + "o", [128, 512], n=2, psum=True)
    os_r = Rot(P, pf + "os", [128, 512], F32R, n=2)
    dn_r = Rot(P, pf + "dn", [64, 512], n=1, psum=True)
    rd_r = Rot(P, pf + "rd", [64, 512], n=2)
    ot_r = Rot(P, pf + "ot", [64, 512], n=2)
    qtiles = [(LC + 256 * i, NT) for i in range(L // 256)]
    if ctx_out:
        qtiles = [(0, 2)] + qtiles
    for (q0, nkb) in qtiles:
        qt, qtk = q_r.next()
        P.d(qt[:], QKT[0:256, q0:q0 + 256].rearrange("(h d) q -> d h q", d=64), reads=["QKT"], writes=[qtk], q="pool")
        for kv in range(2):
            o, ok = o_r.next()
            its = list(range(nkb // 2))
            pend = []

            def issue_s(sp):
                sp_, spk = s_r.next()
                for u in range(2):
                    s = 2 * sp + u
                    P.i("pe", "matmul", reads=[pf + "KT", qtk], writes=[spk], out=sp_[:, u * 512:(u + 1) * 512], lhsT=KT[:, kv, s * 128:(s + 1) * 128],
                        rhs=qt[:, 2 * kv:2 * kv + 2, :], start=True, stop=True)
                pt, ptk = p_r.next()
                P.i("act", "activation", reads=[spk], writes=[ptk], out=pt[:], in_=sp_[:], func=AF.Exp, scale=0.125)
                return (sp, pt, ptk)

            LOOK = 1
            for sp in its[:LOOK]:
                pend.append(issue_s(sp))
            for idx, sp in enumerate(its):
                (sp_i, pt, ptk) = pend.pop(0)
                if idx + LOOK < len(its):
                    pend.append(issue_s(its[idx + LOOK]))
                for u in range(2):
                    s_ = 2 * sp_i + u
                    P.i("pe", "matmul", reads=[ptk] + vkeys, writes=[ok], out=o[:], lhsT=V[:, s_, kv, :], rhs=pt[:, u * 512:(u + 1) * 512],
                        start=(idx == 0 and u == 0), stop=(idx == len(its) - 1 and u == 1))
            osb, osk = os_r.next()
            P.i("act", "activation", reads=[ok], writes=[osk], out=osb[:], in_=o[:], func=AF.Copy)
            dn, dnk = dn_r.next()
            P.i("pe", "matmul", reads=[osk, pf + "shift"], writes=[dnk], out=dn[:], lhsT=cst["shift"][:], rhs=osb[:], start=True, stop=True)
            rd, rdk = rd_r.next()
            P.i("dve", "reciprocal", reads=[dnk], writes=[rdk], out=rd[:], in_=dn[:])
            ot, otk = ot_r.next()
            P.i("dve", "tensor_tensor", reads=[osk, rdk], writes=[otk], out=ot[:], in0=osb[0:64, :].bitcast(F32), in1=rd[:], op=ALU.mult)
            P.d(CAT[256 + kv * 128:256 + (kv + 1) * 128, q0:q0 + 256].rearrange("(hh d) q -> d hh q", d=64),
                ot[:].rearrange("d (hh q) -> d hh q", hh=2), reads=[otk], writes=[P.u("CAT")])


def phase_e(P, Dm, l, ctx_out):
    pf = "e%d_" % l
    QKT = Dm.get("QKT", [768, T])
    TMS = Dm.get("TMS", [T, 520])
    CAT = Dm.get("CAT", [1024, T])
    sink_d = Dm.get("sink%d" % l, [1, 4], kind="ExternalInput")
    mask_d = Dm.get("wmask", [128, 2, 256], kind="ExternalInput")
    cst = attn_consts(P, Dm, pf)
    KT = P.sb(pf + "KT", [64, 2, T], F32R)
    V = P.sb(pf + "V", [128, NT, 2, 128], F32R)
    for kv in range(2):
        P.d(KT[:, kv, :], QKT[640 + kv * 64:640 + (kv + 1) * 64, :], reads=["QKT"], writes=[pf + "KT"], q="pool")
    P.i("dve", "memset", writes=[pf + "V/1"], ap=V[:].bitcast(F32)[:, :, :, 64:128], constant=1.0)
    for kv in range(2):
        P.d(V[:, :, kv, 0:64], TMS[:, 128 + kv * 64:128 + (kv + 1) * 64].rearrange("(n p) d -> p n d", p=128), reads=["TMS"], writes=[pf + "V/0%d" % kv], q="pool")
    vkeys = [pf + "V"]
    mask = P.sb(pf + "mask", [128, 2, 256])
    P.d(mask[:], mask_d[:, :, :], writes=[pf + "mask"])
    esk = P.sb(pf + "esk", [64, 4])
    P.d(esk[:], sink_d[0:1, :].to_broadcast([64, 4]), writes=[pf + "esk"])
    P.i("act", "activation", reads=[pf + "esk"], writes=[pf + "esk"], out=esk[:], in_=esk[:], func=AF.Exp)
    q_r = Rot(P, pf + "q", [64, 4, 128], F32R, n=2)
    s_r = Rot(P, pf + "s", [128, 256], n=3, psum=True)
    p_r = Rot(P, pf + "p", [128, 256], F32R, n=3)
    o_r = Rot(P, pf + "o", [128, 256], n=2, psum=True)
    os_r = Rot(P, pf + "os", [128, 256], F32R, n=2)
    dn_r = Rot(P, pf + "dn", [64, 256], n=1, psum=True)
    rd_r = Rot(P, pf + "rd", [64, 256], n=2)
    ot_r = Rot(P, pf + "ot", [64, 256], n=2)
    qtiles = []
    if ctx_out:
        qtiles += [(i, [(0, None), (1, None)]) for i in range(2)]
    for n in range(L // 128):
        ti = 2 + n
        kb = [(0, None), (1, None)]
        if n > 0:
            kb.append((ti - 1, 0))
        kb.append((ti, None))
        if n < L // 128 - 1:
            kb.append((ti + 1, 1))
        qtiles.append((ti, kb))
    for (ti, kbs) in qtiles:
        q0 = ti * 128
        qt, qtk = q_r.next()
        P.d(qt[:], QKT[384:640, q0:q0 + 128].rearrange("(h d) q -> d h q", d=64), reads=["QKT"], writes=[qtk], q="pool")
        for kv in range(2):
            o, ok = o_r.next()
            for idx, (s, mi) in enumerate(kbs):
                sp_, spk = s_r.next()
                P.i("pe", "matmul", reads=[pf + "KT", qtk], writes=[spk], out=sp_[:], lhsT=KT[:, kv, s * 128:(s + 1) * 128],
                    rhs=qt[:, 2 * kv:2 * kv + 2, :], start=True, stop=True)
                pt, ptk = p_r.next()
                P.i("act", "activation", reads=[spk], writes=[ptk], out=pt[:], in_=sp_[:], func=AF.Exp, scale=0.125)
                if mi is not None:
                    P.i("dve", "tensor_tensor", reads=[ptk, pf + "mask"], writes=[ptk], out=pt[:], in0=pt[:].bitcast(F32), in1=mask[:, mi, :], op=ALU.mult)
                P.i("pe", "matmul", reads=[ptk] + vkeys, writes=[ok], out=o[:], lhsT=V[:, s, kv, :], rhs=pt[:],
                    start=(idx == 0), stop=(idx == len(kbs) - 1))
            osb, osk = os_r.next()
            P.i("act", "activation", reads=[ok], writes=[osk], out=osb[:], in_=o[:], func=AF.Copy)
            dn, dnk = dn_r.next()
            P.i("pe", "matmul", reads=[osk, pf + "shift"], writes=[dnk], out=dn[:], lhsT=cst["shift"][:], rhs=osb[:], start=True, stop=True)
            rd, rdk = rd_r.next()
            for hh in range(2):
                h = 2 * kv + hh
                P.i("dve", "tensor_scalar", reads=[dnk, pf + "esk"], writes=[rdk + "/%d" % hh], out=rd[:, hh * 128:(hh + 1) * 128], in0=dn[:, hh * 128:(hh + 1) * 128],
                    scalar1=esk[:, h:h + 1], scalar2=None, op0=ALU.add)
            P.i("dve", "reciprocal", reads=[rdk], writes=[rdk], out=rd[:], in_=rd[:])
            ot, otk = ot_r.next()
            P.i("dve", "tensor_tensor", reads=[osk, rdk], writes=[otk], out=ot[:], in0=osb[0:64, :].bitcast(F32), in1=rd[:], op=ALU.mult)
            P.d(CAT[768 + kv * 128:768 + (kv + 1) * 128, q0:q0 + 128].rearrange("(hh d) q -> d hh q", d=64),
                ot[:].rearrange("d (hh q) -> d hh q", hh=2), reads=[otk], writes=[P.u("CAT")])


BIG = 1.0e30
S5_ENG2 = "dve"


def phase_f(P, Dm, l, src_name, tok_chunks=None):
    pf = "f%d_" % l
    xsrc = Dm.get(src_name, [T, D])
    CAT = Dm.get("CAT", [1024, T])
    w_out = Dm.get("w_out%d" % l, [128, 8, 1024], kind="ExternalInput")
    modrow = Dm.get("modrow%d" % l, [2, 6144])
    n2g = Dm.get("norm2_g%d" % l, [1, D], kind="ExternalInput")
    wr_d = Dm.get("wr%d" % l, [128, 8, 36], kind="ExternalInput")
    rb_d = Dm.get("rb%d" % l, [1, 36], kind="ExternalInput")
    ident_d = Dm.get("ident", [128, 128], kind="ExternalInput")
    X1 = Dm.get("X1", [T, D])
    H2T = Dm.get("H2T", [D, T])
    WTd = Dm.get("WTd", [32, T])
    mk = "modrow%d" % l
    ident = P.sb(pf + "ident", [128, 128])
    P.d(ident[:], ident_d[:, :], writes=[pf + "ident"])
    wo = P.sb(pf + "wo", [128, 8, 1024], F32R)
    for k in range(8):
        P.d(wo[:, k, :], w_out[:, k, :], writes=[pf + "wo/%d" % k], q="pool")
    wr = P.sb(pf + "wr", [128, 8, 36])
    P.d(wr[:], wr_d[:, :, :], writes=[pf + "wr"])
    rb = P.sb(pf + "rb", [128, 36])
    load_bcast(P, rb[:], pf + "rb", rb_d[0:1, :])
    G1 = [P.sb(pf + "G1%d" % r, [128, D]) for r in range(2)]
    G2 = [P.sb(pf + "G2%d" % r, [128, D]) for r in range(2)]
    SH2 = [P.sb(pf + "SH2%d" % r, [128, D]) for r in range(2)]
    gn = P.sb(pf + "gn", [128, D])
    load_bcast(P, gn[:], pf + "gn", n2g[0:1, :])
    for r in range(2):
        load_bcast(P, G1[r][:], pf + "G1%d" % r, modrow[r:r + 1, 2048:3072], reads=[mk])
        load_bcast(P, SH2[r][:], pf + "SH2%d" % r, modrow[r:r + 1, 3072:4096], reads=[mk])
        load_bcast(P, G2[r][:], pf + "G2%d" % r, modrow[r:r + 1, 4096:5120], reads=[mk])
        P.i("dve", "scalar_tensor_tensor", reads=[pf + "G2%d" % r, pf + "gn"], writes=[pf + "G2%d" % r], out=G2[r][:], in0=G2[r][:], scalar=1.0, in1=gn[:], op0=ALU.add, op1=ALU.mult)
    epsc = P.sb(pf + "eps", [128, 1])
    P.i("dve", "memset", writes=[pf + "eps"], ap=epsc[:], constant=EPS)
    ct_r = Rot(P, pf + "ct", [128, 8, 512], F32R, n=2)
    po_r = Rot(P, pf + "po", [128, 1024], n=1, psum=True)
    xt_r = Rot(P, pf + "xt", [128, D], n=2)
    x1_r = Rot(P, pf + "x1", [128, D], n=2)
    h_r = Rot(P, pf + "h", [128, D], n=2)
    junk = P.sb(pf + "junk", [128, D])
    st_r = Rot(P, pf + "st", [128, 64], n=3)
    tp_r = Rot(P, pf + "tp", [128, 1024], n=1, psum=True)
    hT_r = Rot(P, pf + "hT", [128, 8, 128], F32R, n=2)
    hTf_r = Rot(P, pf + "hTf", [128, 8, 128], n=2)
    pr_r = Rot(P, pf + "pr", [128, 512], n=1, psum=True)
    lg_r = Rot(P, pf + "lg", [128, 36], n=2)
    mk_r = Rot(P, pf + "mk", [128, 4, 8], n=2)
    oh_r = Rot(P, pf + "oh", [128, 3, 32], n=2)
    wt_r = Rot(P, pf + "wt", [128, 32], n=2)
    pw_r = Rot(P, pf + "pw", [128, 512], n=1, psum=True)
    wT_r = Rot(P, pf + "wT", [32, 128], n=2)
    for (c0, cn) in (tok_chunks or CHUNKS):
        ct, ctk = ct_r.next()
        P.d(ct[:, :, 0:cn], CAT[:, c0:c0 + cn].rearrange("(k p) t -> p k t", p=128), reads=["CAT"], writes=[ctk], q="pool")
        for i in range(cn // 128):
            t0 = c0 + i * 128
            r = 1 if t0 < LC else 0
            po, pok = po_r.next()
            for hf in range(2):
                for k in range(8):
                    P.i("pe", "matmul", reads=[ctk, pf + "wo/%d" % k], writes=[pok], out=po[:, hf * 512:(hf + 1) * 512], lhsT=ct[:, k, i * 128:(i + 1) * 128],
                        rhs=wo[:, k, hf * 512:(hf + 1) * 512], start=(k == 0), stop=(k == 7))
            xt, xk = xt_r.next()
            P.d(xt[:], xsrc[t0:t0 + 128, :], reads=[src_name], writes=[xk])
            x1, x1k = x1_r.next()
            for hf in range(2):
                sl = slice(hf * 512, (hf + 1) * 512)
                P.i("dve", "tensor_tensor", reads=[pok, pf + "G1%d" % r], writes=[x1k + "/%d" % hf], out=x1[:, sl], in0=po[:, sl], in1=G1[r][:, sl], op=ALU.mult)
            P.i("pool", "tensor_tensor", reads=[x1k, xk], writes=[x1k], out=x1[:], in0=x1[:], in1=xt[:], op=ALU.add)
            P.d(X1[t0:t0 + 128, :], x1[:], reads=[x1k], writes=[P.u("X1")])
            st, sk = st_r.next()
            P.i("act", "activation", reads=[x1k], writes=[pf + "junk", sk], out=junk[:], in_=x1[:], func=AF.Square, accum_out=st[:, 0:1])
            P.i("act", "activation", reads=[sk, pf + "eps"], writes=[sk], out=st[:, 1:2], in_=st[:, 0:1], func=AF.Ln, bias=epsc[:], scale=1.0 / D)
            P.i("act", "activation", reads=[sk], writes=[sk], out=st[:, 2:3], in_=st[:, 1:2], func=AF.Exp, scale=-0.5)
            h, hk = h_r.next()
            P.i("dve", "scalar_tensor_tensor", reads=[x1k, sk, pf + "G2%d" % r], writes=[hk], out=h[:], in0=x1[:], scalar=st[:, 2:3], in1=G2[r][:], op0=ALU.mult, op1=ALU.mult)
            P.i("pool", "tensor_tensor", reads=[hk, pf + "SH2%d" % r], writes=[hk], out=h[:], in0=h[:], in1=SH2[r][:], op=ALU.add)
            tp, tpk = tp_r.next()
            for k in range(8):
                P.i("pe", "transpose", reads=[hk, pf + "ident"], writes=[tpk], noinc=(k != 7), out=tp[:, k * 128:(k + 1) * 128], in_=h[:, k * 128:(k + 1) * 128], identity=ident[:])
            hT, hTk = hT_r.next()
            hTf, hTfk = hTf_r.next()
            P.i("act", "activation", reads=[tpk], writes=[hTk], out=hT[:], in_=tp[:].rearrange("p (k t) -> p k t", k=8), func=AF.Copy)
            P.i("dve", "tensor_copy", reads=[tpk], writes=[hTfk], out=hTf[:], in_=tp[:].rearrange("p (k t) -> p k t", k=8))
            P.d(H2T[:, t0:t0 + 128].rearrange("(k p) t -> p k t", p=128), hT[:].bitcast(F32), reads=[hTk], writes=[P.u("H2T")])
            pr, prk = pr_r.next()
            for k in range(8):
                P.i("pe", "matmul", reads=[hTfk, pf + "wr"], writes=[prk], out=pr[:, 0:36], lhsT=hTf[:, k, :], rhs=wr[:, k, :], start=(k == 0), stop=(k == 7))
            lg, lgk = lg_r.next()
            P.i("dve", "tensor_tensor", reads=[prk, pf + "rb"], writes=[lgk], out=lg[:], in0=pr[:, 0:36], in1=rb[:], op=ALU.add)
            P.i("dve", "tensor_reduce", reads=[lgk], writes=[sk], out=st[:, 4:5], in_=lg[:, 0:4], axis=AX.X, op=ALU.max)
            P.i("dve", "tensor_scalar", reads=[sk], writes=[sk], out=st[:, 5:6], in0=st[:, 4:5], scalar1=-1.0, scalar2=None, op0=ALU.mult)
            P.i("act", "activation", reads=[lgk, sk], writes=[sk], out=st[:, 32:36], in_=lg[:, 0:4], func=AF.Exp, bias=st[:, 5:6], accum_out=st[:, 6:7])
            P.i("dve", "reciprocal", reads=[sk], writes=[sk], out=st[:, 7:8], in_=st[:, 6:7])
            P.i("dve", "tensor_scalar", reads=[lgk, sk], writes=[sk], out=st[:, 8:12], in0=lg[:, 0:4], scalar1=st[:, 4:5], scalar2=None, op0=ALU.is_equal)
            P.i("dve", "tensor_scalar", reads=[sk], writes=[sk], out=st[:, 12:16], in0=st[:, 8:12], scalar1=BIG, scalar2=-BIG, op0=ALU.mult, op1=ALU.add)
            mkd, mkk = mk_r.next()
            P.i("dve", "tensor_tensor", reads=[lgk, sk], writes=[mkk], out=mkd[:], in0=lg[:, 4:36].rearrange("p (g e) -> p g e", g=4),
                in1=st[:, 12:16].unsqueeze(2).to_broadcast([128, 4, 8]), op=ALU.add)
            mflat = mkd[:].rearrange("p g e -> p (g e)")
            P.i("dve", "max", reads=[mkk], writes=[sk], out=st[:, 16:24], in_=mflat)
            oh, ohk = oh_r.next()
            P.i("dve", "tensor_scalar", reads=[mkk, sk], writes=[ohk + "/1"], out=oh[:, 0, :], in0=mflat, scalar1=st[:, 16:17], scalar2=None, op0=ALU.is_equal)
            P.i("dve", "tensor_scalar", reads=[mkk, sk], writes=[ohk + "/2"], out=oh[:, 1, :], in0=mflat, scalar1=st[:, 17:18], scalar2=None, op0=ALU.is_equal)
            P.i("dve", "tensor_tensor", reads=[sk], writes=[sk], out=st[:, 24:25], in0=st[:, 17:18], in1=st[:, 16:17], op=ALU.subtract)
            P.i("act", "activation", reads=[sk], writes=[sk], out=st[:, 25:26], in_=st[:, 24:25], func=AF.Exp)
            P.i("dve", "tensor_scalar", reads=[sk], writes=[sk], out=st[:, 26:27], in0=st[:, 25:26], scalar1=1.0, scalar2=None, op0=ALU.add)
            P.i("dve", "reciprocal", reads=[sk], writes=[sk], out=st[:, 27:28], in_=st[:, 26:27])
            P.i("dve", "tensor_tensor", reads=[sk], writes=[sk], out=st[:, 28:29], in0=st[:, 27:28], in1=st[:, 7:8], op=ALU.mult)
            P.i("dve", "tensor_tensor", reads=[sk], writes=[sk], out=st[:, 29:30], in0=st[:, 7:8], in1=st[:, 28:29], op=ALU.subtract)
            P.i("dve", "tensor_scalar", reads=[ohk + "/1", sk], writes=[ohk + "/3"], out=oh[:, 2, :], in0=oh[:, 0, :], scalar1=st[:, 28:29], scalar2=None, op0=ALU.mult)
            wt, wtk = wt_r.next()
            P.i("dve", "scalar_tensor_tensor", reads=[ohk + "/2", ohk + "/3", sk], writes=[wtk], out=wt[:], in0=oh[:, 1, :], scalar=st[:, 29:30], in1=oh[:, 2, :], op0=ALU.mult, op1=ALU.add)
            pw, pwk = pw_r.next()
            P.i("pe", "transpose", reads=[wtk, pf + "ident"], writes=[pwk], out=pw[0:32, 0:128], in_=wt[:], identity=ident[:])
            wT, wTk = wT_r.next()
            P.i("act", "activation", reads=[pwk], writes=[wTk], out=wT[:], in_=pw[0:32, 0:128], func=AF.Copy)
            P.d(WTd[:, t0:t0 + 128], wT[:], reads=[wTk], writes=[P.u("WTd")])


def phase_f2(P, Dm, l, src_name, tok_chunks=None):
    pf = "F%d_" % l
    xsrc = Dm.get(src_name, [T, D])
    CAT = Dm.get("CAT", [1024, T])
    w_out = Dm.get("w_out%d" % l, [128, 8, 1024], kind="ExternalInput")
    modrow = Dm.get("modrow%d" % l, [2, 6144])
    n2g = Dm.get("norm2_g%d" % l, [1, D], kind="ExternalInput")
    wr_d = Dm.get("wr%d" % l, [128, 8, 36], kind="ExternalInput")
    rb_d = Dm.get("rb%d" % l, [1, 36], kind="ExternalInput")
    ident_d = Dm.get("ident", [128, 128], kind="ExternalInput")
    X1 = Dm.get("X1", [T, D])
    chunks_ = (tok_chunks or CHUNKS)
    ntf = sum(cn for _, cn in chunks_) // 128
    NB = (2 * ntf * 128) // 128 + 32
    BUF = Dm.get("BUF%d" % l, [NB * 128, D])
    IDXW = Dm.get("IDXW%d" % l, [128, NB], I32)
    DEST = Dm.get("DEST%d" % l, [128, ntf * 2], I32)
    WWd = Dm.get("WWd%d" % l, [128, ntf * 2])
    ramp_d = Dm.get("ramp", [1, 72], kind="ExternalInput")
    bst_d = Dm.get("bst", [1, 104], kind="ExternalInput")
    pidx_d = Dm.get("pidx", [128, 1], kind="ExternalInput")
    stri_d = Dm.get("stri", [128, 2, 128], kind="ExternalInput")
    OH = P.sb(pf + "OH", [128, ntf, 2, 32])
    WW = P.sb(pf + "WW", [128, ntf, 2])
    RK = P.sb(pf + "RK", [128, ntf, 32])
    Msum = P.sb(pf + "Msum", [128, 32])
    P.i("dve", "memset", writes=[pf + "Msum"], ap=Msum[:], constant=0.0)
    stri = P.sb(pf + "stri", [128, 2, 128]); P.d(stri[:], stri_d[:, :, :], writes=[pf + "stri"])
    Mt_r = Rot(P, pf + "Mt", [128, 32], n=2)
    zz = P.sb(pf + "zz", [128, 4096])
    P.i("pool", "memset", writes=[pf + "zz"], ap=zz[:], constant=0.0)
    rows_per = 128 * 4
    for r0 in range(0, NB * 128, rows_per):
        P.d(BUF[r0:r0 + rows_per, :].rearrange("(p a) n -> p (a n)", p=128), zz[:], reads=[pf + "zz"], writes=[P.u("BUFz%d" % l)])
    mk = "modrow%d" % l
    ident = P.sb(pf + "ident", [128, 128])
    P.d(ident[:], ident_d[:, :], writes=[pf + "ident"])
    wo = P.sb(pf + "wo", [128, 8, 1024], F32R)
    for k in range(8):
        P.d(wo[:, k, :], w_out[:, k, :], writes=[pf + "wo/%d" % k], q="pool")
    wr = P.sb(pf + "wr", [128, 8, 36])
    P.d(wr[:], wr_d[:, :, :], writes=[pf + "wr"])
    rb = P.sb(pf + "rb", [128, 36])
    load_bcast(P, rb[:], pf + "rb", rb_d[0:1, :])
    G1 = [P.sb(pf + "G1%d" % r, [128, D]) for r in range(2)]
    G2 = [P.sb(pf + "G2%d" % r, [128, D]) for r in range(2)]
    SH2 = [P.sb(pf + "SH2%d" % r, [128, D]) for r in range(2)]
    gn = P.sb(pf + "gn", [128, D])
    load_bcast(P, gn[:], pf + "gn", n2g[0:1, :])
    for r in range(2):
        load_bcast(P, G1[r][:], pf + "G1%d" % r, modrow[r:r + 1, 2048:3072], reads=[mk])
        load_bcast(P, SH2[r][:], pf + "SH2%d" % r, modrow[r:r + 1, 3072:4096], reads=[mk])
        load_bcast(P, G2[r][:], pf + "G2%d" % r, modrow[r:r + 1, 4096:5120], reads=[mk])
        P.i("dve", "scalar_tensor_tensor", reads=[pf + "G2%d" % r, pf + "gn"], writes=[pf + "G2%d" % r], out=G2[r][:], in0=G2[r][:], scalar=1.0, in1=gn[:], op0=ALU.add, op1=ALU.mult)
    epsc = P.sb(pf + "eps", [128, 1])
    P.i("dve", "memset", writes=[pf + "eps"], ap=epsc[:], constant=EPS)
    ct_r = Rot(P, pf + "ct", [128, 8, 512], F32R, n=2)
    po_r = Rot(P, pf + "po", [128, 1024], n=1, psum=True)
    xt_r = Rot(P, pf + "xt", [128, D], n=2)
    x1_r = Rot(P, pf + "x1", [128, D], n=2)
    h_r = Rot(P, pf + "h", [128, D], n=2)
    junk = P.sb(pf + "junk", [128, D])
    st_r = Rot(P, pf + "st", [128, 64], n=3)
    tp_r = Rot(P, pf + "tp", [128, 1024], n=1, psum=True)
    H2 = Dm.get("H2_%d" % l, [T, D])
    hTf_r = Rot(P, pf + "hTf", [128, 8, 128], n=2)
    pr_r = Rot(P, pf + "pr", [128, 512], n=1, psum=True)
    lg_r = Rot(P, pf + "lg", [128, 36], n=2)
    mk_r = Rot(P, pf + "mk", [128, 4, 8], n=2)
    oh_r = Rot(P, pf + "oh", [128, 3, 32], n=2)
    pw_r = Rot(P, pf + "pw", [128, 512], n=1, psum=True)
    tile_i = 0
    for (c0, cn) in chunks_:
        ct, ctk = ct_r.next()
        P.d(ct[:, :, 0:cn], CAT[:, c0:c0 + cn].rearrange("(k p) t -> p k t", p=128), reads=["CAT"], writes=[ctk], q="pool")
        for i in range(cn // 128):
            t0 = c0 + i * 128
            r = 1 if t0 < LC else 0
            po, pok = po_r.next()
            for hf in range(2):
                for k in range(8):
                    P.i("pe", "matmul", reads=[ctk, pf + "wo/%d" % k], writes=[pok], out=po[:, hf * 512:(hf + 1) * 512], lhsT=ct[:, k, i * 128:(i + 1) * 128],
                        rhs=wo[:, k, hf * 512:(hf + 1) * 512], start=(k == 0), stop=(k == 7))
            xt, xk = xt_r.next()
            P.d(xt[:], xsrc[t0:t0 + 128, :], reads=[src_name], writes=[xk])
            x1, x1k = x1_r.next()
            for hf in range(2):
                sl = slice(hf * 512, (hf + 1) * 512)
                P.i("dve", "tensor_tensor", reads=[pok, pf + "G1%d" % r], writes=[x1k + "/%d" % hf], out=x1[:, sl], in0=po[:, sl], in1=G1[r][:, sl], op=ALU.mult)
            P.i("pool", "tensor_tensor", reads=[x1k, xk], writes=[x1k], out=x1[:], in0=x1[:], in1=xt[:], op=ALU.add)
            P.d(X1[t0:t0 + 128, :], x1[:], reads=[x1k], writes=[P.u("X1")])
            st, sk = st_r.next()
            P.i("act", "activation", reads=[x1k], writes=[pf + "junk", sk], out=junk[:], in_=x1[:], func=AF.Square, accum_out=st[:, 0:1])
            P.i("act", "activation", reads=[sk, pf + "eps"], writes=[sk], out=st[:, 1:2], in_=st[:, 0:1], func=AF.Ln, bias=epsc[:], scale=1.0 / D)
            P.i("act", "activation", reads=[sk], writes=[sk], out=st[:, 2:3], in_=st[:, 1:2], func=AF.Exp, scale=-0.5)
            h, hk = h_r.next()
            P.i("dve", "scalar_tensor_tensor", reads=[x1k, sk, pf + "G2%d" % r], writes=[hk], out=h[:], in0=x1[:], scalar=st[:, 2:3], in1=G2[r][:], op0=ALU.mult, op1=ALU.mult)
            P.i("pool", "tensor_tensor", reads=[hk, pf + "SH2%d" % r], writes=[hk], out=h[:], in0=h[:], in1=SH2[r][:], op=ALU.add)
            P.d(H2[t0:t0 + 128, :], h[:], reads=[hk], writes=[P.u("H2_%d" % l)])
            tp, tpk = tp_r.next()
            for k in range(8):
                P.i("pe", "transpose", reads=[hk, pf + "ident"], writes=[tpk], noinc=(k != 7), out=tp[:, k * 128:(k + 1) * 128], in_=h[:, k * 128:(k + 1) * 128], identity=ident[:])
            hTf, hTfk = hTf_r.next()
            P.i("dve", "tensor_copy", reads=[tpk], writes=[hTfk], out=hTf[:], in_=tp[:].rearrange("p (k t) -> p k t", k=8))
            pr, prk = pr_r.next()
            for k in range(8):
                P.i("pe", "matmul", reads=[hTfk, pf + "wr"], writes=[prk], out=pr[:, 0:36], lhsT=hTf[:, k, :], rhs=wr[:, k, :], start=(k == 0), stop=(k == 7))
            lg, lgk = lg_r.next()
            P.i("dve", "tensor_tensor", reads=[prk, pf + "rb"], writes=[lgk], out=lg[:], in0=pr[:, 0:36], in1=rb[:], op=ALU.add)
            P.i("dve", "tensor_reduce", reads=[lgk], writes=[sk], out=st[:, 4:5], in_=lg[:, 0:4], axis=AX.X, op=ALU.max)
            P.i("dve", "tensor_scalar", reads=[sk], writes=[sk], out=st[:, 5:6], in0=st[:, 4:5], scalar1=-1.0, scalar2=None, op0=ALU.mult)
            P.i("act", "activation", reads=[lgk, sk], writes=[sk], out=st[:, 32:36], in_=lg[:, 0:4], func=AF.Exp, bias=st[:, 5:6], accum_out=st[:, 6:7])
            P.i("dve", "reciprocal", reads=[sk], writes=[sk], out=st[:, 7:8], in_=st[:, 6:7])
            P.i("dve", "tensor_scalar", reads=[lgk, sk], writes=[sk], out=st[:, 8:12], in0=lg[:, 0:4], scalar1=st[:, 4:5], scalar2=None, op0=ALU.is_equal)
            P.i("dve", "tensor_scalar", reads=[sk], writes=[sk], out=st[:, 12:16], in0=st[:, 8:12], scalar1=BIG, scalar2=-BIG, op0=ALU.mult, op1=ALU.add)
            mkd, mkk = mk_r.next()
            P.i("dve", "tensor_tensor", reads=[lgk, sk], writes=[mkk], out=mkd[:], in0=lg[:, 4:36].rearrange("p (g e) -> p g e", g=4),
                in1=st[:, 12:16].unsqueeze(2).to_broadcast([128, 4, 8]), op=ALU.add)
            mflat = mkd[:].rearrange("p g e -> p (g e)")
            P.i("dve", "max", reads=[mkk], writes=[sk], out=st[:, 16:24], in_=mflat)
            oh, ohk = oh_r.next()
            P.i("dve", "tensor_scalar", reads=[mkk, sk], writes=[ohk + "/1"], out=oh[:, 0, :], in0=mflat, scalar1=st[:, 16:17], scalar2=None, op0=ALU.is_equal)
            P.i("dve", "tensor_scalar", reads=[mkk, sk], writes=[ohk + "/2"], out=oh[:, 1, :], in0=mflat, scalar1=st[:, 17:18], scalar2=None, op0=ALU.is_equal)
            P.i("dve", "tensor_tensor", reads=[sk], writes=[sk], out=st[:, 24:25], in0=st[:, 17:18], in1=st[:, 16:17], op=ALU.subtract)
            P.i("act", "activation", reads=[sk], writes=[sk], out=st[:, 25:26], in_=st[:, 24:25], func=AF.Exp)
            P.i("dve", "tensor_scalar", reads=[sk], writes=[sk], out=st[:, 26:27], in0=st[:, 25:26], scalar1=1.0, scalar2=None, op0=ALU.add)
            P.i("dve", "reciprocal", reads=[sk], writes=[sk], out=st[:, 27:28], in_=st[:, 26:27])
            P.i("dve", "tensor_tensor", reads=[sk], writes=[sk], out=st[:, 28:29], in0=st[:, 27:28], in1=st[:, 7:8], op=ALU.mult)
            P.i("dve", "tensor_tensor", reads=[sk], writes=[sk], out=st[:, 29:30], in0=st[:, 7:8], in1=st[:, 28:29], op=ALU.subtract)
            ti = tile_i
            tile_i += 1
            P.i("act", "activation", reads=[ohk + "/1"], writes=[pf + "OH/%d_0" % ti], out=OH[:, ti, 0, :], in_=oh[:, 0, :], func=AF.Copy)
            P.i("act", "activation", reads=[ohk + "/2"], writes=[pf + "OH/%d_1" % ti], out=OH[:, ti, 1, :], in_=oh[:, 1, :], func=AF.Copy)
            P.i("act", "activation", reads=[sk], writes=[pf + "WW/%d" % ti], out=WW[:, ti, :], in_=st[:, 28:30], func=AF.Copy)
            Mt, Mtk = Mt_r.next()
            P.i("dve", "tensor_tensor", reads=[ohk + "/1", ohk + "/2"], writes=[Mtk], out=Mt[:], in0=oh[:, 0, :], in1=oh[:, 1, :], op=ALU.add)
            pw, pwk = pw_r.next()
            P.i("pe", "matmul", reads=[Mtk, pf + "stri"], writes=[pwk], out=pw[:, 0:32], lhsT=stri[:, 0, :], rhs=Mt[:], start=True, stop=False)
            P.i("pe", "matmul", reads=[pf + "Msum", pf + "stri"], writes=[pwk], out=pw[:, 0:32], lhsT=stri[:, 1, :], rhs=Msum[:], start=False, stop=True)
            P.i("act", "activation", reads=[pwk], writes=[pf + "RK/%d" % ti], out=RK[:, ti, :], in_=pw[:, 0:32], func=AF.Copy)
            P.i("dve", "tensor_tensor", reads=[Mtk, pf + "Msum"], writes=[pf + "Msum"], out=Msum[:], in0=Msum[:], in1=Mt[:], op=ALU.add)
    ramp = P.sb(pf + "ramp", [128, 72]); load_bcast(P, ramp[:], pf + "ramp", ramp_d[0:1, :])
    bst = P.sb(pf + "bst", [128, 104]); load_bcast(P, bst[:], pf + "bst", bst_d[0:1, :])
    pidx = P.sb(pf + "pidx", [128, 1]); P.d(pidx[:], pidx_d[:, :], writes=[pf + "pidx"])
    q = P.sb(pf + "q", [128, 8, 32])
    QK = pf + "q"
    big = P.sb(pf + "big", [128, 104 * 32])
    ones32 = P.sb(pf + "ones32", [128, 32]); P.i("dve", "memset", writes=[pf + "ones32"], ap=ones32[:], constant=1.0)
    pw, pwk = pw_r.next()
    P.i("pe", "matmul", reads=[pf + "Msum", pf + "stri"], writes=[pwk], out=pw[:, 0:32], lhsT=stri[:, 1, :], rhs=Msum[:], start=True, stop=True)
    P.i("dve", "tensor_copy", reads=[pwk], writes=[QK], out=q[:, 0, :], in_=pw[:, 0:32])
    NM = 2 * ntf
    b3 = big[:, 0:32 * NM].rearrange("p (e m) -> p e m", e=32)
    P.i("dve", "tensor_tensor", reads=[QK, pf + "ramp"], writes=[pf + "big"], out=b3, in0=q[:, 0, :].unsqueeze(2).to_broadcast([128, 32, NM]),
        in1=ramp[:, 0:NM].unsqueeze(1).to_broadcast([128, 32, NM]), op=ALU.is_ge)
    P.i("dve", "tensor_reduce", reads=[pf + "big"], writes=[QK], out=q[:, 1, :], in_=b3, axis=AX.X, op=ALU.add)
    P.i("dve", "tensor_scalar", reads=[QK], writes=[QK], out=q[:, 1, :], in0=q[:, 1, :], scalar1=128.0, scalar2=None, op0=ALU.mult)
    P.i("dve", "tensor_tensor_scan", reads=[QK, pf + "ones32"], writes=[QK], out=q[:, 2, :], data0=ones32[:], data1=q[:, 1, :], initial=0.0, op0=ALU.mult, op1=ALU.add)
    P.i("dve", "tensor_tensor", reads=[QK], writes=[QK], out=q[:, 3, :], in0=q[:, 2, :], in1=q[:, 1, :], op=ALU.subtract)
    be = P.sb(pf + "be", [128, 104])
    b3 = big[:, 0:NB * 32].rearrange("p (b e) -> p b e", e=32)
    P.i("dve", "tensor_tensor", reads=[QK, pf + "bst"], writes=[pf + "big"], out=b3, in0=q[:, 2, :].unsqueeze(1).to_broadcast([128, NB, 32]),
        in1=bst[:, 0:NB].unsqueeze(2).to_broadcast([128, NB, 32]), op=ALU.is_le)
    P.i("dve", "tensor_reduce", reads=[pf + "big"], writes=[pf + "be"], out=be[:, 0:NB], in_=b3, axis=AX.X, op=ALU.add)
    P.i("dve", "tensor_scalar", reads=[pf + "be"], writes=[pf + "be"], out=be[:, 0:NB], in0=be[:, 0:NB], scalar1=31.0, scalar2=None, op0=ALU.min)
    same = P.sb(pf + "same", [128, 104])
    P.i("dve", "memset", writes=[pf + "same"], ap=same[:, 0:1], constant=0.0)
    P.i("dve", "tensor_tensor", reads=[pf + "be"], writes=[pf + "same"], out=same[:, 1:NB], in0=be[:, 1:NB], in1=be[:, 0:NB - 1], op=ALU.is_equal)
    P.i("dve", "memset", writes=[pf + "same"], ap=same[:, NB // 2:NB // 2 + 1], constant=0.0)
    P.i("dve", "tensor_scalar", reads=[pf + "be", pf + "pidx"], writes=[pf + "be"], out=be[:, 0:NB], in0=be[:, 0:NB], scalar1=128.0, scalar2=pidx[:, 0:1], op0=ALU.mult, op1=ALU.add)
    P.i("dve", "scalar_tensor_tensor", reads=[pf + "be", pf + "same"], writes=[pf + "be"], out=be[:, 0:NB], in0=same[:, 0:NB], scalar=1048576.0, in1=be[:, 0:NB], op0=ALU.mult, op1=ALU.add)
    bei = P.sb(pf + "bei", [128, 104], I32)
    P.i("dve", "tensor_copy", reads=[pf + "be"], writes=[pf + "bei"], out=bei[:, 0:NB], in_=be[:, 0:NB])
    P.d(IDXW[:, :], bei[:, 0:NB], reads=[pf + "bei"], writes=[P.u("IDXW%d" % l)])
    dsf = P.sb(pf + "dsf", [128, ntf, 2])
    pos_r = Rot(P, pf + "pos", [128, 2, 32], n=2)
    for ti in range(ntf):
        pos, posk = pos_r.next()
        P.i("dve", "tensor_tensor", reads=[pf + "RK/%d" % ti, QK], writes=[posk + "/p"], out=pos[:, 0, :], in0=RK[:, ti, :], in1=q[:, 3, :], op=ALU.add)
        for k in range(2):
            P.i("dve", "tensor_tensor", reads=[posk + "/p", pf + "OH/%d_%d" % (ti, k)], writes=[posk + "/t"], out=pos[:, 1, :], in0=pos[:, 0, :], in1=OH[:, ti, k, :], op=ALU.mult)
            P.i("dve", "tensor_reduce", reads=[posk + "/t"], writes=[pf + "dsf/%d_%d" % (ti, k)], out=dsf[:, ti, k:k + 1], in_=pos[:, 1, :], axis=AX.X, op=ALU.add)
    dsi = P.sb(pf + "dsi", [128, ntf * 2], I32)
    P.i("dve", "tensor_copy", reads=[pf + "dsf"], writes=[pf + "dsi"], out=dsi[:], in_=dsf[:].rearrange("p t k -> p (t k)"))
    P.d(DEST[:, :], dsi[:], reads=[pf + "dsi"], writes=[P.u("DEST%d" % l)])
    P.d(WWd[:, :], WW[:].rearrange("p t k -> p (t k)"), reads=[pf + "WW"], writes=[P.u("WWd%d" % l)])
    h2_r = Rot(P, pf + "h2s", [128, D], n=3)
    ti = 0
    for (c0, cn) in chunks_:
        for i in range(cn // 128):
            t0 = c0 + i * 128
            h2, h2k = h2_r.next()
            P.d(h2[:], H2[t0:t0 + 128, :], reads=["H2_%d" % l], writes=[h2k])
            for k in range(2):
                P.dma(lambda e, h2=h2, col=ti * 2 + k: e.indirect_dma_start(out=BUF[:, :], out_offset=bass.IndirectOffsetOnAxis(ap=dsi[:, col:col + 1], axis=0), in_=h2[:], in_offset=None),
                      reads=[h2k, pf + "dsi", "BUFz%d" % l], writes=[P.u("BUF%d" % l)], q="pool")
            ti += 1


def phase_g(P, Dm, l, tok_chunks, final):
    pf = "g%d_" % l
    X1 = Dm.get("X1", [T, D])
    H2T = Dm.get("H2T", [D, T])
    WTd = Dm.get("WTd", [32, T])
    WG = Dm.get("moe_g%d" % l, [32, 128, 8, 512], kind="ExternalInput")
    WU = Dm.get("moe_u%d" % l, [32, 128, 8, 512], kind="ExternalInput")
    WD = Dm.get("moe_d%d" % l, [32, 128, 4, 1024], kind="ExternalInput")
    modrow = Dm.get("modrow%d" % l, [2, 6144])
    mk = "modrow%d" % l
    if final:
        fg_d = Dm.get("final_g", [1, D], kind="ExternalInput")
        OUT = Dm.get("out", [L, D], kind="ExternalOutput")
        fg = P.sb(pf + "fg", [128, D])
        load_bcast(P, fg[:], pf + "fg", fg_d[0:1, :])
        epsc = P.sb(pf + "eps", [128, 1])
        P.i("dve", "memset", writes=[pf + "eps"], ap=epsc[:], constant=EPS)
        junk = P.sb(pf + "junk", [128, D])
    else:
        XN = Dm.get("XN%d" % l, [T, D])
    G2g = [P.sb(pf + "g2%d" % r, [128, D]) for r in range(2)]
    for r in range(2):
        load_bcast(P, G2g[r][:], pf + "g2%d" % r, modrow[r:r + 1, 5120:6144], reads=[mk])
    hT_r = Rot(P, pf + "hT", [128, 8, 512], F32R, n=2)
    acc_r = Rot(P, pf + "acc", [128, 4, 1024], n=1)
    wg_r = Rot(P, pf + "wg", [128, 8, 512], F32R, n=2)
    wu_r = Rot(P, pf + "wu", [128, 8, 512], F32R, n=2)
    wd_r = Rot(P, pf + "wd", [128, 4, 1024], F32R, n=2)
    wb_r = Rot(P, pf + "wb", [128, 512], n=2)
    pg_r = Rot(P, pf + "pg", [128, 512], n=2, psum=True)
    pu_r = Rot(P, pf + "pu", [128, 512], n=2, psum=True)
    pd_r = Rot(P, pf + "pd", [128, 512], n=2, psum=True)
    sg_r = Rot(P, pf + "sg", [128, 512], n=2)
    hu_r = Rot(P, pf + "hu", [128, 512], n=2)
    hid_r = Rot(P, pf + "hid", [128, 4, 512], F32R, n=2)
    xt_r = Rot(P, pf + "xt", [128, D], n=2)
    st_r = Rot(P, pf + "st", [128, 8], n=2)
    for (c0, cn) in tok_chunks:
        nt = cn // 128
        hT, hTk = hT_r.next()
        P.d(hT[:, :, 0:cn], H2T[:, c0:c0 + cn].rearrange("(k p) t -> p k t", p=128), reads=["H2T"], writes=[hTk], q="pool")
        acc, acck = acc_r.next()
        for e in range(32):
            wg, wgk = wg_r.next()
            wu, wuk = wu_r.next()
            wd, wdk = wd_r.next()
            P.d(wg[:], WG[e], writes=[wgk], q="pool")
            P.d(wu[:], WU[e], writes=[wuk], q="pool")
            P.d(wd[:], WD[e], writes=[wdk], q="pool")
            wb, wbk = wb_r.next()
            P.d(wb[:, 0:cn], WTd[e:e + 1, c0:c0 + cn].to_broadcast([128, cn]), reads=["WTd"], writes=[wbk])
            hid, hidk = hid_r.next()
            for hc in range(4):
                pg, pgk = pg_r.next()
                pu, puk = pu_r.next()
                for k in range(8):
                    P.i("pe", "matmul", reads=[wgk, hTk], writes=[pgk], out=pg[:, 0:cn], lhsT=wg[:, k, hc * 128:(hc + 1) * 128], rhs=hT[:, k, 0:cn], start=(k == 0), stop=(k == 7))
                for k in range(8):
                    P.i("pe", "matmul", reads=[wuk, hTk], writes=[puk], out=pu[:, 0:cn], lhsT=wu[:, k, hc * 128:(hc + 1) * 128], rhs=hT[:, k, 0:cn], start=(k == 0), stop=(k == 7))
                sg, sgk = sg_r.next()
                P.i("act", "activation", reads=[pgk], writes=[sgk], out=sg[:, 0:cn], in_=pg[:, 0:cn], func=AF.Silu)
                hu, huk = hu_r.next()
                P.i("dve", "tensor_tensor", reads=[sgk, puk], writes=[huk], out=hu[:, 0:cn], in0=sg[:, 0:cn], in1=pu[:, 0:cn], op=ALU.mult)
                P.i("pool", "tensor_tensor", reads=[huk, wbk], writes=[hidk + "/%d" % hc], out=hid[:, hc, 0:cn], in0=hu[:, 0:cn], in1=wb[:, 0:cn], op=ALU.mult)
            hkeys = [hidk + "/%d" % hc for hc in range(4)]
            for tt in range(nt):
                for hf in range(2):
                    pd, pdk = pd_r.next()
                    for hc in range(4):
                        P.i("pe", "matmul", reads=hkeys + [wdk], writes=[pdk], out=pd[:], lhsT=hid[:, hc, tt * 128:(tt + 1) * 128], rhs=wd[:, hc, hf * 512:(hf + 1) * 512],
                            start=(hc == 0), stop=(hc == 3))
                    ak = acck + "/%d_%d" % (tt, hf)
                    if e == 0:
                        P.i("dve", "tensor_copy", reads=[pdk], writes=[ak], out=acc[:, tt, hf * 512:(hf + 1) * 512], in_=pd[:])
                    else:
                        P.i("dve", "tensor_tensor", reads=[pdk, ak], writes=[ak], out=acc[:, tt, hf * 512:(hf + 1) * 512], in0=acc[:, tt, hf * 512:(hf + 1) * 512], in1=pd[:], op=ALU.add)
        for tt in range(nt):
            t0 = c0 + tt * 128
            r = 1 if t0 < LC else 0
            aks = [acck + "/%d_%d" % (tt, hf) for hf in range(2)]
            xt, xk = xt_r.next()
            P.d(xt[:], X1[t0:t0 + 128, :], reads=["X1"], writes=[xk])
            P.i("pool", "tensor_tensor", reads=aks + [pf + "g2%d" % r], writes=aks, out=acc[:, tt, :], in0=acc[:, tt, :], in1=G2g[r][:], op=ALU.mult)
            P.i("dve", "tensor_tensor", reads=aks + [xk], writes=[xk], out=xt[:], in0=xt[:], in1=acc[:, tt, :], op=ALU.add)
            if not final:
                P.d(XN[t0:t0 + 128, :], xt[:], reads=[xk], writes=[P.u("XN%d" % l)])
            else:
                st, sk = st_r.next()
                P.i("act", "activation", reads=[xk], writes=[pf + "junk", sk], out=junk[:], in_=xt[:], func=AF.Square, accum_out=st[:, 0:1])
                P.i("act", "activation", reads=[sk, pf + "eps"], writes=[sk], out=st[:, 1:2], in_=st[:, 0:1], func=AF.Ln, bias=epsc[:], scale=1.0 / D)
                P.i("act", "activation", reads=[sk], writes=[sk], out=st[:, 2:3], in_=st[:, 1:2], func=AF.Exp, scale=-0.5)
                P.i("dve", "scalar_tensor_tensor", reads=[xk, sk, pf + "fg"], writes=[xk], out=xt[:], in0=xt[:], scalar=st[:, 2:3], in1=fg[:], op0=ALU.mult, op1=ALU.mult)
                P.d(OUT[t0 - LC:t0 - LC + 128, :], xt[:], reads=[xk], writes=[P.u("out")], final=True)


def phase_b(P, Dm, l):
    pf = "b%d_" % l
    FMT = Dm.get("FMT", [1024, T])
    CAT = Dm.get("CAT", [1024, T])
    Bd = Dm.get("s5B%d" % l, [2, 2, 8, 128, 128], kind="ExternalInput")
    Cd = Dm.get("s5C%d" % l, [2, 2, 8, 128, 128], kind="ExternalInput")
    lam_d = Dm.get("s5lam%d" % l, [128, 3, 16], kind="ExternalInput")
    dsk_d = Dm.get("s5d%d" % l, [128, 2], kind="ExternalInput")
    gw_d = Dm.get("gluw%d" % l, [128, 2, 256], kind="ExternalInput")
    gb_d = Dm.get("glub%d" % l, [128, 2], kind="ExternalInput")
    NMAX = 512
    lam = P.sb(pf + "lam", [128, 3, 16])
    P.d(lam[:], lam_d[:, :, :], writes=[pf + "lam"])
    dsk = P.sb(pf + "dsk", [128, 2]); P.d(dsk[:], dsk_d[:, :], writes=[pf + "dsk"])
    gb = P.sb(pf + "gb", [128, 2]); P.d(gb[:], gb_d[:, :], writes=[pf + "gb"])
    gw = P.sb(pf + "gw", [128, 2, 256], F32R); P.d(gw[:], gw_d[:, :, :], writes=[pf + "gw"], q="pool")
    Bb = P.sb(pf + "Bb", [128, 16, 128], F32R)
    Cb = P.sb(pf + "Cb", [128, 16, 128], F32R)
    sc = P.sb(pf + "sc", [128, 24, 16])
    K_ = pf + "sc"
    LR, LI, LDT = lam[:, 0, :], lam[:, 1, :], lam[:, 2, :]
    (DT, RM, TH, C_, S_, T1, T2, T3, ARE, AIM, DEN, AM1, FRE, FIM, HP) = range(15)

    def S(i):
        return sc[:, i, :]

    def tt(o, a, b, op, eng="dve"):
        P.i(eng, "tensor_tensor", reads=[K_, pf + "lam"], writes=[K_], out=o, in0=a, in1=b, op=op)

    hpi = P.sb(pf + "hpi", [128, 1])
    P.i("dve", "memset", writes=[pf + "hpi"], ap=hpi[:], constant=float(np.pi / 2))
    P.i("act", "activation", reads=[pf + "lam"], writes=[K_], out=S(DT), in_=LDT, func=AF.Exp)
    tt(S(RM), LR, S(DT), ALU.mult)
    P.i("act", "activation", reads=[K_], writes=[K_], out=S(RM), in_=S(RM), func=AF.Exp)
    tt(S(TH), LI, S(DT), ALU.mult)
    P.i("act", "activation", reads=[K_], writes=[K_], out=S(S_), in_=S(TH), func=AF.Sin, scale=1.0 / 32)
    P.i("act", "activation", reads=[K_, pf + "hpi"], writes=[K_], out=S(C_), in_=S(TH), func=AF.Sin, scale=1.0 / 32, bias=hpi[:])
    for _ in range(5):
        tt(S(T1), S(C_), S(C_), ALU.mult)
        tt(S(T2), S(S_), S(S_), ALU.mult)
        tt(S(T3), S(C_), S(S_), ALU.mult)
        tt(S(C_), S(T1), S(T2), ALU.subtract)
        tt(S(S_), S(T3), S(T3), ALU.add)
    tt(S(ARE), S(RM), S(C_), ALU.mult)
    tt(S(AIM), S(RM), S(S_), ALU.mult)
    tt(S(T1), LR, LR, ALU.mult)
    tt(S(T2), LI, LI, ALU.mult)
    tt(S(DEN), S(T1), S(T2), ALU.add)
    P.i("dve", "reciprocal", reads=[K_], writes=[K_], out=S(DEN), in_=S(DEN))
    P.i("dve", "tensor_scalar", reads=[K_], writes=[K_], out=S(AM1), in0=S(ARE), scalar1=-1.0, scalar2=None, op0=ALU.add)
    tt(S(T1), S(AM1), LR, ALU.mult)
    tt(S(T2), S(AIM), LI, ALU.mult)
    tt(S(T1), S(T1), S(T2), ALU.add)
    tt(S(FRE), S(T1), S(DEN), ALU.mult)
    tt(S(T1), S(AIM), LR, ALU.mult)
    tt(S(T2), S(AM1), LI, ALU.mult)
    tt(S(T1), S(T1), S(T2), ALU.subtract)
    tt(S(FIM), S(T1), S(DEN), ALU.mult)
    Ep = P.sb(pf + "Ep", [128, 2, 8, NMAX])
    Tm = P.sb(pf + "Tm", [128, 2, 8, NMAX])
    pw = P.sb(pf + "pw", [128, 4, 8])
    tb = P.sb(pf + "tb", [128, 2, 8, NMAX // 2])
    carry = P.sb(pf + "carry", [128, 16, 2])
    P.i("dve", "memset", writes=[pf + "carry"], ap=carry[:], constant=0.0)
    yacc = P.sb(pf + "yacc", [128, 2, T])
    uc_r = Rot(P, pf + "uc", [128, 2, NMAX], F32R, n=2)
    pb_r = Rot(P, pf + "pb", [128, 512], n=4, psum=True)
    py_r = Rot(P, pf + "py", [128, 512], n=2, psum=True)
    br_r = Rot(P, pf + "br", [128, 2, NMAX], n=2)
    t_r = Rot(P, pf + "t", [128, 4, NMAX], n=2)
    v_r = Rot(P, pf + "v", [128, 2, NMAX], n=2)
    g_r = Rot(P, pf + "g", [128, 2, NMAX], n=2)
    h_r = Rot(P, pf + "h", [128, 2, NMAX], F32R, n=2)
    TK, EK = pf + "Tm", pf + "Ep"
    for d in range(2):
        dsl = slice(d * 8, d * 8 + 8)
        P.d(Bb[:], Bd[d].rearrange("c j k m -> k (c j) m"), writes=[pf + "Bb"], q="pool")
        P.d(Cb[:], Cd[d].rearrange("c j k m -> k (c j) m"), writes=[pf + "Cb"], q="pool")
        P.i("act", "activation", reads=[pf + "Cb"], writes=[pf + "Cb"], out=Cb[:, 8:16, :], in_=Cb[:, 8:16, :].bitcast(F32), func=AF.Copy, scale=-1.0)
        P.i("dve", "tensor_copy", reads=[K_], writes=[EK], out=Ep[:, 0, :, 0:1], in_=S(C_)[:, dsl].unsqueeze(2))
        P.i("dve", "tensor_copy", reads=[K_], writes=[EK], out=Ep[:, 1, :, 0:1], in_=S(S_)[:, dsl].unsqueeze(2))
        P.i("dve", "tensor_copy", reads=[K_], writes=[pf + "pw"], out=pw[:, 0, :], in_=S(C_)[:, dsl])
        P.i("dve", "tensor_copy", reads=[K_], writes=[pf + "pw"], out=pw[:, 1, :], in_=S(S_)[:, dsl])
        n = 1
        while n < NMAX:
            cn_b = pw[:, 0, :].unsqueeze(2).to_broadcast([128, 8, n])
            sn_b = pw[:, 1, :].unsqueeze(2).to_broadcast([128, 8, n])
            ire, iim = Ep[:, 0, :, 0:n], Ep[:, 1, :, 0:n]
            ore, oim = Ep[:, 0, :, n:2 * n], Ep[:, 1, :, n:2 * n]
            P.i("dve", "tensor_tensor", reads=[EK, pf + "pw"], writes=[pf + "tb"], out=tb[:, 0, :, 0:n], in0=iim, in1=sn_b, op=ALU.mult)
            P.i("pool", "tensor_tensor", reads=[EK, pf + "pw"], writes=[pf + "tb2"], out=tb[:, 1, :, 0:n], in0=iim, in1=cn_b, op=ALU.mult)
            P.i("dve", "tensor_tensor", reads=[EK, pf + "pw"], writes=[EK + "/a"], out=ore, in0=ire, in1=cn_b, op=ALU.mult)
            P.i("pool", "tensor_tensor", reads=[EK, pf + "pw"], writes=[EK + "/b"], out=oim, in0=ire, in1=sn_b, op=ALU.mult)
            P.i("dve", "tensor_tensor", reads=[EK + "/a", pf + "tb"], writes=[EK + "/a"], out=ore, in0=ore, in1=tb[:, 0, :, 0:n], op=ALU.subtract)
            P.i("pool", "tensor_tensor", reads=[EK + "/b", pf + "tb2"], writes=[EK + "/b"], out=oim, in0=oim, in1=tb[:, 1, :, 0:n], op=ALU.add)
            P.i("dve", "tensor_tensor", reads=[pf + "pw"], writes=[pf + "pw"], out=pw[:, 2, :], in0=pw[:, 0, :], in1=pw[:, 1, :], op=ALU.mult)
            P.i("dve", "tensor_tensor", reads=[pf + "pw"], writes=[pf + "pw"], out=pw[:, 0, :], in0=pw[:, 0, :], in1=pw[:, 0, :], op=ALU.mult)
            P.i("dve", "tensor_tensor", reads=[pf + "pw"], writes=[pf + "pw"], out=pw[:, 3, :], in0=pw[:, 1, :], in1=pw[:, 1, :], op=ALU.mult)
            P.i("dve", "tensor_tensor", reads=[pf + "pw"], writes=[pf + "pw"], out=pw[:, 0, :], in0=pw[:, 0, :], in1=pw[:, 3, :], op=ALU.subtract)
            P.i("dve", "tensor_tensor", reads=[pf + "pw"], writes=[pf + "pw"], out=pw[:, 1, :], in0=pw[:, 2, :], in1=pw[:, 2, :], op=ALU.add)
            n *= 2
        for j in range(8):
            fr = S(FRE)[:, d * 8 + j:d * 8 + j + 1]
            fi = S(FIM)[:, d * 8 + j:d * 8 + j + 1]
            t, tk = t_r.next()
            P.i("dve", "tensor_scalar", reads=[EK, K_], writes=[tk + "/0"], out=t[:, 0, :], in0=Ep[:, 1, j, :], scalar1=fi, scalar2=None, op0=ALU.mult)
            P.i("dve", "scalar_tensor_tensor", reads=[EK, K_, tk + "/0"], writes=[TK + "/a%d" % j], out=Tm[:, 0, j, :], in0=Ep[:, 0, j, :], scalar=fr, in1=t[:, 0, :], op0=ALU.mult, op1=ALU.add)
            P.i("dve", "tensor_scalar", reads=[EK, K_], writes=[tk + "/1"], out=t[:, 1, :], in0=Ep[:, 1, j, :], scalar1=fr, scalar2=None, op0=ALU.mult)
            P.i("dve", "scalar_tensor_tensor", reads=[EK, K_, tk + "/1"], writes=[TK + "/b%d" % j], out=Tm[:, 1, j, :], in0=Ep[:, 0, j, :], scalar=fi, in1=t[:, 1, :], op0=ALU.mult, op1=ALU.subtract)
        order = CHUNKS if d == 0 else [CHUNKS[0]] + CHUNKS[:0:-1]
        rev = (d == 1)

        def R(ap):
            return ap[:, ::-1] if rev else ap

        for (c0, N) in order:
            uc, uck = uc_r.next()
            P.d(uc[:, :, 0:N], FMT[0:256, c0:c0 + N].rearrange("(oc p) t -> p oc t", p=128), reads=["FMT"], writes=[uck], q="pool")
            pys = [py_r.next() for _ in range(2)]
            def s5_iter(j):
                    oc = j // 4
                    dj = d * 8 + j
                    pbr, pbrk = pb_r.next()
                    pbi, pbik = pb_r.next()
                    P.i("pe", "matmul", reads=[pf + "Bb", uck], writes=[pbrk], out=pbr[:, 0:N], lhsT=Bb[:, j, :], rhs=uc[:, oc, 0:N], start=True, stop=True)
                    P.i("pe", "matmul", reads=[pf + "Bb", uck], writes=[pbik], out=pbi[:, 0:N], lhsT=Bb[:, 8 + j, :], rhs=uc[:, oc, 0:N], start=True, stop=True)
                    yield
                    br, brk = br_r.next()
                    P.i("act", "activation", reads=[pbrk], writes=[brk + "/0"], out=br[:, 0, 0:N], in_=R(pbr[:, 0:N]), func=AF.Copy)
                    P.i("act", "activation", reads=[pbik], writes=[brk + "/1"], out=br[:, 1, 0:N], in_=R(pbi[:, 0:N]), func=AF.Copy)
                    yield
                    t, tk = t_r.next()
                    v, vk = v_r.next()
                    b0, b1 = br[:, 0, 0:N], br[:, 1, 0:N]
                    tmr, tmi = Tm[:, 0, j, 0:N], Tm[:, 1, j, 0:N]
                    P.i("dve", "tensor_tensor", reads=[brk + "/0", TK], writes=[tk + "/0"], out=t[:, 0, 0:N], in0=tmr, in1=b0, op=ALU.mult)
                    P.i(S5_ENG2, "tensor_tensor", reads=[brk + "/1", TK], writes=[tk + "/1"], out=t[:, 1, 0:N], in0=tmi, in1=b1, op=ALU.mult)
                    P.i(S5_ENG2, "tensor_tensor", reads=[brk + "/1", TK], writes=[tk + "/2"], out=t[:, 2, 0:N], in0=tmr, in1=b1, op=ALU.mult)
                    P.i(S5_ENG2, "tensor_tensor", reads=[brk + "/0", TK], writes=[tk + "/3"], out=t[:, 3, 0:N], in0=tmi, in1=b0, op=ALU.mult)
                    P.i("dve", "tensor_tensor", reads=[tk + "/0", tk + "/1"], writes=[vk + "/0"], out=v[:, 0, 0:N], in0=t[:, 0, 0:N], in1=t[:, 1, 0:N], op=ALU.subtract)
                    P.i("dve", "tensor_tensor", reads=[tk + "/2", tk + "/3"], writes=[vk + "/1"], out=v[:, 1, 0:N], in0=t[:, 2, 0:N], in1=t[:, 3, 0:N], op=ALU.add)
                    yield
                    g, gk = g_r.next()
                    for c in range(2):
                        P.i("dve", "tensor_tensor_scan", reads=[vk + "/%d" % c, K_, pf + "carry"], writes=[gk + "/%d" % c], out=g[:, c, 0:N], data0=S(RM)[:, dj:dj + 1].to_broadcast([128, N]), data1=v[:, c, 0:N],
                            initial=carry[:, dj, c:c + 1], op0=ALU.mult, op1=ALU.add)
                    yield
                    h, hk = h_r.next()
                    t, tk = t_r.next()
                    epr, epi = Ep[:, 0, j, 0:N], Ep[:, 1, j, 0:N]
                    g0, g1 = g[:, 0, 0:N], g[:, 1, 0:N]
                    P.i("dve", "tensor_tensor", reads=[gk + "/0", EK], writes=[tk + "/0"], out=t[:, 0, 0:N], in0=epr, in1=g0, op=ALU.mult)
                    P.i(S5_ENG2, "tensor_tensor", reads=[gk + "/1", EK], writes=[tk + "/1"], out=t[:, 1, 0:N], in0=epi, in1=g1, op=ALU.mult)
                    P.i(S5_ENG2, "tensor_tensor", reads=[gk + "/1", EK], writes=[tk + "/2"], out=t[:, 2, 0:N], in0=epr, in1=g1, op=ALU.mult)
                    P.i("dve", "tensor_tensor", reads=[gk + "/0", EK], writes=[tk + "/3"], out=t[:, 3, 0:N], in0=epi, in1=g0, op=ALU.mult)
                    P.i("dve", "tensor_tensor", reads=[tk + "/0", tk + "/1"], writes=[hk + "/0"], out=h[:, 0, 0:N], in0=t[:, 0, 0:N], in1=t[:, 1, 0:N], op=ALU.subtract)
                    P.i("dve", "tensor_tensor", reads=[tk + "/2", tk + "/3"], writes=[hk + "/1"], out=h[:, 1, 0:N], in0=t[:, 2, 0:N], in1=t[:, 3, 0:N], op=ALU.add)
                    yield
                    P.i("act", "activation", reads=[hk], writes=[pf + "carry"], out=carry[:, dj, :], in_=h[:].bitcast(F32)[:, :, N - 1], func=AF.Copy)
                    py, pyk = pys[oc]
                    P.i("pe", "matmul", reads=[pf + "Cb", hk + "/0"], writes=[pyk], out=py[:, 0:N], lhsT=Cb[:, j, :], rhs=h[:, 0, 0:N], start=(j % 4 == 0), stop=False)
                    P.i("pe", "matmul", reads=[pf + "Cb", hk + "/1"], writes=[pyk], out=py[:, 0:N], lhsT=Cb[:, 8 + j, :], rhs=h[:, 1, 0:N], start=False, stop=(j % 4 == 3))
            for jp in range(0, 8, 2):
                gens = [s5_iter(jp), s5_iter(jp + 1)]
                alive = True
                while alive:
                    alive = False
                    for g_ in gens:
                        try:
                            next(g_)
                            alive = True
                        except StopIteration:
                            pass
            for oc in range(2):
                py, pyk = pys[oc]
                ya = yacc[:, oc, c0:c0 + N]
                yk = pf + "yacc/%d_%d" % (oc, c0)
                if d == 0:
                    P.i("dve", "scalar_tensor_tensor", reads=[uck, pf + "dsk", pyk], writes=[yk], out=ya, in0=uc[:, oc, 0:N].bitcast(F32), scalar=dsk[:, oc:oc + 1], in1=py[:, 0:N], op0=ALU.mult, op1=ALU.add)
                else:
                    P.i("dve", "tensor_tensor", reads=[yk, pyk], writes=[yk], out=ya, in0=ya, in1=R(py[:, 0:N]), op=ALU.add)
    e_r = Rot(P, pf + "e", [128, 3, NMAX], n=1)
    gT_r = Rot(P, pf + "gT", [128, 2, NMAX], F32R, n=1)
    pq_r = Rot(P, pf + "pq", [128, 512], n=2, psum=True)
    a_r = Rot(P, pf + "a", [128, NMAX], n=2)
    for (c0, N) in CHUNKS:
        gT, gTk = gT_r.next()
        for oc in range(2):
            y = yacc[:, oc, c0:c0 + N]
            yk = pf + "yacc/%d_%d" % (oc, c0)
            ee, ek = e_r.next()
            P.i("pool", "tensor_tensor", reads=[yk], writes=[ek + "/0"], out=ee[:, 0, 0:N], in0=y, in1=y, op=ALU.mult)
            P.i("dve", "tensor_scalar", reads=[ek + "/0"], writes=[ek + "/0"], out=ee[:, 0, 0:N], in0=ee[:, 0, 0:N], scalar1=0.044715, scalar2=1.0, op0=ALU.mult, op1=ALU.add)
            P.i("pool", "tensor_tensor", reads=[ek + "/0", yk], writes=[ek + "/1"], out=ee[:, 1, 0:N], in0=ee[:, 0, 0:N], in1=y, op=ALU.mult)
            P.i("act", "activation", reads=[ek + "/1"], writes=[ek + "/2"], out=ee[:, 2, 0:N], in_=ee[:, 1, 0:N], func=AF.Sigmoid, scale=1.5957691216057308)
            P.i("dve", "tensor_tensor", reads=[ek + "/2", yk], writes=[gTk + "/%d" % oc], out=gT[:, oc, 0:N], in0=ee[:, 2, 0:N], in1=y, op=ALU.mult)
        for oc2 in range(2):
            pq, pqk = pq_r.next()
            for oc in range(2):
                P.i("pe", "matmul", reads=[gTk, pf + "gw"], writes=[pqk], out=pq[:, 0:N], lhsT=gw[:, oc, oc2 * 128:(oc2 + 1) * 128], rhs=gT[:, oc, 0:N], start=(oc == 0), stop=(oc == 1))
            a, ak = a_r.next()
            P.i("act", "activation", reads=[pqk, pf + "gb"], writes=[ak], out=a[:, 0:N], in_=pq[:, 0:N], func=AF.Sigmoid, bias=gb[:, oc2:oc2 + 1])
            P.i("dve", "tensor_tensor", reads=[ak, gTk], writes=[ak], out=a[:, 0:N], in0=a[:, 0:N], in1=gT[:, oc2, 0:N].bitcast(F32), op=ALU.mult)
            P.d(CAT[oc2 * 128:(oc2 + 1) * 128, c0:c0 + N], a[:, 0:N], reads=[ak], writes=[P.u("CAT")])


def phase_d1(P, Dm, l):
    pf = "d%d_" % l
    FMT = Dm.get("FMT", [1024, T])
    XBCT = Dm.get("XBCT", [768, T])
    XBtok = Dm.get("XBtok", [T, 512])
    cw_d = Dm.get("convw%d" % l, [128, 6, 4], kind="ExternalInput")
    ident_d = Dm.get("ident", [128, 128], kind="ExternalInput")
    ident = P.sb(pf + "ident", [128, 128]); P.d(ident[:], ident_d[:, :], writes=[pf + "ident"])
    cw = P.sb(pf + "cw", [128, 6, 4]); P.d(cw[:], cw_d[:, :, :], writes=[pf + "cw"])
    xi_r = Rot(P, pf + "xi", [128, 6, 514], n=2)
    ac_r = Rot(P, pf + "ac", [128, 512], n=2)
    co_r = Rot(P, pf + "co", [128, 6, 512], n=2)
    tp_r = Rot(P, pf + "tp", [128, 512], n=2, psum=True)
    tk_r = Rot(P, pf + "tk", [128, 512], n=2)
    for (c0, N) in CHUNKS:
        xi, xik = xi_r.next()
        lz = c0 in (0, LC)
        rz = (c0 + N) in (LC, T)
        lo = c0 - (0 if lz else 1)
        hi = c0 + N + (0 if rz else 1)
        if lz:
            P.i("dve", "memset", writes=[xik + "/l"], ap=xi[:, :, 0:1], constant=0.0)
        if rz:
            P.i("dve", "memset", writes=[xik + "/r"], ap=xi[:, :, N + 1:N + 2], constant=0.0)
        P.d(xi[:, :, (1 if lz else 0):(1 if lz else 0) + hi - lo], FMT[256:1024, lo:hi].rearrange("(r p) t -> p r t", p=128), reads=["FMT"], writes=[xik + "/m"])
        co, cok = co_r.next()
        for rc in range(6):
            ac, ack = ac_r.next()
            P.i("dve", "tensor_scalar", reads=[xik, pf + "cw"], writes=[ack], out=ac[:, 0:N], in0=xi[:, rc, 1:N + 1], scalar1=cw[:, rc, 1:2], scalar2=None, op0=ALU.mult)
            P.i("dve", "scalar_tensor_tensor", reads=[xik, pf + "cw", ack], writes=[ack], out=ac[:, 0:N], in0=xi[:, rc, 0:N], scalar=cw[:, rc, 0:1], in1=ac[:, 0:N], op0=ALU.mult, op1=ALU.add)
            P.i("dve", "scalar_tensor_tensor", reads=[xik, pf + "cw", ack], writes=[ack], out=ac[:, 0:N], in0=xi[:, rc, 2:N + 2], scalar=cw[:, rc, 2:3], in1=ac[:, 0:N], op0=ALU.mult, op1=ALU.add)
            P.i("act", "activation", reads=[ack, pf + "cw"], writes=[cok + "/%d" % rc], out=co[:, rc, 0:N], in_=ac[:, 0:N], func=AF.Silu, bias=cw[:, rc, 3:4])
        P.d(XBCT[:, c0:c0 + N].rearrange("(r p) t -> p r t", p=128), co[:, :, 0:N], reads=[cok], writes=[P.u("XBCT")])
        for i in range(N // 128):
            tp, tpk = tp_r.next()
            for rc in range(4):
                P.i("pe", "transpose", reads=[cok + "/%d" % rc, pf + "ident"], writes=[tpk], noinc=(rc != 3), out=tp[:, rc * 128:(rc + 1) * 128], in_=co[:, rc, i * 128:(i + 1) * 128], identity=ident[:])
            tk, tkk = tk_r.next()
            P.i("act", "activation", reads=[tpk], writes=[tkk], out=tk[:], in_=tp[:], func=AF.Copy)
            P.d(XBtok[c0 + i * 128:c0 + (i + 1) * 128, :], tk[:], reads=[tkk], writes=[P.u("XBtok")])


def phase_d2(P, Dm, l):
    pf = "D%d_" % l
    XBCT = Dm.get("XBCT", [768, T])
    XBtok = Dm.get("XBtok", [T, 512])
    TMS = Dm.get("TMS", [T, 520])
    CAT = Dm.get("CAT", [1024, T])
    ident_d = Dm.get("ident", [128, 128], kind="ExternalInput")
    tri_d = Dm.get("tri", [128, 2, 128], kind="ExternalInput")
    alog_d = Dm.get("alog%d" % l, [1, 8], kind="ExternalInput")
    dsk_d = Dm.get("ssdd%d" % l, [1, 4], kind="ExternalInput")
    ng_d = Dm.get("ssdng%d" % l, [1, 256], kind="ExternalInput")
    ident = P.sb(pf + "ident", [128, 128]); P.d(ident[:], ident_d[:, :], writes=[pf + "ident"])
    tri = P.sb(pf + "tri", [128, 2, 128]); P.d(tri[:], tri_d[:, :, :], writes=[pf + "tri"])
    A = P.sb(pf + "A", [128, 8]); load_bcast(P, A[:], pf + "A", alog_d[0:1, :])
    P.i("act", "activation", reads=[pf + "A"], writes=[pf + "A"], out=A[:], in_=A[:], func=AF.Exp)
    P.i("dve", "tensor_scalar", reads=[pf + "A"], writes=[pf + "A"], out=A[:], in0=A[:], scalar1=-1.0, scalar2=None, op0=ALU.mult)
    dsk = P.sb(pf + "dsk", [128, 4]); load_bcast(P, dsk[:], pf + "dsk", dsk_d[0:1, :])
    ng = P.sb(pf + "ng", [128, 256]); load_bcast(P, ng[:], pf + "ng", ng_d[0:1, :])
    epsc = P.sb(pf + "eps", [128, 1]); P.i("dve", "memset", writes=[pf + "eps"], ap=epsc[:], constant=EPS)
    Yacc = [P.sb(pf + "Yacc%d" % d_, [128, NT, 256]) for d_ in range(2)]
    Sst = [P.sb(pf + "S%d" % d_, [128, 4, 64], F32R) for d_ in range(2)]
    bc_r = Rot(P, pf + "bc", [128, 4, 128], F32R, n=4)
    xb_r = Rot(P, pf + "xb", [128, 512], n=4)
    xbr_r = Rot(P, pf + "xbr", [128, 256], F32R, n=4)
    dt_r = Rot(P, pf + "dt", [128, 8], n=4)
    sm_r = Rot(P, pf + "sm", [128, 64], n=4)
    abc_r = Rot(P, pf + "abc", [128, 4, 128], n=4)
    X_r = Rot(P, pf + "X", [128, 4, 64], F32R, n=4)
    Xd_r = Rot(P, pf + "Xd", [128, 4, 64], F32R, n=4)
    pG_r = Rot(P, pf + "pG", [128, 512], n=1, psum=True)
    pR_r = Rot(P, pf + "pR", [128, 512], n=1, psum=True)
    pC_r = Rot(P, pf + "pC", [128, 512], n=1, psum=True)
    pY_r = Rot(P, pf + "pY", [128, 512], n=2, psum=True)
    pS_r = Rot(P, pf + "pS", [128, 512], n=1, psum=True)
    Gm_r = Rot(P, pf + "Gm", [128, 2, 128], n=4)
    df_r = Rot(P, pf + "df", [128, 4, 128], n=4)
    sc_r = Rot(P, pf + "scT", [128, 4, 128], F32R, n=4)
    yo_r = Rot(P, pf + "yo", [128, 4, 64], n=4)
    orders = [list(range(NT)), [1, 0] + list(range(NT - 1, 1, -1))]
    for d in range(2):
        P.i("dve", "memset", writes=[pf + "S%d" % d], ap=Sst[d][:].bitcast(F32), constant=0.0)

    def ssd_iter(d, c):
            t0 = c * 128
            bc, bck = bc_r.next()
            P.d(bc[:], XBCT[256:768, t0:t0 + 128].rearrange("(r p) t -> p r t", p=128), reads=["XBCT"], writes=[bck], q="pool")
            xb, xbk = xb_r.next()
            P.d(xb[:], XBtok[t0:t0 + 128, :], reads=["XBtok"], writes=[xbk])
            xbr, xbrk = xbr_r.next()
            P.i("act", "activation", reads=[xbk], writes=[xbrk], out=xbr[:], in_=xb[:, 256:512], func=AF.Copy)
            dt, dtk = dt_r.next()
            P.d(dt[:], TMS[t0:t0 + 128, 512:520], reads=["TMS"], writes=[dtk])
            sm, smk = sm_r.next()
            P.i("dve", "tensor_tensor", reads=[dtk, pf + "A"], writes=[smk], out=sm[:, 0:4], in0=dt[:, d * 4:d * 4 + 4], in1=A[:, d * 4:d * 4 + 4], op=ALU.mult)
            abc, abck = abc_r.next()
            P.i("dve", "tensor_copy", reads=[smk], writes=[abck], out=abc[:], in_=sm[:, 0:4].unsqueeze(2).to_broadcast([128, 4, 128]))
            X, Xk = X_r.next()
            P.i("pool", "tensor_tensor", reads=[xbk, dtk], writes=[Xk], out=X[:], in0=xb[:, 0:256].rearrange("p (h e) -> p h e", h=4),
                in1=dt[:, d * 4:d * 4 + 4].unsqueeze(2).to_broadcast([128, 4, 64]), op=ALU.mult)
            pG, pGk = pG_r.next()
            for g in range(2):
                P.i("pe", "matmul", reads=[bck], writes=[pGk], out=pG[:, g * 128:(g + 1) * 128], lhsT=bc[:, g, :], rhs=bc[:, 2 + g, :], start=True, stop=True)
            Gm, Gmk = Gm_r.next()
            P.i("dve", "tensor_tensor", reads=[pGk, pf + "tri"], writes=[Gmk], out=Gm[:], in0=pG[:, 0:256].rearrange("p (g l) -> p g l", g=2),
                in1=tri[:, d, :].unsqueeze(1).to_broadcast([128, 2, 128]), op=ALU.mult)
            pC, pCk = pC_r.next()
            P.i("pe", "matmul", reads=[smk, pf + "tri"], writes=[pCk], out=pC[:, 0:4], lhsT=tri[:, d, :], rhs=sm[:, 0:4], start=True, stop=True)
            pR, pRk = pR_r.next()
            for h in range(4):
                P.i("pe", "matmul", reads=[abck, pf + "tri"], writes=[pRk], out=pR[:, h * 128:(h + 1) * 128], lhsT=abc[:, h, :], rhs=tri[:, d, :], start=True, stop=True)
            P.i("dve", "tensor_copy", reads=[pCk], writes=[smk], out=sm[:, 4:8], in_=pC[:, 0:4])
            P.i("act", "activation", reads=[smk], writes=[smk], out=sm[:, 8:12], in_=sm[:, 4:8], func=AF.Exp)
            last = 127 if d == 0 else 0
            P.i("dve", "tensor_copy", reads=[pRk], writes=[smk], out=sm[:, 20:24], in_=pR[:].rearrange("p (h l) -> p h l", h=4)[:, :, last])
            P.i("dve", "tensor_tensor", reads=[smk], writes=[smk], out=sm[:, 12:16], in0=sm[:, 20:24], in1=sm[:, 4:8], op=ALU.subtract)
            P.i("act", "activation", reads=[smk], writes=[smk], out=sm[:, 12:16], in_=sm[:, 12:16], func=AF.Exp)
            P.i("act", "activation", reads=[smk], writes=[smk], out=sm[:, 16:20], in_=sm[:, 20:24], func=AF.Exp)
            Xd, Xdk = Xd_r.next()
            P.i("pool", "tensor_tensor", reads=[Xk, smk], writes=[Xdk], out=Xd[:], in0=X[:].bitcast(F32), in1=sm[:, 12:16].unsqueeze(2).to_broadcast([128, 4, 64]), op=ALU.mult)
            pY, pYk = pY_r.next()
            pS, pSk = pS_r.next()
            df, dfk = df_r.next()
            pR4 = pR[:].rearrange("p (h l) -> p h l", h=4)
            P.i("dve", "tensor_tensor", reads=[pRk, smk], writes=[dfk], out=df[:], in0=pR4, in1=sm[:, 4:8].unsqueeze(2).to_broadcast([128, 4, 128]), op=ALU.subtract)
            P.i("dve", "tensor_scalar", reads=[dfk], writes=[dfk], out=df[:], in0=df[:], scalar1=0.0, scalar2=None, op0=ALU.min)
            P.i("act", "activation", reads=[dfk], writes=[dfk], out=df[:], in_=df[:], func=AF.Exp)
            scT, scTk = sc_r.next()
            P.i("dve", "tensor_tensor", reads=[dfk, Gmk], writes=[scTk], out=scT[:].rearrange("p (g u) l -> p g u l", g=2), in0=df[:].rearrange("p (g u) l -> p g u l", g=2),
                in1=Gm[:].unsqueeze(2).to_broadcast([128, 2, 2, 128]), op=ALU.mult)
            for h in range(4):
                g = h // 2
                P.i("pe", "matmul", reads=[scTk, Xk], writes=[pYk], out=pY[:, h * 128:h * 128 + 64], lhsT=scT[:, h, :], rhs=X[:, h, :], start=True, stop=True)
                P.i("pe", "matmul", reads=[bck, pf + "S%d" % d], writes=[pYk], out=pY[:, h * 128 + 64:h * 128 + 128], lhsT=bc[:, 2 + g, :], rhs=Sst[d][:, h, :], start=True, stop=True)
                P.i("pe", "matmul", reads=[xbrk, Xdk], writes=[pSk], out=pS[:, h * 64:(h + 1) * 64], lhsT=xbr[:, g * 128:(g + 1) * 128], rhs=Xd[:, h, :], start=True, stop=True)
            pY4 = pY[:].rearrange("p (h t e) -> p h t e", h=4, t=2)
            yo, yok = yo_r.next()
            P.i("dve", "tensor_tensor", reads=[pYk, smk], writes=[yok], out=yo[:], in0=pY4[:, :, 1, :], in1=sm[:, 8:12].unsqueeze(2).to_broadcast([128, 4, 64]), op=ALU.mult)
            ya = Yacc[d][:, c, :].rearrange("p (h e) -> p h e", h=4)
            yk = pf + "Yacc%d/%d" % (d, c)
            P.i("dve", "tensor_tensor", reads=[pYk, yok], writes=[yk], out=ya, in0=pY4[:, :, 0, :], in1=yo[:], op=ALU.add)
            S4 = Sst[d][:].bitcast(F32)
            P.i("pool", "tensor_tensor", reads=[pf + "S%d" % d, smk], writes=[pf + "S%d" % d], out=Sst[d][:], in0=S4, in1=sm[:, 16:20].unsqueeze(2).to_broadcast([128, 4, 64]), op=ALU.mult)
            P.i("dve", "tensor_tensor", reads=[pf + "S%d" % d, pSk], writes=[pf + "S%d" % d], out=Sst[d][:], in0=S4, in1=pS[:, 0:256].rearrange("p (h e) -> p h e", h=4), op=ALU.add)
    for ci in range(NT):
        for d in range(2):
            ssd_iter(d, orders[d][ci])
    z_r = Rot(P, pf + "z", [128, 256], n=2)
    yt_r = Rot(P, pf + "yt", [128, 256], n=2)
    jk = P.sb(pf + "jk", [128, 256])
    pT_r = Rot(P, pf + "pT", [128, 512], n=2, psum=True)
    oT_r = Rot(P, pf + "oT", [128, 2, 128], n=2)
    for c in range(NT):
        t0 = c * 128
        xb, xbk = xb_r.next()
        P.d(xb[:], XBtok[t0:t0 + 128, :], reads=["XBtok"], writes=[xbk])
        z, zk = z_r.next()
        P.d(z[:], TMS[t0:t0 + 128, 256:512], reads=["TMS"], writes=[zk])
        yt, ytk = yt_r.next()
        P.i("pool", "tensor_tensor", reads=[xbk, pf + "dsk"], writes=[ytk], out=yt[:].rearrange("p (h e) -> p h e", h=4), in0=xb[:, 0:256].rearrange("p (h e) -> p h e", h=4),
            in1=dsk[:].unsqueeze(2).to_broadcast([128, 4, 64]), op=ALU.mult)
        P.i("dve", "tensor_tensor", reads=[ytk, pf + "Yacc0/%d" % c], writes=[ytk], out=yt[:], in0=yt[:], in1=Yacc[0][:, c, :], op=ALU.add)
        P.i("dve", "tensor_tensor", reads=[ytk, pf + "Yacc1/%d" % c], writes=[ytk], out=yt[:], in0=yt[:], in1=Yacc[1][:, c, :], op=ALU.add)
        P.i("act", "activation", reads=[zk], writes=[zk], out=z[:], in_=z[:], func=AF.Silu)
        P.i("dve", "tensor_tensor", reads=[ytk, zk], writes=[ytk], out=yt[:], in0=yt[:], in1=z[:], op=ALU.mult)
        sm, smk = sm_r.next()
        P.i("act", "activation", reads=[ytk], writes=[pf + "jk", smk], out=jk[:], in_=yt[:], func=AF.Square, accum_out=sm[:, 0:1])
        P.i("act", "activation", reads=[smk, pf + "eps"], writes=[smk], out=sm[:, 1:2], in_=sm[:, 0:1], func=AF.Ln, bias=epsc[:], scale=1.0 / 256)
        P.i("act", "activation", reads=[smk], writes=[smk], out=sm[:, 2:3], in_=sm[:, 1:2], func=AF.Exp, scale=-0.5)
        P.i("dve", "scalar_tensor_tensor", reads=[ytk, smk, pf + "ng"], writes=[ytk], out=yt[:], in0=yt[:], scalar=sm[:, 2:3], in1=ng[:], op0=ALU.mult, op1=ALU.mult)
        pT, pTk = pT_r.next()
        for q in range(2):
            P.i("pe", "transpose", reads=[ytk, pf + "ident"], writes=[pTk], out=pT[:, q * 128:(q + 1) * 128], in_=yt[:, q * 128:(q + 1) * 128], identity=ident[:])
        oT, oTk = oT_r.next()
        P.i("act", "activation", reads=[pTk], writes=[oTk], out=oT[:], in_=pT[:, 0:256].rearrange("p (q t) -> p q t", q=2), func=AF.Copy)
        P.d(CAT[512:768, t0:t0 + 128].rearrange("(q p) t -> p q t", p=128), oT[:], reads=[oTk], writes=[P.u("CAT")])


LAT_CHUNKS = [(256 + 512 * i, 512) for i in range(8)]
ALL_CHUNKS512 = [(512 * i, 512) for i in range(8)] + [(4096, 256)]


def build_program(scopes=False):
    nc = bass.Bass("TRN2", target_bir_lowering=False)
    P = Prog(nc)
    P.scopes = scopes
    Dm = Dram(nc, ext_in=["xin"])
    src = "xin"
    for l in range(2):
        last = (l == 1)
        ctx_out = not last
        for fi, fn in enumerate((lambda: phase_mod(P, Dm, l),
                   lambda: phase_a(P, Dm, l, src),
                   lambda: phase_b(P, Dm, l),
                   lambda: phase_c(P, Dm, l, ctx_out),
                   lambda: phase_d1(P, Dm, l),
                   lambda: phase_d2(P, Dm, l),
                   lambda: phase_e(P, Dm, l, ctx_out),
                   lambda: phase_f2(P, Dm, l, src, tok_chunks=(LAT_CHUNKS if last else None)),
                   lambda: phase_g2(P, Dm, l, (LAT_CHUNKS if last else CHUNKS), last))):
            P.begin_phase("L%d_%s" % (l, "mabcdDefg"[fi]))
            fn()
            P.end_phase()
        src = "XN%d" % l
    P.emit()
    return nc, P, Dm


_CACHE = {}


def kernel(**inputs):
    inp = {k: np.asarray(v) for k, v in inputs.items()}
    if "nc" not in _CACHE:
        _CACHE["nc"] = build_program()
    nc, P, Dm = _CACHE["nc"]
    n_cores = 8
    maps = []
    per_b = {}
    for cidx in range(n_cores):
        b = cidx % 4
        if b not in per_b:
            m = core_inputs(inp, b)
            per_b[b] = {k: v for k, v in m.items() if k in Dm.t}
        maps.append(per_b[b])
    res = run_bass_kernel_spmd(nc, maps, core_ids=list(range(n_cores)))
    out = np.stack([np.asarray(res.results[b]["out"]) for b in range(4)], 0)
    return out.astype(np.float32)
```

```python
import numpy as np
from contextlib import ExitStack
import concourse.bass as bass
import concourse.mybir as mybir
from concourse.bass_utils import run_bass_kernel_spmd

F32 = mybir.dt.float32
F32R = mybir.dt.float32r
I32 = mybir.dt.int32
AF = mybir.ActivationFunctionType
ALU = mybir.AluOpType
AX = mybir.AxisListType

LC, L, T, D = 256, 4096, 4352, 1024
NT = T // 128
EPS = 1e-6
CHUNKS = [(0, 256)] + [(256 + 512 * i, 512) for i in range(8)]
PERM = np.concatenate([np.arange(256, 768), np.arange(1800, 2312), np.arange(768, 1024), np.arange(1792, 1800),
                       np.arange(0, 256), np.arange(1024, 1792)])
TM_COLS = 1288
FM_OFF = 1288

ENGS = ["pe", "act", "dve", "pool", "sp"]
N_DMA_SEMS = 8


class Prog:
    def __init__(self, nc, same_engine_sync=True):
        self.nc = nc
        self.st = ExitStack()
        self.same = same_engine_sync
        self.ops = {e: [] for e in ENGS}
        self.sems = {}
        self.cnt = {}
        for e in ["pe", "act", "dve", "pool"]:
            self.sems[e] = self.st.enter_context(nc.semaphore("s_" + e))
            self.cnt[e] = 0
        for q in ["sp", "pool"]:
            for i in range(N_DMA_SEMS):
                nm = "d_%s%d" % (q, i)
                self.sems[nm] = self.st.enter_context(nc.semaphore(nm))
                self.cnt[nm] = 0
        self.drr = {"sp": 0, "pool": 0}
        self.waited = {e: {} for e in ENGS}
        self.last_w = {}
        self.readers = {}
        self.n_ops = 0
        self.final = []
        self.uid = 0
        self.pst = None
        self.barrier = {e: {} for e in ENGS}
        self.children = {}
        self.known = set()
        self.scopes = False
        self.bound_reg = None
        self.pending_noinc = {e: False for e in ENGS}
        self.psum_keys = set()

    def begin_phase(self, name=None):
        self.pst = ExitStack()
        self.phase_name = name

    def end_phase(self):
        self.pst.close()
        self.pst = None
        assert not any(self.pending_noinc.values()), self.pending_noinc
        snap = {s: v for s, v in self.cnt.items() if v > 0}
        for e in ENGS:
            self.barrier[e] = dict(snap)

    def bound(self):
        if self.bound_reg is None:
            self.bound_reg = self.nc.gpsimd.alloc_register("bc4095")
        return self.bound_reg

    def sb(self, name, shape, dtype=F32):
        return (self.pst or self.st).enter_context(self.nc.sbuf_tensor(name, list(shape), dtype))

    def ps(self, name, shape, dtype=F32):
        return (self.pst or self.st).enter_context(self.nc.psum_tensor(name, list(shape), dtype))

    def _related(self, k):
        rel = [k]
        parts = k.split("/")
        for n in range(1, len(parts)):
            rel.append("/".join(parts[:n]))
        rel.extend(self.children.get(k, ()))
        return rel

    def _register(self, k):
        if k in self.known:
            return
        self.known.add(k)
        parts = k.split("/")
        for n in range(1, len(parts)):
            self.children.setdefault("/".join(parts[:n]), set()).add(k)

    def _deps(self, eng, reads, writes):
        deps = {}

        def add(s, v):
            if v > deps.get(s, 0):
                deps[s] = v

        for k in list(reads) + list(writes):
            self._register(k)
        for k in reads:
            for kk in self._related(k):
                t = self.last_w.get(kk)
                if t is not None:
                    add(*t)
        for k in writes:
            for kk in self._related(k):
                t = self.last_w.get(kk)
                if t is not None:
                    add(*t)
                for s_, v_ in self.readers.get(kk, {}).items():
                    add(s_, v_)
        if self.barrier[eng]:
            for s_, v_ in self.barrier[eng].items():
                add(s_, v_)
            self.barrier[eng] = {}
        out = []
        for s, v in deps.items():
            if s == eng and (eng == "pe" or not self.same):
                continue
            if v > self.waited[eng].get(s, 0):
                self.waited[eng][s] = v
                out.append((s, v))
        return out

    def _commit(self, tok, reads, writes):
        for k in writes:
            self.last_w[k] = tok
            self.readers[k] = {}
        for k in reads:
            if k in writes:
                continue
            rd = self.readers.setdefault(k, {})
            if tok[1] > rd.get(tok[0], 0):
                rd[tok[0]] = tok[1]

    def op(self, eng, fn, reads=(), writes=(), noinc=False):
        pr = [k for k in reads if k in self.psum_keys]
        if pr:
            reads = [k for k in reads if k not in self.psum_keys]
            writes = list(writes) + pr
        waits = self._deps(eng, reads, writes)
        if noinc:
            tok = (eng, self.cnt[eng] + 1)
            self.pending_noinc[eng] = True
            self.ops[eng].append((waits, fn, tok, 0, getattr(self, "phase_name", None)))
        else:
            self.cnt[eng] += 1
            tok = (eng, self.cnt[eng])
            self.pending_noinc[eng] = False
            self.ops[eng].append((waits, fn, tok, 1, getattr(self, "phase_name", None)))
        self._commit(tok, reads, writes)
        self.n_ops += 1
        return tok

    def u(self, base):
        self.uid += 1
        return "%s/%d" % (base, self.uid)

    def i(self, eng, name, reads=(), writes=(), noinc=False, **kw):
        if name == "matmul" and kw.get("stop") is False:
            noinc = True
        return self.op(eng, lambda e: getattr(e, name)(**kw), reads, writes, noinc=noinc)

    def d(self, out, in_, reads=(), writes=(), q="sp", final=False):
        return self.dma(lambda e: e.dma_start(out=out, in_=in_), reads, writes, q=q, final=final)

    def dma(self, fn, reads=(), writes=(), q="sp", final=False):
        waits = self._deps(q, reads, writes)
        i = self.drr[q]
        self.drr[q] = (i + 1) % N_DMA_SEMS
        nm = "d_%s%d" % (q, i)
        prev = self.cnt[nm]
        if prev > self.waited[q].get(nm, 0):
            self.waited[q][nm] = prev
            waits.append((nm, prev))
        self.cnt[nm] += 16
        tok = (nm, self.cnt[nm])
        self.ops[q].append((waits, fn, tok, 16, getattr(self, "phase_name", None)))
        self._commit(tok, reads, writes)
        self.n_ops += 1
        if final:
            self.final.append(tok)
        return tok

    def emit(self):
        nc = self.nc
        fin = list(self.final)
        engmap = {"pe": "tensor", "act": "scalar", "dve": "vector", "pool": "gpsimd", "sp": "sync"}
        with nc.Block() as block:
            for e in ENGS:
                lst = self.ops[e]
                extra = fin if e == "sp" else []
                if not lst and not extra:
                    continue

                def body(engine, lst=lst, extra=extra, e=e):
                    cur = None
                    scope = None
                    if e == "pool" and self.bound_reg is not None:
                        engine.reg_mov(self.bound_reg, 4095)
                    for (waits, fn, tok, amt, ph) in lst:
                        if self.scopes and ph != cur:
                            if scope is not None:
                                scope.__exit__(None, None, None)
                            scope = nc.named_scope(ph or "none")
                            scope.__enter__()
                            cur = ph
                        for (s, v) in waits:
                            engine.wait_ge(self.sems[s], v)
                        ins = fn(engine)
                        if amt:
                            ins.then_inc(self.sems[tok[0]], amt)
                    if scope is not None:
                        scope.__exit__(None, None, None)
                    for (s, v) in extra:
                        engine.wait_ge(self.sems[s], v)

                getattr(block, engmap[e])(body)
        self.st.close()


class Rot:
    def __init__(self, P, name, shape, dtype=F32, n=2, psum=False):
        self.bufs = [(P.ps if psum else P.sb)("%s%d" % (name, i), shape, dtype) for i in range(n)]
        self.keys = ["%s%d" % (name, i) for i in range(n)]
        self.i = 0
        if psum:
            P.psum_keys.update(self.keys)

    def next(self):
        j = self.i % len(self.bufs)
        self.i += 1
        return self.bufs[j], self.keys[j]


class Dram:
    def __init__(self, nc, ext_in=(), ext_out=()):
        self.nc, self.t = nc, {}
        self.ext_in, self.ext_out = set(ext_in), set(ext_out)

    def get(self, name, shape=None, dtype=F32, kind=None):
        if name not in self.t:
            if kind is None:
                kind = "ExternalInput" if name in self.ext_in else ("ExternalOutput" if name in self.ext_out else "Internal")
            self.t[name] = self.nc.dram_tensor(name, list(shape), dtype, kind=kind).ap()
        return self.t[name]


def phase_mod(P, Dm, l):
    nc = P.nc
    cvec = Dm.get("cvec", [128, 8, 2], kind="ExternalInput")
    ada_w = Dm.get("ada_w%d" % l, [128, 8, 6144], kind="ExternalInput")
    ada_b = Dm.get("ada_b%d" % l, [1, 6144], kind="ExternalInput")
    modrow = Dm.get("modrow%d" % l, [2, 6144])
    pf = "m%d_" % l
    cv = P.sb(pf + "cv", [128, 8, 2])
    sg = P.sb(pf + "sg", [128, 8, 2])
    sc = P.sb(pf + "sc", [128, 8, 128], F32R)
    ab = P.sb(pf + "ab", [2, 6144])
    mr = P.sb(pf + "mr", [2, 6144])
    wch = Rot(P, pf + "w", [128, 8, 512], F32R, n=2)
    pm = Rot(P, pf + "pm", [128, 512], F32, n=2, psum=True)
    P.dma(lambda e: e.dma_start(out=cv[:], in_=cvec[:, :, :]), writes=[pf + "cv"])
    P.dma(lambda e: e.dma_start(out=ab[:], in_=ada_b[0:1, :].to_broadcast([2, 6144])), writes=[pf + "ab"])
    P.op("act", lambda e: e.activation(out=sg[:], in_=cv[:], func=AF.Sigmoid), reads=[pf + "cv"], writes=[pf + "sg"])
    P.op("dve", lambda e: e.memset(sc[:].bitcast(F32), 0.0), writes=[pf + "sc"])
    P.op("dve", lambda e: e.tensor_tensor(out=sc[:, :, 0:2], in0=cv[:], in1=sg[:], op=ALU.mult), reads=[pf + "cv", pf + "sg"], writes=[pf + "sc"])
    for j in range(12):
        w, wk = wch.next()
        P.dma(lambda e, w=w, j=j: e.dma_start(out=w[:], in_=ada_w[:, :, j * 512:(j + 1) * 512]), writes=[wk], q="pool")
        pt, pk = pm.next()
        for k in range(8):
            P.op("pe", lambda e, w=w, pt=pt, k=k: e.matmul(pt[:], sc[:, k, :], w[:, k, :], start=(k == 0), stop=(k == 7)),
                 reads=[wk, pf + "sc"], writes=[pk], noinc=(k != 7))
        P.op("dve", lambda e, pt=pt, j=j: e.tensor_tensor(out=mr[:, j * 512:(j + 1) * 512], in0=pt[0:2, :], in1=ab[:, j * 512:(j + 1) * 512], op=ALU.add),
             reads=[pk, pf + "ab"], writes=[pf + "mr"])
    P.dma(lambda e: e.dma_start(out=modrow[:, :], in_=mr[:]), reads=[pf + "mr"], writes=["modrow%d" % l])


def load_bcast(P, dst, dkey, src_row, reads=()):
    n = src_row.shape[-1]
    P.dma(lambda e: e.dma_start(out=dst, in_=src_row.to_broadcast([128, n])), reads=list(reads), writes=[dkey])


def phase_a(P, Dm, l, src_name, chunks=None, stop=99):
    nc = P.nc
    pf = "a%d_" % l
    xsrc = Dm.get(src_name, [T, D])
    w_in = Dm.get("w_in%d" % l, [128, 8, 2312], kind="ExternalInput")
    modrow = Dm.get("modrow%d" % l, [2, 6144])
    n1g = Dm.get("norm1_g%d" % l, [1, D], kind="ExternalInput")
    qkg = Dm.get("qkg%d" % l, [1, 384], kind="ExternalInput")
    dtb = Dm.get("dtb%d" % l, [1, 8], kind="ExternalInput")
    ropec = Dm.get("rope_cos", [L, 384], kind="ExternalInput")
    ropes = Dm.get("rope_sin", [L, 384], kind="ExternalInput")
    ident_d = Dm.get("ident", [128, 128], kind="ExternalInput")
    FMT = Dm.get("FMT", [1024, T])
    QKT = Dm.get("QKT", [768, T])
    TMS = Dm.get("TMS", [T, 520])
    mk = "modrow%d" % l

    ident = P.sb(pf + "ident", [128, 128])
    P.dma(lambda e: e.dma_start(out=ident[:], in_=ident_d[:, :]), writes=[pf + "ident"])
    win = P.sb(pf + "win", [128, 8, 2312], F32R)
    for k in range(8):
        P.dma(lambda e, k=k: e.dma_start(out=win[:, k, :], in_=w_in[:, k, :]), writes=[pf + "win/%d" % k], q="pool")
    wkeys = [pf + "win/%d" % k for k in range(8)]
    G = [P.sb(pf + "G%d" % r, [128, D]) for r in range(2)]
    SH = [P.sb(pf + "SH%d" % r, [128, D]) for r in range(2)]
    gn = P.sb(pf + "gn", [128, D])
    load_bcast(P, gn[:], pf + "gn", n1g[0:1, :])
    for r in range(2):
        load_bcast(P, SH[r][:], pf + "SH%d" % r, modrow[r:r + 1, 0:1024], reads=[mk])
        load_bcast(P, G[r][:], pf + "G%d" % r, modrow[r:r + 1, 1024:2048], reads=[mk])
        P.op("dve", lambda e, r=r: e.scalar_tensor_tensor(out=G[r][:], in0=G[r][:], scalar=1.0, in1=gn[:], op0=ALU.add, op1=ALU.mult),
             reads=[pf + "G%d" % r, pf + "gn"], writes=[pf + "G%d" % r])
    qkgb = P.sb(pf + "qkgb", [128, 384])
    load_bcast(P, qkgb[:], pf + "qkgb", qkg[0:1, :])
    dtbb = P.sb(pf + "dtbb", [128, 8])
    load_bcast(P, dtbb[:], pf + "dtbb", dtb[0:1, :])
    epsc = P.sb(pf + "eps", [128, 1])
    P.op("dve", lambda e: e.memset(epsc[:], EPS), writes=[pf + "eps"])
    onec = P.sb(pf + "one", [128, 1])
    P.op("dve", lambda e: e.memset(onec[:], 1.0), writes=[pf + "one"])

    xt_r = Rot(P, pf + "xt", [128, D], n=3)
    junk = P.sb(pf + "junk", [128, D])
    st_r = Rot(P, pf + "st", [128, 32], n=3)
    h_r = Rot(P, pf + "h", [128, D], n=2)
    tp_r = Rot(P, pf + "tp", [128, 1024], n=1, psum=True)
    hT_r = Rot(P, pf + "hT", [128, 8, 512], F32R, n=2)
    pj_r = Rot(P, pf + "pj", [128, 512], n=3, psum=True)
    qk_r = Rot(P, pf + "qk", [128, 12, 64], n=2)
    sq_r = Rot(P, pf + "sq", [128, 6, 64], n=2)
    rp_r = Rot(P, pf + "rp", [128, 24, 2, 16], n=2)
    tmp_r = Rot(P, pf + "tmp", [128, 24, 16], n=2)
    cs_r = Rot(P, pf + "cs", [128, 2, 384], n=2)
    tq_r = Rot(P, pf + "tq", [128, 1024], n=1, psum=True)
    qT_r = Rot(P, pf + "qT", [128, 6, 128], n=2)
    tm_r = Rot(P, pf + "tm", [128, 520], n=2)
    fm_r = Rot(P, pf + "fm", [128, 512], n=1, psum=True)
    fo_r = Rot(P, pf + "fo", [128, 512], n=3)

    for (c0, cn) in (chunks or CHUNKS):
        hT, hTk = hT_r.next()
        ntile = cn // 128
        for i in range(ntile):
            t0 = c0 + i * 128
            is_ctx = t0 < LC
            r = 1 if is_ctx else 0
            xt, xk = xt_r.next()
            P.dma(lambda e, xt=xt, t0=t0: e.dma_start(out=xt[:], in_=xsrc[t0:t0 + 128, :]), reads=[src_name], writes=[xk])
            st, sk = st_r.next()
            P.op("act", lambda e, xt=xt, st=st: e.activation(out=junk[:], in_=xt[:], func=AF.Square, accum_out=st[:, 0:1]),
                 reads=[xk], writes=[pf + "junk", sk])
            P.op("act", lambda e, st=st: e.activation(out=st[:, 1:2], in_=st[:, 0:1], func=AF.Ln, bias=epsc[:], scale=1.0 / D),
                 reads=[sk, pf + "eps"], writes=[sk])
            P.op("act", lambda e, st=st: e.activation(out=st[:, 2:3], in_=st[:, 1:2], func=AF.Exp, scale=-0.5), reads=[sk], writes=[sk])
            h, hk = h_r.next()
            P.op("dve", lambda e, xt=xt, st=st, h=h, r=r: e.scalar_tensor_tensor(out=h[:], in0=xt[:], scalar=st[:, 2:3], in1=G[r][:], op0=ALU.mult, op1=ALU.mult),
                 reads=[xk, sk, pf + "G%d" % r], writes=[hk])
            P.op("pool", lambda e, h=h, r=r: e.tensor_tensor(out=h[:], in0=h[:], in1=SH[r][:], op=ALU.add),
                 reads=[hk, pf + "SH%d" % r], writes=[hk])
            if stop == 1:
                continue
            tp, tpk = tp_r.next()
            for k in range(8):
                P.op("pe", lambda e, h=h, tp=tp, k=k: e.transpose(tp[:, k * 128:(k + 1) * 128], h[:, k * 128:(k + 1) * 128], ident[:]),
                     reads=[hk, pf + "ident"], writes=[tpk], noinc=(k != 7))
            P.op("act", lambda e, hT=hT, tp=tp, i=i: e.activation(out=hT[:, :, i * 128:(i + 1) * 128], in_=tp[:].rearrange("p (k t) -> p k t", k=8), func=AF.Copy),
                 reads=[tpk], writes=[hTk + "/%d" % i])
            if stop == 2:
                continue
            pjs = []
            for (o, n) in [(0, 512), (512, 512), (1024, 264)]:
                pj, pjk = pj_r.next()
                for k in range(8):
                    P.op("pe", lambda e, pj=pj, hT=hT, k=k, o=o, n=n, i=i: e.matmul(pj[:, 0:n], hT[:, k, i * 128:(i + 1) * 128], win[:, k, o:o + n], start=(k == 0), stop=(k == 7)),
                         reads=[hTk + "/%d" % i, wkeys[k]], writes=[pjk], noinc=(k != 7))
                pjs.append((pj, pjk))
            (pA, pAk), (pB, pBk), (pC, pCk) = pjs
            if stop == 3:
                continue
            qk, qkk = qk_r.next()
            sq, sqk = sq_r.next()
            pA6 = pA[:, 0:384].rearrange("p (h d) -> p h d", h=6)
            P.op("act", lambda e, sq=sq, pA6=pA6: e.activation(out=sq[:], in_=pA6, func=AF.Square), reads=[pAk], writes=[sqk])
            P.op("dve", lambda e, sq=sq, st=st: e.tensor_reduce(out=st[:, 4:10], in_=sq[:], axis=AX.X, op=ALU.add), reads=[sqk], writes=[sk])
            P.op("act", lambda e, st=st: e.activation(out=st[:, 4:10], in_=st[:, 4:10], func=AF.Ln, bias=epsc[:], scale=1.0 / 64), reads=[sk, pf + "eps"], writes=[sk])
            P.op("act", lambda e, st=st: e.activation(out=st[:, 10:16], in_=st[:, 4:10], func=AF.Exp, scale=-0.5), reads=[sk], writes=[sk])
            P.op("dve", lambda e, sq=sq, pA6=pA6, st=st: e.tensor_tensor(out=sq[:], in0=pA6, in1=st[:, 10:16].unsqueeze(2).to_broadcast([128, 6, 64]), op=ALU.mult),
                 reads=[pAk, sk], writes=[sqk])
            P.op("pool", lambda e, sq=sq, qk=qk: e.tensor_tensor(out=qk[:, 0:6, :], in0=sq[:], in1=qkgb[:].rearrange("p (h d) -> p h d", h=6), op=ALU.mult),
                 reads=[sqk, pf + "qkgb"], writes=[qkk + "/a"])
            P.op("act", lambda e, qk=qk, pB=pB: e.activation(out=qk[:, 6:12, :], in_=pB[:, 0:384].rearrange("p (h d) -> p h d", h=6), func=AF.Copy),
                 reads=[pBk], writes=[qkk + "/b"])
            if stop == 4:
                continue
            tm, tmk = tm_r.next()
            P.op("act", lambda e, tm=tm, pA=pA: e.activation(out=tm[:, 0:128], in_=pA[:, 384:512], func=AF.Copy), reads=[pAk], writes=[tmk + "/a"])
            P.op("act", lambda e, tm=tm, pB=pB: e.activation(out=tm[:, 128:256], in_=pB[:, 384:512], func=AF.Copy), reads=[pBk], writes=[tmk + "/b"])
            P.op("act", lambda e, tm=tm, pC=pC: e.activation(out=tm[:, 256:512], in_=pC[:, 0:256], func=AF.Copy), reads=[pCk], writes=[tmk + "/c"])
            P.op("dve", lambda e, tm=tm, pC=pC: e.tensor_tensor(out=tm[:, 512:520], in0=pC[:, 256:264], in1=dtbb[:], op=ALU.add), reads=[pCk, pf + "dtbb"], writes=[tmk + "/d"])
            P.op("act", lambda e, st=st, tm=tm: e.activation(out=st[:, 16:24], in_=tm[:, 512:520], func=AF.Abs), reads=[tmk + "/d", sk], writes=[sk])
            P.op("act", lambda e, st=st: e.activation(out=st[:, 16:24], in_=st[:, 16:24], func=AF.Exp, scale=-1.0), reads=[sk], writes=[sk])
            P.op("act", lambda e, st=st: e.activation(out=st[:, 16:24], in_=st[:, 16:24], func=AF.Ln, bias=onec[:]), reads=[sk], writes=[sk])
            P.op("dve", lambda e, st=st, tm=tm: e.scalar_tensor_tensor(out=tm[:, 512:520], in0=tm[:, 512:520], scalar=0.0, in1=st[:, 16:24], op0=ALU.max, op1=ALU.add),
                 reads=[tmk + "/d", sk], writes=[tmk + "/d"])
            P.dma(lambda e, tm=tm, t0=t0: e.dma_start(out=TMS[t0:t0 + 128, :], in_=tm[:]), reads=[tmk], writes=[P.u("TMS")])
            if stop == 5:
                continue
            rp, rpk = rp_r.next()
            if is_ctx:
                P.op("pool", lambda e, rp=rp, qk=qk: e.tensor_copy(out=rp[:].rearrange("p (h a) b f -> p h (a b f)", a=2), in_=qk[:]),
                     reads=[qkk + "/a", qkk + "/b"], writes=[rpk])
            else:
                cs, csk = cs_r.next()
                tl = t0 - LC
                P.dma(lambda e, cs=cs, tl=tl: e.dma_start(out=cs[:, 0, :], in_=ropec[tl:tl + 128, :]), writes=[csk + "/c"])
                P.dma(lambda e, cs=cs, tl=tl: e.dma_start(out=cs[:, 1, :], in_=ropes[tl:tl + 128, :]), writes=[csk + "/s"])
                qv = qk[:].rearrange("p h (a b f) -> p (h a) b f", a=2, b=2)
                x1, x2 = qv[:, :, 0, :], qv[:, :, 1, :]
                cb = cs[:, 0, :].rearrange("p (g f) -> p g f", f=16)
                sb_ = cs[:, 1, :].rearrange("p (g f) -> p g f", f=16)
                tmp, tmpk = tmp_r.next()
                qkr = [qkk + "/a", qkk + "/b"]
                P.op("dve", lambda e, rp=rp, x1=x1, cb=cb: e.tensor_tensor(out=rp[:, :, 0, :], in0=x1, in1=cb, op=ALU.mult), reads=qkr + [csk + "/c"], writes=[rpk + "/0"])
                P.op("pool", lambda e, tmp=tmp, x2=x2, sb_=sb_: e.tensor_tensor(out=tmp[:], in0=x2, in1=sb_, op=ALU.mult), reads=qkr + [csk + "/s"], writes=[tmpk])
                P.op("dve", lambda e, rp=rp, tmp=tmp: e.tensor_tensor(out=rp[:, :, 0, :], in0=rp[:, :, 0, :], in1=tmp[:], op=ALU.subtract), reads=[rpk + "/0", tmpk], writes=[rpk + "/0"])
                P.op("dve", lambda e, rp=rp, x2=x2, cb=cb: e.tensor_tensor(out=rp[:, :, 1, :], in0=x2, in1=cb, op=ALU.mult), reads=qkr + [csk + "/c"], writes=[rpk + "/1"])
                P.op("pool", lambda e, tmp=tmp, x1=x1, sb_=sb_: e.tensor_tensor(out=tmp[:], in0=x1, in1=sb_, op=ALU.mult), reads=qkr + [csk + "/s"], writes=[tmpk])
                P.op("dve", lambda e, rp=rp, tmp=tmp: e.tensor_tensor(out=rp[:, :, 1, :], in0=rp[:, :, 1, :], in1=tmp[:], op=ALU.add), reads=[rpk + "/1", tmpk], writes=[rpk + "/1"])
            rkeys = [rpk] if is_ctx else [rpk + "/0", rpk + "/1"]
            if stop == 6:
                continue
            rpf = rp[:].rearrange("p g b f -> p (g b f)")
            tq, tqk = tq_r.next()
            for j in range(6):
                P.op("pe", lambda e, tq=tq, rpf=rpf, j=j: e.transpose(tq[:, j * 128:(j + 1) * 128], rpf[:, j * 128:(j + 1) * 128], ident[:]),
                     reads=rkeys + [pf + "ident"], writes=[tqk], noinc=(j != 5))
            qT, qTk = qT_r.next()
            P.op("act", lambda e, qT=qT, tq=tq: e.activation(out=qT[:], in_=tq[:, 0:768].rearrange("p (j t) -> p j t", j=6), func=AF.Copy), reads=[tqk], writes=[qTk])
            P.dma(lambda e, qT=qT, t0=t0: e.dma_start(out=QKT[:, t0:t0 + 128].rearrange("(j p) t -> p j t", p=128), in_=qT[:]), reads=[qTk], writes=[P.u("QKT")])
        if stop <= 7:
            continue
        hkeys = [hTk + "/%d" % i for i in range(ntile)]
        for j in range(8):
            fm, fmk = fm_r.next()
            for k in range(8):
                P.op("pe", lambda e, fm=fm, hT=hT, k=k, j=j, cn=cn: e.matmul(fm[:, 0:cn], win[:, k, FM_OFF + j * 128:FM_OFF + (j + 1) * 128], hT[:, k, 0:cn], start=(k == 0), stop=(k == 7)),
                     reads=hkeys + [wkeys[k]], writes=[fmk], noinc=(k != 7))
            fo, fok = fo_r.next()
            eng = "act" if j % 2 == 0 else "dve"
            if eng == "act":
                P.op("act", lambda e, fo=fo, fm=fm, cn=cn: e.activation(out=fo[:, 0:cn], in_=fm[:, 0:cn], func=AF.Copy), reads=[fmk], writes=[fok])
            else:
                P.op("dve", lambda e, fo=fo, fm=fm, cn=cn: e.tensor_copy(out=fo[:, 0:cn], in_=fm[:, 0:cn]), reads=[fmk], writes=[fok])
            P.dma(lambda e, fo=fo, j=j, c0=c0, cn=cn: e.dma_start(out=FMT[j * 128:(j + 1) * 128, c0:c0 + cn], in_=fo[:, 0:cn]), reads=[fok], writes=[P.u("FMT")])


def phase_g2(P, Dm, l, tok_chunks, final):
    pf = "G%d_" % l
    ntf = sum(cn for _, cn in tok_chunks) // 128
    NB = 2 * ntf + 32
    X1 = Dm.get("X1", [T, D])
    BUF = Dm.get("BUF%d" % l, [NB * 128, D])
    OBUF = Dm.get("OBUF%d" % l, [NB * 128, D])
    IDXW = Dm.get("IDXW%d" % l, [128, NB], I32)
    DEST = Dm.get("DEST%d" % l, [128, ntf * 2], I32)
    WWd = Dm.get("WWd%d" % l, [128, ntf * 2])
    WGUA = Dm.get("moe_gua%d" % l, [32, 128, 4, 1024], kind="ExternalInput")
    WGUB = Dm.get("moe_gub%d" % l, [32, 128, 4, 1024], kind="ExternalInput")
    WD = Dm.get("moe_d%d" % l, [32, 128, 4, 1024], kind="ExternalInput")
    ident_d = Dm.get("ident", [128, 128], kind="ExternalInput")
    modrow = Dm.get("modrow%d" % l, [2, 6144])
    mk = "modrow%d" % l
    ident = P.sb(pf + "ident", [128, 128]); P.d(ident[:], ident_d[:, :], writes=[pf + "ident"])
    idxw = P.sb(pf + "idxw", [128, NB], I32); P.d(idxw[:], IDXW[:, :], reads=["IDXW%d" % l], writes=[pf + "idxw"])
    dest = P.sb(pf + "dest", [128, ntf * 2], I32); P.d(dest[:], DEST[:, :], reads=["DEST%d" % l], writes=[pf + "dest"])
    ww = P.sb(pf + "ww", [128, ntf * 2]); P.d(ww[:], WWd[:, :], reads=["WWd%d" % l], writes=[pf + "ww"])
    wgu_r = Rot(P, pf + "wgu", [128, 8, 1024], F32R, n=2)
    wd_r = Rot(P, pf + "wd", [128, 4, 1024], F32R, n=2)
    xs_r = Rot(P, pf + "xs", [128, D], n=3)
    tp_r = Rot(P, pf + "tp", [128, 1024], n=1, psum=True)
    xT_r = Rot(P, pf + "xT", [128, 8, 128], F32R, n=2)
    pg_r = Rot(P, pf + "pg", [128, 512], n=1, psum=True)
    pu_r = Rot(P, pf + "pu", [128, 512], n=1, psum=True)
    sg_r = Rot(P, pf + "sg", [128, 512], n=2)
    hu_r = Rot(P, pf + "hu", [128, 512], n=2)
    ph_r = Rot(P, pf + "ph", [128, 512], n=1, psum=True)
    hT_r = Rot(P, pf + "hT", [128, 4, 128], F32R, n=2)
    po_r = Rot(P, pf + "po", [128, 1024], n=1, psum=True)
    ob_r = Rot(P, pf + "ob", [128, D], n=2)
    WGUAf = WGUA.rearrange("e p k n -> (e p) (k n)")
    WGUBf = WGUB.rearrange("e p k n -> (e p) (k n)")
    WDf = WD.rearrange("e p k n -> (e p) (k n)")
    st1 = {}
    breg = P.bound()
    hNB = NB // 2
    seq = []
    for i_ in range(hNB):
        seq += [i_, hNB + i_]

    def load_gu(b):
        off = bass.IndirectOffsetOnAxis(ap=idxw[:, seq[b]:seq[b] + 1], axis=0)
        wgu, wguk = wgu_r.next()
        P.dma(lambda e, wgu=wgu, off=off: e.indirect_dma_start(out=wgu[:, 0:4, :].rearrange("p k n -> p (k n)"), out_offset=None, in_=WGUAf, in_offset=off, bounds_check=breg, oob_is_err=False), reads=[pf + "idxw"], writes=[wguk + "/a"], q="pool")
        P.dma(lambda e, wgu=wgu, off=off: e.indirect_dma_start(out=wgu[:, 4:8, :].rearrange("p k n -> p (k n)"), out_offset=None, in_=WGUBf, in_offset=off, bounds_check=breg, oob_is_err=False), reads=[pf + "idxw"], writes=[wguk + "/b"], q="pool")
        st1[("gu", b)] = (wgu, wguk)

    def load_d(b):
        off = bass.IndirectOffsetOnAxis(ap=idxw[:, seq[b]:seq[b] + 1], axis=0)
        wd, wdk = wd_r.next()
        P.dma(lambda e, wd=wd, off=off: e.indirect_dma_start(out=wd[:].rearrange("p k n -> p (k n)"), out_offset=None, in_=WDf, in_offset=off, bounds_check=breg, oob_is_err=False), reads=[pf + "idxw"], writes=[wdk], q="pool")
        st1[("d", b)] = (wd, wdk)

    def s1a(b):
        xs, xsk = xs_r.next()
        P.d(xs[:], BUF[seq[b] * 128:(seq[b] + 1) * 128, :], reads=["BUF%d" % l, "BUFz%d" % l], writes=[xsk])
        tp, tpk = tp_r.next()
        for k in range(8):
            P.i("pe", "transpose", reads=[xsk, pf + "ident"], writes=[tpk], noinc=(k != 7), out=tp[:, k * 128:(k + 1) * 128], in_=xs[:, k * 128:(k + 1) * 128], identity=ident[:])
        xT, xTk = xT_r.next()
        P.i("act", "activation", reads=[tpk], writes=[xTk + "/0"], out=xT[:, 0:4, :], in_=tp[:, 0:512].rearrange("p (k t) -> p k t", k=4), func=AF.Copy)
        P.i("dve", "tensor_copy", reads=[tpk], writes=[xTk + "/1"], out=xT[:, 4:8, :], in_=tp[:, 512:1024].rearrange("p (k t) -> p k t", k=4))
        st1[("xT", b)] = (xT, xTk)

    def s1b(b):
        (wgu, wguk) = st1.pop(("gu", b))
        (xT, xTk) = st1.pop(("xT", b))
        pg, pgk = pg_r.next(); pu, puk = pu_r.next()
        for k in range(8):
            P.i("pe", "matmul", reads=[xTk, wguk + ("/a" if k < 4 else "/b")], writes=[pgk], out=pg[:], lhsT=xT[:, k, :], rhs=wgu[:, k, 0:512], start=(k == 0), stop=(k == 7))
        for k in range(8):
            P.i("pe", "matmul", reads=[xTk, wguk + ("/a" if k < 4 else "/b")], writes=[puk], out=pu[:], lhsT=xT[:, k, :], rhs=wgu[:, k, 512:1024], start=(k == 0), stop=(k == 7))
        sg, sgk = sg_r.next()
        P.i("act", "activation", reads=[pgk], writes=[sgk], out=sg[:], in_=pg[:], func=AF.Silu)
        hu, huk = hu_r.next()
        P.i("dve", "tensor_tensor", reads=[sgk, puk], writes=[huk], out=hu[:], in0=sg[:], in1=pu[:], op=ALU.mult)
        st1[("h", b)] = (hu, huk)

    def stage2(b):
        (hu, huk) = st1.pop(("h", b))
        (wd, wdk) = st1.pop(("d", b))
        ph, phk = ph_r.next()
        for hc in range(4):
            P.i("pe", "transpose", reads=[huk, pf + "ident"], writes=[phk], noinc=(hc != 3), out=ph[:, hc * 128:(hc + 1) * 128], in_=hu[:, hc * 128:(hc + 1) * 128], identity=ident[:])
        hT, hTk = hT_r.next()
        P.i("act", "activation", reads=[phk], writes=[hTk], out=hT[:], in_=ph[:].rearrange("p (k t) -> p k t", k=4), func=AF.Copy)
        po, pok = po_r.next()
        for hf in range(2):
            for hc in range(4):
                P.i("pe", "matmul", reads=[hTk, wdk], writes=[pok], out=po[:, hf * 512:(hf + 1) * 512], lhsT=hT[:, hc, :], rhs=wd[:, hc, hf * 512:(hf + 1) * 512], start=(hc == 0), stop=(hc == 3))
        ob, obk = ob_r.next()
        P.i("act", "activation", reads=[pok], writes=[obk + "/0"], out=ob[:, 0:512], in_=po[:, 0:512], func=AF.Copy)
        P.i("dve", "tensor_copy", reads=[pok], writes=[obk + "/1"], out=ob[:, 512:1024], in_=po[:, 512:1024])
        P.d(OBUF[seq[b] * 128:(seq[b] + 1) * 128, :], ob[:], reads=[obk], writes=[P.u("OBUF%d" % l)])

    load_gu(0); load_d(0); load_gu(1); load_d(1)
    s1a(0); s1b(0)
    if NB > 2:
        load_gu(2)
    s1a(1)
    for b in range(NB):
        stage2(b)
        if b + 2 < NB:
            load_d(b + 2)
        if b + 1 < NB:
            s1b(b + 1)
            if b + 3 < NB:
                load_gu(b + 3)
        if b + 2 < NB:
            s1a(b + 2)
    if final:
        fg_d = Dm.get("final_g", [1, D], kind="ExternalInput")
        OUT = Dm.get("out", [L, D], kind="ExternalOutput")
        fg = P.sb(pf + "fg", [128, D]); load_bcast(P, fg[:], pf + "fg", fg_d[0:1, :])
        epsc = P.sb(pf + "eps", [128, 1]); P.i("dve", "memset", writes=[pf + "eps"], ap=epsc[:], constant=EPS)
        junk = P.sb(pf + "junk", [128, D])
    else:
        XN = Dm.get("XN%d" % l, [T, D])
    G2g = [P.sb(pf + "g2%d" % r, [128, D]) for r in range(2)]
    for r in range(2):
        load_bcast(P, G2g[r][:], pf + "g2%d" % r, modrow[r:r + 1, 5120:6144], reads=[mk])
    o1_r = Rot(P, pf + "o1", [128, D], n=2)
    o2_r = Rot(P, pf + "o2", [128, D], n=2)
    xt_r = Rot(P, pf + "xt", [128, D], n=2)
    st_r = Rot(P, pf + "st", [128, 8], n=2)
    ti = 0
    for (c0, cn) in tok_chunks:
        for i in range(cn // 128):
            t0 = c0 + i * 128
            r = 1 if t0 < LC else 0
            o1, o1k = o1_r.next(); o2, o2k = o2_r.next()
            for (o, ok_, col) in ((o1, o1k, ti * 2), (o2, o2k, ti * 2 + 1)):
                P.dma(lambda e, o=o, col=col: e.indirect_dma_start(out=o[:], out_offset=None, in_=OBUF[:, :], in_offset=bass.IndirectOffsetOnAxis(ap=dest[:, col:col + 1], axis=0)),
                      reads=[pf + "dest", "OBUF%d" % l], writes=[ok_], q="pool")
            xt, xk = xt_r.next()
            P.d(xt[:], X1[t0:t0 + 128, :], reads=["X1"], writes=[xk])
            P.i("dve", "tensor_scalar", reads=[o1k, pf + "ww"], writes=[o1k], out=o1[:], in0=o1[:], scalar1=ww[:, ti * 2:ti * 2 + 1], scalar2=None, op0=ALU.mult)
            P.i("dve", "scalar_tensor_tensor", reads=[o1k, o2k, pf + "ww"], writes=[o1k], out=o1[:], in0=o2[:], scalar=ww[:, ti * 2 + 1:ti * 2 + 2], in1=o1[:], op0=ALU.mult, op1=ALU.add)
            P.i("pool", "tensor_tensor", reads=[o1k, pf + "g2%d" % r], writes=[o1k], out=o1[:], in0=o1[:], in1=G2g[r][:], op=ALU.mult)
            P.i("dve", "tensor_tensor", reads=[o1k, xk], writes=[xk], out=xt[:], in0=xt[:], in1=o1[:], op=ALU.add)
            if not final:
                P.d(XN[t0:t0 + 128, :], xt[:], reads=[xk], writes=[P.u("XN%d" % l)])
            else:
                st, sk = st_r.next()
                P.i("act", "activation", reads=[xk], writes=[pf + "junk", sk], out=junk[:], in_=xt[:], func=AF.Square, accum_out=st[:, 0:1])
                P.i("act", "activation", reads=[sk, pf + "eps"], writes=[sk], out=st[:, 1:2], in_=st[:, 0:1], func=AF.Ln, bias=epsc[:], scale=1.0 / D)
                P.i("act", "activation", reads=[sk], writes=[sk], out=st[:, 2:3], in_=st[:, 1:2], func=AF.Exp, scale=-0.5)
                P.i("dve", "scalar_tensor_tensor", reads=[xk, sk, pf + "fg"], writes=[xk], out=xt[:], in0=xt[:], scalar=st[:, 2:3], in1=fg[:], op0=ALU.mult, op1=ALU.mult)
                P.d(OUT[t0 - LC:t0 - LC + 128, :], xt[:], reads=[xk], writes=[P.u("out")], final=True)
            ti += 1


def rope_tables_np():
    rows = np.repeat(np.arange(L // 64), 64)
    cols = np.tile(np.arange(64), L // 64)
    inv = np.power(np.float32(10000.0), -np.arange(16, dtype=np.float32) / np.float32(16)).astype(np.float32)
    ang = np.stack([rows, cols], -1).astype(np.float32)[..., None] * inv
    cos = np.cos(ang).astype(np.float32).reshape(L, 1, 32)
    sin = np.sin(ang).astype(np.float32).reshape(L, 1, 32)
    return (np.ascontiguousarray(np.broadcast_to(cos, (L, 12, 32)).reshape(L, 384)),
            np.ascontiguousarray(np.broadcast_to(sin, (L, 12, 32)).reshape(L, 384)))


def kmajor(w):
    K, N = w.shape
    return np.ascontiguousarray(w.reshape(K // 128, 128, N).transpose(1, 0, 2))


def core_inputs(inp, b):
    f = np.float32
    m = {}
    m["xin"] = np.ascontiguousarray(np.concatenate([inp["ctx"][b], inp["x"][b]], 0).astype(f))
    cv = np.stack([inp["c"][b], inp["c_ctx"]], -1)
    m["cvec"] = np.ascontiguousarray(cv.reshape(8, 128, 2).transpose(1, 0, 2).astype(f))
    m["ident"] = np.eye(128, dtype=f)
    sh = np.zeros((128, 64), f); sh[64 + np.arange(64), np.arange(64)] = 1.0
    m["shiftm"] = sh
    jj, ii = np.meshgrid(np.arange(128), np.arange(128), indexing="ij")
    wm = np.zeros((128, 2, 2, 128), f)
    wm[:, 0] = (ii <= jj).astype(f)[:, None, :]
    wm[:, 1] = (jj <= ii).astype(f)[:, None, :]
    m["wmask"] = np.ascontiguousarray(wm.reshape(128, 2, 256))
    m["rope_cos"], m["rope_sin"] = rope_tables_np()
    for l in range(2):
        m["ada_w%d" % l] = kmajor(inp["ada_w"][l])
        m["ada_b%d" % l] = np.ascontiguousarray(inp["ada_b"][l].reshape(1, 6144))
        m["w_in%d" % l] = kmajor(inp["w_in"][l][:, PERM])
        m["norm1_g%d" % l] = np.ascontiguousarray(inp["norm1_g"][l].reshape(1, D))
        m["qkg%d" % l] = np.ascontiguousarray(np.concatenate([np.tile(inp["ga_qn_g"][l], 4), np.tile(inp["ga_kn_g"][l], 2)]).reshape(1, 384))
        m["dtb%d" % l] = np.ascontiguousarray(inp["ssd_dt_bias"][l].reshape(1, 8))
        m["sink%d" % l] = np.ascontiguousarray(inp["wa_sink"][l].reshape(1, 4))
        m["w_out%d" % l] = kmajor(inp["w_out"][l])
        m["norm2_g%d" % l] = np.ascontiguousarray(inp["norm2_g"][l].reshape(1, D))
        m["wr%d" % l] = kmajor(np.concatenate([inp["moe_coarse_w"][l], inp["moe_fine_w"][l]], 1))
        m["rb%d" % l] = np.ascontiguousarray(np.concatenate([inp["moe_coarse_b"][l], inp["moe_fine_b"][l]]).reshape(1, 36))
        m["moe_g%d" % l] = np.ascontiguousarray(inp["moe_w_gate"][l].reshape(32, 8, 128, 512).transpose(0, 2, 1, 3))
        m["moe_u%d" % l] = np.ascontiguousarray(inp["moe_w_up"][l].reshape(32, 8, 128, 512).transpose(0, 2, 1, 3))
        gu = np.concatenate([m["moe_g%d" % l], m["moe_u%d" % l]], -1)
        m["moe_gua%d" % l] = np.ascontiguousarray(gu[:, :, 0:4, :])
        m["moe_gub%d" % l] = np.ascontiguousarray(gu[:, :, 4:8, :])
        m["moe_d%d" % l] = np.ascontiguousarray(inp["moe_w_down"][l].reshape(32, 4, 128, 1024).transpose(0, 2, 1, 3))
    m["final_g"] = np.ascontiguousarray(inp["final_g"].reshape(1, D))
    m["ramp"] = (128.0 * np.arange(72) + 1.0).astype(f).reshape(1, 72)
    m["bst"] = (128.0 * np.arange(104)).astype(f).reshape(1, 104)
    m["pidx"] = np.arange(128).astype(f).reshape(128, 1)
    sp, s_ = np.meshgrid(np.arange(128), np.arange(128), indexing="ij")
    m["stri"] = np.ascontiguousarray(np.stack([(sp < s_), np.ones_like(sp, dtype=bool)], 1).astype(f))
    m["tri"] = np.ascontiguousarray(np.stack([(sp <= s_), (sp >= s_)], 1).astype(f))
    for l in range(2):
        Bm = np.zeros((2, 2, 8, 128, 128), f)
        Cm = np.zeros((2, 2, 8, 128, 128), f)
        for d in range(2):
            for j in range(8):
                for gg in range(2):
                    g = 2 * j + gg
                    r0 = (2 * (j % 4) + gg) * 16
                    Bm[d, 0, j, r0:r0 + 16, gg * 64:(gg + 1) * 64] = inp["s5_b_re"][l, d, g].T
                    Bm[d, 1, j, r0:r0 + 16, gg * 64:(gg + 1) * 64] = inp["s5_b_im"][l, d, g].T
                    Cm[d, 0, j, gg * 64:(gg + 1) * 64, r0:r0 + 16] = inp["s5_c_re"][l, d, g].T
                    Cm[d, 1, j, gg * 64:(gg + 1) * 64, r0:r0 + 16] = inp["s5_c_im"][l, d, g].T
        m["s5B%d" % l] = Bm
        m["s5C%d" % l] = Cm
        lamt = np.zeros((128, 3, 16), f)
        for d in range(2):
            for j in range(8):
                for gg in range(2):
                    g = 2 * j + gg
                    lamt[gg * 64:(gg + 1) * 64, 0, d * 8 + j] = inp["s5_lam_re"][l, d, g]
                    lamt[gg * 64:(gg + 1) * 64, 1, d * 8 + j] = inp["s5_lam_im"][l, d, g]
                    lamt[gg * 64:(gg + 1) * 64, 2, d * 8 + j] = inp["s5_log_dt"][l, d, g]
        m["s5lam%d" % l] = lamt
        cw = np.concatenate([inp["ssd_conv_w"][l], inp["ssd_conv_b"][l][None]], 0)
        m["convw%d" % l] = np.ascontiguousarray(cw.reshape(4, 6, 128).transpose(2, 1, 0).astype(f))
        m["alog%d" % l] = np.ascontiguousarray(inp["ssd_a_log"][l].reshape(1, 8))
        m["ssdd%d" % l] = np.ascontiguousarray(inp["ssd_d"][l].reshape(1, 4))
        m["ssdng%d" % l] = np.ascontiguousarray(inp["ssd_norm_g"][l].reshape(1, 256))
        m["s5d%d" % l] = np.ascontiguousarray(inp["s5_d"][l].reshape(2, 128).T.astype(f))
        m["gluw%d" % l] = np.ascontiguousarray(inp["s5_glu_w"][l].reshape(2, 128, 256).transpose(1, 0, 2).astype(f))
        m["glub%d" % l] = np.ascontiguousarray(inp["s5_glu_b"][l].reshape(2, 128).T.astype(f))
    for l in range(0):
        pass
    return m


def attn_consts(P, Dm, pf):
    c = {}
    shift_d = Dm.get("shiftm", [128, 64], kind="ExternalInput")
    c["shift"] = P.sb(pf + "shift", [128, 64], F32R)
    P.d(c["shift"][:], shift_d[:, :], writes=[pf + "shift"], q="pool")
    return c


def phase_c(P, Dm, l, ctx_out):
    pf = "c%d_" % l
    QKT = Dm.get("QKT", [768, T])
    TMS = Dm.get("TMS", [T, 520])
    CAT = Dm.get("CAT", [1024, T])
    cst = attn_consts(P, Dm, pf)
    KT = P.sb(pf + "KT", [64, 2, T], F32R)
    V = P.sb(pf + "V", [128, NT, 2, 128], F32R)
    for kv in range(2):
        P.d(KT[:, kv, :], QKT[256 + kv * 64:256 + (kv + 1) * 64, :], reads=["QKT"], writes=[pf + "KT"], q="pool")
    P.i("dve", "memset", writes=[pf + "V/1"], ap=V[:].bitcast(F32)[:, :, :, 64:128], constant=1.0)
    for kv in range(2):
        P.d(V[:, :, kv, 0:64], TMS[:, kv * 64:(kv + 1) * 64].rearrange("(n p) d -> p n d", p=128), reads=["TMS"], writes=[pf + "V/0%d" % kv], q="pool")
    vkeys = [pf + "V"]
    q_r = Rot(P, pf + "q", [64, 4, 256], F32R, n=2)
    s_r = Rot(P, pf + "s", [128, 1024], n=2, psum=True)
    p_r = Rot(P, pf + "p", [128, 1024], F32R, n=3)
    o_r = Rot(P, pf + "o", [128, 512], n=2, psum=True)
    os_r = Rot(P, pf + "os", [128, 512], F32R, n=2)
    dn_r = Rot(P, pf + "dn", [64, 512], n=1, psum=True)
    rd_r = Rot(P, pf + "rd", [64, 512], n=2)
    ot_r = Rot(P, pf + "ot", [64, 512], n=2)
    qtiles = [(LC + 256 * i, NT) for i in range(L // 256)]
    if ctx_out:
        qtiles = [(0, 2)] + qtiles
    for (q0, nkb) in qtiles:
        qt, qtk = q_r.next()
        P.d(qt[:], QKT[0:256, q0:q0 + 256].rearrange("(h d) q -> d h q", d=64), reads=["QKT"], writes=[qtk], q="pool")
        for kv in range(2):
            o, ok = o_r.next()
            its = list(range(nkb // 2))
            pend = []

            def issue_s(sp):
                sp_, spk = s_r.next()
                for u in range(2):
                    s = 2 * sp + u
                    P.i("pe", "matmul", reads=[pf + "KT", qtk], writes=[spk], out=sp_[:, u * 512:(u + 1) * 512], lhsT=KT[:, kv, s * 128:(s + 1) * 128],
                        rhs=qt[:, 2 * kv:2 * kv + 2, :], start=True, stop=True)
                pt, ptk = p_r.next()
                P.i("act", "activation", reads=[spk], writes=[ptk], out=pt[:], in_=sp_[:], func=AF.Exp, scale=0.125)
                return (sp, pt, ptk)

            LOOK = 1
            for sp in its[:LOOK]:
                pend.append(issue_s(sp))
            for idx, sp in enumerate(its):
                (sp_i, pt, ptk) = pend.pop(0)
                if idx + LOOK < len(its):
                    pend.append(issue_s(its[idx + LOOK]))
                for u in range(2):
                    s_ = 2 * sp_i + u
                    P.i("pe", "matmul", reads=[ptk] + vkeys, writes=[ok], out=o[:], lhsT=V[:, s_, kv, :], rhs=pt[:, u * 512:(u + 1) * 512],
                        start=(idx == 0 and u == 0), stop=(idx == len(its) - 1 and u == 1))
            osb, osk = os_r.next()
            P.i("act", "activation", reads=[ok], writes=[osk], out=osb[:], in_=o[:], func=AF.Copy)
            dn, dnk = dn_r.next()
            P.i("pe", "matmul", reads=[osk, pf + "shift"], writes=[dnk], out=dn[:], lhsT=cst["shift"][:], rhs=osb[:], start=True, stop=True)
            rd, rdk = rd_r.next()
            P.i("dve", "reciprocal", reads=[dnk], writes=[rdk], out=rd[:], in_=dn[:])
            ot, otk = ot_r.next()
            P.i("dve", "tensor_tensor", reads=[osk, rdk], writes=[otk], out=ot[:], in0=osb[0:64, :].bitcast(F32), in1=rd[:], op=ALU.mult)
            P.d(CAT[256 + kv * 128:256 + (kv + 1) * 128, q0:q0 + 256].rearrange("(hh d) q -> d hh q", d=64),
                ot[:].rearrange("d (hh q) -> d hh q", hh=2), reads=[otk], writes=[P.u("CAT")])


def phase_e(P, Dm, l, ctx_out):
    pf = "e%d_" % l
    QKT = Dm.get("QKT", [768, T])
    TMS = Dm.get("TMS", [T, 520])
    CAT = Dm.get("CAT", [1024, T])
    sink_d = Dm.get("sink%d" % l, [1, 4], kind="ExternalInput")
    mask_d = Dm.get("wmask", [128, 2, 256], kind="ExternalInput")
    cst = attn_consts(P, Dm, pf)
    KT = P.sb(pf + "KT", [64, 2, T], F32R)
    V = P.sb(pf + "V", [128, NT, 2, 128], F32R)
    for kv in range(2):
        P.d(KT[:, kv, :], QKT[640 + kv * 64:640 + (kv + 1) * 64, :], reads=["QKT"], writes=[pf + "KT"], q="pool")
    P.i("dve", "memset", writes=[pf + "V/1"], ap=V[:].bitcast(F32)[:, :, :, 64:128], constant=1.0)
    for kv in range(2):
        P.d(V[:, :, kv, 0:64], TMS[:, 128 + kv * 64:128 + (kv + 1) * 64].rearrange("(n p) d -> p n d", p=128), reads=["TMS"], writes=[pf + "V/0%d" % kv], q="pool")
    vkeys = [pf + "V"]
    mask = P.sb(pf + "mask", [128, 2, 256])
    P.d(mask[:], mask_d[:, :, :], writes=[pf + "mask"])
    esk = P.sb(pf + "esk", [64, 4])
    P.d(esk[:], sink_d[0:1, :].to_broadcast([64, 4]), writes=[pf + "esk"])
    P.i("act", "activation", reads=[pf + "esk"], writes=[pf + "esk"], out=esk[:], in_=esk[:], func=AF.Exp)
    q_r = Rot(P, pf + "q", [64, 4, 128], F32R, n=2)
    s_r = Rot(P, pf + "s", [128, 256], n=3, psum=True)
    p_r = Rot(P, pf + "p", [128, 256], F32R, n=3)
    o_r = Rot(P, pf + "o", [128, 256], n=2, psum=True)
    os_r = Rot(P, pf + "os", [128, 256], F32R, n=2)
    dn_r = Rot(P, pf + "dn", [64, 256], n=1, psum=True)
    rd_r = Rot(P, pf + "rd", [64, 256], n=2)
    ot_r = Rot(P, pf + "ot", [64, 256], n=2)
    qtiles = []
    if ctx_out:
        qtiles += [(i, [(0, None), (1, None)]) for i in range(2)]
    for n in range(L // 128):
        ti = 2 + n
        kb = [(0, None), (1, None)]
        if n > 0:
            kb.append((ti - 1, 0))
        kb.append((ti, None))
        if n < L // 128 - 1:
            kb.append((ti + 1, 1))
        qtiles.append((ti, kb))
    for (ti, kbs) in qtiles:
        q0 = ti * 128
        qt, qtk = q_r.next()
        P.d(qt[:], QKT[384:640, q0:q0 + 128].rearrange("(h d) q -> d h q", d=64), reads=["QKT"], writes=[qtk], q="pool")
        for kv in range(2):
            o, ok = o_r.next()
            for idx, (s, mi) in enumerate(kbs):
                sp_, spk = s_r.next()
                P.i("pe", "matmul", reads=[pf + "KT", qtk], writes=[spk], out=sp_[:], lhsT=KT[:, kv, s * 128:(s + 1) * 128],
                    rhs=qt[:, 2 * kv:2 * kv + 2, :], start=True, stop=True)
                pt, ptk = p_r.next()
                P.i("act", "activation", reads=[spk], writes=[ptk], out=pt[:], in_=sp_[:], func=AF.Exp, scale=0.125)
                if mi is not None:
                    P.i("dve", "tensor_tensor", reads=[ptk, pf + "mask"], writes=[ptk], out=pt[:], in0=pt[:].bitcast(F32), in1=mask[:, mi, :], op=ALU.mult)
                P.i("pe", "matmul", reads=[ptk] + vkeys, writes=[ok], out=o[:], lhsT=V[:, s, kv, :], rhs=pt[:],
                    start=(idx == 0), stop=(idx == len(kbs) - 1))
            osb, osk = os_r.next()
            P.i("act", "activation", reads=[ok], writes=[osk], out=osb[:], in_=o[:], func=AF.Copy)
            dn, dnk = dn_r.next()
            P.i("pe", "matmul", reads=[osk, pf + "shift"], writes=[dnk], out=dn[:], lhsT=cst["shift"][:], rhs=osb[:], start=True, stop=True)
            rd, rdk = rd_r.next()
            for hh in range(2):
                h = 2 * kv + hh
                P.i("dve", "tensor_scalar", reads=[dnk, pf + "esk"], writes=[rdk + "/%d" % hh], out=rd[:, hh * 128:(hh + 1) * 128], in0=dn[:, hh * 128:(hh + 1) * 128],
                    scalar1=esk[:, h:h + 1], scalar2=None, op0=ALU.add)
            P.i("dve", "reciprocal", reads=[rdk], writes=[rdk], out=rd[:], in_=rd[:])
            ot, otk = ot_r.next()
            P.i("dve", "tensor_tensor", reads=[osk, rdk], writes=[otk], out=ot[:], in0=osb[0:64, :].bitcast(F32), in1=rd[:], op=ALU.mult)
            P.d(CAT[768 + kv * 128:768 + (kv + 1) * 128, q0:q0 + 128].rearrange("(hh d) q -> d hh q", d=64),
                ot[:].rearrange("d (hh q) -> d hh q", hh=2), reads=[otk], writes=[P.u("CAT")])


BIG = 1.0e30
S5_ENG2 = "dve"


def phase_f(P, Dm, l, src_name, tok_chunks=None):
    pf = "f%d_" % l
    xsrc = Dm.get(src_name, [T, D])
    CAT = Dm.get("CAT", [1024, T])
    w_out = Dm.get("w_out%d" % l, [128, 8, 1024], kind="ExternalInput")
    modrow = Dm.get("modrow%d" % l, [2, 6144])
    n2g = Dm.get("norm2_g%d" % l, [1, D], kind="ExternalInput")
    wr_d = Dm.get("wr%d" % l, [128, 8, 36], kind="ExternalInput")
    rb_d = Dm.get("rb%d" % l, [1, 36], kind="ExternalInput")
    ident_d = Dm.get("ident", [128, 128], kind="ExternalInput")
    X1 = Dm.get("X1", [T, D])
    H2T = Dm.get("H2T", [D, T])
    WTd = Dm.get("WTd", [32, T])
    mk = "modrow%d" % l
    ident = P.sb(pf + "ident", [128, 128])
    P.d(ident[:], ident_d[:, :], writes=[pf + "ident"])
    wo = P.sb(pf + "wo", [128, 8, 1024], F32R)
    for k in range(8):
        P.d(wo[:, k, :], w_out[:, k, :], writes=[pf + "wo/%d" % k], q="pool")
    wr = P.sb(pf + "wr", [128, 8, 36])
    P.d(wr[:], wr_d[:, :, :], writes=[pf + "wr"])
    rb = P.sb(pf + "rb", [128, 36])
    load_bcast(P, rb[:], pf + "rb", rb_d[0:1, :])
    G1 = [P.sb(pf + "G1%d" % r, [128, D]) for r in range(2)]
    G2 = [P.sb(pf + "G2%d" % r, [128, D]) for r in range(2)]
    SH2 = [P.sb(pf + "SH2%d" % r, [128, D]) for r in range(2)]
    gn = P.sb(pf + "gn", [128, D])
    load_bcast(P, gn[:], pf + "gn", n2g[0:1, :])
    for r in range(2):
        load_bcast(P, G1[r][:], pf + "G1%d" % r, modrow[r:r + 1, 2048:3072], reads=[mk])
        load_bcast(P, SH2[r][:], pf + "SH2%d" % r, modrow[r:r + 1, 3072:4096], reads=[mk])
        load_bcast(P, G2[r][:], pf + "G2%d" % r, modrow[r:r + 1, 4096:5120], reads=[mk])
        P.i("dve", "scalar_tensor_tensor", reads=[pf + "G2%d" % r, pf + "gn"], writes=[pf + "G2%d" % r], out=G2[r][:], in0=G2[r][:], scalar=1.0, in1=gn[:], op0=ALU.add, op1=ALU.mult)
    epsc = P.sb(pf + "eps", [128, 1])
    P.i("dve", "memset", writes=[pf + "eps"], ap=epsc[:], constant=EPS)
    ct_r = Rot(P, pf + "ct", [128, 8, 512], F32R, n=2)
    po_r = Rot(P, pf + "po", [128, 1024], n=1, psum=True)
    xt_r = Rot(P, pf + "xt", [128, D], n=2)
    x1_r = Rot(P, pf + "x1", [128, D], n=2)
    h_r = Rot(P, pf + "h", [128, D], n=2)
    junk = P.sb(pf + "junk", [128, D])
    st_r = Rot(P, pf + "st", [128, 64], n=3)
    tp_r = Rot(P, pf + "tp", [128, 1024], n=1, psum=True)
    hT_r = Rot(P, pf + "hT", [128, 8, 128], F32R, n=2)
    hTf_r = Rot(P, pf + "hTf", [128, 8, 128], n=2)
    pr_r = Rot(P, pf + "pr", [128, 512], n=1, psum=True)
    lg_r = Rot(P, pf + "lg", [128, 36], n=2)
    mk_r = Rot(P, pf + "mk", [128, 4, 8], n=2)
    oh_r = Rot(P, pf + "oh", [128, 3, 32], n=2)
    wt_r = Rot(P, pf + "wt", [128, 32], n=2)
    pw_r = Rot(P, pf + "pw", [128, 512], n=1, psum=True)
    wT_r = Rot(P, pf + "wT", [32, 128], n=2)
    for (c0, cn) in (tok_chunks or CHUNKS):
        ct, ctk = ct_r.next()
        P.d(ct[:, :, 0:cn], CAT[:, c0:c0 + cn].rearrange("(k p) t -> p k t", p=128), reads=["CAT"], writes=[ctk], q="pool")
        for i in range(cn // 128):
            t0 = c0 + i * 128
            r = 1 if t0 < LC else 0
            po, pok = po_r.next()
            for hf in range(2):
                for k in range(8):
                    P.i("pe", "matmul", reads=[ctk, pf + "wo/%d" % k], writes=[pok], out=po[:, hf * 512:(hf + 1) * 512], lhsT=ct[:, k, i * 128:(i + 1) * 128],
                        rhs=wo[:, k, hf * 512:(hf + 1) * 512], start=(k == 0), stop=(k == 7))
            xt, xk = xt_r.next()
            P.d(xt[:], xsrc[t0:t0 + 128, :], reads=[src_name], writes=[xk])
            x1, x1k = x1_r.next()
            for hf in range(2):
                sl = slice(hf * 512, (hf + 1) * 512)
                P.i("dve", "tensor_tensor", reads=[pok, pf + "G1%d" % r], writes=[x1k + "/%d" % hf], out=x1[:, sl], in0=po[:, sl], in1=G1[r][:, sl], op=ALU.mult)
            P.i("pool", "tensor_tensor", reads=[x1k, xk], writes=[x1k], out=x1[:], in0=x1[:], in1=xt[:], op=ALU.add)
            P.d(X1[t0:t0 + 128, :], x1[:], reads=[x1k], writes=[P.u("X1")])
            st, sk = st_r.next()
            P.i("act", "activation", reads=[x1k], writes=[pf + "junk", sk], out=junk[:], in_=x1[:], func=AF.Square, accum_out=st[:, 0:1])
            P.i("act", "activation", reads=[sk, pf + "eps"], writes=[sk], out=st[:, 1:2], in_=st[:, 0:1], func=AF.Ln, bias=epsc[:], scale=1.0 / D)
            P.i("act", "activation", reads=[sk], writes=[sk], out=st[:, 2:3], in_=st[:, 1:2], func=AF.Exp, scale=-0.5)
            h, hk = h_r.next()
            P.i("dve", "scalar_tensor_tensor", reads=[x1k, sk, pf + "G2%d" % r], writes=[hk], out=h[:], in0=x1[:], scalar=st[:, 2:3], in1=G2[r][:], op0=ALU.mult, op1=ALU.mult)
            P.i("pool", "tensor_tensor", reads=[hk, pf + "SH2%d" % r], writes=[hk], out=h[:], in0=h[:], in1=SH2[r][:], op=ALU.add)
            tp, tpk = tp_r.next()
            for k in range(8):
                P.i("pe", "transpose", reads=[hk, pf + "ident"], writes=[tpk], noinc=(k != 7), out=tp[:, k * 128:(k + 1) * 128], in_=h[:, k * 128:(k + 1) * 128], identity=ident[:])
            hT, hTk = hT_r.next()
            hTf, hTfk = hTf_r.next()
            P.i("act", "activation", reads=[tpk], writes=[hTk], out=hT[:], in_=tp[:].rearrange("p (k t) -> p k t", k=8), func=AF.Copy)
            P.i("dve", "tensor_copy", reads=[tpk], writes=[hTfk], out=hTf[:], in_=tp[:].rearrange("p (k t) -> p k t", k=8))
            P.d(H2T[:, t0:t0 + 128].rearrange("(k p) t -> p k t", p=128), hT[:].bitcast(F32), reads=[hTk], writes=[P.u("H2T")])
            pr, prk = pr_r.next()
            for k in range(8):
                P.i("pe", "matmul", reads=[hTfk, pf + "wr"], writes=[prk], out=pr[:, 0:36], lhsT=hTf[:, k, :], rhs=wr[:, k, :], start=(k == 0), stop=(k == 7))
            lg, lgk = lg_r.next()
            P.i("dve", "tensor_tensor", reads=[prk, pf + "rb"], writes=[lgk], out=lg[:], in0=pr[:, 0:36], in1=rb[:], op=ALU.add)
            P.i("dve", "tensor_reduce", reads=[lgk], writes=[sk], out=st[:, 4:5], in_=lg[:, 0:4], axis=AX.X, op=ALU.max)
            P.i("dve", "tensor_scalar", reads=[sk], writes=[sk], out=st[:, 5:6], in0=st[:, 4:5], scalar1=-1.0, scalar2=None, op0=ALU.mult)
            P.i("act", "activation", reads=[lgk, sk], writes=[sk], out=st[:, 32:36], in_=lg[:, 0:4], func=AF.Exp, bias=st[:, 5:6], accum_out=st[:, 6:7])
            P.i("dve", "reciprocal", reads=[sk], writes=[sk], out=st[:, 7:8], in_=st[:, 6:7])
            P.i("dve", "tensor_scalar", reads=[lgk, sk], writes=[sk], out=st[:, 8:12], in0=lg[:, 0:4], scalar1=st[:, 4:5], scalar2=None, op0=ALU.is_equal)
            P.i("dve", "tensor_scalar", reads=[sk], writes=[sk], out=st[:, 12:16], in0=st[:, 8:12], scalar1=BIG, scalar2=-BIG, op0=ALU.mult, op1=ALU.add)
            mkd, mkk = mk_r.next()
            P.i("dve", "tensor_tensor", reads=[lgk, sk], writes=[mkk], out=mkd[:], in0=lg[:, 4:36].rearrange("p (g e) -> p g e", g=4),
                in1=st[:, 12:16].unsqueeze(2).to_broadcast([128, 4, 8]), op=ALU.add)
            mflat = mkd[:].rearrange("p g e -> p (g e)")
            P.i("dve", "max", reads=[mkk], writes=[sk], out=st[:, 16:24], in_=mflat)
            oh, ohk = oh_r.next()
            P.i("dve", "tensor_scalar", reads=[mkk, sk], writes=[ohk + "/1"], out=oh[:, 0, :], in0=mflat, scalar1=st[:, 16:17], scalar2=None, op0=ALU.is_equal)
            P.i("dve", "tensor_scalar", reads=[mkk, sk], writes=[ohk + "/2"], out=oh[:, 1, :], in0=mflat, scalar1=st[:, 17:18], scalar2=None, op0=ALU.is_equal)
            P.i("dve", "tensor_tensor", reads=[sk], writes=[sk], out=st[:, 24:25], in0=st[:, 17:18], in1=st[:, 16:17], op=ALU.subtract)
            P.i("act", "activation", reads=[sk], writes=[sk], out=st[:, 25:26], in_=st[:, 24:25], func=AF.Exp)
            P.i("dve", "tensor_scalar", reads=[sk], writes=[sk], out=st[:, 26:27], in0=st[:, 25:26], scalar1=1.0, scalar2=None, op0=ALU.add)
            P.i("dve", "reciprocal", reads=[sk], writes=[sk], out=st[:, 27:28], in_=st[:, 26:27])
            P.i("dve", "tensor_tensor", reads=[sk], writes=[sk], out=st[:, 28:29], in0=st[:, 27:28], in1=st[:, 7:8], op=ALU.mult)
            P.i("dve", "tensor_tensor", reads=[sk], writes=[sk], out=st[:, 29:30], in0=st[:, 7:8], in1=st[:, 28:29], op=ALU.subtract)
            P.i("dve", "tensor_scalar", reads=[ohk + "/1", sk], writes=[ohk + "/3"], out=oh[:, 2, :], in0=oh[:, 0, :], scalar1=st[:, 28:29], scalar2=None, op0=ALU.mult)
            wt, wtk = wt_r.next()
            P.i("dve", "scalar_tensor_tensor", reads=[ohk + "/2", ohk + "/3", sk], writes=[wtk], out=wt[:], in0=oh[:, 1, :], scalar=st[:, 29:30], in1=oh[:, 2, :], op0=ALU.mult, op1=ALU.add)
            pw, pwk = pw_r.next()
            P.i("pe", "transpose", reads=[wtk, pf + "ident"], writes=[pwk], out=pw[0:32, 0:128], in_=wt[:], identity=ident[:])
            wT, wTk = wT_r.next()
            P.i("act", "activation", reads=[pwk], writes=[wTk], out=wT[:], in_=pw[0:32, 0:128], func=AF.Copy)
            P.d(WTd[:, t0:t0 + 128], wT[:], reads=[wTk], writes=[P.u("WTd")])


def phase_f2(P, Dm, l, src_name, tok_chunks=None):
    pf = "F%d_" % l
    xsrc = Dm.get(src_name, [T, D])
    CAT = Dm.get("CAT", [1024, T])
    w_out = Dm.get("w_out%d" % l, [128, 8, 1024], kind="ExternalInput")
    modrow = Dm.get("modrow%d" % l, [2, 6144])
    n2g = Dm.get("norm2_g%d" % l, [1, D], kind="ExternalInput")
    wr_d = Dm.get("wr%d" % l, [128, 8, 36], kind="ExternalInput")
    rb_d = Dm.get("rb%d" % l, [1, 36], kind="ExternalInput")
    ident_d = Dm.get("ident", [128, 128], kind="ExternalInput")
    X1 = Dm.get("X1", [T, D])
    chunks_ = (tok_chunks or CHUNKS)
    ntf = sum(cn for _, cn in chunks_) // 128
    NB = (2 * ntf * 128) // 128 + 32
    BUF = Dm.get("BUF%d" % l, [NB * 128, D])
    IDXW = Dm.get("IDXW%d" % l, [128, NB], I32)
    DEST = Dm.get("DEST%d" % l, [128, ntf * 2], I32)
    WWd = Dm.get("WWd%d" % l, [128, ntf * 2])
    ramp_d = Dm.get("ramp", [1, 72], kind="ExternalInput")
    bst_d = Dm.get("bst", [1, 104], kind="ExternalInput")
    pidx_d = Dm.get("pidx", [128, 1], kind="ExternalInput")
    stri_d = Dm.get("stri", [128, 2, 128], kind="ExternalInput")
    OH = P.sb(pf + "OH", [128, ntf, 2, 32])
    WW = P.sb(pf + "WW", [128, ntf, 2])
    RK = P.sb(pf + "RK", [128, ntf, 32])
    Msum = P.sb(pf + "Msum", [128, 32])
    P.i("dve", "memset", writes=[pf + "Msum"], ap=Msum[:], constant=0.0)
    stri = P.sb(pf + "stri", [128, 2, 128]); P.d(stri[:], stri_d[:, :, :], writes=[pf + "stri"])
    Mt_r = Rot(P, pf + "Mt", [128, 32], n=2)
    zz = P.sb(pf + "zz", [128, 4096])
    P.i("pool", "memset", writes=[pf + "zz"], ap=zz[:], constant=0.0)
    rows_per = 128 * 4
    for r0 in range(0, NB * 128, rows_per):
        P.d(BUF[r0:r0 + rows_per, :].rearrange("(p a) n -> p (a n)", p=128), zz[:], reads=[pf + "zz"], writes=[P.u("BUFz%d" % l)])
    mk = "modrow%d" % l
    ident = P.sb(pf + "ident", [128, 128])
    P.d(ident[:], ident_d[:, :], writes=[pf + "ident"])
    wo = P.sb(pf + "wo", [128, 8, 1024], F32R)
    for k in range(8):
        P.d(wo[:, k, :], w_out[:, k, :], writes=[pf + "wo/%d" % k], q="pool")
    wr = P.sb(pf + "wr", [128, 8, 36])
    P.d(wr[:], wr_d[:, :, :], writes=[pf + "wr"])
    rb = P.sb(pf + "rb", [128, 36])
    load_bcast(P, rb[:], pf + "rb", rb_d[0:1, :])
    G1 = [P.sb(pf + "G1%d" % r, [128, D]) for r in range(2)]
    G2 = [P.sb(pf + "G2%d" % r, [128, D]) for r in range(2)]
    SH2 = [P.sb(pf + "SH2%d" % r, [128, D]) for r in range(2)]
    gn = P.sb(pf + "gn", [128, D])
    load_bcast(P, gn[:], pf + "gn", n2g[0:1, :])
    for r in range(2):
        load_bcast(P, G1[r][:], pf + "G1%d" % r, modrow[r:r + 1, 2048:3072], reads=[mk])
        load_bcast(P, SH2[r][:], pf + "SH2%d" % r, modrow[r:r + 1, 3072:4096], reads=[mk])
        load_bcast(P, G2[r][:], pf + "G2%d" % r, modrow[r:r + 1, 4096:5120], reads=[mk])
        P.i("dve", "scalar_tensor_tensor", reads=[pf + "G2%d" % r, pf + "gn"], writes=[pf + "G2%d" % r], out=G2[r][:], in0=G2[r][:], scalar=1.0, in1=gn[:], op0=ALU.add, op1=ALU.mult)
    epsc = P.sb(pf + "eps", [128, 1])
    P.i("dve", "memset", writes=[pf + "eps"], ap=epsc[:], constant=EPS)
    ct_r = Rot(P, pf + "ct", [128, 8, 512], F32R, n=2)
    po_r = Rot(P, pf + "po", [128, 1024], n=1, psum=True)
    xt_r = Rot(P, pf + "xt", [128, D], n=2)
    x1_r = Rot(P, pf + "x1", [128, D], n=2)
    h_r = Rot(P, pf + "h", [128, D], n=2)
    junk = P.sb(pf + "junk", [128, D])
    st_r = Rot(P, pf + "st", [128, 64], n=3)
    tp_r = Rot(P, pf + "tp", [128, 1024], n=1, psum=True)
    H2 = Dm.get("H2_%d" % l, [T, D])
    hTf_r = Rot(P, pf + "hTf", [128, 8, 128], n=2)
    pr_r = Rot(P, pf + "pr", [128, 512], n=1, psum=True)
    lg_r = Rot(P, pf + "lg", [128, 36], n=2)
    mk_r = Rot(P, pf + "mk", [128, 4, 8], n=2)
    oh_r = Rot(P, pf + "oh", [128, 3, 32], n=2)
    pw_r = Rot(P, pf + "pw", [128, 512], n=1, psum=True)
    tile_i = 0
    for (c0, cn) in chunks_:
        ct, ctk = ct_r.next()
        P.d(ct[:, :, 0:cn], CAT[:, c0:c0 + cn].rearrange("(k p) t -> p k t", p=128), reads=["CAT"], writes=[ctk], q="pool")
        for i in range(cn // 128):
            t0 = c0 + i * 128
            r = 1 if t0 < LC else 0
            po, pok = po_r.next()
            for hf in range(2):
                for k in range(8):
                    P.i("pe", "matmul", reads=[ctk, pf + "wo/%d" % k], writes=[pok], out=po[:, hf * 512:(hf + 1) * 512], lhsT=ct[:, k, i * 128:(i + 1) * 128],
                        rhs=wo[:, k, hf * 512:(hf + 1) * 512], start=(k == 0), stop=(k == 7))
            xt, xk = xt_r.next()
            P.d(xt[:], xsrc[t0:t0 + 128, :], reads=[src_name], writes=[xk])
            x1, x1k = x1_r.next()
            for hf in range(2):
                sl = slice(hf * 512, (hf + 1) * 512)
                P.i("dve", "tensor_tensor", reads=[pok, pf + "G1%d" % r], writes=[x1k + "/%d" % hf], out=x1[:, sl], in0=po[:, sl], in1=G1[r][:, sl], op=ALU.mult)
            P.i("pool", "tensor_tensor", reads=[x1k, xk], writes=[x1k], out=x1[:], in0=x1[:], in1=xt[:], op=ALU.add)
            P.d(X1[t0:t0 + 128, :], x1[:], reads=[x1k], writes=[P.u("X1")])
            st, sk = st_r.next()
            P.i("act", "activation", reads=[x1k], writes=[pf + "junk", sk], out=junk[:], in_=x1[:], func=AF.Square, accum_out=st[:, 0:1])
            P.i("act", "activation", reads=[sk, pf + "eps"], writes=[sk], out=st[:, 1:2], in_=st[:, 0:1], func=AF.Ln, bias=epsc[:], scale=1.0 / D)
            P.i("act", "activation", reads=[sk], writes=[sk], out=st[:, 2:3], in_=st[:, 1:2], func=AF.Exp, scale=-0.5)
            h, hk = h_r.next()
            P.i("dve", "scalar_tensor_tensor", reads=[x1k, sk, pf + "G2%d" % r], writes=[hk], out=h[:], in0=x1[:], scalar=st[:, 2:3], in1=G2[r][:], op0=ALU.mult, op1=ALU.mult)
            P.i("pool", "tensor_tensor", reads=[hk, pf + "SH2%d" % r], writes=[hk], out=h[:], in0=h[:], in1=SH2[r][:], op=ALU.add)
            P.d(H2[t0:t0 + 128, :], h[:], reads=[hk], writes=[P.u("H2_%d" % l)])
            tp, tpk = tp_r.next()
            for k in range(8):
                P.i("pe", "transpose", reads=[hk, pf + "ident"], writes=[tpk], noinc=(k != 7), out=tp[:, k * 128:(k + 1) * 128], in_=h[:, k * 128:(k + 1) * 128], identity=ident[:])
            hTf, hTfk = hTf_r.next()
            P.i("dve", "tensor_copy", reads=[tpk], writes=[hTfk], out=hTf[:], in_=tp[:].rearrange("p (k t) -> p k t", k=8))
            pr, prk = pr_r.next()
            for k in range(8):
                P.i("pe", "matmul", reads=[hTfk, pf + "wr"], writes=[prk], out=pr[:, 0:36], lhsT=hTf[:, k, :], rhs=wr[:, k, :], start=(k == 0), stop=(k == 7))
            lg, lgk = lg_r.next()
            P.i("dve", "tensor_tensor", reads=[prk, pf + "rb"], writes=[lgk], out=lg[:], in0=pr[:, 0:36], in1=rb[:], op=ALU.add)
            P.i("dve", "tensor_reduce", reads=[lgk], writes=[sk], out=st[:, 4:5], in_=lg[:, 0:4], axis=AX.X, op=ALU.max)
            P.i("dve", "tensor_scalar", reads=[sk], writes=[sk], out=st[:, 5:6], in0=st[:, 4:5], scalar1=-1.0, scalar2=None, op0=ALU.mult)
            P.i("act", "activation", reads=[lgk, sk], writes=[sk], out=st[:, 32:36], in_=lg[:, 0:4], func=AF.Exp, bias=st[:, 5:6], accum_out=st[:, 6:7])
            P.i("dve", "reciprocal", reads=[sk], writes=[sk], out=st[:, 7:8], in_=st[:, 6:7])
            P.i("dve", "tensor_scalar", reads=[lgk, sk], writes=[sk], out=st[:, 8:12], in0=lg[:, 0:4], scalar1=st[:, 4:5], scalar2=None, op0=ALU.is_equal)
            P.i("dve", "tensor_scalar", reads=[sk], writes=[sk], out=st[:, 12:16], in0=st[:, 8:12], scalar1=BIG, scalar2=-BIG, op0=ALU.mult, op1=ALU.add)
            mkd, mkk = mk_r.next()
            P.i("dve", "tensor_tensor", reads=[lgk, sk], writes=[mkk], out=mkd[:], in0=lg[:, 4:36].rearrange("p (g e) -> p g e", g=4),
                in1=st[:, 12:16].unsqueeze(2).to_broadcast([128, 4, 8]), op=ALU.add)
            mflat = mkd[:].rearrange("p g e -> p (g e)")
            P.i("dve", "max", reads=[mkk], writes=[sk], out=st[:, 16:24], in_=mflat)
            oh, ohk = oh_r.next()
            P.i("dve", "tensor_scalar", reads=[mkk, sk], writes=[ohk + "/1"], out=oh[:, 0, :], in0=mflat, scalar1=st[:, 16:17], scalar2=None, op0=ALU.is_equal)
            P.i("dve", "tensor_scalar", reads=[mkk, sk], writes=[ohk + "/2"], out=oh[:, 1, :], in0=mflat, scalar1=st[:, 17:18], scalar2=None, op0=ALU.is_equal)
            P.i("dve", "tensor_tensor", reads=[sk], writes=[sk], out=st[:, 24:25], in0=st[:, 17:18], in1=st[:, 16:17], op=ALU.subtract)
            P.i("act", "activation", reads=[sk], writes=[sk], out=st[:, 25:26], in_=st[:, 24:25], func=AF.Exp)
            P.i("dve", "tensor_scalar", reads=[sk], writes=[sk], out=st[:, 26:27], in0=st[:, 25:26], scalar1=1.0, scalar2=None, op0=ALU.add)
            P.i("dve", "reciprocal", reads=[sk], writes=[sk], out=st[:, 27:28], in_=st[:, 26:27])
            P.i("dve", "tensor_tensor", reads=[sk], writes=[sk], out=st[:, 28:29], in0=st[:, 27:28], in1=st[:, 7:8], op=ALU.mult)
            P.i("dve", "tensor_tensor", reads=[sk], writes=[sk], out=st[:, 29:30], in0=st[:, 7:8], in1=st[:, 28:29], op=ALU.subtract)
            ti = tile_i
            tile_i += 1
            P.i("act", "activation", reads=[ohk + "/1"], writes=[pf + "OH/%d_0" % ti], out=OH[:, ti, 0, :], in_=oh[:, 0, :], func=AF.Copy)
            P.i("act", "activation", reads=[ohk + "/2"], writes=[pf + "OH/%d_1" % ti], out=OH[:, ti, 1, :], in_=oh[:, 1, :], func=AF.Copy)
            P.i("act", "activation", reads=[sk], writes=[pf + "WW/%d" % ti], out=WW[:, ti, :], in_=st[:, 28:30], func=AF.Copy)
            Mt, Mtk = Mt_r.next()
            P.i("dve", "tensor_tensor", reads=[ohk + "/1", ohk + "/2"], writes=[Mtk], out=Mt[:], in0=oh[:, 0, :], in1=oh[:, 1, :], op=ALU.add)
            pw, pwk = pw_r.next()
            P.i("pe", "matmul", reads=[Mtk, pf + "stri"], writes=[pwk], out=pw[:, 0:32], lhsT=stri[:, 0, :], rhs=Mt[:], start=True, stop=False)
            P.i("pe", "matmul", reads=[pf + "Msum", pf + "stri"], writes=[pwk], out=pw[:, 0:32], lhsT=stri[:, 1, :], rhs=Msum[:], start=False, stop=True)
            P.i("act", "activation", reads=[pwk], writes=[pf + "RK/%d" % ti], out=RK[:, ti, :], in_=pw[:, 0:32], func=AF.Copy)
            P.i("dve", "tensor_tensor", reads=[Mtk, pf + "Msum"], writes=[pf + "Msum"], out=Msum[:], in0=Msum[:], in1=Mt[:], op=ALU.add)
    ramp = P.sb(pf + "ramp", [128, 72]); load_bcast(P, ramp[:], pf + "ramp", ramp_d[0:1, :])
    bst = P.sb(pf + "bst", [128, 104]); load_bcast(P, bst[:], pf + "bst", bst_d[0:1, :])
    pidx = P.sb(pf + "pidx", [128, 1]); P.d(pidx[:], pidx_d[:, :], writes=[pf + "pidx"])
    q = P.sb(pf + "q", [128, 8, 32])
    QK = pf + "q"
    big = P.sb(pf + "big", [128, 104 * 32])
    ones32 = P.sb(pf + "ones32", [128, 32]); P.i("dve", "memset", writes=[pf + "ones32"], ap=ones32[:], constant=1.0)
    pw, pwk = pw_r.next()
    P.i("pe", "matmul", reads=[pf + "Msum", pf + "stri"], writes=[pwk], out=pw[:, 0:32], lhsT=stri[:, 1, :], rhs=Msum[:], start=True, stop=True)
    P.i("dve", "tensor_copy", reads=[pwk], writes=[QK], out=q[:, 0, :], in_=pw[:, 0:32])
    NM = 2 * ntf
    b3 = big[:, 0:32 * NM].rearrange("p (e m) -> p e m", e=32)
    P.i("dve", "tensor_tensor", reads=[QK, pf + "ramp"], writes=[pf + "big"], out=b3, in0=q[:, 0, :].unsqueeze(2).to_broadcast([128, 32, NM]),
        in1=ramp[:, 0:NM].unsqueeze(1).to_broadcast([128, 32, NM]), op=ALU.is_ge)
    P.i("dve", "tensor_reduce", reads=[pf + "big"], writes=[QK], out=q[:, 1, :], in_=b3, axis=AX.X, op=ALU.add)
    P.i("dve", "tensor_scalar", reads=[QK], writes=[QK], out=q[:, 1, :], in0=q[:, 1, :], scalar1=128.0, scalar2=None, op0=ALU.mult)
    P.i("dve", "tensor_tensor_scan", reads=[QK, pf + "ones32"], writes=[QK], out=q[:, 2, :], data0=ones32[:], data1=q[:, 1, :], initial=0.0, op0=ALU.mult, op1=ALU.add)
    P.i("dve", "tensor_tensor", reads=[QK], writes=[QK], out=q[:, 3, :], in0=q[:, 2, :], in1=q[:, 1, :], op=ALU.subtract)
    be = P.sb(pf + "be", [128, 104])
    b3 = big[:, 0:NB * 32].rearrange("p (b e) -> p b e", e=32)
    P.i("dve", "tensor_tensor", reads=[QK, pf + "bst"], writes=[pf + "big"], out=b3, in0=q[:, 2, :].unsqueeze(1).to_broadcast([128, NB, 32]),
        in1=bst[:, 0:NB].unsqueeze(2).to_broadcast([128, NB, 32]), op=ALU.is_le)
    P.i("dve", "tensor_reduce", reads=[pf + "big"], writes=[pf + "be"], out=be[:, 0:NB], in_=b3, axis=AX.X, op=ALU.add)
    P.i("dve", "tensor_scalar", reads=[pf + "be"], writes=[pf + "be"], out=be[:, 0:NB], in0=be[:, 0:NB], scalar1=31.0, scalar2=None, op0=ALU.min)
    same = P.sb(pf + "same", [128, 104])
    P.i("dve", "memset", writes=[pf + "same"], ap=same[:, 0:1], constant=0.0)
    P.i("dve", "tensor_tensor", reads=[pf + "be"], writes=[pf + "same"], out=same[:, 1:NB], in0=be[:, 1:NB], in1=be[:, 0:NB - 1], op=ALU.is_equal)
    P.i("dve", "memset", writes=[pf + "same"], ap=same[:, NB // 2:NB // 2 + 1], constant=0.0)
    P.i("dve", "tensor_scalar", reads=[pf + "be", pf + "pidx"], writes=[pf + "be"], out=be[:, 0:NB], in0=be[:, 0:NB], scalar1=128.0, scalar2=pidx[:, 0:1], op0=ALU.mult, op1=ALU.add)
    P.i("dve", "scalar_tensor_tensor", reads=[pf + "be", pf + "same"], writes=[pf + "be"], out=be[:, 0:NB], in0=same[:, 0:NB], scalar=1048576.0, in1=be[:, 0:NB], op0=ALU.mult, op1=ALU.add)
    bei = P.sb(pf + "bei", [128, 104], I32)
    P.i("dve", "tensor_copy", reads=[pf + "be"], writes=[pf + "bei"], out=bei[:, 0:NB], in_=be[:, 0:NB])
    P.d(IDXW[:, :], bei[:, 0:NB], reads=[pf + "bei"], writes=[P.u("IDXW%d" % l)])
    dsf = P.sb(pf + "dsf", [128, ntf, 2])
    pos_r = Rot(P, pf + "pos", [128, 2, 32], n=2)
    for ti in range(ntf):
        pos, posk = pos_r.next()
        P.i("dve", "tensor_tensor", reads=[pf + "RK/%d" % ti, QK], writes=[posk + "/p"], out=pos[:, 0, :], in0=RK[:, ti, :], in1=q[:, 3, :], op=ALU.add)
        for k in range(2):
            P.i("dve", "tensor_tensor", reads=[posk + "/p", pf + "OH/%d_%d" % (ti, k)], writes=[posk + "/t"], out=pos[:, 1, :], in0=pos[:, 0, :], in1=OH[:, ti, k, :], op=ALU.mult)
            P.i("dve", "tensor_reduce", reads=[posk + "/t"], writes=[pf + "dsf/%d_%d" % (ti, k)], out=dsf[:, ti, k:k + 1], in_=pos[:, 1, :], axis=AX.X, op=ALU.add)
    dsi = P.sb(pf + "dsi", [128, ntf * 2], I32)
    P.i("dve", "tensor_copy", reads=[pf + "dsf"], writes=[pf + "dsi"], out=dsi[:], in_=dsf[:].rearrange("p t k -> p (t k)"))
    P.d(DEST[:, :], dsi[:], reads=[pf + "dsi"], writes=[P.u("DEST%d" % l)])
    P.d(WWd[:, :], WW[:].rearrange("p t k -> p (t k)"), reads=[pf + "WW"], writes=[P.u("WWd%d" % l)])
    h2_r = Rot(P, pf + "h2s", [128, D], n=3)
    ti = 0
    for (c0, cn) in chunks_:
        for i in range(cn // 128):
            t0 = c0 + i * 128
            h2, h2k = h2_r.next()
            P.d(h2[:], H2[t0:t0 + 128, :], reads=["H2_%d" % l], writes=[h2k])
            for k in range(2):
                P.dma(lambda e, h2=h2, col=ti * 2 + k: e.indirect_dma_start(out=BUF[:, :], out_offset=bass.IndirectOffsetOnAxis(ap=dsi[:, col:col + 1], axis=0), in_=h2[:], in_offset=None),
                      reads=[h2k, pf + "dsi", "BUFz%d" % l], writes=[P.u("BUF%d" % l)], q="pool")
            ti += 1


def phase_g(P, Dm, l, tok_chunks, final):
    pf = "g%d_" % l
    X1 = Dm.get("X1", [T, D])
    H2T = Dm.get("H2T", [D, T])
    WTd = Dm.get("WTd", [32, T])
    WG = Dm.get("moe_g%d" % l, [32, 128, 8, 512], kind="ExternalInput")
    WU = Dm.get("moe_u%d" % l, [32, 128, 8, 512], kind="ExternalInput")
    WD = Dm.get("moe_d%d" % l, [32, 128, 4, 1024], kind="ExternalInput")
    modrow = Dm.get("modrow%d" % l, [2, 6144])
    mk = "modrow%d" % l
    if final:
        fg_d = Dm.get("final_g", [1, D], kind="ExternalInput")
        OUT = Dm.get("out", [L, D], kind="ExternalOutput")
        fg = P.sb(pf + "fg", [128, D])
        load_bcast(P, fg[:], pf + "fg", fg_d[0:1, :])
        epsc = P.sb(pf + "eps", [128, 1])
        P.i("dve", "memset", writes=[pf + "eps"], ap=epsc[:], constant=EPS)
        junk = P.sb(pf + "junk", [128, D])
    else:
        XN = Dm.get("XN%d" % l, [T, D])
    G2g = [P.sb(pf + "g2%d" % r, [128, D]) for r in range(2)]
    for r in range(2):
        load_bcast(P, G2g[r][:], pf + "g2%d" % r, modrow[r:r + 1, 5120:6144], reads=[mk])
    hT_r = Rot(P, pf + "hT", [128, 8, 512], F32R, n=2)
    acc_r = Rot(P, pf + "acc", [128, 4, 1024], n=1)
    wg_r = Rot(P, pf + "wg", [128, 8, 512], F32R, n=2)
    wu_r = Rot(P, pf + "wu", [128, 8, 512], F32R, n=2)
    wd_r = Rot(P, pf + "wd", [128, 4, 1024], F32R, n=2)
    wb_r = Rot(P, pf + "wb", [128, 512], n=2)
    pg_r = Rot(P, pf + "pg", [128, 512], n=2, psum=True)
    pu_r = Rot(P, pf + "pu", [128, 512], n=2, psum=True)
    pd_r = Rot(P, pf + "pd", [128, 512], n=2, psum=True)
    sg_r = Rot(P, pf + "sg", [128, 512], n=2)
    hu_r = Rot(P, pf + "hu", [128, 512], n=2)
    hid_r = Rot(P, pf + "hid", [128, 4, 512], F32R, n=2)
    xt_r = Rot(P, pf + "xt", [128, D], n=2)
    st_r = Rot(P, pf + "st", [128, 8], n=2)
    for (c0, cn) in tok_chunks:
        nt = cn // 128
        hT, hTk = hT_r.next()
        P.d(hT[:, :, 0:cn], H2T[:, c0:c0 + cn].rearrange("(k p) t -> p k t", p=128), reads=["H2T"], writes=[hTk], q="pool")
        acc, acck = acc_r.next()
        for e in range(32):
            wg, wgk = wg_r.next()
            wu, wuk = wu_r.next()
            wd, wdk = wd_r.next()
            P.d(wg[:], WG[e], writes=[wgk], q="pool")
            P.d(wu[:], WU[e], writes=[wuk], q="pool")
            P.d(wd[:], WD[e], writes=[wdk], q="pool")
            wb, wbk = wb_r.next()
            P.d(wb[:, 0:cn], WTd[e:e + 1, c0:c0 + cn].to_broadcast([128, cn]), reads=["WTd"], writes=[wbk])
            hid, hidk = hid_r.next()
            for hc in range(4):
                pg, pgk = pg_r.next()
                pu, puk = pu_r.next()
                for k in range(8):
                    P.i("pe", "matmul", reads=[wgk, hTk], writes=[pgk], out=pg[:, 0:cn], lhsT=wg[:, k, hc * 128:(hc + 1) * 128], rhs=hT[:, k, 0:cn], start=(k == 0), stop=(k == 7))
                for k in range(8):
                    P.i("pe", "matmul", reads=[wuk, hTk], writes=[puk], out=pu[:, 0:cn], lhsT=wu[:, k, hc * 128:(hc + 1) * 128], rhs=hT[:, k, 0:cn], start=(k == 0), stop=(k == 7))
                sg, sgk = sg_r.next()
                P.i("act", "activation", reads=[pgk], writes=[sgk], out=sg[:, 0:cn], in_=pg[:, 0:cn], func=AF.Silu)
                hu, huk = hu_r.next()
                P.i("dve", "tensor_tensor", reads=[sgk, puk], writes=[huk], out=hu[:, 0:cn], in0=sg[:, 0:cn], in1=pu[:, 0:cn], op=ALU.mult)
                P.i("pool", "tensor_tensor", reads=[huk, wbk], writes=[hidk + "/%d" % hc], out=hid[:, hc, 0:cn], in0=hu[:, 0:cn], in1=wb[:, 0:cn], op=ALU.mult)
            hkeys = [hidk + "/%d" % hc for hc in range(4)]
            for tt in range(nt):
                for hf in range(2):
                    pd, pdk = pd_r.next()
                    for hc in range(4):
                        P.i("pe", "matmul", reads=hkeys + [wdk], writes=[pdk], out=pd[:], lhsT=hid[:, hc, tt * 128:(tt + 1) * 128], rhs=wd[:, hc, hf * 512:(hf + 1) * 512],
                            start=(hc == 0), stop=(hc == 3))
                    ak = acck + "/%d_%d" % (tt, hf)
                    if e == 0:
                        P.i("dve", "tensor_copy", reads=[pdk], writes=[ak], out=acc[:, tt, hf * 512:(hf + 1) * 512], in_=pd[:])
                    else:
                        P.i("dve", "tensor_tensor", reads=[pdk, ak], writes=[ak], out=acc[:, tt, hf * 512:(hf + 1) * 512], in0=acc[:, tt, hf * 512:(hf + 1) * 512], in1=pd[:], op=ALU.add)
        for tt in range(nt):
            t0 = c0 + tt * 128
            r = 1 if t0 < LC else 0
            aks = [acck + "/%d_%d" % (tt, hf) for hf in range(2)]
            xt, xk = xt_r.next()
            P.d(xt[:], X1[t0:t0 + 128, :], reads=["X1"], writes=[xk])
            P.i("pool", "tensor_tensor", reads=aks + [pf + "g2%d" % r], writes=aks, out=acc[:, tt, :], in0=acc[:, tt, :], in1=G2g[r][:], op=ALU.mult)
            P.i("dve", "tensor_tensor", reads=aks + [xk], writes=[xk], out=xt[:], in0=xt[:], in1=acc[:, tt, :], op=ALU.add)
            if not final:
                P.d(XN[t0:t0 + 128, :], xt[:], reads=[xk], writes=[P.u("XN%d" % l)])
            else:
                st, sk = st_r.next()
                P.i("act", "activation", reads=[xk], writes=[pf + "junk", sk], out=junk[:], in_=xt[:], func=AF.Square, accum_out=st[:, 0:1])
                P.i("act", "activation", reads=[sk, pf + "eps"], writes=[sk], out=st[:, 1:2], in_=st[:, 0:1], func=AF.Ln, bias=epsc[:], scale=1.0 / D)
                P.i("act", "activation", reads=[sk], writes=[sk], out=st[:, 2:3], in_=st[:, 1:2], func=AF.Exp, scale=-0.5)
                P.i("dve", "scalar_tensor_tensor", reads=[xk, sk, pf + "fg"], writes=[xk], out=xt[:], in0=xt[:], scalar=st[:, 2:3], in1=fg[:], op0=ALU.mult, op1=ALU.mult)
                P.d(OUT[t0 - LC:t0 - LC + 128, :], xt[:], reads=[xk], writes=[P.u("out")], final=True)


def phase_b(P, Dm, l):
    pf = "b%d_" % l
    FMT = Dm.get("FMT", [1024, T])
    CAT = Dm.get("CAT", [1024, T])
    Bd = Dm.get("s5B%d" % l, [2, 2, 8, 128, 128], kind="ExternalInput")
    Cd = Dm.get("s5C%d" % l, [2, 2, 8, 128, 128], kind="ExternalInput")
    lam_d = Dm.get("s5lam%d" % l, [128, 3, 16], kind="ExternalInput")
    dsk_d = Dm.get("s5d%d" % l, [128, 2], kind="ExternalInput")
    gw_d = Dm.get("gluw%d" % l, [128, 2, 256], kind="ExternalInput")
    gb_d = Dm.get("glub%d" % l, [128, 2], kind="ExternalInput")
    NMAX = 512
    lam = P.sb(pf + "lam", [128, 3, 16])
    P.d(lam[:], lam_d[:, :, :], writes=[pf + "lam"])
    dsk = P.sb(pf + "dsk", [128, 2]); P.d(dsk[:], dsk_d[:, :], writes=[pf + "dsk"])
    gb = P.sb(pf + "gb", [128, 2]); P.d(gb[:], gb_d[:, :], writes=[pf + "gb"])
    gw = P.sb(pf + "gw", [128, 2, 256], F32R); P.d(gw[:], gw_d[:, :, :], writes=[pf + "gw"], q="pool")
    Bb = P.sb(pf + "Bb", [128, 16, 128], F32R)
    Cb = P.sb(pf + "Cb", [128, 16, 128], F32R)
    sc = P.sb(pf + "sc", [128, 24, 16])
    K_ = pf + "sc"
    LR, LI, LDT = lam[:, 0, :], lam[:, 1, :], lam[:, 2, :]
    (DT, RM, TH, C_, S_, T1, T2, T3, ARE, AIM, DEN, AM1, FRE, FIM, HP) = range(15)

    def S(i):
        return sc[:, i, :]

    def tt(o, a, b, op, eng="dve"):
        P.i(eng, "tensor_tensor", reads=[K_, pf + "lam"], writes=[K_], out=o, in0=a, in1=b, op=op)

    hpi = P.sb(pf + "hpi", [128, 1])
    P.i("dve", "memset", writes=[pf + "hpi"], ap=hpi[:], constant=float(np.pi / 2))
    P.i("act", "activation", reads=[pf + "lam"], writes=[K_], out=S(DT), in_=LDT, func=AF.Exp)
    tt(S(RM), LR, S(DT), ALU.mult)
    P.i("act", "activation", reads=[K_], writes=[K_], out=S(RM), in_=S(RM), func=AF.Exp)
    tt(S(TH), LI, S(DT), ALU.mult)
    P.i("act", "activation", reads=[K_], writes=[K_], out=S(S_), in_=S(TH), func=AF.Sin, scale=1.0 / 32)
    P.i("act", "activation", reads=[K_, pf + "hpi"], writes=[K_], out=S(C_), in_=S(TH), func=AF.Sin, scale=1.0 / 32, bias=hpi[:])
    for _ in range(5):
        tt(S(T1), S(C_), S(C_), ALU.mult)
        tt(S(T2), S(S_), S(S_), ALU.mult)
        tt(S(T3), S(C_), S(S_), ALU.mult)
        tt(S(C_), S(T1), S(T2), ALU.subtract)
        tt(S(S_), S(T3), S(T3), ALU.add)
    tt(S(ARE), S(RM), S(C_), ALU.mult)
    tt(S(AIM), S(RM), S(S_), ALU.mult)
    tt(S(T1), LR, LR, ALU.mult)
    tt(S(T2), LI, LI, ALU.mult)
    tt(S(DEN), S(T1), S(T2), ALU.add)
    P.i("dve", "reciprocal", reads=[K_], writes=[K_], out=S(DEN), in_=S(DEN))
    P.i("dve", "tensor_scalar", reads=[K_], writes=[K_], out=S(AM1), in0=S(ARE), scalar1=-1.0, scalar2=None, op0=ALU.add)
    tt(S(T1), S(AM1), LR, ALU.mult)
    tt(S(T2), S(AIM), LI, ALU.mult)
    tt(S(T1), S(T1), S(T2), ALU.add)
    tt(S(FRE), S(T1), S(DEN), ALU.mult)
    tt(S(T1), S(AIM), LR, ALU.mult)
    tt(S(T2), S(AM1), LI, ALU.mult)
    tt(S(T1), S(T1), S(T2), ALU.subtract)
    tt(S(FIM), S(T1), S(DEN), ALU.mult)
    Ep = P.sb(pf + "Ep", [128, 2, 8, NMAX])
    Tm = P.sb(pf + "Tm", [128, 2, 8, NMAX])
    pw = P.sb(pf + "pw", [128, 4, 8])
    tb = P.sb(pf + "tb", [128, 2, 8, NMAX // 2])
    carry = P.sb(pf + "carry", [128, 16, 2])
    P.i("dve", "memset", writes=[pf + "carry"], ap=carry[:], constant=0.0)
    yacc = P.sb(pf + "yacc", [128, 2, T])
    uc_r = Rot(P, pf + "uc", [128, 2, NMAX], F32R, n=2)
    pb_r = Rot(P, pf + "pb", [128, 512], n=4, psum=True)
    py_r = Rot(P, pf + "py", [128, 512], n=2, psum=True)
    br_r = Rot(P, pf + "br", [128, 2, NMAX], n=2)
    t_r = Rot(P, pf + "t", [128, 4, NMAX], n=2)
    v_r = Rot(P, pf + "v", [128, 2, NMAX], n=2)
    g_r = Rot(P, pf + "g", [128, 2, NMAX], n=2)
    h_r = Rot(P, pf + "h", [128, 2, NMAX], F32R, n=2)
    TK, EK = pf + "Tm", pf + "Ep"
    for d in range(2):
        dsl = slice(d * 8, d * 8 + 8)
        P.d(Bb[:], Bd[d].rearrange("c j k m -> k (c j) m"), writes=[pf + "Bb"], q="pool")
        P.d(Cb[:], Cd[d].rearrange("c j k m -> k (c j) m"), writes=[pf + "Cb"], q="pool")
        P.i("act", "activation", reads=[pf + "Cb"], writes=[pf + "Cb"], out=Cb[:, 8:16, :], in_=Cb[:, 8:16, :].bitcast(F32), func=AF.Copy, scale=-1.0)
        P.i("dve", "tensor_copy", reads=[K_], writes=[EK], out=Ep[:, 0, :, 0:1], in_=S(C_)[:, dsl].unsqueeze(2))
        P.i("dve", "tensor_copy", reads=[K_], writes=[EK], out=Ep[:, 1, :, 0:1], in_=S(S_)[:, dsl].unsqueeze(2))
        P.i("dve", "tensor_copy", reads=[K_], writes=[pf + "pw"], out=pw[:, 0, :], in_=S(C_)[:, dsl])
        P.i("dve", "tensor_copy", reads=[K_], writes=[pf + "pw"], out=pw[:, 1, :], in_=S(S_)[:, dsl])
        n = 1
        while n < NMAX:
            cn_b = pw[:, 0, :].unsqueeze(2).to_broadcast([128, 8, n])
            sn_b = pw[:, 1, :].unsqueeze(2).to_broadcast([128, 8, n])
            ire, iim = Ep[:, 0, :, 0:n], Ep[:, 1, :, 0:n]
            ore, oim = Ep[:, 0, :, n:2 * n], Ep[:, 1, :, n:2 * n]
            P.i("dve", "tensor_tensor", reads=[EK, pf + "pw"], writes=[pf + "tb"], out=tb[:, 0, :, 0:n], in0=iim, in1=sn_b, op=ALU.mult)
            P.i("pool", "tensor_tensor", reads=[EK, pf + "pw"], writes=[pf + "tb2"], out=tb[:, 1, :, 0:n], in0=iim, in1=cn_b, op=ALU.mult)
            P.i("dve", "tensor_tensor", reads=[EK, pf + "pw"], writes=[EK + "/a"], out=ore, in0=ire, in1=cn_b, op=ALU.mult)
            P.i("pool", "tensor_tensor", reads=[EK, pf + "pw"], writes=[EK + "/b"], out=oim, in0=ire, in1=sn_b, op=ALU.mult)
            P.i("dve", "tensor_tensor", reads=[EK + "/a", pf + "tb"], writes=[EK + "/a"], out=ore, in0=ore, in1=tb[:, 0, :, 0:n], op=ALU.subtract)
            P.i("pool", "tensor_tensor", reads=[EK + "/b", pf + "tb2"], writes=[EK + "/b"], out=oim, in0=oim, in1=tb[:, 1, :, 0:n], op=ALU.add)
            P.i("dve", "tensor_tensor", reads=[pf + "pw"], writes=[pf + "pw"], out=pw[:, 2, :], in0=pw[:, 0, :], in1=pw[:, 1, :], op=ALU.mult)
            P.i("dve", "tensor_tensor", reads=[pf + "pw"], writes=[pf + "pw"], out=pw[:, 0, :], in0=pw[:, 0, :], in1=pw[:, 0, :], op=ALU.mult)
            P.i("dve", "tensor_tensor", reads=[pf + "pw"], writes=[pf + "pw"], out=pw[:, 3, :], in0=pw[:, 1, :], in1=pw[:, 1, :], op=ALU.mult)
            P.i("dve", "tensor_tensor", reads=[pf + "pw"], writes=[pf + "pw"], out=pw[:, 0, :], in0=pw[:, 0, :], in1=pw[:, 3, :], op=ALU.subtract)
            P.i("dve", "tensor_tensor", reads=[pf + "pw"], writes=[pf + "pw"], out=pw[:, 1, :], in0=pw[:, 2, :], in1=pw[:, 2, :], op=ALU.add)
            n *= 2
        for j in range(8):
            fr = S(FRE)[:, d * 8 + j:d * 8 + j + 1]
            fi = S(FIM)[:, d * 8 + j:d * 8 + j + 1]
            t, tk = t_r.next()
            P.i("dve", "tensor_scalar", reads=[EK, K_], writes=[tk + "/0"], out=t[:, 0, :], in0=Ep[:, 1, j, :], scalar1=fi, scalar2=None, op0=ALU.mult)
            P.i("dve", "scalar_tensor_tensor", reads=[EK, K_, tk + "/0"], writes=[TK + "/a%d" % j], out=Tm[:, 0, j, :], in0=Ep[:, 0, j, :], scalar=fr, in1=t[:, 0, :], op0=ALU.mult, op1=ALU.add)
            P.i("dve", "tensor_scalar", reads=[EK, K_], writes=[tk + "/1"], out=t[:, 1, :], in0=Ep[:, 1, j, :], scalar1=fr, scalar2=None, op0=ALU.mult)
            P.i("dve", "scalar_tensor_tensor", reads=[EK, K_, tk + "/1"], writes=[TK + "/b%d" % j], out=Tm[:, 1, j, :], in0=Ep[:, 0, j, :], scalar=fi, in1=t[:, 1, :], op0=ALU.mult, op1=ALU.subtract)
        order = CHUNKS if d == 0 else [CHUNKS[0]] + CHUNKS[:0:-1]
        rev = (d == 1)

        def R(ap):
            return ap[:, ::-1] if rev else ap

        for (c0, N) in order:
            uc, uck = uc_r.next()
            P.d(uc[:, :, 0:N], FMT[0:256, c0:c0 + N].rearrange("(oc p) t -> p oc t", p=128), reads=["FMT"], writes=[uck], q="pool")
            pys = [py_r.next() for _ in range(2)]
            def s5_iter(j):
                    oc = j // 4
                    dj = d * 8 + j
                    pbr, pbrk = pb_r.next()
                    pbi, pbik = pb_r.next()
                    P.i("pe", "matmul", reads=[pf + "Bb", uck], writes=[pbrk], out=pbr[:, 0:N], lhsT=Bb[:, j, :], rhs=uc[:, oc, 0:N], start=True, stop=True)
                    P.i("pe", "matmul", reads=[pf + "Bb", uck], writes=[pbik], out=pbi[:, 0:N], lhsT=Bb[:, 8 + j, :], rhs=uc[:, oc, 0:N], start=True, stop=True)
                    yield
                    br, brk = br_r.next()
                    P.i("act", "activation", reads=[pbrk], writes=[brk + "/0"], out=br[:, 0, 0:N], in_=R(pbr[:, 0:N]), func=AF.Copy)
                    P.i("act", "activation", reads=[pbik], writes=[brk + "/1"], out=br[:, 1, 0:N], in_=R(pbi[:, 0:N]), func=AF.Copy)
                    yield
                    t, tk = t_r.next()
                    v, vk = v_r.next()
                    b0, b1 = br[:, 0, 0:N], br[:, 1, 0:N]
                    tmr, tmi = Tm[:, 0, j, 0:N], Tm[:, 1, j, 0:N]
                    P.i("dve", "tensor_tensor", reads=[brk + "/0", TK], writes=[tk + "/0"], out=t[:, 0, 0:N], in0=tmr, in1=b0, op=ALU.mult)
                    P.i(S5_ENG2, "tensor_tensor", reads=[brk + "/1", TK], writes=[tk + "/1"], out=t[:, 1, 0:N], in0=tmi, in1=b1, op=ALU.mult)
                    P.i(S5_ENG2, "tensor_tensor", reads=[brk + "/1", TK], writes=[tk + "/2"], out=t[:, 2, 0:N], in0=tmr, in1=b1, op=ALU.mult)
                    P.i(S5_ENG2, "tensor_tensor", reads=[brk + "/0", TK], writes=[tk + "/3"], out=t[:, 3, 0:N], in0=tmi, in1=b0, op=ALU.mult)
                    P.i("dve", "tensor_tensor", reads=[tk + "/0", tk + "/1"], writes=[vk + "/0"], out=v[:, 0, 0:N], in0=t[:, 0, 0:N], in1=t[:, 1, 0:N], op=ALU.subtract)
                    P.i("dve", "tensor_tensor", reads=[tk + "/2", tk + "/3"], writes=[vk + "/1"], out=v[:, 1, 0:N], in0=t[:, 2, 0:N], in1=t[:, 3, 0:N], op=ALU.add)
                    yield
                    g, gk = g_r.next()
                    for c in range(2):
                        P.i("dve", "tensor_tensor_scan", reads=[vk + "/%d" % c, K_, pf + "carry"], writes=[gk + "/%d" % c], out=g[:, c, 0:N], data0=S(RM)[:, dj:dj + 1].to_broadcast([128, N]), data1=v[:, c, 0:N],
                            initial=carry[:, dj, c:c + 1], op0=ALU.mult, op1=ALU.add)
                    yield
                    h, hk = h_r.next()
                    t, tk = t_r.next()
                    epr, epi = Ep[:, 0, j, 0:N], Ep[:, 1, j, 0:N]
                    g0, g1 = g[:, 0, 0:N], g[:, 1, 0:N]
                    P.i("dve", "tensor_tensor", reads=[gk + "/0", EK], writes=[tk + "/0"], out=t[:, 0, 0:N], in0=epr, in1=g0, op=ALU.mult)
                    P.i(S5_ENG2, "tensor_tensor", reads=[gk + "/1", EK], writes=[tk + "/1"], out=t[:, 1, 0:N], in0=epi, in1=g1, op=ALU.mult)
                    P.i(S5_ENG2, "tensor_tensor", reads=[gk + "/1", EK], writes=[tk + "/2"], out=t[:, 2, 0:N], in0=epr, in1=g1, op=ALU.mult)
                    P.i("dve", "tensor_tensor", reads=[gk + "/0", EK], writes=[tk + "/3"], out=t[:, 3, 0:N], in0=epi, in1=g0, op=ALU.mult)
                    P.i("dve", "tensor_tensor", reads=[tk + "/0", tk + "/1"], writes=[hk + "/0"], out=h[:, 0, 0:N], in0=t[:, 0, 0:N], in1=t[:, 1, 0:N], op=ALU.subtract)
                    P.i("dve", "tensor_tensor", reads=[tk + "/2", tk + "/3"], writes=[hk + "/1"], out=h[:, 1, 0:N], in0=t[:, 2, 0:N], in1=t[:, 3, 0:N], op=ALU.add)
                    yield
                    P.i("act", "activation", reads=[hk], writes=[pf + "carry"], out=carry[:, dj, :], in_=h[:].bitcast(F32)[:, :, N - 1], func=AF.Copy)
                    py, pyk = pys[oc]
                    P.i("pe", "matmul", reads=[pf + "Cb", hk + "/0"], writes=[pyk], out=py[:, 0:N], lhsT=Cb[:, j, :], rhs=h[:, 0, 0:N], start=(j % 4 == 0), stop=False)
                    P.i("pe", "matmul", reads=[pf + "Cb", hk + "/1"], writes=[pyk], out=py[:, 0:N], lhsT=Cb[:, 8 + j, :], rhs=h[:, 1, 0:N], start=False, stop=(j % 4 == 3))
            for jp in range(0, 8, 2):
                gens = [s5_iter(jp), s5_iter(jp + 1)]
                alive = True
                while alive:
                    alive = False
                    for g_ in gens:
                        try:
                            next(g_)
                            alive = True
                        except StopIteration:
                            pass
            for oc in range(2):
                py, pyk = pys[oc]
                ya = yacc[:, oc, c0:c0 + N]
                yk = pf + "yacc/%d_%d" % (oc, c0)
                if d == 0:
                    P.i("dve", "scalar_tensor_tensor", reads=[uck, pf + "dsk", pyk], writes=[yk], out=ya, in0=uc[:, oc, 0:N].bitcast(F32), scalar=dsk[:, oc:oc + 1], in1=py[:, 0:N], op0=ALU.mult, op1=ALU.add)
                else:
                    P.i("dve", "tensor_tensor", reads=[yk, pyk], writes=[yk], out=ya, in0=ya, in1=R(py[:, 0:N]), op=ALU.add)
    e_r = Rot(P, pf + "e", [128, 3, NMAX], n=1)
    gT_r = Rot(P, pf + "gT", [128, 2, NMAX], F32R, n=1)
    pq_r = Rot(P, pf + "pq", [128, 512], n=2, psum=True)
    a_r = Rot(P, pf + "a", [128, NMAX], n=2)
    for (c0, N) in CHUNKS:
        gT, gTk = gT_r.next()
        for oc in range(2):
            y = yacc[:, oc, c0:c0 + N]
            yk = pf + "yacc/%d_%d" % (oc, c0)
            ee, ek = e_r.next()
            P.i("pool", "tensor_tensor", reads=[yk], writes=[ek + "/0"], out=ee[:, 0, 0:N], in0=y, in1=y, op=ALU.mult)
            P.i("dve", "tensor_scalar", reads=[ek + "/0"], writes=[ek + "/0"], out=ee[:, 0, 0:N], in0=ee[:, 0, 0:N], scalar1=0.044715, scalar2=1.0, op0=ALU.mult, op1=ALU.add)
            P.i("pool", "tensor_tensor", reads=[ek + "/0", yk], writes=[ek + "/1"], out=ee[:, 1, 0:N], in0=ee[:, 0, 0:N], in1=y, op=ALU.mult)
            P.i("act", "activation", reads=[ek + "/1"], writes=[ek + "/2"], out=ee[:, 2, 0:N], in_=ee[:, 1, 0:N], func=AF.Sigmoid, scale=1.5957691216057308)
            P.i("dve", "tensor_tensor", reads=[ek + "/2", yk], writes=[gTk + "/%d" % oc], out=gT[:, oc, 0:N], in0=ee[:, 2, 0:N], in1=y, op=ALU.mult)
        for oc2 in range(2):
            pq, pqk = pq_r.next()
            for oc in range(2):
                P.i("pe", "matmul", reads=[gTk, pf + "gw"], writes=[pqk], out=pq[:, 0:N], lhsT=gw[:, oc, oc2 * 128:(oc2 + 1) * 128], rhs=gT[:, oc, 0:N], start=(oc == 0), stop=(oc == 1))
            a, ak = a_r.next()
            P.i("act", "activation", reads=[pqk, pf + "gb"], writes=[ak], out=a[:, 0:N], in_=pq[:, 0:N], func=AF.Sigmoid, bias=gb[:, oc2:oc2 + 1])
            P.i("dve", "tensor_tensor", reads=[ak, gTk], writes=[ak], out=a[:, 0:N], in0=a[:, 0:N], in1=gT[:, oc2, 0:N].bitcast(F32), op=ALU.mult)
            P.d(CAT[oc2 * 128:(oc2 + 1) * 128, c0:c0 + N], a[:, 0:N], reads=[ak], writes=[P.u("CAT")])


def phase_d1(P, Dm, l):
    pf = "d%d_" % l
    FMT = Dm.get("FMT", [1024, T])
    XBCT = Dm.get("XBCT", [768, T])
    XBtok = Dm.get("XBtok", [T, 512])
    cw_d = Dm.get("convw%d" % l, [128, 6, 4], kind="ExternalInput")
    ident_d = Dm.get("ident", [128, 128], kind="ExternalInput")
    ident = P.sb(pf + "ident", [128, 128]); P.d(ident[:], ident_d[:, :], writes=[pf + "ident"])
    cw = P.sb(pf + "cw", [128, 6, 4]); P.d(cw[:], cw_d[:, :, :], writes=[pf + "cw"])
    xi_r = Rot(P, pf + "xi", [128, 6, 514], n=2)
    ac_r = Rot(P, pf + "ac", [128, 512], n=2)
    co_r = Rot(P, pf + "co", [128, 6, 512], n=2)
    tp_r = Rot(P, pf + "tp", [128, 512], n=2, psum=True)
    tk_r = Rot(P, pf + "tk", [128, 512], n=2)
    for (c0, N) in CHUNKS:
        xi, xik = xi_r.next()
        lz = c0 in (0, LC)
        rz = (c0 + N) in (LC, T)
        lo = c0 - (0 if lz else 1)
        hi = c0 + N + (0 if rz else 1)
        if lz:
            P.i("dve", "memset", writes=[xik + "/l"], ap=xi[:, :, 0:1], constant=0.0)
        if rz:
            P.i("dve", "memset", writes=[xik + "/r"], ap=xi[:, :, N + 1:N + 2], constant=0.0)
        P.d(xi[:, :, (1 if lz else 0):(1 if lz else 0) + hi - lo], FMT[256:1024, lo:hi].rearrange("(r p) t -> p r t", p=128), reads=["FMT"], writes=[xik + "/m"])
        co, cok = co_r.next()
        for rc in range(6):
            ac, ack = ac_r.next()
            P.i("dve", "tensor_scalar", reads=[xik, pf + "cw"], writes=[ack], out=ac[:, 0:N], in0=xi[:, rc, 1:N + 1], scalar1=cw[:, rc, 1:2], scalar2=None, op0=ALU.mult)
            P.i("dve", "scalar_tensor_tensor", reads=[xik, pf + "cw", ack], writes=[ack], out=ac[:, 0:N], in0=xi[:, rc, 0:N], scalar=cw[:, rc, 0:1], in1=ac[:, 0:N], op0=ALU.mult, op1=ALU.add)
            P.i("dve", "scalar_tensor_tensor", reads=[xik, pf + "cw", ack], writes=[ack], out=ac[:, 0:N], in0=xi[:, rc, 2:N + 2], scalar=cw[:, rc, 2:3], in1=ac[:, 0:N], op0=ALU.mult, op1=ALU.add)
            P.i("act", "activation", reads=[ack, pf + "cw"], writes=[cok + "/%d" % rc], out=co[:, rc, 0:N], in_=ac[:, 0:N], func=AF.Silu, bias=cw[:, rc, 3:4])
        P.d(XBCT[:, c0:c0 + N].rearrange("(r p) t -> p r t", p=128), co[:, :, 0:N], reads=[cok], writes=[P.u("XBCT")])
        for i in range(N // 128):
            tp, tpk = tp_r.next()
            for rc in range(4):
                P.i("pe", "transpose", reads=[cok + "/%d" % rc, pf + "ident"], writes=[tpk], noinc=(rc != 3), out=tp[:, rc * 128:(rc + 1) * 128], in_=co[:, rc, i * 128:(i + 1) * 128], identity=ident[:])
            tk, tkk = tk_r.next()
            P.i("act", "activation", reads=[tpk], writes=[tkk], out=tk[:], in_=tp[:], func=AF.Copy)
            P.d(XBtok[c0 + i * 128:c0 + (i + 1) * 128, :], tk[:], reads=[tkk], writes=[P.u("XBtok")])


def phase_d2(P, Dm, l):
    pf = "D%d_" % l
    XBCT = Dm.get("XBCT", [768, T])
    XBtok = Dm.get("XBtok", [T, 512])
    TMS = Dm.get("TMS", [T, 520])
    CAT = Dm.get("CAT", [1024, T])
    ident_d = Dm.get("ident", [128, 128], kind="ExternalInput")
    tri_d = Dm.get("tri", [128, 2, 128], kind="ExternalInput")
    alog_d = Dm.get("alog%d" % l, [1, 8], kind="ExternalInput")
    dsk_d = Dm.get("ssdd%d" % l, [1, 4], kind="ExternalInput")
    ng_d = Dm.get("ssdng%d" % l, [1, 256], kind="ExternalInput")
    ident = P.sb(pf + "ident", [128, 128]); P.d(ident[:], ident_d[:, :], writes=[pf + "ident"])
    tri = P.sb(pf + "tri", [128, 2, 128]); P.d(tri[:], tri_d[:, :, :], writes=[pf + "tri"])
    A = P.sb(pf + "A", [128, 8]); load_bcast(P, A[:], pf + "A", alog_d[0:1, :])
    P.i("act", "activation", reads=[pf + "A"], writes=[pf + "A"], out=A[:], in_=A[:], func=AF.Exp)
    P.i("dve", "tensor_scalar", reads=[pf + "A"], writes=[pf + "A"], out=A[:], in0=A[:], scalar1=-1.0, scalar2=None, op0=ALU.mult)
    dsk = P.sb(pf + "dsk", [128, 4]); load_bcast(P, dsk[:], pf + "dsk", dsk_d[0:1, :])
    ng = P.sb(pf + "ng", [128, 256]); load_bcast(P, ng[:], pf + "ng", ng_d[0:1, :])
    epsc = P.sb(pf + "eps", [128, 1]); P.i("dve", "memset", writes=[pf + "eps"], ap=epsc[:], constant=EPS)
    Yacc = [P.sb(pf + "Yacc%d" % d_, [128, NT, 256]) for d_ in range(2)]
    Sst = [P.sb(pf + "S%d" % d_, [128, 4, 64], F32R) for d_ in range(2)]
    bc_r = Rot(P, pf + "bc", [128, 4, 128], F32R, n=4)
    xb_r = Rot(P, pf + "xb", [128, 512], n=4)
    xbr_r = Rot(P, pf + "xbr", [128, 256], F32R, n=4)
    dt_r = Rot(P, pf + "dt", [128, 8], n=4)
    sm_r = Rot(P, pf + "sm", [128, 64], n=4)
    abc_r = Rot(P, pf + "abc", [128, 4, 128], n=4)
    X_r = Rot(P, pf + "X", [128, 4, 64], F32R, n=4)
    Xd_r = Rot(P, pf + "Xd", [128, 4, 64], F32R, n=4)
    pG_r = Rot(P, pf + "pG", [128, 512], n=1, psum=True)
    pR_r = Rot(P, pf + "pR", [128, 512], n=1, psum=True)
    pC_r = Rot(P, pf + "pC", [128, 512], n=1, psum=True)
    pY_r = Rot(P, pf + "pY", [128, 512], n=2, psum=True)
    pS_r = Rot(P, pf + "pS", [128, 512], n=1, psum=True)
    Gm_r = Rot(P, pf + "Gm", [128, 2, 128], n=4)
    df_r = Rot(P, pf + "df", [128, 4, 128], n=4)
    sc_r = Rot(P, pf + "scT", [128, 4, 128], F32R, n=4)
    yo_r = Rot(P, pf + "yo", [128, 4, 64], n=4)
    orders = [list(range(NT)), [1, 0] + list(range(NT - 1, 1, -1))]
    for d in range(2):
        P.i("dve", "memset", writes=[pf + "S%d" % d], ap=Sst[d][:].bitcast(F32), constant=0.0)

    def ssd_iter(d, c):
            t0 = c * 128
            bc, bck = bc_r.next()
            P.d(bc[:], XBCT[256:768, t0:t0 + 128].rearrange("(r p) t -> p r t", p=128), reads=["XBCT"], writes=[bck], q="pool")
            xb, xbk = xb_r.next()
            P.d(xb[:], XBtok[t0:t0 + 128, :], reads=["XBtok"], writes=[xbk])
            xbr, xbrk = xbr_r.next()
            P.i("act", "activation", reads=[xbk], writes=[xbrk], out=xbr[:], in_=xb[:, 256:512], func=AF.Copy)
            dt, dtk = dt_r.next()
            P.d(dt[:], TMS[t0:t0 + 128, 512:520], reads=["TMS"], writes=[dtk])
            sm, smk = sm_r.next()
            P.i("dve", "tensor_tensor", reads=[dtk, pf + "A"], writes=[smk], out=sm[:, 0:4], in0=dt[:, d * 4:d * 4 + 4], in1=A[:, d * 4:d * 4 + 4], op=ALU.mult)
            abc, abck = abc_r.next()
            P.i("dve", "tensor_copy", reads=[smk], writes=[abck], out=abc[:], in_=sm[:, 0:4].unsqueeze(2).to_broadcast([128, 4, 128]))
            X, Xk = X_r.next()
            P.i("pool", "tensor_tensor", reads=[xbk, dtk], writes=[Xk], out=X[:], in0=xb[:, 0:256].rearrange("p (h e) -> p h e", h=4),
                in1=dt[:, d * 4:d * 4 + 4].unsqueeze(2).to_broadcast([128, 4, 64]), op=ALU.mult)
            pG, pGk = pG_r.next()
            for g in range(2):
                P.i("pe", "matmul", reads=[bck], writes=[pGk], out=pG[:, g * 128:(g + 1) * 128], lhsT=bc[:, g, :], rhs=bc[:, 2 + g, :], start=True, stop=True)
            Gm, Gmk = Gm_r.next()
            P.i("dve", "tensor_tensor", reads=[pGk, pf + "tri"], writes=[Gmk], out=Gm[:], in0=pG[:, 0:256].rearrange("p (g l) -> p g l", g=2),
                in1=tri[:, d, :].unsqueeze(1).to_broadcast([128, 2, 128]), op=ALU.mult)
            pC, pCk = pC_r.next()
            P.i("pe", "matmul", reads=[smk, pf + "tri"], writes=[pCk], out=pC[:, 0:4], lhsT=tri[:, d, :], rhs=sm[:, 0:4], start=True, stop=True)
            pR, pRk = pR_r.next()
            for h in range(4):
                P.i("pe", "matmul", reads=[abck, pf + "tri"], writes=[pRk], out=pR[:, h * 128:(h + 1) * 128], lhsT=abc[:, h, :], rhs=tri[:, d, :], start=True, stop=True)
            P.i("dve", "tensor_copy", reads=[pCk], writes=[smk], out=sm[:, 4:8], in_=pC[:, 0:4])
            P.i("act", "activation", reads=[smk], writes=[smk], out=sm[:, 8:12], in_=sm[:, 4:8], func=AF.Exp)
            last = 127 if d == 0 else 0
            P.i("dve", "tensor_copy", reads=[pRk], writes=[smk], out=sm[:, 20:24], in_=pR[:].rearrange("p (h l) -> p h l", h=4)[:, :, last])
            P.i("dve", "tensor_tensor", reads=[smk], writes=[smk], out=sm[:, 12:16], in0=sm[:, 20:24], in1=sm[:, 4:8], op=ALU.subtract)
            P.i("act", "activation", reads=[smk], writes=[smk], out=sm[:, 12:16], in_=sm[:, 12:16], func=AF.Exp)
            P.i("act", "activation", reads=[smk], writes=[smk], out=sm[:, 16:20], in_=sm[:, 20:24], func=AF.Exp)
            Xd, Xdk = Xd_r.next()
            P.i("pool", "tensor_tensor", reads=[Xk, smk], writes=[Xdk], out=Xd[:], in0=X[:].bitcast(F32), in1=sm[:, 12:16].unsqueeze(2).to_broadcast([128, 4, 64]), op=ALU.mult)
            pY, pYk = pY_r.next()
            pS, pSk = pS_r.next()
            df, dfk = df_r.next()
            pR4 = pR[:].rearrange("p (h l) -> p h l", h=4)
            P.i("dve", "tensor_tensor", reads=[pRk, smk], writes=[dfk], out=df[:], in0=pR4, in1=sm[:, 4:8].unsqueeze(2).to_broadcast([128, 4, 128]), op=ALU.subtract)
            P.i("dve", "tensor_scalar", reads=[dfk], writes=[dfk], out=df[:], in0=df[:], scalar1=0.0, scalar2=None, op0=ALU.min)
            P.i("act", "activation", reads=[dfk], writes=[dfk], out=df[:], in_=df[:], func=AF.Exp)
            scT, scTk = sc_r.next()
            P.i("dve", "tensor_tensor", reads=[dfk, Gmk], writes=[scTk], out=scT[:].rearrange("p (g u) l -> p g u l", g=2), in0=df[:].rearrange("p (g u) l -> p g u l", g=2),
                in1=Gm[:].unsqueeze(2).to_broadcast([128, 2, 2, 128]), op=ALU.mult)
            for h in range(4):
                g = h // 2
                P.i("pe", "matmul", reads=[scTk, Xk], writes=[pYk], out=pY[:, h * 128:h * 128 + 64], lhsT=scT[:, h, :], rhs=X[:, h, :], start=True, stop=True)
                P.i("pe", "matmul", reads=[bck, pf + "S%d" % d], writes=[pYk], out=pY[:, h * 128 + 64:h * 128 + 128], lhsT=bc[:, 2 + g, :], rhs=Sst[d][:, h, :], start=True, stop=True)
                P.i("pe", "matmul", reads=[xbrk, Xdk], writes=[pSk], out=pS[:, h * 64:(h + 1) * 64], lhsT=xbr[:, g * 128:(g + 1) * 128], rhs=Xd[:, h, :], start=True, stop=True)
            pY4 = pY[:].rearrange("p (h t e) -> p h t e", h=4, t=2)
            yo, yok = yo_r.next()
            P.i("dve", "tensor_tensor", reads=[pYk, smk], writes=[yok], out=yo[:], in0=pY4[:, :, 1, :], in1=sm[:, 8:12].unsqueeze(2).to_broadcast([128, 4, 64]), op=ALU.mult)
            ya = Yacc[d][:, c, :].rearrange("p (h e) -> p h e", h=4)
            yk = pf + "Yacc%d/%d" % (d, c)
            P.i("dve", "tensor_tensor", reads=[pYk, yok], writes=[yk], out=ya, in0=pY4[:, :, 0, :], in1=yo[:], op=ALU.add)
            S4 = Sst[d][:].bitcast(F32)
            P.i("pool", "tensor_tensor", reads=[pf + "S%d" % d, smk], writes=[pf + "S%d" % d], out=Sst[d][:], in0=S4, in1=sm[:, 16:20].unsqueeze(2).to_broadcast([128, 4, 64]), op=ALU.mult)
            P.i("dve", "tensor_tensor", reads=[pf + "S%d" % d, pSk], writes=[pf + "S%d" % d], out=Sst[d][:], in0=S4, in1=pS[:, 0:256].rearrange("p (h e) -> p h e", h=4), op=ALU.add)
    for ci in range(NT):
        for d in range(2):
            ssd_iter(d, orders[d][ci])
    z_r = Rot(P, pf + "z", [128, 256], n=2)
    yt_r = Rot(P, pf + "yt", [128, 256], n=2)
    jk = P.sb(pf + "jk", [128, 256])
    pT_r = Rot(P, pf + "pT", [128, 512], n=2, psum=True)
    oT_r = Rot(P, pf + "oT", [128, 2, 128], n=2)
    for c in range(NT):
        t0 = c * 128
        xb, xbk = xb_r.next()
        P.d(xb[:], XBtok[t0:t0 + 128, :], reads=["XBtok"], writes=[xbk])
        z, zk = z_r.next()
        P.d(z[:], TMS[t0:t0 + 128, 256:512], reads=["TMS"], writes=[zk])
        yt, ytk = yt_r.next()
        P.i("pool", "tensor_tensor", reads=[xbk, pf + "dsk"], writes=[ytk], out=yt[:].rearrange("p (h e) -> p h e", h=4), in0=xb[:, 0:256].rearrange("p (h e) -> p h e", h=4),
            in1=dsk[:].unsqueeze(2).to_broadcast([128, 4, 64]), op=ALU.mult)
        P.i("dve", "tensor_tensor", reads=[ytk, pf + "Yacc0/%d" % c], writes=[ytk], out=yt[:], in0=yt[:], in1=Yacc[0][:, c, :], op=ALU.add)
        P.i("dve", "tensor_tensor", reads=[ytk, pf + "Yacc1/%d" % c], writes=[ytk], out=yt[:], in0=yt[:], in1=Yacc[1][:, c, :], op=ALU.add)
        P.i("act", "activation", reads=[zk], writes=[zk], out=z[:], in_=z[:], func=AF.Silu)
        P.i("dve", "tensor_tensor", reads=[ytk, zk], writes=[ytk], out=yt[:], in0=yt[:], in1=z[:], op=ALU.mult)
        sm, smk = sm_r.next()
        P.i("act", "activation", reads=[ytk], writes=[pf + "jk", smk], out=jk[:], in_=yt[:], func=AF.Square, accum_out=sm[:, 0:1])
        P.i("act", "activation", reads=[smk, pf + "eps"], writes=[smk], out=sm[:, 1:2], in_=sm[:, 0:1], func=AF.Ln, bias=epsc[:], scale=1.0 / 256)
        P.i("act", "activation", reads=[smk], writes=[smk], out=sm[:, 2:3], in_=sm[:, 1:2], func=AF.Exp, scale=-0.5)
        P.i("dve", "scalar_tensor_tensor", reads=[ytk, smk, pf + "ng"], writes=[ytk], out=yt[:], in0=yt[:], scalar=sm[:, 2:3], in1=ng[:], op0=ALU.mult, op1=ALU.mult)
        pT, pTk = pT_r.next()
        for q in range(2):
            P.i("pe", "transpose", reads=[ytk, pf + "ident"], writes=[pTk], out=pT[:, q * 128:(q + 1) * 128], in_=yt[:, q * 128:(q + 1) * 128], identity=ident[:])
        oT, oTk = oT_r.next()
        P.i("act", "activation", reads=[pTk], writes=[oTk], out=oT[:], in_=pT[:, 0:256].rearrange("p (q t) -> p q t", q=2), func=AF.Copy)
        P.d(CAT[512:768, t0:t0 + 128].rearrange("(q p) t -> p q t", p=128), oT[:], reads=[oTk], writes=[P.u("CAT")])


LAT_CHUNKS = [(256 + 512 * i, 512) for i in range(8)]
ALL_CHUNKS512 = [(512 * i, 512) for i in range(8)] + [(4096, 256)]


def build_program(scopes=False):
    nc = bass.Bass("TRN2", target_bir_lowering=False)
    P = Prog(nc)
    P.scopes = scopes
    Dm = Dram(nc, ext_in=["xin"])
    src = "xin"
    for l in range(2):
        last = (l == 1)
        ctx_out = not last
        for fi, fn in enumerate((lambda: phase_mod(P, Dm, l),
                   lambda: phase_a(P, Dm, l, src),
                   lambda: phase_b(P, Dm, l),
                   lambda: phase_c(P, Dm, l, ctx_out),
                   lambda: phase_d1(P, Dm, l),
                   lambda: phase_d2(P, Dm, l),
                   lambda: phase_e(P, Dm, l, ctx_out),
                   lambda: phase_f2(P, Dm, l, src, tok_chunks=(LAT_CHUNKS if last else None)),
                   lambda: phase_g2(P, Dm, l, (LAT_CHUNKS if last else CHUNKS), last))):
            P.begin_phase("L%d_%s" % (l, "mabcdDefg"[fi]))
            fn()
            P.end_phase()
        src = "XN%d" % l
    P.emit()
    return nc, P, Dm


_CACHE = {}


def kernel(**inputs):
    inp = {k: np.asarray(v) for k, v in inputs.items()}
    if "nc" not in _CACHE:
        _CACHE["nc"] = build_program()
    nc, P, Dm = _CACHE["nc"]
    n_cores = 8
    maps = []
    per_b = {}
    for cidx in range(n_cores):
        b = cidx % 4
        if b not in per_b:
            m = core_inputs(inp, b)
            per_b[b] = {k: v for k, v in m.items() if k in Dm.t}
        maps.append(per_b[b])
    res = run_bass_kernel_spmd(nc, maps, core_ids=list(range(n_cores)))
    out = np.stack([np.asarray(res.results[b]["out"]) for b in range(4)], 0)
    return out.astype(np.float32)
```
